# Optimizing a Trainium2 kernel written in Bass

```python
import jax, jax.numpy as jnp
from jax import lax
import numpy as np

D_MODEL = 1024
BATCH = 4
SEQ = 8192
DEPTH = 4

HEAD_DIM = 64
N_SB_HEADS = 4
N_MOBA_HEADS = 4
N_MLA_HEADS = 4
N_NSA_HEADS = 4
N_MEM_HEADS = 4
MEM_LEN = 256
BRANCH_W = 4 * HEAD_DIM
N_BRANCHES = 5
Q_BLOCK = 128
MOBA_BLOCK = 256
MOBA_TOPK = 3
MLA_Q_RANK = 256
MLA_KV_RANK = 128
MLA_NOPE = 64
MLA_ROPE = 32
MLA_V = 64
ROPE_THETA = 10000.0
NSA_CMP_LEN = 32
NSA_CMP_STRIDE = 16
NSA_SEL_LEN = 64
NSA_TOPN = 16
NSA_WINDOW = 512
NSA_PHI_HIDDEN = 128
N_GROUPS = 4
EXPERTS_PER_GROUP = 4
N_EXPERTS = N_GROUPS * EXPERTS_PER_GROUP
D_EXPERT = 256
TOPK_IN_GROUP = 2
DEEPNORM_ALPHA = (2.0 * DEPTH) ** 0.25
DEEPNORM_BETA = (8.0 * DEPTH) ** -0.25
LN_EPS = 1e-5
RMS_EPS = 1e-6
NEG = -1e30
BIG = 1e30

IN_SIZES = (3 * N_SB_HEADS * HEAD_DIM,
            3 * N_MOBA_HEADS * HEAD_DIM,
            MLA_Q_RANK,
            MLA_KV_RANK,
            MLA_ROPE,
            N_NSA_HEADS * HEAD_DIM,
            6 * HEAD_DIM,
            3 * N_NSA_HEADS,
            N_MEM_HEADS * HEAD_DIM)
IN_TOTAL = sum(IN_SIZES)
IN_SPLITS = tuple(int(c) for c in np.cumsum(IN_SIZES)[:-1])

kernel_name = 'hybrid_gated_sparse_mixers_hmoe'


def layer_norm(x, g, b):
    xf = x.astype(jnp.float32)
    mu = jnp.mean(xf, axis=-1, keepdims=True)
    var = jnp.mean(jnp.square(xf - mu), axis=-1, keepdims=True)
    return ((xf - mu) * lax.rsqrt(var + LN_EPS) * g + b).astype(x.dtype)


def rms_norm(x, g):
    xf = x.astype(jnp.float32)
    return (xf * lax.rsqrt(jnp.mean(jnp.square(xf), axis=-1, keepdims=True) + RMS_EPS) * g).astype(x.dtype)


def heads(x, n):
    b, s, _ = x.shape
    return x.reshape(b, s, n, -1).transpose(0, 2, 1, 3)


def merge_heads(o):
    b, n, s, d = o.shape
    return o.transpose(0, 2, 1, 3).reshape(b, s, n * d)


def alibi_slopes(n):
    return jnp.power(2.0, -8.0 * jnp.arange(1, n + 1, dtype=jnp.float32) / n)


def rope(x, pos):
    half = x.shape[-1] // 2
    freqs = jnp.power(ROPE_THETA, -jnp.arange(half, dtype=jnp.float32) / half)
    ang = pos.astype(jnp.float32)[:, None] * freqs
    cos, sin = jnp.cos(ang), jnp.sin(ang)
    xf = x.astype(jnp.float32)
    x1, x2 = xf[..., :half], xf[..., half:]
    return jnp.concatenate([x1 * cos - x2 * sin, x1 * sin + x2 * cos], axis=-1).astype(x.dtype)


def sweep_query_blocks(block_fn, seq_len):
    out = lax.map(block_fn, jnp.arange(seq_len // Q_BLOCK))
    nb, b, h, q, dv = out.shape
    return out.transpose(1, 2, 0, 3, 4).reshape(b, h, nb * q, dv)


def stick_breaking_attention(q, k, v):
    s_len, d = q.shape[2], q.shape[3]
    scale = d ** -0.5
    kpos = jnp.arange(s_len)

    def block(i):
        q0 = i * Q_BLOCK
        qb = lax.dynamic_slice_in_dim(q, q0, Q_BLOCK, axis=2)
        qpos = q0 + jnp.arange(Q_BLOCK)
        z = jnp.einsum('bhqd,bhkd->bhqk', qb, k).astype(jnp.float32) * scale
        past = kpos[None, :] < qpos[:, None]
        log_keep = jnp.where(past, jax.nn.log_sigmoid(-z), 0.0)
        later = lax.cumsum(log_keep, axis=3, reverse=True) - log_keep
        a = jnp.where(past, jnp.exp(jax.nn.log_sigmoid(z) + later), 0.0)
        return jnp.einsum('bhqk,bhkd->bhqd', a.astype(v.dtype), v)

    return sweep_query_blocks(block, s_len)


def moba_attention(q, k, v, slopes):
    b, h, s_len, d = q.shape
    scale = d ** -0.5
    nblk = s_len // MOBA_BLOCK
    kb = k.reshape(b, h, nblk, MOBA_BLOCK, d)
    vb = v.reshape(b, h, nblk, MOBA_BLOCK, d)
    k_mean = jnp.mean(kb.astype(jnp.float32), axis=3).astype(k.dtype)
    topk = min(MOBA_TOPK, nblk - 1)
    blk_ids = jnp.arange(nblk)
    in_blk = jnp.arange(MOBA_BLOCK)
    bi = jnp.arange(b)[:, None, None, None]
    hi = jnp.arange(h)[None, :, None, None]

    def block(i):
        q0 = i * Q_BLOCK
        qb = lax.dynamic_slice_in_dim(q, q0, Q_BLOCK, axis=2)
        qpos = q0 + jnp.arange(Q_BLOCK)
        own = q0 // MOBA_BLOCK
        k_own = lax.dynamic_index_in_dim(kb, own, axis=2, keepdims=False)
        v_own = lax.dynamic_index_in_dim(vb, own, axis=2, keepdims=False)
        own_pos = own * MOBA_BLOCK + in_blk
        dist_own = (qpos[:, None] - own_pos[None, :]).astype(jnp.float32)
        s_own = jnp.einsum('bhqd,bhkd->bhqk', qb, k_own).astype(jnp.float32) * scale - slopes[:, None, None] * dist_own
        s_own = jnp.where(own_pos[None, :] <= qpos[:, None], s_own, NEG)
        if topk == 0:
            p = jax.nn.softmax(s_own, axis=-1).astype(v.dtype)
            return jnp.einsum('bhqk,bhkd->bhqd', p, v_own)
        gscore = jnp.einsum('bhqd,bhnd->bhqn', qb, k_mean).astype(jnp.float32)
        gscore = jnp.where(blk_ids[None, None, None, :] < own, gscore, NEG)
        gval, gidx = lax.top_k(gscore, topk)
        sel_ok = gval > 0.5 * NEG
        k_sel = kb[bi, hi, gidx]
        v_sel = vb[bi, hi, gidx]
        sel_pos = gidx[..., None] * MOBA_BLOCK + in_blk
        dist_sel = (qpos[:, None, None] - sel_pos).astype(jnp.float32)
        s_sel = jnp.einsum('bhqd,bhqnkd->bhqnk', qb, k_sel).astype(jnp.float32) * scale - slopes[:, None, None, None] * dist_sel
        s_sel = jnp.where(sel_ok[..., None], s_sel, NEG).reshape(b, h, Q_BLOCK, topk * MOBA_BLOCK)
        p = jax.nn.softmax(jnp.concatenate([s_sel, s_own], axis=-1), axis=-1).astype(v.dtype)
        p_sel = p[..., :topk * MOBA_BLOCK].reshape(b, h, Q_BLOCK, topk, MOBA_BLOCK)
        p_own = p[..., topk * MOBA_BLOCK:]
        return (jnp.einsum('bhqnk,bhqnkd->bhqd', p_sel, v_sel)
                + jnp.einsum('bhqk,bhkd->bhqd', p_own, v_own))

    return sweep_query_blocks(block, s_len)


def dense_causal_attention(q, k, v):
    s_len, dq = q.shape[2], q.shape[3]
    scale = dq ** -0.5
    kpos = jnp.arange(s_len)

    def block(i):
        q0 = i * Q_BLOCK
        qb = lax.dynamic_slice_in_dim(q, q0, Q_BLOCK, axis=2)
        qpos = q0 + jnp.arange(Q_BLOCK)
        s = jnp.einsum('bhqd,bhkd->bhqk', qb, k).astype(jnp.float32) * scale
        s = jnp.where(kpos[None, :] <= qpos[:, None], s, NEG)
        p = jax.nn.softmax(s, axis=-1).astype(v.dtype)
        return jnp.einsum('bhqk,bhkd->bhqd', p, v)

    return sweep_query_blocks(block, s_len)


def mla_attention(c_q, c_kv, k_rope_in, g_cq, g_ckv, w_uq, w_ukv):
    b, s_len, _ = c_q.shape
    pos = jnp.arange(s_len)
    q = heads(rms_norm(c_q, g_cq) @ w_uq, N_MLA_HEADS)
    q = jnp.concatenate([q[..., :MLA_NOPE], rope(q[..., MLA_NOPE:], pos)], axis=-1)
    kv = heads(rms_norm(c_kv, g_ckv) @ w_ukv, N_MLA_HEADS)
    k_r = jnp.broadcast_to(rope(k_rope_in, pos)[:, None], (b, N_MLA_HEADS, s_len, MLA_ROPE))
    k = jnp.concatenate([kv[..., :MLA_NOPE], k_r], axis=-1)
    v = kv[..., MLA_NOPE:]
    return merge_heads(dense_causal_attention(q, k, v))


def nsa_compress(x, pe, w1, w2):
    b, s_len, d = x.shape
    n_chunks = s_len // NSA_CMP_STRIDE
    ratio = NSA_CMP_LEN // NSA_CMP_STRIDE
    chunks = x.reshape(b, n_chunks, NSA_CMP_STRIDE, d)
    blocks = jnp.concatenate([chunks[:, r:n_chunks - ratio + 1 + r] for r in range(ratio)], axis=2)
    flat = (blocks + pe).reshape(b, n_chunks - ratio + 1, NSA_CMP_LEN * d)
    return jax.nn.gelu(flat @ w1) @ w2


def nsa_attention(q, kv, gate_logits, pe, w_k1, w_k2, w_v1, w_v2, slopes):
    b, h, s_len, d = q.shape
    scale = d ** -0.5
    k_cmp, v_cmp, k_slc, v_slc, k_win, v_win = jnp.split(kv, 6, axis=-1)
    gates = jax.nn.sigmoid(gate_logits).reshape(b, s_len, h, 3).transpose(0, 2, 1, 3)
    k_c = nsa_compress(k_cmp, pe, w_k1, w_k2)
    v_c = nsa_compress(v_cmp, pe, w_v1, w_v2)
    n_cmp = k_c.shape[1]
    cmp_start = jnp.arange(n_cmp) * NSA_CMP_STRIDE
    cmp_end = cmp_start + NSA_CMP_LEN - 1
    n_sel = s_len // NSA_SEL_LEN
    sel_ids = jnp.arange(n_sel)
    sel_start = sel_ids * NSA_SEL_LEN
    overlap = ((cmp_start[:, None] < sel_start[None, :] + NSA_SEL_LEN)
               & (cmp_start[:, None] + NSA_CMP_LEN > sel_start[None, :])).astype(jnp.float32)
    topn = min(NSA_TOPN, n_sel)
    ks_b = k_slc.reshape(b, n_sel, NSA_SEL_LEN, d)
    vs_b = v_slc.reshape(b, n_sel, NSA_SEL_LEN, d)
    pad = jnp.zeros((b, NSA_WINDOW, d), k_win.dtype)
    kw_pad = jnp.concatenate([pad, k_win], axis=1)
    vw_pad = jnp.concatenate([pad, v_win], axis=1)
    bi = jnp.arange(b)[:, None, None]
    in_sel = jnp.arange(NSA_SEL_LEN)

    def block(i):
        q0 = i * Q_BLOCK
        qb = lax.dynamic_slice_in_dim(q, q0, Q_BLOCK, axis=2)
        gb = lax.dynamic_slice_in_dim(gates, q0, Q_BLOCK, axis=2)
        qpos = q0 + jnp.arange(Q_BLOCK)
        dist_c = (qpos[:, None] - cmp_end[None, :]).astype(jnp.float32)
        m_c = cmp_end[None, :] <= qpos[:, None]
        s_c = jnp.einsum('bhqd,bnd->bhqn', qb, k_c).astype(jnp.float32) * scale - slopes[:, None, None] * dist_c
        p_c = jnp.where(m_c, jax.nn.softmax(jnp.where(m_c, s_c, NEG), axis=-1), 0.0)
        o_c = jnp.einsum('bhqn,bnd->bhqd', p_c.astype(v_c.dtype), v_c)
        imp = jnp.einsum('bhqn,ns->bqs', p_c, overlap)
        cur = qpos // NSA_SEL_LEN
        forced = (sel_ids[None, :] == 0) | (sel_ids[None, :] == cur[:, None])
        past = sel_ids[None, :] < cur[:, None]
        score = jnp.where(forced, BIG, jnp.where(past, imp, NEG))
        val, idx = lax.top_k(score, topn)
        ok = val > 0.5 * NEG
        k_s = ks_b[bi, idx]
        v_s = vs_b[bi, idx]
        pos_s = idx[..., None] * NSA_SEL_LEN + in_sel
        dist_s = (qpos[:, None, None] - pos_s).astype(jnp.float32)[:, None]
        m_s = (ok[..., None] & (pos_s <= qpos[:, None, None]))[:, None]
        s_s = jnp.einsum('bhqd,bqnkd->bhqnk', qb, k_s).astype(jnp.float32) * scale - slopes[:, None, None, None] * dist_s
        s_s = jnp.where(m_s, s_s, NEG).reshape(b, h, Q_BLOCK, topn * NSA_SEL_LEN)
        p_s = jax.nn.softmax(s_s, axis=-1).astype(v_s.dtype).reshape(b, h, Q_BLOCK, topn, NSA_SEL_LEN)
        o_s = jnp.einsum('bhqnk,bqnkd->bhqd', p_s, v_s)
        k_w = lax.dynamic_slice_in_dim(kw_pad, q0, Q_BLOCK + NSA_WINDOW, axis=1)
        v_w = lax.dynamic_slice_in_dim(vw_pad, q0, Q_BLOCK + NSA_WINDOW, axis=1)
        pos_w = q0 - NSA_WINDOW + jnp.arange(Q_BLOCK + NSA_WINDOW)
        dist_w = qpos[:, None] - pos_w[None, :]
        m_w = (dist_w >= 0) & (dist_w < NSA_WINDOW) & (pos_w[None, :] >= 0)
        s_w = jnp.einsum('bhqd,bkd->bhqk', qb, k_w).astype(jnp.float32) * scale - slopes[:, None, None] * dist_w.astype(jnp.float32)
        p_w = jax.nn.softmax(jnp.where(m_w, s_w, NEG), axis=-1).astype(v_w.dtype)
        o_w = jnp.einsum('bhqk,bkd->bhqd', p_w, v_w)
        return gb[..., 0:1] * o_c + gb[..., 1:2] * o_s + gb[..., 2:3] * o_w

    return merge_heads(sweep_query_blocks(block, s_len))


def memory_cross_attention(q_mem, mem, w_mem_kv):
    q = heads(q_mem, N_MEM_HEADS)
    k, v = jnp.split(mem @ w_mem_kv, 2, axis=-1)
    k, v = heads(k, N_MEM_HEADS), heads(v, N_MEM_HEADS)
    s = jnp.einsum('bhqd,bhkd->bhqk', q, k).astype(jnp.float32) * (HEAD_DIM ** -0.5)
    p = jax.nn.softmax(s, axis=-1).astype(v.dtype)
    return merge_heads(jnp.einsum('bhqk,bhkd->bhqd', p, v))


def hybrid_mixer(x, mem, w_in, g_cq, g_ckv, w_uq, w_ukv, nsa_pe, w_phi_k1, w_phi_k2, w_phi_v1, w_phi_v2,
                 w_mem_kv, w_br, w_gate, b_gate, w_out):
    h = x @ w_in
    h_sb, h_moba, c_q, c_kv, k_rope_in, q_nsa, kv_nsa, g_nsa, q_mem = jnp.split(h, IN_SPLITS, axis=-1)
    q, k, v = jnp.split(h_sb, 3, axis=-1)
    o_sb = merge_heads(stick_breaking_attention(heads(q, N_SB_HEADS), heads(k, N_SB_HEADS), heads(v, N_SB_HEADS)))
    q, k, v = jnp.split(h_moba, 3, axis=-1)
    o_moba = merge_heads(moba_attention(heads(q, N_MOBA_HEADS), heads(k, N_MOBA_HEADS), heads(v, N_MOBA_HEADS),
                                        alibi_slopes(N_MOBA_HEADS)))
    o_mla = mla_attention(c_q, c_kv, k_rope_in, g_cq, g_ckv, w_uq, w_ukv)
    o_nsa = nsa_attention(heads(q_nsa, N_NSA_HEADS), kv_nsa, g_nsa, nsa_pe, w_phi_k1, w_phi_k2, w_phi_v1, w_phi_v2,
                          alibi_slopes(N_NSA_HEADS))
    o_mem = memory_cross_attention(q_mem, mem, w_mem_kv)
    merged = jnp.zeros_like(x)
    for i, o in enumerate((o_sb, o_moba, o_mla, o_nsa, o_mem)):
        gate = jax.nn.sigmoid(x @ w_gate[i] + b_gate[i])
        merged = merged + gate * (o @ w_br[i])
    return merged @ w_out


def hierarchical_moe(x, w_rg, b_rg, w_re, b_re, w_up, w_down):
    b, s_len, d = x.shape
    xt = x.reshape(b * s_len, d)
    glog = (xt @ w_rg + b_rg).astype(jnp.float32)
    pg = jax.nn.softmax(glog, axis=-1)
    g_sel = jnp.argmax(glog, axis=-1)
    g_oh = jax.nn.one_hot(g_sel, N_GROUPS, dtype=jnp.float32)
    pg_sel = jnp.sum(pg * g_oh, axis=-1)
    elog_all = (jnp.einsum('td,gde->tge', xt, w_re) + b_re).astype(jnp.float32)
    elog = jnp.einsum('tge,tg->te', elog_all, g_oh)
    pe = jax.nn.softmax(elog, axis=-1)
    val, idx = lax.top_k(pe, TOPK_IN_GROUP)
    weight = pg_sel[:, None] * val / jnp.sum(val, axis=-1, keepdims=True)
    expert_id = g_sel[:, None] * EXPERTS_PER_GROUP + idx
    gate_dense = jnp.sum(jax.nn.one_hot(expert_id, N_EXPERTS, dtype=jnp.float32) * weight[..., None], axis=1).astype(x.dtype)
    y = jnp.zeros_like(xt)
    for e in range(N_EXPERTS):
        a, u = jnp.split(xt @ w_up[e], 2, axis=-1)
        y = y + gate_dense[:, e:e + 1] * ((jax.nn.silu(a) * u) @ w_down[e])
    return y.reshape(b, s_len, d)


def setup_inputs(seed: int = 0) -> dict:
    key = jax.random.key(seed)
    ks = jax.random.split(key, 27)
    L = DEPTH

    def nrm(k, shape, scale):
        return jax.random.normal(k, shape, jnp.float32) * scale

    def gain(k, shape):
        return 1.0 + 0.02 * jax.random.normal(k, shape, jnp.float32)

    return {
        'x': nrm(ks[0], (BATCH, SEQ, D_MODEL), 1.0),
        'mem': nrm(ks[1], (BATCH, MEM_LEN, D_MODEL), 1.0),
        'w_in': nrm(ks[2], (L, D_MODEL, IN_TOTAL), D_MODEL ** -0.5),
        'g_cq': gain(ks[3], (L, MLA_Q_RANK)),
        'g_ckv': gain(ks[4], (L, MLA_KV_RANK)),
        'w_uq': nrm(ks[5], (L, MLA_Q_RANK, N_MLA_HEADS * (MLA_NOPE + MLA_ROPE)), MLA_Q_RANK ** -0.5),
        'w_ukv': nrm(ks[6], (L, MLA_KV_RANK, N_MLA_HEADS * (MLA_NOPE + MLA_V)), MLA_KV_RANK ** -0.5),
        'nsa_pe': nrm(ks[7], (L, NSA_CMP_LEN, HEAD_DIM), 0.1),
        'w_phi_k1': nrm(ks[8], (L, NSA_CMP_LEN * HEAD_DIM, NSA_PHI_HIDDEN), (NSA_CMP_LEN * HEAD_DIM) ** -0.5),
        'w_phi_k2': nrm(ks[9], (L, NSA_PHI_HIDDEN, HEAD_DIM), NSA_PHI_HIDDEN ** -0.5),
        'w_phi_v1': nrm(ks[10], (L, NSA_CMP_LEN * HEAD_DIM, NSA_PHI_HIDDEN), (NSA_CMP_LEN * HEAD_DIM) ** -0.5),
        'w_phi_v2': nrm(ks[11], (L, NSA_PHI_HIDDEN, HEAD_DIM), NSA_PHI_HIDDEN ** -0.5),
        'w_mem_kv': nrm(ks[12], (L, D_MODEL, 2 * N_MEM_HEADS * HEAD_DIM), D_MODEL ** -0.5),
        'w_br': nrm(ks[13], (L, N_BRANCHES, BRANCH_W, D_MODEL), DEEPNORM_BETA * BRANCH_W ** -0.5),
        'w_gate': nrm(ks[14], (L, N_BRANCHES, D_MODEL, D_MODEL), D_MODEL ** -0.5),
        'b_gate': nrm(ks[15], (L, N_BRANCHES, D_MODEL), 0.02),
        'w_out': nrm(ks[16], (L, D_MODEL, D_MODEL), DEEPNORM_BETA * D_MODEL ** -0.5),
        'ln1_g': gain(ks[17], (L, D_MODEL)),
        'ln1_b': nrm(ks[18], (L, D_MODEL), 0.02),
        'w_rg': nrm(ks[19], (L, D_MODEL, N_GROUPS), D_MODEL ** -0.5),
        'b_rg': nrm(ks[20], (L, N_GROUPS), 0.01),
        'w_re': nrm(ks[21], (L, N_GROUPS, D_MODEL, EXPERTS_PER_GROUP), D_MODEL ** -0.5),
        'b_re': nrm(ks[22], (L, N_GROUPS, EXPERTS_PER_GROUP), 0.01),
        'w_up': nrm(ks[23], (L, N_EXPERTS, D_MODEL, 2 * D_EXPERT), D_MODEL ** -0.5),
        'w_down': nrm(ks[24], (L, N_EXPERTS, D_EXPERT, D_MODEL), DEEPNORM_BETA * D_EXPERT ** -0.5),
        'ln2_g': gain(ks[25], (L, D_MODEL)),
        'ln2_b': nrm(ks[26], (L, D_MODEL), 0.02),
    }


def reference(x, mem, w_in, g_cq, g_ckv, w_uq, w_ukv, nsa_pe, w_phi_k1, w_phi_k2, w_phi_v1, w_phi_v2,
              w_mem_kv, w_br, w_gate, b_gate, w_out, ln1_g, ln1_b, w_rg, b_rg, w_re, b_re, w_up, w_down,
              ln2_g, ln2_b):
    s_len = x.shape[1]
    s_pad = -(-s_len // MOBA_BLOCK) * MOBA_BLOCK
    h = jnp.pad(x, ((0, 0), (0, s_pad - s_len), (0, 0)))
    for l in range(DEPTH):
        y = hybrid_mixer(h, mem, w_in[l], g_cq[l], g_ckv[l], w_uq[l], w_ukv[l], nsa_pe[l],
                         w_phi_k1[l], w_phi_k2[l], w_phi_v1[l], w_phi_v2[l], w_mem_kv[l],
                         w_br[l], w_gate[l], b_gate[l], w_out[l])
        h = layer_norm(DEEPNORM_ALPHA * h + y, ln1_g[l], ln1_b[l])
        y = hierarchical_moe(h, w_rg[l], b_rg[l], w_re[l], b_re[l], w_up[l], w_down[l])
        h = layer_norm(DEEPNORM_ALPHA * h + y, ln2_g[l], ln2_b[l])
    return h[:, :s_len]
```

```python
import contextlib
import types
import numpy as np
import ml_dtypes
import concourse.bass as bass
import concourse.mybir as mybir
from concourse.bass_utils import run_bass_kernel_spmd

F32 = mybir.dt.float32
BF16 = mybir.dt.bfloat16
AF = mybir.ActivationFunctionType
ALU = mybir.AluOpType
AX = mybir.AxisListType

D = 1024
S = 8192
NB = 4
DEPTH = 4
TOK = 4096
NSLOT = 8
IN_TOTAL = 2860
ALPHA = (2.0 * DEPTH) ** 0.25
LN_EPS = 1e-5
RMS_EPS = 1e-6
MASKV = -30000.0
SLOPES = [2.0 ** (-2.0 * (i + 1)) for i in range(4)]

ENGS = ("pe", "act", "dve", "pool", "sp")
SIG_EPOCH = 30000


def _freeze(fn):
    if fn.__closure__ is None:
        return fn
    cells = []
    for cl in fn.__closure__:
        try:
            cells.append(types.CellType(cl.cell_contents))
        except ValueError:
            cells.append(cl)
    g = types.FunctionType(fn.__code__, fn.__globals__, fn.__name__, fn.__defaults__, tuple(cells))
    g.__kwdefaults__ = fn.__kwdefaults__
    return g


class Op:
    __slots__ = ("eng", "fn", "reads", "writes", "dma", "deps", "sig", "dticket", "idx", "dprev")

    def __init__(self, eng, fn, reads, writes, dma):
        self.eng = eng
        self.fn = fn
        self.reads = tuple(reads)
        self.writes = tuple(writes)
        self.dma = dma
        self.deps = []
        self.sig = None
        self.dticket = None
        self.dprev = None


class Prog:
    NDSEM = 12
    _phase_id = 0

    def __init__(self, nc):
        self.nc = nc
        self.ops = []

    def add(self, eng, fn, reads=(), writes=(), dma=False):
        op = Op(eng, _freeze(fn), reads, writes, dma)
        op.idx = len(self.ops)
        self.ops.append(op)
        return op

    def pe(self, fn, reads=(), writes=()):
        return self.add("pe", fn, reads, writes)

    def act(self, fn, reads=(), writes=()):
        return self.add("act", fn, reads, writes)

    def dve(self, fn, reads=(), writes=()):
        return self.add("dve", fn, reads, writes)

    def pool(self, fn, reads=(), writes=()):
        return self.add("pool", fn, reads, writes)

    def allgather(self, out, in_, groups, reads=(), writes=()):
        return self.add("pool", lambda e: e.collective_compute("AllGather", ALU.bypass, replica_groups=groups, ins=[in_.opt()], outs=[out.opt()]),
                        reads, writes, dma="cc")

    def dma(self, out, in_, reads=(), writes=(), q="sp", slow=False):
        if slow:
            return self.add(q, lambda e: e.dma_start(out=out, in_=in_, allow_slow_non_contiguous=True), reads, writes, dma=True)
        return self.add(q, lambda e: e.dma_start(out=out, in_=in_), reads, writes, dma=True)

    def analyze(self):
        last_w = {}
        readers = {}
        for op in self.ops:
            deps = set()
            for t in op.reads:
                if t in last_w:
                    deps.add(last_w[t])
            for t in op.writes:
                if t in last_w:
                    deps.add(last_w[t])
                for r in readers.get(t, ()):
                    deps.add(r)
            deps.discard(op.idx)
            op.deps = sorted(deps)
            for t in op.reads:
                readers.setdefault(t, []).append(op.idx)
            for t in op.writes:
                last_w[t] = op.idx
                readers[t] = []
        qcount = {e: 0 for e in ENGS}
        qhist = {e: [] for e in ENGS}
        for op in self.ops:
            if op.dma == "cc":
                op.dticket = ("cc", op.idx, 1)
                continue
            if op.dma:
                n = qcount[op.eng]
                qcount[op.eng] += 1
                op.dticket = (op.eng, n % self.NDSEM, 16 * (n // self.NDSEM + 1))
                if n >= self.NDSEM:
                    op.dprev = qhist[op.eng][n - self.NDSEM]
                qhist[op.eng].append(op.idx)
        waited_eng = {e: {p: -1 for p in ENGS} for e in ENGS}
        waited_dma = {e: set() for e in ENGS}
        last_on = {e: -1 for e in ENGS}
        need_sig = set()
        for op in self.ops:
            e = op.eng
            final = []
            best = {}
            dl = list(op.deps)
            if op.dprev is not None:
                dl.append(op.dprev)
            for d in dl:
                p = self.ops[d]
                if p.dma:
                    if d not in waited_dma[e]:
                        waited_dma[e].add(d)
                        final.append(("dma", d))
                else:
                    if p.eng == e and e == "pe":
                        continue
                    if d <= waited_eng[e][p.eng]:
                        continue
                    if p.eng not in best or d > best[p.eng]:
                        best[p.eng] = d
            for pe_, d in best.items():
                waited_eng[e][pe_] = d
                need_sig.add(d)
                final.append(("eng", d))
            op.deps = final
        cnt = {e: 0 for e in ENGS}
        for op in self.ops:
            if not op.dma and op.idx in need_sig:
                cnt[op.eng] += 1
                op.sig = cnt[op.eng]
        self.sig_total = cnt
        self.dma_total = qcount

    def emit(self, barrier=False):
        nc = self.nc
        self.analyze()
        allsem = []

        Prog._phase_id += 1
        pid = Prog._phase_id

        def newsem(name):
            h = nc.alloc_semaphore(f"{name}_ph{pid}")
            allsem.append(h)
            return h

        if True:
            esem = {}
            for e in ENGS:
                n_ep = self.sig_total[e] // SIG_EPOCH + 1
                esem[e] = [newsem(f"s_{e}_{i}") for i in range(n_ep)]
            dsem = {}
            for e in ENGS:
                if self.dma_total[e]:
                    dsem[e] = [newsem(f"d_{e}_{i}") for i in range(self.NDSEM)]
            dsem["cc"] = {op.idx: newsem(f"cc_{op.idx}") for op in self.ops if op.dma == "cc"}

            def waitspec(dep):
                kind, d = dep
                p = self.ops[d]
                if kind == "dma":
                    q, si, val = p.dticket
                    return dsem[q][si], val
                k = p.sig - 1
                return esem[p.eng][k // SIG_EPOCH], k % SIG_EPOCH + 1

            def run(engname):
                def body(eng):
                    last_dma = {}
                    for op in self.ops:
                        if op.eng != engname:
                            continue
                        ws = [waitspec(d) for d in op.deps]
                        for (sem, val) in ws[1:]:
                            eng.wait_ge(sem, val)
                        ins = op.fn(eng)
                        if ws:
                            ins._wait_ge(ws[0][0], ws[0][1])
                        if op.dma == "cc":
                            ins.then_inc(dsem["cc"][op.idx])
                            eng.wait_ge(dsem["cc"][op.idx], 1)
                        elif op.dma:
                            q, si, val = op.dticket
                            ins.then_inc(dsem[q][si], 16)
                            last_dma[si] = val
                        elif op.sig is not None:
                            k = op.sig - 1
                            ins.then_inc(esem[engname][k // SIG_EPOCH], 1)
                    for si, val in last_dma.items():
                        eng.wait_ge(dsem[engname][si], val)
                return body

            with nc.Block() as block:
                block.tensor(run("pe"))
                block.scalar(run("act"))
                block.vector(run("dve"))
                block.gpsimd(run("pool"))
                block.sync(run("sp"))
        if barrier:
            nc.all_engine_barrier()
            nc.clear_and_free_semaphores(allsem)
            nc.all_engine_barrier()
        else:
            for h in allsem:
                nc.release_semaphore(h)


class KB:
    def __init__(self):
        self.nc = bass.Bass("TRN2", target_bir_lowering=False)
        self.p = Prog(self.nc)
        self.es = contextlib.ExitStack()
        self.dram = {}
        self._uid = 0
        self.es0 = contextlib.ExitStack()
        self.psf = [self.es0.enter_context(self.nc.psum_tensor(f"psb{i}", [128, 512], F32)) for i in range(8)]

    def uid(self, s):
        self._uid += 1
        return f"{s}_{self._uid}"

    def din(self, name, shape, dt=F32):
        t = self.nc.dram_tensor(name, list(shape), dt, kind="ExternalInput").ap()
        self.dram[name] = t
        return t

    def dout(self, name, shape, dt=F32, kind="ExternalOutput"):
        t = self.nc.dram_tensor(name, list(shape), dt, kind=kind).ap()
        self.dram[name] = t
        return t

    def sb(self, name, shape, dt=F32):
        return self.es.enter_context(self.nc.sbuf_tensor(self.uid(name), list(shape), dt))

    def dscratch(self, name, shape, dt=F32):
        t = self.nc.dram_tensor(name, list(shape), dt, kind="Internal").ap()
        self.dram[name] = t
        return t

    def end_phase(self):
        self.p.emit(barrier=True)
        self.es.close()
        self.es = contextlib.ExitStack()
        self.p = Prog(self.nc)

    def ps(self, i):
        return self.psf[i]

    def finish(self):
        self.p.emit()
        self.es.close()
        self.es0.close()
        return self.nc


C_SBQ, C_SBK, C_SBV = 0, 256, 512
C_MOQ, C_MOK, C_MOV = 768, 1024, 1280
C_CQ, C_CKV, C_KR = 1536, 1792, 1920
C_NSQ, C_NSKV, C_NSG, C_MEQ = 1952, 2208, 2592, 2604


def consts_common(kb):
    nc, p = kb.nc, kb.p
    c = {}
    c["identf"] = kb.sb("identf", [128, 128], F32)
    c["identb"] = kb.sb("identb", [128, 128], BF16)
    c["onesb"] = kb.sb("onesb", [128, 128], BF16)
    c["onesf"] = kb.sb("onesf", [128, 128], F32)
    idf, idb = c["identf"], c["identb"]
    p.pool(lambda e: e.memset(c["onesf"][:], 1.0), writes=["onesf"])
    p.pool(lambda e: e.memset(c["onesb"][:], 1.0), writes=["onesb"])
    p.pool(lambda e: e.affine_select(out=idf[:], in_=c["onesf"][:], pattern=[[-1, 128]], compare_op=ALU.is_equal,
                                     fill=0.0, base=0, channel_multiplier=1), reads=["onesf"], writes=["identf"])
    p.pool(lambda e: e.tensor_copy(out=idb[:], in_=idf[:]), reads=["identf"], writes=["identb"])
    return c


def tm_view(ap2d, p=128):
    return ap2d.rearrange("(n p) c -> p n c", p=p)


def phase_a(kb, c, io, w):
    nc, p = kb.nc, kb.p
    identf, onesb = c["identf"], c["onesb"]

    hT = kb.sb("hT", [128, 8, TOK], BF16)
    win = kb.sb("win", [128, 8, IN_TOTAL], BF16)
    wkrot = kb.sb("wkrot", [128, 8, 32], BF16)
    for f in range(8):
        p.dma(win[:, f, :], w["w_in"][f * 128:(f + 1) * 128, :], writes=[("win", f)], q="pool")
    for f in range(8):
        p.dve(lambda e, f=f: e.tensor_scalar_mul(out=wkrot[:, f, 0:16], in0=win[:, f, C_KR + 16:C_KR + 32], scalar1=-1.0),
              reads=[("win", f)], writes=[("wkrot", f)])
        p.dve(lambda e, f=f: e.tensor_copy(out=wkrot[:, f, 16:32], in_=win[:, f, C_KR:C_KR + 16]),
              reads=[("win", f)], writes=[("wkrot", f)])
    wuq_f = kb.sb("wuq_f", [128, 2, 384], F32)
    wuq = kb.sb("wuq", [128, 2, 384], BF16)
    wuqr = kb.sb("wuqr", [128, 2, 384], BF16)
    gcq = kb.sb("gcq", [128, 2], F32)
    wukv_f = kb.sb("wukv_f", [128, 512], F32)
    wukv = kb.sb("wukv", [128, 512], BF16)
    gckv = kb.sb("gckv", [128, 1], F32)
    p.dma(wuq_f[:], w["w_uq"].rearrange("(n p) c -> p n c", p=128), writes=["wuq_f"])
    p.dma(gcq[:], w["g_cq"].rearrange("(n p) -> p n", p=128), writes=["gcq"], slow=True)
    p.dma(wukv_f[:], w["w_ukv"], writes=["wukv_f"])
    p.dma(gckv[:], w["g_ckv"].rearrange("(n p) -> p n", p=128), writes=["gckv"], slow=True)
    for rc in range(2):
        p.dve(lambda e, rc=rc: e.tensor_scalar_mul(out=wuq[:, rc, :], in0=wuq_f[:, rc, :], scalar1=gcq[:, rc:rc + 1]),
              reads=["wuq_f", "gcq"], writes=["wuq"])
    p.dve(lambda e: e.memset(wuqr[:], 0.0), writes=["wuqr"])
    for rc in range(2):
        for h in range(4):
            b0 = h * 96
            p.dve(lambda e, rc=rc, b0=b0: e.tensor_scalar_mul(out=wuqr[:, rc, b0 + 64:b0 + 80], in0=wuq[:, rc, b0 + 80:b0 + 96], scalar1=-1.0),
                  reads=["wuq"], writes=["wuqr"])
            p.dve(lambda e, rc=rc, b0=b0: e.tensor_copy(out=wuqr[:, rc, b0 + 80:b0 + 96], in_=wuq[:, rc, b0 + 64:b0 + 80]),
                  reads=["wuq"], writes=["wuqr"])
    p.dve(lambda e: e.tensor_scalar_mul(out=wukv[:], in0=wukv_f[:], scalar1=gckv[:, 0:1]), reads=["wukv_f", "gckv"], writes=["wukv"])

    hst = [kb.sb("hst", [128, 1024], F32) for _ in range(2)]
    for ck in range(TOK // 128):
        st = hst[ck % 2]
        tk = ("hst", ck % 2)
        p.dma(st[:], io["h_tok"][ck * 128:(ck + 1) * 128, :], writes=[tk])
        for half in range(2):
            bank = (ck * 2 + half) % 2
            ps = kb.ps(bank)
            for j in range(4):
                f = half * 4 + j
                p.pe(lambda e, ps=ps, st=st, f=f, j=j: e.transpose(out=ps[:, j * 128:(j + 1) * 128], in_=st[:, f * 128:(f + 1) * 128], identity=identf[:]),
                     reads=[tk, "identf"], writes=[("ps", bank)])
            dst = hT[:, half * 4:half * 4 + 4, ck * 128:(ck + 1) * 128]
            src = ps[:].rearrange("p (j t) -> p j t", j=4)
            if half == 0:
                p.act(lambda e, dst=dst, src=src: e.copy(out=dst, in_=src), reads=[("ps", bank)], writes=[("hT", ck // 4)])
            else:
                p.dve(lambda e, dst=dst, src=src: e.tensor_copy(out=dst, in_=src), reads=[("ps", bank)], writes=[("hT", ck // 4)])
    for f in range(8):
        p.dma(io["hT_d"][f * 128:(f + 1) * 128, :], hT[:, f, :], reads=[("hT", s) for s in range(8)], writes=[("hT_d", f)])

    ostage = [kb.sb("ostg", [128, 512], BF16) for _ in range(4)]
    gstage = [kb.sb("gstg", [12, 512], F32) for _ in range(2)]
    cq_sb = [kb.sb("cq_sb", [128, 2, 512], BF16) for _ in range(2)]
    ckv_sb = [kb.sb("ckv_sb", [128, 512], BF16) for _ in range(2)]
    krr_sb = [kb.sb("krr", [32, 2, 512], F32) for _ in range(2)]
    sq_sb = [kb.sb("sq", [128, 3, 512], BF16) for _ in range(2)]
    rstd_q = [kb.sb("rstdq", [128, 512], F32) for _ in range(2)]
    rstd_kv = [kb.sb("rstdkv", [128, 512], F32) for _ in range(2)]
    rkv_tok = [kb.sb("rkvtok", [128, 4], F32) for _ in range(2)]
    ropeq = [kb.sb("ropeq", [96, 2, 512], F32) for _ in range(2)]
    ropek = [kb.sb("ropek", [32, 2, 512], F32) for _ in range(2)]
    t1 = [kb.sb("t1", [96, 512], F32) for _ in range(2)]
    t2 = [kb.sb("t2", [96, 512], F32) for _ in range(2)]
    vstage = [kb.sb("vstg", [128, 640], BF16) for _ in range(2)]
    vmst = [kb.sb("vmst", [128, 256], BF16) for _ in range(2)]
    cnt = {"o": 0, "bank": 0, "v": 0}

    def nbank():
        b = 2 + cnt["bank"] % 6
        cnt["bank"] += 1
        return b

    fm_list = [
        ("qsb_d", 0, C_SBQ, 128, 0.125), ("qsb_d", 128, C_SBQ + 128, 128, 0.125),
        ("ksb_d", 0, C_SBK, 128, 1.0), ("ksb_d", 128, C_SBK + 128, 128, 1.0),
        ("qmo_d", 0, C_MOQ, 128, 0.125), ("qmo_d", 128, C_MOQ + 128, 128, 0.125),
        ("kmo_d", 0, C_MOK, 128, 1.0), ("kmo_d", 128, C_MOK + 128, 128, 1.0),
        ("qns_d", 0, C_NSQ, 128, 0.125), ("qns_d", 128, C_NSQ + 128, 128, 0.125),
        ("kcv_d", 0, C_NSKV, 128, 1.0),
        ("ksl_d", 0, C_NSKV + 128, 64, 1.0),
        ("kwi_d", 0, C_NSKV + 256, 64, 1.0),
        ("qme_d", 0, C_MEQ, 128, 0.125), ("qme_d", 128, C_MEQ + 128, 128, 0.125),
    ]

    def proj_fm(s, col0, ncols, wsrc=None):
        b = nbank()
        ps = kb.ps(b)
        for f in range(8):
            if wsrc is None:
                lhsT = win[:, f, col0:col0 + ncols]
                rd = [("win", f)]
            else:
                lhsT = wsrc[:, f, col0:col0 + ncols]
                rd = [("wkrot", f)]
            p.pe(lambda e, ps=ps, lhsT=lhsT, f=f, s=s, ncols=ncols: e.matmul(ps[0:ncols, :], lhsT=lhsT, rhs=hT[:, f, s * 512:(s + 1) * 512],
                                                                            start=(f == 0), stop=(f == 7)),
                 reads=rd + [("hT", s)], writes=[("ps", b)])
        return b

    for s in range(NSLOT):
        tsl = slice(s * 512, (s + 1) * 512)
        for (dn, r0, col0, ncols, scale) in fm_list:
            b = proj_fm(s, col0, ncols)
            ps = kb.ps(b)
            k = cnt["o"] % 4
            cnt["o"] += 1
            og = ostage[k]
            p.act(lambda e, og=og, ps=ps, ncols=ncols, scale=scale: e.activation(out=og[0:ncols, :], in_=ps[0:ncols, :], func=AF.Copy, scale=scale),
                  reads=[("ps", b)], writes=[("ostg", k)])
            p.dma(io[dn][r0:r0 + ncols, tsl], og[0:ncols, :], reads=[("ostg", k)], writes=[(dn, s)])
        b = proj_fm(s, C_NSG, 12)
        ps = kb.ps(b)
        gs = gstage[s % 2]
        p.act(lambda e, gs=gs, ps=ps: e.activation(out=gs[:], in_=ps[0:12, :], func=AF.Sigmoid), reads=[("ps", b)], writes=[("gstg", s % 2)])
        p.dma(io["gns_d"][:, tsl], gs[:], reads=[("gstg", s % 2)], writes=[("gns_d", s)])

        d2 = s % 2
        cq, ckv, sq = cq_sb[d2], ckv_sb[d2], sq_sb[d2]
        for rc in range(2):
            b = proj_fm(s, C_CQ + rc * 128, 128)
            ps = kb.ps(b)
            p.act(lambda e, cq=cq, ps=ps, rc=rc: e.copy(out=cq[:, rc, :], in_=ps[:]), reads=[("ps", b)], writes=[("cq", d2)])
            p.act(lambda e, sq=sq, ps=ps, rc=rc: e.activation(out=sq[:, rc, :], in_=ps[:], func=AF.Square), reads=[("ps", b)], writes=[("sq", d2)])
        b = proj_fm(s, C_CKV, 128)
        ps = kb.ps(b)
        p.act(lambda e, ckv=ckv, ps=ps: e.copy(out=ckv[:], in_=ps[:]), reads=[("ps", b)], writes=[("ckv", d2)])
        p.act(lambda e, sq=sq, ps=ps: e.activation(out=sq[:, 2, :], in_=ps[:], func=AF.Square), reads=[("ps", b)], writes=[("sq", d2)])
        krr = krr_sb[d2]
        b = proj_fm(s, C_KR, 32)
        ps = kb.ps(b)
        p.act(lambda e, krr=krr, ps=ps: e.copy(out=krr[:, 0, :], in_=ps[0:32, :]), reads=[("ps", b)], writes=[("krr", d2)])
        b = proj_fm(s, 0, 32, wsrc=wkrot)
        ps = kb.ps(b)
        p.act(lambda e, krr=krr, ps=ps: e.copy(out=krr[:, 1, :], in_=ps[0:32, :]), reads=[("ps", b)], writes=[("krr", d2)])
        rq, rkv = rstd_q[d2], rstd_kv[d2]
        b = nbank()
        ps = kb.ps(b)
        for rc in range(2):
            p.pe(lambda e, ps=ps, sq=sq, rc=rc: e.matmul(ps[:], lhsT=onesb[:], rhs=sq[:, rc, :], start=(rc == 0), stop=(rc == 1)),
                 reads=[("sq", d2), "onesb"], writes=[("ps", b)])
        p.act(lambda e, rq=rq, ps=ps: e.activation(out=rq[:], in_=ps[:], func=AF.Ln, scale=1.0 / 256.0, bias=c["eps_rms"][:, 0:1]),
              reads=[("ps", b), "cst"], writes=[("rq", d2)])
        p.act(lambda e, rq=rq: e.activation(out=rq[:], in_=rq[:], func=AF.Exp, scale=-0.5), reads=[("rq", d2)], writes=[("rq", d2)])
        b = nbank()
        ps = kb.ps(b)
        p.pe(lambda e, ps=ps, sq=sq: e.matmul(ps[:], lhsT=onesb[:], rhs=sq[:, 2, :], start=True, stop=True),
             reads=[("sq", d2), "onesb"], writes=[("ps", b)])
        p.act(lambda e, rkv=rkv, ps=ps: e.activation(out=rkv[:], in_=ps[:], func=AF.Ln, scale=1.0 / 128.0, bias=c["eps_rms"][:, 0:1]),
              reads=[("ps", b), "cst"], writes=[("rkv", d2)])
        p.act(lambda e, rkv=rkv: e.activation(out=rkv[:], in_=rkv[:], func=AF.Exp, scale=-0.5), reads=[("rkv", d2)], writes=[("rkv", d2)])
        rkt = rkv_tok[d2]
        b = nbank()
        ps = kb.ps(b)
        for ck in range(4):
            p.pe(lambda e, ps=ps, sq=sq, ck=ck: e.matmul(ps[:, ck:ck + 1], lhsT=sq[:, 2, ck * 128:(ck + 1) * 128], rhs=onesb[:, 0:1], start=True, stop=True),
                 reads=[("sq", d2), "onesb"], writes=[("ps", b)])
        p.act(lambda e, rkt=rkt, ps=ps: e.activation(out=rkt[:], in_=ps[:, 0:4], func=AF.Ln, scale=1.0 / 128.0, bias=c["eps_rms"][:, 0:1]),
              reads=[("ps", b), "cst"], writes=[("rkt", d2)])
        p.act(lambda e, rkt=rkt: e.activation(out=rkt[:], in_=rkt[:], func=AF.Exp, scale=-0.5), reads=[("rkt", d2)], writes=[("rkt", d2)])
        rpq, rpk = ropeq[d2], ropek[d2]
        p.dma(rpq[:], io["ropeq_t"][s], writes=[("rpq", d2)])
        p.dma(rpk[:], io["ropek_t"][s], writes=[("rpk", d2)])
        for h in range(4):
            bA, bB = nbank(), nbank()
            psA, psB = kb.ps(bA), kb.ps(bB)
            for rc in range(2):
                p.pe(lambda e, psA=psA, cq=cq, rc=rc, h=h: e.matmul(psA[0:96, :], lhsT=wuq[:, rc, h * 96:(h + 1) * 96], rhs=cq[:, rc, :], start=(rc == 0), stop=(rc == 1)),
                     reads=["wuq", ("cq", d2)], writes=[("ps", bA)])
            for rc in range(2):
                p.pe(lambda e, psB=psB, cq=cq, rc=rc, h=h: e.matmul(psB[0:96, :], lhsT=wuqr[:, rc, h * 96:(h + 1) * 96], rhs=cq[:, rc, :], start=(rc == 0), stop=(rc == 1)),
                     reads=["wuqr", ("cq", d2)], writes=[("ps", bB)])
            a1, a2 = t1[h % 2], t2[h % 2]
            k = cnt["o"] % 4
            cnt["o"] += 1
            og = ostage[k]
            p.dve(lambda e, a1=a1, psA=psA, rpq=rpq: e.tensor_tensor(out=a1[:], in0=psA[0:96, :], in1=rpq[:, 0, :], op=ALU.mult),
                  reads=[("ps", bA), ("rpq", d2)], writes=[("t1", h % 2)])
            p.dve(lambda e, a2=a2, psB=psB, rpq=rpq: e.tensor_tensor(out=a2[:], in0=psB[0:96, :], in1=rpq[:, 1, :], op=ALU.mult),
                  reads=[("ps", bB), ("rpq", d2)], writes=[("t2", h % 2)])
            p.dve(lambda e, a1=a1, a2=a2: e.tensor_tensor(out=a1[:], in0=a1[:], in1=a2[:], op=ALU.add),
                  reads=[("t1", h % 2), ("t2", h % 2)], writes=[("t1", h % 2)])
            p.dve(lambda e, a1=a1, og=og, rq=rq: e.tensor_tensor(out=og[0:96, :], in0=a1[:], in1=rq[0:96, :], op=ALU.mult),
                  reads=[("t1", h % 2), ("rq", d2)], writes=[("ostg", k)])
            p.dma(io["qml_d"][h, :, tsl], og[0:96, :], reads=[("ostg", k)], writes=[("qml_d", s, h)])
            b = nbank()
            ps = kb.ps(b)
            p.pe(lambda e, ps=ps, ckv=ckv, h=h: e.matmul(ps[0:64, :], lhsT=wukv[:, h * 128:h * 128 + 64], rhs=ckv[:], start=True, stop=True),
                 reads=["wukv", ("ckv", d2)], writes=[("ps", b)])
            k = cnt["o"] % 4
            cnt["o"] += 1
            og = ostage[k]
            p.dve(lambda e, og=og, ps=ps, rkv=rkv: e.tensor_tensor(out=og[0:64, :], in0=ps[0:64, :], in1=rkv[0:64, :], op=ALU.mult),
                  reads=[("ps", b), ("rkv", d2)], writes=[("ostg", k)])
            p.dma(io["kml_d"][h, :, tsl], og[0:64, :], reads=[("ostg", k)], writes=[("kml_d", s, h)])
        a1, a2 = t1[0], t2[0]
        k = cnt["o"] % 4
        cnt["o"] += 1
        og = ostage[k]
        p.dve(lambda e, a1=a1, krr=krr, rpk=rpk: e.tensor_tensor(out=a1[0:32, :], in0=krr[:, 0, :], in1=rpk[:, 0, :], op=ALU.mult),
              reads=[("krr", d2), ("rpk", d2)], writes=[("t1", 0)])
        p.dve(lambda e, a2=a2, krr=krr, rpk=rpk: e.tensor_tensor(out=a2[0:32, :], in0=krr[:, 1, :], in1=rpk[:, 1, :], op=ALU.mult),
              reads=[("krr", d2), ("rpk", d2)], writes=[("t2", 0)])
        p.dve(lambda e, a1=a1, a2=a2, og=og: e.tensor_tensor(out=og[0:32, :], in0=a1[0:32, :], in1=a2[0:32, :], op=ALU.add),
              reads=[("t1", 0), ("t2", 0)], writes=[("ostg", k)])
        p.dma(io["krl_d"][:, tsl], og[0:32, :], reads=[("ostg", k)], writes=[("krl_d", s)])
        for ck in range(4):
            gck = s * 4 + ck
            b = nbank()
            ps = kb.ps(b)
            for h in range(4):
                p.pe(lambda e, ps=ps, ckv=ckv, ck=ck, h=h: e.matmul(ps[:, h * 64:(h + 1) * 64], lhsT=ckv[:, ck * 128:(ck + 1) * 128], rhs=wukv[:, h * 128 + 64:h * 128 + 128],
                                                                 start=True, stop=True),
                     reads=["wukv", ("ckv", d2)], writes=[("ps", b)])
            vm = vmst[gck % 2]
            p.act(lambda e, vm=vm, ps=ps, rkt=rkt, ck=ck: e.activation(out=vm[:], in_=ps[:, 0:256], func=AF.Copy, scale=rkt[:, ck:ck + 1]),
                  reads=[("ps", b), ("rkt", d2)], writes=[("vmst", gck % 2)])
            p.dma(io["vml_d"][gck * 128:(gck + 1) * 128, :], vm[:], reads=[("vmst", gck % 2)], writes=[("vml_d", gck)])

        for ck in range(4):
            gck = s * 4 + ck
            tcs = slice(gck * 128, (gck + 1) * 128)
            vs = vstage[gck % 2]
            b1, b2 = nbank(), nbank()
            ps1, ps2 = kb.ps(b1), kb.ps(b2)
            for f in range(8):
                p.pe(lambda e, ps1=ps1, f=f, tcs=tcs: e.matmul(ps1[:, 0:256], lhsT=hT[:, f, tcs], rhs=win[:, f, C_SBV:C_SBV + 256], start=(f == 0), stop=(f == 7)),
                     reads=[("win", f), ("hT", s)], writes=[("ps", b1)])
            for f in range(8):
                p.pe(lambda e, ps1=ps1, f=f, tcs=tcs: e.matmul(ps1[:, 256:512], lhsT=hT[:, f, tcs], rhs=win[:, f, C_MOV:C_MOV + 256], start=(f == 0), stop=(f == 7)),
                     reads=[("win", f), ("hT", s)], writes=[("ps", b1)])
            for f in range(8):
                p.pe(lambda e, ps2=ps2, f=f, tcs=tcs: e.matmul(ps2[:, 0:64], lhsT=hT[:, f, tcs], rhs=win[:, f, C_NSKV + 192:C_NSKV + 256], start=(f == 0), stop=(f == 7)),
                     reads=[("win", f), ("hT", s)], writes=[("ps", b2)])
            for f in range(8):
                p.pe(lambda e, ps2=ps2, f=f, tcs=tcs: e.matmul(ps2[:, 64:128], lhsT=hT[:, f, tcs], rhs=win[:, f, C_NSKV + 320:C_NSKV + 384], start=(f == 0), stop=(f == 7)),
                     reads=[("win", f), ("hT", s)], writes=[("ps", b2)])
            p.act(lambda e, vs=vs, ps1=ps1: e.copy(out=vs[:, 0:512], in_=ps1[:]), reads=[("ps", b1)], writes=[("vstg", gck % 2)])
            p.dve(lambda e, vs=vs, ps2=ps2: e.tensor_copy(out=vs[:, 512:640], in_=ps2[:, 0:128]), reads=[("ps", b2)], writes=[("vstg", gck % 2)])
            p.dma(io["vsb_d"][tcs, :], vs[:, 0:256], reads=[("vstg", gck % 2)], writes=[("vsb_d", gck)])
            p.dma(io["vmo_d"][tcs, :], vs[:, 256:512], reads=[("vstg", gck % 2)], writes=[("vmo_d", gck)])
            p.dma(io["vsw_d"][tcs, :], vs[:, 512:640], reads=[("vstg", gck % 2)], writes=[("vsw_d", gck)])


A_OUTS = {
    "hT_d": ([1024, TOK], BF16),
    "qsb_d": ([256, TOK], BF16), "ksb_d": ([256, TOK], BF16), "vsb_d": ([TOK, 256], BF16),
    "qmo_d": ([256, TOK], BF16), "kmo_d": ([256, TOK], BF16), "vmo_d": ([TOK, 256], BF16),
    "qml_d": ([4, 96, TOK], BF16), "kml_d": ([4, 64, TOK], BF16), "krl_d": ([32, TOK], BF16), "vml_d": ([TOK, 256], BF16),
    "qns_d": ([256, TOK], BF16), "kcv_d": ([128, TOK], BF16), "ksl_d": ([64, TOK], BF16), "kwi_d": ([64, TOK], BF16),
    "vsw_d": ([TOK, 128], BF16), "gns_d": ([12, TOK], F32), "qme_d": ([256, TOK], BF16),
}


def load_consts(kb, io):
    p = kb.p
    c = {}
    c["identf"] = kb.sb("identf", [128, 128], F32)
    c["identb"] = kb.sb("identb", [128, 128], BF16)
    c["onesb"] = kb.sb("onesb", [128, 128], BF16)
    c["onesf"] = kb.sb("onesf", [128, 128], F32)
    c["cstf"] = kb.sb("cstf", [128, 8], F32)
    p.dma(c["identf"][:], io["c_ident"], writes=["identf"])
    p.dma(c["identb"][:], io["c_ident"], writes=["identb"], q="pool")
    p.dma(c["onesf"][:], io["c_ones"], writes=["onesf"])
    p.dma(c["onesb"][:], io["c_ones"], writes=["onesb"], q="pool")
    p.dma(c["cstf"][:], io["c_cst"], writes=["cst"])
    c["eps_rms"] = c["cstf"][:, 0:1]
    c["eps_ln"] = c["cstf"][:, 1:2]
    c["tiny"] = c["cstf"][:, 2:3]
    c["zrow"] = kb.sb("zrow", [1, 8], BF16)
    p.pool(lambda e: e.memset(c["zrow"][:], 0.0), writes=["zrow"])
    return c


def host_consts():
    cst = np.zeros((128, 8), np.float32)
    cst[:, 0] = RMS_EPS
    cst[:, 1] = LN_EPS
    cst[:, 2] = 1e-30
    cst[:, 3] = 1.0
    return {"c_ident": np.eye(128, dtype=np.float32), "c_ones": np.ones((128, 128), np.float32), "c_cst": cst}


def rope_tables(r):
    half = 16
    freqs = np.power(np.float32(10000.0), -np.arange(half, dtype=np.float32) / half).astype(np.float32)
    rq = np.zeros((NSLOT, 96, 2, 512), np.float32)
    rk = np.zeros((NSLOT, 32, 2, 512), np.float32)
    sc = np.float32(96.0 ** -0.5)
    for s in range(NSLOT):
        pos = (512 * (2 * s + r) + np.arange(512)).astype(np.float32)
        ang = pos[None, :] * freqs[:, None]
        cos, sin = np.cos(ang).astype(np.float32), np.sin(ang).astype(np.float32)
        c2 = np.concatenate([cos, cos], 0)
        s2 = np.concatenate([sin, sin], 0)
        rq[s, 0:64, 0, :] = sc
        rq[s, 64:96, 0, :] = sc * c2
        rq[s, 64:96, 1, :] = sc * s2
        rk[s, :, 0, :] = c2
        rk[s, :, 1, :] = s2
    return rq, rk


def build_a():
    kb = KB()
    io = {}
    io["h_tok"] = kb.din("h_tok", [TOK, D])
    io["c_ident"] = kb.din("c_ident", [128, 128])
    io["c_ones"] = kb.din("c_ones", [128, 128])
    io["c_cst"] = kb.din("c_cst", [128, 8])
    io["ropeq_t"] = kb.din("ropeq_t", [NSLOT, 96, 2, 512])
    io["ropek_t"] = kb.din("ropek_t", [NSLOT, 32, 2, 512])
    w = {"w_in": kb.din("w_in", [D, IN_TOTAL]), "w_uq": kb.din("w_uq", [256, 384]), "g_cq": kb.din("g_cq", [256]),
         "w_ukv": kb.din("w_ukv", [128, 512]), "g_ckv": kb.din("g_ckv", [128])}
    for n, (shp, dt) in A_OUTS.items():
        io[n] = kb.dout(n, shp, dt)
    c = load_consts(kb, io)
    phase_a(kb, c, io, w)
    return kb.finish()


def bconsts_host(r):
    bf = ml_dtypes.bfloat16
    o = {}
    kl = np.arange(128)[:, None]
    ql = np.arange(512)[None, :]
    cm = np.zeros((8, 128, 512), np.float32)
    cms = np.zeros((8, 128, 512), np.float32)
    for jj in range(8):
        kp = 128 * jj + kl
        qp = 512 * r + ql
        cm[jj] = np.where(kp <= qp, 0.0, MASKV)
        cms[jj] = np.where(kp < qp, 0.0, MASKV)
    o["c_cm"] = cm.transpose(1, 0, 2).astype(bf)
    o["c_cms"] = cms.transpose(1, 0, 2).astype(bf)
    wm = np.zeros((12, 128, 512), np.float32)
    for ji, jrel in enumerate(range(-4, 8)):
        dist = 512 * r + ql - 128 * jrel - kl
        wm[ji] = np.where((dist >= 0) & (dist < 512), 0.0, MASKV)
    o["c_wm"] = wm.transpose(1, 0, 2).astype(bf)
    pm = np.zeros((3, 128, 512), np.float32)
    for ii, idx in enumerate((6, 7, 8)):
        pm[ii] = np.where(16 * kl + 31 - 512 * r - ql <= 1024 * (idx - 6), 0.0, MASKV)
    o["c_pm"] = pm.transpose(1, 0, 2).astype(bf)
    kp = np.arange(S)
    o["c_kaug"] = np.stack([kp // 128, kp % 128, np.ones(S), np.ones(S)]).astype(bf)
    ce = 16 * np.arange(512) + 31
    caug = np.stack([ce // 128, ce % 128, np.ones(512), np.ones(512)]).astype(np.float32)
    caug[0, 511] = -30000.0
    o["c_caug"] = caug.astype(bf)
    tl = np.arange(TOK)
    qp = 512 * (2 * (tl // 512) + r) + tl % 512
    qa = np.zeros((4, 4, TOK), np.float32)
    for h in range(4):
        sl = SLOPES[h]
        qa[h, 0] = 128 * sl
        qa[h, 1] = sl
        qa[h, 2] = -sl * 128 * (qp // 128)
        qa[h, 3] = -sl * (qp % 128)
    o["c_qaug"] = qa.astype(bf)
    o["c_g"] = (np.arange(S)[None, :] // 64 == np.arange(128)[:, None]).astype(np.float32).astype(bf)
    o["c_tm"] = (np.arange(S)[None, :] // 256 == np.arange(32)[:, None]).astype(np.float32).astype(bf)
    n = np.arange(512)[:, None]
    s_ = np.arange(128)[None, :]
    ov = ((16 * n < 64 * s_ + 64) & (16 * n + 32 > 64 * s_)).astype(np.float32)
    ov = np.concatenate([ov, np.ones((512, 1), np.float32)], 1)
    ov[511] = 0.0
    o["c_nui"] = -(np.arange(128)[:, None] >= np.arange(128)[None, :]).astype(np.float32).astype(bf)
    o["c_ov"] = ov.reshape(4, 128, 129).transpose(1, 0, 2).astype(bf)
    vb = np.zeros((8, 4, 32), np.float32)
    own_t = np.zeros((8, 4, 32), np.float32)
    for s in range(8):
        for qb in range(4):
            own = (4 * (2 * s + r) + qb) // 2
            vb[s, qb] = np.where(np.arange(32) < own, 0.0, -1e30)
            own_t[s, qb, own] = 1.0
    o["c_vb"] = np.broadcast_to(vb[None], (128, 8, 4, 32)).copy()
    o["c_own"] = np.broadcast_to(own_t[None], (128, 8, 4, 32)).copy()
    M = np.zeros((8, 128, 4, 128), np.float32)
    C = np.zeros((8, 128, 4, 128), np.float32)
    sid = np.arange(128)[None, :]
    for s in range(8):
        for qb in range(4):
            qpos = 512 * (2 * s + r) + 128 * qb + np.arange(128)[:, None]
            cur = qpos // 64
            forced_cur = sid == cur
            forced0 = (sid == 0) & ~forced_cur
            past = (sid < cur) & ~forced0 & ~forced_cur
            M[s, :, qb] = past
            C[s, :, qb] = np.where(forced_cur, 1e30, np.where(forced0, 5e29, np.where(past, 0.0, -1e30)))
    o["c_selm"] = M
    o["c_selc"] = C
    return o


B_CONST_SHAPES = {
    "c_cm": ([128, 8, 512], BF16), "c_cms": ([128, 8, 512], BF16), "c_wm": ([128, 12, 512], BF16), "c_pm": ([128, 3, 512], BF16),
    "c_kaug": ([4, S], BF16), "c_caug": ([4, 512], BF16), "c_qaug": ([4, 4, TOK], BF16),
    "c_g": ([128, S], BF16), "c_tm": ([32, S], BF16), "c_ov": ([128, 4, 129], BF16),
    "c_nui": ([128, 128], BF16), "c_vb": ([128, 8, 4, 32], F32), "c_own": ([128, 8, 4, 32], F32),
    "c_selm": ([8, 128, 4, 128], F32), "c_selc": ([8, 128, 4, 128], F32),
}

B_INS = {
    "qsb_d": ([256, TOK], BF16), "qmo_d": ([256, TOK], BF16), "qml_d": ([4, 96, TOK], BF16), "qns_d": ([256, TOK], BF16),
    "qme_d": ([256, TOK], BF16), "gns_d": ([12, TOK], F32),
    "ksb_f": ([256, S], BF16), "vsb_f": ([S, 256], BF16), "kmo_f": ([256, S], BF16), "vmo_f": ([S, 256], BF16),
    "kml_f": ([4, 64, S], BF16), "krl_f": ([32, S], BF16), "vml_f": ([S, 256], BF16),
    "kcv_f": ([128, S], BF16), "ksl_f": ([64, S], BF16), "kwi_f": ([64, S], BF16), "vsw_f": ([S, 128], BF16),
    "mem": ([256, D], F32),
}


class AttnBufs:
    pass


class KFull:
    def __init__(self, ap):
        self.ap = ap

    def rows(self, lo, hi):
        return ("full", self.ap[lo:hi, :])


class KPair:
    def __init__(self, ap, R, base=0):
        self.ap, self.R, self.base = ap, R, base

    def rows(self, lo, hi):
        return ("pair", self.ap, self.R, self.base + lo, hi - lo)


class VFull:
    def __init__(self, ap, cbase=0):
        self.ap, self.cbase = ap, cbase

    def cols(self, c0):
        return ("full", self.ap, self.cbase + c0)


class VPair:
    def __init__(self, chunks, cbase=0):
        self.chunks, self.cbase = chunks, cbase

    def cols(self, c0):
        return ("pair", self.chunks, self.cbase + c0)


class TMChunks:
    def __init__(self, chunks, c0, c1):
        self.chunks, self.c0, self.c1 = chunks, c0, c1

    def __getitem__(self, key):
        rs, cs = key
        k = rs.start // 1024
        a = self.chunks[k][rs.start - 1024 * k:rs.stop - 1024 * k, self.c0:self.c1]
        return a[:, cs]


def phase_b(kb, c, io, w, branches=("sb", "moba", "mla", "nsa", "mem")):
    nc, p = kb.nc, kb.p
    A = AttnBufs()
    A.KT = [kb.sb("KT", [128, S], BF16) for _ in range(2)]
    A.V = [kb.sb("V", [128, 64, 65], BF16) for _ in range(2)]
    A.QT = kb.sb("QT", [128, 4, TOK], BF16)
    A.cm = kb.sb("cm", [128, 8, 512], BF16)
    A.cms = kb.sb("cms", [128, 8, 512], BF16)
    A.P = [kb.sb("P", [128, 512], BF16) for _ in range(3)]
    A.rden = [kb.sb("rden", [65, 512], F32) for _ in range(2)]
    A.bcs = [kb.sb("bcs", [64, 512], F32) for _ in range(2)]
    A.ost = [kb.sb("ost", [64, 512], BF16) for _ in range(2)]
    A.cnt = {"kt": 0, "v": 0, "P": 0, "fin": 0, "sc": 0}
    p.dma(A.cm[:], io["c_cm"], writes=["cm"])
    p.dma(A.cms[:], io["c_cms"], writes=["cms"])
    for i in range(2):
        p.pool(lambda e, i=i: e.memset(A.V[i][:, :, 64:65], 1.0), writes=[("V", i)])

    def load_K(rows_src, dk, aug=None):
        i = A.cnt["kt"] % 2
        A.cnt["kt"] += 1
        kt = A.KT[i]
        for (src, r0) in rows_src:
            if src[0] == "full":
                n = src[1].shape[0]
                p.dma(kt[r0:r0 + n, :], src[1], writes=[("KT", i)])
            else:
                _, ap, R, row0, n = src
                for rr in range(2):
                    p.dma(kt[r0:r0 + n, :].rearrange("p (s r i) -> p s r i", r=2, i=512)[:, :, rr, :],
                          ap[rr * R + row0:rr * R + row0 + n, :].rearrange("p (s i) -> p s i", i=512), writes=[("KT", i)])
        if aug is not None:
            p.dma(kt[dk:dk + 4, :], aug, writes=[("KT", i)])
        return kt, ("KT", i)

    def load_V(src, col0):
        i = A.cnt["v"] % 2
        A.cnt["v"] += 1
        v = A.V[i]
        sp_ = src.cols(col0)
        if sp_[0] == "full":
            p.dma(v[:, :, 0:64], sp_[1].rearrange("(n p) c -> p n c", p=128)[:, :, sp_[2]:sp_[2] + 64], writes=[("V", i)])
        else:
            _, chunks, cc0 = sp_
            for k, ch in enumerate(chunks):
                for rr in range(2):
                    for s2 in range(2):
                        n0 = 16 * k + 8 * s2 + 4 * rr
                        p.dma(v[:, n0:n0 + 4, 0:64],
                              ch[rr * 1024 + s2 * 512:rr * 1024 + (s2 + 1) * 512, cc0:cc0 + 64].rearrange("(q p) c -> p q c", p=128),
                              writes=[("V", i)])
        return v, ("V", i)

    def load_Q(src_rows, h, dk, aug=None):
        p.dma(A.QT[0:dk, h, :], src_rows, writes=[("QT", h)])
        if aug is not None:
            p.dma(A.QT[dk:dk + 4, h, :], aug, writes=[("QT", h)])
        return ("QT", h)

    def finalize_plain(ops_bank, dst, h, s, gate=None, acc=None, acc_tok=None, first=True, last=True):
        k = A.cnt["fin"] % 2
        A.cnt["fin"] += 1
        ps = kb.ps(ops_bank)
        rd, bcs, ost = A.rden[k], A.bcs[k], A.ost[k]
        bcb = 6 + k

        def part1():
            p.dve(lambda e: e.tensor_scalar(out=rd[64:65, :], in0=ps[64:65, :], scalar1=1e-30, scalar2=None, op0=ALU.max),
                  reads=[("ps", ops_bank)], writes=[("rden", k)])
            p.dve(lambda e: e.reciprocal(out=rd[64:65, :], in_=rd[64:65, :]), reads=[("rden", k)], writes=[("rden", k)])
            if gate is not None:
                gt, gtok, gidx = gate
                p.dve(lambda e: e.tensor_tensor(out=rd[64:65, :], in0=rd[64:65, :], in1=gt[64:65, gidx, :], op=ALU.mult),
                      reads=[("rden", k), gtok], writes=[("rden", k)])

        def part2():
            pb = kb.ps(bcb)
            p.pe(lambda e: e.matmul(pb[0:64, :], lhsT=c["onesf"][64:65, 0:64], rhs=rd[64:65, :], start=True, stop=True),
                 reads=[("rden", k), "onesf"], writes=[("ps", bcb)])
            p.act(lambda e: e.copy(out=bcs[:], in_=pb[0:64, :]), reads=[("ps", bcb)], writes=[("bcs", k)])
            if acc is None:
                p.dve(lambda e: e.tensor_tensor(out=ost[:], in0=ps[0:64, :], in1=bcs[:], op=ALU.mult),
                      reads=[("ps", ops_bank), ("bcs", k)], writes=[("ost", k)])
                p.dma(dst, ost[:], reads=[("ost", k)], writes=[("o_d", h, s, id(dst) % 997)])
            else:
                if first:
                    p.dve(lambda e: e.tensor_tensor(out=acc, in0=ps[0:64, :], in1=bcs[:], op=ALU.mult),
                          reads=[("ps", ops_bank), ("bcs", k)], writes=[acc_tok])
                else:
                    p.dve(lambda e: e.tensor_tensor(out=bcs[:], in0=ps[0:64, :], in1=bcs[:], op=ALU.mult),
                          reads=[("ps", ops_bank), ("bcs", k)], writes=[("bcs", k)])
                    p.dve(lambda e: e.tensor_tensor(out=acc, in0=acc, in1=bcs[:], op=ALU.add),
                          reads=[acc_tok, ("bcs", k)], writes=[acc_tok])
                if last:
                    p.dve(lambda e: e.tensor_copy(out=ost[:], in_=acc), reads=[acc_tok], writes=[("ost", k)])
                    p.dma(dst, ost[:], reads=[("ost", k)], writes=[("o_d", h, s, id(dst) % 997)])
        return part1, part2

    def run_softmax(items, KT, ktok, dk, V, vtok, qh, qtok, pending, hook=None):
        n = len(items)

        def stage1(i):
            it = items[i]
            if "pre" in it:
                it["pre"]()
            b = i % 2
            ps = kb.ps(b)
            ex = it["extras"]
            s, j = it["s"], it["j"]
            p.pe(lambda e: e.matmul(ps[:], lhsT=KT[0:dk, j * 128:(j + 1) * 128], rhs=A.QT[0:dk, qh, s * 512:(s + 1) * 512],
                                    start=True, stop=(len(ex) == 0)),
                 reads=[ktok, qtok], writes=[("ps", b)])
            for xi, (lh, rh, toks) in enumerate(ex):
                p.pe(lambda e, lh=lh, rh=rh, xi=xi: e.matmul(ps[:], lhsT=lh, rhs=rh, start=False, stop=(xi == len(ex) - 1)),
                     reads=list(toks), writes=[("ps", b)])
            if "pdst" in it:
                P, ptok = it["pdst"]
            else:
                pk = A.cnt["P"] % 3
                A.cnt["P"] += 1
                P, ptok = A.P[pk][:], ("P", pk)
            it["P"], it["ptok"] = P, ptok
            p.act(lambda e: e.activation(out=P, in_=ps[:], func=AF.Exp), reads=[("ps", b)], writes=[ptok])

        def stage2(i):
            it = items[i]
            P, ptok = it["P"], it["ptok"]
            ob = it["obank"]
            po = kb.ps(ob)
            jv = it.get("jv", it["j"])
            p.pe(lambda e: e.matmul(po[0:65, :], lhsT=V[:, jv, 0:65], rhs=P, start=it["first"], stop=it["last"]),
                 reads=[vtok, ptok], writes=[("ps", ob)])
            if it["last"]:
                p1, p2 = it["fin"]
                p1()
                pending.append([2, p2])

        for i in range(n + 1):
            if i < n:
                stage1(i)
            if i >= 1:
                stage2(i - 1)
            for pd in list(pending):
                pd[0] -= 1
                if pd[0] <= 0:
                    pd[1]()
                    pending.remove(pd)

    def flush(pending):
        for pd in pending:
            pd[1]()
        pending.clear()

    A.load_K, A.load_V, A.load_Q = load_K, load_V, load_Q
    A.finalize_plain, A.run_softmax, A.flush = finalize_plain, run_softmax, flush
    oT = io["oT_d"]

    if "mla" in branches:
        pending = []
        for h in range(4):
            KT, ktok = load_K([(io["kml_f"].rows(h * 64, h * 64 + 64), 0), (io["krl_f"].rows(0, 32), 64)], 96)
            V, vtok = load_V(io["vml_f"], h * 64)
            qtok = load_Q(io["qml_d"][h], h, 96)
            items = []
            for s in range(NSLOT):
                nkb = 8 * s + 8
                ob = 4 + (s % 2)
                for j in range(nkb):
                    jj = j - 8 * s
                    ex = []
                    if jj >= 0:
                        ex.append((c["identb"][:], A.cm[:, jj, :], ["identb", "cm"]))
                    it = dict(j=j, s=s, extras=ex, first=(j == 0), last=(j == nkb - 1), obank=ob)
                    if j == nkb - 1:
                        it["fin"] = finalize_plain(ob, oT[2, h * 64:(h + 1) * 64, s * 512:(s + 1) * 512], h, s)
                    items.append(it)
            run_softmax(items, KT, ktok, 96, V, vtok, h, qtok, pending)
        flush(pending)

    if "mem" in branches:
        pending = []
        memst = kb.sb("memst", [128, 2, 1024], F32)
        memT = kb.sb("memT", [128, 8, 256], BF16)
        wmk = kb.sb("wmk", [128, 8, 512], BF16)
        KTm = kb.sb("KTm", [64, 4, 256], BF16)
        Vm = kb.sb("Vm", [128, 2, 4, 65], BF16)
        p.dma(memst[:], io["mem"].rearrange("(n p) c -> p n c", p=128), writes=["memst"])
        p.dma(wmk[:], w["w_mem_kv"].rearrange("(f p) c -> p f c", p=128), writes=["wmk"], q="pool")
        p.pool(lambda e: e.memset(Vm[:, :, :, 64:65], 1.0), writes=["Vm"])
        for kc in range(2):
            for half in range(2):
                ps = kb.ps(7)
                for jx in range(4):
                    f = half * 4 + jx
                    p.pe(lambda e, ps=ps, kc=kc, f=f, jx=jx: e.transpose(out=ps[:, jx * 128:(jx + 1) * 128], in_=memst[:, kc, f * 128:(f + 1) * 128], identity=c["identf"][:]),
                         reads=["memst", "identf"], writes=[("ps", 7)])
                p.dve(lambda e, ps=ps, kc=kc, half=half: e.tensor_copy(out=memT[:, half * 4:half * 4 + 4, kc * 128:(kc + 1) * 128], in_=ps[:].rearrange("p (j t) -> p j t", j=4)),
                      reads=[("ps", 7)], writes=["memT"])
        for h in range(4):
            ps = kb.ps(7)
            for f in range(8):
                p.pe(lambda e, ps=ps, f=f, h=h: e.matmul(ps[0:64, 0:256], lhsT=wmk[:, f, h * 64:(h + 1) * 64], rhs=memT[:, f, :], start=(f == 0), stop=(f == 7)),
                     reads=["wmk", "memT"], writes=[("ps", 7)])
            p.dve(lambda e, ps=ps, h=h: e.tensor_copy(out=KTm[:, h, :], in_=ps[0:64, 0:256]), reads=[("ps", 7)], writes=["KTm"])
        for kc in range(2):
            ps = kb.ps(7)
            for f in range(8):
                p.pe(lambda e, ps=ps, f=f, kc=kc: e.matmul(ps[:, 0:256], lhsT=memT[:, f, kc * 128:(kc + 1) * 128], rhs=wmk[:, f, 256:512], start=(f == 0), stop=(f == 7)),
                     reads=["wmk", "memT"], writes=[("ps", 7)])
            p.dve(lambda e, ps=ps, kc=kc: e.tensor_copy(out=Vm[:, kc, :, 0:64], in_=ps[:, 0:256].rearrange("p (h d) -> p h d", h=4)),
                  reads=[("ps", 7)], writes=["Vm"])
        for h in range(4):
            qtok = load_Q(io["qme_d"][h * 64:(h + 1) * 64, :], h, 64)
            items = []
            for s in range(NSLOT):
                ob = 4 + (s % 2)
                for j in range(2):
                    it = dict(j=j, s=s, extras=[], first=(j == 0), last=(j == 1), obank=ob)
                    if j == 1:
                        it["fin"] = finalize_plain(ob, oT[4, h * 64:(h + 1) * 64, s * 512:(s + 1) * 512], h, s)
                    items.append(it)
            run_softmax(items, KTm[:, h, :], "KTm", 64, Vm[:, :, h, :], "Vm", h, qtok, pending)
        flush(pending)

    if "moba" in branches:
        pending = []
        tm = kb.sb("tm", [32, S], BF16)
        vbt = kb.sb("vbt", [128, 8, 4, 32], F32)
        ownt = kb.sb("ownt", [128, 8, 4, 32], F32)
        kmf = kb.sb("kmf", [64, 32], F32)
        kmb = kb.sb("kmb", [64, 32], BF16)
        gsv = kb.sb("gsv", [128, 4, 32], F32)
        m8 = kb.sb("m8", [128, 4, 8], F32)
        m1 = kb.sb("m1", [128, 4, 32], F32)
        m2 = kb.sb("m2", [128, 4, 32], F32)
        MT = [kb.sb("MT", [32, 512], BF16) for _ in range(2)]
        p.dma(tm[:], io["c_tm"], writes=["tm"])
        p.dma(vbt[:], io["c_vb"], writes=["vbt"])
        p.dma(ownt[:], io["c_own"], writes=["ownt"])
        for h in range(4):
            KT, ktok = load_K([(io["kmo_f"].rows(h * 64, h * 64 + 64), 0)], 64, aug=io["c_kaug"])
            V, vtok = load_V(io["vmo_f"], h * 64)
            qtok = load_Q(io["qmo_d"][h * 64:(h + 1) * 64, :], h, 64, aug=io["c_qaug"][h])
            p.dve(lambda e, KT=KT: e.tensor_reduce(out=kmf[:], in_=KT[0:64, :].rearrange("p (n k) -> p n k", k=256), axis=AX.X, op=ALU.add),
                  reads=[ktok], writes=["kmf"])
            p.dve(lambda e: e.tensor_scalar_mul(out=kmb[:], in0=kmf[:], scalar1=1.0 / 256.0), reads=["kmf"], writes=["kmb"])

            def make_pre(s, h=h, qtok=qtok):
                def pre():
                    mt = MT[s % 2]
                    ps = kb.ps(7)
                    for qb in range(4):
                        c0 = s * 512 + qb * 128
                        p.pe(lambda e, qb=qb, c0=c0: e.matmul(ps[:, qb * 32:(qb + 1) * 32], lhsT=A.QT[0:64, h, c0:c0 + 128], rhs=kmb[:], start=True, stop=True),
                             reads=[qtok, "kmb"], writes=[("ps", 7)])
                    p.dve(lambda e: e.tensor_tensor(out=gsv[:], in0=ps[:, 0:128].rearrange("p (a b) -> p a b", a=4), in1=vbt[:, s, :, :], op=ALU.add),
                          reads=[("ps", 7), "vbt"], writes=["gsv"])
                    for qb in range(4):
                        p.dve(lambda e, qb=qb: e.max(out=m8[:, qb, :], in_=gsv[:, qb, :]), reads=["gsv"], writes=["m8"])
                    for qb in range(4):
                        p.dve(lambda e, qb=qb: e.tensor_scalar(out=m1[:, qb, :], in0=gsv[:, qb, :], scalar1=m8[:, qb, 2:3], scalar2=None, op0=ALU.is_ge),
                              reads=["gsv", "m8"], writes=["m1"])
                    p.dve(lambda e: e.tensor_scalar(out=m2[:], in0=gsv[:], scalar1=-1e29, scalar2=None, op0=ALU.is_gt), reads=["gsv"], writes=["m2"])
                    p.dve(lambda e: e.tensor_tensor(out=m1[:], in0=m1[:], in1=m2[:], op=ALU.mult), reads=["m1", "m2"], writes=["m1"])
                    p.dve(lambda e: e.tensor_tensor(out=m1[:], in0=m1[:], in1=ownt[:, s, :, :], op=ALU.add), reads=["m1", "ownt"], writes=["m1"])
                    p.dve(lambda e: e.tensor_scalar(out=m1[:], in0=m1[:], scalar1=1.0, scalar2=-MASKV, op0=ALU.subtract, op1=ALU.mult),
                          reads=["m1"], writes=["m1"])
                    ps2 = kb.ps(7)
                    for qb in range(4):
                        p.pe(lambda e, qb=qb: e.transpose(out=ps2[0:32, qb * 128:(qb + 1) * 128], in_=m1[:, qb, :], identity=c["identf"][:]),
                             reads=["m1", "identf"], writes=[("ps", 7)])
                    p.act(lambda e: e.copy(out=mt[:], in_=ps2[0:32, :]), reads=[("ps", 7)], writes=[("MT", s % 2)])
                return pre

            items = []
            for s in range(NSLOT):
                nkb = 8 * s + 8
                ob = 4 + (s % 2)
                mt = MT[s % 2]
                for j in range(nkb):
                    jj = j - 8 * s
                    ex = [(tm[0:32, j * 128:(j + 1) * 128], mt[:], ["tm", ("MT", s % 2)])]
                    if jj >= 0:
                        ex.append((c["identb"][:], A.cm[:, jj, :], ["identb", "cm"]))
                    it = dict(j=j, s=s, extras=ex, first=(j == 0), last=(j == nkb - 1), obank=ob)
                    if j == 0:
                        it["pre"] = make_pre(s)
                    if j == nkb - 1:
                        it["fin"] = finalize_plain(ob, oT[1, h * 64:(h + 1) * 64, s * 512:(s + 1) * 512], h, s)
                    items.append(it)
            run_softmax(items, KT, ktok, 68, V, vtok, h, qtok, pending)
        flush(pending)

    if "sb" in branches:
        nui = kb.sb("nui", [128, 128], BF16)
        negone = kb.sb("negone", [1, 128], BF16)
        p.dma(nui[:], io["c_nui"], writes=["nui"])
        p.pool(lambda e: e.memset(negone[:], -1.0), writes=["negone"])
        E = [kb.sb("E", [128, 512], F32) for _ in range(2)]
        SP = [kb.sb("SP", [128, 512], BF16) for _ in range(2)]
        AB = [kb.sb("AB", [128, 512], BF16) for _ in range(2)]
        carf = [kb.sb("carf", [1, 512], F32) for _ in range(2)]
        carb = [kb.sb("carb", [1, 512], BF16) for _ in range(2)]
        sbo = [kb.sb("sbo", [64, 512], BF16) for _ in range(2)]
        one_ap = c["cstf"][:, 3:4]
        for hp in range(2):
            hs = (2 * hp, 2 * hp + 1)
            KTs, ktoks, Vs, vtoks, qtoks = [], [], [], [], []
            for h in hs:
                KT, ktok = load_K([(io["ksb_f"].rows(h * 64, h * 64 + 64), 0)], 64)
                V, vtok = load_V(io["vsb_f"], h * 64)
                qtok = load_Q(io["qsb_d"][h * 64:(h + 1) * 64, :], h, 64)
                KTs.append(KT); ktoks.append(ktok); Vs.append(V); vtoks.append(vtok); qtoks.append(qtok)
            merged = []
            for s_ in range(NSLOT):
                nkb = 8 * s_ + 8
                for j in range(nkb - 1, -1, -1):
                    for st in range(2):
                        merged.append(dict(j=j, s=s_, st=st, h=hs[st], first=(j == nkb - 1), last=(j == 0)))
            for i, it in enumerate(merged):
                it["i"] = i

            def qk(it, bank, more):
                ps = kb.ps(bank)
                j, s_, st, h = it["j"], it["s"], it["st"], it["h"]
                jj = j - 8 * s_
                KT = KTs[st]
                p.pe(lambda e: e.matmul(ps[:], lhsT=KT[0:64, j * 128:(j + 1) * 128], rhs=A.QT[0:64, h, s_ * 512:(s_ + 1) * 512],
                                        start=True, stop=(jj < 0 and not more)),
                     reads=[ktoks[st], qtoks[st]], writes=[("ps", bank)])
                if jj >= 0:
                    p.pe(lambda e: e.matmul(ps[:], lhsT=c["identb"][:], rhs=A.cms[:, jj, :], start=False, stop=(not more)),
                         reads=["identb", "cms"], writes=[("ps", bank)])
                return ps

            def s1(it):
                k = it["i"] % 2
                ps = qk(it, k, False)
                p.act(lambda e: e.activation(out=E[k][:], in_=ps[:], func=AF.Exp), reads=[("ps", k)], writes=[("E", k)])
                p.act(lambda e: e.activation(out=SP[k][:], in_=E[k][:], func=AF.Ln, bias=one_ap), reads=[("E", k), "cst"], writes=[("SP", k)])

            def s2a(it):
                k = it["i"] % 2
                st = it["st"]
                ps = qk(it, 2 + k, True)
                fin_carry = not it["first"]
                p.pe(lambda e: e.matmul(ps[:], lhsT=nui[:], rhs=SP[k][:], start=False, stop=(not fin_carry)),
                     reads=["nui", ("SP", k)], writes=[("ps", 2 + k)])
                if fin_carry:
                    p.pe(lambda e: e.matmul(ps[:], lhsT=negone[0:1, :], rhs=carb[st][0:1, :], start=False, stop=True),
                         reads=["negone", ("carb", st)], writes=[("ps", 2 + k)])
                if not it["last"]:
                    pc = kb.ps(6 + st)
                    p.pe(lambda e: e.matmul(pc[0:1, :], lhsT=c["onesb"][:, 0:1], rhs=SP[k][:], start=True, stop=True),
                         reads=["onesb", ("SP", k)], writes=[("ps", 6 + st)])
                    if it["first"]:
                        p.dve(lambda e: e.tensor_copy(out=carf[st][:], in_=pc[0:1, :]), reads=[("ps", 6 + st)], writes=[("carf", st)])
                    else:
                        p.dve(lambda e: e.tensor_tensor(out=carf[st][:], in0=carf[st][:], in1=pc[0:1, :], op=ALU.add),
                              reads=[("ps", 6 + st), ("carf", st)], writes=[("carf", st)])
                    p.dve(lambda e: e.tensor_copy(out=carb[st][:], in_=carf[st][:]), reads=[("carf", st)], writes=[("carb", st)])
                p.act(lambda e: e.activation(out=AB[k][:], in_=ps[:], func=AF.Exp), reads=[("ps", 2 + k)], writes=[("AB", k)])

            def s2b(it):
                k = it["i"] % 2
                st, j, s_, h = it["st"], it["j"], it["s"], it["h"]
                po = kb.ps(4 + st)
                V = Vs[st]
                p.pe(lambda e: e.matmul(po[0:64, :], lhsT=V[:, j, 0:64], rhs=AB[k][:], start=it["first"], stop=it["last"]),
                     reads=[vtoks[st], ("AB", k)], writes=[("ps", 4 + st)])
                if it["last"]:
                    p.dve(lambda e: e.tensor_copy(out=sbo[st][:], in_=po[0:64, :]), reads=[("ps", 4 + st)], writes=[("sbo", st)])
                    p.dma(oT[0, h * 64:(h + 1) * 64, s_ * 512:(s_ + 1) * 512], sbo[st][:], reads=[("sbo", st)], writes=[("o_sb", h, s_)])

            n = len(merged)
            for i in range(n + 2):
                if i < n:
                    s1(merged[i])
                if 1 <= i <= n:
                    s2a(merged[i - 1])
                if i >= 2:
                    s2b(merged[i - 2])

    if "nsa" in branches:
        pending = []
        G = kb.sb("G", [128, S], BF16)
        OV = kb.sb("OV", [128, 4, 129], BF16)
        pm = kb.sb("pm", [128, 3, 512], BF16)
        wm = kb.sb("wm", [128, 12, 512], BF16)
        wphi = kb.sb("wphi", [128, 32, 128], BF16)
        w2 = kb.sb("w2", [128, 2, 64], BF16)
        peT = kb.sb("peT", [128, 32], BF16)
        peb = kb.sb("peb", [128, 2], F32)
        gx = kb.sb("gx", [128, 512], F32)
        gt_ = kb.sb("gtmp", [128, 512], F32)
        gact = [kb.sb("gact", [128, 512], BF16) for _ in range(2)]
        kcT = kb.sb("kcT", [68, 512], BF16)
        Vc = kb.sb("Vc", [128, 4, 65], BF16)
        psave = kb.sb("psave", [128, 4, 4, 512], BF16)
        gts = [kb.sb("gts", [65, 512], F32) for _ in range(4)]
        nacc = kb.sb("nacc", [64, 4, 512], F32)
        impacc = kb.sb("impacc", [128, 4, 128], F32)
        rdn = kb.sb("rdn", [128, 2], F32)
        selm = [kb.sb("selm", [128, 4, 128], F32) for _ in range(2)]
        selc = [kb.sb("selc", [128, 4, 128], F32) for _ in range(2)]
        scr2 = kb.sb("scr2", [128, 4, 128], F32)
        m16 = kb.sb("m16", [128, 4, 16], F32)
        selv = kb.sb("selv", [128, 4, 128], F32)
        SelT = [kb.sb("SelT", [128, 512], BF16) for _ in range(2)]
        gcnt = {"g": 0}
        p.dma(G[:], io["c_g"], writes=["G"])
        p.dma(OV[:], io["c_ov"], writes=["OV"])
        p.dma(pm[:], io["c_pm"], writes=["pm"])
        p.dma(wm[:], io["c_wm"], writes=["wm"])
        p.dma(wphi[0:64, :, :], w["w_phi_k1"].rearrange("(t d) h -> d t h", d=64), writes=["wphi"], q="pool")
        p.dma(wphi[64:128, :, :], w["w_phi_v1"].rearrange("(t d) h -> d t h", d=64), writes=["wphi"], q="pool")
        p.dma(w2[:, 0, :], w["w_phi_k2"], writes=["w2"], q="pool")
        p.dma(w2[:, 1, :], w["w_phi_v2"], writes=["w2"], q="pool")
        p.dma(peT[0:64, :], w["nsa_pe"].rearrange("t d -> d t"), writes=["peT"], q="pool", slow=True)
        p.dma(peT[64:128, :], w["nsa_pe"].rearrange("t d -> d t"), writes=["peT"], q="pool", slow=True)
        kcv, kcvtok = load_K([(io["kcv_f"].rows(0, 128), 0)], 128)
        p.pool(lambda e: e.memset(kcT[:], 0.0), writes=["kcT"])
        p.pool(lambda e: e.memset(Vc[:, :, 64:65], 1.0), writes=["Vc"])
        for which in range(2):
            lo = which * 64
            pb = kb.ps(7)
            for t in range(32):
                p.pe(lambda e, t=t, lo=lo, pb=pb: e.matmul(pb[:, 0:1], lhsT=wphi[lo:lo + 64, t, :], rhs=peT[lo:lo + 64, t:t + 1], start=(t == 0), stop=(t == 31)),
                     reads=["wphi", "peT"], writes=[("ps", 7)])
            p.dve(lambda e, pb=pb, which=which: e.tensor_copy(out=peb[:, which:which + 1], in_=pb[:, 0:1]), reads=[("ps", 7)], writes=["peb"])
            ph = kb.ps(6)
            for t in range(32):
                p.pe(lambda e, t=t, lo=lo, ph=ph: e.matmul(ph[:, 0:511], lhsT=wphi[lo:lo + 64, t, :], rhs=kcv[lo:lo + 64, t:t + 16 * 510 + 1:16], start=(t == 0), stop=(t == 31)),
                     reads=["wphi", kcvtok], writes=[("ps", 6)])
            ga = gact[which]
            p.act(lambda e, ph=ph, which=which: e.activation(out=gx[:, 0:511], in_=ph[:, 0:511], func=AF.Identity, bias=peb[:, which:which + 1]),
                  reads=[("ps", 6), "peb"], writes=["gx"])
            p.dve(lambda e: e.tensor_tensor(out=gt_[:, 0:511], in0=gx[:, 0:511], in1=gx[:, 0:511], op=ALU.mult), reads=["gx"], writes=["gtmp"])
            p.dve(lambda e: e.tensor_scalar(out=gt_[:, 0:511], in0=gt_[:, 0:511], scalar1=0.044715, scalar2=1.0, op0=ALU.mult, op1=ALU.add), reads=["gtmp"], writes=["gtmp"])
            p.dve(lambda e: e.tensor_tensor(out=gt_[:, 0:511], in0=gt_[:, 0:511], in1=gx[:, 0:511], op=ALU.mult), reads=["gtmp", "gx"], writes=["gtmp"])
            p.act(lambda e: e.activation(out=gt_[:, 0:511], in_=gt_[:, 0:511], func=AF.Tanh, scale=0.7978845608028654), reads=["gtmp"], writes=["gtmp"])
            p.dve(lambda e: e.tensor_scalar(out=gt_[:, 0:511], in0=gt_[:, 0:511], scalar1=1.0, scalar2=0.5, op0=ALU.add, op1=ALU.mult), reads=["gtmp"], writes=["gtmp"])
            p.pool(lambda e, ga=ga: e.memset(ga[:], 0.0), writes=[("gact", which)])
            p.dve(lambda e, ga=ga: e.tensor_tensor(out=ga[:, 0:511], in0=gt_[:, 0:511], in1=gx[:, 0:511], op=ALU.mult), reads=["gtmp", "gx"], writes=[("gact", which)])
        pk_ = kb.ps(7)
        p.pe(lambda e: e.matmul(pk_[0:64, :], lhsT=w2[:, 0, :], rhs=gact[0][:], start=True, stop=True), reads=["w2", ("gact", 0)], writes=[("ps", 7)])
        p.dve(lambda e: e.tensor_copy(out=kcT[0:64, 0:511], in_=pk_[0:64, 0:511]), reads=[("ps", 7)], writes=["kcT"])
        p.dma(kcT[64:68, :], io["c_caug"], writes=["kcT"])
        pv_ = kb.ps(6)
        for cc in range(4):
            p.pe(lambda e, cc=cc: e.matmul(pv_[:, cc * 64:(cc + 1) * 64], lhsT=gact[1][:, cc * 128:(cc + 1) * 128], rhs=w2[:, 1, :], start=True, stop=True),
                 reads=["w2", ("gact", 1)], writes=[("ps", 6)])
        p.dve(lambda e: e.tensor_copy(out=Vc[:, :, 0:64], in_=pv_[:, 0:256].rearrange("p (c d) -> p c d", c=4)), reads=[("ps", 6)], writes=["Vc"])

        KsT, kstok = load_K([(io["ksl_f"].rows(0, 64), 0)], 64, aug=io["c_kaug"])
        KwT, kwtok = load_K([(io["kwi_f"].rows(0, 64), 0)], 64, aug=io["c_kaug"])
        Vs, vstok = load_V(io["vsw_f"], 0)
        Vw, vwtok = load_V(io["vsw_f"], 64)
        qtoks = [load_Q(io["qns_d"][h * 64:(h + 1) * 64, :], h, 64, aug=io["c_qaug"][h]) for h in range(4)]

        def gate_row(h, br, s):
            k = gcnt["g"] % 4
            gcnt["g"] += 1
            g = gts[k]
            p.dma(g[64:65, :], io["gns_d"][3 * h + br:3 * h + br + 1, s * 512:(s + 1) * 512], writes=[("gts", k)])
            return (g[:].rearrange("p (o n) -> p o n", o=1), ("gts", k), 0)

        for s in range(NSLOT):
            sl = slice(s * 512, (s + 1) * 512)
            ncmp = s // 2 + 1
            dst = lambda h: oT[3, h * 64:(h + 1) * 64, sl]
            p.dma(selm[s % 2][:], io["c_selm"][s], writes=[("selm", s % 2)])
            p.dma(selc[s % 2][:], io["c_selc"][s], writes=[("selc", s % 2)])
            for h in range(4):
                items = []
                for cc in range(ncmp):
                    idx = s - 2 * cc + 6
                    ex = []
                    if idx <= 8:
                        ex.append((c["identb"][:], pm[:, idx - 6, :], ["identb", "pm"]))
                    it = dict(j=cc, s=s, extras=ex, first=(cc == 0), last=(cc == ncmp - 1), obank=4 + (h % 2),
                              pdst=(psave[:, h, cc, :], ("psave", h, cc)))
                    if cc == ncmp - 1:
                        it["fin"] = finalize_plain(4 + (h % 2), dst(h), h, s, gate=gate_row(h, 0, s), acc=nacc[:, h, :], acc_tok=("nacc", h), first=True, last=False)
                    items.append(it)
                run_softmax(items, kcT, "kcT", 68, Vc, "Vc", h, qtoks[h], pending)
            flush(pending)
            for qb in range(4):
                for h in range(4):
                    bk = 6 + ((qb * 4 + h) % 2)
                    pi = kb.ps(bk)
                    for cc in range(ncmp):
                        p.pe(lambda e, pi=pi, h=h, cc=cc, qb=qb: e.matmul(pi[:, 0:129], lhsT=psave[:, h, cc, qb * 128:(qb + 1) * 128], rhs=OV[:, cc, :],
                                                                          start=(cc == 0), stop=(cc == ncmp - 1)),
                             reads=[("psave", h, cc), "OV"], writes=[("ps", bk)])
                    p.dve(lambda e, pi=pi: e.tensor_scalar(out=rdn[:, 0:1], in0=pi[:, 128:129], scalar1=1e-30, scalar2=None, op0=ALU.max),
                          reads=[("ps", bk)], writes=["rdn"])
                    p.dve(lambda e: e.reciprocal(out=rdn[:, 1:2], in_=rdn[:, 0:1]), reads=["rdn"], writes=["rdn"])
                    if h == 0:
                        p.dve(lambda e, pi=pi, qb=qb: e.tensor_scalar(out=impacc[:, qb, :], in0=pi[:, 0:128], scalar1=rdn[:, 1:2], scalar2=None, op0=ALU.mult),
                              reads=[("ps", bk), "rdn"], writes=["impacc"])
                    else:
                        p.dve(lambda e, pi=pi, qb=qb: e.scalar_tensor_tensor(out=impacc[:, qb, :], in0=pi[:, 0:128], scalar=rdn[:, 1:2], in1=impacc[:, qb, :],
                                                                             op0=ALU.mult, op1=ALU.add),
                              reads=[("ps", bk), "rdn", "impacc"], writes=["impacc"])
            sm, sc_ = selm[s % 2], selc[s % 2]
            p.dve(lambda e, sm=sm: e.tensor_tensor(out=impacc[:], in0=impacc[:], in1=sm[:], op=ALU.mult), reads=["impacc", ("selm", s % 2)], writes=["impacc"])
            p.dve(lambda e, sc_=sc_: e.tensor_tensor(out=impacc[:], in0=impacc[:], in1=sc_[:], op=ALU.add), reads=["impacc", ("selc", s % 2)], writes=["impacc"])
            for qb in range(4):
                p.dve(lambda e, qb=qb: e.max(out=m16[:, qb, 0:8], in_=impacc[:, qb, :]), reads=["impacc"], writes=["m16"])
                p.dve(lambda e, qb=qb: e.match_replace(out=scr2[:, qb, :], in_to_replace=m16[:, qb, 0:8], in_values=impacc[:, qb, :], imm_value=-3.0e38),
                      reads=["impacc", "m16"], writes=["scr2"])
                p.dve(lambda e, qb=qb: e.max(out=m16[:, qb, 8:16], in_=scr2[:, qb, :]), reads=["scr2"], writes=["m16"])
                p.dve(lambda e, qb=qb: e.tensor_scalar(out=selv[:, qb, :], in0=impacc[:, qb, :], scalar1=m16[:, qb, 15:16], scalar2=None, op0=ALU.is_ge),
                      reads=["impacc", "m16"], writes=["selv"])
            p.dve(lambda e: e.tensor_scalar(out=scr2[:], in0=impacc[:], scalar1=-5e29, scalar2=None, op0=ALU.is_gt), reads=["impacc"], writes=["scr2"])
            p.dve(lambda e: e.tensor_tensor(out=selv[:], in0=selv[:], in1=scr2[:], op=ALU.mult), reads=["selv", "scr2"], writes=["selv"])
            p.dve(lambda e: e.tensor_scalar(out=selv[:], in0=selv[:], scalar1=1.0, scalar2=-MASKV, op0=ALU.subtract, op1=ALU.mult), reads=["selv"], writes=["selv"])
            for h in range(4):
                items = []
                js = [j for j in range(8 * s - 4, 8 * s + 8) if j >= 0]
                for j in js:
                    jrel = j - 8 * s
                    it = dict(j=j, s=s, extras=[(c["identb"][:], wm[:, jrel + 4, :], ["identb", "wm"])], first=(j == js[0]), last=(j == js[-1]), obank=4 + (h % 2))
                    if j == js[-1]:
                        it["fin"] = finalize_plain(4 + (h % 2), dst(h), h, s, gate=gate_row(h, 2, s), acc=nacc[:, h, :], acc_tok=("nacc", h), first=False, last=False)
                    items.append(it)
                run_softmax(items, KwT, kwtok, 68, Vw, vwtok, h, qtoks[h], pending)
            flush(pending)
            st_ = SelT[s % 2]
            pt = kb.ps(7)
            for qb in range(4):
                p.pe(lambda e, qb=qb: e.transpose(out=pt[:, qb * 128:(qb + 1) * 128], in_=selv[:, qb, :], identity=c["identf"][:]),
                     reads=["selv", "identf"], writes=[("ps", 7)])
            p.act(lambda e, st_=st_: e.copy(out=st_[:], in_=pt[:]), reads=[("ps", 7)], writes=[("SelT", s % 2)])
            for h in range(4):
                items = []
                nkb = 8 * s + 8
                for j in range(nkb):
                    jj = j - 8 * s
                    ex = [(G[:, j * 128:(j + 1) * 128], st_[:], ["G", ("SelT", s % 2)])]
                    if jj >= 0:
                        ex.append((c["identb"][:], A.cm[:, jj, :], ["identb", "cm"]))
                    it = dict(j=j, s=s, extras=ex, first=(j == 0), last=(j == nkb - 1), obank=4 + (h % 2))
                    if j == nkb - 1:
                        it["fin"] = finalize_plain(4 + (h % 2), dst(h), h, s, gate=gate_row(h, 1, s), acc=nacc[:, h, :], acc_tok=("nacc", h), first=False, last=True)
                    items.append(it)
                run_softmax(items, KsT, kstok, 68, Vs, vstok, h, qtoks[h], pending)
            flush(pending)
    return A


B_WEIGHTS = {"w_mem_kv": [D, 512], "nsa_pe": [32, 64], "w_phi_k1": [2048, 128], "w_phi_k2": [128, 64],
             "w_phi_v1": [2048, 128], "w_phi_v2": [128, 64]}


def build_b(branches):
    kb = KB()
    io = {}
    for n in ("c_ident", "c_ones"):
        io[n] = kb.din(n, [128, 128])
    io["c_cst"] = kb.din("c_cst", [128, 8])
    for n, (shp, dt) in B_CONST_SHAPES.items():
        io[n] = kb.din(n, shp, dt)
    for n, (shp, dt) in B_INS.items():
        io[n] = kb.din(n, shp, dt)
    w = {n: kb.din(n, shp) for n, shp in B_WEIGHTS.items()}
    io["oT_d"] = kb.dout("oT_d", [5, 256, TOK], BF16)
    io["kml_f"] = KFull(io["kml_f"].rearrange("h d t -> (h d) t"))
    for n in ("ksb_f", "kmo_f", "krl_f", "kcv_f", "ksl_f", "kwi_f"):
        io[n] = KFull(io[n])
    for n in ("vsb_f", "vmo_f", "vml_f", "vsw_f"):
        io[n] = VFull(io[n])
    c = load_consts(kb, io)
    phase_b(kb, c, io, w, branches)
    return kb.finish()


def layer_norm_chunk(kb, c, v, vtok, gbc, bbc, out, otok, tmp):
    p = kb.p
    st, mv, sm = tmp["st"], tmp["mv"], tmp["sm"]
    for hf in range(2):
        p.dve(lambda e, hf=hf: e.bn_stats(out=st[:, hf, :], in_=v[:, hf * 512:(hf + 1) * 512]), reads=[vtok], writes=["lnst"])
    p.dve(lambda e: e.bn_aggr(out=mv[:], in_=st[:]), reads=["lnst"], writes=["lnmv"])
    p.act(lambda e: e.activation(out=sm[:, 0:1], in_=mv[:, 1:2], func=AF.Ln, bias=c["eps_ln"]), reads=["lnmv", "cst"], writes=["lnsm"])
    p.act(lambda e: e.activation(out=sm[:, 0:1], in_=sm[:, 0:1], func=AF.Exp, scale=-0.5), reads=["lnsm"], writes=["lnsm"])
    p.dve(lambda e: e.scalar_tensor_tensor(out=sm[:, 1:2], in0=mv[:, 0:1], scalar=-1.0, in1=sm[:, 0:1], op0=ALU.mult, op1=ALU.mult),
          reads=["lnmv", "lnsm"], writes=["lnsm2"])
    p.act(lambda e: e.activation(out=v[:], in_=v[:], func=AF.Identity, scale=sm[:, 0:1], bias=sm[:, 1:2]), reads=[vtok, "lnsm", "lnsm2"], writes=[vtok])
    p.dve(lambda e: e.tensor_tensor(out=v[:], in0=v[:], in1=gbc[:], op=ALU.mult), reads=[vtok, "lngb"], writes=[vtok])
    p.dve(lambda e: e.tensor_tensor(out=out[:], in0=v[:], in1=bbc[:], op=ALU.add), reads=[vtok, "lngb"], writes=[otok])


def phase_c1(kb, c, io, w):
    nc, p = kb.nc, kb.p
    wg = kb.sb("wg", [128, 5, 8, 1024], BF16)
    wbr = kb.sb("wbr", [128, 5, 2, 1024], BF16)
    wout = kb.sb("wout", [128, 8, 1024], BF16)
    bg = kb.sb("bg", [128, 5, 8], F32)
    gbc = kb.sb("gbc", [128, 1024], F32)
    bbc = kb.sb("bbc", [128, 1024], F32)
    wr = kb.sb("wr", [128, 8, 20], F32)
    brow = kb.sb("brow", [1, 20], F32)
    for i in range(5):
        for f in range(8):
            p.dma(wg[:, i, f, :], w["w_gate"][i, f * 128:(f + 1) * 128, :], writes=[("wg", i)], q="pool")
        p.dma(wbr[:, i, :, :], w["w_br"][i].rearrange("(j p) c -> p j c", p=128), writes=["wbr"], q="pool")
    p.dma(wout[:], w["w_out"].rearrange("(f p) c -> p f c", p=128), writes=["wout"], q="pool")
    p.dma(bg[:], w["b_gate"].rearrange("i (c p) -> p i c", p=128), writes=["bg"], slow=True)
    p.dma(gbc[:], w["ln1_g"].partition_broadcast(128), writes=["lngb"])
    p.dma(bbc[:], w["ln1_b"].partition_broadcast(128), writes=["lngb"])
    p.dma(wr[:, :, 0:4], w["w_rg"].rearrange("(f p) g -> p f g", p=128), writes=["wr"], slow=True)
    for g in range(4):
        p.dma(wr[:, :, 4 + 4 * g:8 + 4 * g], w["w_re"][g].rearrange("(f p) e -> p f e", p=128), writes=["wr"], slow=True)
    p.dma(brow[0:1, 0:4], w["b_rg"].rearrange("(o g) -> o g", o=1), writes=["brow"])
    p.dma(brow[0:1, 4:20], w["b_re"].rearrange("(o g) e -> o (g e)", o=1), writes=["brow"])

    hTt = [kb.sb("hTt", [128, 8, 512], BF16) for _ in range(2)]
    oTt = [kb.sb("oTt", [128, 5, 2, 512], BF16)] * 2
    mT = [kb.sb("mT", [128, 8, 512], BF16) for _ in range(2)]
    sg = [kb.sb("sg", [128, 512], F32) for _ in range(2)]
    acc = kb.sb("macc", [128, 512], F32)
    tmpm = kb.sb("tmpm", [128, 512], F32)
    hch = [kb.sb("hch", [128, 1024], F32)] * 2
    vch = [kb.sb("vch", [128, 1024], F32)] * 2
    h1c = [kb.sb("h1c", [128, 1024], F32) for _ in range(2)]
    h1Tf = [kb.sb("h1Tf", [128, 8, 128], F32) for _ in range(2)]
    h1Tb = [kb.sb("h1Tb", [128, 8, 128], BF16) for _ in range(2)]
    lnt = {"st": kb.sb("lnst", [128, 2, 6], F32), "mv": kb.sb("lnmv", [128, 2], F32), "sm": kb.sb("lnsm", [128, 2], F32)}
    lg = kb.sb("lg", [128, 20], F32)
    r1 = kb.sb("r1", [128, 8], F32)
    goh = kb.sb("goh", [128, 4], F32)
    el = kb.sb("el", [128, 4], F32)
    ee = kb.sb("ee", [128, 4], F32)
    ee2 = kb.sb("ee2", [128, 4], F32)
    gd = [kb.sb("gd", [128, 16], F32) for _ in range(2)]
    bk = {"n": 0}

    def nbank():
        b = bk["n"] % 6
        bk["n"] += 1
        return b

    def do_slot(s):
        d2 = s % 2
        tsl = slice(s * 512, (s + 1) * 512)
        ht, ot, mt = hTt[d2], oTt[d2], mT[d2]
        p.dma(ht[:], io["hT_d"].rearrange("(f p) t -> p f t", p=128)[:, :, tsl], writes=[("hTt", d2)])
        for i in range(5):
            p.dma(ot[:, i, :, :], io["oT_d"][i].rearrange("(j p) t -> p j t", p=128)[:, :, tsl], writes=["oTt"])
        for cc in range(8):
            csl = slice(cc * 128, (cc + 1) * 128)
            for i in range(5):
                bgt, bbr = nbank(), nbank()
                pg, pb = kb.ps(bgt), kb.ps(bbr)
                for f in range(8):
                    p.pe(lambda e, pg=pg, i=i, f=f, csl=csl: e.matmul(pg[:], lhsT=wg[:, i, f, csl], rhs=ht[:, f, :], start=(f == 0), stop=(f == 7)),
                         reads=[("wg", i), ("hTt", d2)], writes=[("ps", bgt)])
                for jc in range(2):
                    p.pe(lambda e, pb=pb, i=i, jc=jc, csl=csl: e.matmul(pb[:], lhsT=wbr[:, i, jc, csl], rhs=ot[:, i, jc, :], start=(jc == 0), stop=(jc == 1)),
                         reads=["wbr", "oTt"], writes=[("ps", bbr)])
                sgi = sg[i % 2]
                p.act(lambda e, sgi=sgi, pg=pg, i=i, cc=cc: e.activation(out=sgi[:], in_=pg[:], func=AF.Sigmoid, bias=bg[:, i, cc:cc + 1]),
                      reads=[("ps", bgt), "bg"], writes=[("sg", i % 2)])
                if i == 0:
                    p.dve(lambda e, sgi=sgi, pb=pb: e.tensor_tensor(out=acc[:], in0=sgi[:], in1=pb[:], op=ALU.mult),
                          reads=[("sg", i % 2), ("ps", bbr)], writes=["macc"])
                else:
                    p.dve(lambda e, sgi=sgi, pb=pb: e.tensor_tensor(out=tmpm[:], in0=sgi[:], in1=pb[:], op=ALU.mult),
                          reads=[("sg", i % 2), ("ps", bbr)], writes=["tmpm"])
                    if i < 4:
                        p.dve(lambda e: e.tensor_tensor(out=acc[:], in0=acc[:], in1=tmpm[:], op=ALU.add), reads=["macc", "tmpm"], writes=["macc"])
                    else:
                        p.dve(lambda e, cc=cc: e.tensor_tensor(out=mt[:, cc, :], in0=acc[:], in1=tmpm[:], op=ALU.add), reads=["macc", "tmpm"], writes=[("mT", d2)])
        if "dbg_mt" in io and s == 0:
            p.dma(io["dbg_mt"], mt[:], reads=[("mT", d2)], writes=["dbg_mt"])
        def do_chunk(tc):
            gck = s * 4 + tc
            k2 = gck % 2
            rows = slice(gck * 128, (gck + 1) * 128)
            hc, vc, h1 = hch[k2], vch[k2], h1c[k2]
            p.dma(hc[:], io["h_tok"][rows, :], writes=["hch"])
            for hf in range(2):
                b = nbank()
                ps = kb.ps(b)
                for cc in range(8):
                    p.pe(lambda e, ps=ps, cc=cc, tc=tc, hf=hf: e.matmul(ps[:], lhsT=mt[:, cc, tc * 128:(tc + 1) * 128], rhs=wout[:, cc, hf * 512:(hf + 1) * 512],
                                                                      start=(cc == 0), stop=(cc == 7)),
                         reads=["wout", ("mT", d2)], writes=[("ps", b)])
                p.dve(lambda e, ps=ps, hf=hf: e.scalar_tensor_tensor(out=vc[:, hf * 512:(hf + 1) * 512], in0=hc[:, hf * 512:(hf + 1) * 512], scalar=ALPHA, in1=ps[:],
                                                                      op0=ALU.mult, op1=ALU.add),
                      reads=["hch", ("ps", b)], writes=["vch"])
            layer_norm_chunk(kb, c, vc, "vch", gbc, bbc, h1, ("h1c", k2), lnt)
            p.dma(io["h1_d"][rows, :], h1[:], reads=[("h1c", k2)], writes=[("h1_d", gck)])
            tf, tb = h1Tf[k2], h1Tb[k2]
            for hf in range(2):
                b = nbank()
                ps = kb.ps(b)
                for jx in range(4):
                    f = hf * 4 + jx
                    p.pe(lambda e, ps=ps, f=f, jx=jx: e.transpose(out=ps[:, jx * 128:(jx + 1) * 128], in_=h1[:, f * 128:(f + 1) * 128], identity=c["identf"][:]),
                         reads=[("h1c", k2), "identf"], writes=[("ps", b)])
                p.act(lambda e, ps=ps, hf=hf: e.copy(out=tf[:, hf * 4:hf * 4 + 4, :], in_=ps[:].rearrange("p (j t) -> p j t", j=4)),
                      reads=[("ps", b)], writes=[("h1Tf", k2)])
            p.pool(lambda e: e.tensor_copy(out=tb[:], in_=tf[:]), reads=[("h1Tf", k2)], writes=[("h1Tb", k2)])
            p.dma(io["h1T_d"].rearrange("(f p) t -> p f t", p=128)[:, :, rows], tb[:], reads=[("h1Tb", k2)], writes=[("h1T_d", gck)])
            b = nbank()
            ps = kb.ps(b)
            for f in range(8):
                p.pe(lambda e, ps=ps, f=f: e.matmul(ps[:, 0:20], lhsT=tf[:, f, :], rhs=wr[:, f, :], start=(f == 0), stop=False),
                     reads=[("h1Tf", k2), "wr"], writes=[("ps", b)])
            p.pe(lambda e, ps=ps: e.matmul(ps[:, 0:20], lhsT=c["onesf"][0:1, :], rhs=brow[0:1, :], start=False, stop=True),
                 reads=["onesf", "brow"], writes=[("ps", b)])
            gdt = gd[k2]
            p.dve(lambda e, ps=ps: e.tensor_copy(out=lg[:], in_=ps[:, 0:20]), reads=[("ps", b)], writes=["lg"])
            p.dve(lambda e: e.tensor_reduce(out=r1[:, 0:1], in_=lg[:, 0:4], axis=AX.X, op=ALU.max), reads=["lg"], writes=["r1a"])
            p.dve(lambda e: e.tensor_scalar(out=goh[:], in0=lg[:, 0:4], scalar1=r1[:, 0:1], scalar2=None, op0=ALU.is_equal), reads=["lg", "r1a"], writes=["goh"])
            p.dve(lambda e: e.tensor_scalar(out=ee[:], in0=lg[:, 0:4], scalar1=r1[:, 0:1], scalar2=None, op0=ALU.subtract), reads=["lg", "r1a"], writes=["ee"])
            p.act(lambda e: e.activation(out=ee[:], in_=ee[:], func=AF.Exp), reads=["ee"], writes=["ee"])
            p.dve(lambda e: e.tensor_reduce(out=r1[:, 1:2], in_=ee[:], axis=AX.X, op=ALU.add), reads=["ee"], writes=["r1b"])
            p.dve(lambda e: e.reciprocal(out=r1[:, 1:2], in_=r1[:, 1:2]), reads=["r1b"], writes=["r1b"])
            p.dve(lambda e: e.tensor_scalar(out=el[:], in0=lg[:, 4:8], scalar1=goh[:, 0:1], scalar2=None, op0=ALU.mult), reads=["lg", "goh"], writes=["el"])
            for g in range(1, 4):
                p.dve(lambda e, g=g: e.scalar_tensor_tensor(out=el[:], in0=lg[:, 4 + 4 * g:8 + 4 * g], scalar=goh[:, g:g + 1], in1=el[:], op0=ALU.mult, op1=ALU.add),
                      reads=["lg", "goh", "el"], writes=["el"])
            p.dve(lambda e: e.tensor_reduce(out=r1[:, 2:3], in_=el[:], axis=AX.X, op=ALU.max), reads=["el"], writes=["r1c"])
            p.dve(lambda e: e.tensor_scalar(out=ee[:], in0=el[:], scalar1=r1[:, 2:3], scalar2=None, op0=ALU.subtract), reads=["el", "r1c"], writes=["ee"])
            p.act(lambda e: e.activation(out=ee[:], in_=ee[:], func=AF.Exp), reads=["ee"], writes=["ee"])
            p.dve(lambda e: e.tensor_scalar(out=ee2[:], in0=ee[:], scalar1=1.0, scalar2=-2.0, op0=ALU.is_ge, op1=ALU.mult), reads=["ee"], writes=["ee2"])
            p.dve(lambda e: e.tensor_tensor(out=ee2[:], in0=ee2[:], in1=ee[:], op=ALU.add), reads=["ee2", "ee"], writes=["ee2"])
            p.dve(lambda e: e.tensor_reduce(out=r1[:, 3:4], in_=ee2[:], axis=AX.X, op=ALU.max), reads=["ee2"], writes=["r1d"])
            p.dve(lambda e: e.tensor_scalar(out=ee2[:], in0=ee[:], scalar1=r1[:, 3:4], scalar2=None, op0=ALU.is_ge), reads=["ee", "r1d"], writes=["ee2"])
            p.dve(lambda e: e.tensor_tensor(out=ee[:], in0=ee[:], in1=ee2[:], op=ALU.mult), reads=["ee", "ee2"], writes=["ee"])
            p.dve(lambda e: e.tensor_scalar(out=r1[:, 4:5], in0=r1[:, 3:4], scalar1=1.0, scalar2=None, op0=ALU.add), reads=["r1d"], writes=["r1e"])
            p.dve(lambda e: e.reciprocal(out=r1[:, 4:5], in_=r1[:, 4:5]), reads=["r1e"], writes=["r1e"])
            p.dve(lambda e: e.tensor_tensor(out=r1[:, 4:5], in0=r1[:, 4:5], in1=r1[:, 1:2], op=ALU.mult), reads=["r1e", "r1b"], writes=["r1e"])
            p.dve(lambda e: e.tensor_scalar(out=ee[:], in0=ee[:], scalar1=r1[:, 4:5], scalar2=None, op0=ALU.mult), reads=["ee", "r1e"], writes=["ee"])
            for g in range(4):
                p.dve(lambda e, g=g: e.tensor_scalar(out=gdt[:, 4 * g:4 * g + 4], in0=ee[:], scalar1=goh[:, g:g + 1], scalar2=None, op0=ALU.mult),
                      reads=["ee", "goh"], writes=[("gd", k2)])
            p.dma(io["gd_d"][rows, :], gdt[:], reads=[("gd", k2)], writes=[("gd_d", gck)])

        for tc in range(4):
            do_chunk(tc)

    for s in range(NSLOT):
        do_slot(s)

C1_W = {"w_br": [5, 256, D], "w_gate": [5, D, D], "b_gate": [5, D], "w_out": [D, D], "ln1_g": [D], "ln1_b": [D],
        "w_rg": [D, 4], "b_rg": [4], "w_re": [4, D, 4], "b_re": [4, 4]}


def build_c1(dbg=False):
    kb = KB()
    io = {}
    if dbg:
        io["dbg_mt"] = kb.dout("dbg_mt", [128, 8, 512], BF16)
    for n in ("c_ident", "c_ones"):
        io[n] = kb.din(n, [128, 128])
    io["c_cst"] = kb.din("c_cst", [128, 8])
    io["h_tok"] = kb.din("h_tok", [TOK, D])
    io["hT_d"] = kb.din("hT_d", [D, TOK], BF16)
    io["oT_d"] = kb.din("oT_d", [5, 256, TOK], BF16)
    w = {n: kb.din(n, shp) for n, shp in C1_W.items()}
    io["h1_d"] = kb.dout("h1_d", [TOK, D])
    io["h1T_d"] = kb.dout("h1T_d", [D, TOK], BF16)
    io["gd_d"] = kb.dout("gd_d", [TOK, 16])
    c = load_consts(kb, io)
    phase_c1(kb, c, io, w)
    return kb.finish()


def phase_c2(kb, c, io, w, nT=4, nE=16):
    nc, p = kb.nc, kb.p
    gbc = kb.sb("gbc2", [128, 1024], F32)
    bbc = kb.sb("bbc2", [128, 1024], F32)
    p.dma(gbc[:], w["ln2_g"].partition_broadcast(128), writes=["lngb"])
    p.dma(bbc[:], w["ln2_b"].partition_broadcast(128), writes=["lngb"])
    hT = [kb.sb("h1Tt", [128, 8, 1024], BF16) for _ in range(2)]
    gdt = [kb.sb("gdt", [128, 8, 16], F32) for _ in range(2)]
    wup = [kb.sb("wup", [128, 8, 512], BF16) for _ in range(2)]
    wdn = [kb.sb("wdn", [128, 2, 1024], BF16) for _ in range(2)]
    yacc = kb.sb("yacc", [128, 8, 1024], F32)
    gT = [kb.sb("gT", [128, 2, 1024], BF16) for _ in range(2)]
    sa = [kb.sb("sa", [128, 512], F32) for _ in range(2)]
    hch = [kb.sb("h1ch", [128, 1024], F32) for _ in range(2)]
    och = [kb.sb("och", [128, 1024], F32) for _ in range(2)]
    lnt = {"st": kb.sb("lnst2", [128, 2, 6], F32), "mv": kb.sb("lnmv2", [128, 2], F32), "sm": kb.sb("lnsm2", [128, 2], F32)}
    st = {"bank": 0, "w": 0, "sa": 0}

    def nbank():
        b = st["bank"] % 8
        st["bank"] += 1
        return b

    def do_expert(T, e, ht, httok, gd, gdtok):
        k = st["w"] % 2
        st["w"] += 1
        wu, wd, g = wup[k], wdn[k], gT[k]
        p.dma(wu[:], w["w_up"][e].rearrange("(f p) c -> p f c", p=128), writes=[("wup", k)], q="pool")
        p.dma(wd[:], w["w_down"][e].rearrange("(j p) c -> p j c", p=128), writes=[("wdn", k)], q="pool")
        for ts in range(2):
            tsl = slice(ts * 512, (ts + 1) * 512)
            for jc in range(2):
                ba, bu = nbank(), nbank()
                pa, pu = kb.ps(ba), kb.ps(bu)
                for f in range(8):
                    p.pe(lambda e_, f=f: e_.matmul(pa[:], lhsT=wu[:, f, jc * 128:(jc + 1) * 128], rhs=ht[:, f, tsl], start=(f == 0), stop=(f == 7)),
                         reads=[("wup", k), httok], writes=[("ps", ba)])
                for f in range(8):
                    p.pe(lambda e_, f=f: e_.matmul(pu[:], lhsT=wu[:, f, 256 + jc * 128:256 + (jc + 1) * 128], rhs=ht[:, f, tsl], start=(f == 0), stop=(f == 7)),
                         reads=[("wup", k), httok], writes=[("ps", bu)])
                si = st["sa"] % 2
                st["sa"] += 1
                sat = sa[si]
                p.act(lambda e_, sat=sat, pa=pa: e_.activation(out=sat[:], in_=pa[:], func=AF.Silu), reads=[("ps", ba)], writes=[("sa", si)])
                p.dve(lambda e_, sat=sat, pu=pu, jc=jc, tsl=tsl: e_.tensor_tensor(out=g[:, jc, tsl], in0=sat[:], in1=pu[:], op=ALU.mult),
                      reads=[("sa", si), ("ps", bu)], writes=[("gT", k)])
        for tc in range(8):
            for hf in range(2):
                b = nbank()
                ps = kb.ps(b)
                for jc in range(2):
                    p.pe(lambda e_, jc=jc, ps=ps, tc=tc, hf=hf: e_.matmul(ps[:], lhsT=g[:, jc, tc * 128:(tc + 1) * 128], rhs=wd[:, jc, hf * 512:(hf + 1) * 512],
                                                                       start=(jc == 0), stop=(jc == 1)),
                         reads=[("gT", k), ("wdn", k)], writes=[("ps", b)])
                ya = yacc[:, tc, hf * 512:(hf + 1) * 512]
                if e == 0:
                    p.dve(lambda e_, ps=ps, ya=ya, tc=tc: e_.tensor_scalar(out=ya, in0=ps[:], scalar1=gd[:, tc, e:e + 1], scalar2=None, op0=ALU.mult),
                          reads=[("ps", b), gdtok], writes=[("yacc", tc)])
                else:
                    p.dve(lambda e_, ps=ps, ya=ya, tc=tc: e_.scalar_tensor_tensor(out=ya, in0=ps[:], scalar=gd[:, tc, e:e + 1], in1=ya, op0=ALU.mult, op1=ALU.add),
                          reads=[("ps", b), gdtok, ("yacc", tc)], writes=[("yacc", tc)])

    def do_chunk_out(T, tc):
        gck = T * 8 + tc
        k2 = gck % 2
        rows = slice(gck * 128, (gck + 1) * 128)
        hc, oc = hch[k2], och[k2]
        p.dma(hc[:], io["h1_d"][rows, :], writes=[("h1ch", k2)])
        p.dve(lambda e_: e_.scalar_tensor_tensor(out=hc[:], in0=hc[:], scalar=ALPHA, in1=yacc[:, tc, :], op0=ALU.mult, op1=ALU.add),
              reads=[("h1ch", k2), ("yacc", tc)], writes=[("h1ch", k2)])
        layer_norm_chunk(kb, c, hc, ("h1ch", k2), gbc, bbc, oc, ("och", k2), lnt)
        p.dma(io["h_out"][rows, :], oc[:], reads=[("och", k2)], writes=[("h_out", gck)])

    def do_tile(T):
        d2 = T % 2
        ht, gd = hT[d2], gdt[d2]
        cols = slice(T * 1024, (T + 1) * 1024)
        p.dma(ht[:], io["h1T_d"].rearrange("(f p) t -> p f t", p=128)[:, :, cols], writes=[("h1Tt", d2)])
        p.dma(gd[:], io["gd_d"][T * 1024:(T + 1) * 1024, :].rearrange("(n p) e -> p n e", p=128), writes=[("gdt", d2)])
        for e in range(nE):
            do_expert(T, e, ht, ("h1Tt", d2), gd, ("gdt", d2))
        for tc in range(8):
            do_chunk_out(T, tc)

    for T in range(nT):
        do_tile(T)


C2_W = {"w_up": [16, D, 512], "w_down": [16, 256, D], "ln2_g": [D], "ln2_b": [D]}


def build_c2(nT=4, nE=16):
    kb = KB()
    io = {}
    for n in ("c_ident", "c_ones"):
        io[n] = kb.din(n, [128, 128])
    io["c_cst"] = kb.din("c_cst", [128, 8])
    io["h1_d"] = kb.din("h1_d", [TOK, D])
    io["h1T_d"] = kb.din("h1T_d", [D, TOK], BF16)
    io["gd_d"] = kb.din("gd_d", [TOK, 16])
    w = {n: kb.din(n, shp) for n, shp in C2_W.items()}
    io["h_out"] = kb.dout("h_out", [TOK, D])
    c = load_consts(kb, io)
    phase_c2(kb, c, io, w, nT, nE)
    return kb.finish()


W_SHAPES = {
    "w_in": [D, IN_TOTAL], "g_cq": [256], "g_ckv": [128], "w_uq": [256, 384], "w_ukv": [128, 512],
    "nsa_pe": [32, 64], "w_phi_k1": [2048, 128], "w_phi_k2": [128, 64], "w_phi_v1": [2048, 128], "w_phi_v2": [128, 64],
    "w_mem_kv": [D, 512], "w_br": [5, 256, D], "w_gate": [5, D, D], "b_gate": [5, D], "w_out": [D, D],
    "ln1_g": [D], "ln1_b": [D], "w_rg": [D, 4], "b_rg": [4], "w_re": [4, D, 4], "b_re": [4, 4],
    "w_up": [16, D, 512], "w_down": [16, 256, D], "ln2_g": [D], "ln2_b": [D],
}
PAIRS = [[0, 1], [2, 3], [4, 5], [6, 7]]


def build_fused(depth=DEPTH, nlw=DEPTH, stop=None):
    kb = KB()
    io = {}
    for n in ("c_ident", "c_ones"):
        io[n] = kb.din(n, [128, 128])
    io["c_cst"] = kb.din("c_cst", [128, 8])
    for n, (shp, dt) in B_CONST_SHAPES.items():
        io[n] = kb.din(n, shp, dt)
    io["ropeq_t"] = kb.din("ropeq_t", [NSLOT, 96, 2, 512])
    io["ropek_t"] = kb.din("ropek_t", [NSLOT, 32, 2, 512])
    io["x_own"] = kb.din("x_own", [TOK, D])
    io["mem"] = kb.din("mem", [256, D])
    wfull = {n: kb.din(n, [nlw] + shp) for n, shp in W_SHAPES.items()}
    io["h_final"] = kb.dout("h_final", [TOK, D])
    hbuf = [kb.dscratch(f"hbuf{i}", [TOK, D]) for i in range(2)]
    io["hT_d"] = kb.dscratch("hT_d", [D, TOK], BF16)
    for n in ("qsb_d", "qmo_d", "qns_d", "qme_d"):
        io[n] = kb.dscratch(n, [256, TOK], BF16)
    io["qml_d"] = kb.dscratch("qml_d", [4, 96, TOK], BF16)
    io["gns_d"] = kb.dscratch("gns_d", [12, TOK])
    io["oT_d"] = kb.dscratch("oT_d", [5, 256, TOK], BF16)
    io["h1_d"] = kb.dscratch("h1_d", [TOK, D])
    io["h1T_d"] = kb.dscratch("h1T_d", [D, TOK], BF16)
    io["gd_d"] = kb.dscratch("gd_d", [TOK, 16])
    xk_rows = [256, 256, 256, 256, 64]
    xk_in = [kb.dscratch(f"xk_in{k}", [r, TOK], BF16) for k, r in enumerate(xk_rows)]
    xk_out = [kb.dscratch(f"xk_out{k}", [2 * r, TOK], BF16) for k, r in enumerate(xk_rows)]
    xv_in = [kb.dscratch(f"xv_in{k}", [1024, 896], BF16) for k in range(4)]
    xv_out = [kb.dscratch(f"xv_out{k}", [2048, 896], BF16) for k in range(4)]
    io["ksb_d"], io["kmo_d"] = xk_in[0], xk_in[1]
    io["kml_d"] = xk_in[2].rearrange("(h d) t -> h d t", h=4)
    io["krl_d"], io["kcv_d"], io["ksl_d"] = xk_in[3][0:32, :], xk_in[3][32:160, :], xk_in[3][160:224, :]
    io["kwi_d"] = xk_in[4]
    io["vsb_d"], io["vmo_d"] = TMChunks(xv_in, 0, 256), TMChunks(xv_in, 256, 512)
    io["vml_d"], io["vsw_d"] = TMChunks(xv_in, 512, 768), TMChunks(xv_in, 768, 896)
    io["ksb_f"], io["kmo_f"], io["kml_f"] = KPair(xk_out[0], 256), KPair(xk_out[1], 256), KPair(xk_out[2], 256)
    io["krl_f"], io["kcv_f"], io["ksl_f"] = KPair(xk_out[3], 256, 0), KPair(xk_out[3], 256, 32), KPair(xk_out[3], 256, 160)
    io["kwi_f"] = KPair(xk_out[4], 64)
    io["vsb_f"], io["vmo_f"] = VPair(xv_out, 0), VPair(xv_out, 256)
    io["vml_f"], io["vsw_f"] = VPair(xv_out, 512), VPair(xv_out, 768)

    for l in range(depth):
        w = {n: ap[l] for n, ap in wfull.items()}
        io["h_tok"] = io["x_own"] if l == 0 else hbuf[(l - 1) % 2]
        io["h_out"] = io["h_final"] if l == depth - 1 else hbuf[l % 2]
        c = load_consts(kb, io)
        phase_a(kb, c, io, w)
        kb.end_phase()
        if stop == "A":
            break
        for k in range(5):
            kb.p.allgather(xk_out[k], xk_in[k], PAIRS, writes=[("xk", k)])
        for k in range(4):
            kb.p.allgather(xv_out[k], xv_in[k], PAIRS, writes=[("xv", k)])
        kb.end_phase()
        if stop == "AG":
            break
        c = load_consts(kb, io)
        phase_b(kb, c, io, w, ("sb", "moba", "mla", "mem"))
        kb.end_phase()
        if stop == "B1":
            break
        c = load_consts(kb, io)
        phase_b(kb, c, io, w, ("nsa",))
        kb.end_phase()
        if stop == "B2":
            break
        c = load_consts(kb, io)
        phase_c1(kb, c, io, w)
        kb.end_phase()
        if stop == "C1":
            break
        c = load_consts(kb, io)
        phase_c2(kb, c, io, w)
        kb.end_phase()
    return kb.finish()


def fused_inputs(inp, c):
    b, r = c // 2, c % 2
    m = dict(host_consts())
    m.update(bconsts_host(r))
    m["ropeq_t"], m["ropek_t"] = rope_tables(r)
    m["x_own"] = np.ascontiguousarray(inp["x"][b][_own_tokens(r)])
    m["mem"] = inp["mem"][b]
    for n in W_SHAPES:
        m[n] = inp[n]
    return m


_PROGS = {}


def _prog(name, fn):
    if name not in _PROGS:
        _PROGS[name] = fn()
    return _PROGS[name]


def _own_tokens(r):
    t = np.arange(TOK)
    return t // 512 * 1024 + r * 512 + t % 512


def _interleave_fm(a0, a1):
    sh = a0.shape[:-1]
    out = np.empty(sh + (S,), a0.dtype)
    o = out.reshape(sh + (8, 2, 512))
    o[..., 0, :] = a0.reshape(sh + (8, 512))
    o[..., 1, :] = a1.reshape(sh + (8, 512))
    return out


def _interleave_tm(a0, a1):
    C = a0.shape[1]
    out = np.empty((S, C), a0.dtype)
    o = out.reshape(8, 2, 512, C)
    o[:, 0] = a0.reshape(8, 512, C)
    o[:, 1] = a1.reshape(8, 512, C)
    return out


A_WEIGHTS = ("w_in", "w_uq", "g_cq", "w_ukv", "g_ckv")
K_FM = {"ksb_d": "ksb_f", "kmo_d": "kmo_f", "kml_d": "kml_f", "krl_d": "krl_f", "kcv_d": "kcv_f", "ksl_d": "ksl_f", "kwi_d": "kwi_f"}
V_TM = {"vsb_d": "vsb_f", "vmo_d": "vmo_f", "vml_d": "vml_f", "vsw_d": "vsw_f"}
Q_OWN = ("qsb_d", "qmo_d", "qml_d", "qns_d", "qme_d", "gns_d")


def kernel_unfused(**inputs):
    inp = {k: np.ascontiguousarray(np.asarray(v)) for k, v in inputs.items()}
    ncores = 8
    cores = list(range(ncores))
    hc = host_consts()
    bc = [bconsts_host(r) for r in range(2)]
    rp = [rope_tables(r) for r in range(2)]
    own = [_own_tokens(r) for r in range(2)]
    h = [np.ascontiguousarray(inp["x"][c // 2][own[c % 2]]) for c in cores]
    nca = _prog("a", build_a)
    ncb1 = _prog("b1", lambda: build_b(("sb", "moba", "mla", "mem")))
    ncb2 = _prog("b2", lambda: build_b(("nsa",)))
    ncc1 = _prog("c1", build_c1)
    ncc2 = _prog("c2", build_c2)
    for l in range(DEPTH):
        maps = []
        for c in cores:
            m = dict(hc)
            m["h_tok"] = h[c]
            m["ropeq_t"], m["ropek_t"] = rp[c % 2]
            for n in A_WEIGHTS:
                m[n] = inp[n][l]
            maps.append(m)
        ra = run_bass_kernel_spmd(nca, maps, core_ids=cores).results
        maps = []
        for c in cores:
            b, r = c // 2, c % 2
            m = dict(hc)
            m.update(bc[r])
            for n in Q_OWN:
                m[n] = ra[c][n]
            for kd, kf in K_FM.items():
                m[kf] = _interleave_fm(np.asarray(ra[2 * b][kd]), np.asarray(ra[2 * b + 1][kd]))
            for vd, vf in V_TM.items():
                m[vf] = _interleave_tm(np.asarray(ra[2 * b][vd]), np.asarray(ra[2 * b + 1][vd]))
            m["mem"] = inp["mem"][b]
            for n in B_WEIGHTS:
                m[n] = inp[n][l]
            maps.append(m)
        rb1 = run_bass_kernel_spmd(ncb1, maps, core_ids=cores).results
        rb2 = run_bass_kernel_spmd(ncb2, maps, core_ids=cores).results
        rb = []
        for c in cores:
            o = np.array(rb1[c]["oT_d"])
            o[3] = np.asarray(rb2[c]["oT_d"])[3]
            rb.append({"oT_d": o})
        maps = []
        for c in cores:
            m = dict(hc)
            m["h_tok"] = h[c]
            m["hT_d"] = ra[c]["hT_d"]
            m["oT_d"] = rb[c]["oT_d"]
            for n in C1_W:
                m[n] = inp[n][l]
            maps.append(m)
        rc1 = run_bass_kernel_spmd(ncc1, maps, core_ids=cores).results
        maps = []
        for c in cores:
            m = dict(hc)
            for n in ("h1_d", "h1T_d", "gd_d"):
                m[n] = rc1[c][n]
            for n in C2_W:
                m[n] = inp[n][l]
            maps.append(m)
        rc2 = run_bass_kernel_spmd(ncc2, maps, core_ids=cores).results
        h = [np.asarray(rc2[c]["h_out"]) for c in cores]
    out = np.empty((NB, S, D), np.float32)
    for c in cores:
        out[c // 2][own[c % 2]] = h[c]
    return out


def kernel(**inputs):
    inp = {k: np.ascontiguousarray(np.asarray(v)) for k, v in inputs.items()}
    cores = list(range(8))
    nc = _prog("fused", build_fused)
    maps = [fused_inputs(inp, c) for c in cores]
    res = run_bass_kernel_spmd(nc, maps, core_ids=cores).results
    out = np.empty((NB, S, D), np.float32)
    for c in cores:
        out[c // 2][_own_tokens(c % 2)] = np.asarray(res[c]["h_final"])
    return out
```

```python
import contextlib
import types
import numpy as np
import ml_dtypes
import concourse.bass as bass
import concourse.mybir as mybir
from concourse.bass_utils import run_bass_kernel_spmd

F32 = mybir.dt.float32
BF16 = mybir.dt.bfloat16
AF = mybir.ActivationFunctionType
ALU = mybir.AluOpType
AX = mybir.AxisListType

D = 1024
S = 8192
NB = 4
DEPTH = 4
TOK = 4096
NSLOT = 8
IN_TOTAL = 2860
ALPHA = (2.0 * DEPTH) ** 0.25
LN_EPS = 1e-5
RMS_EPS = 1e-6
MASKV = -30000.0
SLOPES = [2.0 ** (-2.0 * (i + 1)) for i in range(4)]

ENGS = ("pe", "act", "dve", "pool", "sp")
SIG_EPOCH = 30000


def _freeze(fn):
    if fn.__closure__ is None:
        return fn
    cells = []
    for cl in fn.__closure__:
        try:
            cells.append(types.CellType(cl.cell_contents))
        except ValueError:
            cells.append(cl)
    g = types.FunctionType(fn.__code__, fn.__globals__, fn.__name__, fn.__defaults__, tuple(cells))
    g.__kwdefaults__ = fn.__kwdefaults__
    return g


class Op:
    __slots__ = ("eng", "fn", "reads", "writes", "dma", "deps", "sig", "dticket", "idx", "dprev")

    def __init__(self, eng, fn, reads, writes, dma):
        self.eng = eng
        self.fn = fn
        self.reads = tuple(reads)
        self.writes = tuple(writes)
        self.dma = dma
        self.deps = []
        self.sig = None
        self.dticket = None
        self.dprev = None


class Prog:
    NDSEM = 12
    _phase_id = 0

    def __init__(self, nc):
        self.nc = nc
        self.ops = []

    def add(self, eng, fn, reads=(), writes=(), dma=False):
        op = Op(eng, _freeze(fn), reads, writes, dma)
        op.idx = len(self.ops)
        self.ops.append(op)
        return op

    def pe(self, fn, reads=(), writes=()):
        return self.add("pe", fn, reads, writes)

    def act(self, fn, reads=(), writes=()):
        return self.add("act", fn, reads, writes)

    def dve(self, fn, reads=(), writes=()):
        return self.add("dve", fn, reads, writes)

    def pool(self, fn, reads=(), writes=()):
        return self.add("pool", fn, reads, writes)

    def allgather(self, out, in_, groups, reads=(), writes=()):
        return self.add("pool", lambda e: e.collective_compute("AllGather", ALU.bypass, replica_groups=groups, ins=[in_.opt()], outs=[out.opt()]),
                        reads, writes, dma="cc")

    def dma(self, out, in_, reads=(), writes=(), q="sp", slow=False):
        if slow:
            return self.add(q, lambda e: e.dma_start(out=out, in_=in_, allow_slow_non_contiguous=True), reads, writes, dma=True)
        return self.add(q, lambda e: e.dma_start(out=out, in_=in_), reads, writes, dma=True)

    def analyze(self):
        last_w = {}
        readers = {}
        for op in self.ops:
            deps = set()
            for t in op.reads:
                if t in last_w:
                    deps.add(last_w[t])
            for t in op.writes:
                if t in last_w:
                    deps.add(last_w[t])
                for r in readers.get(t, ()):
                    deps.add(r)
            deps.discard(op.idx)
            op.deps = sorted(deps)
            for t in op.reads:
                readers.setdefault(t, []).append(op.idx)
            for t in op.writes:
                last_w[t] = op.idx
                readers[t] = []
        qcount = {e: 0 for e in ENGS}
        qhist = {e: [] for e in ENGS}
        for op in self.ops:
            if op.dma == "cc":
                op.dticket = ("cc", op.idx, 1)
                continue
            if op.dma:
                n = qcount[op.eng]
                qcount[op.eng] += 1
                op.dticket = (op.eng, n % self.NDSEM, 16 * (n // self.NDSEM + 1))
                if n >= self.NDSEM:
                    op.dprev = qhist[op.eng][n - self.NDSEM]
                qhist[op.eng].append(op.idx)
        waited_eng = {e: {p: -1 for p in ENGS} for e in ENGS}
        waited_dma = {e: set() for e in ENGS}
        last_on = {e: -1 for e in ENGS}
        need_sig = set()
        for op in self.ops:
            e = op.eng
            final = []
            best = {}
            dl = list(op.deps)
            if op.dprev is not None:
                dl.append(op.dprev)
            for d in dl:
                p = self.ops[d]
                if p.dma:
                    if d not in waited_dma[e]:
                        waited_dma[e].add(d)
                        final.append(("dma", d))
                else:
                    if p.eng == e and e == "pe":
                        continue
                    if d <= waited_eng[e][p.eng]:
                        continue
                    if p.eng not in best or d > best[p.eng]:
                        best[p.eng] = d
            for pe_, d in best.items():
                waited_eng[e][pe_] = d
                need_sig.add(d)
                final.append(("eng", d))
            op.deps = final
        cnt = {e: 0 for e in ENGS}
        for op in self.ops:
            if not op.dma and op.idx in need_sig:
                cnt[op.eng] += 1
                op.sig = cnt[op.eng]
        self.sig_total = cnt
        self.dma_total = qcount

    def emit(self, barrier=False):
        nc = self.nc
        self.analyze()
        allsem = []

        Prog._phase_id += 1
        pid = Prog._phase_id

        def newsem(name):
            h = nc.alloc_semaphore(f"{name}_ph{pid}")
            allsem.append(h)
            return h

        if True:
            esem = {}
            for e in ENGS:
                n_ep = self.sig_total[e] // SIG_EPOCH + 1
                esem[e] = [newsem(f"s_{e}_{i}") for i in range(n_ep)]
            dsem = {}
            for e in ENGS:
                if self.dma_total[e]:
                    dsem[e] = [newsem(f"d_{e}_{i}") for i in range(self.NDSEM)]
            dsem["cc"] = {op.idx: newsem(f"cc_{op.idx}") for op in self.ops if op.dma == "cc"}

            def waitspec(dep):
                kind, d = dep
                p = self.ops[d]
                if kind == "dma":
                    q, si, val = p.dticket
                    return dsem[q][si], val
                k = p.sig - 1
                return esem[p.eng][k // SIG_EPOCH], k % SIG_EPOCH + 1

            def run(engname):
                def body(eng):
                    last_dma = {}
                    for op in self.ops:
                        if op.eng != engname:
                            continue
                        ws = [waitspec(d) for d in op.deps]
                        for (sem, val) in ws[1:]:
                            eng.wait_ge(sem, val)
                        ins = op.fn(eng)
                        if ws:
                            ins._wait_ge(ws[0][0], ws[0][1])
                        if op.dma == "cc":
                            ins.then_inc(dsem["cc"][op.idx])
                            eng.wait_ge(dsem["cc"][op.idx], 1)
                        elif op.dma:
                            q, si, val = op.dticket
                            ins.then_inc(dsem[q][si], 16)
                            last_dma[si] = val
                        elif op.sig is not None:
                            k = op.sig - 1
                            ins.then_inc(esem[engname][k // SIG_EPOCH], 1)
                    for si, val in last_dma.items():
                        eng.wait_ge(dsem[engname][si], val)
                return body

            with nc.Block() as block:
                block.tensor(run("pe"))
                block.scalar(run("act"))
                block.vector(run("dve"))
                block.gpsimd(run("pool"))
                block.sync(run("sp"))
        if barrier:
            nc.all_engine_barrier()
            nc.clear_and_free_semaphores(allsem)
            nc.all_engine_barrier()
        else:
            for h in allsem:
                nc.release_semaphore(h)


class KB:
    def __init__(self):
        self.nc = bass.Bass("TRN2", target_bir_lowering=False)
        self.p = Prog(self.nc)
        self.es = contextlib.ExitStack()
        self.dram = {}
        self._uid = 0
        self.es0 = contextlib.ExitStack()
        self.psf = [self.es0.enter_context(self.nc.psum_tensor(f"psb{i}", [128, 512], F32)) for i in range(8)]

    def uid(self, s):
        self._uid += 1
        return f"{s}_{self._uid}"

    def din(self, name, shape, dt=F32):
        t = self.nc.dram_tensor(name, list(shape), dt, kind="ExternalInput").ap()
        self.dram[name] = t
        return t

    def dout(self, name, shape, dt=F32, kind="ExternalOutput"):
        t = self.nc.dram_tensor(name, list(shape), dt, kind=kind).ap()
        self.dram[name] = t
        return t

    def sb(self, name, shape, dt=F32):
        return self.es.enter_context(self.nc.sbuf_tensor(self.uid(name), list(shape), dt))

    def dscratch(self, name, shape, dt=F32):
        t = self.nc.dram_tensor(name, list(shape), dt, kind="Internal").ap()
        self.dram[name] = t
        return t

    def end_phase(self):
        self.p.emit(barrier=True)
        self.es.close()
        self.es = contextlib.ExitStack()
        self.p = Prog(self.nc)

    def ps(self, i):
        return self.psf[i]

    def finish(self):
        self.p.emit()
        self.es.close()
        self.es0.close()
        return self.nc


C_SBQ, C_SBK, C_SBV = 0, 256, 512
C_MOQ, C_MOK, C_MOV = 768, 1024, 1280
C_CQ, C_CKV, C_KR = 1536, 1792, 1920
C_NSQ, C_NSKV, C_NSG, C_MEQ = 1952, 2208, 2592, 2604


def consts_common(kb):
    nc, p = kb.nc, kb.p
    c = {}
    c["identf"] = kb.sb("identf", [128, 128], F32)
    c["identb"] = kb.sb("identb", [128, 128], BF16)
    c["onesb"] = kb.sb("onesb", [128, 128], BF16)
    c["onesf"] = kb.sb("onesf", [128, 128], F32)
    idf, idb = c["identf"], c["identb"]
    p.pool(lambda e: e.memset(c["onesf"][:], 1.0), writes=["onesf"])
    p.pool(lambda e: e.memset(c["onesb"][:], 1.0), writes=["onesb"])
    p.pool(lambda e: e.affine_select(out=idf[:], in_=c["onesf"][:], pattern=[[-1, 128]], compare_op=ALU.is_equal,
                                     fill=0.0, base=0, channel_multiplier=1), reads=["onesf"], writes=["identf"])
    p.pool(lambda e: e.tensor_copy(out=idb[:], in_=idf[:]), reads=["identf"], writes=["identb"])
    return c


def tm_view(ap2d, p=128):
    return ap2d.rearrange("(n p) c -> p n c", p=p)


def phase_a(kb, c, io, w):
    nc, p = kb.nc, kb.p
    identf, onesb = c["identf"], c["onesb"]

    hT = kb.sb("hT", [128, 8, TOK], BF16)
    win = kb.sb("win", [128, 8, IN_TOTAL], BF16)
    wkrot = kb.sb("wkrot", [128, 8, 32], BF16)
    for f in range(8):
        p.dma(win[:, f, :], w["w_in"][f * 128:(f + 1) * 128, :], writes=[("win", f)], q="pool")
    for f in range(8):
        p.dve(lambda e, f=f: e.tensor_scalar_mul(out=wkrot[:, f, 0:16], in0=win[:, f, C_KR + 16:C_KR + 32], scalar1=-1.0),
              reads=[("win", f)], writes=[("wkrot", f)])
        p.dve(lambda e, f=f: e.tensor_copy(out=wkrot[:, f, 16:32], in_=win[:, f, C_KR:C_KR + 16]),
              reads=[("win", f)], writes=[("wkrot", f)])
    wuq_f = kb.sb("wuq_f", [128, 2, 384], F32)
    wuq = kb.sb("wuq", [128, 2, 384], BF16)
    wuqr = kb.sb("wuqr", [128, 2, 384], BF16)
    gcq = kb.sb("gcq", [128, 2], F32)
    wukv_f = kb.sb("wukv_f", [128, 512], F32)
    wukv = kb.sb("wukv", [128, 512], BF16)
    gckv = kb.sb("gckv", [128, 1], F32)
    p.dma(wuq_f[:], w["w_uq"].rearrange("(n p) c -> p n c", p=128), writes=["wuq_f"])
    p.dma(gcq[:], w["g_cq"].rearrange("(n p) -> p n", p=128), writes=["gcq"], slow=True)
    p.dma(wukv_f[:], w["w_ukv"], writes=["wukv_f"])
    p.dma(gckv[:], w["g_ckv"].rearrange("(n p) -> p n", p=128), writes=["gckv"], slow=True)
    for rc in range(2):
        p.dve(lambda e, rc=rc: e.tensor_scalar_mul(out=wuq[:, rc, :], in0=wuq_f[:, rc, :], scalar1=gcq[:, rc:rc + 1]),
              reads=["wuq_f", "gcq"], writes=["wuq"])
    p.dve(lambda e: e.memset(wuqr[:], 0.0), writes=["wuqr"])
    for rc in range(2):
        for h in range(4):
            b0 = h * 96
            p.dve(lambda e, rc=rc, b0=b0: e.tensor_scalar_mul(out=wuqr[:, rc, b0 + 64:b0 + 80], in0=wuq[:, rc, b0 + 80:b0 + 96], scalar1=-1.0),
                  reads=["wuq"], writes=["wuqr"])
            p.dve(lambda e, rc=rc, b0=b0: e.tensor_copy(out=wuqr[:, rc, b0 + 80:b0 + 96], in_=wuq[:, rc, b0 + 64:b0 + 80]),
                  reads=["wuq"], writes=["wuqr"])
    p.dve(lambda e: e.tensor_scalar_mul(out=wukv[:], in0=wukv_f[:], scalar1=gckv[:, 0:1]), reads=["wukv_f", "gckv"], writes=["wukv"])

    hst = [kb.sb("hst", [128, 1024], F32) for _ in range(2)]
    for ck in range(TOK // 128):
        st = hst[ck % 2]
        tk = ("hst", ck % 2)
        p.dma(st[:], io["h_tok"][ck * 128:(ck + 1) * 128, :], writes=[tk])
        for half in range(2):
            bank = (ck * 2 + half) % 2
            ps = kb.ps(bank)
            for j in range(4):
                f = half * 4 + j
                p.pe(lambda e, ps=ps, st=st, f=f, j=j: e.transpose(out=ps[:, j * 128:(j + 1) * 128], in_=st[:, f * 128:(f + 1) * 128], identity=identf[:]),
                     reads=[tk, "identf"], writes=[("ps", bank)])
            dst = hT[:, half * 4:half * 4 + 4, ck * 128:(ck + 1) * 128]
            src = ps[:].rearrange("p (j t) -> p j t", j=4)
            if half == 0:
                p.act(lambda e, dst=dst, src=src: e.copy(out=dst, in_=src), reads=[("ps", bank)], writes=[("hT", ck // 4)])
            else:
                p.dve(lambda e, dst=dst, src=src: e.tensor_copy(out=dst, in_=src), reads=[("ps", bank)], writes=[("hT", ck // 4)])
    for f in range(8):
        p.dma(io["hT_d"][f * 128:(f + 1) * 128, :], hT[:, f, :], reads=[("hT", s) for s in range(8)], writes=[("hT_d", f)])

    ostage = [kb.sb("ostg", [128, 512], BF16) for _ in range(4)]
    gstage = [kb.sb("gstg", [12, 512], F32) for _ in range(2)]
    cq_sb = [kb.sb("cq_sb", [128, 2, 512], BF16) for _ in range(2)]
    ckv_sb = [kb.sb("ckv_sb", [128, 512], BF16) for _ in range(2)]
    krr_sb = [kb.sb("krr", [32, 2, 512], F32) for _ in range(2)]
    sq_sb = [kb.sb("sq", [128, 3, 512], BF16) for _ in range(2)]
    rstd_q = [kb.sb("rstdq", [128, 512], F32) for _ in range(2)]
    rstd_kv = [kb.sb("rstdkv", [128, 512], F32) for _ in range(2)]
    rkv_tok = [kb.sb("rkvtok", [128, 4], F32) for _ in range(2)]
    ropeq = [kb.sb("ropeq", [96, 2, 512], F32) for _ in range(2)]
    ropek = [kb.sb("ropek", [32, 2, 512], F32) for _ in range(2)]
    t1 = [kb.sb("t1", [96, 512], F32) for _ in range(2)]
    t2 = [kb.sb("t2", [96, 512], F32) for _ in range(2)]
    vstage = [kb.sb("vstg", [128, 640], BF16) for _ in range(2)]
    vmst = [kb.sb("vmst", [128, 256], BF16) for _ in range(2)]
    cnt = {"o": 0, "bank": 0, "v": 0}

    def nbank():
        b = 2 + cnt["bank"] % 6
        cnt["bank"] += 1
        return b

    fm_list = [
        ("qsb_d", 0, C_SBQ, 128, 0.125), ("qsb_d", 128, C_SBQ + 128, 128, 0.125),
        ("ksb_d", 0, C_SBK, 128, 1.0), ("ksb_d", 128, C_SBK + 128, 128, 1.0),
        ("qmo_d", 0, C_MOQ, 128, 0.125), ("qmo_d", 128, C_MOQ + 128, 128, 0.125),
        ("kmo_d", 0, C_MOK, 128, 1.0), ("kmo_d", 128, C_MOK + 128, 128, 1.0),
        ("qns_d", 0, C_NSQ, 128, 0.125), ("qns_d", 128, C_NSQ + 128, 128, 0.125),
        ("kcv_d", 0, C_NSKV, 128, 1.0),
        ("ksl_d", 0, C_NSKV + 128, 64, 1.0),
        ("kwi_d", 0, C_NSKV + 256, 64, 1.0),
        ("qme_d", 0, C_MEQ, 128, 0.125), ("qme_d", 128, C_MEQ + 128, 128, 0.125),
    ]

    def proj_fm(s, col0, ncols, wsrc=None):
        b = nbank()
        ps = kb.ps(b)
        for f in range(8):
            if wsrc is None:
                lhsT = win[:, f, col0:col0 + ncols]
                rd = [("win", f)]
            else:
                lhsT = wsrc[:, f, col0:col0 + ncols]
                rd = [("wkrot", f)]
            p.pe(lambda e, ps=ps, lhsT=lhsT, f=f, s=s, ncols=ncols: e.matmul(ps[0:ncols, :], lhsT=lhsT, rhs=hT[:, f, s * 512:(s + 1) * 512],
                                                                            start=(f == 0), stop=(f == 7)),
                 reads=rd + [("hT", s)], writes=[("ps", b)])
        return b

    for s in range(NSLOT):
        tsl = slice(s * 512, (s + 1) * 512)
        for (dn, r0, col0, ncols, scale) in fm_list:
            b = proj_fm(s, col0, ncols)
            ps = kb.ps(b)
            k = cnt["o"] % 4
            cnt["o"] += 1
            og = ostage[k]
            p.act(lambda e, og=og, ps=ps, ncols=ncols, scale=scale: e.activation(out=og[0:ncols, :], in_=ps[0:ncols, :], func=AF.Copy, scale=scale),
                  reads=[("ps", b)], writes=[("ostg", k)])
            p.dma(io[dn][r0:r0 + ncols, tsl], og[0:ncols, :], reads=[("ostg", k)], writes=[(dn, s)])
        b = proj_fm(s, C_NSG, 12)
        ps = kb.ps(b)
        gs = gstage[s % 2]
        p.act(lambda e, gs=gs, ps=ps: e.activation(out=gs[:], in_=ps[0:12, :], func=AF.Sigmoid), reads=[("ps", b)], writes=[("gstg", s % 2)])
        p.dma(io["gns_d"][:, tsl], gs[:], reads=[("gstg", s % 2)], writes=[("gns_d", s)])

        d2 = s % 2
        cq, ckv, sq = cq_sb[d2], ckv_sb[d2], sq_sb[d2]
        for rc in range(2):
            b = proj_fm(s, C_CQ + rc * 128, 128)
            ps = kb.ps(b)
            p.act(lambda e, cq=cq, ps=ps, rc=rc: e.copy(out=cq[:, rc, :], in_=ps[:]), reads=[("ps", b)], writes=[("cq", d2)])
            p.act(lambda e, sq=sq, ps=ps, rc=rc: e.activation(out=sq[:, rc, :], in_=ps[:], func=AF.Square), reads=[("ps", b)], writes=[("sq", d2)])
        b = proj_fm(s, C_CKV, 128)
        ps = kb.ps(b)
        p.act(lambda e, ckv=ckv, ps=ps: e.copy(out=ckv[:], in_=ps[:]), reads=[("ps", b)], writes=[("ckv", d2)])
        p.act(lambda e, sq=sq, ps=ps: e.activation(out=sq[:, 2, :], in_=ps[:], func=AF.Square), reads=[("ps", b)], writes=[("sq", d2)])
        krr = krr_sb[d2]
        b = proj_fm(s, C_KR, 32)
        ps = kb.ps(b)
        p.act(lambda e, krr=krr, ps=ps: e.copy(out=krr[:, 0, :], in_=ps[0:32, :]), reads=[("ps", b)], writes=[("krr", d2)])
        b = proj_fm(s, 0, 32, wsrc=wkrot)
        ps = kb.ps(b)
        p.act(lambda e, krr=krr, ps=ps: e.copy(out=krr[:, 1, :], in_=ps[0:32, :]), reads=[("ps", b)], writes=[("krr", d2)])
        rq, rkv = rstd_q[d2], rstd_kv[d2]
        b = nbank()
        ps = kb.ps(b)
        for rc in range(2):
            p.pe(lambda e, ps=ps, sq=sq, rc=rc: e.matmul(ps[:], lhsT=onesb[:], rhs=sq[:, rc, :], start=(rc == 0), stop=(rc == 1)),
                 reads=[("sq", d2), "onesb"], writes=[("ps", b)])
        p.act(lambda e, rq=rq, ps=ps: e.activation(out=rq[:], in_=ps[:], func=AF.Ln, scale=1.0 / 256.0, bias=c["eps_rms"][:, 0:1]),
              reads=[("ps", b), "cst"], writes=[("rq", d2)])
        p.act(lambda e, rq=rq: e.activation(out=rq[:], in_=rq[:], func=AF.Exp, scale=-0.5), reads=[("rq", d2)], writes=[("rq", d2)])
        b = nbank()
        ps = kb.ps(b)
        p.pe(lambda e, ps=ps, sq=sq: e.matmul(ps[:], lhsT=onesb[:], rhs=sq[:, 2, :], start=True, stop=True),
             reads=[("sq", d2), "onesb"], writes=[("ps", b)])
        p.act(lambda e, rkv=rkv, ps=ps: e.activation(out=rkv[:], in_=ps[:], func=AF.Ln, scale=1.0 / 128.0, bias=c["eps_rms"][:, 0:1]),
              reads=[("ps", b), "cst"], writes=[("rkv", d2)])
        p.act(lambda e, rkv=rkv: e.activation(out=rkv[:], in_=rkv[:], func=AF.Exp, scale=-0.5), reads=[("rkv", d2)], writes=[("rkv", d2)])
        rkt = rkv_tok[d2]
        b = nbank()
        ps = kb.ps(b)
        for ck in range(4):
            p.pe(lambda e, ps=ps, sq=sq, ck=ck: e.matmul(ps[:, ck:ck + 1], lhsT=sq[:, 2, ck * 128:(ck + 1) * 128], rhs=onesb[:, 0:1], start=True, stop=True),
                 reads=[("sq", d2), "onesb"], writes=[("ps", b)])
        p.act(lambda e, rkt=rkt, ps=ps: e.activation(out=rkt[:], in_=ps[:, 0:4], func=AF.Ln, scale=1.0 / 128.0, bias=c["eps_rms"][:, 0:1]),
              reads=[("ps", b), "cst"], writes=[("rkt", d2)])
        p.act(lambda e, rkt=rkt: e.activation(out=rkt[:], in_=rkt[:], func=AF.Exp, scale=-0.5), reads=[("rkt", d2)], writes=[("rkt", d2)])
        rpq, rpk = ropeq[d2], ropek[d2]
        p.dma(rpq[:], io["ropeq_t"][s], writes=[("rpq", d2)])
        p.dma(rpk[:], io["ropek_t"][s], writes=[("rpk", d2)])
        for h in range(4):
            bA, bB = nbank(), nbank()
            psA, psB = kb.ps(bA), kb.ps(bB)
            for rc in range(2):
                p.pe(lambda e, psA=psA, cq=cq, rc=rc, h=h: e.matmul(psA[0:96, :], lhsT=wuq[:, rc, h * 96:(h + 1) * 96], rhs=cq[:, rc, :], start=(rc == 0), stop=(rc == 1)),
                     reads=["wuq", ("cq", d2)], writes=[("ps", bA)])
            for rc in range(2):
                p.pe(lambda e, psB=psB, cq=cq, rc=rc, h=h: e.matmul(psB[0:96, :], lhsT=wuqr[:, rc, h * 96:(h + 1) * 96], rhs=cq[:, rc, :], start=(rc == 0), stop=(rc == 1)),
                     reads=["wuqr", ("cq", d2)], writes=[("ps", bB)])
            a1, a2 = t1[h % 2], t2[h % 2]
            k = cnt["o"] % 4
            cnt["o"] += 1
            og = ostage[k]
            p.dve(lambda e, a1=a1, psA=psA, rpq=rpq: e.tensor_tensor(out=a1[:], in0=psA[0:96, :], in1=rpq[:, 0, :], op=ALU.mult),
                  reads=[("ps", bA), ("rpq", d2)], writes=[("t1", h % 2)])
            p.dve(lambda e, a2=a2, psB=psB, rpq=rpq: e.tensor_tensor(out=a2[:], in0=psB[0:96, :], in1=rpq[:, 1, :], op=ALU.mult),
                  reads=[("ps", bB), ("rpq", d2)], writes=[("t2", h % 2)])
            p.dve(lambda e, a1=a1, a2=a2: e.tensor_tensor(out=a1[:], in0=a1[:], in1=a2[:], op=ALU.add),
                  reads=[("t1", h % 2), ("t2", h % 2)], writes=[("t1", h % 2)])
            p.dve(lambda e, a1=a1, og=og, rq=rq: e.tensor_tensor(out=og[0:96, :], in0=a1[:], in1=rq[0:96, :], op=ALU.mult),
                  reads=[("t1", h % 2), ("rq", d2)], writes=[("ostg", k)])
            p.dma(io["qml_d"][h, :, tsl], og[0:96, :], reads=[("ostg", k)], writes=[("qml_d", s, h)])
            b = nbank()
            ps = kb.ps(b)
            p.pe(lambda e, ps=ps, ckv=ckv, h=h: e.matmul(ps[0:64, :], lhsT=wukv[:, h * 128:h * 128 + 64], rhs=ckv[:], start=True, stop=True),
                 reads=["wukv", ("ckv", d2)], writes=[("ps", b)])
            k = cnt["o"] % 4
            cnt["o"] += 1
            og = ostage[k]
            p.dve(lambda e, og=og, ps=ps, rkv=rkv: e.tensor_tensor(out=og[0:64, :], in0=ps[0:64, :], in1=rkv[0:64, :], op=ALU.mult),
                  reads=[("ps", b), ("rkv", d2)], writes=[("ostg", k)])
            p.dma(io["kml_d"][h, :, tsl], og[0:64, :], reads=[("ostg", k)], writes=[("kml_d", s, h)])
        a1, a2 = t1[0], t2[0]
        k = cnt["o"] % 4
        cnt["o"] += 1
        og = ostage[k]
        p.dve(lambda e, a1=a1, krr=krr, rpk=rpk: e.tensor_tensor(out=a1[0:32, :], in0=krr[:, 0, :], in1=rpk[:, 0, :], op=ALU.mult),
              reads=[("krr", d2), ("rpk", d2)], writes=[("t1", 0)])
        p.dve(lambda e, a2=a2, krr=krr, rpk=rpk: e.tensor_tensor(out=a2[0:32, :], in0=krr[:, 1, :], in1=rpk[:, 1, :], op=ALU.mult),
              reads=[("krr", d2), ("rpk", d2)], writes=[("t2", 0)])
        p.dve(lambda e, a1=a1, a2=a2, og=og: e.tensor_tensor(out=og[0:32, :], in0=a1[0:32, :], in1=a2[0:32, :], op=ALU.add),
              reads=[("t1", 0), ("t2", 0)], writes=[("ostg", k)])
        p.dma(io["krl_d"][:, tsl], og[0:32, :], reads=[("ostg", k)], writes=[("krl_d", s)])
        for ck in range(4):
            gck = s * 4 + ck
            b = nbank()
            ps = kb.ps(b)
            for h in range(4):
                p.pe(lambda e, ps=ps, ckv=ckv, ck=ck, h=h: e.matmul(ps[:, h * 64:(h + 1) * 64], lhsT=ckv[:, ck * 128:(ck + 1) * 128], rhs=wukv[:, h * 128 + 64:h * 128 + 128],
                                                                 start=True, stop=True),
                     reads=["wukv", ("ckv", d2)], writes=[("ps", b)])
            vm = vmst[gck % 2]
            p.act(lambda e, vm=vm, ps=ps, rkt=rkt, ck=ck: e.activation(out=vm[:], in_=ps[:, 0:256], func=AF.Copy, scale=rkt[:, ck:ck + 1]),
                  reads=[("ps", b), ("rkt", d2)], writes=[("vmst", gck % 2)])
            p.dma(io["vml_d"][gck * 128:(gck + 1) * 128, :], vm[:], reads=[("vmst", gck % 2)], writes=[("vml_d", gck)])

        for ck in range(4):
            gck = s * 4 + ck
            tcs = slice(gck * 128, (gck + 1) * 128)
            vs = vstage[gck % 2]
            b1, b2 = nbank(), nbank()
            ps1, ps2 = kb.ps(b1), kb.ps(b2)
            for f in range(8):
                p.pe(lambda e, ps1=ps1, f=f, tcs=tcs: e.matmul(ps1[:, 0:256], lhsT=hT[:, f, tcs], rhs=win[:, f, C_SBV:C_SBV + 256], start=(f == 0), stop=(f == 7)),
                     reads=[("win", f), ("hT", s)], writes=[("ps", b1)])
            for f in range(8):
                p.pe(lambda e, ps1=ps1, f=f, tcs=tcs: e.matmul(ps1[:, 256:512], lhsT=hT[:, f, tcs], rhs=win[:, f, C_MOV:C_MOV + 256], start=(f == 0), stop=(f == 7)),
                     reads=[("win", f), ("hT", s)], writes=[("ps", b1)])
            for f in range(8):
                p.pe(lambda e, ps2=ps2, f=f, tcs=tcs: e.matmul(ps2[:, 0:64], lhsT=hT[:, f, tcs], rhs=win[:, f, C_NSKV + 192:C_NSKV + 256], start=(f == 0), stop=(f == 7)),
                     reads=[("win", f), ("hT", s)], writes=[("ps", b2)])
            for f in range(8):
                p.pe(lambda e, ps2=ps2, f=f, tcs=tcs: e.matmul(ps2[:, 64:128], lhsT=hT[:, f, tcs], rhs=win[:, f, C_NSKV + 320:C_NSKV + 384], start=(f == 0), stop=(f == 7)),
                     reads=[("win", f), ("hT", s)], writes=[("ps", b2)])
            p.act(lambda e, vs=vs, ps1=ps1: e.copy(out=vs[:, 0:512], in_=ps1[:]), reads=[("ps", b1)], writes=[("vstg", gck % 2)])
            p.dve(lambda e, vs=vs, ps2=ps2: e.tensor_copy(out=vs[:, 512:640], in_=ps2[:, 0:128]), reads=[("ps", b2)], writes=[("vstg", gck % 2)])
            p.dma(io["vsb_d"][tcs, :], vs[:, 0:256], reads=[("vstg", gck % 2)], writes=[("vsb_d", gck)])
            p.dma(io["vmo_d"][tcs, :], vs[:, 256:512], reads=[("vstg", gck % 2)], writes=[("vmo_d", gck)])
            p.dma(io["vsw_d"][tcs, :], vs[:, 512:640], reads=[("vstg", gck % 2)], writes=[("vsw_d", gck)])


A_OUTS = {
    "hT_d": ([1024, TOK], BF16),
    "qsb_d": ([256, TOK], BF16), "ksb_d": ([256, TOK], BF16), "vsb_d": ([TOK, 256], BF16),
    "qmo_d": ([256, TOK], BF16), "kmo_d": ([256, TOK], BF16), "vmo_d": ([TOK, 256], BF16),
    "qml_d": ([4, 96, TOK], BF16), "kml_d": ([4, 64, TOK], BF16), "krl_d": ([32, TOK], BF16), "vml_d": ([TOK, 256], BF16),
    "qns_d": ([256, TOK], BF16), "kcv_d": ([128, TOK], BF16), "ksl_d": ([64, TOK], BF16), "kwi_d": ([64, TOK], BF16),
    "vsw_d": ([TOK, 128], BF16), "gns_d": ([12, TOK], F32), "qme_d": ([256, TOK], BF16),
}


def load_consts(kb, io):
    p = kb.p
    c = {}
    c["identf"] = kb.sb("identf", [128, 128], F32)
    c["identb"] = kb.sb("identb", [128, 128], BF16)
    c["onesb"] = kb.sb("onesb", [128, 128], BF16)
    c["onesf"] = kb.sb("onesf", [128, 128], F32)
    c["cstf"] = kb.sb("cstf", [128, 8], F32)
    p.dma(c["identf"][:], io["c_ident"], writes=["identf"])
    p.dma(c["identb"][:], io["c_ident"], writes=["identb"], q="pool")
    p.dma(c["onesf"][:], io["c_ones"], writes=["onesf"])
    p.dma(c["onesb"][:], io["c_ones"], writes=["onesb"], q="pool")
    p.dma(c["cstf"][:], io["c_cst"], writes=["cst"])
    c["eps_rms"] = c["cstf"][:, 0:1]
    c["eps_ln"] = c["cstf"][:, 1:2]
    c["tiny"] = c["cstf"][:, 2:3]
    c["zrow"] = kb.sb("zrow", [1, 8], BF16)
    p.pool(lambda e: e.memset(c["zrow"][:], 0.0), writes=["zrow"])
    return c


def host_consts():
    cst = np.zeros((128, 8), np.float32)
    cst[:, 0] = RMS_EPS
    cst[:, 1] = LN_EPS
    cst[:, 2] = 1e-30
    cst[:, 3] = 1.0
    return {"c_ident": np.eye(128, dtype=np.float32), "c_ones": np.ones((128, 128), np.float32), "c_cst": cst}


def rope_tables(r):
    half = 16
    freqs = np.power(np.float32(10000.0), -np.arange(half, dtype=np.float32) / half).astype(np.float32)
    rq = np.zeros((NSLOT, 96, 2, 512), np.float32)
    rk = np.zeros((NSLOT, 32, 2, 512), np.float32)
    sc = np.float32(96.0 ** -0.5)
    for s in range(NSLOT):
        pos = (512 * (2 * s + r) + np.arange(512)).astype(np.float32)
        ang = pos[None, :] * freqs[:, None]
        cos, sin = np.cos(ang).astype(np.float32), np.sin(ang).astype(np.float32)
        c2 = np.concatenate([cos, cos], 0)
        s2 = np.concatenate([sin, sin], 0)
        rq[s, 0:64, 0, :] = sc
        rq[s, 64:96, 0, :] = sc * c2
        rq[s, 64:96, 1, :] = sc * s2
        rk[s, :, 0, :] = c2
        rk[s, :, 1, :] = s2
    return rq, rk


def build_a():
    kb = KB()
    io = {}
    io["h_tok"] = kb.din("h_tok", [TOK, D])
    io["c_ident"] = kb.din("c_ident", [128, 128])
    io["c_ones"] = kb.din("c_ones", [128, 128])
    io["c_cst"] = kb.din("c_cst", [128, 8])
    io["ropeq_t"] = kb.din("ropeq_t", [NSLOT, 96, 2, 512])
    io["ropek_t"] = kb.din("ropek_t", [NSLOT, 32, 2, 512])
    w = {"w_in": kb.din("w_in", [D, IN_TOTAL]), "w_uq": kb.din("w_uq", [256, 384]), "g_cq": kb.din("g_cq", [256]),
         "w_ukv": kb.din("w_ukv", [128, 512]), "g_ckv": kb.din("g_ckv", [128])}
    for n, (shp, dt) in A_OUTS.items():
        io[n] = kb.dout(n, shp, dt)
    c = load_consts(kb, io)
    phase_a(kb, c, io, w)
    return kb.finish()


def bconsts_host(r):
    bf = ml_dtypes.bfloat16
    o = {}
    kl = np.arange(128)[:, None]
    ql = np.arange(512)[None, :]
    cm = np.zeros((8, 128, 512), np.float32)
    cms = np.zeros((8, 128, 512), np.float32)
    for jj in range(8):
        kp = 128 * jj + kl
        qp = 512 * r + ql
        cm[jj] = np.where(kp <= qp, 0.0, MASKV)
        cms[jj] = np.where(kp < qp, 0.0, MASKV)
    o["c_cm"] = cm.transpose(1, 0, 2).astype(bf)
    o["c_cms"] = cms.transpose(1, 0, 2).astype(bf)
    wm = np.zeros((12, 128, 512), np.float32)
    for ji, jrel in enumerate(range(-4, 8)):
        dist = 512 * r + ql - 128 * jrel - kl
        wm[ji] = np.where((dist >= 0) & (dist < 512), 0.0, MASKV)
    o["c_wm"] = wm.transpose(1, 0, 2).astype(bf)
    pm = np.zeros((3, 128, 512), np.float32)
    for ii, idx in enumerate((6, 7, 8)):
        pm[ii] = np.where(16 * kl + 31 - 512 * r - ql <= 1024 * (idx - 6), 0.0, MASKV)
    o["c_pm"] = pm.transpose(1, 0, 2).astype(bf)
    kp = np.arange(S)
    o["c_kaug"] = np.stack([kp // 128, kp % 128, np.ones(S), np.ones(S)]).astype(bf)
    ce = 16 * np.arange(512) + 31
    caug = np.stack([ce // 128, ce % 128, np.ones(512), np.ones(512)]).astype(np.float32)
    caug[0, 511] = -30000.0
    o["c_caug"] = caug.astype(bf)
    tl = np.arange(TOK)
    qp = 512 * (2 * (tl // 512) + r) + tl % 512
    qa = np.zeros((4, 4, TOK), np.float32)
    for h in range(4):
        sl = SLOPES[h]
        qa[h, 0] = 128 * sl
        qa[h, 1] = sl
        qa[h, 2] = -sl * 128 * (qp // 128)
        qa[h, 3] = -sl * (qp % 128)
    o["c_qaug"] = qa.astype(bf)
    o["c_g"] = (np.arange(S)[None, :] // 64 == np.arange(128)[:, None]).astype(np.float32).astype(bf)
    o["c_tm"] = (np.arange(S)[None, :] // 256 == np.arange(32)[:, None]).astype(np.float32).astype(bf)
    n = np.arange(512)[:, None]
    s_ = np.arange(128)[None, :]
    ov = ((16 * n < 64 * s_ + 64) & (16 * n + 32 > 64 * s_)).astype(np.float32)
    ov = np.concatenate([ov, np.ones((512, 1), np.float32)], 1)
    ov[511] = 0.0
    o["c_nui"] = -(np.arange(128)[:, None] >= np.arange(128)[None, :]).astype(np.float32).astype(bf)
    o["c_ov"] = ov.reshape(4, 128, 129).transpose(1, 0, 2).astype(bf)
    vb = np.zeros((8, 4, 32), np.float32)
    own_t = np.zeros((8, 4, 32), np.float32)
    for s in range(8):
        for qb in range(4):
            own = (4 * (2 * s + r) + qb) // 2
            vb[s, qb] = np.where(np.arange(32) < own, 0.0, -1e30)
            own_t[s, qb, own] = 1.0
    o["c_vb"] = np.broadcast_to(vb[None], (128, 8, 4, 32)).copy()
    o["c_own"] = np.broadcast_to(own_t[None], (128, 8, 4, 32)).copy()
    M = np.zeros((8, 128, 4, 128), np.float32)
    C = np.zeros((8, 128, 4, 128), np.float32)
    sid = np.arange(128)[None, :]
    for s in range(8):
        for qb in range(4):
            qpos = 512 * (2 * s + r) + 128 * qb + np.arange(128)[:, None]
            cur = qpos // 64
            forced_cur = sid == cur
            forced0 = (sid == 0) & ~forced_cur
            past = (sid < cur) & ~forced0 & ~forced_cur
            M[s, :, qb] = past
            C[s, :, qb] = np.where(forced_cur, 1e30, np.where(forced0, 5e29, np.where(past, 0.0, -1e30)))
    o["c_selm"] = M
    o["c_selc"] = C
    return o


B_CONST_SHAPES = {
    "c_cm": ([128, 8, 512], BF16), "c_cms": ([128, 8, 512], BF16), "c_wm": ([128, 12, 512], BF16), "c_pm": ([128, 3, 512], BF16),
    "c_kaug": ([4, S], BF16), "c_caug": ([4, 512], BF16), "c_qaug": ([4, 4, TOK], BF16),
    "c_g": ([128, S], BF16), "c_tm": ([32, S], BF16), "c_ov": ([128, 4, 129], BF16),
    "c_nui": ([128, 128], BF16), "c_vb": ([128, 8, 4, 32], F32), "c_own": ([128, 8, 4, 32], F32),
    "c_selm": ([8, 128, 4, 128], F32), "c_selc": ([8, 128, 4, 128], F32),
}

B_INS = {
    "qsb_d": ([256, TOK], BF16), "qmo_d": ([256, TOK], BF16), "qml_d": ([4, 96, TOK], BF16), "qns_d": ([256, TOK], BF16),
    "qme_d": ([256, TOK], BF16), "gns_d": ([12, TOK], F32),
    "ksb_f": ([256, S], BF16), "vsb_f": ([S, 256], BF16), "kmo_f": ([256, S], BF16), "vmo_f": ([S, 256], BF16),
    "kml_f": ([4, 64, S], BF16), "krl_f": ([32, S], BF16), "vml_f": ([S, 256], BF16),
    "kcv_f": ([128, S], BF16), "ksl_f": ([64, S], BF16), "kwi_f": ([64, S], BF16), "vsw_f": ([S, 128], BF16),
    "mem": ([256, D], F32),
}


class AttnBufs:
    pass


class KFull:
    def __init__(self, ap):
        self.ap = ap

    def rows(self, lo, hi):
        return ("full", self.ap[lo:hi, :])


class KPair:
    def __init__(self, ap, R, base=0):
        self.ap, self.R, self.base = ap, R, base

    def rows(self, lo, hi):
        return ("pair", self.ap, self.R, self.base + lo, hi - lo)


class VFull:
    def __init__(self, ap, cbase=0):
        self.ap, self.cbase = ap, cbase

    def cols(self, c0):
        return ("full", self.ap, self.cbase + c0)


class VPair:
    def __init__(self, chunks, cbase=0):
        self.chunks, self.cbase = chunks, cbase

    def cols(self, c0):
        return ("pair", self.chunks, self.cbase + c0)


class TMChunks:
    def __init__(self, chunks, c0, c1):
        self.chunks, self.c0, self.c1 = chunks, c0, c1

    def __getitem__(self, key):
        rs, cs = key
        k = rs.start // 1024
        a = self.chunks[k][rs.start - 1024 * k:rs.stop - 1024 * k, self.c0:self.c1]
        return a[:, cs]


def phase_b(kb, c, io, w, branches=("sb", "moba", "mla", "nsa", "mem")):
    nc, p = kb.nc, kb.p
    A = AttnBufs()
    A.KT = [kb.sb("KT", [128, S], BF16) for _ in range(2)]
    A.V = [kb.sb("V", [128, 64, 65], BF16) for _ in range(2)]
    A.QT = kb.sb("QT", [128, 4, TOK], BF16)
    A.cm = kb.sb("cm", [128, 8, 512], BF16)
    A.cms = kb.sb("cms", [128, 8, 512], BF16)
    A.P = [kb.sb("P", [128, 512], BF16) for _ in range(3)]
    A.rden = [kb.sb("rden", [65, 512], F32) for _ in range(2)]
    A.bcs = [kb.sb("bcs", [64, 512], F32) for _ in range(2)]
    A.ost = [kb.sb("ost", [64, 512], BF16) for _ in range(2)]
    A.cnt = {"kt": 0, "v": 0, "P": 0, "fin": 0, "sc": 0}
    p.dma(A.cm[:], io["c_cm"], writes=["cm"])
    p.dma(A.cms[:], io["c_cms"], writes=["cms"])
    for i in range(2):
        p.pool(lambda e, i=i: e.memset(A.V[i][:, :, 64:65], 1.0), writes=[("V", i)])

    def load_K(rows_src, dk, aug=None):
        i = A.cnt["kt"] % 2
        A.cnt["kt"] += 1
        kt = A.KT[i]
        for (src, r0) in rows_src:
            if src[0] == "full":
                n = src[1].shape[0]
                p.dma(kt[r0:r0 + n, :], src[1], writes=[("KT", i)])
            else:
                _, ap, R, row0, n = src
                for rr in range(2):
                    p.dma(kt[r0:r0 + n, :].rearrange("p (s r i) -> p s r i", r=2, i=512)[:, :, rr, :],
                          ap[rr * R + row0:rr * R + row0 + n, :].rearrange("p (s i) -> p s i", i=512), writes=[("KT", i)])
        if aug is not None:
            p.dma(kt[dk:dk + 4, :], aug, writes=[("KT", i)])
        return kt, ("KT", i)

    def load_V(src, col0):
        i = A.cnt["v"] % 2
        A.cnt["v"] += 1
        v = A.V[i]
        sp_ = src.cols(col0)
        if sp_[0] == "full":
            p.dma(v[:, :, 0:64], sp_[1].rearrange("(n p) c -> p n c", p=128)[:, :, sp_[2]:sp_[2] + 64], writes=[("V", i)])
        else:
            _, chunks, cc0 = sp_
            for k, ch in enumerate(chunks):
                for rr in range(2):
                    for s2 in range(2):
                        n0 = 16 * k + 8 * s2 + 4 * rr
                        p.dma(v[:, n0:n0 + 4, 0:64],
                              ch[rr * 1024 + s2 * 512:rr * 1024 + (s2 + 1) * 512, cc0:cc0 + 64].rearrange("(q p) c -> p q c", p=128),
                              writes=[("V", i)])
        return v, ("V", i)

    def load_Q(src_rows, h, dk, aug=None):
        p.dma(A.QT[0:dk, h, :], src_rows, writes=[("QT", h)])
        if aug is not None:
            p.dma(A.QT[dk:dk + 4, h, :], aug, writes=[("QT", h)])
        return ("QT", h)

    def finalize_plain(ops_bank, dst, h, s, gate=None, acc=None, acc_tok=None, first=True, last=True):
        k = A.cnt["fin"] % 2
        A.cnt["fin"] += 1
        ps = kb.ps(ops_bank)
        rd, bcs, ost = A.rden[k], A.bcs[k], A.ost[k]
        bcb = 6 + k

        def part1():
            p.dve(lambda e: e.tensor_scalar(out=rd[64:65, :], in0=ps[64:65, :], scalar1=1e-30, scalar2=None, op0=ALU.max),
                  reads=[("ps", ops_bank)], writes=[("rden", k)])
            p.dve(lambda e: e.reciprocal(out=rd[64:65, :], in_=rd[64:65, :]), reads=[("rden", k)], writes=[("rden", k)])
            if gate is not None:
                gt, gtok, gidx = gate
                p.dve(lambda e: e.tensor_tensor(out=rd[64:65, :], in0=rd[64:65, :], in1=gt[64:65, gidx, :], op=ALU.mult),
                      reads=[("rden", k), gtok], writes=[("rden", k)])

        def part2():
            pb = kb.ps(bcb)
            p.pe(lambda e: e.matmul(pb[0:64, :], lhsT=c["onesf"][64:65, 0:64], rhs=rd[64:65, :], start=True, stop=True),
                 reads=[("rden", k), "onesf"], writes=[("ps", bcb)])
            p.act(lambda e: e.copy(out=bcs[:], in_=pb[0:64, :]), reads=[("ps", bcb)], writes=[("bcs", k)])
            if acc is None:
                p.dve(lambda e: e.tensor_tensor(out=ost[:], in0=ps[0:64, :], in1=bcs[:], op=ALU.mult),
                      reads=[("ps", ops_bank), ("bcs", k)], writes=[("ost", k)])
                p.dma(dst, ost[:], reads=[("ost", k)], writes=[("o_d", h, s, id(dst) % 997)])
            else:
                if first:
                    p.dve(lambda e: e.tensor_tensor(out=acc, in0=ps[0:64, :], in1=bcs[:], op=ALU.mult),
                          reads=[("ps", ops_bank), ("bcs", k)], writes=[acc_tok])
                else:
                    p.dve(lambda e: e.tensor_tensor(out=bcs[:], in0=ps[0:64, :], in1=bcs[:], op=ALU.mult),
                          reads=[("ps", ops_bank), ("bcs", k)], writes=[("bcs", k)])
                    p.dve(lambda e: e.tensor_tensor(out=acc, in0=acc, in1=bcs[:], op=ALU.add),
                          reads=[acc_tok, ("bcs", k)], writes=[acc_tok])
                if last:
                    p.dve(lambda e: e.tensor_copy(out=ost[:], in_=acc), reads=[acc_tok], writes=[("ost", k)])
                    p.dma(dst, ost[:], reads=[("ost", k)], writes=[("o_d", h, s, id(dst) % 997)])
        return part1, part2

    def run_softmax(items, KT, ktok, dk, V, vtok, qh, qtok, pending, hook=None):
        n = len(items)

        def stage1(i):
            it = items[i]
            if "pre" in it:
                it["pre"]()
            b = i % 2
            ps = kb.ps(b)
            ex = it["extras"]
            s, j = it["s"], it["j"]
            p.pe(lambda e: e.matmul(ps[:], lhsT=KT[0:dk, j * 128:(j + 1) * 128], rhs=A.QT[0:dk, qh, s * 512:(s + 1) * 512],
                                    start=True, stop=(len(ex) == 0)),
                 reads=[ktok, qtok] + list(it.get("qreads", ())), writes=[("ps", b)])
            for xi, (lh, rh, toks) in enumerate(ex):
                p.pe(lambda e, lh=lh, rh=rh, xi=xi: e.matmul(ps[:], lhsT=lh, rhs=rh, start=False, stop=(xi == len(ex) - 1)),
                     reads=list(toks), writes=[("ps", b)])
            if "pdst" in it:
                P, ptok = it["pdst"]
            else:
                pk = A.cnt["P"] % 3
                A.cnt["P"] += 1
                P, ptok = A.P[pk][:], ("P", pk)
            it["P"], it["ptok"] = P, ptok
            p.act(lambda e: e.activation(out=P, in_=ps[:], func=AF.Exp), reads=[("ps", b)], writes=[ptok])

        def stage2(i):
            it = items[i]
            P, ptok = it["P"], it["ptok"]
            ob = it["obank"]
            po = kb.ps(ob)
            jv = it.get("jv", it["j"])
            p.pe(lambda e: e.matmul(po[0:65, :], lhsT=V[:, jv, 0:65], rhs=P, start=it["first"], stop=it["last"]),
                 reads=[vtok, ptok], writes=[("ps", ob)])
            if it["last"]:
                p1, p2 = it["fin"]
                p1()
                pending.append([2, p2])

        for i in range(n + 1):
            if i < n:
                stage1(i)
            if i >= 1:
                stage2(i - 1)
            for pd in list(pending):
                pd[0] -= 1
                if pd[0] <= 0:
                    pd[1]()
                    pending.remove(pd)

    def flush(pending):
        for pd in pending:
            pd[1]()
        pending.clear()

    A.load_K, A.load_V, A.load_Q = load_K, load_V, load_Q
    A.finalize_plain, A.run_softmax, A.flush = finalize_plain, run_softmax, flush
    oT = io["oT_d"]

    if "mla" in branches:
        pending = []
        for h in range(4):
            KT, ktok = load_K([(io["kml_f"].rows(h * 64, h * 64 + 64), 0), (io["krl_f"].rows(0, 32), 64)], 96)
            V, vtok = load_V(io["vml_f"], h * 64)
            qtok = load_Q(io["qml_d"][h], h, 96)
            items = []
            for s in range(NSLOT):
                nkb = 8 * s + 8
                ob = 4 + (s % 2)
                for j in range(nkb):
                    jj = j - 8 * s
                    ex = []
                    if jj >= 0:
                        ex.append((c["identb"][:], A.cm[:, jj, :], ["identb", "cm"]))
                    it = dict(j=j, s=s, extras=ex, first=(j == 0), last=(j == nkb - 1), obank=ob)
                    if j == nkb - 1:
                        it["fin"] = finalize_plain(ob, oT[2, h * 64:(h + 1) * 64, s * 512:(s + 1) * 512], h, s)
                    items.append(it)
            run_softmax(items, KT, ktok, 96, V, vtok, h, qtok, pending)
        flush(pending)

    if "mem" in branches:
        pending = []
        memst = kb.sb("memst", [128, 2, 1024], F32)
        memT = kb.sb("memT", [128, 8, 256], BF16)
        wmk = kb.sb("wmk", [128, 8, 512], BF16)
        KTm = kb.sb("KTm", [64, 4, 256], BF16)
        Vm = kb.sb("Vm", [128, 2, 4, 65], BF16)
        p.dma(memst[:], io["mem"].rearrange("(n p) c -> p n c", p=128), writes=["memst"])
        p.dma(wmk[:], w["w_mem_kv"].rearrange("(f p) c -> p f c", p=128), writes=["wmk"], q="pool")
        p.pool(lambda e: e.memset(Vm[:, :, :, 64:65], 1.0), writes=["Vm"])
        for kc in range(2):
            for half in range(2):
                ps = kb.ps(7)
                for jx in range(4):
                    f = half * 4 + jx
                    p.pe(lambda e, ps=ps, kc=kc, f=f, jx=jx: e.transpose(out=ps[:, jx * 128:(jx + 1) * 128], in_=memst[:, kc, f * 128:(f + 1) * 128], identity=c["identf"][:]),
                         reads=["memst", "identf"], writes=[("ps", 7)])
                p.dve(lambda e, ps=ps, kc=kc, half=half: e.tensor_copy(out=memT[:, half * 4:half * 4 + 4, kc * 128:(kc + 1) * 128], in_=ps[:].rearrange("p (j t) -> p j t", j=4)),
                      reads=[("ps", 7)], writes=["memT"])
        for h in range(4):
            ps = kb.ps(7)
            for f in range(8):
                p.pe(lambda e, ps=ps, f=f, h=h: e.matmul(ps[0:64, 0:256], lhsT=wmk[:, f, h * 64:(h + 1) * 64], rhs=memT[:, f, :], start=(f == 0), stop=(f == 7)),
                     reads=["wmk", "memT"], writes=[("ps", 7)])
            p.dve(lambda e, ps=ps, h=h: e.tensor_copy(out=KTm[:, h, :], in_=ps[0:64, 0:256]), reads=[("ps", 7)], writes=["KTm"])
        for kc in range(2):
            ps = kb.ps(7)
            for f in range(8):
                p.pe(lambda e, ps=ps, f=f, kc=kc: e.matmul(ps[:, 0:256], lhsT=memT[:, f, kc * 128:(kc + 1) * 128], rhs=wmk[:, f, 256:512], start=(f == 0), stop=(f == 7)),
                     reads=["wmk", "memT"], writes=[("ps", 7)])
            p.dve(lambda e, ps=ps, kc=kc: e.tensor_copy(out=Vm[:, kc, :, 0:64], in_=ps[:, 0:256].rearrange("p (h d) -> p h d", h=4)),
                  reads=[("ps", 7)], writes=["Vm"])
        for h in range(4):
            qtok = load_Q(io["qme_d"][h * 64:(h + 1) * 64, :], h, 64)
            items = []
            for s in range(NSLOT):
                ob = 4 + (s % 2)
                for j in range(2):
                    it = dict(j=j, s=s, extras=[], first=(j == 0), last=(j == 1), obank=ob)
                    if j == 1:
                        it["fin"] = finalize_plain(ob, oT[4, h * 64:(h + 1) * 64, s * 512:(s + 1) * 512], h, s)
                    items.append(it)
            run_softmax(items, KTm[:, h, :], "KTm", 64, Vm[:, :, h, :], "Vm", h, qtok, pending)
        flush(pending)

    if "moba" in branches:
        pending = []
        vbt = kb.sb("vbt", [128, 8, 4, 32], F32)
        ownt = kb.sb("ownt", [128, 8, 4, 32], F32)
        kmf = kb.sb("kmf", [64, 32], F32)
        kmb = kb.sb("kmb", [64, 32], BF16)
        gsv = kb.sb("gsv", [128, 4, 32], F32)
        m8 = kb.sb("m8", [128, 4, 8], F32)
        m1p = kb.sb("m1p", [128, 4, 128], F32)
        m1 = m1p[:, :, 64:96]
        m2 = kb.sb("m2", [128, 4, 32], F32)
        p.pool(lambda e: e.memset(m1p[:], 0.0), writes=["m1"])
        p.dma(vbt[:], io["c_vb"], writes=["vbt"])
        p.dma(ownt[:], io["c_own"], writes=["ownt"])
        for h in range(4):
            KT, ktok = load_K([(io["kmo_f"].rows(h * 64, h * 64 + 64), 0), (("full", io["c_tm"]), 64)], 96, aug=io["c_kaug"])
            V, vtok = load_V(io["vmo_f"], h * 64)
            qtok = ("QT", h)
            p.dma(A.QT[0:64, h, :], io["qmo_d"][h * 64:(h + 1) * 64, :], writes=[qtok])
            p.dma(A.QT[96:100, h, :], io["c_qaug"][h], writes=[qtok])
            p.dve(lambda e, KT=KT: e.tensor_reduce(out=kmf[:], in_=KT[0:64, :].rearrange("p (n k) -> p n k", k=256), axis=AX.X, op=ALU.add),
                  reads=[ktok], writes=["kmf"])
            p.dve(lambda e: e.tensor_scalar_mul(out=kmb[:], in0=kmf[:], scalar1=1.0 / 256.0), reads=["kmf"], writes=["kmb"])

            def make_pre(s, h=h, qtok=qtok):
                def pre():
                    ps = kb.ps(7)
                    for qb in range(4):
                        c0 = s * 512 + qb * 128
                        p.pe(lambda e, qb=qb, c0=c0: e.matmul(ps[:, qb * 32:(qb + 1) * 32], lhsT=A.QT[0:64, h, c0:c0 + 128], rhs=kmb[:], start=True, stop=True),
                             reads=[qtok, "kmb"], writes=[("ps", 7)])
                    p.dve(lambda e: e.tensor_tensor(out=gsv[:], in0=ps[:, 0:128].rearrange("p (a b) -> p a b", a=4), in1=vbt[:, s, :, :], op=ALU.add),
                          reads=[("ps", 7), "vbt"], writes=["gsv"])
                    for qb in range(4):
                        p.dve(lambda e, qb=qb: e.max(out=m8[:, qb, :], in_=gsv[:, qb, :]), reads=["gsv"], writes=["m8"])
                    for qb in range(4):
                        p.dve(lambda e, qb=qb: e.tensor_scalar(out=m1[:, qb, :], in0=gsv[:, qb, :], scalar1=m8[:, qb, 2:3], scalar2=None, op0=ALU.is_ge),
                              reads=["gsv", "m8"], writes=["m1"])
                    p.dve(lambda e: e.tensor_scalar(out=m2[:], in0=gsv[:], scalar1=-1e29, scalar2=None, op0=ALU.is_gt), reads=["gsv"], writes=["m2"])
                    p.dve(lambda e: e.tensor_tensor(out=m1, in0=m1, in1=m2[:], op=ALU.mult), reads=["m1", "m2"], writes=["m1"])
                    p.dve(lambda e: e.tensor_tensor(out=m1, in0=m1, in1=ownt[:, s, :, :], op=ALU.add), reads=["m1", "ownt"], writes=["m1"])
                    p.dve(lambda e: e.tensor_scalar(out=m1, in0=m1, scalar1=1.0, scalar2=-MASKV, op0=ALU.subtract, op1=ALU.mult),
                          reads=["m1"], writes=["m1"])
                    ps2 = kb.ps(7)
                    for qb in range(4):
                        p.pe(lambda e, qb=qb: e.transpose(out=ps2[:, qb * 128:(qb + 1) * 128], in_=m1p[:, qb, :], identity=c["identf"][:]),
                             reads=["m1", "identf"], writes=[("ps", 7)])
                    p.act(lambda e: e.copy(out=A.QT[64:96, h, s * 512:(s + 1) * 512], in_=ps2[64:96, :]), reads=[("ps", 7)], writes=[("QTs", h, s)])
                return pre

            items = []
            for s in range(NSLOT):
                nkb = 8 * s + 8
                ob = 4 + (s % 2)
                for j in range(nkb):
                    jj = j - 8 * s
                    ex = []
                    if jj >= 0:
                        ex.append((c["identb"][:], A.cm[:, jj, :], ["identb", "cm"]))
                    it = dict(j=j, s=s, extras=ex, first=(j == 0), last=(j == nkb - 1), obank=ob, qreads=[("QTs", h, s)])
                    if j == 0:
                        it["pre"] = make_pre(s)
                    if j == nkb - 1:
                        it["fin"] = finalize_plain(ob, oT[1, h * 64:(h + 1) * 64, s * 512:(s + 1) * 512], h, s)
                    items.append(it)
            run_softmax(items, KT, ktok, 100, V, vtok, h, qtok, pending)
        flush(pending)

    if "sb" in branches:
        nui = kb.sb("nui", [128, 128], BF16)
        negone = kb.sb("negone", [1, 128], BF16)
        p.dma(nui[:], io["c_nui"], writes=["nui"])
        p.pool(lambda e: e.memset(negone[:], -1.0), writes=["negone"])
        E = [kb.sb("E", [128, 512], F32) for _ in range(2)]
        SP = [kb.sb("SP", [128, 512], BF16) for _ in range(2)]
        AB = [kb.sb("AB", [128, 512], BF16) for _ in range(2)]
        carf = [kb.sb("carf", [1, 512], F32) for _ in range(2)]
        carb = [kb.sb("carb", [1, 512], BF16) for _ in range(2)]
        sbo = [kb.sb("sbo", [64, 512], BF16) for _ in range(2)]
        one_ap = c["cstf"][:, 3:4]
        for hp in range(2):
            hs = (2 * hp, 2 * hp + 1)
            KTs, ktoks, Vs, vtoks, qtoks = [], [], [], [], []
            for h in hs:
                KT, ktok = load_K([(io["ksb_f"].rows(h * 64, h * 64 + 64), 0)], 64)
                V, vtok = load_V(io["vsb_f"], h * 64)
                qtok = load_Q(io["qsb_d"][h * 64:(h + 1) * 64, :], h, 64)
                KTs.append(KT); ktoks.append(ktok); Vs.append(V); vtoks.append(vtok); qtoks.append(qtok)
            merged = []
            for s_ in range(NSLOT):
                nkb = 8 * s_ + 8
                for j in range(nkb - 1, -1, -1):
                    for st in range(2):
                        merged.append(dict(j=j, s=s_, st=st, h=hs[st], first=(j == nkb - 1), last=(j == 0)))
            for i, it in enumerate(merged):
                it["i"] = i

            def qk(it, bank, more):
                ps = kb.ps(bank)
                j, s_, st, h = it["j"], it["s"], it["st"], it["h"]
                jj = j - 8 * s_
                KT = KTs[st]
                p.pe(lambda e: e.matmul(ps[:], lhsT=KT[0:64, j * 128:(j + 1) * 128], rhs=A.QT[0:64, h, s_ * 512:(s_ + 1) * 512],
                                        start=True, stop=(jj < 0 and not more)),
                     reads=[ktoks[st], qtoks[st]], writes=[("ps", bank)])
                if jj >= 0:
                    p.pe(lambda e: e.matmul(ps[:], lhsT=c["identb"][:], rhs=A.cms[:, jj, :], start=False, stop=(not more)),
                         reads=["identb", "cms"], writes=[("ps", bank)])
                return ps

            def s1(it):
                k = it["i"] % 2
                b4 = it["i"] % 4
                ps = qk(it, b4, False)
                p.act(lambda e: e.activation(out=E[k][:], in_=ps[:], func=AF.Exp), reads=[("ps", b4)], writes=[("E", k)])
                p.act(lambda e: e.activation(out=SP[k][:], in_=E[k][:], func=AF.Ln, bias=one_ap), reads=[("E", k), "cst"], writes=[("SP", k)])

            def s2a(it):
                k = it["i"] % 2
                b4 = it["i"] % 4
                st = it["st"]
                ps = kb.ps(b4)
                fin_carry = not it["first"]
                p.pe(lambda e: e.matmul(ps[:], lhsT=nui[:], rhs=SP[k][:], start=False, stop=(not fin_carry)),
                     reads=["nui", ("SP", k)], writes=[("ps", b4)])
                if fin_carry:
                    p.pe(lambda e: e.matmul(ps[:], lhsT=negone[0:1, :], rhs=carb[st][0:1, :], start=False, stop=True),
                         reads=["negone", ("carb", st)], writes=[("ps", b4)])
                if not it["last"]:
                    pc = kb.ps(6 + st)
                    p.pe(lambda e: e.matmul(pc[0:1, :], lhsT=c["onesb"][:, 0:1], rhs=SP[k][:], start=True, stop=True),
                         reads=["onesb", ("SP", k)], writes=[("ps", 6 + st)])
                    if it["first"]:
                        p.dve(lambda e: e.tensor_copy(out=carf[st][:], in_=pc[0:1, :]), reads=[("ps", 6 + st)], writes=[("carf", st)])
                    else:
                        p.dve(lambda e: e.tensor_tensor(out=carf[st][:], in0=carf[st][:], in1=pc[0:1, :], op=ALU.add),
                              reads=[("ps", 6 + st), ("carf", st)], writes=[("carf", st)])
                    p.dve(lambda e: e.tensor_copy(out=carb[st][:], in_=carf[st][:]), reads=[("carf", st)], writes=[("carb", st)])
                p.act(lambda e: e.activation(out=AB[k][:], in_=ps[:], func=AF.Exp), reads=[("ps", b4)], writes=[("AB", k)])

            def s2b(it):
                k = it["i"] % 2
                st, j, s_, h = it["st"], it["j"], it["s"], it["h"]
                po = kb.ps(4 + st)
                V = Vs[st]
                p.pe(lambda e: e.matmul(po[0:64, :], lhsT=V[:, j, 0:64], rhs=AB[k][:], start=it["first"], stop=it["last"]),
                     reads=[vtoks[st], ("AB", k)], writes=[("ps", 4 + st)])
                if it["last"]:
                    p.dve(lambda e: e.tensor_copy(out=sbo[st][:], in_=po[0:64, :]), reads=[("ps", 4 + st)], writes=[("sbo", st)])
                    p.dma(oT[0, h * 64:(h + 1) * 64, s_ * 512:(s_ + 1) * 512], sbo[st][:], reads=[("sbo", st)], writes=[("o_sb", h, s_)])

            n = len(merged)
            for i in range(n + 2):
                if i < n:
                    s1(merged[i])
                if 1 <= i <= n:
                    s2a(merged[i - 1])
                if i >= 2:
                    s2b(merged[i - 2])

    if "nsa" in branches:
        pending = []
        G = kb.sb("G", [128, S], BF16)
        OV = kb.sb("OV", [128, 4, 129], BF16)
        pm = kb.sb("pm", [128, 3, 512], BF16)
        wm = kb.sb("wm", [128, 12, 512], BF16)
        wphi = kb.sb("wphi", [128, 32, 128], BF16)
        w2 = kb.sb("w2", [128, 2, 64], BF16)
        peT = kb.sb("peT", [128, 32], BF16)
        peb = kb.sb("peb", [128, 2], F32)
        gx = kb.sb("gx", [128, 512], F32)
        gt_ = kb.sb("gtmp", [128, 512], F32)
        gact = [kb.sb("gact", [128, 512], BF16) for _ in range(2)]
        kcT = kb.sb("kcT", [68, 512], BF16)
        Vc = kb.sb("Vc", [128, 4, 65], BF16)
        psave = kb.sb("psave", [128, 4, 4, 512], BF16)
        gts = [kb.sb("gts", [65, 512], F32) for _ in range(4)]
        nacc = kb.sb("nacc", [64, 4, 512], F32)
        impacc = kb.sb("impacc", [128, 4, 128], F32)
        rdn = kb.sb("rdn", [128, 2], F32)
        selm = [kb.sb("selm", [128, 4, 128], F32) for _ in range(2)]
        selc = [kb.sb("selc", [128, 4, 128], F32) for _ in range(2)]
        scr2 = kb.sb("scr2", [128, 4, 128], F32)
        m16 = kb.sb("m16", [128, 4, 16], F32)
        selv = kb.sb("selv", [128, 4, 128], F32)
        SelT = [kb.sb("SelT", [128, 512], BF16) for _ in range(2)]
        gcnt = {"g": 0}
        p.dma(G[:], io["c_g"], writes=["G"])
        p.dma(OV[:], io["c_ov"], writes=["OV"])
        p.dma(pm[:], io["c_pm"], writes=["pm"])
        p.dma(wm[:], io["c_wm"], writes=["wm"])
        p.dma(wphi[0:64, :, :], w["w_phi_k1"].rearrange("(t d) h -> d t h", d=64), writes=["wphi"], q="pool")
        p.dma(wphi[64:128, :, :], w["w_phi_v1"].rearrange("(t d) h -> d t h", d=64), writes=["wphi"], q="pool")
        p.dma(w2[:, 0, :], w["w_phi_k2"], writes=["w2"], q="pool")
        p.dma(w2[:, 1, :], w["w_phi_v2"], writes=["w2"], q="pool")
        p.dma(peT[0:64, :], w["nsa_pe"].rearrange("t d -> d t"), writes=["peT"], q="pool", slow=True)
        p.dma(peT[64:128, :], w["nsa_pe"].rearrange("t d -> d t"), writes=["peT"], q="pool", slow=True)
        kcv, kcvtok = load_K([(io["kcv_f"].rows(0, 128), 0)], 128)
        p.pool(lambda e: e.memset(kcT[:], 0.0), writes=["kcT"])
        p.pool(lambda e: e.memset(Vc[:, :, 64:65], 1.0), writes=["Vc"])
        for which in range(2):
            lo = which * 64
            pb = kb.ps(7)
            for t in range(32):
                p.pe(lambda e, t=t, lo=lo, pb=pb: e.matmul(pb[:, 0:1], lhsT=wphi[lo:lo + 64, t, :], rhs=peT[lo:lo + 64, t:t + 1], start=(t == 0), stop=(t == 31)),
                     reads=["wphi", "peT"], writes=[("ps", 7)])
            p.dve(lambda e, pb=pb, which=which: e.tensor_copy(out=peb[:, which:which + 1], in_=pb[:, 0:1]), reads=[("ps", 7)], writes=["peb"])
            ph = kb.ps(6)
            for t in range(32):
                p.pe(lambda e, t=t, lo=lo, ph=ph: e.matmul(ph[:, 0:511], lhsT=wphi[lo:lo + 64, t, :], rhs=kcv[lo:lo + 64, t:t + 16 * 510 + 1:16], start=(t == 0), stop=(t == 31)),
                     reads=["wphi", kcvtok], writes=[("ps", 6)])
            ga = gact[which]
            p.act(lambda e, ph=ph, which=which: e.activation(out=gx[:, 0:511], in_=ph[:, 0:511], func=AF.Identity, bias=peb[:, which:which + 1]),
                  reads=[("ps", 6), "peb"], writes=["gx"])
            p.dve(lambda e: e.tensor_tensor(out=gt_[:, 0:511], in0=gx[:, 0:511], in1=gx[:, 0:511], op=ALU.mult), reads=["gx"], writes=["gtmp"])
            p.dve(lambda e: e.tensor_scalar(out=gt_[:, 0:511], in0=gt_[:, 0:511], scalar1=0.044715, scalar2=1.0, op0=ALU.mult, op1=ALU.add), reads=["gtmp"], writes=["gtmp"])
            p.dve(lambda e: e.tensor_tensor(out=gt_[:, 0:511], in0=gt_[:, 0:511], in1=gx[:, 0:511], op=ALU.mult), reads=["gtmp", "gx"], writes=["gtmp"])
            p.act(lambda e: e.activation(out=gt_[:, 0:511], in_=gt_[:, 0:511], func=AF.Tanh, scale=0.7978845608028654), reads=["gtmp"], writes=["gtmp"])
            p.dve(lambda e: e.tensor_scalar(out=gt_[:, 0:511], in0=gt_[:, 0:511], scalar1=1.0, scalar2=0.5, op0=ALU.add, op1=ALU.mult), reads=["gtmp"], writes=["gtmp"])
            p.pool(lambda e, ga=ga: e.memset(ga[:], 0.0), writes=[("gact", which)])
            p.dve(lambda e, ga=ga: e.tensor_tensor(out=ga[:, 0:511], in0=gt_[:, 0:511], in1=gx[:, 0:511], op=ALU.mult), reads=["gtmp", "gx"], writes=[("gact", which)])
        pk_ = kb.ps(7)
        p.pe(lambda e: e.matmul(pk_[0:64, :], lhsT=w2[:, 0, :], rhs=gact[0][:], start=True, stop=True), reads=["w2", ("gact", 0)], writes=[("ps", 7)])
        p.dve(lambda e: e.tensor_copy(out=kcT[0:64, 0:511], in_=pk_[0:64, 0:511]), reads=[("ps", 7)], writes=["kcT"])
        p.dma(kcT[64:68, :], io["c_caug"], writes=["kcT"])
        pv_ = kb.ps(6)
        for cc in range(4):
            p.pe(lambda e, cc=cc: e.matmul(pv_[:, cc * 64:(cc + 1) * 64], lhsT=gact[1][:, cc * 128:(cc + 1) * 128], rhs=w2[:, 1, :], start=True, stop=True),
                 reads=["w2", ("gact", 1)], writes=[("ps", 6)])
        p.dve(lambda e: e.tensor_copy(out=Vc[:, :, 0:64], in_=pv_[:, 0:256].rearrange("p (c d) -> p c d", c=4)), reads=[("ps", 6)], writes=["Vc"])

        KsT, kstok = load_K([(io["ksl_f"].rows(0, 64), 0)], 64, aug=io["c_kaug"])
        KwT, kwtok = load_K([(io["kwi_f"].rows(0, 64), 0)], 64, aug=io["c_kaug"])
        Vs, vstok = load_V(io["vsw_f"], 0)
        Vw, vwtok = load_V(io["vsw_f"], 64)
        qtoks = [load_Q(io["qns_d"][h * 64:(h + 1) * 64, :], h, 64, aug=io["c_qaug"][h]) for h in range(4)]

        def gate_row(h, br, s):
            k = gcnt["g"] % 4
            gcnt["g"] += 1
            g = gts[k]
            p.dma(g[64:65, :], io["gns_d"][3 * h + br:3 * h + br + 1, s * 512:(s + 1) * 512], writes=[("gts", k)])
            return (g[:].rearrange("p (o n) -> p o n", o=1), ("gts", k), 0)

        for s in range(NSLOT):
            sl = slice(s * 512, (s + 1) * 512)
            ncmp = s // 2 + 1
            dst = lambda h: oT[3, h * 64:(h + 1) * 64, sl]
            p.dma(selm[s % 2][:], io["c_selm"][s], writes=[("selm", s % 2)])
            p.dma(selc[s % 2][:], io["c_selc"][s], writes=[("selc", s % 2)])
            for h in range(4):
                items = []
                for cc in range(ncmp):
                    idx = s - 2 * cc + 6
                    ex = []
                    if idx <= 8:
                        ex.append((c["identb"][:], pm[:, idx - 6, :], ["identb", "pm"]))
                    it = dict(j=cc, s=s, extras=ex, first=(cc == 0), last=(cc == ncmp - 1), obank=4 + (h % 2),
                              pdst=(psave[:, h, cc, :], ("psave", h, cc)))
                    if cc == ncmp - 1:
                        it["fin"] = finalize_plain(4 + (h % 2), dst(h), h, s, gate=gate_row(h, 0, s), acc=nacc[:, h, :], acc_tok=("nacc", h), first=True, last=False)
                    items.append(it)
                run_softmax(items, kcT, "kcT", 68, Vc, "Vc", h, qtoks[h], pending)
            flush(pending)
            for qb in range(4):
                for h in range(4):
                    bk = 6 + ((qb * 4 + h) % 2)
                    pi = kb.ps(bk)
                    for cc in range(ncmp):
                        p.pe(lambda e, pi=pi, h=h, cc=cc, qb=qb: e.matmul(pi[:, 0:129], lhsT=psave[:, h, cc, qb * 128:(qb + 1) * 128], rhs=OV[:, cc, :],
                                                                          start=(cc == 0), stop=(cc == ncmp - 1)),
                             reads=[("psave", h, cc), "OV"], writes=[("ps", bk)])
                    p.dve(lambda e, pi=pi: e.tensor_scalar(out=rdn[:, 0:1], in0=pi[:, 128:129], scalar1=1e-30, scalar2=None, op0=ALU.max),
                          reads=[("ps", bk)], writes=["rdn"])
                    p.dve(lambda e: e.reciprocal(out=rdn[:, 1:2], in_=rdn[:, 0:1]), reads=["rdn"], writes=["rdn"])
                    if h == 0:
                        p.dve(lambda e, pi=pi, qb=qb: e.tensor_scalar(out=impacc[:, qb, :], in0=pi[:, 0:128], scalar1=rdn[:, 1:2], scalar2=None, op0=ALU.mult),
                              reads=[("ps", bk), "rdn"], writes=["impacc"])
                    else:
                        p.dve(lambda e, pi=pi, qb=qb: e.scalar_tensor_tensor(out=impacc[:, qb, :], in0=pi[:, 0:128], scalar=rdn[:, 1:2], in1=impacc[:, qb, :],
                                                                             op0=ALU.mult, op1=ALU.add),
                              reads=[("ps", bk), "rdn", "impacc"], writes=["impacc"])
            sm, sc_ = selm[s % 2], selc[s % 2]
            p.dve(lambda e, sm=sm: e.tensor_tensor(out=impacc[:], in0=impacc[:], in1=sm[:], op=ALU.mult), reads=["impacc", ("selm", s % 2)], writes=["impacc"])
            p.dve(lambda e, sc_=sc_: e.tensor_tensor(out=impacc[:], in0=impacc[:], in1=sc_[:], op=ALU.add), reads=["impacc", ("selc", s % 2)], writes=["impacc"])
            for qb in range(4):
                p.dve(lambda e, qb=qb: e.max(out=m16[:, qb, 0:8], in_=impacc[:, qb, :]), reads=["impacc"], writes=["m16"])
                p.dve(lambda e, qb=qb: e.match_replace(out=scr2[:, qb, :], in_to_replace=m16[:, qb, 0:8], in_values=impacc[:, qb, :], imm_value=-3.0e38),
                      reads=["impacc", "m16"], writes=["scr2"])
                p.dve(lambda e, qb=qb: e.max(out=m16[:, qb, 8:16], in_=scr2[:, qb, :]), reads=["scr2"], writes=["m16"])
                p.dve(lambda e, qb=qb: e.tensor_scalar(out=selv[:, qb, :], in0=impacc[:, qb, :], scalar1=m16[:, qb, 15:16], scalar2=None, op0=ALU.is_ge),
                      reads=["impacc", "m16"], writes=["selv"])
            p.dve(lambda e: e.tensor_scalar(out=scr2[:], in0=impacc[:], scalar1=-5e29, scalar2=None, op0=ALU.is_gt), reads=["impacc"], writes=["scr2"])
            p.dve(lambda e: e.tensor_tensor(out=selv[:], in0=selv[:], in1=scr2[:], op=ALU.mult), reads=["selv", "scr2"], writes=["selv"])
            p.dve(lambda e: e.tensor_scalar(out=selv[:], in0=selv[:], scalar1=1.0, scalar2=-MASKV, op0=ALU.subtract, op1=ALU.mult), reads=["selv"], writes=["selv"])
            for h in range(4):
                items = []
                js = [j for j in range(8 * s - 4, 8 * s + 8) if j >= 0]
                for j in js:
                    jrel = j - 8 * s
                    it = dict(j=j, s=s, extras=[(c["identb"][:], wm[:, jrel + 4, :], ["identb", "wm"])], first=(j == js[0]), last=(j == js[-1]), obank=4 + (h % 2))
                    if j == js[-1]:
                        it["fin"] = finalize_plain(4 + (h % 2), dst(h), h, s, gate=gate_row(h, 2, s), acc=nacc[:, h, :], acc_tok=("nacc", h), first=False, last=False)
                    items.append(it)
                run_softmax(items, KwT, kwtok, 68, Vw, vwtok, h, qtoks[h], pending)
            flush(pending)
            st_ = SelT[s % 2]
            pt = kb.ps(7)
            for qb in range(4):
                p.pe(lambda e, qb=qb: e.transpose(out=pt[:, qb * 128:(qb + 1) * 128], in_=selv[:, qb, :], identity=c["identf"][:]),
                     reads=["selv", "identf"], writes=[("ps", 7)])
            p.act(lambda e, st_=st_: e.copy(out=st_[:], in_=pt[:]), reads=[("ps", 7)], writes=[("SelT", s % 2)])
            for h in range(4):
                items = []
                nkb = 8 * s + 8
                for j in range(nkb):
                    jj = j - 8 * s
                    ex = [(G[:, j * 128:(j + 1) * 128], st_[:], ["G", ("SelT", s % 2)])]
                    if jj >= 0:
                        ex.append((c["identb"][:], A.cm[:, jj, :], ["identb", "cm"]))
                    it = dict(j=j, s=s, extras=ex, first=(j == 0), last=(j == nkb - 1), obank=4 + (h % 2))
                    if j == nkb - 1:
                        it["fin"] = finalize_plain(4 + (h % 2), dst(h), h, s, gate=gate_row(h, 1, s), acc=nacc[:, h, :], acc_tok=("nacc", h), first=False, last=True)
                    items.append(it)
                run_softmax(items, KsT, kstok, 68, Vs, vstok, h, qtoks[h], pending)
            flush(pending)
    return A


B_WEIGHTS = {"w_mem_kv": [D, 512], "nsa_pe": [32, 64], "w_phi_k1": [2048, 128], "w_phi_k2": [128, 64],
             "w_phi_v1": [2048, 128], "w_phi_v2": [128, 64]}


def build_b(branches):
    kb = KB()
    io = {}
    for n in ("c_ident", "c_ones"):
        io[n] = kb.din(n, [128, 128])
    io["c_cst"] = kb.din("c_cst", [128, 8])
    for n, (shp, dt) in B_CONST_SHAPES.items():
        io[n] = kb.din(n, shp, dt)
    for n, (shp, dt) in B_INS.items():
        io[n] = kb.din(n, shp, dt)
    w = {n: kb.din(n, shp) for n, shp in B_WEIGHTS.items()}
    io["oT_d"] = kb.dout("oT_d", [5, 256, TOK], BF16)
    io["kml_f"] = KFull(io["kml_f"].rearrange("h d t -> (h d) t"))
    for n in ("ksb_f", "kmo_f", "krl_f", "kcv_f", "ksl_f", "kwi_f"):
        io[n] = KFull(io[n])
    for n in ("vsb_f", "vmo_f", "vml_f", "vsw_f"):
        io[n] = VFull(io[n])
    c = load_consts(kb, io)
    phase_b(kb, c, io, w, branches)
    return kb.finish()


def layer_norm_chunk(kb, c, v, vtok, gbc, bbc, out, otok, tmp):
    p = kb.p
    st, mv, sm = tmp["st"], tmp["mv"], tmp["sm"]
    for hf in range(2):
        p.dve(lambda e, hf=hf: e.bn_stats(out=st[:, hf, :], in_=v[:, hf * 512:(hf + 1) * 512]), reads=[vtok], writes=["lnst"])
    p.dve(lambda e: e.bn_aggr(out=mv[:], in_=st[:]), reads=["lnst"], writes=["lnmv"])
    p.act(lambda e: e.activation(out=sm[:, 0:1], in_=mv[:, 1:2], func=AF.Ln, bias=c["eps_ln"]), reads=["lnmv", "cst"], writes=["lnsm"])
    p.act(lambda e: e.activation(out=sm[:, 0:1], in_=sm[:, 0:1], func=AF.Exp, scale=-0.5), reads=["lnsm"], writes=["lnsm"])
    p.dve(lambda e: e.scalar_tensor_tensor(out=sm[:, 1:2], in0=mv[:, 0:1], scalar=-1.0, in1=sm[:, 0:1], op0=ALU.mult, op1=ALU.mult),
          reads=["lnmv", "lnsm"], writes=["lnsm2"])
    p.act(lambda e: e.activation(out=v[:], in_=v[:], func=AF.Identity, scale=sm[:, 0:1], bias=sm[:, 1:2]), reads=[vtok, "lnsm", "lnsm2"], writes=[vtok])
    p.dve(lambda e: e.tensor_tensor(out=v[:], in0=v[:], in1=gbc[:], op=ALU.mult), reads=[vtok, "lngb"], writes=[vtok])
    p.dve(lambda e: e.tensor_tensor(out=out[:], in0=v[:], in1=bbc[:], op=ALU.add), reads=[vtok, "lngb"], writes=[otok])


def phase_c1(kb, c, io, w):
    nc, p = kb.nc, kb.p
    wg = kb.sb("wg", [128, 5, 8, 1024], BF16)
    wbr = kb.sb("wbr", [128, 5, 2, 1024], BF16)
    wout = kb.sb("wout", [128, 8, 1024], BF16)
    bg = kb.sb("bg", [128, 5, 8], F32)
    gbc = kb.sb("gbc", [128, 1024], F32)
    bbc = kb.sb("bbc", [128, 1024], F32)
    wr = kb.sb("wr", [128, 8, 20], F32)
    brow = kb.sb("brow", [1, 20], F32)
    for i in range(5):
        for f in range(8):
            p.dma(wg[:, i, f, :], w["w_gate"][i, f * 128:(f + 1) * 128, :], writes=[("wg", i)], q="pool")
        p.dma(wbr[:, i, :, :], w["w_br"][i].rearrange("(j p) c -> p j c", p=128), writes=["wbr"], q="pool")
    p.dma(wout[:], w["w_out"].rearrange("(f p) c -> p f c", p=128), writes=["wout"], q="pool")
    p.dma(bg[:], w["b_gate"].rearrange("i (c p) -> p i c", p=128), writes=["bg"], slow=True)
    p.dma(gbc[:], w["ln1_g"].partition_broadcast(128), writes=["lngb"])
    p.dma(bbc[:], w["ln1_b"].partition_broadcast(128), writes=["lngb"])
    p.dma(wr[:, :, 0:4], w["w_rg"].rearrange("(f p) g -> p f g", p=128), writes=["wr"], slow=True)
    for g in range(4):
        p.dma(wr[:, :, 4 + 4 * g:8 + 4 * g], w["w_re"][g].rearrange("(f p) e -> p f e", p=128), writes=["wr"], slow=True)
    p.dma(brow[0:1, 0:4], w["b_rg"].rearrange("(o g) -> o g", o=1), writes=["brow"])
    p.dma(brow[0:1, 4:20], w["b_re"].rearrange("(o g) e -> o (g e)", o=1), writes=["brow"])

    hTt = [kb.sb("hTt", [128, 8, 512], BF16) for _ in range(2)]
    oTt = [kb.sb("oTt", [128, 5, 2, 512], BF16)] * 2
    mT = [kb.sb("mT", [128, 8, 512], BF16) for _ in range(2)]
    sg = [kb.sb("sg", [128, 512], F32) for _ in range(2)]
    acc = kb.sb("macc", [128, 512], F32)
    tmpm = kb.sb("tmpm", [128, 512], F32)
    hch = [kb.sb("hch", [128, 1024], F32)] * 2
    vch = [kb.sb("vch", [128, 1024], F32)] * 2
    h1c = [kb.sb("h1c", [128, 1024], F32) for _ in range(2)]
    h1Tf = [kb.sb("h1Tf", [128, 8, 128], F32) for _ in range(2)]
    h1Tb = [kb.sb("h1Tb", [128, 8, 128], BF16) for _ in range(2)]
    lnt = {"st": kb.sb("lnst", [128, 2, 6], F32), "mv": kb.sb("lnmv", [128, 2], F32), "sm": kb.sb("lnsm", [128, 2], F32)}
    lg = kb.sb("lg", [128, 20], F32)
    r1 = kb.sb("r1", [128, 8], F32)
    goh = kb.sb("goh", [128, 4], F32)
    el = kb.sb("el", [128, 4], F32)
    ee = kb.sb("ee", [128, 4], F32)
    ee2 = kb.sb("ee2", [128, 4], F32)
    gd = [kb.sb("gd", [128, 16], F32) for _ in range(2)]
    bk = {"n": 0}

    def nbank():
        b = bk["n"] % 6
        bk["n"] += 1
        return b

    def do_slot(s):
        d2 = s % 2
        tsl = slice(s * 512, (s + 1) * 512)
        ht, ot, mt = hTt[d2], oTt[d2], mT[d2]
        p.dma(ht[:], io["hT_d"].rearrange("(f p) t -> p f t", p=128)[:, :, tsl], writes=[("hTt", d2)])
        for i in range(5):
            p.dma(ot[:, i, :, :], io["oT_d"][i].rearrange("(j p) t -> p j t", p=128)[:, :, tsl], writes=["oTt"])
        for cc in range(8):
            csl = slice(cc * 128, (cc + 1) * 128)
            for i in range(5):
                bgt, bbr = nbank(), nbank()
                pg, pb = kb.ps(bgt), kb.ps(bbr)
                for f in range(8):
                    p.pe(lambda e, pg=pg, i=i, f=f, csl=csl: e.matmul(pg[:], lhsT=wg[:, i, f, csl], rhs=ht[:, f, :], start=(f == 0), stop=(f == 7)),
                         reads=[("wg", i), ("hTt", d2)], writes=[("ps", bgt)])
                for jc in range(2):
                    p.pe(lambda e, pb=pb, i=i, jc=jc, csl=csl: e.matmul(pb[:], lhsT=wbr[:, i, jc, csl], rhs=ot[:, i, jc, :], start=(jc == 0), stop=(jc == 1)),
                         reads=["wbr", "oTt"], writes=[("ps", bbr)])
                sgi = sg[i % 2]
                p.act(lambda e, sgi=sgi, pg=pg, i=i, cc=cc: e.activation(out=sgi[:], in_=pg[:], func=AF.Sigmoid, bias=bg[:, i, cc:cc + 1]),
                      reads=[("ps", bgt), "bg"], writes=[("sg", i % 2)])
                if i == 0:
                    p.dve(lambda e, sgi=sgi, pb=pb: e.tensor_tensor(out=acc[:], in0=sgi[:], in1=pb[:], op=ALU.mult),
                          reads=[("sg", i % 2), ("ps", bbr)], writes=["macc"])
                else:
                    p.dve(lambda e, sgi=sgi, pb=pb: e.tensor_tensor(out=tmpm[:], in0=sgi[:], in1=pb[:], op=ALU.mult),
                          reads=[("sg", i % 2), ("ps", bbr)], writes=["tmpm"])
                    if i < 4:
                        p.dve(lambda e: e.tensor_tensor(out=acc[:], in0=acc[:], in1=tmpm[:], op=ALU.add), reads=["macc", "tmpm"], writes=["macc"])
                    else:
                        p.dve(lambda e, cc=cc: e.tensor_tensor(out=mt[:, cc, :], in0=acc[:], in1=tmpm[:], op=ALU.add), reads=["macc", "tmpm"], writes=[("mT", d2)])
        if "dbg_mt" in io and s == 0:
            p.dma(io["dbg_mt"], mt[:], reads=[("mT", d2)], writes=["dbg_mt"])
        def do_chunk(tc):
            gck = s * 4 + tc
            k2 = gck % 2
            rows = slice(gck * 128, (gck + 1) * 128)
            hc, vc, h1 = hch[k2], vch[k2], h1c[k2]
            p.dma(hc[:], io["h_tok"][rows, :], writes=["hch"])
            for hf in range(2):
                b = nbank()
                ps = kb.ps(b)
                for cc in range(8):
                    p.pe(lambda e, ps=ps, cc=cc, tc=tc, hf=hf: e.matmul(ps[:], lhsT=mt[:, cc, tc * 128:(tc + 1) * 128], rhs=wout[:, cc, hf * 512:(hf + 1) * 512],
                                                                      start=(cc == 0), stop=(cc == 7)),
                         reads=["wout", ("mT", d2)], writes=[("ps", b)])
                p.dve(lambda e, ps=ps, hf=hf: e.scalar_tensor_tensor(out=vc[:, hf * 512:(hf + 1) * 512], in0=hc[:, hf * 512:(hf + 1) * 512], scalar=ALPHA, in1=ps[:],
                                                                      op0=ALU.mult, op1=ALU.add),
                      reads=["hch", ("ps", b)], writes=["vch"])
            layer_norm_chunk(kb, c, vc, "vch", gbc, bbc, h1, ("h1c", k2), lnt)
            p.dma(io["h1_d"][rows, :], h1[:], reads=[("h1c", k2)], writes=[("h1_d", gck)])
            tf, tb = h1Tf[k2], h1Tb[k2]
            for hf in range(2):
                b = nbank()
                ps = kb.ps(b)
                for jx in range(4):
                    f = hf * 4 + jx
                    p.pe(lambda e, ps=ps, f=f, jx=jx: e.transpose(out=ps[:, jx * 128:(jx + 1) * 128], in_=h1[:, f * 128:(f + 1) * 128], identity=c["identf"][:]),
                         reads=[("h1c", k2), "identf"], writes=[("ps", b)])
                p.act(lambda e, ps=ps, hf=hf: e.copy(out=tf[:, hf * 4:hf * 4 + 4, :], in_=ps[:].rearrange("p (j t) -> p j t", j=4)),
                      reads=[("ps", b)], writes=[("h1Tf", k2)])
            p.pool(lambda e: e.tensor_copy(out=tb[:], in_=tf[:]), reads=[("h1Tf", k2)], writes=[("h1Tb", k2)])
            p.dma(io["h1T_d"].rearrange("(f p) t -> p f t", p=128)[:, :, rows], tb[:], reads=[("h1Tb", k2)], writes=[("h1T_d", gck)])
            b = nbank()
            ps = kb.ps(b)
            for f in range(8):
                p.pe(lambda e, ps=ps, f=f: e.matmul(ps[:, 0:20], lhsT=tf[:, f, :], rhs=wr[:, f, :], start=(f == 0), stop=False),
                     reads=[("h1Tf", k2), "wr"], writes=[("ps", b)])
            p.pe(lambda e, ps=ps: e.matmul(ps[:, 0:20], lhsT=c["onesf"][0:1, :], rhs=brow[0:1, :], start=False, stop=True),
                 reads=["onesf", "brow"], writes=[("ps", b)])
            gdt = gd[k2]
            p.dve(lambda e, ps=ps: e.tensor_copy(out=lg[:], in_=ps[:, 0:20]), reads=[("ps", b)], writes=["lg"])
            p.dve(lambda e: e.tensor_reduce(out=r1[:, 0:1], in_=lg[:, 0:4], axis=AX.X, op=ALU.max), reads=["lg"], writes=["r1a"])
            p.dve(lambda e: e.tensor_scalar(out=goh[:], in0=lg[:, 0:4], scalar1=r1[:, 0:1], scalar2=None, op0=ALU.is_equal), reads=["lg", "r1a"], writes=["goh"])
            p.dve(lambda e: e.tensor_scalar(out=ee[:], in0=lg[:, 0:4], scalar1=r1[:, 0:1], scalar2=None, op0=ALU.subtract), reads=["lg", "r1a"], writes=["ee"])
            p.act(lambda e: e.activation(out=ee[:], in_=ee[:], func=AF.Exp), reads=["ee"], writes=["ee"])
            p.dve(lambda e: e.tensor_reduce(out=r1[:, 1:2], in_=ee[:], axis=AX.X, op=ALU.add), reads=["ee"], writes=["r1b"])
            p.dve(lambda e: e.reciprocal(out=r1[:, 1:2], in_=r1[:, 1:2]), reads=["r1b"], writes=["r1b"])
            p.dve(lambda e: e.tensor_scalar(out=el[:], in0=lg[:, 4:8], scalar1=goh[:, 0:1], scalar2=None, op0=ALU.mult), reads=["lg", "goh"], writes=["el"])
            for g in range(1, 4):
                p.dve(lambda e, g=g: e.scalar_tensor_tensor(out=el[:], in0=lg[:, 4 + 4 * g:8 + 4 * g], scalar=goh[:, g:g + 1], in1=el[:], op0=ALU.mult, op1=ALU.add),
                      reads=["lg", "goh", "el"], writes=["el"])
            p.dve(lambda e: e.tensor_reduce(out=r1[:, 2:3], in_=el[:], axis=AX.X, op=ALU.max), reads=["el"], writes=["r1c"])
            p.dve(lambda e: e.tensor_scalar(out=ee[:], in0=el[:], scalar1=r1[:, 2:3], scalar2=None, op0=ALU.subtract), reads=["el", "r1c"], writes=["ee"])
            p.act(lambda e: e.activation(out=ee[:], in_=ee[:], func=AF.Exp), reads=["ee"], writes=["ee"])
            p.dve(lambda e: e.tensor_scalar(out=ee2[:], in0=ee[:], scalar1=1.0, scalar2=-2.0, op0=ALU.is_ge, op1=ALU.mult), reads=["ee"], writes=["ee2"])
            p.dve(lambda e: e.tensor_tensor(out=ee2[:], in0=ee2[:], in1=ee[:], op=ALU.add), reads=["ee2", "ee"], writes=["ee2"])
            p.dve(lambda e: e.tensor_reduce(out=r1[:, 3:4], in_=ee2[:], axis=AX.X, op=ALU.max), reads=["ee2"], writes=["r1d"])
            p.dve(lambda e: e.tensor_scalar(out=ee2[:], in0=ee[:], scalar1=r1[:, 3:4], scalar2=None, op0=ALU.is_ge), reads=["ee", "r1d"], writes=["ee2"])
            p.dve(lambda e: e.tensor_tensor(out=ee[:], in0=ee[:], in1=ee2[:], op=ALU.mult), reads=["ee", "ee2"], writes=["ee"])
            p.dve(lambda e: e.tensor_scalar(out=r1[:, 4:5], in0=r1[:, 3:4], scalar1=1.0, scalar2=None, op0=ALU.add), reads=["r1d"], writes=["r1e"])
            p.dve(lambda e: e.reciprocal(out=r1[:, 4:5], in_=r1[:, 4:5]), reads=["r1e"], writes=["r1e"])
            p.dve(lambda e: e.tensor_tensor(out=r1[:, 4:5], in0=r1[:, 4:5], in1=r1[:, 1:2], op=ALU.mult), reads=["r1e", "r1b"], writes=["r1e"])
            p.dve(lambda e: e.tensor_scalar(out=ee[:], in0=ee[:], scalar1=r1[:, 4:5], scalar2=None, op0=ALU.mult), reads=["ee", "r1e"], writes=["ee"])
            for g in range(4):
                p.dve(lambda e, g=g: e.tensor_scalar(out=gdt[:, 4 * g:4 * g + 4], in0=ee[:], scalar1=goh[:, g:g + 1], scalar2=None, op0=ALU.mult),
                      reads=["ee", "goh"], writes=[("gd", k2)])
            p.dma(io["gd_d"][rows, :], gdt[:], reads=[("gd", k2)], writes=[("gd_d", gck)])

        for tc in range(4):
            do_chunk(tc)

    for s in range(NSLOT):
        do_slot(s)

C1_W = {"w_br": [5, 256, D], "w_gate": [5, D, D], "b_gate": [5, D], "w_out": [D, D], "ln1_g": [D], "ln1_b": [D],
        "w_rg": [D, 4], "b_rg": [4], "w_re": [4, D, 4], "b_re": [4, 4]}


def build_c1(dbg=False):
    kb = KB()
    io = {}
    if dbg:
        io["dbg_mt"] = kb.dout("dbg_mt", [128, 8, 512], BF16)
    for n in ("c_ident", "c_ones"):
        io[n] = kb.din(n, [128, 128])
    io["c_cst"] = kb.din("c_cst", [128, 8])
    io["h_tok"] = kb.din("h_tok", [TOK, D])
    io["hT_d"] = kb.din("hT_d", [D, TOK], BF16)
    io["oT_d"] = kb.din("oT_d", [5, 256, TOK], BF16)
    w = {n: kb.din(n, shp) for n, shp in C1_W.items()}
    io["h1_d"] = kb.dout("h1_d", [TOK, D])
    io["h1T_d"] = kb.dout("h1T_d", [D, TOK], BF16)
    io["gd_d"] = kb.dout("gd_d", [TOK, 16])
    c = load_consts(kb, io)
    phase_c1(kb, c, io, w)
    return kb.finish()


def phase_c2(kb, c, io, w, nT=4, nE=16):
    nc, p = kb.nc, kb.p
    gbc = kb.sb("gbc2", [128, 1024], F32)
    bbc = kb.sb("bbc2", [128, 1024], F32)
    p.dma(gbc[:], w["ln2_g"].partition_broadcast(128), writes=["lngb"])
    p.dma(bbc[:], w["ln2_b"].partition_broadcast(128), writes=["lngb"])
    hT = [kb.sb("h1Tt", [128, 8, 1024], BF16) for _ in range(2)]
    gdt = [kb.sb("gdt", [128, 8, 16], F32) for _ in range(2)]
    wup = [kb.sb("wup", [128, 8, 512], BF16) for _ in range(2)]
    wdn = [kb.sb("wdn", [128, 2, 1024], BF16) for _ in range(2)]
    yacc = kb.sb("yacc", [128, 8, 1024], F32)
    gT = [kb.sb("gT", [128, 2, 1024], BF16) for _ in range(2)]
    sa = [kb.sb("sa", [128, 512], F32) for _ in range(2)]
    hch = [kb.sb("h1ch", [128, 1024], F32) for _ in range(2)]
    och = [kb.sb("och", [128, 1024], F32) for _ in range(2)]
    lnt = {"st": kb.sb("lnst2", [128, 2, 6], F32), "mv": kb.sb("lnmv2", [128, 2], F32), "sm": kb.sb("lnsm2", [128, 2], F32)}
    st = {"bank": 0, "w": 0, "sa": 0}

    def nbank():
        b = st["bank"] % 8
        st["bank"] += 1
        return b

    def do_expert(T, e, ht, httok, gd, gdtok):
        k = st["w"] % 2
        st["w"] += 1
        wu, wd, g = wup[k], wdn[k], gT[k]
        p.dma(wu[:], w["w_up"][e].rearrange("(f p) c -> p f c", p=128), writes=[("wup", k)], q="pool")
        p.dma(wd[:], w["w_down"][e].rearrange("(j p) c -> p j c", p=128), writes=[("wdn", k)], q="pool")
        for ts in range(2):
            tsl = slice(ts * 512, (ts + 1) * 512)
            for jc in range(2):
                ba, bu = nbank(), nbank()
                pa, pu = kb.ps(ba), kb.ps(bu)
                for f in range(8):
                    p.pe(lambda e_, f=f: e_.matmul(pa[:], lhsT=wu[:, f, jc * 128:(jc + 1) * 128], rhs=ht[:, f, tsl], start=(f == 0), stop=(f == 7)),
                         reads=[("wup", k), httok], writes=[("ps", ba)])
                for f in range(8):
                    p.pe(lambda e_, f=f: e_.matmul(pu[:], lhsT=wu[:, f, 256 + jc * 128:256 + (jc + 1) * 128], rhs=ht[:, f, tsl], start=(f == 0), stop=(f == 7)),
                         reads=[("wup", k), httok], writes=[("ps", bu)])
                si = st["sa"] % 2
                st["sa"] += 1
                sat = sa[si]
                p.act(lambda e_, sat=sat, pa=pa: e_.activation(out=sat[:], in_=pa[:], func=AF.Silu), reads=[("ps", ba)], writes=[("sa", si)])
                p.dve(lambda e_, sat=sat, pu=pu, jc=jc, tsl=tsl: e_.tensor_tensor(out=g[:, jc, tsl], in0=sat[:], in1=pu[:], op=ALU.mult),
                      reads=[("sa", si), ("ps", bu)], writes=[("gT", k)])
        for tc in range(8):
            for hf in range(2):
                b = nbank()
                ps = kb.ps(b)
                for jc in range(2):
                    p.pe(lambda e_, jc=jc, ps=ps, tc=tc, hf=hf: e_.matmul(ps[:], lhsT=g[:, jc, tc * 128:(tc + 1) * 128], rhs=wd[:, jc, hf * 512:(hf + 1) * 512],
                                                                       start=(jc == 0), stop=(jc == 1)),
                         reads=[("gT", k), ("wdn", k)], writes=[("ps", b)])
                ya = yacc[:, tc, hf * 512:(hf + 1) * 512]
                if e == 0:
                    p.dve(lambda e_, ps=ps, ya=ya, tc=tc: e_.tensor_scalar(out=ya, in0=ps[:], scalar1=gd[:, tc, e:e + 1], scalar2=None, op0=ALU.mult),
                          reads=[("ps", b), gdtok], writes=[("yacc", tc)])
                else:
                    p.dve(lambda e_, ps=ps, ya=ya, tc=tc: e_.scalar_tensor_tensor(out=ya, in0=ps[:], scalar=gd[:, tc, e:e + 1], in1=ya, op0=ALU.mult, op1=ALU.add),
                          reads=[("ps", b), gdtok, ("yacc", tc)], writes=[("yacc", tc)])

    def do_chunk_out(T, tc):
        gck = T * 8 + tc
        k2 = gck % 2
        rows = slice(gck * 128, (gck + 1) * 128)
        hc, oc = hch[k2], och[k2]
        p.dma(hc[:], io["h1_d"][rows, :], writes=[("h1ch", k2)])
        p.dve(lambda e_: e_.scalar_tensor_tensor(out=hc[:], in0=hc[:], scalar=ALPHA, in1=yacc[:, tc, :], op0=ALU.mult, op1=ALU.add),
              reads=[("h1ch", k2), ("yacc", tc)], writes=[("h1ch", k2)])
        layer_norm_chunk(kb, c, hc, ("h1ch", k2), gbc, bbc, oc, ("och", k2), lnt)
        p.dma(io["h_out"][rows, :], oc[:], reads=[("och", k2)], writes=[("h_out", gck)])

    def do_tile(T):
        d2 = T % 2
        ht, gd = hT[d2], gdt[d2]
        cols = slice(T * 1024, (T + 1) * 1024)
        p.dma(ht[:], io["h1T_d"].rearrange("(f p) t -> p f t", p=128)[:, :, cols], writes=[("h1Tt", d2)])
        p.dma(gd[:], io["gd_d"][T * 1024:(T + 1) * 1024, :].rearrange("(n p) e -> p n e", p=128), writes=[("gdt", d2)])
        for e in range(nE):
            do_expert(T, e, ht, ("h1Tt", d2), gd, ("gdt", d2))
        for tc in range(8):
            do_chunk_out(T, tc)

    for T in range(nT):
        do_tile(T)


C2_W = {"w_up": [16, D, 512], "w_down": [16, 256, D], "ln2_g": [D], "ln2_b": [D]}


def build_c2(nT=4, nE=16):
    kb = KB()
    io = {}
    for n in ("c_ident", "c_ones"):
        io[n] = kb.din(n, [128, 128])
    io["c_cst"] = kb.din("c_cst", [128, 8])
    io["h1_d"] = kb.din("h1_d", [TOK, D])
    io["h1T_d"] = kb.din("h1T_d", [D, TOK], BF16)
    io["gd_d"] = kb.din("gd_d", [TOK, 16])
    w = {n: kb.din(n, shp) for n, shp in C2_W.items()}
    io["h_out"] = kb.dout("h_out", [TOK, D])
    c = load_consts(kb, io)
    phase_c2(kb, c, io, w, nT, nE)
    return kb.finish()


W_SHAPES = {
    "w_in": [D, IN_TOTAL], "g_cq": [256], "g_ckv": [128], "w_uq": [256, 384], "w_ukv": [128, 512],
    "nsa_pe": [32, 64], "w_phi_k1": [2048, 128], "w_phi_k2": [128, 64], "w_phi_v1": [2048, 128], "w_phi_v2": [128, 64],
    "w_mem_kv": [D, 512], "w_br": [5, 256, D], "w_gate": [5, D, D], "b_gate": [5, D], "w_out": [D, D],
    "ln1_g": [D], "ln1_b": [D], "w_rg": [D, 4], "b_rg": [4], "w_re": [4, D, 4], "b_re": [4, 4],
    "w_up": [16, D, 512], "w_down": [16, 256, D], "ln2_g": [D], "ln2_b": [D],
}
PAIRS = [[0, 1], [2, 3], [4, 5], [6, 7]]


def build_fused(depth=DEPTH, nlw=DEPTH, stop=None):
    kb = KB()
    io = {}
    for n in ("c_ident", "c_ones"):
        io[n] = kb.din(n, [128, 128])
    io["c_cst"] = kb.din("c_cst", [128, 8])
    for n, (shp, dt) in B_CONST_SHAPES.items():
        io[n] = kb.din(n, shp, dt)
    io["ropeq_t"] = kb.din("ropeq_t", [NSLOT, 96, 2, 512])
    io["ropek_t"] = kb.din("ropek_t", [NSLOT, 32, 2, 512])
    io["x_own"] = kb.din("x_own", [TOK, D])
    io["mem"] = kb.din("mem", [256, D])
    wfull = {n: kb.din(n, [nlw] + shp) for n, shp in W_SHAPES.items()}
    io["h_final"] = kb.dout("h_final", [TOK, D])
    hbuf = [kb.dscratch(f"hbuf{i}", [TOK, D]) for i in range(2)]
    io["hT_d"] = kb.dscratch("hT_d", [D, TOK], BF16)
    for n in ("qsb_d", "qmo_d", "qns_d", "qme_d"):
        io[n] = kb.dscratch(n, [256, TOK], BF16)
    io["qml_d"] = kb.dscratch("qml_d", [4, 96, TOK], BF16)
    io["gns_d"] = kb.dscratch("gns_d", [12, TOK])
    io["oT_d"] = kb.dscratch("oT_d", [5, 256, TOK], BF16)
    io["h1_d"] = kb.dscratch("h1_d", [TOK, D])
    io["h1T_d"] = kb.dscratch("h1T_d", [D, TOK], BF16)
    io["gd_d"] = kb.dscratch("gd_d", [TOK, 16])
    xk_rows = [256, 256, 256, 256, 64]
    xk_in = [kb.dscratch(f"xk_in{k}", [r, TOK], BF16) for k, r in enumerate(xk_rows)]
    xk_out = [kb.dscratch(f"xk_out{k}", [2 * r, TOK], BF16) for k, r in enumerate(xk_rows)]
    xv_in = [kb.dscratch(f"xv_in{k}", [1024, 896], BF16) for k in range(4)]
    xv_out = [kb.dscratch(f"xv_out{k}", [2048, 896], BF16) for k in range(4)]
    io["ksb_d"], io["kmo_d"] = xk_in[0], xk_in[1]
    io["kml_d"] = xk_in[2].rearrange("(h d) t -> h d t", h=4)
    io["krl_d"], io["kcv_d"], io["ksl_d"] = xk_in[3][0:32, :], xk_in[3][32:160, :], xk_in[3][160:224, :]
    io["kwi_d"] = xk_in[4]
    io["vsb_d"], io["vmo_d"] = TMChunks(xv_in, 0, 256), TMChunks(xv_in, 256, 512)
    io["vml_d"], io["vsw_d"] = TMChunks(xv_in, 512, 768), TMChunks(xv_in, 768, 896)
    io["ksb_f"], io["kmo_f"], io["kml_f"] = KPair(xk_out[0], 256), KPair(xk_out[1], 256), KPair(xk_out[2], 256)
    io["krl_f"], io["kcv_f"], io["ksl_f"] = KPair(xk_out[3], 256, 0), KPair(xk_out[3], 256, 32), KPair(xk_out[3], 256, 160)
    io["kwi_f"] = KPair(xk_out[4], 64)
    io["vsb_f"], io["vmo_f"] = VPair(xv_out, 0), VPair(xv_out, 256)
    io["vml_f"], io["vsw_f"] = VPair(xv_out, 512), VPair(xv_out, 768)

    for l in range(depth):
        w = {n: ap[l] for n, ap in wfull.items()}
        io["h_tok"] = io["x_own"] if l == 0 else hbuf[(l - 1) % 2]
        io["h_out"] = io["h_final"] if l == depth - 1 else hbuf[l % 2]
        c = load_consts(kb, io)
        phase_a(kb, c, io, w)
        kb.end_phase()
        if stop == "A":
            break
        for k in range(5):
            kb.p.allgather(xk_out[k], xk_in[k], PAIRS, writes=[("xk", k)])
        for k in range(4):
            kb.p.allgather(xv_out[k], xv_in[k], PAIRS, writes=[("xv", k)])
        kb.end_phase()
        if stop == "AG":
            break
        c = load_consts(kb, io)
        phase_b(kb, c, io, w, ("sb", "moba", "mla", "mem"))
        kb.end_phase()
        if stop == "B1":
            break
        c = load_consts(kb, io)
        phase_b(kb, c, io, w, ("nsa",))
        kb.end_phase()
        if stop == "B2":
            break
        c = load_consts(kb, io)
        phase_c1(kb, c, io, w)
        kb.end_phase()
        if stop == "C1":
            break
        c = load_consts(kb, io)
        phase_c2(kb, c, io, w)
        kb.end_phase()
    return kb.finish()


def fused_inputs(inp, c):
    b, r = c // 2, c % 2
    m = dict(host_consts())
    m.update(bconsts_host(r))
    m["ropeq_t"], m["ropek_t"] = rope_tables(r)
    m["x_own"] = np.ascontiguousarray(inp["x"][b][_own_tokens(r)])
    m["mem"] = inp["mem"][b]
    for n in W_SHAPES:
        m[n] = inp[n]
    return m


_PROGS = {}


def _prog(name, fn):
    if name not in _PROGS:
        _PROGS[name] = fn()
    return _PROGS[name]


def _own_tokens(r):
    t = np.arange(TOK)
    return t // 512 * 1024 + r * 512 + t % 512


def _interleave_fm(a0, a1):
    sh = a0.shape[:-1]
    out = np.empty(sh + (S,), a0.dtype)
    o = out.reshape(sh + (8, 2, 512))
    o[..., 0, :] = a0.reshape(sh + (8, 512))
    o[..., 1, :] = a1.reshape(sh + (8, 512))
    return out


def _interleave_tm(a0, a1):
    C = a0.shape[1]
    out = np.empty((S, C), a0.dtype)
    o = out.reshape(8, 2, 512, C)
    o[:, 0] = a0.reshape(8, 512, C)
    o[:, 1] = a1.reshape(8, 512, C)
    return out


A_WEIGHTS = ("w_in", "w_uq", "g_cq", "w_ukv", "g_ckv")
K_FM = {"ksb_d": "ksb_f", "kmo_d": "kmo_f", "kml_d": "kml_f", "krl_d": "krl_f", "kcv_d": "kcv_f", "ksl_d": "ksl_f", "kwi_d": "kwi_f"}
V_TM = {"vsb_d": "vsb_f", "vmo_d": "vmo_f", "vml_d": "vml_f", "vsw_d": "vsw_f"}
Q_OWN = ("qsb_d", "qmo_d", "qml_d", "qns_d", "qme_d", "gns_d")


def kernel_unfused(**inputs):
    inp = {k: np.ascontiguousarray(np.asarray(v)) for k, v in inputs.items()}
    ncores = 8
    cores = list(range(ncores))
    hc = host_consts()
    bc = [bconsts_host(r) for r in range(2)]
    rp = [rope_tables(r) for r in range(2)]
    own = [_own_tokens(r) for r in range(2)]
    h = [np.ascontiguousarray(inp["x"][c // 2][own[c % 2]]) for c in cores]
    nca = _prog("a", build_a)
    ncb1 = _prog("b1", lambda: build_b(("sb", "moba", "mla", "mem")))
    ncb2 = _prog("b2", lambda: build_b(("nsa",)))
    ncc1 = _prog("c1", build_c1)
    ncc2 = _prog("c2", build_c2)
    for l in range(DEPTH):
        maps = []
        for c in cores:
            m = dict(hc)
            m["h_tok"] = h[c]
            m["ropeq_t"], m["ropek_t"] = rp[c % 2]
            for n in A_WEIGHTS:
                m[n] = inp[n][l]
            maps.append(m)
        ra = run_bass_kernel_spmd(nca, maps, core_ids=cores).results
        maps = []
        for c in cores:
            b, r = c // 2, c % 2
            m = dict(hc)
            m.update(bc[r])
            for n in Q_OWN:
                m[n] = ra[c][n]
            for kd, kf in K_FM.items():
                m[kf] = _interleave_fm(np.asarray(ra[2 * b][kd]), np.asarray(ra[2 * b + 1][kd]))
            for vd, vf in V_TM.items():
                m[vf] = _interleave_tm(np.asarray(ra[2 * b][vd]), np.asarray(ra[2 * b + 1][vd]))
            m["mem"] = inp["mem"][b]
            for n in B_WEIGHTS:
                m[n] = inp[n][l]
            maps.append(m)
        rb1 = run_bass_kernel_spmd(ncb1, maps, core_ids=cores).results
        rb2 = run_bass_kernel_spmd(ncb2, maps, core_ids=cores).results
        rb = []
        for c in cores:
            o = np.array(rb1[c]["oT_d"])
            o[3] = np.asarray(rb2[c]["oT_d"])[3]
            rb.append({"oT_d": o})
        maps = []
        for c in cores:
            m = dict(hc)
            m["h_tok"] = h[c]
            m["hT_d"] = ra[c]["hT_d"]
            m["oT_d"] = rb[c]["oT_d"]
            for n in C1_W:
                m[n] = inp[n][l]
            maps.append(m)
        rc1 = run_bass_kernel_spmd(ncc1, maps, core_ids=cores).results
        maps = []
        for c in cores:
            m = dict(hc)
            for n in ("h1_d", "h1T_d", "gd_d"):
                m[n] = rc1[c][n]
            for n in C2_W:
                m[n] = inp[n][l]
            maps.append(m)
        rc2 = run_bass_kernel_spmd(ncc2, maps, core_ids=cores).results
        h = [np.asarray(rc2[c]["h_out"]) for c in cores]
    out = np.empty((NB, S, D), np.float32)
    for c in cores:
        out[c // 2][own[c % 2]] = h[c]
    return out


def kernel(**inputs):
    inp = {k: np.ascontiguousarray(np.asarray(v)) for k, v in inputs.items()}
    cores = list(range(8))
    nc = _prog("fused", build_fused)
    maps = [fused_inputs(inp, c) for c in cores]
    res = run_bass_kernel_spmd(nc, maps, core_ids=cores).results
    out = np.empty((NB, S, D), np.float32)
    for c in cores:
        out[c // 2][_own_tokens(c % 2)] = np.asarray(res[c]["h_final"])
    return out
```

```python
import contextlib
import types
import numpy as np
import ml_dtypes
import concourse.bass as bass
import concourse.mybir as mybir
from concourse.bass_utils import run_bass_kernel_spmd

F32 = mybir.dt.float32
BF16 = mybir.dt.bfloat16
AF = mybir.ActivationFunctionType
ALU = mybir.AluOpType
AX = mybir.AxisListType

D = 1024
S = 8192
NB = 4
DEPTH = 4
TOK = 4096
NSLOT = 8
IN_TOTAL = 2860
ALPHA = (2.0 * DEPTH) ** 0.25
LN_EPS = 1e-5
RMS_EPS = 1e-6
MASKV = -30000.0
SLOPES = [2.0 ** (-2.0 * (i + 1)) for i in range(4)]

ENGS = ("pe", "act", "dve", "pool", "sp")
SIG_EPOCH = 30000


def _freeze(fn):
    if fn.__closure__ is None:
        return fn
    cells = []
    for cl in fn.__closure__:
        try:
            cells.append(types.CellType(cl.cell_contents))
        except ValueError:
            cells.append(cl)
    g = types.FunctionType(fn.__code__, fn.__globals__, fn.__name__, fn.__defaults__, tuple(cells))
    g.__kwdefaults__ = fn.__kwdefaults__
    return g


class Op:
    __slots__ = ("eng", "fn", "reads", "writes", "dma", "deps", "sig", "dticket", "idx", "dprev")

    def __init__(self, eng, fn, reads, writes, dma):
        self.eng = eng
        self.fn = fn
        self.reads = tuple(reads)
        self.writes = tuple(writes)
        self.dma = dma
        self.deps = []
        self.sig = None
        self.dticket = None
        self.dprev = None


class Prog:
    NDSEM = 12
    _phase_id = 0

    def __init__(self, nc):
        self.nc = nc
        self.ops = []

    def add(self, eng, fn, reads=(), writes=(), dma=False):
        op = Op(eng, _freeze(fn), reads, writes, dma)
        op.idx = len(self.ops)
        self.ops.append(op)
        return op

    def pe(self, fn, reads=(), writes=()):
        return self.add("pe", fn, reads, writes)

    def act(self, fn, reads=(), writes=()):
        return self.add("act", fn, reads, writes)

    def dve(self, fn, reads=(), writes=()):
        return self.add("dve", fn, reads, writes)

    def pool(self, fn, reads=(), writes=()):
        return self.add("pool", fn, reads, writes)

    def allgather(self, out, in_, groups, reads=(), writes=()):
        return self.add("pool", lambda e: e.collective_compute("AllGather", ALU.bypass, replica_groups=groups, ins=[in_.opt()], outs=[out.opt()]),
                        reads, writes, dma="cc")

    def dma(self, out, in_, reads=(), writes=(), q="sp", slow=False):
        if slow:
            return self.add(q, lambda e: e.dma_start(out=out, in_=in_, allow_slow_non_contiguous=True), reads, writes, dma=True)
        return self.add(q, lambda e: e.dma_start(out=out, in_=in_), reads, writes, dma=True)

    def analyze(self):
        last_w = {}
        readers = {}
        for op in self.ops:
            deps = set()
            for t in op.reads:
                if t in last_w:
                    deps.add(last_w[t])
            for t in op.writes:
                if t in last_w:
                    deps.add(last_w[t])
                for r in readers.get(t, ()):
                    deps.add(r)
            deps.discard(op.idx)
            op.deps = sorted(deps)
            for t in op.reads:
                readers.setdefault(t, []).append(op.idx)
            for t in op.writes:
                last_w[t] = op.idx
                readers[t] = []
        qcount = {e: 0 for e in ENGS}
        qhist = {e: [] for e in ENGS}
        for op in self.ops:
            if op.dma == "cc":
                op.dticket = ("cc", op.idx, 1)
                continue
            if op.dma:
                n = qcount[op.eng]
                qcount[op.eng] += 1
                op.dticket = (op.eng, n % self.NDSEM, 16 * (n // self.NDSEM + 1))
                if n >= self.NDSEM:
                    op.dprev = qhist[op.eng][n - self.NDSEM]
                qhist[op.eng].append(op.idx)
        waited_eng = {e: {p: -1 for p in ENGS} for e in ENGS}
        waited_dma = {e: set() for e in ENGS}
        last_on = {e: -1 for e in ENGS}
        need_sig = set()
        for op in self.ops:
            e = op.eng
            final = []
            best = {}
            dl = list(op.deps)
            if op.dprev is not None:
                dl.append(op.dprev)
            for d in dl:
                p = self.ops[d]
                if p.dma:
                    if d not in waited_dma[e]:
                        waited_dma[e].add(d)
                        final.append(("dma", d))
                else:
                    if p.eng == e and e == "pe":
                        continue
                    if d <= waited_eng[e][p.eng]:
                        continue
                    if p.eng not in best or d > best[p.eng]:
                        best[p.eng] = d
            for pe_, d in best.items():
                waited_eng[e][pe_] = d
                need_sig.add(d)
                final.append(("eng", d))
            op.deps = final
        cnt = {e: 0 for e in ENGS}
        for op in self.ops:
            if not op.dma and op.idx in need_sig:
                cnt[op.eng] += 1
                op.sig = cnt[op.eng]
        self.sig_total = cnt
        self.dma_total = qcount

    def emit(self, barrier=False):
        nc = self.nc
        self.analyze()
        allsem = []

        Prog._phase_id += 1
        pid = Prog._phase_id

        def newsem(name):
            h = nc.alloc_semaphore(f"{name}_ph{pid}")
            allsem.append(h)
            return h

        if True:
            esem = {}
            for e in ENGS:
                n_ep = self.sig_total[e] // SIG_EPOCH + 1
                esem[e] = [newsem(f"s_{e}_{i}") for i in range(n_ep)]
            dsem = {}
            for e in ENGS:
                if self.dma_total[e]:
                    dsem[e] = [newsem(f"d_{e}_{i}") for i in range(self.NDSEM)]
            dsem["cc"] = {op.idx: newsem(f"cc_{op.idx}") for op in self.ops if op.dma == "cc"}

            def waitspec(dep):
                kind, d = dep
                p = self.ops[d]
                if kind == "dma":
                    q, si, val = p.dticket
                    return dsem[q][si], val
                k = p.sig - 1
                return esem[p.eng][k // SIG_EPOCH], k % SIG_EPOCH + 1

            def run(engname):
                def body(eng):
                    last_dma = {}
                    for op in self.ops:
                        if op.eng != engname:
                            continue
                        ws = [waitspec(d) for d in op.deps]
                        for (sem, val) in ws[1:]:
                            eng.wait_ge(sem, val)
                        ins = op.fn(eng)
                        if ws:
                            ins._wait_ge(ws[0][0], ws[0][1])
                        if op.dma == "cc":
                            ins.then_inc(dsem["cc"][op.idx])
                            eng.wait_ge(dsem["cc"][op.idx], 1)
                        elif op.dma:
                            q, si, val = op.dticket
                            ins.then_inc(dsem[q][si], 16)
                            last_dma[si] = val
                        elif op.sig is not None:
                            k = op.sig - 1
                            ins.then_inc(esem[engname][k // SIG_EPOCH], 1)
                    for si, val in last_dma.items():
                        eng.wait_ge(dsem[engname][si], val)
                return body

            with nc.Block() as block:
                block.tensor(run("pe"))
                block.scalar(run("act"))
                block.vector(run("dve"))
                block.gpsimd(run("pool"))
                block.sync(run("sp"))
        if barrier:
            nc.all_engine_barrier()
            nc.clear_and_free_semaphores(allsem)
            nc.all_engine_barrier()
        else:
            for h in allsem:
                nc.release_semaphore(h)


class KB:
    def __init__(self):
        self.nc = bass.Bass("TRN2", target_bir_lowering=False)
        self.p = Prog(self.nc)
        self.es = contextlib.ExitStack()
        self.dram = {}
        self._uid = 0
        self.es0 = contextlib.ExitStack()
        self.psf = [self.es0.enter_context(self.nc.psum_tensor(f"psb{i}", [128, 512], F32)) for i in range(8)]

    def uid(self, s):
        self._uid += 1
        return f"{s}_{self._uid}"

    def din(self, name, shape, dt=F32):
        t = self.nc.dram_tensor(name, list(shape), dt, kind="ExternalInput").ap()
        self.dram[name] = t
        return t

    def dout(self, name, shape, dt=F32, kind="ExternalOutput"):
        t = self.nc.dram_tensor(name, list(shape), dt, kind=kind).ap()
        self.dram[name] = t
        return t

    def sb(self, name, shape, dt=F32):
        return self.es.enter_context(self.nc.sbuf_tensor(self.uid(name), list(shape), dt))

    def dscratch(self, name, shape, dt=F32):
        t = self.nc.dram_tensor(name, list(shape), dt, kind="Internal").ap()
        self.dram[name] = t
        return t

    def end_phase(self):
        self.p.emit(barrier=True)
        self.es.close()
        self.es = contextlib.ExitStack()
        self.p = Prog(self.nc)

    def ps(self, i):
        return self.psf[i]

    def finish(self):
        self.p.emit()
        self.es.close()
        self.es0.close()
        return self.nc


C_SBQ, C_SBK, C_SBV = 0, 256, 512
C_MOQ, C_MOK, C_MOV = 768, 1024, 1280
C_CQ, C_CKV, C_KR = 1536, 1792, 1920
C_NSQ, C_NSKV, C_NSG, C_MEQ = 1952, 2208, 2592, 2604


def consts_common(kb):
    nc, p = kb.nc, kb.p
    c = {}
    c["identf"] = kb.sb("identf", [128, 128], F32)
    c["identb"] = kb.sb("identb", [128, 128], BF16)
    c["onesb"] = kb.sb("onesb", [128, 128], BF16)
    c["onesf"] = kb.sb("onesf", [128, 128], F32)
    idf, idb = c["identf"], c["identb"]
    p.pool(lambda e: e.memset(c["onesf"][:], 1.0), writes=["onesf"])
    p.pool(lambda e: e.memset(c["onesb"][:], 1.0), writes=["onesb"])
    p.pool(lambda e: e.affine_select(out=idf[:], in_=c["onesf"][:], pattern=[[-1, 128]], compare_op=ALU.is_equal,
                                     fill=0.0, base=0, channel_multiplier=1), reads=["onesf"], writes=["identf"])
    p.pool(lambda e: e.tensor_copy(out=idb[:], in_=idf[:]), reads=["identf"], writes=["identb"])
    return c


def tm_view(ap2d, p=128):
    return ap2d.rearrange("(n p) c -> p n c", p=p)


def phase_a(kb, c, io, w):
    nc, p = kb.nc, kb.p
    identf, onesb = c["identf"], c["onesb"]

    hT = kb.sb("hT", [128, 8, TOK], BF16)
    win = kb.sb("win", [128, 8, IN_TOTAL], BF16)
    wkrot = kb.sb("wkrot", [128, 8, 32], BF16)
    for f in range(8):
        p.dma(win[:, f, :], w["w_in"][f * 128:(f + 1) * 128, :], writes=[("win", f)], q="pool")
    for f in range(8):
        p.dve(lambda e, f=f: e.tensor_scalar_mul(out=wkrot[:, f, 0:16], in0=win[:, f, C_KR + 16:C_KR + 32], scalar1=-1.0),
              reads=[("win", f)], writes=[("wkrot", f)])
        p.dve(lambda e, f=f: e.tensor_copy(out=wkrot[:, f, 16:32], in_=win[:, f, C_KR:C_KR + 16]),
              reads=[("win", f)], writes=[("wkrot", f)])
    wuq_f = kb.sb("wuq_f", [128, 2, 384], F32)
    wuq = kb.sb("wuq", [128, 2, 384], BF16)
    wuqr = kb.sb("wuqr", [128, 2, 384], BF16)
    gcq = kb.sb("gcq", [128, 2], F32)
    wukv_f = kb.sb("wukv_f", [128, 512], F32)
    wukv = kb.sb("wukv", [128, 512], BF16)
    gckv = kb.sb("gckv", [128, 1], F32)
    p.dma(wuq_f[:], w["w_uq"].rearrange("(n p) c -> p n c", p=128), writes=["wuq_f"])
    p.dma(gcq[:], w["g_cq"].rearrange("(n p) -> p n", p=128), writes=["gcq"], slow=True)
    p.dma(wukv_f[:], w["w_ukv"], writes=["wukv_f"])
    p.dma(gckv[:], w["g_ckv"].rearrange("(n p) -> p n", p=128), writes=["gckv"], slow=True)
    for rc in range(2):
        p.dve(lambda e, rc=rc: e.tensor_scalar_mul(out=wuq[:, rc, :], in0=wuq_f[:, rc, :], scalar1=gcq[:, rc:rc + 1]),
              reads=["wuq_f", "gcq"], writes=["wuq"])
    p.dve(lambda e: e.memset(wuqr[:], 0.0), writes=["wuqr"])
    for rc in range(2):
        for h in range(4):
            b0 = h * 96
            p.dve(lambda e, rc=rc, b0=b0: e.tensor_scalar_mul(out=wuqr[:, rc, b0 + 64:b0 + 80], in0=wuq[:, rc, b0 + 80:b0 + 96], scalar1=-1.0),
                  reads=["wuq"], writes=["wuqr"])
            p.dve(lambda e, rc=rc, b0=b0: e.tensor_copy(out=wuqr[:, rc, b0 + 80:b0 + 96], in_=wuq[:, rc, b0 + 64:b0 + 80]),
                  reads=["wuq"], writes=["wuqr"])
    p.dve(lambda e: e.tensor_scalar_mul(out=wukv[:], in0=wukv_f[:], scalar1=gckv[:, 0:1]), reads=["wukv_f", "gckv"], writes=["wukv"])

    hst = [kb.sb("hst", [128, 1024], F32) for _ in range(2)]
    for ck in range(TOK // 128):
        st = hst[ck % 2]
        tk = ("hst", ck % 2)
        p.dma(st[:], io["h_tok"][ck * 128:(ck + 1) * 128, :], writes=[tk])
        for half in range(2):
            bank = (ck * 2 + half) % 2
            ps = kb.ps(bank)
            for j in range(4):
                f = half * 4 + j
                p.pe(lambda e, ps=ps, st=st, f=f, j=j: e.transpose(out=ps[:, j * 128:(j + 1) * 128], in_=st[:, f * 128:(f + 1) * 128], identity=identf[:]),
                     reads=[tk, "identf"], writes=[("ps", bank)])
            dst = hT[:, half * 4:half * 4 + 4, ck * 128:(ck + 1) * 128]
            src = ps[:].rearrange("p (j t) -> p j t", j=4)
            if half == 0:
                p.act(lambda e, dst=dst, src=src: e.copy(out=dst, in_=src), reads=[("ps", bank)], writes=[("hT", ck // 4)])
            else:
                p.dve(lambda e, dst=dst, src=src: e.tensor_copy(out=dst, in_=src), reads=[("ps", bank)], writes=[("hT", ck // 4)])
    for f in range(8):
        p.dma(io["hT_d"][f * 128:(f + 1) * 128, :], hT[:, f, :], reads=[("hT", s) for s in range(8)], writes=[("hT_d", f)])

    ostage = [kb.sb("ostg", [128, 512], BF16) for _ in range(4)]
    gstage = [kb.sb("gstg", [12, 512], F32) for _ in range(2)]
    cq_sb = [kb.sb("cq_sb", [128, 2, 512], BF16) for _ in range(2)]
    ckv_sb = [kb.sb("ckv_sb", [128, 512], BF16) for _ in range(2)]
    krr_sb = [kb.sb("krr", [32, 2, 512], F32) for _ in range(2)]
    sq_sb = [kb.sb("sq", [128, 3, 512], BF16) for _ in range(2)]
    rstd_q = [kb.sb("rstdq", [128, 512], F32) for _ in range(2)]
    rstd_kv = [kb.sb("rstdkv", [128, 512], F32) for _ in range(2)]
    rkv_tok = [kb.sb("rkvtok", [128, 4], F32) for _ in range(2)]
    ropeq = [kb.sb("ropeq", [96, 2, 512], F32) for _ in range(2)]
    ropek = [kb.sb("ropek", [32, 2, 512], F32) for _ in range(2)]
    t1 = [kb.sb("t1", [96, 512], F32) for _ in range(2)]
    t2 = [kb.sb("t2", [96, 512], F32) for _ in range(2)]
    vstage = [kb.sb("vstg", [128, 640], BF16) for _ in range(2)]
    vmst = [kb.sb("vmst", [128, 256], BF16) for _ in range(2)]
    cnt = {"o": 0, "bank": 0, "v": 0}

    def nbank():
        b = 2 + cnt["bank"] % 6
        cnt["bank"] += 1
        return b

    fm_list = [
        ("qsb_d", 0, C_SBQ, 128, 0.125), ("qsb_d", 128, C_SBQ + 128, 128, 0.125),
        ("ksb_d", 0, C_SBK, 128, 1.0), ("ksb_d", 128, C_SBK + 128, 128, 1.0),
        ("qmo_d", 0, C_MOQ, 128, 0.125), ("qmo_d", 128, C_MOQ + 128, 128, 0.125),
        ("kmo_d", 0, C_MOK, 128, 1.0), ("kmo_d", 128, C_MOK + 128, 128, 1.0),
        ("qns_d", 0, C_NSQ, 128, 0.125), ("qns_d", 128, C_NSQ + 128, 128, 0.125),
        ("kcv_d", 0, C_NSKV, 128, 1.0),
        ("ksl_d", 0, C_NSKV + 128, 64, 1.0),
        ("kwi_d", 0, C_NSKV + 256, 64, 1.0),
        ("qme_d", 0, C_MEQ, 128, 0.125), ("qme_d", 128, C_MEQ + 128, 128, 0.125),
    ]

    def proj_fm(s, col0, ncols, wsrc=None):
        b = nbank()
        ps = kb.ps(b)
        for f in range(8):
            if wsrc is None:
                lhsT = win[:, f, col0:col0 + ncols]
                rd = [("win", f)]
            else:
                lhsT = wsrc[:, f, col0:col0 + ncols]
                rd = [("wkrot", f)]
            p.pe(lambda e, ps=ps, lhsT=lhsT, f=f, s=s, ncols=ncols: e.matmul(ps[0:ncols, :], lhsT=lhsT, rhs=hT[:, f, s * 512:(s + 1) * 512],
                                                                            start=(f == 0), stop=(f == 7)),
                 reads=rd + [("hT", s)], writes=[("ps", b)])
        return b

    for s in range(NSLOT):
        tsl = slice(s * 512, (s + 1) * 512)
        for (dn, r0, col0, ncols, scale) in fm_list:
            b = proj_fm(s, col0, ncols)
            ps = kb.ps(b)
            k = cnt["o"] % 4
            cnt["o"] += 1
            og = ostage[k]
            p.act(lambda e, og=og, ps=ps, ncols=ncols, scale=scale: e.activation(out=og[0:ncols, :], in_=ps[0:ncols, :], func=AF.Copy, scale=scale),
                  reads=[("ps", b)], writes=[("ostg", k)])
            p.dma(io[dn][r0:r0 + ncols, tsl], og[0:ncols, :], reads=[("ostg", k)], writes=[(dn, s)])
        b = proj_fm(s, C_NSG, 12)
        ps = kb.ps(b)
        gs = gstage[s % 2]
        p.act(lambda e, gs=gs, ps=ps: e.activation(out=gs[:], in_=ps[0:12, :], func=AF.Sigmoid), reads=[("ps", b)], writes=[("gstg", s % 2)])
        p.dma(io["gns_d"][:, tsl], gs[:], reads=[("gstg", s % 2)], writes=[("gns_d", s)])

        d2 = s % 2
        cq, ckv, sq = cq_sb[d2], ckv_sb[d2], sq_sb[d2]
        for rc in range(2):
            b = proj_fm(s, C_CQ + rc * 128, 128)
            ps = kb.ps(b)
            p.act(lambda e, cq=cq, ps=ps, rc=rc: e.copy(out=cq[:, rc, :], in_=ps[:]), reads=[("ps", b)], writes=[("cq", d2)])
            p.act(lambda e, sq=sq, ps=ps, rc=rc: e.activation(out=sq[:, rc, :], in_=ps[:], func=AF.Square), reads=[("ps", b)], writes=[("sq", d2)])
        b = proj_fm(s, C_CKV, 128)
        ps = kb.ps(b)
        p.act(lambda e, ckv=ckv, ps=ps: e.copy(out=ckv[:], in_=ps[:]), reads=[("ps", b)], writes=[("ckv", d2)])
        p.act(lambda e, sq=sq, ps=ps: e.activation(out=sq[:, 2, :], in_=ps[:], func=AF.Square), reads=[("ps", b)], writes=[("sq", d2)])
        krr = krr_sb[d2]
        b = proj_fm(s, C_KR, 32)
        ps = kb.ps(b)
        p.act(lambda e, krr=krr, ps=ps: e.copy(out=krr[:, 0, :], in_=ps[0:32, :]), reads=[("ps", b)], writes=[("krr", d2)])
        b = proj_fm(s, 0, 32, wsrc=wkrot)
        ps = kb.ps(b)
        p.act(lambda e, krr=krr, ps=ps: e.copy(out=krr[:, 1, :], in_=ps[0:32, :]), reads=[("ps", b)], writes=[("krr", d2)])
        rq, rkv = rstd_q[d2], rstd_kv[d2]
        b = nbank()
        ps = kb.ps(b)
        for rc in range(2):
            p.pe(lambda e, ps=ps, sq=sq, rc=rc: e.matmul(ps[:], lhsT=onesb[:], rhs=sq[:, rc, :], start=(rc == 0), stop=(rc == 1)),
                 reads=[("sq", d2), "onesb"], writes=[("ps", b)])
        p.act(lambda e, rq=rq, ps=ps: e.activation(out=rq[:], in_=ps[:], func=AF.Ln, scale=1.0 / 256.0, bias=c["eps_rms"][:, 0:1]),
              reads=[("ps", b), "cst"], writes=[("rq", d2)])
        p.act(lambda e, rq=rq: e.activation(out=rq[:], in_=rq[:], func=AF.Exp, scale=-0.5), reads=[("rq", d2)], writes=[("rq", d2)])
        b = nbank()
        ps = kb.ps(b)
        p.pe(lambda e, ps=ps, sq=sq: e.matmul(ps[:], lhsT=onesb[:], rhs=sq[:, 2, :], start=True, stop=True),
             reads=[("sq", d2), "onesb"], writes=[("ps", b)])
        p.act(lambda e, rkv=rkv, ps=ps: e.activation(out=rkv[:], in_=ps[:], func=AF.Ln, scale=1.0 / 128.0, bias=c["eps_rms"][:, 0:1]),
              reads=[("ps", b), "cst"], writes=[("rkv", d2)])
        p.act(lambda e, rkv=rkv: e.activation(out=rkv[:], in_=rkv[:], func=AF.Exp, scale=-0.5), reads=[("rkv", d2)], writes=[("rkv", d2)])
        rkt = rkv_tok[d2]
        b = nbank()
        ps = kb.ps(b)
        for ck in range(4):
            p.pe(lambda e, ps=ps, sq=sq, ck=ck: e.matmul(ps[:, ck:ck + 1], lhsT=sq[:, 2, ck * 128:(ck + 1) * 128], rhs=onesb[:, 0:1], start=True, stop=True),
                 reads=[("sq", d2), "onesb"], writes=[("ps", b)])
        p.act(lambda e, rkt=rkt, ps=ps: e.activation(out=rkt[:], in_=ps[:, 0:4], func=AF.Ln, scale=1.0 / 128.0, bias=c["eps_rms"][:, 0:1]),
              reads=[("ps", b), "cst"], writes=[("rkt", d2)])
        p.act(lambda e, rkt=rkt: e.activation(out=rkt[:], in_=rkt[:], func=AF.Exp, scale=-0.5), reads=[("rkt", d2)], writes=[("rkt", d2)])
        rpq, rpk = ropeq[d2], ropek[d2]
        p.dma(rpq[:], io["ropeq_t"][s], writes=[("rpq", d2)])
        p.dma(rpk[:], io["ropek_t"][s], writes=[("rpk", d2)])
        for h in range(4):
            bA, bB = nbank(), nbank()
            psA, psB = kb.ps(bA), kb.ps(bB)
            for rc in range(2):
                p.pe(lambda e, psA=psA, cq=cq, rc=rc, h=h: e.matmul(psA[0:96, :], lhsT=wuq[:, rc, h * 96:(h + 1) * 96], rhs=cq[:, rc, :], start=(rc == 0), stop=(rc == 1)),
                     reads=["wuq", ("cq", d2)], writes=[("ps", bA)])
            for rc in range(2):
                p.pe(lambda e, psB=psB, cq=cq, rc=rc, h=h: e.matmul(psB[0:96, :], lhsT=wuqr[:, rc, h * 96:(h + 1) * 96], rhs=cq[:, rc, :], start=(rc == 0), stop=(rc == 1)),
                     reads=["wuqr", ("cq", d2)], writes=[("ps", bB)])
            a1, a2 = t1[h % 2], t2[h % 2]
            k = cnt["o"] % 4
            cnt["o"] += 1
            og = ostage[k]
            p.dve(lambda e, a1=a1, psA=psA, rpq=rpq: e.tensor_tensor(out=a1[:], in0=psA[0:96, :], in1=rpq[:, 0, :], op=ALU.mult),
                  reads=[("ps", bA), ("rpq", d2)], writes=[("t1", h % 2)])
            p.dve(lambda e, a2=a2, psB=psB, rpq=rpq: e.tensor_tensor(out=a2[:], in0=psB[0:96, :], in1=rpq[:, 1, :], op=ALU.mult),
                  reads=[("ps", bB), ("rpq", d2)], writes=[("t2", h % 2)])
            p.dve(lambda e, a1=a1, a2=a2: e.tensor_tensor(out=a1[:], in0=a1[:], in1=a2[:], op=ALU.add),
                  reads=[("t1", h % 2), ("t2", h % 2)], writes=[("t1", h % 2)])
            p.dve(lambda e, a1=a1, og=og, rq=rq: e.tensor_tensor(out=og[0:96, :], in0=a1[:], in1=rq[0:96, :], op=ALU.mult),
                  reads=[("t1", h % 2), ("rq", d2)], writes=[("ostg", k)])
            p.dma(io["qml_d"][h, :, tsl], og[0:96, :], reads=[("ostg", k)], writes=[("qml_d", s, h)])
            b = nbank()
            ps = kb.ps(b)
            p.pe(lambda e, ps=ps, ckv=ckv, h=h: e.matmul(ps[0:64, :], lhsT=wukv[:, h * 128:h * 128 + 64], rhs=ckv[:], start=True, stop=True),
                 reads=["wukv", ("ckv", d2)], writes=[("ps", b)])
            k = cnt["o"] % 4
            cnt["o"] += 1
            og = ostage[k]
            p.dve(lambda e, og=og, ps=ps, rkv=rkv: e.tensor_tensor(out=og[0:64, :], in0=ps[0:64, :], in1=rkv[0:64, :], op=ALU.mult),
                  reads=[("ps", b), ("rkv", d2)], writes=[("ostg", k)])
            p.dma(io["kml_d"][h, :, tsl], og[0:64, :], reads=[("ostg", k)], writes=[("kml_d", s, h)])
        a1, a2 = t1[0], t2[0]
        k = cnt["o"] % 4
        cnt["o"] += 1
        og = ostage[k]
        p.dve(lambda e, a1=a1, krr=krr, rpk=rpk: e.tensor_tensor(out=a1[0:32, :], in0=krr[:, 0, :], in1=rpk[:, 0, :], op=ALU.mult),
              reads=[("krr", d2), ("rpk", d2)], writes=[("t1", 0)])
        p.dve(lambda e, a2=a2, krr=krr, rpk=rpk: e.tensor_tensor(out=a2[0:32, :], in0=krr[:, 1, :], in1=rpk[:, 1, :], op=ALU.mult),
              reads=[("krr", d2), ("rpk", d2)], writes=[("t2", 0)])
        p.dve(lambda e, a1=a1, a2=a2, og=og: e.tensor_tensor(out=og[0:32, :], in0=a1[0:32, :], in1=a2[0:32, :], op=ALU.add),
              reads=[("t1", 0), ("t2", 0)], writes=[("ostg", k)])
        p.dma(io["krl_d"][:, tsl], og[0:32, :], reads=[("ostg", k)], writes=[("krl_d", s)])
        for ck in range(4):
            gck = s * 4 + ck
            b = nbank()
            ps = kb.ps(b)
            for h in range(4):
                p.pe(lambda e, ps=ps, ckv=ckv, ck=ck, h=h: e.matmul(ps[:, h * 64:(h + 1) * 64], lhsT=ckv[:, ck * 128:(ck + 1) * 128], rhs=wukv[:, h * 128 + 64:h * 128 + 128],
                                                                 start=True, stop=True),
                     reads=["wukv", ("ckv", d2)], writes=[("ps", b)])
            vm = vmst[gck % 2]
            p.act(lambda e, vm=vm, ps=ps, rkt=rkt, ck=ck: e.activation(out=vm[:], in_=ps[:, 0:256], func=AF.Copy, scale=rkt[:, ck:ck + 1]),
                  reads=[("ps", b), ("rkt", d2)], writes=[("vmst", gck % 2)])
            p.dma(io["vml_d"][gck * 128:(gck + 1) * 128, :], vm[:], reads=[("vmst", gck % 2)], writes=[("vml_d", gck)])

        for ck in range(4):
            gck = s * 4 + ck
            tcs = slice(gck * 128, (gck + 1) * 128)
            vs = vstage[gck % 2]
            b1, b2 = nbank(), nbank()
            ps1, ps2 = kb.ps(b1), kb.ps(b2)
            for f in range(8):
                p.pe(lambda e, ps1=ps1, f=f, tcs=tcs: e.matmul(ps1[:, 0:256], lhsT=hT[:, f, tcs], rhs=win[:, f, C_SBV:C_SBV + 256], start=(f == 0), stop=(f == 7)),
                     reads=[("win", f), ("hT", s)], writes=[("ps", b1)])
            for f in range(8):
                p.pe(lambda e, ps1=ps1, f=f, tcs=tcs: e.matmul(ps1[:, 256:512], lhsT=hT[:, f, tcs], rhs=win[:, f, C_MOV:C_MOV + 256], start=(f == 0), stop=(f == 7)),
                     reads=[("win", f), ("hT", s)], writes=[("ps", b1)])
            for f in range(8):
                p.pe(lambda e, ps2=ps2, f=f, tcs=tcs: e.matmul(ps2[:, 0:64], lhsT=hT[:, f, tcs], rhs=win[:, f, C_NSKV + 192:C_NSKV + 256], start=(f == 0), stop=(f == 7)),
                     reads=[("win", f), ("hT", s)], writes=[("ps", b2)])
            for f in range(8):
                p.pe(lambda e, ps2=ps2, f=f, tcs=tcs: e.matmul(ps2[:, 64:128], lhsT=hT[:, f, tcs], rhs=win[:, f, C_NSKV + 320:C_NSKV + 384], start=(f == 0), stop=(f == 7)),
                     reads=[("win", f), ("hT", s)], writes=[("ps", b2)])
            p.act(lambda e, vs=vs, ps1=ps1: e.copy(out=vs[:, 0:512], in_=ps1[:]), reads=[("ps", b1)], writes=[("vstg", gck % 2)])
            p.dve(lambda e, vs=vs, ps2=ps2: e.tensor_copy(out=vs[:, 512:640], in_=ps2[:, 0:128]), reads=[("ps", b2)], writes=[("vstg", gck % 2)])
            p.dma(io["vsb_d"][tcs, :], vs[:, 0:256], reads=[("vstg", gck % 2)], writes=[("vsb_d", gck)])
            p.dma(io["vmo_d"][tcs, :], vs[:, 256:512], reads=[("vstg", gck % 2)], writes=[("vmo_d", gck)])
            p.dma(io["vsw_d"][tcs, :], vs[:, 512:640], reads=[("vstg", gck % 2)], writes=[("vsw_d", gck)])


A_OUTS = {
    "hT_d": ([1024, TOK], BF16),
    "qsb_d": ([256, TOK], BF16), "ksb_d": ([256, TOK], BF16), "vsb_d": ([TOK, 256], BF16),
    "qmo_d": ([256, TOK], BF16), "kmo_d": ([256, TOK], BF16), "vmo_d": ([TOK, 256], BF16),
    "qml_d": ([4, 96, TOK], BF16), "kml_d": ([4, 64, TOK], BF16), "krl_d": ([32, TOK], BF16), "vml_d": ([TOK, 256], BF16),
    "qns_d": ([256, TOK], BF16), "kcv_d": ([128, TOK], BF16), "ksl_d": ([64, TOK], BF16), "kwi_d": ([64, TOK], BF16),
    "vsw_d": ([TOK, 128], BF16), "gns_d": ([12, TOK], F32), "qme_d": ([256, TOK], BF16),
}


def load_consts(kb, io):
    p = kb.p
    c = {}
    c["identf"] = kb.sb("identf", [128, 128], F32)
    c["identb"] = kb.sb("identb", [128, 128], BF16)
    c["onesb"] = kb.sb("onesb", [128, 128], BF16)
    c["onesf"] = kb.sb("onesf", [128, 128], F32)
    c["cstf"] = kb.sb("cstf", [128, 8], F32)
    p.dma(c["identf"][:], io["c_ident"], writes=["identf"])
    p.dma(c["identb"][:], io["c_ident"], writes=["identb"], q="pool")
    p.dma(c["onesf"][:], io["c_ones"], writes=["onesf"])
    p.dma(c["onesb"][:], io["c_ones"], writes=["onesb"], q="pool")
    p.dma(c["cstf"][:], io["c_cst"], writes=["cst"])
    c["eps_rms"] = c["cstf"][:, 0:1]
    c["eps_ln"] = c["cstf"][:, 1:2]
    c["tiny"] = c["cstf"][:, 2:3]
    c["zrow"] = kb.sb("zrow", [1, 8], BF16)
    p.pool(lambda e: e.memset(c["zrow"][:], 0.0), writes=["zrow"])
    return c


def host_consts():
    cst = np.zeros((128, 8), np.float32)
    cst[:, 0] = RMS_EPS
    cst[:, 1] = LN_EPS
    cst[:, 2] = 1e-30
    cst[:, 3] = 1.0
    return {"c_ident": np.eye(128, dtype=np.float32), "c_ones": np.ones((128, 128), np.float32), "c_cst": cst}


def rope_tables(r):
    half = 16
    freqs = np.power(np.float32(10000.0), -np.arange(half, dtype=np.float32) / half).astype(np.float32)
    rq = np.zeros((NSLOT, 96, 2, 512), np.float32)
    rk = np.zeros((NSLOT, 32, 2, 512), np.float32)
    sc = np.float32(96.0 ** -0.5)
    for s in range(NSLOT):
        pos = (512 * (2 * s + r) + np.arange(512)).astype(np.float32)
        ang = pos[None, :] * freqs[:, None]
        cos, sin = np.cos(ang).astype(np.float32), np.sin(ang).astype(np.float32)
        c2 = np.concatenate([cos, cos], 0)
        s2 = np.concatenate([sin, sin], 0)
        rq[s, 0:64, 0, :] = sc
        rq[s, 64:96, 0, :] = sc * c2
        rq[s, 64:96, 1, :] = sc * s2
        rk[s, :, 0, :] = c2
        rk[s, :, 1, :] = s2
    return rq, rk


def build_a():
    kb = KB()
    io = {}
    io["h_tok"] = kb.din("h_tok", [TOK, D])
    io["c_ident"] = kb.din("c_ident", [128, 128])
    io["c_ones"] = kb.din("c_ones", [128, 128])
    io["c_cst"] = kb.din("c_cst", [128, 8])
    io["ropeq_t"] = kb.din("ropeq_t", [NSLOT, 96, 2, 512])
    io["ropek_t"] = kb.din("ropek_t", [NSLOT, 32, 2, 512])
    w = {"w_in": kb.din("w_in", [D, IN_TOTAL]), "w_uq": kb.din("w_uq", [256, 384]), "g_cq": kb.din("g_cq", [256]),
         "w_ukv": kb.din("w_ukv", [128, 512]), "g_ckv": kb.din("g_ckv", [128])}
    for n, (shp, dt) in A_OUTS.items():
        io[n] = kb.dout(n, shp, dt)
    c = load_consts(kb, io)
    phase_a(kb, c, io, w)
    return kb.finish()


def bconsts_host(r):
    bf = ml_dtypes.bfloat16
    o = {}
    kl = np.arange(128)[:, None]
    ql = np.arange(512)[None, :]
    cm = np.zeros((8, 128, 512), np.float32)
    cms = np.zeros((8, 128, 512), np.float32)
    for jj in range(8):
        kp = 128 * jj + kl
        qp = 512 * r + ql
        cm[jj] = np.where(kp <= qp, 0.0, MASKV)
        cms[jj] = np.where(kp < qp, 0.0, MASKV)
    o["c_cm"] = cm.transpose(1, 0, 2).astype(bf)
    o["c_cms"] = cms.transpose(1, 0, 2).astype(bf)
    wm = np.zeros((12, 128, 512), np.float32)
    for ji, jrel in enumerate(range(-4, 8)):
        dist = 512 * r + ql - 128 * jrel - kl
        wm[ji] = np.where((dist >= 0) & (dist < 512), 0.0, MASKV)
    o["c_wm"] = wm.transpose(1, 0, 2).astype(bf)
    pm = np.zeros((3, 128, 512), np.float32)
    for ii, idx in enumerate((6, 7, 8)):
        pm[ii] = np.where(16 * kl + 31 - 512 * r - ql <= 1024 * (idx - 6), 0.0, MASKV)
    o["c_pm"] = pm.transpose(1, 0, 2).astype(bf)
    kp = np.arange(S)
    o["c_kaug"] = np.stack([kp // 128, kp % 128, np.ones(S), np.ones(S)]).astype(bf)
    ce = 16 * np.arange(512) + 31
    caug = np.stack([ce // 128, ce % 128, np.ones(512), np.ones(512)]).astype(np.float32)
    caug[0, 511] = -30000.0
    o["c_caug"] = caug.astype(bf)
    tl = np.arange(TOK)
    qp = 512 * (2 * (tl // 512) + r) + tl % 512
    qa = np.zeros((4, 4, TOK), np.float32)
    for h in range(4):
        sl = SLOPES[h]
        qa[h, 0] = 128 * sl
        qa[h, 1] = sl
        qa[h, 2] = -sl * 128 * (qp // 128)
        qa[h, 3] = -sl * (qp % 128)
    o["c_qaug"] = qa.astype(bf)
    o["c_g32"] = ((np.arange(S)[None, :] // 64) % 32 == np.arange(32)[:, None]).astype(np.float32).astype(bf)
    o["c_tm"] = (np.arange(S)[None, :] // 256 == np.arange(32)[:, None]).astype(np.float32).astype(bf)
    n = np.arange(512)[:, None]
    s_ = np.arange(128)[None, :]
    ov = ((16 * n < 64 * s_ + 64) & (16 * n + 32 > 64 * s_)).astype(np.float32)
    ov = np.concatenate([ov, np.ones((512, 1), np.float32)], 1)
    ov[511] = 0.0
    o["c_nui"] = -(np.arange(128)[:, None] >= np.arange(128)[None, :]).astype(np.float32).astype(bf)
    o["c_ov"] = ov.reshape(4, 128, 129).transpose(1, 0, 2).astype(bf)
    vb = np.zeros((8, 4, 32), np.float32)
    own_t = np.zeros((8, 4, 32), np.float32)
    for s in range(8):
        for qb in range(4):
            own = (4 * (2 * s + r) + qb) // 2
            vb[s, qb] = np.where(np.arange(32) < own, 0.0, -1e30)
            own_t[s, qb, own] = 1.0
    o["c_vb"] = np.broadcast_to(vb[None], (128, 8, 4, 32)).copy()
    o["c_own"] = np.broadcast_to(own_t[None], (128, 8, 4, 32)).copy()
    M = np.zeros((8, 128, 4, 128), np.float32)
    C = np.zeros((8, 128, 4, 128), np.float32)
    sid = np.arange(128)[None, :]
    for s in range(8):
        for qb in range(4):
            qpos = 512 * (2 * s + r) + 128 * qb + np.arange(128)[:, None]
            cur = qpos // 64
            forced_cur = sid == cur
            forced0 = (sid == 0) & ~forced_cur
            past = (sid < cur) & ~forced0 & ~forced_cur
            M[s, :, qb] = past
            C[s, :, qb] = np.where(forced_cur, 1e30, np.where(forced0, 5e29, np.where(past, 0.0, -1e30)))
    o["c_selm"] = M
    o["c_selc"] = C
    return o


B_CONST_SHAPES = {
    "c_cm": ([128, 8, 512], BF16), "c_cms": ([128, 8, 512], BF16), "c_wm": ([128, 12, 512], BF16), "c_pm": ([128, 3, 512], BF16),
    "c_kaug": ([4, S], BF16), "c_caug": ([4, 512], BF16), "c_qaug": ([4, 4, TOK], BF16),
    "c_g32": ([32, S], BF16), "c_tm": ([32, S], BF16), "c_ov": ([128, 4, 129], BF16),
    "c_nui": ([128, 128], BF16), "c_vb": ([128, 8, 4, 32], F32), "c_own": ([128, 8, 4, 32], F32),
    "c_selm": ([8, 128, 4, 128], F32), "c_selc": ([8, 128, 4, 128], F32),
}

B_INS = {
    "qsb_d": ([256, TOK], BF16), "qmo_d": ([256, TOK], BF16), "qml_d": ([4, 96, TOK], BF16), "qns_d": ([256, TOK], BF16),
    "qme_d": ([256, TOK], BF16), "gns_d": ([12, TOK], F32),
    "ksb_f": ([256, S], BF16), "vsb_f": ([S, 256], BF16), "kmo_f": ([256, S], BF16), "vmo_f": ([S, 256], BF16),
    "kml_f": ([4, 64, S], BF16), "krl_f": ([32, S], BF16), "vml_f": ([S, 256], BF16),
    "kcv_f": ([128, S], BF16), "ksl_f": ([64, S], BF16), "kwi_f": ([64, S], BF16), "vsw_f": ([S, 128], BF16),
    "mem": ([256, D], F32),
}


class AttnBufs:
    pass


class KFull:
    def __init__(self, ap):
        self.ap = ap

    def rows(self, lo, hi):
        return ("full", self.ap[lo:hi, :])


class KPair:
    def __init__(self, ap, R, base=0):
        self.ap, self.R, self.base = ap, R, base

    def rows(self, lo, hi):
        return ("pair", self.ap, self.R, self.base + lo, hi - lo)


class VFull:
    def __init__(self, ap, cbase=0):
        self.ap, self.cbase = ap, cbase

    def cols(self, c0):
        return ("full", self.ap, self.cbase + c0)


class VPair:
    def __init__(self, chunks, cbase=0):
        self.chunks, self.cbase = chunks, cbase

    def cols(self, c0):
        return ("pair", self.chunks, self.cbase + c0)


class TMChunks:
    def __init__(self, chunks, c0, c1):
        self.chunks, self.c0, self.c1 = chunks, c0, c1

    def __getitem__(self, key):
        rs, cs = key
        k = rs.start // 1024
        a = self.chunks[k][rs.start - 1024 * k:rs.stop - 1024 * k, self.c0:self.c1]
        return a[:, cs]


def phase_b(kb, c, io, w, branches=("sb", "moba", "mla", "nsa", "mem")):
    nc, p = kb.nc, kb.p
    A = AttnBufs()
    A.KT = [kb.sb("KT", [128, S], BF16) for _ in range(2)]
    A.V = [kb.sb("V", [128, 64, 65], BF16) for _ in range(2)]
    A.QT = kb.sb("QT", [128, 4, TOK], BF16)
    A.cm = kb.sb("cm", [128, 8, 512], BF16)
    A.cms = kb.sb("cms", [128, 8, 512], BF16)
    A.P = [kb.sb("P", [128, 512], BF16) for _ in range(3)]
    A.rden = [kb.sb("rden", [65, 512], F32) for _ in range(2)]
    A.bcs = [kb.sb("bcs", [64, 512], F32) for _ in range(2)]
    A.ost = [kb.sb("ost", [64, 512], BF16) for _ in range(2)]
    A.cnt = {"kt": 0, "v": 0, "P": 0, "fin": 0, "sc": 0}
    p.dma(A.cm[:], io["c_cm"], writes=["cm"])
    p.dma(A.cms[:], io["c_cms"], writes=["cms"])
    for i in range(2):
        p.pool(lambda e, i=i: e.memset(A.V[i][:, :, 64:65], 1.0), writes=[("V", i)])

    def load_K(rows_src, dk, aug=None):
        i = A.cnt["kt"] % 2
        A.cnt["kt"] += 1
        kt = A.KT[i]
        for (src, r0) in rows_src:
            if src[0] == "full":
                n = src[1].shape[0]
                p.dma(kt[r0:r0 + n, :], src[1], writes=[("KT", i)])
            else:
                _, ap, R, row0, n = src
                for rr in range(2):
                    p.dma(kt[r0:r0 + n, :].rearrange("p (s r i) -> p s r i", r=2, i=512)[:, :, rr, :],
                          ap[rr * R + row0:rr * R + row0 + n, :].rearrange("p (s i) -> p s i", i=512), writes=[("KT", i)])
        if aug is not None:
            p.dma(kt[dk:dk + 4, :], aug, writes=[("KT", i)])
        return kt, ("KT", i)

    def load_V(src, col0):
        i = A.cnt["v"] % 2
        A.cnt["v"] += 1
        v = A.V[i]
        sp_ = src.cols(col0)
        if sp_[0] == "full":
            p.dma(v[:, :, 0:64], sp_[1].rearrange("(n p) c -> p n c", p=128)[:, :, sp_[2]:sp_[2] + 64], writes=[("V", i)])
        else:
            _, chunks, cc0 = sp_
            for k, ch in enumerate(chunks):
                for rr in range(2):
                    for s2 in range(2):
                        n0 = 16 * k + 8 * s2 + 4 * rr
                        p.dma(v[:, n0:n0 + 4, 0:64],
                              ch[rr * 1024 + s2 * 512:rr * 1024 + (s2 + 1) * 512, cc0:cc0 + 64].rearrange("(q p) c -> p q c", p=128),
                              writes=[("V", i)])
        return v, ("V", i)

    def load_Q(src_rows, h, dk, aug=None):
        p.dma(A.QT[0:dk, h, :], src_rows, writes=[("QT", h)])
        if aug is not None:
            p.dma(A.QT[dk:dk + 4, h, :], aug, writes=[("QT", h)])
        return ("QT", h)

    def finalize_plain(ops_bank, dst, h, s, gate=None, acc=None, acc_tok=None, first=True, last=True):
        k = A.cnt["fin"] % 2
        A.cnt["fin"] += 1
        ps = kb.ps(ops_bank)
        rd, bcs, ost = A.rden[k], A.bcs[k], A.ost[k]
        bcb = 6 + k

        def part1():
            p.dve(lambda e: e.tensor_scalar(out=rd[64:65, :], in0=ps[64:65, :], scalar1=1e-30, scalar2=None, op0=ALU.max),
                  reads=[("ps", ops_bank)], writes=[("rden", k)])
            p.dve(lambda e: e.reciprocal(out=rd[64:65, :], in_=rd[64:65, :]), reads=[("rden", k)], writes=[("rden", k)])
            if gate is not None:
                gt, gtok, gidx = gate
                p.dve(lambda e: e.tensor_tensor(out=rd[64:65, :], in0=rd[64:65, :], in1=gt[64:65, gidx, :], op=ALU.mult),
                      reads=[("rden", k), gtok], writes=[("rden", k)])

        def part2():
            pb = kb.ps(bcb)
            p.pe(lambda e: e.matmul(pb[0:64, :], lhsT=c["onesf"][64:65, 0:64], rhs=rd[64:65, :], start=True, stop=True),
                 reads=[("rden", k), "onesf"], writes=[("ps", bcb)])
            p.act(lambda e: e.copy(out=bcs[:], in_=pb[0:64, :]), reads=[("ps", bcb)], writes=[("bcs", k)])
            if acc is None:
                p.dve(lambda e: e.tensor_tensor(out=ost[:], in0=ps[0:64, :], in1=bcs[:], op=ALU.mult),
                      reads=[("ps", ops_bank), ("bcs", k)], writes=[("ost", k)])
                p.dma(dst, ost[:], reads=[("ost", k)], writes=[("o_d", h, s, id(dst) % 997)])
            else:
                if first:
                    p.dve(lambda e: e.tensor_tensor(out=acc, in0=ps[0:64, :], in1=bcs[:], op=ALU.mult),
                          reads=[("ps", ops_bank), ("bcs", k)], writes=[acc_tok])
                else:
                    p.dve(lambda e: e.tensor_tensor(out=bcs[:], in0=ps[0:64, :], in1=bcs[:], op=ALU.mult),
                          reads=[("ps", ops_bank), ("bcs", k)], writes=[("bcs", k)])
                    p.dve(lambda e: e.tensor_tensor(out=acc, in0=acc, in1=bcs[:], op=ALU.add),
                          reads=[acc_tok, ("bcs", k)], writes=[acc_tok])
                if last:
                    p.dve(lambda e: e.tensor_copy(out=ost[:], in_=acc), reads=[acc_tok], writes=[("ost", k)])
                    p.dma(dst, ost[:], reads=[("ost", k)], writes=[("o_d", h, s, id(dst) % 997)])
        return part1, part2

    def run_softmax(items, KT, ktok, dk, V, vtok, qh, qtok, pending, hook=None):
        n = len(items)

        def stage1(i):
            it = items[i]
            if "pre" in it:
                it["pre"]()
            b = i % 2
            ps = kb.ps(b)
            ex = it["extras"]
            s, j = it["s"], it["j"]
            qr = it["qrhs"] if "qrhs" in it else A.QT[0:dk, qh, s * 512:(s + 1) * 512]
            p.pe(lambda e: e.matmul(ps[:], lhsT=KT[0:dk, j * 128:(j + 1) * 128], rhs=qr,
                                    start=True, stop=(len(ex) == 0)),
                 reads=[ktok, qtok] + list(it.get("qreads", ())), writes=[("ps", b)])
            for xi, (lh, rh, toks) in enumerate(ex):
                p.pe(lambda e, lh=lh, rh=rh, xi=xi: e.matmul(ps[:], lhsT=lh, rhs=rh, start=False, stop=(xi == len(ex) - 1)),
                     reads=list(toks), writes=[("ps", b)])
            if "pdst" in it:
                P, ptok = it["pdst"]
            else:
                pk = A.cnt["P"] % 3
                A.cnt["P"] += 1
                P, ptok = A.P[pk][:], ("P", pk)
            it["P"], it["ptok"] = P, ptok
            p.act(lambda e: e.activation(out=P, in_=ps[:], func=AF.Exp), reads=[("ps", b)], writes=[ptok])

        def stage2(i):
            it = items[i]
            P, ptok = it["P"], it["ptok"]
            ob = it["obank"]
            po = kb.ps(ob)
            jv = it.get("jv", it["j"])
            p.pe(lambda e: e.matmul(po[0:65, :], lhsT=V[:, jv, 0:65], rhs=P, start=it["first"], stop=it["last"]),
                 reads=[vtok, ptok], writes=[("ps", ob)])
            if it["last"]:
                p1, p2 = it["fin"]
                p1()
                pending.append([2, p2])

        for i in range(n + 1):
            if i < n:
                stage1(i)
            if i >= 1:
                stage2(i - 1)
            for pd in list(pending):
                pd[0] -= 1
                if pd[0] <= 0:
                    pd[1]()
                    pending.remove(pd)

    def flush(pending):
        for pd in pending:
            pd[1]()
        pending.clear()

    A.load_K, A.load_V, A.load_Q = load_K, load_V, load_Q
    A.finalize_plain, A.run_softmax, A.flush = finalize_plain, run_softmax, flush
    oT = io["oT_d"]

    if "mla" in branches:
        pending = []
        for h in range(4):
            KT, ktok = load_K([(io["kml_f"].rows(h * 64, h * 64 + 64), 0), (io["krl_f"].rows(0, 32), 64)], 96)
            V, vtok = load_V(io["vml_f"], h * 64)
            qtok = load_Q(io["qml_d"][h], h, 96)
            items = []
            for s in range(NSLOT):
                nkb = 8 * s + 8
                ob = 4 + (s % 2)
                for j in range(nkb):
                    jj = j - 8 * s
                    ex = []
                    if jj >= 0:
                        ex.append((c["identb"][:], A.cm[:, jj, :], ["identb", "cm"]))
                    it = dict(j=j, s=s, extras=ex, first=(j == 0), last=(j == nkb - 1), obank=ob)
                    if j == nkb - 1:
                        it["fin"] = finalize_plain(ob, oT[2, h * 64:(h + 1) * 64, s * 512:(s + 1) * 512], h, s)
                    items.append(it)
            run_softmax(items, KT, ktok, 96, V, vtok, h, qtok, pending)
        flush(pending)

    if "mem" in branches:
        pending = []
        memst = kb.sb("memst", [128, 2, 1024], F32)
        memT = kb.sb("memT", [128, 8, 256], BF16)
        wmk = kb.sb("wmk", [128, 8, 512], BF16)
        KTm = kb.sb("KTm", [64, 4, 256], BF16)
        Vm = kb.sb("Vm", [128, 2, 4, 65], BF16)
        p.dma(memst[:], io["mem"].rearrange("(n p) c -> p n c", p=128), writes=["memst"])
        p.dma(wmk[:], w["w_mem_kv"].rearrange("(f p) c -> p f c", p=128), writes=["wmk"], q="pool")
        p.pool(lambda e: e.memset(Vm[:, :, :, 64:65], 1.0), writes=["Vm"])
        for kc in range(2):
            for half in range(2):
                ps = kb.ps(7)
                for jx in range(4):
                    f = half * 4 + jx
                    p.pe(lambda e, ps=ps, kc=kc, f=f, jx=jx: e.transpose(out=ps[:, jx * 128:(jx + 1) * 128], in_=memst[:, kc, f * 128:(f + 1) * 128], identity=c["identf"][:]),
                         reads=["memst", "identf"], writes=[("ps", 7)])
                p.dve(lambda e, ps=ps, kc=kc, half=half: e.tensor_copy(out=memT[:, half * 4:half * 4 + 4, kc * 128:(kc + 1) * 128], in_=ps[:].rearrange("p (j t) -> p j t", j=4)),
                      reads=[("ps", 7)], writes=["memT"])
        for h in range(4):
            ps = kb.ps(7)
            for f in range(8):
                p.pe(lambda e, ps=ps, f=f, h=h: e.matmul(ps[0:64, 0:256], lhsT=wmk[:, f, h * 64:(h + 1) * 64], rhs=memT[:, f, :], start=(f == 0), stop=(f == 7)),
                     reads=["wmk", "memT"], writes=[("ps", 7)])
            p.dve(lambda e, ps=ps, h=h: e.tensor_copy(out=KTm[:, h, :], in_=ps[0:64, 0:256]), reads=[("ps", 7)], writes=["KTm"])
        for kc in range(2):
            ps = kb.ps(7)
            for f in range(8):
                p.pe(lambda e, ps=ps, f=f, kc=kc: e.matmul(ps[:, 0:256], lhsT=memT[:, f, kc * 128:(kc + 1) * 128], rhs=wmk[:, f, 256:512], start=(f == 0), stop=(f == 7)),
                     reads=["wmk", "memT"], writes=[("ps", 7)])
            p.dve(lambda e, ps=ps, kc=kc: e.tensor_copy(out=Vm[:, kc, :, 0:64], in_=ps[:, 0:256].rearrange("p (h d) -> p h d", h=4)),
                  reads=[("ps", 7)], writes=["Vm"])
        for h in range(4):
            qtok = load_Q(io["qme_d"][h * 64:(h + 1) * 64, :], h, 64)
            items = []
            for s in range(NSLOT):
                ob = 4 + (s % 2)
                for j in range(2):
                    it = dict(j=j, s=s, extras=[], first=(j == 0), last=(j == 1), obank=ob)
                    if j == 1:
                        it["fin"] = finalize_plain(ob, oT[4, h * 64:(h + 1) * 64, s * 512:(s + 1) * 512], h, s)
                    items.append(it)
            run_softmax(items, KTm[:, h, :], "KTm", 64, Vm[:, :, h, :], "Vm", h, qtok, pending)
        flush(pending)

    if "moba" in branches:
        pending = []
        vbt = kb.sb("vbt", [128, 8, 4, 32], F32)
        ownt = kb.sb("ownt", [128, 8, 4, 32], F32)
        kmf = kb.sb("kmf", [64, 32], F32)
        kmb = kb.sb("kmb", [64, 32], BF16)
        gsv = kb.sb("gsv", [128, 4, 32], F32)
        m8 = kb.sb("m8", [128, 4, 8], F32)
        m1p = kb.sb("m1p", [128, 4, 128], F32)
        m1 = m1p[:, :, 64:96]
        m2 = kb.sb("m2", [128, 4, 32], F32)
        p.pool(lambda e: e.memset(m1p[:], 0.0), writes=["m1"])
        p.dma(vbt[:], io["c_vb"], writes=["vbt"])
        p.dma(ownt[:], io["c_own"], writes=["ownt"])
        for h in range(4):
            KT, ktok = load_K([(io["kmo_f"].rows(h * 64, h * 64 + 64), 0), (("full", io["c_tm"]), 64)], 96, aug=io["c_kaug"])
            V, vtok = load_V(io["vmo_f"], h * 64)
            qtok = ("QT", h)
            p.dma(A.QT[0:64, h, :], io["qmo_d"][h * 64:(h + 1) * 64, :], writes=[qtok])
            p.dma(A.QT[96:100, h, :], io["c_qaug"][h], writes=[qtok])
            p.dve(lambda e, KT=KT: e.tensor_reduce(out=kmf[:], in_=KT[0:64, :].rearrange("p (n k) -> p n k", k=256), axis=AX.X, op=ALU.add),
                  reads=[ktok], writes=["kmf"])
            p.dve(lambda e: e.tensor_scalar_mul(out=kmb[:], in0=kmf[:], scalar1=1.0 / 256.0), reads=["kmf"], writes=["kmb"])

            def make_pre(s, h=h, qtok=qtok):
                def pre():
                    ps = kb.ps(7)
                    for qb in range(4):
                        c0 = s * 512 + qb * 128
                        p.pe(lambda e, qb=qb, c0=c0: e.matmul(ps[:, qb * 32:(qb + 1) * 32], lhsT=A.QT[0:64, h, c0:c0 + 128], rhs=kmb[:], start=True, stop=True),
                             reads=[qtok, "kmb"], writes=[("ps", 7)])
                    p.dve(lambda e: e.tensor_tensor(out=gsv[:], in0=ps[:, 0:128].rearrange("p (a b) -> p a b", a=4), in1=vbt[:, s, :, :], op=ALU.add),
                          reads=[("ps", 7), "vbt"], writes=["gsv"])
                    for qb in range(4):
                        p.dve(lambda e, qb=qb: e.max(out=m8[:, qb, :], in_=gsv[:, qb, :]), reads=["gsv"], writes=["m8"])
                    for qb in range(4):
                        p.dve(lambda e, qb=qb: e.tensor_scalar(out=m1[:, qb, :], in0=gsv[:, qb, :], scalar1=m8[:, qb, 2:3], scalar2=None, op0=ALU.is_ge),
                              reads=["gsv", "m8"], writes=["m1"])
                    p.dve(lambda e: e.tensor_scalar(out=m2[:], in0=gsv[:], scalar1=-1e29, scalar2=None, op0=ALU.is_gt), reads=["gsv"], writes=["m2"])
                    p.dve(lambda e: e.tensor_tensor(out=m1, in0=m1, in1=m2[:], op=ALU.mult), reads=["m1", "m2"], writes=["m1"])
                    p.dve(lambda e: e.tensor_tensor(out=m1, in0=m1, in1=ownt[:, s, :, :], op=ALU.add), reads=["m1", "ownt"], writes=["m1"])
                    p.dve(lambda e: e.tensor_scalar(out=m1, in0=m1, scalar1=1.0, scalar2=-MASKV, op0=ALU.subtract, op1=ALU.mult),
                          reads=["m1"], writes=["m1"])
                    ps2 = kb.ps(7)
                    for qb in range(4):
                        p.pe(lambda e, qb=qb: e.transpose(out=ps2[:, qb * 128:(qb + 1) * 128], in_=m1p[:, qb, :], identity=c["identf"][:]),
                             reads=["m1", "identf"], writes=[("ps", 7)])
                    p.act(lambda e: e.copy(out=A.QT[64:96, h, s * 512:(s + 1) * 512], in_=ps2[64:96, :]), reads=[("ps", 7)], writes=[("QTs", h, s)])
                return pre

            items = []
            for s in range(NSLOT):
                nkb = 8 * s + 8
                ob = 4 + (s % 2)
                for j in range(nkb):
                    jj = j - 8 * s
                    ex = []
                    if jj >= 0:
                        ex.append((c["identb"][:], A.cm[:, jj, :], ["identb", "cm"]))
                    it = dict(j=j, s=s, extras=ex, first=(j == 0), last=(j == nkb - 1), obank=ob, qreads=[("QTs", h, s)])
                    if j == 0:
                        it["pre"] = make_pre(s)
                    if j == nkb - 1:
                        it["fin"] = finalize_plain(ob, oT[1, h * 64:(h + 1) * 64, s * 512:(s + 1) * 512], h, s)
                    items.append(it)
            run_softmax(items, KT, ktok, 100, V, vtok, h, qtok, pending)
        flush(pending)

    if "sb" in branches:
        nui = kb.sb("nui", [128, 128], BF16)
        negone = kb.sb("negone", [1, 128], BF16)
        p.dma(nui[:], io["c_nui"], writes=["nui"])
        p.pool(lambda e: e.memset(negone[:], -1.0), writes=["negone"])
        E = [kb.sb("E", [128, 512], F32) for _ in range(2)]
        SP = [kb.sb("SP", [128, 512], BF16) for _ in range(2)]
        AB = [kb.sb("AB", [128, 512], BF16) for _ in range(2)]
        carf = [kb.sb("carf", [1, 512], F32) for _ in range(2)]
        carb = [kb.sb("carb", [1, 512], BF16) for _ in range(2)]
        sbo = [kb.sb("sbo", [64, 512], BF16) for _ in range(2)]
        one_ap = c["cstf"][:, 3:4]
        for hp in range(2):
            hs = (2 * hp, 2 * hp + 1)
            KTs, ktoks, Vs, vtoks, qtoks = [], [], [], [], []
            for h in hs:
                KT, ktok = load_K([(io["ksb_f"].rows(h * 64, h * 64 + 64), 0)], 64)
                V, vtok = load_V(io["vsb_f"], h * 64)
                qtok = load_Q(io["qsb_d"][h * 64:(h + 1) * 64, :], h, 64)
                KTs.append(KT); ktoks.append(ktok); Vs.append(V); vtoks.append(vtok); qtoks.append(qtok)
            merged = []
            for s_ in range(NSLOT):
                nkb = 8 * s_ + 8
                for j in range(nkb - 1, -1, -1):
                    for st in range(2):
                        merged.append(dict(j=j, s=s_, st=st, h=hs[st], first=(j == nkb - 1), last=(j == 0)))
            for i, it in enumerate(merged):
                it["i"] = i

            def qk(it, bank, more):
                ps = kb.ps(bank)
                j, s_, st, h = it["j"], it["s"], it["st"], it["h"]
                jj = j - 8 * s_
                KT = KTs[st]
                p.pe(lambda e: e.matmul(ps[:], lhsT=KT[0:64, j * 128:(j + 1) * 128], rhs=A.QT[0:64, h, s_ * 512:(s_ + 1) * 512],
                                        start=True, stop=(jj < 0 and not more)),
                     reads=[ktoks[st], qtoks[st]], writes=[("ps", bank)])
                if jj >= 0:
                    p.pe(lambda e: e.matmul(ps[:], lhsT=c["identb"][:], rhs=A.cms[:, jj, :], start=False, stop=(not more)),
                         reads=["identb", "cms"], writes=[("ps", bank)])
                return ps

            def s1(it):
                k = it["i"] % 2
                b4 = it["i"] % 4
                ps = qk(it, b4, False)
                p.act(lambda e: e.activation(out=E[k][:], in_=ps[:], func=AF.Exp), reads=[("ps", b4)], writes=[("E", k)])
                p.act(lambda e: e.activation(out=SP[k][:], in_=E[k][:], func=AF.Ln, bias=one_ap), reads=[("E", k), "cst"], writes=[("SP", k)])

            def s2a(it):
                k = it["i"] % 2
                b4 = it["i"] % 4
                st = it["st"]
                ps = kb.ps(b4)
                fin_carry = not it["first"]
                p.pe(lambda e: e.matmul(ps[:], lhsT=nui[:], rhs=SP[k][:], start=False, stop=(not fin_carry)),
                     reads=["nui", ("SP", k)], writes=[("ps", b4)])
                if fin_carry:
                    p.pe(lambda e: e.matmul(ps[:], lhsT=negone[0:1, :], rhs=carb[st][0:1, :], start=False, stop=True),
                         reads=["negone", ("carb", st)], writes=[("ps", b4)])
                if not it["last"]:
                    pc = kb.ps(6 + st)
                    p.pe(lambda e: e.matmul(pc[0:1, :], lhsT=c["onesb"][:, 0:1], rhs=SP[k][:], start=True, stop=True),
                         reads=["onesb", ("SP", k)], writes=[("ps", 6 + st)])
                    if it["first"]:
                        p.dve(lambda e: e.tensor_copy(out=carf[st][:], in_=pc[0:1, :]), reads=[("ps", 6 + st)], writes=[("carf", st)])
                    else:
                        p.dve(lambda e: e.tensor_tensor(out=carf[st][:], in0=carf[st][:], in1=pc[0:1, :], op=ALU.add),
                              reads=[("ps", 6 + st), ("carf", st)], writes=[("carf", st)])
                    p.dve(lambda e: e.tensor_copy(out=carb[st][:], in_=carf[st][:]), reads=[("carf", st)], writes=[("carb", st)])
                p.act(lambda e: e.activation(out=AB[k][:], in_=ps[:], func=AF.Exp), reads=[("ps", b4)], writes=[("AB", k)])

            def s2b(it):
                k = it["i"] % 2
                st, j, s_, h = it["st"], it["j"], it["s"], it["h"]
                po = kb.ps(4 + st)
                V = Vs[st]
                p.pe(lambda e: e.matmul(po[0:64, :], lhsT=V[:, j, 0:64], rhs=AB[k][:], start=it["first"], stop=it["last"]),
                     reads=[vtoks[st], ("AB", k)], writes=[("ps", 4 + st)])
                if it["last"]:
                    p.dve(lambda e: e.tensor_copy(out=sbo[st][:], in_=po[0:64, :]), reads=[("ps", 4 + st)], writes=[("sbo", st)])
                    p.dma(oT[0, h * 64:(h + 1) * 64, s_ * 512:(s_ + 1) * 512], sbo[st][:], reads=[("sbo", st)], writes=[("o_sb", h, s_)])

            n = len(merged)
            for i in range(n + 2):
                if i < n:
                    s1(merged[i])
                if 1 <= i <= n:
                    s2a(merged[i - 1])
                if i >= 2:
                    s2b(merged[i - 2])

    if "nsa" in branches:
        pending = []
        QS = kb.sb("QS", [128, 4, 4, 512], BF16)
        OV = kb.sb("OV", [128, 4, 129], BF16)
        pm = kb.sb("pm", [128, 3, 512], BF16)
        wm = kb.sb("wm", [128, 12, 512], BF16)
        wphi = kb.sb("wphi", [128, 32, 128], BF16)
        w2 = kb.sb("w2", [128, 2, 64], BF16)
        peT = kb.sb("peT", [128, 32], BF16)
        peb = kb.sb("peb", [128, 2], F32)
        gx = kb.sb("gx", [128, 512], F32)
        gt_ = kb.sb("gtmp", [128, 512], F32)
        gact = [kb.sb("gact", [128, 512], BF16) for _ in range(2)]
        kcT = kb.sb("kcT", [68, 512], BF16)
        Vc = kb.sb("Vc", [128, 4, 65], BF16)
        psave = kb.sb("psave", [128, 4, 4, 512], BF16)
        gts = [kb.sb("gts", [65, 512], F32) for _ in range(4)]
        nacc = kb.sb("nacc", [64, 4, 512], F32)
        impacc = kb.sb("impacc", [128, 4, 128], F32)
        rdn = kb.sb("rdn", [128, 2], F32)
        selm = [kb.sb("selm", [128, 4, 128], F32) for _ in range(2)]
        selc = [kb.sb("selc", [128, 4, 128], F32) for _ in range(2)]
        scr2 = kb.sb("scr2", [128, 4, 128], F32)
        m16 = kb.sb("m16", [128, 4, 16], F32)
        selvp = kb.sb("selvp", [128, 4, 256], F32)
        selv = selvp[:, :, 64:192]
        gcnt = {"g": 0}
        p.pool(lambda e: e.memset(selvp[:], 0.0), writes=["selv"])
        p.dma(OV[:], io["c_ov"], writes=["OV"])
        p.dma(pm[:], io["c_pm"], writes=["pm"])
        p.dma(wm[:], io["c_wm"], writes=["wm"])
        p.dma(wphi[0:64, :, :], w["w_phi_k1"].rearrange("(t d) h -> d t h", d=64), writes=["wphi"], q="pool")
        p.dma(wphi[64:128, :, :], w["w_phi_v1"].rearrange("(t d) h -> d t h", d=64), writes=["wphi"], q="pool")
        p.dma(w2[:, 0, :], w["w_phi_k2"], writes=["w2"], q="pool")
        p.dma(w2[:, 1, :], w["w_phi_v2"], writes=["w2"], q="pool")
        p.dma(peT[0:64, :], w["nsa_pe"].rearrange("t d -> d t"), writes=["peT"], q="pool", slow=True)
        p.dma(peT[64:128, :], w["nsa_pe"].rearrange("t d -> d t"), writes=["peT"], q="pool", slow=True)
        kcv, kcvtok = load_K([(io["kcv_f"].rows(0, 128), 0)], 128)
        p.pool(lambda e: e.memset(kcT[:], 0.0), writes=["kcT"])
        p.pool(lambda e: e.memset(Vc[:, :, 64:65], 1.0), writes=["Vc"])
        for which in range(2):
            lo = which * 64
            pb = kb.ps(7)
            for t in range(32):
                p.pe(lambda e, t=t, lo=lo, pb=pb: e.matmul(pb[:, 0:1], lhsT=wphi[lo:lo + 64, t, :], rhs=peT[lo:lo + 64, t:t + 1], start=(t == 0), stop=(t == 31)),
                     reads=["wphi", "peT"], writes=[("ps", 7)])
            p.dve(lambda e, pb=pb, which=which: e.tensor_copy(out=peb[:, which:which + 1], in_=pb[:, 0:1]), reads=[("ps", 7)], writes=["peb"])
            ph = kb.ps(6)
            for t in range(32):
                p.pe(lambda e, t=t, lo=lo, ph=ph: e.matmul(ph[:, 0:511], lhsT=wphi[lo:lo + 64, t, :], rhs=kcv[lo:lo + 64, t:t + 16 * 510 + 1:16], start=(t == 0), stop=(t == 31)),
                     reads=["wphi", kcvtok], writes=[("ps", 6)])
            ga = gact[which]
            p.act(lambda e, ph=ph, which=which: e.activation(out=gx[:, 0:511], in_=ph[:, 0:511], func=AF.Identity, bias=peb[:, which:which + 1]),
                  reads=[("ps", 6), "peb"], writes=["gx"])
            p.dve(lambda e: e.tensor_tensor(out=gt_[:, 0:511], in0=gx[:, 0:511], in1=gx[:, 0:511], op=ALU.mult), reads=["gx"], writes=["gtmp"])
            p.dve(lambda e: e.tensor_scalar(out=gt_[:, 0:511], in0=gt_[:, 0:511], scalar1=0.044715, scalar2=1.0, op0=ALU.mult, op1=ALU.add), reads=["gtmp"], writes=["gtmp"])
            p.dve(lambda e: e.tensor_tensor(out=gt_[:, 0:511], in0=gt_[:, 0:511], in1=gx[:, 0:511], op=ALU.mult), reads=["gtmp", "gx"], writes=["gtmp"])
            p.act(lambda e: e.activation(out=gt_[:, 0:511], in_=gt_[:, 0:511], func=AF.Tanh, scale=0.7978845608028654), reads=["gtmp"], writes=["gtmp"])
            p.dve(lambda e: e.tensor_scalar(out=gt_[:, 0:511], in0=gt_[:, 0:511], scalar1=1.0, scalar2=0.5, op0=ALU.add, op1=ALU.mult), reads=["gtmp"], writes=["gtmp"])
            p.pool(lambda e, ga=ga: e.memset(ga[:], 0.0), writes=[("gact", which)])
            p.dve(lambda e, ga=ga: e.tensor_tensor(out=ga[:, 0:511], in0=gt_[:, 0:511], in1=gx[:, 0:511], op=ALU.mult), reads=["gtmp", "gx"], writes=[("gact", which)])
        pk_ = kb.ps(7)
        p.pe(lambda e: e.matmul(pk_[0:64, :], lhsT=w2[:, 0, :], rhs=gact[0][:], start=True, stop=True), reads=["w2", ("gact", 0)], writes=[("ps", 7)])
        p.dve(lambda e: e.tensor_copy(out=kcT[0:64, 0:511], in_=pk_[0:64, 0:511]), reads=[("ps", 7)], writes=["kcT"])
        p.dma(kcT[64:68, :], io["c_caug"], writes=["kcT"])
        pv_ = kb.ps(6)
        for cc in range(4):
            p.pe(lambda e, cc=cc: e.matmul(pv_[:, cc * 64:(cc + 1) * 64], lhsT=gact[1][:, cc * 128:(cc + 1) * 128], rhs=w2[:, 1, :], start=True, stop=True),
                 reads=["w2", ("gact", 1)], writes=[("ps", 6)])
        p.dve(lambda e: e.tensor_copy(out=Vc[:, :, 0:64], in_=pv_[:, 0:256].rearrange("p (c d) -> p c d", c=4)), reads=[("ps", 6)], writes=["Vc"])

        KsT, kstok = load_K([(io["ksl_f"].rows(0, 64), 0), (("full", io["c_g32"]), 64)], 96, aug=io["c_kaug"])
        KwT, kwtok = load_K([(io["kwi_f"].rows(0, 64), 0)], 64, aug=io["c_kaug"])
        Vs, vstok = load_V(io["vsw_f"], 0)
        Vw, vwtok = load_V(io["vsw_f"], 64)
        qtoks = [load_Q(io["qns_d"][h * 64:(h + 1) * 64, :], h, 64, aug=io["c_qaug"][h]) for h in range(4)]

        def gate_row(h, br, s):
            k = gcnt["g"] % 4
            gcnt["g"] += 1
            g = gts[k]
            p.dma(g[64:65, :], io["gns_d"][3 * h + br:3 * h + br + 1, s * 512:(s + 1) * 512], writes=[("gts", k)])
            return (g[:].rearrange("p (o n) -> p o n", o=1), ("gts", k), 0)

        for s in range(NSLOT):
            sl = slice(s * 512, (s + 1) * 512)
            ncmp = s // 2 + 1
            dst = lambda h: oT[3, h * 64:(h + 1) * 64, sl]
            p.dma(selm[s % 2][:], io["c_selm"][s], writes=[("selm", s % 2)])
            p.dma(selc[s % 2][:], io["c_selc"][s], writes=[("selc", s % 2)])
            for h in range(4):
                items = []
                for cc in range(ncmp):
                    idx = s - 2 * cc + 6
                    ex = []
                    if idx <= 8:
                        ex.append((c["identb"][:], pm[:, idx - 6, :], ["identb", "pm"]))
                    it = dict(j=cc, s=s, extras=ex, first=(cc == 0), last=(cc == ncmp - 1), obank=4 + (h % 2),
                              pdst=(psave[:, h, cc, :], ("psave", h, cc)))
                    if cc == ncmp - 1:
                        it["fin"] = finalize_plain(4 + (h % 2), dst(h), h, s, gate=gate_row(h, 0, s), acc=nacc[:, h, :], acc_tok=("nacc", h), first=True, last=False)
                    items.append(it)
                run_softmax(items, kcT, "kcT", 68, Vc, "Vc", h, qtoks[h], pending)
            flush(pending)
            for qb in range(4):
                for h in range(4):
                    bk = 6 + ((qb * 4 + h) % 2)
                    pi = kb.ps(bk)
                    for cc in range(ncmp):
                        p.pe(lambda e, pi=pi, h=h, cc=cc, qb=qb: e.matmul(pi[:, 0:129], lhsT=psave[:, h, cc, qb * 128:(qb + 1) * 128], rhs=OV[:, cc, :],
                                                                          start=(cc == 0), stop=(cc == ncmp - 1)),
                             reads=[("psave", h, cc), "OV"], writes=[("ps", bk)])
                    p.dve(lambda e, pi=pi: e.tensor_scalar(out=rdn[:, 0:1], in0=pi[:, 128:129], scalar1=1e-30, scalar2=None, op0=ALU.max),
                          reads=[("ps", bk)], writes=["rdn"])
                    p.dve(lambda e: e.reciprocal(out=rdn[:, 1:2], in_=rdn[:, 0:1]), reads=["rdn"], writes=["rdn"])
                    if h == 0:
                        p.dve(lambda e, pi=pi, qb=qb: e.tensor_scalar(out=impacc[:, qb, :], in0=pi[:, 0:128], scalar1=rdn[:, 1:2], scalar2=None, op0=ALU.mult),
                              reads=[("ps", bk), "rdn"], writes=["impacc"])
                    else:
                        p.dve(lambda e, pi=pi, qb=qb: e.scalar_tensor_tensor(out=impacc[:, qb, :], in0=pi[:, 0:128], scalar=rdn[:, 1:2], in1=impacc[:, qb, :],
                                                                             op0=ALU.mult, op1=ALU.add),
                              reads=[("ps", bk), "rdn", "impacc"], writes=["impacc"])
            sm, sc_ = selm[s % 2], selc[s % 2]
            p.dve(lambda e, sm=sm: e.tensor_tensor(out=impacc[:], in0=impacc[:], in1=sm[:], op=ALU.mult), reads=["impacc", ("selm", s % 2)], writes=["impacc"])
            p.dve(lambda e, sc_=sc_: e.tensor_tensor(out=impacc[:], in0=impacc[:], in1=sc_[:], op=ALU.add), reads=["impacc", ("selc", s % 2)], writes=["impacc"])
            for qb in range(4):
                p.dve(lambda e, qb=qb: e.max(out=m16[:, qb, 0:8], in_=impacc[:, qb, :]), reads=["impacc"], writes=["m16"])
                p.dve(lambda e, qb=qb: e.match_replace(out=scr2[:, qb, :], in_to_replace=m16[:, qb, 0:8], in_values=impacc[:, qb, :], imm_value=-3.0e38),
                      reads=["impacc", "m16"], writes=["scr2"])
                p.dve(lambda e, qb=qb: e.max(out=m16[:, qb, 8:16], in_=scr2[:, qb, :]), reads=["scr2"], writes=["m16"])
                p.dve(lambda e, qb=qb: e.tensor_scalar(out=selv[:, qb, :], in0=impacc[:, qb, :], scalar1=m16[:, qb, 15:16], scalar2=None, op0=ALU.is_ge),
                      reads=["impacc", "m16"], writes=["selv"])
            p.dve(lambda e: e.tensor_scalar(out=scr2[:], in0=impacc[:], scalar1=-5e29, scalar2=None, op0=ALU.is_gt), reads=["impacc"], writes=["scr2"])
            p.dve(lambda e: e.tensor_tensor(out=selv, in0=selv, in1=scr2[:], op=ALU.mult), reads=["selv", "scr2"], writes=["selv"])
            p.dve(lambda e: e.tensor_scalar(out=selv, in0=selv, scalar1=1.0, scalar2=-MASKV, op0=ALU.subtract, op1=ALU.mult), reads=["selv"], writes=["selv"])
            for h in range(4):
                items = []
                js = [j for j in range(8 * s - 4, 8 * s + 8) if j >= 0]
                for j in js:
                    jrel = j - 8 * s
                    it = dict(j=j, s=s, extras=[(c["identb"][:], wm[:, jrel + 4, :], ["identb", "wm"])], first=(j == js[0]), last=(j == js[-1]), obank=4 + (h % 2))
                    if j == js[-1]:
                        it["fin"] = finalize_plain(4 + (h % 2), dst(h), h, s, gate=gate_row(h, 2, s), acc=nacc[:, h, :], acc_tok=("nacc", h), first=False, last=False)
                    items.append(it)
                run_softmax(items, KwT, kwtok, 68, Vw, vwtok, h, qtoks[h], pending)
            flush(pending)
            nkb = 8 * s + 8
            ngi = (nkb - 1) // 16 + 1
            for gi in range(ngi):
                pt = kb.ps(7)
                for qb in range(4):
                    p.pe(lambda e, qb=qb, gi=gi, pt=pt: e.transpose(out=pt[:, qb * 128:(qb + 1) * 128], in_=selvp[:, qb, 32 * gi:32 * gi + 128], identity=c["identf"][:]),
                         reads=["selv", "identf"], writes=[("ps", 7)])
                p.act(lambda e, gi=gi, pt=pt: e.copy(out=QS[64:96, 0, gi, :], in_=pt[64:96, :]), reads=[("ps", 7)], writes=[("QS", gi)])
                for h in range(4):
                    if h > 0:
                        p.pool(lambda e, gi=gi, h=h: e.tensor_copy(out=QS[64:96, h, gi, :], in_=QS[64:96, 0, gi, :]), reads=[("QS", gi)], writes=[("QS", gi)])
                    p.pool(lambda e, gi=gi, h=h: e.tensor_copy(out=QS[0:64, h, gi, :], in_=A.QT[0:64, h, sl]), reads=[qtoks[h]], writes=[("QS", gi)])
                    p.dma(QS[96:100, h, gi, :], io["c_qaug"][h][:, sl], writes=[("QS", gi)])
            for h in range(4):
                items = []
                for j in range(nkb):
                    jj = j - 8 * s
                    ex = []
                    if jj >= 0:
                        ex.append((c["identb"][:], A.cm[:, jj, :], ["identb", "cm"]))
                    it = dict(j=j, s=s, extras=ex, first=(j == 0), last=(j == nkb - 1), obank=4 + (h % 2),
                              qrhs=QS[0:100, h, j // 16, :], qreads=[("QS", j // 16)])
                    if j == nkb - 1:
                        it["fin"] = finalize_plain(4 + (h % 2), dst(h), h, s, gate=gate_row(h, 1, s), acc=nacc[:, h, :], acc_tok=("nacc", h), first=False, last=True)
                    items.append(it)
                run_softmax(items, KsT, kstok, 100, Vs, vstok, h, qtoks[h], pending)
            flush(pending)
    return A


B_WEIGHTS = {"w_mem_kv": [D, 512], "nsa_pe": [32, 64], "w_phi_k1": [2048, 128], "w_phi_k2": [128, 64],
             "w_phi_v1": [2048, 128], "w_phi_v2": [128, 64]}


def build_b(branches):
    kb = KB()
    io = {}
    for n in ("c_ident", "c_ones"):
        io[n] = kb.din(n, [128, 128])
    io["c_cst"] = kb.din("c_cst", [128, 8])
    for n, (shp, dt) in B_CONST_SHAPES.items():
        io[n] = kb.din(n, shp, dt)
    for n, (shp, dt) in B_INS.items():
        io[n] = kb.din(n, shp, dt)
    w = {n: kb.din(n, shp) for n, shp in B_WEIGHTS.items()}
    io["oT_d"] = kb.dout("oT_d", [5, 256, TOK], BF16)
    io["kml_f"] = KFull(io["kml_f"].rearrange("h d t -> (h d) t"))
    for n in ("ksb_f", "kmo_f", "krl_f", "kcv_f", "ksl_f", "kwi_f"):
        io[n] = KFull(io[n])
    for n in ("vsb_f", "vmo_f", "vml_f", "vsw_f"):
        io[n] = VFull(io[n])
    c = load_consts(kb, io)
    phase_b(kb, c, io, w, branches)
    return kb.finish()


def layer_norm_chunk(kb, c, v, vtok, gbc, bbc, out, otok, tmp):
    p = kb.p
    st, mv, sm = tmp["st"], tmp["mv"], tmp["sm"]
    for hf in range(2):
        p.dve(lambda e, hf=hf: e.bn_stats(out=st[:, hf, :], in_=v[:, hf * 512:(hf + 1) * 512]), reads=[vtok], writes=["lnst"])
    p.dve(lambda e: e.bn_aggr(out=mv[:], in_=st[:]), reads=["lnst"], writes=["lnmv"])
    p.act(lambda e: e.activation(out=sm[:, 0:1], in_=mv[:, 1:2], func=AF.Ln, bias=c["eps_ln"]), reads=["lnmv", "cst"], writes=["lnsm"])
    p.act(lambda e: e.activation(out=sm[:, 0:1], in_=sm[:, 0:1], func=AF.Exp, scale=-0.5), reads=["lnsm"], writes=["lnsm"])
    p.dve(lambda e: e.scalar_tensor_tensor(out=sm[:, 1:2], in0=mv[:, 0:1], scalar=-1.0, in1=sm[:, 0:1], op0=ALU.mult, op1=ALU.mult),
          reads=["lnmv", "lnsm"], writes=["lnsm2"])
    p.act(lambda e: e.activation(out=v[:], in_=v[:], func=AF.Identity, scale=sm[:, 0:1], bias=sm[:, 1:2]), reads=[vtok, "lnsm", "lnsm2"], writes=[vtok])
    p.dve(lambda e: e.tensor_tensor(out=v[:], in0=v[:], in1=gbc[:], op=ALU.mult), reads=[vtok, "lngb"], writes=[vtok])
    p.dve(lambda e: e.tensor_tensor(out=out[:], in0=v[:], in1=bbc[:], op=ALU.add), reads=[vtok, "lngb"], writes=[otok])


def phase_c1(kb, c, io, w):
    nc, p = kb.nc, kb.p
    wg = kb.sb("wg", [128, 5, 8, 1024], BF16)
    wbr = kb.sb("wbr", [128, 5, 2, 1024], BF16)
    wout = kb.sb("wout", [128, 8, 1024], BF16)
    bg = kb.sb("bg", [128, 5, 8], F32)
    gbc = kb.sb("gbc", [128, 1024], F32)
    bbc = kb.sb("bbc", [128, 1024], F32)
    wr = kb.sb("wr", [128, 8, 20], F32)
    brow = kb.sb("brow", [1, 20], F32)
    for i in range(5):
        for f in range(8):
            p.dma(wg[:, i, f, :], w["w_gate"][i, f * 128:(f + 1) * 128, :], writes=[("wg", i)], q="pool")
        p.dma(wbr[:, i, :, :], w["w_br"][i].rearrange("(j p) c -> p j c", p=128), writes=["wbr"], q="pool")
    p.dma(wout[:], w["w_out"].rearrange("(f p) c -> p f c", p=128), writes=["wout"], q="pool")
    p.dma(bg[:], w["b_gate"].rearrange("i (c p) -> p i c", p=128), writes=["bg"], slow=True)
    p.dma(gbc[:], w["ln1_g"].partition_broadcast(128), writes=["lngb"])
    p.dma(bbc[:], w["ln1_b"].partition_broadcast(128), writes=["lngb"])
    p.dma(wr[:, :, 0:4], w["w_rg"].rearrange("(f p) g -> p f g", p=128), writes=["wr"], slow=True)
    for g in range(4):
        p.dma(wr[:, :, 4 + 4 * g:8 + 4 * g], w["w_re"][g].rearrange("(f p) e -> p f e", p=128), writes=["wr"], slow=True)
    p.dma(brow[0:1, 0:4], w["b_rg"].rearrange("(o g) -> o g", o=1), writes=["brow"])
    p.dma(brow[0:1, 4:20], w["b_re"].rearrange("(o g) e -> o (g e)", o=1), writes=["brow"])

    hTt = [kb.sb("hTt", [128, 8, 512], BF16) for _ in range(2)]
    oTt = [kb.sb("oTt", [128, 5, 2, 512], BF16)] * 2
    mT = [kb.sb("mT", [128, 8, 512], BF16) for _ in range(2)]
    sg = [kb.sb("sg", [128, 512], F32) for _ in range(2)]
    acc = kb.sb("macc", [128, 512], F32)
    tmpm = kb.sb("tmpm", [128, 512], F32)
    hch = [kb.sb("hch", [128, 1024], F32)] * 2
    vch = [kb.sb("vch", [128, 1024], F32)] * 2
    h1c = [kb.sb("h1c", [128, 1024], F32) for _ in range(2)]
    h1Tf = [kb.sb("h1Tf", [128, 8, 128], F32) for _ in range(2)]
    h1Tb = [kb.sb("h1Tb", [128, 8, 128], BF16) for _ in range(2)]
    lnt = {"st": kb.sb("lnst", [128, 2, 6], F32), "mv": kb.sb("lnmv", [128, 2], F32), "sm": kb.sb("lnsm", [128, 2], F32)}
    lg = kb.sb("lg", [128, 20], F32)
    r1 = kb.sb("r1", [128, 8], F32)
    goh = kb.sb("goh", [128, 4], F32)
    el = kb.sb("el", [128, 4], F32)
    ee = kb.sb("ee", [128, 4], F32)
    ee2 = kb.sb("ee2", [128, 4], F32)
    gd = [kb.sb("gd", [128, 16], F32) for _ in range(2)]
    bk = {"n": 0}

    def nbank():
        b = bk["n"] % 6
        bk["n"] += 1
        return b

    def do_slot(s):
        d2 = s % 2
        tsl = slice(s * 512, (s + 1) * 512)
        ht, ot, mt = hTt[d2], oTt[d2], mT[d2]
        p.dma(ht[:], io["hT_d"].rearrange("(f p) t -> p f t", p=128)[:, :, tsl], writes=[("hTt", d2)])
        for i in range(5):
            p.dma(ot[:, i, :, :], io["oT_d"][i].rearrange("(j p) t -> p j t", p=128)[:, :, tsl], writes=["oTt"])
        for cc in range(8):
            csl = slice(cc * 128, (cc + 1) * 128)
            for i in range(5):
                bgt, bbr = nbank(), nbank()
                pg, pb = kb.ps(bgt), kb.ps(bbr)
                for f in range(8):
                    p.pe(lambda e, pg=pg, i=i, f=f, csl=csl: e.matmul(pg[:], lhsT=wg[:, i, f, csl], rhs=ht[:, f, :], start=(f == 0), stop=(f == 7)),
                         reads=[("wg", i), ("hTt", d2)], writes=[("ps", bgt)])
                for jc in range(2):
                    p.pe(lambda e, pb=pb, i=i, jc=jc, csl=csl: e.matmul(pb[:], lhsT=wbr[:, i, jc, csl], rhs=ot[:, i, jc, :], start=(jc == 0), stop=(jc == 1)),
                         reads=["wbr", "oTt"], writes=[("ps", bbr)])
                sgi = sg[i % 2]
                p.act(lambda e, sgi=sgi, pg=pg, i=i, cc=cc: e.activation(out=sgi[:], in_=pg[:], func=AF.Sigmoid, bias=bg[:, i, cc:cc + 1]),
                      reads=[("ps", bgt), "bg"], writes=[("sg", i % 2)])
                if i == 0:
                    p.dve(lambda e, sgi=sgi, pb=pb: e.tensor_tensor(out=acc[:], in0=sgi[:], in1=pb[:], op=ALU.mult),
                          reads=[("sg", i % 2), ("ps", bbr)], writes=["macc"])
                else:
                    p.dve(lambda e, sgi=sgi, pb=pb: e.tensor_tensor(out=tmpm[:], in0=sgi[:], in1=pb[:], op=ALU.mult),
                          reads=[("sg", i % 2), ("ps", bbr)], writes=["tmpm"])
                    if i < 4:
                        p.dve(lambda e: e.tensor_tensor(out=acc[:], in0=acc[:], in1=tmpm[:], op=ALU.add), reads=["macc", "tmpm"], writes=["macc"])
                    else:
                        p.dve(lambda e, cc=cc: e.tensor_tensor(out=mt[:, cc, :], in0=acc[:], in1=tmpm[:], op=ALU.add), reads=["macc", "tmpm"], writes=[("mT", d2)])
        if "dbg_mt" in io and s == 0:
            p.dma(io["dbg_mt"], mt[:], reads=[("mT", d2)], writes=["dbg_mt"])
        def do_chunk(tc):
            gck = s * 4 + tc
            k2 = gck % 2
            rows = slice(gck * 128, (gck + 1) * 128)
            hc, vc, h1 = hch[k2], vch[k2], h1c[k2]
            p.dma(hc[:], io["h_tok"][rows, :], writes=["hch"])
            for hf in range(2):
                b = nbank()
                ps = kb.ps(b)
                for cc in range(8):
                    p.pe(lambda e, ps=ps, cc=cc, tc=tc, hf=hf: e.matmul(ps[:], lhsT=mt[:, cc, tc * 128:(tc + 1) * 128], rhs=wout[:, cc, hf * 512:(hf + 1) * 512],
                                                                      start=(cc == 0), stop=(cc == 7)),
                         reads=["wout", ("mT", d2)], writes=[("ps", b)])
                p.dve(lambda e, ps=ps, hf=hf: e.scalar_tensor_tensor(out=vc[:, hf * 512:(hf + 1) * 512], in0=hc[:, hf * 512:(hf + 1) * 512], scalar=ALPHA, in1=ps[:],
                                                                      op0=ALU.mult, op1=ALU.add),
                      reads=["hch", ("ps", b)], writes=["vch"])
            layer_norm_chunk(kb, c, vc, "vch", gbc, bbc, h1, ("h1c", k2), lnt)
            p.dma(io["h1_d"][rows, :], h1[:], reads=[("h1c", k2)], writes=[("h1_d", gck)])
            tf, tb = h1Tf[k2], h1Tb[k2]
            for hf in range(2):
                b = nbank()
                ps = kb.ps(b)
                for jx in range(4):
                    f = hf * 4 + jx
                    p.pe(lambda e, ps=ps, f=f, jx=jx: e.transpose(out=ps[:, jx * 128:(jx + 1) * 128], in_=h1[:, f * 128:(f + 1) * 128], identity=c["identf"][:]),
                         reads=[("h1c", k2), "identf"], writes=[("ps", b)])
                p.act(lambda e, ps=ps, hf=hf: e.copy(out=tf[:, hf * 4:hf * 4 + 4, :], in_=ps[:].rearrange("p (j t) -> p j t", j=4)),
                      reads=[("ps", b)], writes=[("h1Tf", k2)])
            p.pool(lambda e: e.tensor_copy(out=tb[:], in_=tf[:]), reads=[("h1Tf", k2)], writes=[("h1Tb", k2)])
            p.dma(io["h1T_d"].rearrange("(f p) t -> p f t", p=128)[:, :, rows], tb[:], reads=[("h1Tb", k2)], writes=[("h1T_d", gck)])
            b = nbank()
            ps = kb.ps(b)
            for f in range(8):
                p.pe(lambda e, ps=ps, f=f: e.matmul(ps[:, 0:20], lhsT=tf[:, f, :], rhs=wr[:, f, :], start=(f == 0), stop=False),
                     reads=[("h1Tf", k2), "wr"], writes=[("ps", b)])
            p.pe(lambda e, ps=ps: e.matmul(ps[:, 0:20], lhsT=c["onesf"][0:1, :], rhs=brow[0:1, :], start=False, stop=True),
                 reads=["onesf", "brow"], writes=[("ps", b)])
            gdt = gd[k2]
            p.dve(lambda e, ps=ps: e.tensor_copy(out=lg[:], in_=ps[:, 0:20]), reads=[("ps", b)], writes=["lg"])
            p.dve(lambda e: e.tensor_reduce(out=r1[:, 0:1], in_=lg[:, 0:4], axis=AX.X, op=ALU.max), reads=["lg"], writes=["r1a"])
            p.dve(lambda e: e.tensor_scalar(out=goh[:], in0=lg[:, 0:4], scalar1=r1[:, 0:1], scalar2=None, op0=ALU.is_equal), reads=["lg", "r1a"], writes=["goh"])
            p.dve(lambda e: e.tensor_scalar(out=ee[:], in0=lg[:, 0:4], scalar1=r1[:, 0:1], scalar2=None, op0=ALU.subtract), reads=["lg", "r1a"], writes=["ee"])
            p.act(lambda e: e.activation(out=ee[:], in_=ee[:], func=AF.Exp), reads=["ee"], writes=["ee"])
            p.dve(lambda e: e.tensor_reduce(out=r1[:, 1:2], in_=ee[:], axis=AX.X, op=ALU.add), reads=["ee"], writes=["r1b"])
            p.dve(lambda e: e.reciprocal(out=r1[:, 1:2], in_=r1[:, 1:2]), reads=["r1b"], writes=["r1b"])
            p.dve(lambda e: e.tensor_scalar(out=el[:], in0=lg[:, 4:8], scalar1=goh[:, 0:1], scalar2=None, op0=ALU.mult), reads=["lg", "goh"], writes=["el"])
            for g in range(1, 4):
                p.dve(lambda e, g=g: e.scalar_tensor_tensor(out=el[:], in0=lg[:, 4 + 4 * g:8 + 4 * g], scalar=goh[:, g:g + 1], in1=el[:], op0=ALU.mult, op1=ALU.add),
                      reads=["lg", "goh", "el"], writes=["el"])
            p.dve(lambda e: e.tensor_reduce(out=r1[:, 2:3], in_=el[:], axis=AX.X, op=ALU.max), reads=["el"], writes=["r1c"])
            p.dve(lambda e: e.tensor_scalar(out=ee[:], in0=el[:], scalar1=r1[:, 2:3], scalar2=None, op0=ALU.subtract), reads=["el", "r1c"], writes=["ee"])
            p.act(lambda e: e.activation(out=ee[:], in_=ee[:], func=AF.Exp), reads=["ee"], writes=["ee"])
            p.dve(lambda e: e.tensor_scalar(out=ee2[:], in0=ee[:], scalar1=1.0, scalar2=-2.0, op0=ALU.is_ge, op1=ALU.mult), reads=["ee"], writes=["ee2"])
            p.dve(lambda e: e.tensor_tensor(out=ee2[:], in0=ee2[:], in1=ee[:], op=ALU.add), reads=["ee2", "ee"], writes=["ee2"])
            p.dve(lambda e: e.tensor_reduce(out=r1[:, 3:4], in_=ee2[:], axis=AX.X, op=ALU.max), reads=["ee2"], writes=["r1d"])
            p.dve(lambda e: e.tensor_scalar(out=ee2[:], in0=ee[:], scalar1=r1[:, 3:4], scalar2=None, op0=ALU.is_ge), reads=["ee", "r1d"], writes=["ee2"])
            p.dve(lambda e: e.tensor_tensor(out=ee[:], in0=ee[:], in1=ee2[:], op=ALU.mult), reads=["ee", "ee2"], writes=["ee"])
            p.dve(lambda e: e.tensor_scalar(out=r1[:, 4:5], in0=r1[:, 3:4], scalar1=1.0, scalar2=None, op0=ALU.add), reads=["r1d"], writes=["r1e"])
            p.dve(lambda e: e.reciprocal(out=r1[:, 4:5], in_=r1[:, 4:5]), reads=["r1e"], writes=["r1e"])
            p.dve(lambda e: e.tensor_tensor(out=r1[:, 4:5], in0=r1[:, 4:5], in1=r1[:, 1:2], op=ALU.mult), reads=["r1e", "r1b"], writes=["r1e"])
            p.dve(lambda e: e.tensor_scalar(out=ee[:], in0=ee[:], scalar1=r1[:, 4:5], scalar2=None, op0=ALU.mult), reads=["ee", "r1e"], writes=["ee"])
            for g in range(4):
                p.dve(lambda e, g=g: e.tensor_scalar(out=gdt[:, 4 * g:4 * g + 4], in0=ee[:], scalar1=goh[:, g:g + 1], scalar2=None, op0=ALU.mult),
                      reads=["ee", "goh"], writes=[("gd", k2)])
            p.dma(io["gd_d"][rows, :], gdt[:], reads=[("gd", k2)], writes=[("gd_d", gck)])

        for tc in range(4):
            do_chunk(tc)

    for s in range(NSLOT):
        do_slot(s)

C1_W = {"w_br": [5, 256, D], "w_gate": [5, D, D], "b_gate": [5, D], "w_out": [D, D], "ln1_g": [D], "ln1_b": [D],
        "w_rg": [D, 4], "b_rg": [4], "w_re": [4, D, 4], "b_re": [4, 4]}


def build_c1(dbg=False):
    kb = KB()
    io = {}
    if dbg:
        io["dbg_mt"] = kb.dout("dbg_mt", [128, 8, 512], BF16)
    for n in ("c_ident", "c_ones"):
        io[n] = kb.din(n, [128, 128])
    io["c_cst"] = kb.din("c_cst", [128, 8])
    io["h_tok"] = kb.din("h_tok", [TOK, D])
    io["hT_d"] = kb.din("hT_d", [D, TOK], BF16)
    io["oT_d"] = kb.din("oT_d", [5, 256, TOK], BF16)
    w = {n: kb.din(n, shp) for n, shp in C1_W.items()}
    io["h1_d"] = kb.dout("h1_d", [TOK, D])
    io["h1T_d"] = kb.dout("h1T_d", [D, TOK], BF16)
    io["gd_d"] = kb.dout("gd_d", [TOK, 16])
    c = load_consts(kb, io)
    phase_c1(kb, c, io, w)
    return kb.finish()


def phase_c2(kb, c, io, w, nT=4, nE=16):
    nc, p = kb.nc, kb.p
    gbc = kb.sb("gbc2", [128, 1024], F32)
    bbc = kb.sb("bbc2", [128, 1024], F32)
    p.dma(gbc[:], w["ln2_g"].partition_broadcast(128), writes=["lngb"])
    p.dma(bbc[:], w["ln2_b"].partition_broadcast(128), writes=["lngb"])
    hT = [kb.sb("h1Tt", [128, 8, 1024], BF16) for _ in range(2)]
    gdt = [kb.sb("gdt", [128, 8, 16], F32) for _ in range(2)]
    wup = [kb.sb("wup", [128, 8, 512], BF16) for _ in range(2)]
    wdn = [kb.sb("wdn", [128, 2, 1024], BF16) for _ in range(2)]
    yacc = kb.sb("yacc", [128, 8, 1024], F32)
    gT = [kb.sb("gT", [128, 2, 1024], BF16) for _ in range(2)]
    sa = [kb.sb("sa", [128, 512], F32) for _ in range(2)]
    hch = [kb.sb("h1ch", [128, 1024], F32) for _ in range(2)]
    och = [kb.sb("och", [128, 1024], F32) for _ in range(2)]
    lnt = {"st": kb.sb("lnst2", [128, 2, 6], F32), "mv": kb.sb("lnmv2", [128, 2], F32), "sm": kb.sb("lnsm2", [128, 2], F32)}
    st = {"bank": 0, "w": 0, "sa": 0}

    def nbank():
        b = st["bank"] % 8
        st["bank"] += 1
        return b

    def do_expert(T, e, ht, httok, gd, gdtok):
        k = st["w"] % 2
        st["w"] += 1
        wu, wd, g = wup[k], wdn[k], gT[k]
        p.dma(wu[:], w["w_up"][e].rearrange("(f p) c -> p f c", p=128), writes=[("wup", k)], q="pool")
        p.dma(wd[:], w["w_down"][e].rearrange("(j p) c -> p j c", p=128), writes=[("wdn", k)], q="pool")
        for ts in range(2):
            tsl = slice(ts * 512, (ts + 1) * 512)
            for jc in range(2):
                ba, bu = nbank(), nbank()
                pa, pu = kb.ps(ba), kb.ps(bu)
                for f in range(8):
                    p.pe(lambda e_, f=f: e_.matmul(pa[:], lhsT=wu[:, f, jc * 128:(jc + 1) * 128], rhs=ht[:, f, tsl], start=(f == 0), stop=(f == 7)),
                         reads=[("wup", k), httok], writes=[("ps", ba)])
                for f in range(8):
                    p.pe(lambda e_, f=f: e_.matmul(pu[:], lhsT=wu[:, f, 256 + jc * 128:256 + (jc + 1) * 128], rhs=ht[:, f, tsl], start=(f == 0), stop=(f == 7)),
                         reads=[("wup", k), httok], writes=[("ps", bu)])
                si = st["sa"] % 2
                st["sa"] += 1
                sat = sa[si]
                p.act(lambda e_, sat=sat, pa=pa: e_.activation(out=sat[:], in_=pa[:], func=AF.Silu), reads=[("ps", ba)], writes=[("sa", si)])
                p.dve(lambda e_, sat=sat, pu=pu, jc=jc, tsl=tsl: e_.tensor_tensor(out=g[:, jc, tsl], in0=sat[:], in1=pu[:], op=ALU.mult),
                      reads=[("sa", si), ("ps", bu)], writes=[("gT", k)])
        for tc in range(8):
            for hf in range(2):
                b = nbank()
                ps = kb.ps(b)
                for jc in range(2):
                    p.pe(lambda e_, jc=jc, ps=ps, tc=tc, hf=hf: e_.matmul(ps[:], lhsT=g[:, jc, tc * 128:(tc + 1) * 128], rhs=wd[:, jc, hf * 512:(hf + 1) * 512],
                                                                       start=(jc == 0), stop=(jc == 1)),
                         reads=[("gT", k), ("wdn", k)], writes=[("ps", b)])
                ya = yacc[:, tc, hf * 512:(hf + 1) * 512]
                if e == 0:
                    p.dve(lambda e_, ps=ps, ya=ya, tc=tc: e_.tensor_scalar(out=ya, in0=ps[:], scalar1=gd[:, tc, e:e + 1], scalar2=None, op0=ALU.mult),
                          reads=[("ps", b), gdtok], writes=[("yacc", tc)])
                else:
                    p.dve(lambda e_, ps=ps, ya=ya, tc=tc: e_.scalar_tensor_tensor(out=ya, in0=ps[:], scalar=gd[:, tc, e:e + 1], in1=ya, op0=ALU.mult, op1=ALU.add),
                          reads=[("ps", b), gdtok, ("yacc", tc)], writes=[("yacc", tc)])

    def do_chunk_out(T, tc):
        gck = T * 8 + tc
        k2 = gck % 2
        rows = slice(gck * 128, (gck + 1) * 128)
        hc, oc = hch[k2], och[k2]
        p.dma(hc[:], io["h1_d"][rows, :], writes=[("h1ch", k2)])
        p.dve(lambda e_: e_.scalar_tensor_tensor(out=hc[:], in0=hc[:], scalar=ALPHA, in1=yacc[:, tc, :], op0=ALU.mult, op1=ALU.add),
              reads=[("h1ch", k2), ("yacc", tc)], writes=[("h1ch", k2)])
        layer_norm_chunk(kb, c, hc, ("h1ch", k2), gbc, bbc, oc, ("och", k2), lnt)
        p.dma(io["h_out"][rows, :], oc[:], reads=[("och", k2)], writes=[("h_out", gck)])

    def do_tile(T):
        d2 = T % 2
        ht, gd = hT[d2], gdt[d2]
        cols = slice(T * 1024, (T + 1) * 1024)
        p.dma(ht[:], io["h1T_d"].rearrange("(f p) t -> p f t", p=128)[:, :, cols], writes=[("h1Tt", d2)])
        p.dma(gd[:], io["gd_d"][T * 1024:(T + 1) * 1024, :].rearrange("(n p) e -> p n e", p=128), writes=[("gdt", d2)])
        for e in range(nE):
            do_expert(T, e, ht, ("h1Tt", d2), gd, ("gdt", d2))
        for tc in range(8):
            do_chunk_out(T, tc)

    for T in range(nT):
        do_tile(T)


C2_W = {"w_up": [16, D, 512], "w_down": [16, 256, D], "ln2_g": [D], "ln2_b": [D]}


def build_c2(nT=4, nE=16):
    kb = KB()
    io = {}
    for n in ("c_ident", "c_ones"):
        io[n] = kb.din(n, [128, 128])
    io["c_cst"] = kb.din("c_cst", [128, 8])
    io["h1_d"] = kb.din("h1_d", [TOK, D])
    io["h1T_d"] = kb.din("h1T_d", [D, TOK], BF16)
    io["gd_d"] = kb.din("gd_d", [TOK, 16])
    w = {n: kb.din(n, shp) for n, shp in C2_W.items()}
    io["h_out"] = kb.dout("h_out", [TOK, D])
    c = load_consts(kb, io)
    phase_c2(kb, c, io, w, nT, nE)
    return kb.finish()


W_SHAPES = {
    "w_in": [D, IN_TOTAL], "g_cq": [256], "g_ckv": [128], "w_uq": [256, 384], "w_ukv": [128, 512],
    "nsa_pe": [32, 64], "w_phi_k1": [2048, 128], "w_phi_k2": [128, 64], "w_phi_v1": [2048, 128], "w_phi_v2": [128, 64],
    "w_mem_kv": [D, 512], "w_br": [5, 256, D], "w_gate": [5, D, D], "b_gate": [5, D], "w_out": [D, D],
    "ln1_g": [D], "ln1_b": [D], "w_rg": [D, 4], "b_rg": [4], "w_re": [4, D, 4], "b_re": [4, 4],
    "w_up": [16, D, 512], "w_down": [16, 256, D], "ln2_g": [D], "ln2_b": [D],
}
PAIRS = [[0, 1], [2, 3], [4, 5], [6, 7]]


def build_fused(depth=DEPTH, nlw=DEPTH, stop=None):
    kb = KB()
    io = {}
    for n in ("c_ident", "c_ones"):
        io[n] = kb.din(n, [128, 128])
    io["c_cst"] = kb.din("c_cst", [128, 8])
    for n, (shp, dt) in B_CONST_SHAPES.items():
        io[n] = kb.din(n, shp, dt)
    io["ropeq_t"] = kb.din("ropeq_t", [NSLOT, 96, 2, 512])
    io["ropek_t"] = kb.din("ropek_t", [NSLOT, 32, 2, 512])
    io["x_own"] = kb.din("x_own", [TOK, D])
    io["mem"] = kb.din("mem", [256, D])
    wfull = {n: kb.din(n, [nlw] + shp) for n, shp in W_SHAPES.items()}
    io["h_final"] = kb.dout("h_final", [TOK, D])
    hbuf = [kb.dscratch(f"hbuf{i}", [TOK, D]) for i in range(2)]
    io["hT_d"] = kb.dscratch("hT_d", [D, TOK], BF16)
    for n in ("qsb_d", "qmo_d", "qns_d", "qme_d"):
        io[n] = kb.dscratch(n, [256, TOK], BF16)
    io["qml_d"] = kb.dscratch("qml_d", [4, 96, TOK], BF16)
    io["gns_d"] = kb.dscratch("gns_d", [12, TOK])
    io["oT_d"] = kb.dscratch("oT_d", [5, 256, TOK], BF16)
    io["h1_d"] = kb.dscratch("h1_d", [TOK, D])
    io["h1T_d"] = kb.dscratch("h1T_d", [D, TOK], BF16)
    io["gd_d"] = kb.dscratch("gd_d", [TOK, 16])
    xk_rows = [256, 256, 256, 256, 64]
    xk_in = [kb.dscratch(f"xk_in{k}", [r, TOK], BF16) for k, r in enumerate(xk_rows)]
    xk_out = [kb.dscratch(f"xk_out{k}", [2 * r, TOK], BF16) for k, r in enumerate(xk_rows)]
    xv_in = [kb.dscratch(f"xv_in{k}", [1024, 896], BF16) for k in range(4)]
    xv_out = [kb.dscratch(f"xv_out{k}", [2048, 896], BF16) for k in range(4)]
    io["ksb_d"], io["kmo_d"] = xk_in[0], xk_in[1]
    io["kml_d"] = xk_in[2].rearrange("(h d) t -> h d t", h=4)
    io["krl_d"], io["kcv_d"], io["ksl_d"] = xk_in[3][0:32, :], xk_in[3][32:160, :], xk_in[3][160:224, :]
    io["kwi_d"] = xk_in[4]
    io["vsb_d"], io["vmo_d"] = TMChunks(xv_in, 0, 256), TMChunks(xv_in, 256, 512)
    io["vml_d"], io["vsw_d"] = TMChunks(xv_in, 512, 768), TMChunks(xv_in, 768, 896)
    io["ksb_f"], io["kmo_f"], io["kml_f"] = KPair(xk_out[0], 256), KPair(xk_out[1], 256), KPair(xk_out[2], 256)
    io["krl_f"], io["kcv_f"], io["ksl_f"] = KPair(xk_out[3], 256, 0), KPair(xk_out[3], 256, 32), KPair(xk_out[3], 256, 160)
    io["kwi_f"] = KPair(xk_out[4], 64)
    io["vsb_f"], io["vmo_f"] = VPair(xv_out, 0), VPair(xv_out, 256)
    io["vml_f"], io["vsw_f"] = VPair(xv_out, 512), VPair(xv_out, 768)

    for l in range(depth):
        w = {n: ap[l] for n, ap in wfull.items()}
        io["h_tok"] = io["x_own"] if l == 0 else hbuf[(l - 1) % 2]
        io["h_out"] = io["h_final"] if l == depth - 1 else hbuf[l % 2]
        c = load_consts(kb, io)
        phase_a(kb, c, io, w)
        kb.end_phase()
        if stop == "A":
            break
        for k in range(5):
            kb.p.allgather(xk_out[k], xk_in[k], PAIRS, writes=[("xk", k)])
        for k in range(4):
            kb.p.allgather(xv_out[k], xv_in[k], PAIRS, writes=[("xv", k)])
        kb.end_phase()
        if stop == "AG":
            break
        c = load_consts(kb, io)
        phase_b(kb, c, io, w, ("sb", "moba", "mla", "mem"))
        kb.end_phase()
        if stop == "B1":
            break
        c = load_consts(kb, io)
        phase_b(kb, c, io, w, ("nsa",))
        kb.end_phase()
        if stop == "B2":
            break
        c = load_consts(kb, io)
        phase_c1(kb, c, io, w)
        kb.end_phase()
        if stop == "C1":
            break
        c = load_consts(kb, io)
        phase_c2(kb, c, io, w)
        kb.end_phase()
    return kb.finish()


def fused_inputs(inp, c):
    b, r = c // 2, c % 2
    m = dict(host_consts())
    m.update(bconsts_host(r))
    m["ropeq_t"], m["ropek_t"] = rope_tables(r)
    m["x_own"] = np.ascontiguousarray(inp["x"][b][_own_tokens(r)])
    m["mem"] = inp["mem"][b]
    for n in W_SHAPES:
        m[n] = inp[n]
    return m


_PROGS = {}


def _prog(name, fn):
    if name not in _PROGS:
        _PROGS[name] = fn()
    return _PROGS[name]


def _own_tokens(r):
    t = np.arange(TOK)
    return t // 512 * 1024 + r * 512 + t % 512


def _interleave_fm(a0, a1):
    sh = a0.shape[:-1]
    out = np.empty(sh + (S,), a0.dtype)
    o = out.reshape(sh + (8, 2, 512))
    o[..., 0, :] = a0.reshape(sh + (8, 512))
    o[..., 1, :] = a1.reshape(sh + (8, 512))
    return out


def _interleave_tm(a0, a1):
    C = a0.shape[1]
    out = np.empty((S, C), a0.dtype)
    o = out.reshape(8, 2, 512, C)
    o[:, 0] = a0.reshape(8, 512, C)
    o[:, 1] = a1.reshape(8, 512, C)
    return out


A_WEIGHTS = ("w_in", "w_uq", "g_cq", "w_ukv", "g_ckv")
K_FM = {"ksb_d": "ksb_f", "kmo_d": "kmo_f", "kml_d": "kml_f", "krl_d": "krl_f", "kcv_d": "kcv_f", "ksl_d": "ksl_f", "kwi_d": "kwi_f"}
V_TM = {"vsb_d": "vsb_f", "vmo_d": "vmo_f", "vml_d": "vml_f", "vsw_d": "vsw_f"}
Q_OWN = ("qsb_d", "qmo_d", "qml_d", "qns_d", "qme_d", "gns_d")


def kernel_unfused(**inputs):
    inp = {k: np.ascontiguousarray(np.asarray(v)) for k, v in inputs.items()}
    ncores = 8
    cores = list(range(ncores))
    hc = host_consts()
    bc = [bconsts_host(r) for r in range(2)]
    rp = [rope_tables(r) for r in range(2)]
    own = [_own_tokens(r) for r in range(2)]
    h = [np.ascontiguousarray(inp["x"][c // 2][own[c % 2]]) for c in cores]
    nca = _prog("a", build_a)
    ncb1 = _prog("b1", lambda: build_b(("sb", "moba", "mla", "mem")))
    ncb2 = _prog("b2", lambda: build_b(("nsa",)))
    ncc1 = _prog("c1", build_c1)
    ncc2 = _prog("c2", build_c2)
    for l in range(DEPTH):
        maps = []
        for c in cores:
            m = dict(hc)
            m["h_tok"] = h[c]
            m["ropeq_t"], m["ropek_t"] = rp[c % 2]
            for n in A_WEIGHTS:
                m[n] = inp[n][l]
            maps.append(m)
        ra = run_bass_kernel_spmd(nca, maps, core_ids=cores).results
        maps = []
        for c in cores:
            b, r = c // 2, c % 2
            m = dict(hc)
            m.update(bc[r])
            for n in Q_OWN:
                m[n] = ra[c][n]
            for kd, kf in K_FM.items():
                m[kf] = _interleave_fm(np.asarray(ra[2 * b][kd]), np.asarray(ra[2 * b + 1][kd]))
            for vd, vf in V_TM.items():
                m[vf] = _interleave_tm(np.asarray(ra[2 * b][vd]), np.asarray(ra[2 * b + 1][vd]))
            m["mem"] = inp["mem"][b]
            for n in B_WEIGHTS:
                m[n] = inp[n][l]
            maps.append(m)
        rb1 = run_bass_kernel_spmd(ncb1, maps, core_ids=cores).results
        rb2 = run_bass_kernel_spmd(ncb2, maps, core_ids=cores).results
        rb = []
        for c in cores:
            o = np.array(rb1[c]["oT_d"])
            o[3] = np.asarray(rb2[c]["oT_d"])[3]
            rb.append({"oT_d": o})
        maps = []
        for c in cores:
            m = dict(hc)
            m["h_tok"] = h[c]
            m["hT_d"] = ra[c]["hT_d"]
            m["oT_d"] = rb[c]["oT_d"]
            for n in C1_W:
                m[n] = inp[n][l]
            maps.append(m)
        rc1 = run_bass_kernel_spmd(ncc1, maps, core_ids=cores).results
        maps = []
        for c in cores:
            m = dict(hc)
            for n in ("h1_d", "h1T_d", "gd_d"):
                m[n] = rc1[c][n]
            for n in C2_W:
                m[n] = inp[n][l]
            maps.append(m)
        rc2 = run_bass_kernel_spmd(ncc2, maps, core_ids=cores).results
        h = [np.asarray(rc2[c]["h_out"]) for c in cores]
    out = np.empty((NB, S, D), np.float32)
    for c in cores:
        out[c // 2][own[c % 2]] = h[c]
    return out


def kernel(**inputs):
    inp = {k: np.ascontiguousarray(np.asarray(v)) for k, v in inputs.items()}
    cores = list(range(8))
    nc = _prog("fused", build_fused)
    maps = [fused_inputs(inp, c) for c in cores]
    res = run_bass_kernel_spmd(nc, maps, core_ids=cores).results
    out = np.empty((NB, S, D), np.float32)
    for c in cores:
        out[c // 2][_own_tokens(c % 2)] = np.asarray(res[c]["h_final"])
    return out
```

```python
import contextlib
import types
import numpy as np
import ml_dtypes
import concourse.bass as bass
import concourse.mybir as mybir
from concourse.bass_utils import run_bass_kernel_spmd

F32 = mybir.dt.float32
BF16 = mybir.dt.bfloat16
AF = mybir.ActivationFunctionType
ALU = mybir.AluOpType
AX = mybir.AxisListType

D = 1024
S = 8192
NB = 4
DEPTH = 4
TOK = 4096
NSLOT = 8
IN_TOTAL = 2860
ALPHA = (2.0 * DEPTH) ** 0.25
LN_EPS = 1e-5
RMS_EPS = 1e-6
MASKV = -30000.0
SLOPES = [2.0 ** (-2.0 * (i + 1)) for i in range(4)]

ENGS = ("pe", "act", "dve", "pool", "sp")
SIG_EPOCH = 30000


def _freeze(fn):
    if fn.__closure__ is None:
        return fn
    cells = []
    for cl in fn.__closure__:
        try:
            cells.append(types.CellType(cl.cell_contents))
        except ValueError:
            cells.append(cl)
    g = types.FunctionType(fn.__code__, fn.__globals__, fn.__name__, fn.__defaults__, tuple(cells))
    g.__kwdefaults__ = fn.__kwdefaults__
    return g


class Op:
    __slots__ = ("eng", "fn", "reads", "writes", "dma", "deps", "sig", "dticket", "idx", "dprev")

    def __init__(self, eng, fn, reads, writes, dma):
        self.eng = eng
        self.fn = fn
        self.reads = tuple(reads)
        self.writes = tuple(writes)
        self.dma = dma
        self.deps = []
        self.sig = None
        self.dticket = None
        self.dprev = None


class Prog:
    NDSEM = 12
    _phase_id = 0

    def __init__(self, nc):
        self.nc = nc
        self.ops = []

    def add(self, eng, fn, reads=(), writes=(), dma=False):
        op = Op(eng, _freeze(fn), reads, writes, dma)
        op.idx = len(self.ops)
        self.ops.append(op)
        return op

    def pe(self, fn, reads=(), writes=()):
        return self.add("pe", fn, reads, writes)

    def act(self, fn, reads=(), writes=()):
        return self.add("act", fn, reads, writes)

    def dve(self, fn, reads=(), writes=()):
        return self.add("dve", fn, reads, writes)

    def pool(self, fn, reads=(), writes=()):
        return self.add("pool", fn, reads, writes)

    def allgather(self, out, in_, groups, reads=(), writes=()):
        return self.add("pool", lambda e: e.collective_compute("AllGather", ALU.bypass, replica_groups=groups, ins=[in_.opt()], outs=[out.opt()]),
                        reads, writes, dma="cc")

    def dma(self, out, in_, reads=(), writes=(), q="sp", slow=False):
        if slow:
            return self.add(q, lambda e: e.dma_start(out=out, in_=in_, allow_slow_non_contiguous=True), reads, writes, dma=True)
        return self.add(q, lambda e: e.dma_start(out=out, in_=in_), reads, writes, dma=True)

    def analyze(self):
        last_w = {}
        readers = {}
        for op in self.ops:
            deps = set()
            for t in op.reads:
                if t in last_w:
                    deps.add(last_w[t])
            for t in op.writes:
                if t in last_w:
                    deps.add(last_w[t])
                for r in readers.get(t, ()):
                    deps.add(r)
            deps.discard(op.idx)
            op.deps = sorted(deps)
            for t in op.reads:
                readers.setdefault(t, []).append(op.idx)
            for t in op.writes:
                last_w[t] = op.idx
                readers[t] = []
        qcount = {e: 0 for e in ENGS}
        qhist = {e: [] for e in ENGS}
        for op in self.ops:
            if op.dma == "cc":
                op.dticket = ("cc", op.idx, 1)
                continue
            if op.dma:
                n = qcount[op.eng]
                qcount[op.eng] += 1
                op.dticket = (op.eng, n % self.NDSEM, 16 * (n // self.NDSEM + 1))
                if n >= self.NDSEM:
                    op.dprev = qhist[op.eng][n - self.NDSEM]
                qhist[op.eng].append(op.idx)
        waited_eng = {e: {p: -1 for p in ENGS} for e in ENGS}
        waited_dma = {e: set() for e in ENGS}
        last_on = {e: -1 for e in ENGS}
        need_sig = set()
        for op in self.ops:
            e = op.eng
            final = []
            best = {}
            dl = list(op.deps)
            if op.dprev is not None:
                dl.append(op.dprev)
            for d in dl:
                p = self.ops[d]
                if p.dma:
                    if d not in waited_dma[e]:
                        waited_dma[e].add(d)
                        final.append(("dma", d))
                else:
                    if p.eng == e and e == "pe":
                        continue
                    if d <= waited_eng[e][p.eng]:
                        continue
                    if p.eng not in best or d > best[p.eng]:
                        best[p.eng] = d
            for pe_, d in best.items():
                waited_eng[e][pe_] = d
                need_sig.add(d)
                final.append(("eng", d))
            op.deps = final
        cnt = {e: 0 for e in ENGS}
        for op in self.ops:
            if not op.dma and op.idx in need_sig:
                cnt[op.eng] += 1
                op.sig = cnt[op.eng]
        self.sig_total = cnt
        self.dma_total = qcount

    def emit(self, barrier=False):
        nc = self.nc
        self.analyze()
        allsem = []

        Prog._phase_id += 1
        pid = Prog._phase_id

        def newsem(name):
            h = nc.alloc_semaphore(f"{name}_ph{pid}")
            allsem.append(h)
            return h

        if True:
            esem = {}
            for e in ENGS:
                n_ep = self.sig_total[e] // SIG_EPOCH + 1
                esem[e] = [newsem(f"s_{e}_{i}") for i in range(n_ep)]
            dsem = {}
            for e in ENGS:
                if self.dma_total[e]:
                    dsem[e] = [newsem(f"d_{e}_{i}") for i in range(self.NDSEM)]
            dsem["cc"] = {op.idx: newsem(f"cc_{op.idx}") for op in self.ops if op.dma == "cc"}

            def waitspec(dep):
                kind, d = dep
                p = self.ops[d]
                if kind == "dma":
                    q, si, val = p.dticket
                    return dsem[q][si], val
                k = p.sig - 1
                return esem[p.eng][k // SIG_EPOCH], k % SIG_EPOCH + 1

            def run(engname):
                def body(eng):
                    last_dma = {}
                    for op in self.ops:
                        if op.eng != engname:
                            continue
                        ws = [waitspec(d) for d in op.deps]
                        for (sem, val) in ws[1:]:
                            eng.wait_ge(sem, val)
                        ins = op.fn(eng)
                        if ws:
                            ins._wait_ge(ws[0][0], ws[0][1])
                        if op.dma == "cc":
                            ins.then_inc(dsem["cc"][op.idx])
                            eng.wait_ge(dsem["cc"][op.idx], 1)
                        elif op.dma:
                            q, si, val = op.dticket
                            ins.then_inc(dsem[q][si], 16)
                            last_dma[si] = val
                        elif op.sig is not None:
                            k = op.sig - 1
                            ins.then_inc(esem[engname][k // SIG_EPOCH], 1)
                    for si, val in last_dma.items():
                        eng.wait_ge(dsem[engname][si], val)
                return body

            with nc.Block() as block:
                block.tensor(run("pe"))
                block.scalar(run("act"))
                block.vector(run("dve"))
                block.gpsimd(run("pool"))
                block.sync(run("sp"))
        if barrier:
            nc.all_engine_barrier()
            nc.clear_and_free_semaphores(allsem)
            nc.all_engine_barrier()
        else:
            for h in allsem:
                nc.release_semaphore(h)


class KB:
    def __init__(self):
        self.nc = bass.Bass("TRN2", target_bir_lowering=False)
        self.p = Prog(self.nc)
        self.es = contextlib.ExitStack()
        self.dram = {}
        self._uid = 0
        self.es0 = contextlib.ExitStack()
        self.psf = [self.es0.enter_context(self.nc.psum_tensor(f"psb{i}", [128, 512], F32)) for i in range(8)]

    def uid(self, s):
        self._uid += 1
        return f"{s}_{self._uid}"

    def din(self, name, shape, dt=F32):
        t = self.nc.dram_tensor(name, list(shape), dt, kind="ExternalInput").ap()
        self.dram[name] = t
        return t

    def dout(self, name, shape, dt=F32, kind="ExternalOutput"):
        t = self.nc.dram_tensor(name, list(shape), dt, kind=kind).ap()
        self.dram[name] = t
        return t

    def sb(self, name, shape, dt=F32):
        return self.es.enter_context(self.nc.sbuf_tensor(self.uid(name), list(shape), dt))

    def dscratch(self, name, shape, dt=F32):
        t = self.nc.dram_tensor(name, list(shape), dt, kind="Internal").ap()
        self.dram[name] = t
        return t

    def end_phase(self):
        self.p.emit(barrier=True)
        self.es.close()
        self.es = contextlib.ExitStack()
        self.p = Prog(self.nc)

    def ps(self, i):
        return self.psf[i]

    def finish(self):
        self.p.emit()
        self.es.close()
        self.es0.close()
        return self.nc


C_SBQ, C_SBK, C_SBV = 0, 256, 512
C_MOQ, C_MOK, C_MOV = 768, 1024, 1280
C_CQ, C_CKV, C_KR = 1536, 1792, 1920
C_NSQ, C_NSKV, C_NSG, C_MEQ = 1952, 2208, 2592, 2604


def consts_common(kb):
    nc, p = kb.nc, kb.p
    c = {}
    c["identf"] = kb.sb("identf", [128, 128], F32)
    c["identb"] = kb.sb("identb", [128, 128], BF16)
    c["onesb"] = kb.sb("onesb", [128, 128], BF16)
    c["onesf"] = kb.sb("onesf", [128, 128], F32)
    idf, idb = c["identf"], c["identb"]
    p.pool(lambda e: e.memset(c["onesf"][:], 1.0), writes=["onesf"])
    p.pool(lambda e: e.memset(c["onesb"][:], 1.0), writes=["onesb"])
    p.pool(lambda e: e.affine_select(out=idf[:], in_=c["onesf"][:], pattern=[[-1, 128]], compare_op=ALU.is_equal,
                                     fill=0.0, base=0, channel_multiplier=1), reads=["onesf"], writes=["identf"])
    p.pool(lambda e: e.tensor_copy(out=idb[:], in_=idf[:]), reads=["identf"], writes=["identb"])
    return c


def tm_view(ap2d, p=128):
    return ap2d.rearrange("(n p) c -> p n c", p=p)


def phase_a(kb, c, io, w):
    nc, p = kb.nc, kb.p
    identf, onesb = c["identf"], c["onesb"]

    hT = kb.sb("hT", [128, 8, TOK], BF16)
    win = kb.sb("win", [128, 8, IN_TOTAL], BF16)
    wkrot = kb.sb("wkrot", [128, 8, 32], BF16)
    for f in range(8):
        p.dma(win[:, f, :], w["w_in"][f * 128:(f + 1) * 128, :], writes=[("win", f)], q="pool")
    for f in range(8):
        p.dve(lambda e, f=f: e.tensor_scalar_mul(out=wkrot[:, f, 0:16], in0=win[:, f, C_KR + 16:C_KR + 32], scalar1=-1.0),
              reads=[("win", f)], writes=[("wkrot", f)])
        p.dve(lambda e, f=f: e.tensor_copy(out=wkrot[:, f, 16:32], in_=win[:, f, C_KR:C_KR + 16]),
              reads=[("win", f)], writes=[("wkrot", f)])
    wuq_f = kb.sb("wuq_f", [128, 2, 384], F32)
    wuq = kb.sb("wuq", [128, 2, 384], BF16)
    wuqr = kb.sb("wuqr", [128, 2, 384], BF16)
    gcq = kb.sb("gcq", [128, 2], F32)
    wukv_f = kb.sb("wukv_f", [128, 512], F32)
    wukv = kb.sb("wukv", [128, 512], BF16)
    gckv = kb.sb("gckv", [128, 1], F32)
    p.dma(wuq_f[:], w["w_uq"].rearrange("(n p) c -> p n c", p=128), writes=["wuq_f"])
    p.dma(gcq[:], w["g_cq"].rearrange("(n p) -> p n", p=128), writes=["gcq"], slow=True)
    p.dma(wukv_f[:], w["w_ukv"], writes=["wukv_f"])
    p.dma(gckv[:], w["g_ckv"].rearrange("(n p) -> p n", p=128), writes=["gckv"], slow=True)
    for rc in range(2):
        p.dve(lambda e, rc=rc: e.tensor_scalar_mul(out=wuq[:, rc, :], in0=wuq_f[:, rc, :], scalar1=gcq[:, rc:rc + 1]),
              reads=["wuq_f", "gcq"], writes=["wuq"])
    p.dve(lambda e: e.memset(wuqr[:], 0.0), writes=["wuqr"])
    for rc in range(2):
        for h in range(4):
            b0 = h * 96
            p.dve(lambda e, rc=rc, b0=b0: e.tensor_scalar_mul(out=wuqr[:, rc, b0 + 64:b0 + 80], in0=wuq[:, rc, b0 + 80:b0 + 96], scalar1=-1.0),
                  reads=["wuq"], writes=["wuqr"])
            p.dve(lambda e, rc=rc, b0=b0: e.tensor_copy(out=wuqr[:, rc, b0 + 80:b0 + 96], in_=wuq[:, rc, b0 + 64:b0 + 80]),
                  reads=["wuq"], writes=["wuqr"])
    p.dve(lambda e: e.tensor_scalar_mul(out=wukv[:], in0=wukv_f[:], scalar1=gckv[:, 0:1]), reads=["wukv_f", "gckv"], writes=["wukv"])

    hst = [kb.sb("hst", [128, 1024], F32) for _ in range(2)]
    for ck in range(TOK // 128):
        st = hst[ck % 2]
        tk = ("hst", ck % 2)
        p.dma(st[:], io["h_tok"][ck * 128:(ck + 1) * 128, :], writes=[tk])
        for half in range(2):
            bank = (ck * 2 + half) % 2
            ps = kb.ps(bank)
            for j in range(4):
                f = half * 4 + j
                p.pe(lambda e, ps=ps, st=st, f=f, j=j: e.transpose(out=ps[:, j * 128:(j + 1) * 128], in_=st[:, f * 128:(f + 1) * 128], identity=identf[:]),
                     reads=[tk, "identf"], writes=[("ps", bank)])
            dst = hT[:, half * 4:half * 4 + 4, ck * 128:(ck + 1) * 128]
            src = ps[:].rearrange("p (j t) -> p j t", j=4)
            if half == 0:
                p.act(lambda e, dst=dst, src=src: e.copy(out=dst, in_=src), reads=[("ps", bank)], writes=[("hT", ck // 4)])
            else:
                p.dve(lambda e, dst=dst, src=src: e.tensor_copy(out=dst, in_=src), reads=[("ps", bank)], writes=[("hT", ck // 4)])
    for f in range(8):
        p.dma(io["hT_d"][f * 128:(f + 1) * 128, :], hT[:, f, :], reads=[("hT", s) for s in range(8)], writes=[("hT_d", f)])

    ostage = [kb.sb("ostg", [128, 512], BF16) for _ in range(4)]
    gstage = [kb.sb("gstg", [12, 512], F32) for _ in range(2)]
    cq_sb = [kb.sb("cq_sb", [128, 2, 512], BF16) for _ in range(2)]
    ckv_sb = [kb.sb("ckv_sb", [128, 512], BF16) for _ in range(2)]
    krr_sb = [kb.sb("krr", [32, 2, 512], F32) for _ in range(2)]
    sq_sb = [kb.sb("sq", [128, 3, 512], BF16) for _ in range(2)]
    rstd_q = [kb.sb("rstdq", [128, 512], F32) for _ in range(2)]
    rstd_kv = [kb.sb("rstdkv", [128, 512], F32) for _ in range(2)]
    rkv_tok = [kb.sb("rkvtok", [128, 4], F32) for _ in range(2)]
    ropeq = [kb.sb("ropeq", [96, 2, 512], F32) for _ in range(2)]
    ropek = [kb.sb("ropek", [32, 2, 512], F32) for _ in range(2)]
    t1 = [kb.sb("t1", [96, 512], F32) for _ in range(2)]
    t2 = [kb.sb("t2", [96, 512], F32) for _ in range(2)]
    vstage = [kb.sb("vstg", [128, 640], BF16) for _ in range(2)]
    vmst = [kb.sb("vmst", [128, 256], BF16) for _ in range(2)]
    cnt = {"o": 0, "bank": 0, "v": 0}

    def nbank():
        b = 2 + cnt["bank"] % 6
        cnt["bank"] += 1
        return b

    fm_list = [
        ("qsb_d", 0, C_SBQ, 128, 0.125), ("qsb_d", 128, C_SBQ + 128, 128, 0.125),
        ("ksb_d", 0, C_SBK, 128, 1.0), ("ksb_d", 128, C_SBK + 128, 128, 1.0),
        ("qmo_d", 0, C_MOQ, 128, 0.125), ("qmo_d", 128, C_MOQ + 128, 128, 0.125),
        ("kmo_d", 0, C_MOK, 128, 1.0), ("kmo_d", 128, C_MOK + 128, 128, 1.0),
        ("qns_d", 0, C_NSQ, 128, 0.125), ("qns_d", 128, C_NSQ + 128, 128, 0.125),
        ("kcv_d", 0, C_NSKV, 128, 1.0),
        ("ksl_d", 0, C_NSKV + 128, 64, 1.0),
        ("kwi_d", 0, C_NSKV + 256, 64, 1.0),
        ("qme_d", 0, C_MEQ, 128, 0.125), ("qme_d", 128, C_MEQ + 128, 128, 0.125),
    ]

    def proj_fm(s, col0, ncols, wsrc=None):
        b = nbank()
        ps = kb.ps(b)
        for f in range(8):
            if wsrc is None:
                lhsT = win[:, f, col0:col0 + ncols]
                rd = [("win", f)]
            else:
                lhsT = wsrc[:, f, col0:col0 + ncols]
                rd = [("wkrot", f)]
            p.pe(lambda e, ps=ps, lhsT=lhsT, f=f, s=s, ncols=ncols: e.matmul(ps[0:ncols, :], lhsT=lhsT, rhs=hT[:, f, s * 512:(s + 1) * 512],
                                                                            start=(f == 0), stop=(f == 7)),
                 reads=rd + [("hT", s)], writes=[("ps", b)])
        return b

    for s in range(NSLOT):
        tsl = slice(s * 512, (s + 1) * 512)
        for (dn, r0, col0, ncols, scale) in fm_list:
            b = proj_fm(s, col0, ncols)
            ps = kb.ps(b)
            k = cnt["o"] % 4
            cnt["o"] += 1
            og = ostage[k]
            p.act(lambda e, og=og, ps=ps, ncols=ncols, scale=scale: e.activation(out=og[0:ncols, :], in_=ps[0:ncols, :], func=AF.Copy, scale=scale),
                  reads=[("ps", b)], writes=[("ostg", k)])
            p.dma(io[dn][r0:r0 + ncols, tsl], og[0:ncols, :], reads=[("ostg", k)], writes=[(dn, s)])
        b = proj_fm(s, C_NSG, 12)
        ps = kb.ps(b)
        gs = gstage[s % 2]
        p.act(lambda e, gs=gs, ps=ps: e.activation(out=gs[:], in_=ps[0:12, :], func=AF.Sigmoid), reads=[("ps", b)], writes=[("gstg", s % 2)])
        p.dma(io["gns_d"][:, tsl], gs[:], reads=[("gstg", s % 2)], writes=[("gns_d", s)])

        d2 = s % 2
        cq, ckv, sq = cq_sb[d2], ckv_sb[d2], sq_sb[d2]
        for rc in range(2):
            b = proj_fm(s, C_CQ + rc * 128, 128)
            ps = kb.ps(b)
            p.act(lambda e, cq=cq, ps=ps, rc=rc: e.copy(out=cq[:, rc, :], in_=ps[:]), reads=[("ps", b)], writes=[("cq", d2)])
            p.act(lambda e, sq=sq, ps=ps, rc=rc: e.activation(out=sq[:, rc, :], in_=ps[:], func=AF.Square), reads=[("ps", b)], writes=[("sq", d2)])
        b = proj_fm(s, C_CKV, 128)
        ps = kb.ps(b)
        p.act(lambda e, ckv=ckv, ps=ps: e.copy(out=ckv[:], in_=ps[:]), reads=[("ps", b)], writes=[("ckv", d2)])
        p.act(lambda e, sq=sq, ps=ps: e.activation(out=sq[:, 2, :], in_=ps[:], func=AF.Square), reads=[("ps", b)], writes=[("sq", d2)])
        krr = krr_sb[d2]
        b = proj_fm(s, C_KR, 32)
        ps = kb.ps(b)
        p.act(lambda e, krr=krr, ps=ps: e.copy(out=krr[:, 0, :], in_=ps[0:32, :]), reads=[("ps", b)], writes=[("krr", d2)])
        b = proj_fm(s, 0, 32, wsrc=wkrot)
        ps = kb.ps(b)
        p.act(lambda e, krr=krr, ps=ps: e.copy(out=krr[:, 1, :], in_=ps[0:32, :]), reads=[("ps", b)], writes=[("krr", d2)])
        rq, rkv = rstd_q[d2], rstd_kv[d2]
        b = nbank()
        ps = kb.ps(b)
        for rc in range(2):
            p.pe(lambda e, ps=ps, sq=sq, rc=rc: e.matmul(ps[:], lhsT=onesb[:], rhs=sq[:, rc, :], start=(rc == 0), stop=(rc == 1)),
                 reads=[("sq", d2), "onesb"], writes=[("ps", b)])
        p.act(lambda e, rq=rq, ps=ps: e.activation(out=rq[:], in_=ps[:], func=AF.Ln, scale=1.0 / 256.0, bias=c["eps_rms"][:, 0:1]),
              reads=[("ps", b), "cst"], writes=[("rq", d2)])
        p.act(lambda e, rq=rq: e.activation(out=rq[:], in_=rq[:], func=AF.Exp, scale=-0.5), reads=[("rq", d2)], writes=[("rq", d2)])
        b = nbank()
        ps = kb.ps(b)
        p.pe(lambda e, ps=ps, sq=sq: e.matmul(ps[:], lhsT=onesb[:], rhs=sq[:, 2, :], start=True, stop=True),
             reads=[("sq", d2), "onesb"], writes=[("ps", b)])
        p.act(lambda e, rkv=rkv, ps=ps: e.activation(out=rkv[:], in_=ps[:], func=AF.Ln, scale=1.0 / 128.0, bias=c["eps_rms"][:, 0:1]),
              reads=[("ps", b), "cst"], writes=[("rkv", d2)])
        p.act(lambda e, rkv=rkv: e.activation(out=rkv[:], in_=rkv[:], func=AF.Exp, scale=-0.5), reads=[("rkv", d2)], writes=[("rkv", d2)])
        rkt = rkv_tok[d2]
        b = nbank()
        ps = kb.ps(b)
        for ck in range(4):
            p.pe(lambda e, ps=ps, sq=sq, ck=ck: e.matmul(ps[:, ck:ck + 1], lhsT=sq[:, 2, ck * 128:(ck + 1) * 128], rhs=onesb[:, 0:1], start=True, stop=True),
                 reads=[("sq", d2), "onesb"], writes=[("ps", b)])
        p.act(lambda e, rkt=rkt, ps=ps: e.activation(out=rkt[:], in_=ps[:, 0:4], func=AF.Ln, scale=1.0 / 128.0, bias=c["eps_rms"][:, 0:1]),
              reads=[("ps", b), "cst"], writes=[("rkt", d2)])
        p.act(lambda e, rkt=rkt: e.activation(out=rkt[:], in_=rkt[:], func=AF.Exp, scale=-0.5), reads=[("rkt", d2)], writes=[("rkt", d2)])
        rpq, rpk = ropeq[d2], ropek[d2]
        p.dma(rpq[:], io["ropeq_t"][s], writes=[("rpq", d2)])
        p.dma(rpk[:], io["ropek_t"][s], writes=[("rpk", d2)])
        for h in range(4):
            bA, bB = nbank(), nbank()
            psA, psB = kb.ps(bA), kb.ps(bB)
            for rc in range(2):
                p.pe(lambda e, psA=psA, cq=cq, rc=rc, h=h: e.matmul(psA[0:96, :], lhsT=wuq[:, rc, h * 96:(h + 1) * 96], rhs=cq[:, rc, :], start=(rc == 0), stop=(rc == 1)),
                     reads=["wuq", ("cq", d2)], writes=[("ps", bA)])
            for rc in range(2):
                p.pe(lambda e, psB=psB, cq=cq, rc=rc, h=h: e.matmul(psB[0:96, :], lhsT=wuqr[:, rc, h * 96:(h + 1) * 96], rhs=cq[:, rc, :], start=(rc == 0), stop=(rc == 1)),
                     reads=["wuqr", ("cq", d2)], writes=[("ps", bB)])
            a1, a2 = t1[h % 2], t2[h % 2]
            k = cnt["o"] % 4
            cnt["o"] += 1
            og = ostage[k]
            p.dve(lambda e, a1=a1, psA=psA, rpq=rpq: e.tensor_tensor(out=a1[:], in0=psA[0:96, :], in1=rpq[:, 0, :], op=ALU.mult),
                  reads=[("ps", bA), ("rpq", d2)], writes=[("t1", h % 2)])
            p.dve(lambda e, a2=a2, psB=psB, rpq=rpq: e.tensor_tensor(out=a2[:], in0=psB[0:96, :], in1=rpq[:, 1, :], op=ALU.mult),
                  reads=[("ps", bB), ("rpq", d2)], writes=[("t2", h % 2)])
            p.dve(lambda e, a1=a1, a2=a2: e.tensor_tensor(out=a1[:], in0=a1[:], in1=a2[:], op=ALU.add),
                  reads=[("t1", h % 2), ("t2", h % 2)], writes=[("t1", h % 2)])
            p.dve(lambda e, a1=a1, og=og, rq=rq: e.tensor_tensor(out=og[0:96, :], in0=a1[:], in1=rq[0:96, :], op=ALU.mult),
                  reads=[("t1", h % 2), ("rq", d2)], writes=[("ostg", k)])
            p.dma(io["qml_d"][h, :, tsl], og[0:96, :], reads=[("ostg", k)], writes=[("qml_d", s, h)])
            b = nbank()
            ps = kb.ps(b)
            p.pe(lambda e, ps=ps, ckv=ckv, h=h: e.matmul(ps[0:64, :], lhsT=wukv[:, h * 128:h * 128 + 64], rhs=ckv[:], start=True, stop=True),
                 reads=["wukv", ("ckv", d2)], writes=[("ps", b)])
            k = cnt["o"] % 4
            cnt["o"] += 1
            og = ostage[k]
            p.dve(lambda e, og=og, ps=ps, rkv=rkv: e.tensor_tensor(out=og[0:64, :], in0=ps[0:64, :], in1=rkv[0:64, :], op=ALU.mult),
                  reads=[("ps", b), ("rkv", d2)], writes=[("ostg", k)])
            p.dma(io["kml_d"][h, :, tsl], og[0:64, :], reads=[("ostg", k)], writes=[("kml_d", s, h)])
        a1, a2 = t1[0], t2[0]
        k = cnt["o"] % 4
        cnt["o"] += 1
        og = ostage[k]
        p.dve(lambda e, a1=a1, krr=krr, rpk=rpk: e.tensor_tensor(out=a1[0:32, :], in0=krr[:, 0, :], in1=rpk[:, 0, :], op=ALU.mult),
              reads=[("krr", d2), ("rpk", d2)], writes=[("t1", 0)])
        p.dve(lambda e, a2=a2, krr=krr, rpk=rpk: e.tensor_tensor(out=a2[0:32, :], in0=krr[:, 1, :], in1=rpk[:, 1, :], op=ALU.mult),
              reads=[("krr", d2), ("rpk", d2)], writes=[("t2", 0)])
        p.dve(lambda e, a1=a1, a2=a2, og=og: e.tensor_tensor(out=og[0:32, :], in0=a1[0:32, :], in1=a2[0:32, :], op=ALU.add),
              reads=[("t1", 0), ("t2", 0)], writes=[("ostg", k)])
        p.dma(io["krl_d"][:, tsl], og[0:32, :], reads=[("ostg", k)], writes=[("krl_d", s)])
        for ck in range(4):
            gck = s * 4 + ck
            b = nbank()
            ps = kb.ps(b)
            for h in range(4):
                p.pe(lambda e, ps=ps, ckv=ckv, ck=ck, h=h: e.matmul(ps[:, h * 64:(h + 1) * 64], lhsT=ckv[:, ck * 128:(ck + 1) * 128], rhs=wukv[:, h * 128 + 64:h * 128 + 128],
                                                                 start=True, stop=True),
                     reads=["wukv", ("ckv", d2)], writes=[("ps", b)])
            vm = vmst[gck % 2]
            p.act(lambda e, vm=vm, ps=ps, rkt=rkt, ck=ck: e.activation(out=vm[:], in_=ps[:, 0:256], func=AF.Copy, scale=rkt[:, ck:ck + 1]),
                  reads=[("ps", b), ("rkt", d2)], writes=[("vmst", gck % 2)])
            p.dma(io["vml_d"][gck * 128:(gck + 1) * 128, :], vm[:], reads=[("vmst", gck % 2)], writes=[("vml_d", gck)])

        for ck in range(4):
            gck = s * 4 + ck
            tcs = slice(gck * 128, (gck + 1) * 128)
            vs = vstage[gck % 2]
            b1, b2 = nbank(), nbank()
            ps1, ps2 = kb.ps(b1), kb.ps(b2)
            for f in range(8):
                p.pe(lambda e, ps1=ps1, f=f, tcs=tcs: e.matmul(ps1[:, 0:256], lhsT=hT[:, f, tcs], rhs=win[:, f, C_SBV:C_SBV + 256], start=(f == 0), stop=(f == 7)),
                     reads=[("win", f), ("hT", s)], writes=[("ps", b1)])
            for f in range(8):
                p.pe(lambda e, ps1=ps1, f=f, tcs=tcs: e.matmul(ps1[:, 256:512], lhsT=hT[:, f, tcs], rhs=win[:, f, C_MOV:C_MOV + 256], start=(f == 0), stop=(f == 7)),
                     reads=[("win", f), ("hT", s)], writes=[("ps", b1)])
            for f in range(8):
                p.pe(lambda e, ps2=ps2, f=f, tcs=tcs: e.matmul(ps2[:, 0:64], lhsT=hT[:, f, tcs], rhs=win[:, f, C_NSKV + 192:C_NSKV + 256], start=(f == 0), stop=(f == 7)),
                     reads=[("win", f), ("hT", s)], writes=[("ps", b2)])
            for f in range(8):
                p.pe(lambda e, ps2=ps2, f=f, tcs=tcs: e.matmul(ps2[:, 64:128], lhsT=hT[:, f, tcs], rhs=win[:, f, C_NSKV + 320:C_NSKV + 384], start=(f == 0), stop=(f == 7)),
                     reads=[("win", f), ("hT", s)], writes=[("ps", b2)])
            p.act(lambda e, vs=vs, ps1=ps1: e.copy(out=vs[:, 0:512], in_=ps1[:]), reads=[("ps", b1)], writes=[("vstg", gck % 2)])
            p.dve(lambda e, vs=vs, ps2=ps2: e.tensor_copy(out=vs[:, 512:640], in_=ps2[:, 0:128]), reads=[("ps", b2)], writes=[("vstg", gck % 2)])
            p.dma(io["vsb_d"][tcs, :], vs[:, 0:256], reads=[("vstg", gck % 2)], writes=[("vsb_d", gck)])
            p.dma(io["vmo_d"][tcs, :], vs[:, 256:512], reads=[("vstg", gck % 2)], writes=[("vmo_d", gck)])
            p.dma(io["vsw_d"][tcs, :], vs[:, 512:640], reads=[("vstg", gck % 2)], writes=[("vsw_d", gck)])


A_OUTS = {
    "hT_d": ([1024, TOK], BF16),
    "qsb_d": ([256, TOK], BF16), "ksb_d": ([256, TOK], BF16), "vsb_d": ([TOK, 256], BF16),
    "qmo_d": ([256, TOK], BF16), "kmo_d": ([256, TOK], BF16), "vmo_d": ([TOK, 256], BF16),
    "qml_d": ([4, 96, TOK], BF16), "kml_d": ([4, 64, TOK], BF16), "krl_d": ([32, TOK], BF16), "vml_d": ([TOK, 256], BF16),
    "qns_d": ([256, TOK], BF16), "kcv_d": ([128, TOK], BF16), "ksl_d": ([64, TOK], BF16), "kwi_d": ([64, TOK], BF16),
    "vsw_d": ([TOK, 128], BF16), "gns_d": ([12, TOK], F32), "qme_d": ([256, TOK], BF16),
}


def load_consts(kb, io):
    p = kb.p
    c = {}
    c["identf"] = kb.sb("identf", [128, 128], F32)
    c["identb"] = kb.sb("identb", [128, 128], BF16)
    c["onesb"] = kb.sb("onesb", [128, 128], BF16)
    c["onesf"] = kb.sb("onesf", [128, 128], F32)
    c["cstf"] = kb.sb("cstf", [128, 8], F32)
    p.dma(c["identf"][:], io["c_ident"], writes=["identf"])
    p.dma(c["identb"][:], io["c_ident"], writes=["identb"], q="pool")
    p.dma(c["onesf"][:], io["c_ones"], writes=["onesf"])
    p.dma(c["onesb"][:], io["c_ones"], writes=["onesb"], q="pool")
    p.dma(c["cstf"][:], io["c_cst"], writes=["cst"])
    c["eps_rms"] = c["cstf"][:, 0:1]
    c["eps_ln"] = c["cstf"][:, 1:2]
    c["tiny"] = c["cstf"][:, 2:3]
    c["zrow"] = kb.sb("zrow", [1, 8], BF16)
    p.pool(lambda e: e.memset(c["zrow"][:], 0.0), writes=["zrow"])
    return c


def host_consts():
    cst = np.zeros((128, 8), np.float32)
    cst[:, 0] = RMS_EPS
    cst[:, 1] = LN_EPS
    cst[:, 2] = 1e-30
    cst[:, 3] = 1.0
    return {"c_ident": np.eye(128, dtype=np.float32), "c_ones": np.ones((128, 128), np.float32), "c_cst": cst}


def rope_tables(r):
    half = 16
    freqs = np.power(np.float32(10000.0), -np.arange(half, dtype=np.float32) / half).astype(np.float32)
    rq = np.zeros((NSLOT, 96, 2, 512), np.float32)
    rk = np.zeros((NSLOT, 32, 2, 512), np.float32)
    sc = np.float32(96.0 ** -0.5)
    for s in range(NSLOT):
        pos = (512 * (2 * s + r) + np.arange(512)).astype(np.float32)
        ang = pos[None, :] * freqs[:, None]
        cos, sin = np.cos(ang).astype(np.float32), np.sin(ang).astype(np.float32)
        c2 = np.concatenate([cos, cos], 0)
        s2 = np.concatenate([sin, sin], 0)
        rq[s, 0:64, 0, :] = sc
        rq[s, 64:96, 0, :] = sc * c2
        rq[s, 64:96, 1, :] = sc * s2
        rk[s, :, 0, :] = c2
        rk[s, :, 1, :] = s2
    return rq, rk


def build_a():
    kb = KB()
    io = {}
    io["h_tok"] = kb.din("h_tok", [TOK, D])
    io["c_ident"] = kb.din("c_ident", [128, 128])
    io["c_ones"] = kb.din("c_ones", [128, 128])
    io["c_cst"] = kb.din("c_cst", [128, 8])
    io["ropeq_t"] = kb.din("ropeq_t", [NSLOT, 96, 2, 512])
    io["ropek_t"] = kb.din("ropek_t", [NSLOT, 32, 2, 512])
    w = {"w_in": kb.din("w_in", [D, IN_TOTAL]), "w_uq": kb.din("w_uq", [256, 384]), "g_cq": kb.din("g_cq", [256]),
         "w_ukv": kb.din("w_ukv", [128, 512]), "g_ckv": kb.din("g_ckv", [128])}
    for n, (shp, dt) in A_OUTS.items():
        io[n] = kb.dout(n, shp, dt)
    c = load_consts(kb, io)
    phase_a(kb, c, io, w)
    return kb.finish()


def bconsts_host(r):
    bf = ml_dtypes.bfloat16
    o = {}
    kl = np.arange(128)[:, None]
    ql = np.arange(512)[None, :]
    cm = np.zeros((8, 128, 512), np.float32)
    cms = np.zeros((8, 128, 512), np.float32)
    for jj in range(8):
        kp = 128 * jj + kl
        qp = 512 * r + ql
        cm[jj] = np.where(kp <= qp, 0.0, MASKV)
        cms[jj] = np.where(kp < qp, 0.0, MASKV)
    o["c_cm"] = cm.transpose(1, 0, 2).astype(bf)
    o["c_cms"] = cms.transpose(1, 0, 2).astype(bf)
    wm = np.zeros((12, 128, 512), np.float32)
    for ji, jrel in enumerate(range(-4, 8)):
        dist = 512 * r + ql - 128 * jrel - kl
        wm[ji] = np.where((dist >= 0) & (dist < 512), 0.0, MASKV)
    o["c_wm"] = wm.transpose(1, 0, 2).astype(bf)
    pm = np.zeros((3, 128, 512), np.float32)
    for ii, idx in enumerate((6, 7, 8)):
        pm[ii] = np.where(16 * kl + 31 - 512 * r - ql <= 1024 * (idx - 6), 0.0, MASKV)
    o["c_pm"] = pm.transpose(1, 0, 2).astype(bf)
    kp = np.arange(S)
    o["c_kaug"] = np.stack([kp // 128, kp % 128, np.ones(S), np.ones(S)]).astype(bf)
    ce = 16 * np.arange(512) + 31
    caug = np.stack([ce // 128, ce % 128, np.ones(512), np.ones(512)]).astype(np.float32)
    caug[0, 511] = -30000.0
    o["c_caug"] = caug.astype(bf)
    tl = np.arange(TOK)
    qp = 512 * (2 * (tl // 512) + r) + tl % 512
    qa = np.zeros((4, 4, TOK), np.float32)
    for h in range(4):
        sl = SLOPES[h]
        qa[h, 0] = 128 * sl
        qa[h, 1] = sl
        qa[h, 2] = -sl * 128 * (qp // 128)
        qa[h, 3] = -sl * (qp % 128)
    o["c_qaug"] = qa.astype(bf)
    o["c_g32"] = ((np.arange(S)[None, :] // 64) % 32 == np.arange(32)[:, None]).astype(np.float32).astype(bf)
    o["c_tm"] = (np.arange(S)[None, :] // 256 == np.arange(32)[:, None]).astype(np.float32).astype(bf)
    n = np.arange(512)[:, None]
    s_ = np.arange(128)[None, :]
    ov = ((16 * n < 64 * s_ + 64) & (16 * n + 32 > 64 * s_)).astype(np.float32)
    ov = np.concatenate([ov, np.ones((512, 1), np.float32)], 1)
    ov[511] = 0.0
    o["c_nui"] = -(np.arange(128)[:, None] >= np.arange(128)[None, :]).astype(np.float32).astype(bf)
    o["c_ov"] = ov.reshape(4, 128, 129).transpose(1, 0, 2).astype(bf)
    vb = np.zeros((8, 4, 32), np.float32)
    own_t = np.zeros((8, 4, 32), np.float32)
    for s in range(8):
        for qb in range(4):
            own = (4 * (2 * s + r) + qb) // 2
            vb[s, qb] = np.where(np.arange(32) < own, 0.0, -1e30)
            own_t[s, qb, own] = 1.0
    o["c_vb"] = np.broadcast_to(vb[None], (128, 8, 4, 32)).copy()
    o["c_own"] = np.broadcast_to(own_t[None], (128, 8, 4, 32)).copy()
    M = np.zeros((8, 128, 4, 128), np.float32)
    C = np.zeros((8, 128, 4, 128), np.float32)
    sid = np.arange(128)[None, :]
    for s in range(8):
        for qb in range(4):
            qpos = 512 * (2 * s + r) + 128 * qb + np.arange(128)[:, None]
            cur = qpos // 64
            forced_cur = sid == cur
            forced0 = (sid == 0) & ~forced_cur
            past = (sid < cur) & ~forced0 & ~forced_cur
            M[s, :, qb] = past
            C[s, :, qb] = np.where(forced_cur, 1e30, np.where(forced0, 5e29, np.where(past, 0.0, -1e30)))
    o["c_selm"] = M
    o["c_selc"] = C
    return o


B_CONST_SHAPES = {
    "c_cm": ([128, 8, 512], BF16), "c_cms": ([128, 8, 512], BF16), "c_wm": ([128, 12, 512], BF16), "c_pm": ([128, 3, 512], BF16),
    "c_kaug": ([4, S], BF16), "c_caug": ([4, 512], BF16), "c_qaug": ([4, 4, TOK], BF16),
    "c_g32": ([32, S], BF16), "c_tm": ([32, S], BF16), "c_ov": ([128, 4, 129], BF16),
    "c_nui": ([128, 128], BF16), "c_vb": ([128, 8, 4, 32], F32), "c_own": ([128, 8, 4, 32], F32),
    "c_selm": ([8, 128, 4, 128], F32), "c_selc": ([8, 128, 4, 128], F32),
}

B_INS = {
    "qsb_d": ([256, TOK], BF16), "qmo_d": ([256, TOK], BF16), "qml_d": ([4, 96, TOK], BF16), "qns_d": ([256, TOK], BF16),
    "qme_d": ([256, TOK], BF16), "gns_d": ([12, TOK], F32),
    "ksb_f": ([256, S], BF16), "vsb_f": ([S, 256], BF16), "kmo_f": ([256, S], BF16), "vmo_f": ([S, 256], BF16),
    "kml_f": ([4, 64, S], BF16), "krl_f": ([32, S], BF16), "vml_f": ([S, 256], BF16),
    "kcv_f": ([128, S], BF16), "ksl_f": ([64, S], BF16), "kwi_f": ([64, S], BF16), "vsw_f": ([S, 128], BF16),
    "mem": ([256, D], F32),
}


class AttnBufs:
    pass


class KFull:
    def __init__(self, ap):
        self.ap = ap

    def rows(self, lo, hi):
        return ("full", self.ap[lo:hi, :])


class KPair:
    def __init__(self, ap, R, base=0):
        self.ap, self.R, self.base = ap, R, base

    def rows(self, lo, hi):
        return ("pair", self.ap, self.R, self.base + lo, hi - lo)


class VFull:
    def __init__(self, ap, cbase=0):
        self.ap, self.cbase = ap, cbase

    def cols(self, c0):
        return ("full", self.ap, self.cbase + c0)


class VPair:
    def __init__(self, chunks, cbase=0):
        self.chunks, self.cbase = chunks, cbase

    def cols(self, c0):
        return ("pair", self.chunks, self.cbase + c0)


class TMChunks:
    def __init__(self, chunks, c0, c1):
        self.chunks, self.c0, self.c1 = chunks, c0, c1

    def __getitem__(self, key):
        rs, cs = key
        k = rs.start // 1024
        a = self.chunks[k][rs.start - 1024 * k:rs.stop - 1024 * k, self.c0:self.c1]
        return a[:, cs]


def phase_b(kb, c, io, w, branches=("sb", "moba", "mla", "nsa", "mem")):
    nc, p = kb.nc, kb.p
    A = AttnBufs()
    A.KT = [kb.sb("KT", [128, S], BF16) for _ in range(2)]
    A.V = [kb.sb("V", [128, 64, 65], BF16) for _ in range(2)]
    A.QT = kb.sb("QT", [128, 4, TOK], BF16)
    A.cm = kb.sb("cm", [128, 8, 512], BF16)
    A.cms = kb.sb("cms", [128, 8, 512], BF16)
    A.P = [kb.sb("P", [128, 512], BF16) for _ in range(3)]
    A.rden = [kb.sb("rden", [65, 512], F32) for _ in range(2)]
    A.bcs = [kb.sb("bcs", [64, 512], F32) for _ in range(2)]
    A.ost = [kb.sb("ost", [64, 512], BF16) for _ in range(2)]
    A.cnt = {"kt": 0, "v": 0, "P": 0, "fin": 0, "sc": 0}
    p.dma(A.cm[:], io["c_cm"], writes=["cm"])
    p.dma(A.cms[:], io["c_cms"], writes=["cms"])
    for i in range(2):
        p.pool(lambda e, i=i: e.memset(A.V[i][:, :, 64:65], 1.0), writes=[("V", i)])

    def load_K(rows_src, dk, aug=None):
        i = A.cnt["kt"] % 2
        A.cnt["kt"] += 1
        kt = A.KT[i]
        for (src, r0) in rows_src:
            if src[0] == "full":
                n = src[1].shape[0]
                p.dma(kt[r0:r0 + n, :], src[1], writes=[("KT", i)])
            else:
                _, ap, R, row0, n = src
                for rr in range(2):
                    p.dma(kt[r0:r0 + n, :].rearrange("p (s r i) -> p s r i", r=2, i=512)[:, :, rr, :],
                          ap[rr * R + row0:rr * R + row0 + n, :].rearrange("p (s i) -> p s i", i=512), writes=[("KT", i)])
        if aug is not None:
            p.dma(kt[dk:dk + 4, :], aug, writes=[("KT", i)])
        return kt, ("KT", i)

    def load_V(src, col0):
        i = A.cnt["v"] % 2
        A.cnt["v"] += 1
        v = A.V[i]
        sp_ = src.cols(col0)
        if sp_[0] == "full":
            p.dma(v[:, :, 0:64], sp_[1].rearrange("(n p) c -> p n c", p=128)[:, :, sp_[2]:sp_[2] + 64], writes=[("V", i)])
        else:
            _, chunks, cc0 = sp_
            for k, ch in enumerate(chunks):
                for rr in range(2):
                    for s2 in range(2):
                        n0 = 16 * k + 8 * s2 + 4 * rr
                        p.dma(v[:, n0:n0 + 4, 0:64],
                              ch[rr * 1024 + s2 * 512:rr * 1024 + (s2 + 1) * 512, cc0:cc0 + 64].rearrange("(q p) c -> p q c", p=128),
                              writes=[("V", i)])
        return v, ("V", i)

    def load_Q(src_rows, h, dk, aug=None):
        p.dma(A.QT[0:dk, h, :], src_rows, writes=[("QT", h)])
        if aug is not None:
            p.dma(A.QT[dk:dk + 4, h, :], aug, writes=[("QT", h)])
        return ("QT", h)

    def finalize_plain(ops_bank, dst, h, s, gate=None, acc=None, acc_tok=None, first=True, last=True):
        k = A.cnt["fin"] % 2
        A.cnt["fin"] += 1
        ps = kb.ps(ops_bank)
        rd, bcs, ost = A.rden[k], A.bcs[k], A.ost[k]
        bcb = 6 + k

        def part1():
            p.dve(lambda e: e.tensor_scalar(out=rd[64:65, :], in0=ps[64:65, :], scalar1=1e-30, scalar2=None, op0=ALU.max),
                  reads=[("ps", ops_bank)], writes=[("rden", k)])
            p.dve(lambda e: e.reciprocal(out=rd[64:65, :], in_=rd[64:65, :]), reads=[("rden", k)], writes=[("rden", k)])
            if gate is not None:
                gt, gtok, gidx = gate
                p.dve(lambda e: e.tensor_tensor(out=rd[64:65, :], in0=rd[64:65, :], in1=gt[64:65, gidx, :], op=ALU.mult),
                      reads=[("rden", k), gtok], writes=[("rden", k)])

        def part2():
            pb = kb.ps(bcb)
            p.pe(lambda e: e.matmul(pb[0:64, :], lhsT=c["onesf"][64:65, 0:64], rhs=rd[64:65, :], start=True, stop=True),
                 reads=[("rden", k), "onesf"], writes=[("ps", bcb)])
            p.act(lambda e: e.copy(out=bcs[:], in_=pb[0:64, :]), reads=[("ps", bcb)], writes=[("bcs", k)])
            if acc is None:
                p.dve(lambda e: e.tensor_tensor(out=ost[:], in0=ps[0:64, :], in1=bcs[:], op=ALU.mult),
                      reads=[("ps", ops_bank), ("bcs", k)], writes=[("ost", k)])
                p.dma(dst, ost[:], reads=[("ost", k)], writes=[("o_d", h, s, id(dst) % 997)])
            else:
                if first:
                    p.dve(lambda e: e.tensor_tensor(out=acc, in0=ps[0:64, :], in1=bcs[:], op=ALU.mult),
                          reads=[("ps", ops_bank), ("bcs", k)], writes=[acc_tok])
                else:
                    p.dve(lambda e: e.tensor_tensor(out=bcs[:], in0=ps[0:64, :], in1=bcs[:], op=ALU.mult),
                          reads=[("ps", ops_bank), ("bcs", k)], writes=[("bcs", k)])
                    p.dve(lambda e: e.tensor_tensor(out=acc, in0=acc, in1=bcs[:], op=ALU.add),
                          reads=[acc_tok, ("bcs", k)], writes=[acc_tok])
                if last:
                    p.dve(lambda e: e.tensor_copy(out=ost[:], in_=acc), reads=[acc_tok], writes=[("ost", k)])
                    p.dma(dst, ost[:], reads=[("ost", k)], writes=[("o_d", h, s, id(dst) % 997)])
        return part1, part2

    def run_softmax(items, KT, ktok, dk, V, vtok, qh, qtok, pending, hook=None):
        n = len(items)

        def stage1(i):
            it = items[i]
            if "pre" in it:
                it["pre"]()
            b = i % 2
            ps = kb.ps(b)
            ex = it["extras"]
            s, j = it["s"], it["j"]
            qr = it["qrhs"] if "qrhs" in it else A.QT[0:dk, qh, s * 512:(s + 1) * 512]
            p.pe(lambda e: e.matmul(ps[:], lhsT=KT[0:dk, j * 128:(j + 1) * 128], rhs=qr,
                                    start=True, stop=(len(ex) == 0)),
                 reads=[ktok, qtok] + list(it.get("qreads", ())), writes=[("ps", b)])
            for xi, (lh, rh, toks) in enumerate(ex):
                p.pe(lambda e, lh=lh, rh=rh, xi=xi: e.matmul(ps[:], lhsT=lh, rhs=rh, start=False, stop=(xi == len(ex) - 1)),
                     reads=list(toks), writes=[("ps", b)])
            if "pdst" in it:
                P, ptok = it["pdst"]
            else:
                pk = A.cnt["P"] % 3
                A.cnt["P"] += 1
                P, ptok = A.P[pk][:], ("P", pk)
            it["P"], it["ptok"] = P, ptok
            p.act(lambda e: e.activation(out=P, in_=ps[:], func=AF.Exp), reads=[("ps", b)], writes=[ptok])

        def stage2(i):
            it = items[i]
            P, ptok = it["P"], it["ptok"]
            ob = it["obank"]
            po = kb.ps(ob)
            jv = it.get("jv", it["j"])
            p.pe(lambda e: e.matmul(po[0:65, :], lhsT=V[:, jv, 0:65], rhs=P, start=it["first"], stop=it["last"]),
                 reads=[vtok, ptok], writes=[("ps", ob)])
            if it["last"]:
                p1, p2 = it["fin"]
                p1()
                pending.append([2, p2])

        for i in range(n + 1):
            if i < n:
                stage1(i)
            if i >= 1:
                stage2(i - 1)
            for pd in list(pending):
                pd[0] -= 1
                if pd[0] <= 0:
                    pd[1]()
                    pending.remove(pd)

    def flush(pending):
        for pd in pending:
            pd[1]()
        pending.clear()

    A.load_K, A.load_V, A.load_Q = load_K, load_V, load_Q
    A.finalize_plain, A.run_softmax, A.flush = finalize_plain, run_softmax, flush
    oT = io["oT_d"]

    if "mla" in branches:
        pending = []
        for h in range(4):
            KT, ktok = load_K([(io["kml_f"].rows(h * 64, h * 64 + 64), 0), (io["krl_f"].rows(0, 32), 64)], 96)
            V, vtok = load_V(io["vml_f"], h * 64)
            qtok = load_Q(io["qml_d"][h], h, 96)
            items = []
            for s in range(NSLOT):
                nkb = 8 * s + 8
                ob = 4 + (s % 2)
                for j in range(nkb):
                    jj = j - 8 * s
                    ex = []
                    if jj >= 0:
                        ex.append((c["identb"][:], A.cm[:, jj, :], ["identb", "cm"]))
                    it = dict(j=j, s=s, extras=ex, first=(j == 0), last=(j == nkb - 1), obank=ob)
                    if j == nkb - 1:
                        it["fin"] = finalize_plain(ob, oT[2, h * 64:(h + 1) * 64, s * 512:(s + 1) * 512], h, s)
                    items.append(it)
            run_softmax(items, KT, ktok, 96, V, vtok, h, qtok, pending)
        flush(pending)

    if "mem" in branches:
        pending = []
        memst = kb.sb("memst", [128, 2, 1024], F32)
        memT = kb.sb("memT", [128, 8, 256], BF16)
        wmk = kb.sb("wmk", [128, 8, 512], BF16)
        KTm = kb.sb("KTm", [64, 4, 256], BF16)
        Vm = kb.sb("Vm", [128, 2, 4, 65], BF16)
        p.dma(memst[:], io["mem"].rearrange("(n p) c -> p n c", p=128), writes=["memst"])
        p.dma(wmk[:], w["w_mem_kv"].rearrange("(f p) c -> p f c", p=128), writes=["wmk"], q="pool")
        p.pool(lambda e: e.memset(Vm[:, :, :, 64:65], 1.0), writes=["Vm"])
        for kc in range(2):
            for half in range(2):
                ps = kb.ps(7)
                for jx in range(4):
                    f = half * 4 + jx
                    p.pe(lambda e, ps=ps, kc=kc, f=f, jx=jx: e.transpose(out=ps[:, jx * 128:(jx + 1) * 128], in_=memst[:, kc, f * 128:(f + 1) * 128], identity=c["identf"][:]),
                         reads=["memst", "identf"], writes=[("ps", 7)])
                p.dve(lambda e, ps=ps, kc=kc, half=half: e.tensor_copy(out=memT[:, half * 4:half * 4 + 4, kc * 128:(kc + 1) * 128], in_=ps[:].rearrange("p (j t) -> p j t", j=4)),
                      reads=[("ps", 7)], writes=["memT"])
        for h in range(4):
            ps = kb.ps(7)
            for f in range(8):
                p.pe(lambda e, ps=ps, f=f, h=h: e.matmul(ps[0:64, 0:256], lhsT=wmk[:, f, h * 64:(h + 1) * 64], rhs=memT[:, f, :], start=(f == 0), stop=(f == 7)),
                     reads=["wmk", "memT"], writes=[("ps", 7)])
            p.dve(lambda e, ps=ps, h=h: e.tensor_copy(out=KTm[:, h, :], in_=ps[0:64, 0:256]), reads=[("ps", 7)], writes=["KTm"])
        for kc in range(2):
            ps = kb.ps(7)
            for f in range(8):
                p.pe(lambda e, ps=ps, f=f, kc=kc: e.matmul(ps[:, 0:256], lhsT=memT[:, f, kc * 128:(kc + 1) * 128], rhs=wmk[:, f, 256:512], start=(f == 0), stop=(f == 7)),
                     reads=["wmk", "memT"], writes=[("ps", 7)])
            p.dve(lambda e, ps=ps, kc=kc: e.tensor_copy(out=Vm[:, kc, :, 0:64], in_=ps[:, 0:256].rearrange("p (h d) -> p h d", h=4)),
                  reads=[("ps", 7)], writes=["Vm"])
        for h in range(4):
            qtok = load_Q(io["qme_d"][h * 64:(h + 1) * 64, :], h, 64)
            items = []
            for s in range(NSLOT):
                ob = 4 + (s % 2)
                for j in range(2):
                    it = dict(j=j, s=s, extras=[], first=(j == 0), last=(j == 1), obank=ob)
                    if j == 1:
                        it["fin"] = finalize_plain(ob, oT[4, h * 64:(h + 1) * 64, s * 512:(s + 1) * 512], h, s)
                    items.append(it)
            run_softmax(items, KTm[:, h, :], "KTm", 64, Vm[:, :, h, :], "Vm", h, qtok, pending)
        flush(pending)

    if "moba" in branches:
        pending = []
        vbt = kb.sb("vbt", [128, 8, 4, 32], F32)
        ownt = kb.sb("ownt", [128, 8, 4, 32], F32)
        kmf = kb.sb("kmf", [64, 32], F32)
        kmb = kb.sb("kmb", [64, 32], BF16)
        gsv = kb.sb("gsv", [128, 4, 32], F32)
        m8 = kb.sb("m8", [128, 4, 8], F32)
        m1p = kb.sb("m1p", [128, 4, 128], F32)
        m1 = m1p[:, :, 64:96]
        m2 = kb.sb("m2", [128, 4, 32], F32)
        p.pool(lambda e: e.memset(m1p[:], 0.0), writes=["m1"])
        p.dma(vbt[:], io["c_vb"], writes=["vbt"])
        p.dma(ownt[:], io["c_own"], writes=["ownt"])
        for h in range(4):
            KT, ktok = load_K([(io["kmo_f"].rows(h * 64, h * 64 + 64), 0), (("full", io["c_tm"]), 64)], 96, aug=io["c_kaug"])
            V, vtok = load_V(io["vmo_f"], h * 64)
            qtok = ("QT", h)
            p.dma(A.QT[0:64, h, :], io["qmo_d"][h * 64:(h + 1) * 64, :], writes=[qtok])
            p.dma(A.QT[96:100, h, :], io["c_qaug"][h], writes=[qtok])
            p.dve(lambda e, KT=KT: e.tensor_reduce(out=kmf[:], in_=KT[0:64, :].rearrange("p (n k) -> p n k", k=256), axis=AX.X, op=ALU.add),
                  reads=[ktok], writes=["kmf"])
            p.dve(lambda e: e.tensor_scalar_mul(out=kmb[:], in0=kmf[:], scalar1=1.0 / 256.0), reads=["kmf"], writes=["kmb"])

            def make_pre(s, h=h, qtok=qtok):
                def pre():
                    ps = kb.ps(7)
                    for qb in range(4):
                        c0 = s * 512 + qb * 128
                        p.pe(lambda e, qb=qb, c0=c0: e.matmul(ps[:, qb * 32:(qb + 1) * 32], lhsT=A.QT[0:64, h, c0:c0 + 128], rhs=kmb[:], start=True, stop=True),
                             reads=[qtok, "kmb"], writes=[("ps", 7)])
                    p.dve(lambda e: e.tensor_tensor(out=gsv[:], in0=ps[:, 0:128].rearrange("p (a b) -> p a b", a=4), in1=vbt[:, s, :, :], op=ALU.add),
                          reads=[("ps", 7), "vbt"], writes=["gsv"])
                    for qb in range(4):
                        p.dve(lambda e, qb=qb: e.max(out=m8[:, qb, :], in_=gsv[:, qb, :]), reads=["gsv"], writes=["m8"])
                    for qb in range(4):
                        p.dve(lambda e, qb=qb: e.tensor_scalar(out=m1[:, qb, :], in0=gsv[:, qb, :], scalar1=m8[:, qb, 2:3], scalar2=None, op0=ALU.is_ge),
                              reads=["gsv", "m8"], writes=["m1"])
                    p.dve(lambda e: e.tensor_scalar(out=m2[:], in0=gsv[:], scalar1=-1e29, scalar2=None, op0=ALU.is_gt), reads=["gsv"], writes=["m2"])
                    p.dve(lambda e: e.tensor_tensor(out=m1, in0=m1, in1=m2[:], op=ALU.mult), reads=["m1", "m2"], writes=["m1"])
                    p.dve(lambda e: e.tensor_tensor(out=m1, in0=m1, in1=ownt[:, s, :, :], op=ALU.add), reads=["m1", "ownt"], writes=["m1"])
                    p.dve(lambda e: e.tensor_scalar(out=m1, in0=m1, scalar1=1.0, scalar2=-MASKV, op0=ALU.subtract, op1=ALU.mult),
                          reads=["m1"], writes=["m1"])
                    ps2 = kb.ps(7)
                    for qb in range(4):
                        p.pe(lambda e, qb=qb: e.transpose(out=ps2[:, qb * 128:(qb + 1) * 128], in_=m1p[:, qb, :], identity=c["identf"][:]),
                             reads=["m1", "identf"], writes=[("ps", 7)])
                    p.act(lambda e: e.copy(out=A.QT[64:96, h, s * 512:(s + 1) * 512], in_=ps2[64:96, :]), reads=[("ps", 7)], writes=[("QTs", h, s)])
                return pre

            items = []
            for s in range(NSLOT):
                nkb = 8 * s + 8
                ob = 4 + (s % 2)
                for j in range(nkb):
                    jj = j - 8 * s
                    ex = []
                    if jj >= 0:
                        ex.append((c["identb"][:], A.cm[:, jj, :], ["identb", "cm"]))
                    it = dict(j=j, s=s, extras=ex, first=(j == 0), last=(j == nkb - 1), obank=ob, qreads=[("QTs", h, s)])
                    if j == 0:
                        it["pre"] = make_pre(s)
                    if j == nkb - 1:
                        it["fin"] = finalize_plain(ob, oT[1, h * 64:(h + 1) * 64, s * 512:(s + 1) * 512], h, s)
                    items.append(it)
            run_softmax(items, KT, ktok, 100, V, vtok, h, qtok, pending)
        flush(pending)

    if "sb" in branches:
        nui = kb.sb("nui", [128, 128], BF16)
        negone = kb.sb("negone", [1, 128], BF16)
        p.dma(nui[:], io["c_nui"], writes=["nui"])
        p.pool(lambda e: e.memset(negone[:], -1.0), writes=["negone"])
        E = [kb.sb("E", [128, 512], F32) for _ in range(2)]
        SP = [kb.sb("SP", [128, 512], BF16) for _ in range(2)]
        AB = [kb.sb("AB", [128, 512], BF16) for _ in range(2)]
        carf = [kb.sb("carf", [1, 512], F32) for _ in range(2)]
        carb = [kb.sb("carb", [1, 512], BF16) for _ in range(2)]
        sbo = [kb.sb("sbo", [64, 512], BF16) for _ in range(2)]
        one_ap = c["cstf"][:, 3:4]
        for hp in range(2):
            hs = (2 * hp, 2 * hp + 1)
            KTs, ktoks, Vs, vtoks, qtoks = [], [], [], [], []
            for h in hs:
                KT, ktok = load_K([(io["ksb_f"].rows(h * 64, h * 64 + 64), 0)], 64)
                V, vtok = load_V(io["vsb_f"], h * 64)
                qtok = load_Q(io["qsb_d"][h * 64:(h + 1) * 64, :], h, 64)
                KTs.append(KT); ktoks.append(ktok); Vs.append(V); vtoks.append(vtok); qtoks.append(qtok)
            merged = []
            for s_ in range(NSLOT):
                nkb = 8 * s_ + 8
                for j in range(nkb - 1, -1, -1):
                    for st in range(2):
                        merged.append(dict(j=j, s=s_, st=st, h=hs[st], first=(j == nkb - 1), last=(j == 0)))
            for i, it in enumerate(merged):
                it["i"] = i

            def qk(it, bank, more):
                ps = kb.ps(bank)
                j, s_, st, h = it["j"], it["s"], it["st"], it["h"]
                jj = j - 8 * s_
                KT = KTs[st]
                p.pe(lambda e: e.matmul(ps[:], lhsT=KT[0:64, j * 128:(j + 1) * 128], rhs=A.QT[0:64, h, s_ * 512:(s_ + 1) * 512],
                                        start=True, stop=(jj < 0 and not more)),
                     reads=[ktoks[st], qtoks[st]], writes=[("ps", bank)])
                if jj >= 0:
                    p.pe(lambda e: e.matmul(ps[:], lhsT=c["identb"][:], rhs=A.cms[:, jj, :], start=False, stop=(not more)),
                         reads=["identb", "cms"], writes=[("ps", bank)])
                return ps

            def s1(it):
                k = it["i"] % 2
                b4 = it["i"] % 4
                ps = qk(it, b4, False)
                p.act(lambda e: e.activation(out=E[k][:], in_=ps[:], func=AF.Exp), reads=[("ps", b4)], writes=[("E", k)])
                p.act(lambda e: e.activation(out=SP[k][:], in_=E[k][:], func=AF.Ln, bias=one_ap), reads=[("E", k), "cst"], writes=[("SP", k)])

            def s2a(it):
                k = it["i"] % 2
                b4 = it["i"] % 4
                st = it["st"]
                ps = kb.ps(b4)
                fin_carry = not it["first"]
                p.pe(lambda e: e.matmul(ps[:], lhsT=nui[:], rhs=SP[k][:], start=False, stop=(not fin_carry)),
                     reads=["nui", ("SP", k)], writes=[("ps", b4)])
                if fin_carry:
                    p.pe(lambda e: e.matmul(ps[:], lhsT=negone[0:1, :], rhs=carb[st][0:1, :], start=False, stop=True),
                         reads=["negone", ("carb", st)], writes=[("ps", b4)])
                if not it["last"]:
                    pc = kb.ps(6 + st)
                    p.pe(lambda e: e.matmul(pc[0:1, :], lhsT=c["onesb"][:, 0:1], rhs=SP[k][:], start=True, stop=True),
                         reads=["onesb", ("SP", k)], writes=[("ps", 6 + st)])
                    if it["first"]:
                        p.dve(lambda e: e.tensor_copy(out=carf[st][:], in_=pc[0:1, :]), reads=[("ps", 6 + st)], writes=[("carf", st)])
                    else:
                        p.dve(lambda e: e.tensor_tensor(out=carf[st][:], in0=carf[st][:], in1=pc[0:1, :], op=ALU.add),
                              reads=[("ps", 6 + st), ("carf", st)], writes=[("carf", st)])
                    p.dve(lambda e: e.tensor_copy(out=carb[st][:], in_=carf[st][:]), reads=[("carf", st)], writes=[("carb", st)])
                p.act(lambda e: e.activation(out=AB[k][:], in_=ps[:], func=AF.Exp), reads=[("ps", b4)], writes=[("AB", k)])

            def s2b(it):
                k = it["i"] % 2
                st, j, s_, h = it["st"], it["j"], it["s"], it["h"]
                po = kb.ps(4 + st)
                V = Vs[st]
                p.pe(lambda e: e.matmul(po[0:64, :], lhsT=V[:, j, 0:64], rhs=AB[k][:], start=it["first"], stop=it["last"]),
                     reads=[vtoks[st], ("AB", k)], writes=[("ps", 4 + st)])
                if it["last"]:
                    p.dve(lambda e: e.tensor_copy(out=sbo[st][:], in_=po[0:64, :]), reads=[("ps", 4 + st)], writes=[("sbo", st)])
                    p.dma(oT[0, h * 64:(h + 1) * 64, s_ * 512:(s_ + 1) * 512], sbo[st][:], reads=[("sbo", st)], writes=[("o_sb", h, s_)])

            n = len(merged)
            for i in range(n + 2):
                if i < n:
                    s1(merged[i])
                if 1 <= i <= n:
                    s2a(merged[i - 1])
                if i >= 2:
                    s2b(merged[i - 2])

    if "nsa" in branches:
        pending = []
        QS = kb.sb("QS", [128, 4, 4, 512], BF16)
        OV = kb.sb("OV", [128, 4, 129], BF16)
        pm = kb.sb("pm", [128, 3, 512], BF16)
        wm = kb.sb("wm", [128, 12, 512], BF16)
        wphi = kb.sb("wphi", [128, 32, 128], BF16)
        w2 = kb.sb("w2", [128, 2, 64], BF16)
        peT = kb.sb("peT", [128, 32], BF16)
        peb = kb.sb("peb", [128, 2], F32)
        gx = kb.sb("gx", [128, 512], F32)
        gt_ = kb.sb("gtmp", [128, 512], F32)
        gact = [kb.sb("gact", [128, 512], BF16) for _ in range(2)]
        kcT = kb.sb("kcT", [68, 512], BF16)
        Vc = kb.sb("Vc", [128, 4, 65], BF16)
        psave = kb.sb("psave", [128, 4, 4, 512], BF16)
        gts = [kb.sb("gts", [65, 512], F32) for _ in range(4)]
        nacc = kb.sb("nacc", [64, 4, 512], F32)
        impacc = kb.sb("impacc", [128, 4, 128], F32)
        rdn = kb.sb("rdn", [128, 2], F32)
        selm = [kb.sb("selm", [128, 4, 128], F32) for _ in range(2)]
        selc = [kb.sb("selc", [128, 4, 128], F32) for _ in range(2)]
        scr2 = kb.sb("scr2", [128, 4, 128], F32)
        m16 = kb.sb("m16", [128, 4, 16], F32)
        selvp = kb.sb("selvp", [128, 4, 256], F32)
        selv = selvp[:, :, 64:192]
        gcnt = {"g": 0}
        p.pool(lambda e: e.memset(selvp[:], 0.0), writes=["selv"])
        p.dma(OV[:], io["c_ov"], writes=["OV"])
        p.dma(pm[:], io["c_pm"], writes=["pm"])
        p.dma(wm[:], io["c_wm"], writes=["wm"])
        p.dma(wphi[0:64, :, :], w["w_phi_k1"].rearrange("(t d) h -> d t h", d=64), writes=["wphi"], q="pool")
        p.dma(wphi[64:128, :, :], w["w_phi_v1"].rearrange("(t d) h -> d t h", d=64), writes=["wphi"], q="pool")
        p.dma(w2[:, 0, :], w["w_phi_k2"], writes=["w2"], q="pool")
        p.dma(w2[:, 1, :], w["w_phi_v2"], writes=["w2"], q="pool")
        p.dma(peT[0:64, :], w["nsa_pe"].rearrange("t d -> d t"), writes=["peT"], q="pool", slow=True)
        p.dma(peT[64:128, :], w["nsa_pe"].rearrange("t d -> d t"), writes=["peT"], q="pool", slow=True)
        kcv, kcvtok = load_K([(io["kcv_f"].rows(0, 128), 0)], 128)
        p.pool(lambda e: e.memset(kcT[:], 0.0), writes=["kcT"])
        p.pool(lambda e: e.memset(Vc[:, :, 64:65], 1.0), writes=["Vc"])
        for which in range(2):
            lo = which * 64
            pb = kb.ps(7)
            for t in range(32):
                p.pe(lambda e, t=t, lo=lo, pb=pb: e.matmul(pb[:, 0:1], lhsT=wphi[lo:lo + 64, t, :], rhs=peT[lo:lo + 64, t:t + 1], start=(t == 0), stop=(t == 31)),
                     reads=["wphi", "peT"], writes=[("ps", 7)])
            p.dve(lambda e, pb=pb, which=which: e.tensor_copy(out=peb[:, which:which + 1], in_=pb[:, 0:1]), reads=[("ps", 7)], writes=["peb"])
            ph = kb.ps(6)
            for t in range(32):
                p.pe(lambda e, t=t, lo=lo, ph=ph: e.matmul(ph[:, 0:511], lhsT=wphi[lo:lo + 64, t, :], rhs=kcv[lo:lo + 64, t:t + 16 * 510 + 1:16], start=(t == 0), stop=(t == 31)),
                     reads=["wphi", kcvtok], writes=[("ps", 6)])
            ga = gact[which]
            p.act(lambda e, ph=ph, which=which: e.activation(out=gx[:, 0:511], in_=ph[:, 0:511], func=AF.Identity, bias=peb[:, which:which + 1]),
                  reads=[("ps", 6), "peb"], writes=["gx"])
            p.dve(lambda e: e.tensor_tensor(out=gt_[:, 0:511], in0=gx[:, 0:511], in1=gx[:, 0:511], op=ALU.mult), reads=["gx"], writes=["gtmp"])
            p.dve(lambda e: e.tensor_scalar(out=gt_[:, 0:511], in0=gt_[:, 0:511], scalar1=0.044715, scalar2=1.0, op0=ALU.mult, op1=ALU.add), reads=["gtmp"], writes=["gtmp"])
            p.dve(lambda e: e.tensor_tensor(out=gt_[:, 0:511], in0=gt_[:, 0:511], in1=gx[:, 0:511], op=ALU.mult), reads=["gtmp", "gx"], writes=["gtmp"])
            p.act(lambda e: e.activation(out=gt_[:, 0:511], in_=gt_[:, 0:511], func=AF.Tanh, scale=0.7978845608028654), reads=["gtmp"], writes=["gtmp"])
            p.dve(lambda e: e.tensor_scalar(out=gt_[:, 0:511], in0=gt_[:, 0:511], scalar1=1.0, scalar2=0.5, op0=ALU.add, op1=ALU.mult), reads=["gtmp"], writes=["gtmp"])
            p.pool(lambda e, ga=ga: e.memset(ga[:], 0.0), writes=[("gact", which)])
            p.dve(lambda e, ga=ga: e.tensor_tensor(out=ga[:, 0:511], in0=gt_[:, 0:511], in1=gx[:, 0:511], op=ALU.mult), reads=["gtmp", "gx"], writes=[("gact", which)])
        pk_ = kb.ps(7)
        p.pe(lambda e: e.matmul(pk_[0:64, :], lhsT=w2[:, 0, :], rhs=gact[0][:], start=True, stop=True), reads=["w2", ("gact", 0)], writes=[("ps", 7)])
        p.dve(lambda e: e.tensor_copy(out=kcT[0:64, 0:511], in_=pk_[0:64, 0:511]), reads=[("ps", 7)], writes=["kcT"])
        p.dma(kcT[64:68, :], io["c_caug"], writes=["kcT"])
        pv_ = kb.ps(6)
        for cc in range(4):
            p.pe(lambda e, cc=cc: e.matmul(pv_[:, cc * 64:(cc + 1) * 64], lhsT=gact[1][:, cc * 128:(cc + 1) * 128], rhs=w2[:, 1, :], start=True, stop=True),
                 reads=["w2", ("gact", 1)], writes=[("ps", 6)])
        p.dve(lambda e: e.tensor_copy(out=Vc[:, :, 0:64], in_=pv_[:, 0:256].rearrange("p (c d) -> p c d", c=4)), reads=[("ps", 6)], writes=["Vc"])

        KsT, kstok = load_K([(io["ksl_f"].rows(0, 64), 0), (("full", io["c_g32"]), 64)], 96, aug=io["c_kaug"])
        KwT, kwtok = load_K([(io["kwi_f"].rows(0, 64), 0)], 64, aug=io["c_kaug"])
        Vs, vstok = load_V(io["vsw_f"], 0)
        Vw, vwtok = load_V(io["vsw_f"], 64)
        qtoks = [load_Q(io["qns_d"][h * 64:(h + 1) * 64, :], h, 64, aug=io["c_qaug"][h]) for h in range(4)]

        def gate_row(h, br, s):
            k = gcnt["g"] % 4
            gcnt["g"] += 1
            g = gts[k]
            p.dma(g[64:65, :], io["gns_d"][3 * h + br:3 * h + br + 1, s * 512:(s + 1) * 512], writes=[("gts", k)])
            return (g[:].rearrange("p (o n) -> p o n", o=1), ("gts", k), 0)

        for s in range(NSLOT):
            sl = slice(s * 512, (s + 1) * 512)
            ncmp = s // 2 + 1
            dst = lambda h: oT[3, h * 64:(h + 1) * 64, sl]
            p.dma(selm[s % 2][:], io["c_selm"][s], writes=[("selm", s % 2)])
            p.dma(selc[s % 2][:], io["c_selc"][s], writes=[("selc", s % 2)])
            for h in range(4):
                items = []
                for cc in range(ncmp):
                    idx = s - 2 * cc + 6
                    ex = []
                    if idx <= 8:
                        ex.append((c["identb"][:], pm[:, idx - 6, :], ["identb", "pm"]))
                    it = dict(j=cc, s=s, extras=ex, first=(cc == 0), last=(cc == ncmp - 1), obank=4 + (h % 2),
                              pdst=(psave[:, h, cc, :], ("psave", h, cc)))
                    if cc == ncmp - 1:
                        it["fin"] = finalize_plain(4 + (h % 2), dst(h), h, s, gate=gate_row(h, 0, s), acc=nacc[:, h, :], acc_tok=("nacc", h), first=True, last=False)
                    items.append(it)
                run_softmax(items, kcT, "kcT", 68, Vc, "Vc", h, qtoks[h], pending)
            flush(pending)
            for qb in range(4):
                for h in range(4):
                    bk = 6 + ((qb * 4 + h) % 2)
                    pi = kb.ps(bk)
                    for cc in range(ncmp):
                        p.pe(lambda e, pi=pi, h=h, cc=cc, qb=qb: e.matmul(pi[:, 0:129], lhsT=psave[:, h, cc, qb * 128:(qb + 1) * 128], rhs=OV[:, cc, :],
                                                                          start=(cc == 0), stop=(cc == ncmp - 1)),
                             reads=[("psave", h, cc), "OV"], writes=[("ps", bk)])
                    p.dve(lambda e, pi=pi: e.tensor_scalar(out=rdn[:, 0:1], in0=pi[:, 128:129], scalar1=1e-30, scalar2=None, op0=ALU.max),
                          reads=[("ps", bk)], writes=["rdn"])
                    p.dve(lambda e: e.reciprocal(out=rdn[:, 1:2], in_=rdn[:, 0:1]), reads=["rdn"], writes=["rdn"])
                    if h == 0:
                        p.dve(lambda e, pi=pi, qb=qb: e.tensor_scalar(out=impacc[:, qb, :], in0=pi[:, 0:128], scalar1=rdn[:, 1:2], scalar2=None, op0=ALU.mult),
                              reads=[("ps", bk), "rdn"], writes=["impacc"])
                    else:
                        p.dve(lambda e, pi=pi, qb=qb: e.scalar_tensor_tensor(out=impacc[:, qb, :], in0=pi[:, 0:128], scalar=rdn[:, 1:2], in1=impacc[:, qb, :],
                                                                             op0=ALU.mult, op1=ALU.add),
                              reads=[("ps", bk), "rdn", "impacc"], writes=["impacc"])
            sm, sc_ = selm[s % 2], selc[s % 2]
            p.dve(lambda e, sm=sm: e.tensor_tensor(out=impacc[:], in0=impacc[:], in1=sm[:], op=ALU.mult), reads=["impacc", ("selm", s % 2)], writes=["impacc"])
            p.dve(lambda e, sc_=sc_: e.tensor_tensor(out=impacc[:], in0=impacc[:], in1=sc_[:], op=ALU.add), reads=["impacc", ("selc", s % 2)], writes=["impacc"])
            for qb in range(4):
                p.dve(lambda e, qb=qb: e.max(out=m16[:, qb, 0:8], in_=impacc[:, qb, :]), reads=["impacc"], writes=["m16"])
                p.dve(lambda e, qb=qb: e.match_replace(out=scr2[:, qb, :], in_to_replace=m16[:, qb, 0:8], in_values=impacc[:, qb, :], imm_value=-3.0e38),
                      reads=["impacc", "m16"], writes=["scr2"])
                p.dve(lambda e, qb=qb: e.max(out=m16[:, qb, 8:16], in_=scr2[:, qb, :]), reads=["scr2"], writes=["m16"])
                p.dve(lambda e, qb=qb: e.tensor_scalar(out=selv[:, qb, :], in0=impacc[:, qb, :], scalar1=m16[:, qb, 15:16], scalar2=None, op0=ALU.is_ge),
                      reads=["impacc", "m16"], writes=["selv"])
            p.dve(lambda e: e.tensor_scalar(out=scr2[:], in0=impacc[:], scalar1=-5e29, scalar2=None, op0=ALU.is_gt), reads=["impacc"], writes=["scr2"])
            p.dve(lambda e: e.tensor_tensor(out=selv, in0=selv, in1=scr2[:], op=ALU.mult), reads=["selv", "scr2"], writes=["selv"])
            p.dve(lambda e: e.tensor_scalar(out=selv, in0=selv, scalar1=1.0, scalar2=-MASKV, op0=ALU.subtract, op1=ALU.mult), reads=["selv"], writes=["selv"])
            for h in range(4):
                items = []
                js = [j for j in range(8 * s - 4, 8 * s + 8) if j >= 0]
                for j in js:
                    jrel = j - 8 * s
                    it = dict(j=j, s=s, extras=[(c["identb"][:], wm[:, jrel + 4, :], ["identb", "wm"])], first=(j == js[0]), last=(j == js[-1]), obank=4 + (h % 2))
                    if j == js[-1]:
                        it["fin"] = finalize_plain(4 + (h % 2), dst(h), h, s, gate=gate_row(h, 2, s), acc=nacc[:, h, :], acc_tok=("nacc", h), first=False, last=False)
                    items.append(it)
                run_softmax(items, KwT, kwtok, 68, Vw, vwtok, h, qtoks[h], pending)
            flush(pending)
            nkb = 8 * s + 8
            ngi = (nkb - 1) // 16 + 1
            for gi in range(ngi):
                pt = kb.ps(7)
                for qb in range(4):
                    p.pe(lambda e, qb=qb, gi=gi, pt=pt: e.transpose(out=pt[:, qb * 128:(qb + 1) * 128], in_=selvp[:, qb, 32 * gi:32 * gi + 128], identity=c["identf"][:]),
                         reads=["selv", "identf"], writes=[("ps", 7)])
                p.act(lambda e, gi=gi, pt=pt: e.copy(out=QS[64:96, 0, gi, :], in_=pt[64:96, :]), reads=[("ps", 7)], writes=[("QS", gi)])
                for h in range(4):
                    if h > 0:
                        p.pool(lambda e, gi=gi, h=h: e.tensor_copy(out=QS[64:96, h, gi, :], in_=QS[64:96, 0, gi, :]), reads=[("QS", gi)], writes=[("QS", gi)])
                    p.pool(lambda e, gi=gi, h=h: e.tensor_copy(out=QS[0:64, h, gi, :], in_=A.QT[0:64, h, sl]), reads=[qtoks[h]], writes=[("QS", gi)])
                    p.dma(QS[96:100, h, gi, :], io["c_qaug"][h][:, sl], writes=[("QS", gi)])
            for h in range(4):
                items = []
                for j in range(nkb):
                    jj = j - 8 * s
                    ex = []
                    if jj >= 0:
                        ex.append((c["identb"][:], A.cm[:, jj, :], ["identb", "cm"]))
                    it = dict(j=j, s=s, extras=ex, first=(j == 0), last=(j == nkb - 1), obank=4 + (h % 2),
                              qrhs=QS[0:100, h, j // 16, :], qreads=[("QS", j // 16)])
                    if j == nkb - 1:
                        it["fin"] = finalize_plain(4 + (h % 2), dst(h), h, s, gate=gate_row(h, 1, s), acc=nacc[:, h, :], acc_tok=("nacc", h), first=False, last=True)
                    items.append(it)
                run_softmax(items, KsT, kstok, 100, Vs, vstok, h, qtoks[h], pending)
            flush(pending)
    return A


B_WEIGHTS = {"w_mem_kv": [D, 512], "nsa_pe": [32, 64], "w_phi_k1": [2048, 128], "w_phi_k2": [128, 64],
             "w_phi_v1": [2048, 128], "w_phi_v2": [128, 64]}


def build_b(branches):
    kb = KB()
    io = {}
    for n in ("c_ident", "c_ones"):
        io[n] = kb.din(n, [128, 128])
    io["c_cst"] = kb.din("c_cst", [128, 8])
    for n, (shp, dt) in B_CONST_SHAPES.items():
        io[n] = kb.din(n, shp, dt)
    for n, (shp, dt) in B_INS.items():
        io[n] = kb.din(n, shp, dt)
    w = {n: kb.din(n, shp) for n, shp in B_WEIGHTS.items()}
    io["oT_d"] = kb.dout("oT_d", [5, 256, TOK], BF16)
    io["kml_f"] = KFull(io["kml_f"].rearrange("h d t -> (h d) t"))
    for n in ("ksb_f", "kmo_f", "krl_f", "kcv_f", "ksl_f", "kwi_f"):
        io[n] = KFull(io[n])
    for n in ("vsb_f", "vmo_f", "vml_f", "vsw_f"):
        io[n] = VFull(io[n])
    c = load_consts(kb, io)
    phase_b(kb, c, io, w, branches)
    return kb.finish()


def layer_norm_chunk(kb, c, v, vtok, gbc, bbc, out, otok, tmp):
    p = kb.p
    st, mv, sm = tmp["st"], tmp["mv"], tmp["sm"]
    for hf in range(2):
        p.dve(lambda e, hf=hf: e.bn_stats(out=st[:, hf, :], in_=v[:, hf * 512:(hf + 1) * 512]), reads=[vtok], writes=["lnst"])
    p.dve(lambda e: e.bn_aggr(out=mv[:], in_=st[:]), reads=["lnst"], writes=["lnmv"])
    p.act(lambda e: e.activation(out=sm[:, 0:1], in_=mv[:, 1:2], func=AF.Ln, bias=c["eps_ln"]), reads=["lnmv", "cst"], writes=["lnsm"])
    p.act(lambda e: e.activation(out=sm[:, 0:1], in_=sm[:, 0:1], func=AF.Exp, scale=-0.5), reads=["lnsm"], writes=["lnsm"])
    p.dve(lambda e: e.scalar_tensor_tensor(out=sm[:, 1:2], in0=mv[:, 0:1], scalar=-1.0, in1=sm[:, 0:1], op0=ALU.mult, op1=ALU.mult),
          reads=["lnmv", "lnsm"], writes=["lnsm2"])
    p.act(lambda e: e.activation(out=v[:], in_=v[:], func=AF.Identity, scale=sm[:, 0:1], bias=sm[:, 1:2]), reads=[vtok, "lnsm", "lnsm2"], writes=[vtok])
    p.dve(lambda e: e.tensor_tensor(out=v[:], in0=v[:], in1=gbc[:], op=ALU.mult), reads=[vtok, "lngb"], writes=[vtok])
    p.dve(lambda e: e.tensor_tensor(out=out[:], in0=v[:], in1=bbc[:], op=ALU.add), reads=[vtok, "lngb"], writes=[otok])


def phase_c1(kb, c, io, w):
    nc, p = kb.nc, kb.p
    wg = kb.sb("wg", [128, 5, 8, 1024], BF16)
    wbr = kb.sb("wbr", [128, 5, 2, 1024], BF16)
    wout = kb.sb("wout", [128, 8, 1024], BF16)
    bg = kb.sb("bg", [128, 5, 8], F32)
    gbc = kb.sb("gbc", [128, 1024], F32)
    bbc = kb.sb("bbc", [128, 1024], F32)
    wr = kb.sb("wr", [128, 8, 20], F32)
    brow = kb.sb("brow", [1, 20], F32)
    for i in range(5):
        for f in range(8):
            p.dma(wg[:, i, f, :], w["w_gate"][i, f * 128:(f + 1) * 128, :], writes=[("wg", i)], q="pool")
        p.dma(wbr[:, i, :, :], w["w_br"][i].rearrange("(j p) c -> p j c", p=128), writes=["wbr"], q="pool")
    p.dma(wout[:], w["w_out"].rearrange("(f p) c -> p f c", p=128), writes=["wout"], q="pool")
    p.dma(bg[:], w["b_gate"].rearrange("i (c p) -> p i c", p=128), writes=["bg"], slow=True)
    p.dma(gbc[:], w["ln1_g"].partition_broadcast(128), writes=["lngb"])
    p.dma(bbc[:], w["ln1_b"].partition_broadcast(128), writes=["lngb"])
    p.dma(wr[:, :, 0:4], w["w_rg"].rearrange("(f p) g -> p f g", p=128), writes=["wr"], slow=True)
    for g in range(4):
        p.dma(wr[:, :, 4 + 4 * g:8 + 4 * g], w["w_re"][g].rearrange("(f p) e -> p f e", p=128), writes=["wr"], slow=True)
    p.dma(brow[0:1, 0:4], w["b_rg"].rearrange("(o g) -> o g", o=1), writes=["brow"])
    p.dma(brow[0:1, 4:20], w["b_re"].rearrange("(o g) e -> o (g e)", o=1), writes=["brow"])

    hTt = [kb.sb("hTt", [128, 8, 512], BF16) for _ in range(2)]
    oTt = [kb.sb("oTt", [128, 5, 2, 512], BF16)] * 2
    mT = [kb.sb("mT", [128, 8, 512], BF16) for _ in range(2)]
    sg = [kb.sb("sg", [128, 512], F32) for _ in range(2)]
    acc = kb.sb("macc", [128, 512], F32)
    tmpm = kb.sb("tmpm", [128, 512], F32)
    hch = [kb.sb("hch", [128, 1024], F32)] * 2
    vch = [kb.sb("vch", [128, 1024], F32)] * 2
    h1c = [kb.sb("h1c", [128, 1024], F32) for _ in range(2)]
    h1Tf = [kb.sb("h1Tf", [128, 8, 128], F32) for _ in range(2)]
    h1Tb = [kb.sb("h1Tb", [128, 8, 128], BF16) for _ in range(2)]
    lnt = {"st": kb.sb("lnst", [128, 2, 6], F32), "mv": kb.sb("lnmv", [128, 2], F32), "sm": kb.sb("lnsm", [128, 2], F32)}
    lg = kb.sb("lg", [128, 20], F32)
    r1 = kb.sb("r1", [128, 8], F32)
    goh = kb.sb("goh", [128, 4], F32)
    el = kb.sb("el", [128, 4], F32)
    ee = kb.sb("ee", [128, 4], F32)
    ee2 = kb.sb("ee2", [128, 4], F32)
    gd = [kb.sb("gd", [128, 16], F32) for _ in range(2)]
    bk = {"n": 0}

    def nbank():
        b = bk["n"] % 6
        bk["n"] += 1
        return b

    pend = []

    def do_slot(s):
        d2 = s % 2
        tsl = slice(s * 512, (s + 1) * 512)
        ht, ot, mt = hTt[d2], oTt[d2], mT[d2]
        p.dma(ht[:], io["hT_d"].rearrange("(f p) t -> p f t", p=128)[:, :, tsl], writes=[("hTt", d2)])
        for i in range(5):
            p.dma(ot[:, i, :, :], io["oT_d"][i].rearrange("(j p) t -> p j t", p=128)[:, :, tsl], writes=["oTt"])
        for cc in range(8):
            csl = slice(cc * 128, (cc + 1) * 128)
            for i in range(5):
                bgt, bbr = nbank(), nbank()
                pg, pb = kb.ps(bgt), kb.ps(bbr)
                for f in range(8):
                    p.pe(lambda e, pg=pg, i=i, f=f, csl=csl: e.matmul(pg[:], lhsT=wg[:, i, f, csl], rhs=ht[:, f, :], start=(f == 0), stop=(f == 7)),
                         reads=[("wg", i), ("hTt", d2)], writes=[("ps", bgt)])
                for jc in range(2):
                    p.pe(lambda e, pb=pb, i=i, jc=jc, csl=csl: e.matmul(pb[:], lhsT=wbr[:, i, jc, csl], rhs=ot[:, i, jc, :], start=(jc == 0), stop=(jc == 1)),
                         reads=["wbr", "oTt"], writes=[("ps", bbr)])
                sgi = sg[i % 2]
                p.act(lambda e, sgi=sgi, pg=pg, i=i, cc=cc: e.activation(out=sgi[:], in_=pg[:], func=AF.Sigmoid, bias=bg[:, i, cc:cc + 1]),
                      reads=[("ps", bgt), "bg"], writes=[("sg", i % 2)])
                if i == 0:
                    p.dve(lambda e, sgi=sgi, pb=pb: e.tensor_tensor(out=acc[:], in0=sgi[:], in1=pb[:], op=ALU.mult),
                          reads=[("sg", i % 2), ("ps", bbr)], writes=["macc"])
                else:
                    p.dve(lambda e, sgi=sgi, pb=pb: e.tensor_tensor(out=tmpm[:], in0=sgi[:], in1=pb[:], op=ALU.mult),
                          reads=[("sg", i % 2), ("ps", bbr)], writes=["tmpm"])
                    if i < 4:
                        p.dve(lambda e: e.tensor_tensor(out=acc[:], in0=acc[:], in1=tmpm[:], op=ALU.add), reads=["macc", "tmpm"], writes=["macc"])
                    else:
                        p.dve(lambda e, cc=cc: e.tensor_tensor(out=mt[:, cc, :], in0=acc[:], in1=tmpm[:], op=ALU.add), reads=["macc", "tmpm"], writes=[("mT", d2)])
            for _ in range(2 if cc < 4 else 1):
                if pend:
                    pend.pop(0)()
        if "dbg_mt" in io and s == 0:
            p.dma(io["dbg_mt"], mt[:], reads=[("mT", d2)], writes=["dbg_mt"])
        def make_chunk(tc):
            gck = s * 4 + tc
            k2 = gck % 2
            rows = slice(gck * 128, (gck + 1) * 128)
            hc, vc, h1 = hch[k2], vch[k2], h1c[k2]
            tf, tb = h1Tf[k2], h1Tb[k2]
            gdt = gd[k2]
            rb = {}

            def stA():
                p.dma(hc[:], io["h_tok"][rows, :], writes=["hch"])
                for hf in range(2):
                    b = nbank()
                    ps = kb.ps(b)
                    for cc in range(8):
                        p.pe(lambda e, ps=ps, cc=cc, tc=tc, hf=hf: e.matmul(ps[:], lhsT=mt[:, cc, tc * 128:(tc + 1) * 128], rhs=wout[:, cc, hf * 512:(hf + 1) * 512],
                                                                          start=(cc == 0), stop=(cc == 7)),
                             reads=["wout", ("mT", d2)], writes=[("ps", b)])
                    p.dve(lambda e, ps=ps, hf=hf: e.scalar_tensor_tensor(out=vc[:, hf * 512:(hf + 1) * 512], in0=hc[:, hf * 512:(hf + 1) * 512], scalar=ALPHA, in1=ps[:],
                                                                          op0=ALU.mult, op1=ALU.add),
                          reads=["hch", ("ps", b)], writes=["vch"])
                layer_norm_chunk(kb, c, vc, "vch", gbc, bbc, h1, ("h1c", k2), lnt)
                p.dma(io["h1_d"][rows, :], h1[:], reads=[("h1c", k2)], writes=[("h1_d", gck)])

            def stB():
                for hf in range(2):
                    b = nbank()
                    ps = kb.ps(b)
                    for jx in range(4):
                        f = hf * 4 + jx
                        p.pe(lambda e, ps=ps, f=f, jx=jx: e.transpose(out=ps[:, jx * 128:(jx + 1) * 128], in_=h1[:, f * 128:(f + 1) * 128], identity=c["identf"][:]),
                             reads=[("h1c", k2), "identf"], writes=[("ps", b)])
                    p.act(lambda e, ps=ps, hf=hf: e.copy(out=tf[:, hf * 4:hf * 4 + 4, :], in_=ps[:].rearrange("p (j t) -> p j t", j=4)),
                          reads=[("ps", b)], writes=[("h1Tf", k2)])
                p.pool(lambda e: e.tensor_copy(out=tb[:], in_=tf[:]), reads=[("h1Tf", k2)], writes=[("h1Tb", k2)])
                p.dma(io["h1T_d"].rearrange("(f p) t -> p f t", p=128)[:, :, rows], tb[:], reads=[("h1Tb", k2)], writes=[("h1T_d", gck)])
                b = nbank()
                ps = kb.ps(b)
                for f in range(8):
                    p.pe(lambda e, ps=ps, f=f: e.matmul(ps[:, 0:20], lhsT=tf[:, f, :], rhs=wr[:, f, :], start=(f == 0), stop=False),
                         reads=[("h1Tf", k2), "wr"], writes=[("ps", b)])
                p.pe(lambda e, ps=ps: e.matmul(ps[:, 0:20], lhsT=c["onesf"][0:1, :], rhs=brow[0:1, :], start=False, stop=True),
                     reads=["onesf", "brow"], writes=[("ps", b)])
                p.dve(lambda e, ps=ps: e.tensor_copy(out=lg[:], in_=ps[:, 0:20]), reads=[("ps", b)], writes=["lg"])

            def stC():
                p.dve(lambda e: e.tensor_reduce(out=r1[:, 0:1], in_=lg[:, 0:4], axis=AX.X, op=ALU.max), reads=["lg"], writes=["r1a"])
                p.dve(lambda e: e.tensor_scalar(out=goh[:], in0=lg[:, 0:4], scalar1=r1[:, 0:1], scalar2=None, op0=ALU.is_equal), reads=["lg", "r1a"], writes=["goh"])
                p.dve(lambda e: e.tensor_scalar(out=ee[:], in0=lg[:, 0:4], scalar1=r1[:, 0:1], scalar2=None, op0=ALU.subtract), reads=["lg", "r1a"], writes=["ee"])
                p.act(lambda e: e.activation(out=ee[:], in_=ee[:], func=AF.Exp), reads=["ee"], writes=["ee"])
                p.dve(lambda e: e.tensor_reduce(out=r1[:, 1:2], in_=ee[:], axis=AX.X, op=ALU.add), reads=["ee"], writes=["r1b"])
                p.dve(lambda e: e.reciprocal(out=r1[:, 1:2], in_=r1[:, 1:2]), reads=["r1b"], writes=["r1b"])
                p.dve(lambda e: e.tensor_scalar(out=el[:], in0=lg[:, 4:8], scalar1=goh[:, 0:1], scalar2=None, op0=ALU.mult), reads=["lg", "goh"], writes=["el"])
                for g in range(1, 4):
                    p.dve(lambda e, g=g: e.scalar_tensor_tensor(out=el[:], in0=lg[:, 4 + 4 * g:8 + 4 * g], scalar=goh[:, g:g + 1], in1=el[:], op0=ALU.mult, op1=ALU.add),
                          reads=["lg", "goh", "el"], writes=["el"])
                p.dve(lambda e: e.tensor_reduce(out=r1[:, 2:3], in_=el[:], axis=AX.X, op=ALU.max), reads=["el"], writes=["r1c"])
                p.dve(lambda e: e.tensor_scalar(out=ee[:], in0=el[:], scalar1=r1[:, 2:3], scalar2=None, op0=ALU.subtract), reads=["el", "r1c"], writes=["ee"])
                p.act(lambda e: e.activation(out=ee[:], in_=ee[:], func=AF.Exp), reads=["ee"], writes=["ee"])
                p.dve(lambda e: e.tensor_scalar(out=ee2[:], in0=ee[:], scalar1=1.0, scalar2=-2.0, op0=ALU.is_ge, op1=ALU.mult), reads=["ee"], writes=["ee2"])
                p.dve(lambda e: e.tensor_tensor(out=ee2[:], in0=ee2[:], in1=ee[:], op=ALU.add), reads=["ee2", "ee"], writes=["ee2"])
                p.dve(lambda e: e.tensor_reduce(out=r1[:, 3:4], in_=ee2[:], axis=AX.X, op=ALU.max), reads=["ee2"], writes=["r1d"])
                p.dve(lambda e: e.tensor_scalar(out=ee2[:], in0=ee[:], scalar1=r1[:, 3:4], scalar2=None, op0=ALU.is_ge), reads=["ee", "r1d"], writes=["ee2"])
                p.dve(lambda e: e.tensor_tensor(out=ee[:], in0=ee[:], in1=ee2[:], op=ALU.mult), reads=["ee", "ee2"], writes=["ee"])
                p.dve(lambda e: e.tensor_scalar(out=r1[:, 4:5], in0=r1[:, 3:4], scalar1=1.0, scalar2=None, op0=ALU.add), reads=["r1d"], writes=["r1e"])
                p.dve(lambda e: e.reciprocal(out=r1[:, 4:5], in_=r1[:, 4:5]), reads=["r1e"], writes=["r1e"])
                p.dve(lambda e: e.tensor_tensor(out=r1[:, 4:5], in0=r1[:, 4:5], in1=r1[:, 1:2], op=ALU.mult), reads=["r1e", "r1b"], writes=["r1e"])
                p.dve(lambda e: e.tensor_scalar(out=ee[:], in0=ee[:], scalar1=r1[:, 4:5], scalar2=None, op0=ALU.mult), reads=["ee", "r1e"], writes=["ee"])
                for g in range(4):
                    p.dve(lambda e, g=g: e.tensor_scalar(out=gdt[:, 4 * g:4 * g + 4], in0=ee[:], scalar1=goh[:, g:g + 1], scalar2=None, op0=ALU.mult),
                          reads=["ee", "goh"], writes=[("gd", k2)])
                p.dma(io["gd_d"][rows, :], gdt[:], reads=[("gd", k2)], writes=[("gd_d", gck)])
            return stA, stB, stC

        for tc in range(4):
            pend.extend(make_chunk(tc))

    for s in range(NSLOT):
        do_slot(s)
    while pend:
        pend.pop(0)()

C1_W = {"w_br": [5, 256, D], "w_gate": [5, D, D], "b_gate": [5, D], "w_out": [D, D], "ln1_g": [D], "ln1_b": [D],
        "w_rg": [D, 4], "b_rg": [4], "w_re": [4, D, 4], "b_re": [4, 4]}


def build_c1(dbg=False):
    kb = KB()
    io = {}
    if dbg:
        io["dbg_mt"] = kb.dout("dbg_mt", [128, 8, 512], BF16)
    for n in ("c_ident", "c_ones"):
        io[n] = kb.din(n, [128, 128])
    io["c_cst"] = kb.din("c_cst", [128, 8])
    io["h_tok"] = kb.din("h_tok", [TOK, D])
    io["hT_d"] = kb.din("hT_d", [D, TOK], BF16)
    io["oT_d"] = kb.din("oT_d", [5, 256, TOK], BF16)
    w = {n: kb.din(n, shp) for n, shp in C1_W.items()}
    io["h1_d"] = kb.dout("h1_d", [TOK, D])
    io["h1T_d"] = kb.dout("h1T_d", [D, TOK], BF16)
    io["gd_d"] = kb.dout("gd_d", [TOK, 16])
    c = load_consts(kb, io)
    phase_c1(kb, c, io, w)
    return kb.finish()


def phase_c2(kb, c, io, w, nT=4, nE=16):
    nc, p = kb.nc, kb.p
    gbc = kb.sb("gbc2", [128, 1024], F32)
    bbc = kb.sb("bbc2", [128, 1024], F32)
    p.dma(gbc[:], w["ln2_g"].partition_broadcast(128), writes=["lngb"])
    p.dma(bbc[:], w["ln2_b"].partition_broadcast(128), writes=["lngb"])
    hT = [kb.sb("h1Tt", [128, 8, 1024], BF16) for _ in range(2)]
    gdt = [kb.sb("gdt", [128, 8, 16], F32) for _ in range(2)]
    wup = [kb.sb("wup", [128, 8, 512], BF16) for _ in range(2)]
    wdn = [kb.sb("wdn", [128, 2, 1024], BF16) for _ in range(2)]
    yacc = kb.sb("yacc", [128, 8, 1024], F32)
    gT = [kb.sb("gT", [128, 2, 1024], BF16) for _ in range(2)]
    sa = [kb.sb("sa", [128, 512], F32) for _ in range(2)]
    hch = [kb.sb("h1ch", [128, 1024], F32) for _ in range(2)]
    och = [kb.sb("och", [128, 1024], F32) for _ in range(2)]
    lnt = {"st": kb.sb("lnst2", [128, 2, 6], F32), "mv": kb.sb("lnmv2", [128, 2], F32), "sm": kb.sb("lnsm2", [128, 2], F32)}
    st = {"bank": 0, "w": 0, "sa": 0}

    def nbank():
        b = st["bank"] % 8
        st["bank"] += 1
        return b

    def do_expert(T, e, ht, httok, gd, gdtok):
        k = st["w"] % 2
        st["w"] += 1
        wu, wd, g = wup[k], wdn[k], gT[k]
        p.dma(wu[:], w["w_up"][e].rearrange("(f p) c -> p f c", p=128), writes=[("wup", k)], q="pool")
        p.dma(wd[:], w["w_down"][e].rearrange("(j p) c -> p j c", p=128), writes=[("wdn", k)], q="pool")
        for ts in range(2):
            tsl = slice(ts * 512, (ts + 1) * 512)
            for jc in range(2):
                ba, bu = nbank(), nbank()
                pa, pu = kb.ps(ba), kb.ps(bu)
                for f in range(8):
                    p.pe(lambda e_, f=f: e_.matmul(pa[:], lhsT=wu[:, f, jc * 128:(jc + 1) * 128], rhs=ht[:, f, tsl], start=(f == 0), stop=(f == 7)),
                         reads=[("wup", k), httok], writes=[("ps", ba)])
                for f in range(8):
                    p.pe(lambda e_, f=f: e_.matmul(pu[:], lhsT=wu[:, f, 256 + jc * 128:256 + (jc + 1) * 128], rhs=ht[:, f, tsl], start=(f == 0), stop=(f == 7)),
                         reads=[("wup", k), httok], writes=[("ps", bu)])
                si = st["sa"] % 2
                st["sa"] += 1
                sat = sa[si]
                p.act(lambda e_, sat=sat, pa=pa: e_.activation(out=sat[:], in_=pa[:], func=AF.Silu), reads=[("ps", ba)], writes=[("sa", si)])
                p.dve(lambda e_, sat=sat, pu=pu, jc=jc, tsl=tsl: e_.tensor_tensor(out=g[:, jc, tsl], in0=sat[:], in1=pu[:], op=ALU.mult),
                      reads=[("sa", si), ("ps", bu)], writes=[("gT", k)])
        for tc in range(8):
            for hf in range(2):
                b = nbank()
                ps = kb.ps(b)
                for jc in range(2):
                    p.pe(lambda e_, jc=jc, ps=ps, tc=tc, hf=hf: e_.matmul(ps[:], lhsT=g[:, jc, tc * 128:(tc + 1) * 128], rhs=wd[:, jc, hf * 512:(hf + 1) * 512],
                                                                       start=(jc == 0), stop=(jc == 1)),
                         reads=[("gT", k), ("wdn", k)], writes=[("ps", b)])
                ya = yacc[:, tc, hf * 512:(hf + 1) * 512]
                if e == 0:
                    p.dve(lambda e_, ps=ps, ya=ya, tc=tc: e_.tensor_scalar(out=ya, in0=ps[:], scalar1=gd[:, tc, e:e + 1], scalar2=None, op0=ALU.mult),
                          reads=[("ps", b), gdtok], writes=[("yacc", tc)])
                else:
                    p.dve(lambda e_, ps=ps, ya=ya, tc=tc: e_.scalar_tensor_tensor(out=ya, in0=ps[:], scalar=gd[:, tc, e:e + 1], in1=ya, op0=ALU.mult, op1=ALU.add),
                          reads=[("ps", b), gdtok, ("yacc", tc)], writes=[("yacc", tc)])

    def do_chunk_out(T, tc):
        gck = T * 8 + tc
        k2 = gck % 2
        rows = slice(gck * 128, (gck + 1) * 128)
        hc, oc = hch[k2], och[k2]
        p.dma(hc[:], io["h1_d"][rows, :], writes=[("h1ch", k2)])
        p.dve(lambda e_: e_.scalar_tensor_tensor(out=hc[:], in0=hc[:], scalar=ALPHA, in1=yacc[:, tc, :], op0=ALU.mult, op1=ALU.add),
              reads=[("h1ch", k2), ("yacc", tc)], writes=[("h1ch", k2)])
        layer_norm_chunk(kb, c, hc, ("h1ch", k2), gbc, bbc, oc, ("och", k2), lnt)
        p.dma(io["h_out"][rows, :], oc[:], reads=[("och", k2)], writes=[("h_out", gck)])

    def do_tile(T):
        d2 = T % 2
        ht, gd = hT[d2], gdt[d2]
        cols = slice(T * 1024, (T + 1) * 1024)
        p.dma(ht[:], io["h1T_d"].rearrange("(f p) t -> p f t", p=128)[:, :, cols], writes=[("h1Tt", d2)])
        p.dma(gd[:], io["gd_d"][T * 1024:(T + 1) * 1024, :].rearrange("(n p) e -> p n e", p=128), writes=[("gdt", d2)])
        for e in range(nE):
            do_expert(T, e, ht, ("h1Tt", d2), gd, ("gdt", d2))
        for tc in range(8):
            do_chunk_out(T, tc)

    for T in range(nT):
        do_tile(T)


C2_W = {"w_up": [16, D, 512], "w_down": [16, 256, D], "ln2_g": [D], "ln2_b": [D]}


def build_c2(nT=4, nE=16):
    kb = KB()
    io = {}
    for n in ("c_ident", "c_ones"):
        io[n] = kb.din(n, [128, 128])
    io["c_cst"] = kb.din("c_cst", [128, 8])
    io["h1_d"] = kb.din("h1_d", [TOK, D])
    io["h1T_d"] = kb.din("h1T_d", [D, TOK], BF16)
    io["gd_d"] = kb.din("gd_d", [TOK, 16])
    w = {n: kb.din(n, shp) for n, shp in C2_W.items()}
    io["h_out"] = kb.dout("h_out", [TOK, D])
    c = load_consts(kb, io)
    phase_c2(kb, c, io, w, nT, nE)
    return kb.finish()


W_SHAPES = {
    "w_in": [D, IN_TOTAL], "g_cq": [256], "g_ckv": [128], "w_uq": [256, 384], "w_ukv": [128, 512],
    "nsa_pe": [32, 64], "w_phi_k1": [2048, 128], "w_phi_k2": [128, 64], "w_phi_v1": [2048, 128], "w_phi_v2": [128, 64],
    "w_mem_kv": [D, 512], "w_br": [5, 256, D], "w_gate": [5, D, D], "b_gate": [5, D], "w_out": [D, D],
    "ln1_g": [D], "ln1_b": [D], "w_rg": [D, 4], "b_rg": [4], "w_re": [4, D, 4], "b_re": [4, 4],
    "w_up": [16, D, 512], "w_down": [16, 256, D], "ln2_g": [D], "ln2_b": [D],
}
PAIRS = [[0, 1], [2, 3], [4, 5], [6, 7]]


def build_fused(depth=DEPTH, nlw=DEPTH, stop=None):
    kb = KB()
    io = {}
    for n in ("c_ident", "c_ones"):
        io[n] = kb.din(n, [128, 128])
    io["c_cst"] = kb.din("c_cst", [128, 8])
    for n, (shp, dt) in B_CONST_SHAPES.items():
        io[n] = kb.din(n, shp, dt)
    io["ropeq_t"] = kb.din("ropeq_t", [NSLOT, 96, 2, 512])
    io["ropek_t"] = kb.din("ropek_t", [NSLOT, 32, 2, 512])
    io["x_own"] = kb.din("x_own", [TOK, D])
    io["mem"] = kb.din("mem", [256, D])
    wfull = {n: kb.din(n, [nlw] + shp) for n, shp in W_SHAPES.items()}
    io["h_final"] = kb.dout("h_final", [TOK, D])
    hbuf = [kb.dscratch(f"hbuf{i}", [TOK, D]) for i in range(2)]
    io["hT_d"] = kb.dscratch("hT_d", [D, TOK], BF16)
    for n in ("qsb_d", "qmo_d", "qns_d", "qme_d"):
        io[n] = kb.dscratch(n, [256, TOK], BF16)
    io["qml_d"] = kb.dscratch("qml_d", [4, 96, TOK], BF16)
    io["gns_d"] = kb.dscratch("gns_d", [12, TOK])
    io["oT_d"] = kb.dscratch("oT_d", [5, 256, TOK], BF16)
    io["h1_d"] = kb.dscratch("h1_d", [TOK, D])
    io["h1T_d"] = kb.dscratch("h1T_d", [D, TOK], BF16)
    io["gd_d"] = kb.dscratch("gd_d", [TOK, 16])
    xk_rows = [256, 256, 256, 256, 64]
    xk_in = [kb.dscratch(f"xk_in{k}", [r, TOK], BF16) for k, r in enumerate(xk_rows)]
    xk_out = [kb.dscratch(f"xk_out{k}", [2 * r, TOK], BF16) for k, r in enumerate(xk_rows)]
    xv_in = [kb.dscratch(f"xv_in{k}", [1024, 896], BF16) for k in range(4)]
    xv_out = [kb.dscratch(f"xv_out{k}", [2048, 896], BF16) for k in range(4)]
    io["ksb_d"], io["kmo_d"] = xk_in[0], xk_in[1]
    io["kml_d"] = xk_in[2].rearrange("(h d) t -> h d t", h=4)
    io["krl_d"], io["kcv_d"], io["ksl_d"] = xk_in[3][0:32, :], xk_in[3][32:160, :], xk_in[3][160:224, :]
    io["kwi_d"] = xk_in[4]
    io["vsb_d"], io["vmo_d"] = TMChunks(xv_in, 0, 256), TMChunks(xv_in, 256, 512)
    io["vml_d"], io["vsw_d"] = TMChunks(xv_in, 512, 768), TMChunks(xv_in, 768, 896)
    io["ksb_f"], io["kmo_f"], io["kml_f"] = KPair(xk_out[0], 256), KPair(xk_out[1], 256), KPair(xk_out[2], 256)
    io["krl_f"], io["kcv_f"], io["ksl_f"] = KPair(xk_out[3], 256, 0), KPair(xk_out[3], 256, 32), KPair(xk_out[3], 256, 160)
    io["kwi_f"] = KPair(xk_out[4], 64)
    io["vsb_f"], io["vmo_f"] = VPair(xv_out, 0), VPair(xv_out, 256)
    io["vml_f"], io["vsw_f"] = VPair(xv_out, 512), VPair(xv_out, 768)

    for l in range(depth):
        w = {n: ap[l] for n, ap in wfull.items()}
        io["h_tok"] = io["x_own"] if l == 0 else hbuf[(l - 1) % 2]
        io["h_out"] = io["h_final"] if l == depth - 1 else hbuf[l % 2]
        c = load_consts(kb, io)
        phase_a(kb, c, io, w)
        kb.end_phase()
        if stop == "A":
            break
        for k in range(5):
            kb.p.allgather(xk_out[k], xk_in[k], PAIRS, writes=[("xk", k)])
        for k in range(4):
            kb.p.allgather(xv_out[k], xv_in[k], PAIRS, writes=[("xv", k)])
        kb.end_phase()
        if stop == "AG":
            break
        c = load_consts(kb, io)
        phase_b(kb, c, io, w, ("sb", "moba", "mla", "mem"))
        kb.end_phase()
        if stop == "B1":
            break
        c = load_consts(kb, io)
        phase_b(kb, c, io, w, ("nsa",))
        kb.end_phase()
        if stop == "B2":
            break
        c = load_consts(kb, io)
        phase_c1(kb, c, io, w)
        kb.end_phase()
        if stop == "C1":
            break
        c = load_consts(kb, io)
        phase_c2(kb, c, io, w)
        kb.end_phase()
    return kb.finish()


def fused_inputs(inp, c):
    b, r = c // 2, c % 2
    m = dict(host_consts())
    m.update(bconsts_host(r))
    m["ropeq_t"], m["ropek_t"] = rope_tables(r)
    m["x_own"] = np.ascontiguousarray(inp["x"][b][_own_tokens(r)])
    m["mem"] = inp["mem"][b]
    for n in W_SHAPES:
        m[n] = inp[n]
    return m


_PROGS = {}


def _prog(name, fn):
    if name not in _PROGS:
        _PROGS[name] = fn()
    return _PROGS[name]


def _own_tokens(r):
    t = np.arange(TOK)
    return t // 512 * 1024 + r * 512 + t % 512


def _interleave_fm(a0, a1):
    sh = a0.shape[:-1]
    out = np.empty(sh + (S,), a0.dtype)
    o = out.reshape(sh + (8, 2, 512))
    o[..., 0, :] = a0.reshape(sh + (8, 512))
    o[..., 1, :] = a1.reshape(sh + (8, 512))
    return out


def _interleave_tm(a0, a1):
    C = a0.shape[1]
    out = np.empty((S, C), a0.dtype)
    o = out.reshape(8, 2, 512, C)
    o[:, 0] = a0.reshape(8, 512, C)
    o[:, 1] = a1.reshape(8, 512, C)
    return out


A_WEIGHTS = ("w_in", "w_uq", "g_cq", "w_ukv", "g_ckv")
K_FM = {"ksb_d": "ksb_f", "kmo_d": "kmo_f", "kml_d": "kml_f", "krl_d": "krl_f", "kcv_d": "kcv_f", "ksl_d": "ksl_f", "kwi_d": "kwi_f"}
V_TM = {"vsb_d": "vsb_f", "vmo_d": "vmo_f", "vml_d": "vml_f", "vsw_d": "vsw_f"}
Q_OWN = ("qsb_d", "qmo_d", "qml_d", "qns_d", "qme_d", "gns_d")


def kernel_unfused(**inputs):
    inp = {k: np.ascontiguousarray(np.asarray(v)) for k, v in inputs.items()}
    ncores = 8
    cores = list(range(ncores))
    hc = host_consts()
    bc = [bconsts_host(r) for r in range(2)]
    rp = [rope_tables(r) for r in range(2)]
    own = [_own_tokens(r) for r in range(2)]
    h = [np.ascontiguousarray(inp["x"][c // 2][own[c % 2]]) for c in cores]
    nca = _prog("a", build_a)
    ncb1 = _prog("b1", lambda: build_b(("sb", "moba", "mla", "mem")))
    ncb2 = _prog("b2", lambda: build_b(("nsa",)))
    ncc1 = _prog("c1", build_c1)
    ncc2 = _prog("c2", build_c2)
    for l in range(DEPTH):
        maps = []
        for c in cores:
            m = dict(hc)
            m["h_tok"] = h[c]
            m["ropeq_t"], m["ropek_t"] = rp[c % 2]
            for n in A_WEIGHTS:
                m[n] = inp[n][l]
            maps.append(m)
        ra = run_bass_kernel_spmd(nca, maps, core_ids=cores).results
        maps = []
        for c in cores:
            b, r = c // 2, c % 2
            m = dict(hc)
            m.update(bc[r])
            for n in Q_OWN:
                m[n] = ra[c][n]
            for kd, kf in K_FM.items():
                m[kf] = _interleave_fm(np.asarray(ra[2 * b][kd]), np.asarray(ra[2 * b + 1][kd]))
            for vd, vf in V_TM.items():
                m[vf] = _interleave_tm(np.asarray(ra[2 * b][vd]), np.asarray(ra[2 * b + 1][vd]))
            m["mem"] = inp["mem"][b]
            for n in B_WEIGHTS:
                m[n] = inp[n][l]
            maps.append(m)
        rb1 = run_bass_kernel_spmd(ncb1, maps, core_ids=cores).results
        rb2 = run_bass_kernel_spmd(ncb2, maps, core_ids=cores).results
        rb = []
        for c in cores:
            o = np.array(rb1[c]["oT_d"])
            o[3] = np.asarray(rb2[c]["oT_d"])[3]
            rb.append({"oT_d": o})
        maps = []
        for c in cores:
            m = dict(hc)
            m["h_tok"] = h[c]
            m["hT_d"] = ra[c]["hT_d"]
            m["oT_d"] = rb[c]["oT_d"]
            for n in C1_W:
                m[n] = inp[n][l]
            maps.append(m)
        rc1 = run_bass_kernel_spmd(ncc1, maps, core_ids=cores).results
        maps = []
        for c in cores:
            m = dict(hc)
            for n in ("h1_d", "h1T_d", "gd_d"):
                m[n] = rc1[c][n]
            for n in C2_W:
                m[n] = inp[n][l]
            maps.append(m)
        rc2 = run_bass_kernel_spmd(ncc2, maps, core_ids=cores).results
        h = [np.asarray(rc2[c]["h_out"]) for c in cores]
    out = np.empty((NB, S, D), np.float32)
    for c in cores:
        out[c // 2][own[c % 2]] = h[c]
    return out


def kernel(**inputs):
    inp = {k: np.ascontiguousarray(np.asarray(v)) for k, v in inputs.items()}
    cores = list(range(8))
    nc = _prog("fused", build_fused)
    maps = [fused_inputs(inp, c) for c in cores]
    res = run_bass_kernel_spmd(nc, maps, core_ids=cores).results
    out = np.empty((NB, S, D), np.float32)
    for c in cores:
        out[c // 2][_own_tokens(c % 2)] = np.asarray(res[c]["h_final"])
    return out
```

```python
import contextlib
import types
import numpy as np
import ml_dtypes
import concourse.bass as bass
import concourse.mybir as mybir
from concourse.bass_utils import run_bass_kernel_spmd

F32 = mybir.dt.float32
BF16 = mybir.dt.bfloat16
AF = mybir.ActivationFunctionType
ALU = mybir.AluOpType
AX = mybir.AxisListType

D = 1024
S = 8192
NB = 4
DEPTH = 4
TOK = 4096
NSLOT = 8
IN_TOTAL = 2860
ALPHA = (2.0 * DEPTH) ** 0.25
LN_EPS = 1e-5
RMS_EPS = 1e-6
MASKV = -30000.0
SLOPES = [2.0 ** (-2.0 * (i + 1)) for i in range(4)]

ENGS = ("pe", "act", "dve", "pool", "sp")
SIG_EPOCH = 30000


def _freeze(fn):
    if fn.__closure__ is None:
        return fn
    cells = []
    for cl in fn.__closure__:
        try:
            cells.append(types.CellType(cl.cell_contents))
        except ValueError:
            cells.append(cl)
    g = types.FunctionType(fn.__code__, fn.__globals__, fn.__name__, fn.__defaults__, tuple(cells))
    g.__kwdefaults__ = fn.__kwdefaults__
    return g


class Op:
    __slots__ = ("eng", "fn", "reads", "writes", "dma", "deps", "sig", "dticket", "idx", "dprev")

    def __init__(self, eng, fn, reads, writes, dma):
        self.eng = eng
        self.fn = fn
        self.reads = tuple(reads)
        self.writes = tuple(writes)
        self.dma = dma
        self.deps = []
        self.sig = None
        self.dticket = None
        self.dprev = None


class Prog:
    NDSEM = 12
    _phase_id = 0

    def __init__(self, nc):
        self.nc = nc
        self.ops = []

    def add(self, eng, fn, reads=(), writes=(), dma=False):
        op = Op(eng, _freeze(fn), reads, writes, dma)
        op.idx = len(self.ops)
        self.ops.append(op)
        return op

    def pe(self, fn, reads=(), writes=()):
        return self.add("pe", fn, reads, writes)

    def act(self, fn, reads=(), writes=()):
        return self.add("act", fn, reads, writes)

    def dve(self, fn, reads=(), writes=()):
        return self.add("dve", fn, reads, writes)

    def pool(self, fn, reads=(), writes=()):
        return self.add("pool", fn, reads, writes)

    def allgather(self, out, in_, groups, reads=(), writes=()):
        return self.add("pool", lambda e: e.collective_compute("AllGather", ALU.bypass, replica_groups=groups, ins=[in_.opt()], outs=[out.opt()]),
                        reads, writes, dma="cc")

    def dma(self, out, in_, reads=(), writes=(), q="sp", slow=False):
        if slow:
            return self.add(q, lambda e: e.dma_start(out=out, in_=in_, allow_slow_non_contiguous=True), reads, writes, dma=True)
        return self.add(q, lambda e: e.dma_start(out=out, in_=in_), reads, writes, dma=True)

    def analyze(self):
        last_w = {}
        readers = {}
        for op in self.ops:
            deps = set()
            for t in op.reads:
                if t in last_w:
                    deps.add(last_w[t])
            for t in op.writes:
                if t in last_w:
                    deps.add(last_w[t])
                for r in readers.get(t, ()):
                    deps.add(r)
            deps.discard(op.idx)
            op.deps = sorted(deps)
            for t in op.reads:
                readers.setdefault(t, []).append(op.idx)
            for t in op.writes:
                last_w[t] = op.idx
                readers[t] = []
        qcount = {e: 0 for e in ENGS}
        qhist = {e: [] for e in ENGS}
        for op in self.ops:
            if op.dma == "cc":
                op.dticket = ("cc", op.idx, 1)
                continue
            if op.dma:
                n = qcount[op.eng]
                qcount[op.eng] += 1
                op.dticket = (op.eng, n % self.NDSEM, 16 * (n // self.NDSEM + 1))
                if n >= self.NDSEM:
                    op.dprev = qhist[op.eng][n - self.NDSEM]
                qhist[op.eng].append(op.idx)
        waited_eng = {e: {p: -1 for p in ENGS} for e in ENGS}
        waited_dma = {e: set() for e in ENGS}
        last_on = {e: -1 for e in ENGS}
        need_sig = set()
        for op in self.ops:
            e = op.eng
            final = []
            best = {}
            dl = list(op.deps)
            if op.dprev is not None:
                dl.append(op.dprev)
            for d in dl:
                p = self.ops[d]
                if p.dma:
                    if d not in waited_dma[e]:
                        waited_dma[e].add(d)
                        final.append(("dma", d))
                else:
                    if p.eng == e and e == "pe":
                        continue
                    if d <= waited_eng[e][p.eng]:
                        continue
                    if p.eng not in best or d > best[p.eng]:
                        best[p.eng] = d
            for pe_, d in best.items():
                waited_eng[e][pe_] = d
                need_sig.add(d)
                final.append(("eng", d))
            op.deps = final
        cnt = {e: 0 for e in ENGS}
        for op in self.ops:
            if not op.dma and op.idx in need_sig:
                cnt[op.eng] += 1
                op.sig = cnt[op.eng]
        self.sig_total = cnt
        self.dma_total = qcount

    def emit(self, barrier=False):
        nc = self.nc
        self.analyze()
        allsem = []

        Prog._phase_id += 1
        pid = Prog._phase_id

        def newsem(name):
            h = nc.alloc_semaphore(f"{name}_ph{pid}")
            allsem.append(h)
            return h

        if True:
            esem = {}
            for e in ENGS:
                n_ep = self.sig_total[e] // SIG_EPOCH + 1
                esem[e] = [newsem(f"s_{e}_{i}") for i in range(n_ep)]
            dsem = {}
            for e in ENGS:
                if self.dma_total[e]:
                    dsem[e] = [newsem(f"d_{e}_{i}") for i in range(self.NDSEM)]
            dsem["cc"] = {op.idx: newsem(f"cc_{op.idx}") for op in self.ops if op.dma == "cc"}

            def waitspec(dep):
                kind, d = dep
                p = self.ops[d]
                if kind == "dma":
                    q, si, val = p.dticket
                    return dsem[q][si], val
                k = p.sig - 1
                return esem[p.eng][k // SIG_EPOCH], k % SIG_EPOCH + 1

            def run(engname):
                def body(eng):
                    last_dma = {}
                    for op in self.ops:
                        if op.eng != engname:
                            continue
                        ws = [waitspec(d) for d in op.deps]
                        for (sem, val) in ws[1:]:
                            eng.wait_ge(sem, val)
                        ins = op.fn(eng)
                        if ws:
                            ins._wait_ge(ws[0][0], ws[0][1])
                        if op.dma == "cc":
                            ins.then_inc(dsem["cc"][op.idx])
                            eng.wait_ge(dsem["cc"][op.idx], 1)
                        elif op.dma:
                            q, si, val = op.dticket
                            ins.then_inc(dsem[q][si], 16)
                            last_dma[si] = val
                        elif op.sig is not None:
                            k = op.sig - 1
                            ins.then_inc(esem[engname][k // SIG_EPOCH], 1)
                    for si, val in last_dma.items():
                        eng.wait_ge(dsem[engname][si], val)
                return body

            with nc.Block() as block:
                block.tensor(run("pe"))
                block.scalar(run("act"))
                block.vector(run("dve"))
                block.gpsimd(run("pool"))
                block.sync(run("sp"))
        if barrier:
            nc.all_engine_barrier()
            nc.clear_and_free_semaphores(allsem)
            nc.all_engine_barrier()
        else:
            for h in allsem:
                nc.release_semaphore(h)


class KB:
    def __init__(self):
        self.nc = bass.Bass("TRN2", target_bir_lowering=False)
        self.p = Prog(self.nc)
        self.es = contextlib.ExitStack()
        self.dram = {}
        self._uid = 0
        self.es0 = contextlib.ExitStack()
        self.psf = [self.es0.enter_context(self.nc.psum_tensor(f"psb{i}", [128, 512], F32)) for i in range(8)]

    def uid(self, s):
        self._uid += 1
        return f"{s}_{self._uid}"

    def din(self, name, shape, dt=F32):
        t = self.nc.dram_tensor(name, list(shape), dt, kind="ExternalInput").ap()
        self.dram[name] = t
        return t

    def dout(self, name, shape, dt=F32, kind="ExternalOutput"):
        t = self.nc.dram_tensor(name, list(shape), dt, kind=kind).ap()
        self.dram[name] = t
        return t

    def sb(self, name, shape, dt=F32):
        return self.es.enter_context(self.nc.sbuf_tensor(self.uid(name), list(shape), dt))

    def dscratch(self, name, shape, dt=F32):
        t = self.nc.dram_tensor(name, list(shape), dt, kind="Internal").ap()
        self.dram[name] = t
        return t

    def end_phase(self):
        self.p.emit(barrier=True)
        self.es.close()
        self.es = contextlib.ExitStack()
        self.p = Prog(self.nc)

    def ps(self, i):
        return self.psf[i]

    def finish(self):
        self.p.emit()
        self.es.close()
        self.es0.close()
        return self.nc


C_SBQ, C_SBK, C_SBV = 0, 256, 512
C_MOQ, C_MOK, C_MOV = 768, 1024, 1280
C_CQ, C_CKV, C_KR = 1536, 1792, 1920
C_NSQ, C_NSKV, C_NSG, C_MEQ = 1952, 2208, 2592, 2604


def consts_common(kb):
    nc, p = kb.nc, kb.p
    c = {}
    c["identf"] = kb.sb("identf", [128, 128], F32)
    c["identb"] = kb.sb("identb", [128, 128], BF16)
    c["onesb"] = kb.sb("onesb", [128, 128], BF16)
    c["onesf"] = kb.sb("onesf", [128, 128], F32)
    idf, idb = c["identf"], c["identb"]
    p.pool(lambda e: e.memset(c["onesf"][:], 1.0), writes=["onesf"])
    p.pool(lambda e: e.memset(c["onesb"][:], 1.0), writes=["onesb"])
    p.pool(lambda e: e.affine_select(out=idf[:], in_=c["onesf"][:], pattern=[[-1, 128]], compare_op=ALU.is_equal,
                                     fill=0.0, base=0, channel_multiplier=1), reads=["onesf"], writes=["identf"])
    p.pool(lambda e: e.tensor_copy(out=idb[:], in_=idf[:]), reads=["identf"], writes=["identb"])
    return c


def tm_view(ap2d, p=128):
    return ap2d.rearrange("(n p) c -> p n c", p=p)


def phase_a(kb, c, io, w):
    nc, p = kb.nc, kb.p
    identf, onesb = c["identf"], c["onesb"]

    hT = kb.sb("hT", [128, 8, TOK], BF16)
    win = kb.sb("win", [128, 8, IN_TOTAL], BF16)
    wkrot = kb.sb("wkrot", [128, 8, 32], BF16)
    for f in range(8):
        p.dma(win[:, f, :], w["w_in"][f * 128:(f + 1) * 128, :], writes=[("win", f)], q="pool")
    for f in range(8):
        p.dve(lambda e, f=f: e.tensor_scalar_mul(out=wkrot[:, f, 0:16], in0=win[:, f, C_KR + 16:C_KR + 32], scalar1=-1.0),
              reads=[("win", f)], writes=[("wkrot", f)])
        p.dve(lambda e, f=f: e.tensor_copy(out=wkrot[:, f, 16:32], in_=win[:, f, C_KR:C_KR + 16]),
              reads=[("win", f)], writes=[("wkrot", f)])
    wuq_f = kb.sb("wuq_f", [128, 2, 384], F32)
    wuq = kb.sb("wuq", [128, 2, 384], BF16)
    wuqr = kb.sb("wuqr", [128, 2, 384], BF16)
    gcq = kb.sb("gcq", [128, 2], F32)
    wukv_f = kb.sb("wukv_f", [128, 512], F32)
    wukv = kb.sb("wukv", [128, 512], BF16)
    gckv = kb.sb("gckv", [128, 1], F32)
    p.dma(wuq_f[:], w["w_uq"].rearrange("(n p) c -> p n c", p=128), writes=["wuq_f"])
    p.dma(gcq[:], w["g_cq"].rearrange("(n p) -> p n", p=128), writes=["gcq"], slow=True)
    p.dma(wukv_f[:], w["w_ukv"], writes=["wukv_f"])
    p.dma(gckv[:], w["g_ckv"].rearrange("(n p) -> p n", p=128), writes=["gckv"], slow=True)
    for rc in range(2):
        p.dve(lambda e, rc=rc: e.tensor_scalar_mul(out=wuq[:, rc, :], in0=wuq_f[:, rc, :], scalar1=gcq[:, rc:rc + 1]),
              reads=["wuq_f", "gcq"], writes=["wuq"])
    p.dve(lambda e: e.memset(wuqr[:], 0.0), writes=["wuqr"])
    for rc in range(2):
        for h in range(4):
            b0 = h * 96
            p.dve(lambda e, rc=rc, b0=b0: e.tensor_scalar_mul(out=wuqr[:, rc, b0 + 64:b0 + 80], in0=wuq[:, rc, b0 + 80:b0 + 96], scalar1=-1.0),
                  reads=["wuq"], writes=["wuqr"])
            p.dve(lambda e, rc=rc, b0=b0: e.tensor_copy(out=wuqr[:, rc, b0 + 80:b0 + 96], in_=wuq[:, rc, b0 + 64:b0 + 80]),
                  reads=["wuq"], writes=["wuqr"])
    p.dve(lambda e: e.tensor_scalar_mul(out=wukv[:], in0=wukv_f[:], scalar1=gckv[:, 0:1]), reads=["wukv_f", "gckv"], writes=["wukv"])

    hst = [kb.sb("hst", [128, 1024], F32) for _ in range(2)]
    for ck in range(TOK // 128):
        st = hst[ck % 2]
        tk = ("hst", ck % 2)
        p.dma(st[:], io["h_tok"][ck * 128:(ck + 1) * 128, :], writes=[tk])
        for half in range(2):
            bank = (ck * 2 + half) % 2
            ps = kb.ps(bank)
            for j in range(4):
                f = half * 4 + j
                p.pe(lambda e, ps=ps, st=st, f=f, j=j: e.transpose(out=ps[:, j * 128:(j + 1) * 128], in_=st[:, f * 128:(f + 1) * 128], identity=identf[:]),
                     reads=[tk, "identf"], writes=[("ps", bank)])
            dst = hT[:, half * 4:half * 4 + 4, ck * 128:(ck + 1) * 128]
            src = ps[:].rearrange("p (j t) -> p j t", j=4)
            if half == 0:
                p.act(lambda e, dst=dst, src=src: e.copy(out=dst, in_=src), reads=[("ps", bank)], writes=[("hT", ck // 4)])
            else:
                p.dve(lambda e, dst=dst, src=src: e.tensor_copy(out=dst, in_=src), reads=[("ps", bank)], writes=[("hT", ck // 4)])
    for f in range(8):
        p.dma(io["hT_d"][f * 128:(f + 1) * 128, :], hT[:, f, :], reads=[("hT", s) for s in range(8)], writes=[("hT_d", f)])

    ostage = [kb.sb("ostg", [128, 512], BF16) for _ in range(4)]
    gstage = [kb.sb("gstg", [12, 512], F32) for _ in range(2)]
    cq_sb = [kb.sb("cq_sb", [128, 2, 512], BF16) for _ in range(2)]
    ckv_sb = [kb.sb("ckv_sb", [128, 512], BF16) for _ in range(2)]
    krr_sb = [kb.sb("krr", [32, 2, 512], F32) for _ in range(2)]
    sq_sb = [kb.sb("sq", [128, 3, 512], BF16) for _ in range(2)]
    rstd_q = [kb.sb("rstdq", [128, 512], F32) for _ in range(2)]
    rstd_kv = [kb.sb("rstdkv", [128, 512], F32) for _ in range(2)]
    rkv_tok = [kb.sb("rkvtok", [128, 4], F32) for _ in range(2)]
    ropeq = [kb.sb("ropeq", [96, 2, 512], F32) for _ in range(2)]
    ropek = [kb.sb("ropek", [32, 2, 512], F32) for _ in range(2)]
    t1 = [kb.sb("t1", [96, 512], F32) for _ in range(2)]
    t2 = [kb.sb("t2", [96, 512], F32) for _ in range(2)]
    vstage = [kb.sb("vstg", [128, 640], BF16) for _ in range(2)]
    vmst = [kb.sb("vmst", [128, 256], BF16) for _ in range(2)]
    cnt = {"o": 0, "bank": 0, "v": 0}

    def nbank():
        b = 2 + cnt["bank"] % 6
        cnt["bank"] += 1
        return b

    fm_list = [
        ("qsb_d", 0, C_SBQ, 128, 0.125), ("qsb_d", 128, C_SBQ + 128, 128, 0.125),
        ("ksb_d", 0, C_SBK, 128, 1.0), ("ksb_d", 128, C_SBK + 128, 128, 1.0),
        ("qmo_d", 0, C_MOQ, 128, 0.125), ("qmo_d", 128, C_MOQ + 128, 128, 0.125),
        ("kmo_d", 0, C_MOK, 128, 1.0), ("kmo_d", 128, C_MOK + 128, 128, 1.0),
        ("qns_d", 0, C_NSQ, 128, 0.125), ("qns_d", 128, C_NSQ + 128, 128, 0.125),
        ("kcv_d", 0, C_NSKV, 128, 1.0),
        ("ksl_d", 0, C_NSKV + 128, 64, 1.0),
        ("kwi_d", 0, C_NSKV + 256, 64, 1.0),
        ("qme_d", 0, C_MEQ, 128, 0.125), ("qme_d", 128, C_MEQ + 128, 128, 0.125),
    ]

    def proj_fm(s, col0, ncols, wsrc=None):
        b = nbank()
        ps = kb.ps(b)
        for f in range(8):
            if wsrc is None:
                lhsT = win[:, f, col0:col0 + ncols]
                rd = [("win", f)]
            else:
                lhsT = wsrc[:, f, col0:col0 + ncols]
                rd = [("wkrot", f)]
            p.pe(lambda e, ps=ps, lhsT=lhsT, f=f, s=s, ncols=ncols: e.matmul(ps[0:ncols, :], lhsT=lhsT, rhs=hT[:, f, s * 512:(s + 1) * 512],
                                                                            start=(f == 0), stop=(f == 7)),
                 reads=rd + [("hT", s)], writes=[("ps", b)])
        return b

    for s in range(NSLOT):
        tsl = slice(s * 512, (s + 1) * 512)
        for (dn, r0, col0, ncols, scale) in fm_list:
            b = proj_fm(s, col0, ncols)
            ps = kb.ps(b)
            k = cnt["o"] % 4
            cnt["o"] += 1
            og = ostage[k]
            p.act(lambda e, og=og, ps=ps, ncols=ncols, scale=scale: e.activation(out=og[0:ncols, :], in_=ps[0:ncols, :], func=AF.Copy, scale=scale),
                  reads=[("ps", b)], writes=[("ostg", k)])
            p.dma(io[dn][r0:r0 + ncols, tsl], og[0:ncols, :], reads=[("ostg", k)], writes=[(dn, s)])
        b = proj_fm(s, C_NSG, 12)
        ps = kb.ps(b)
        gs = gstage[s % 2]
        p.act(lambda e, gs=gs, ps=ps: e.activation(out=gs[:], in_=ps[0:12, :], func=AF.Sigmoid), reads=[("ps", b)], writes=[("gstg", s % 2)])
        p.dma(io["gns_d"][:, tsl], gs[:], reads=[("gstg", s % 2)], writes=[("gns_d", s)])

        d2 = s % 2
        cq, ckv, sq = cq_sb[d2], ckv_sb[d2], sq_sb[d2]
        for rc in range(2):
            b = proj_fm(s, C_CQ + rc * 128, 128)
            ps = kb.ps(b)
            p.act(lambda e, cq=cq, ps=ps, rc=rc: e.copy(out=cq[:, rc, :], in_=ps[:]), reads=[("ps", b)], writes=[("cq", d2)])
            p.act(lambda e, sq=sq, ps=ps, rc=rc: e.activation(out=sq[:, rc, :], in_=ps[:], func=AF.Square), reads=[("ps", b)], writes=[("sq", d2)])
        b = proj_fm(s, C_CKV, 128)
        ps = kb.ps(b)
        p.act(lambda e, ckv=ckv, ps=ps: e.copy(out=ckv[:], in_=ps[:]), reads=[("ps", b)], writes=[("ckv", d2)])
        p.act(lambda e, sq=sq, ps=ps: e.activation(out=sq[:, 2, :], in_=ps[:], func=AF.Square), reads=[("ps", b)], writes=[("sq", d2)])
        krr = krr_sb[d2]
        b = proj_fm(s, C_KR, 32)
        ps = kb.ps(b)
        p.act(lambda e, krr=krr, ps=ps: e.copy(out=krr[:, 0, :], in_=ps[0:32, :]), reads=[("ps", b)], writes=[("krr", d2)])
        b = proj_fm(s, 0, 32, wsrc=wkrot)
        ps = kb.ps(b)
        p.act(lambda e, krr=krr, ps=ps: e.copy(out=krr[:, 1, :], in_=ps[0:32, :]), reads=[("ps", b)], writes=[("krr", d2)])
        rq, rkv = rstd_q[d2], rstd_kv[d2]
        b = nbank()
        ps = kb.ps(b)
        for rc in range(2):
            p.pe(lambda e, ps=ps, sq=sq, rc=rc: e.matmul(ps[:], lhsT=onesb[:], rhs=sq[:, rc, :], start=(rc == 0), stop=(rc == 1)),
                 reads=[("sq", d2), "onesb"], writes=[("ps", b)])
        p.act(lambda e, rq=rq, ps=ps: e.activation(out=rq[:], in_=ps[:], func=AF.Ln, scale=1.0 / 256.0, bias=c["eps_rms"][:, 0:1]),
              reads=[("ps", b), "cst"], writes=[("rq", d2)])
        p.act(lambda e, rq=rq: e.activation(out=rq[:], in_=rq[:], func=AF.Exp, scale=-0.5), reads=[("rq", d2)], writes=[("rq", d2)])
        b = nbank()
        ps = kb.ps(b)
        p.pe(lambda e, ps=ps, sq=sq: e.matmul(ps[:], lhsT=onesb[:], rhs=sq[:, 2, :], start=True, stop=True),
             reads=[("sq", d2), "onesb"], writes=[("ps", b)])
        p.act(lambda e, rkv=rkv, ps=ps: e.activation(out=rkv[:], in_=ps[:], func=AF.Ln, scale=1.0 / 128.0, bias=c["eps_rms"][:, 0:1]),
              reads=[("ps", b), "cst"], writes=[("rkv", d2)])
        p.act(lambda e, rkv=rkv: e.activation(out=rkv[:], in_=rkv[:], func=AF.Exp, scale=-0.5), reads=[("rkv", d2)], writes=[("rkv", d2)])
        rkt = rkv_tok[d2]
        b = nbank()
        ps = kb.ps(b)
        for ck in range(4):
            p.pe(lambda e, ps=ps, sq=sq, ck=ck: e.matmul(ps[:, ck:ck + 1], lhsT=sq[:, 2, ck * 128:(ck + 1) * 128], rhs=onesb[:, 0:1], start=True, stop=True),
                 reads=[("sq", d2), "onesb"], writes=[("ps", b)])
        p.act(lambda e, rkt=rkt, ps=ps: e.activation(out=rkt[:], in_=ps[:, 0:4], func=AF.Ln, scale=1.0 / 128.0, bias=c["eps_rms"][:, 0:1]),
              reads=[("ps", b), "cst"], writes=[("rkt", d2)])
        p.act(lambda e, rkt=rkt: e.activation(out=rkt[:], in_=rkt[:], func=AF.Exp, scale=-0.5), reads=[("rkt", d2)], writes=[("rkt", d2)])
        rpq, rpk = ropeq[d2], ropek[d2]
        p.dma(rpq[:], io["ropeq_t"][s], writes=[("rpq", d2)])
        p.dma(rpk[:], io["ropek_t"][s], writes=[("rpk", d2)])
        for h in range(4):
            bA, bB = nbank(), nbank()
            psA, psB = kb.ps(bA), kb.ps(bB)
            for rc in range(2):
                p.pe(lambda e, psA=psA, cq=cq, rc=rc, h=h: e.matmul(psA[0:96, :], lhsT=wuq[:, rc, h * 96:(h + 1) * 96], rhs=cq[:, rc, :], start=(rc == 0), stop=(rc == 1)),
                     reads=["wuq", ("cq", d2)], writes=[("ps", bA)])
            for rc in range(2):
                p.pe(lambda e, psB=psB, cq=cq, rc=rc, h=h: e.matmul(psB[0:96, :], lhsT=wuqr[:, rc, h * 96:(h + 1) * 96], rhs=cq[:, rc, :], start=(rc == 0), stop=(rc == 1)),
                     reads=["wuqr", ("cq", d2)], writes=[("ps", bB)])
            a1, a2 = t1[h % 2], t2[h % 2]
            k = cnt["o"] % 4
            cnt["o"] += 1
            og = ostage[k]
            p.dve(lambda e, a1=a1, psA=psA, rpq=rpq: e.tensor_tensor(out=a1[:], in0=psA[0:96, :], in1=rpq[:, 0, :], op=ALU.mult),
                  reads=[("ps", bA), ("rpq", d2)], writes=[("t1", h % 2)])
            p.dve(lambda e, a2=a2, psB=psB, rpq=rpq: e.tensor_tensor(out=a2[:], in0=psB[0:96, :], in1=rpq[:, 1, :], op=ALU.mult),
                  reads=[("ps", bB), ("rpq", d2)], writes=[("t2", h % 2)])
            p.dve(lambda e, a1=a1, a2=a2: e.tensor_tensor(out=a1[:], in0=a1[:], in1=a2[:], op=ALU.add),
                  reads=[("t1", h % 2), ("t2", h % 2)], writes=[("t1", h % 2)])
            p.dve(lambda e, a1=a1, og=og, rq=rq: e.tensor_tensor(out=og[0:96, :], in0=a1[:], in1=rq[0:96, :], op=ALU.mult),
                  reads=[("t1", h % 2), ("rq", d2)], writes=[("ostg", k)])
            p.dma(io["qml_d"][h, :, tsl], og[0:96, :], reads=[("ostg", k)], writes=[("qml_d", s, h)])
            b = nbank()
            ps = kb.ps(b)
            p.pe(lambda e, ps=ps, ckv=ckv, h=h: e.matmul(ps[0:64, :], lhsT=wukv[:, h * 128:h * 128 + 64], rhs=ckv[:], start=True, stop=True),
                 reads=["wukv", ("ckv", d2)], writes=[("ps", b)])
            k = cnt["o"] % 4
            cnt["o"] += 1
            og = ostage[k]
            p.dve(lambda e, og=og, ps=ps, rkv=rkv: e.tensor_tensor(out=og[0:64, :], in0=ps[0:64, :], in1=rkv[0:64, :], op=ALU.mult),
                  reads=[("ps", b), ("rkv", d2)], writes=[("ostg", k)])
            p.dma(io["kml_d"][h, :, tsl], og[0:64, :], reads=[("ostg", k)], writes=[("kml_d", s, h)])
        a1, a2 = t1[0], t2[0]
        k = cnt["o"] % 4
        cnt["o"] += 1
        og = ostage[k]
        p.dve(lambda e, a1=a1, krr=krr, rpk=rpk: e.tensor_tensor(out=a1[0:32, :], in0=krr[:, 0, :], in1=rpk[:, 0, :], op=ALU.mult),
              reads=[("krr", d2), ("rpk", d2)], writes=[("t1", 0)])
        p.dve(lambda e, a2=a2, krr=krr, rpk=rpk: e.tensor_tensor(out=a2[0:32, :], in0=krr[:, 1, :], in1=rpk[:, 1, :], op=ALU.mult),
              reads=[("krr", d2), ("rpk", d2)], writes=[("t2", 0)])
        p.dve(lambda e, a1=a1, a2=a2, og=og: e.tensor_tensor(out=og[0:32, :], in0=a1[0:32, :], in1=a2[0:32, :], op=ALU.add),
              reads=[("t1", 0), ("t2", 0)], writes=[("ostg", k)])
        p.dma(io["krl_d"][:, tsl], og[0:32, :], reads=[("ostg", k)], writes=[("krl_d", s)])
        for ck in range(4):
            gck = s * 4 + ck
            b = nbank()
            ps = kb.ps(b)
            for h in range(4):
                p.pe(lambda e, ps=ps, ckv=ckv, ck=ck, h=h: e.matmul(ps[:, h * 64:(h + 1) * 64], lhsT=ckv[:, ck * 128:(ck + 1) * 128], rhs=wukv[:, h * 128 + 64:h * 128 + 128],
                                                                 start=True, stop=True),
                     reads=["wukv", ("ckv", d2)], writes=[("ps", b)])
            vm = vmst[gck % 2]
            p.act(lambda e, vm=vm, ps=ps, rkt=rkt, ck=ck: e.activation(out=vm[:], in_=ps[:, 0:256], func=AF.Copy, scale=rkt[:, ck:ck + 1]),
                  reads=[("ps", b), ("rkt", d2)], writes=[("vmst", gck % 2)])
            p.dma(io["vml_d"][gck * 128:(gck + 1) * 128, :], vm[:], reads=[("vmst", gck % 2)], writes=[("vml_d", gck)])

        for ck in range(4):
            gck = s * 4 + ck
            tcs = slice(gck * 128, (gck + 1) * 128)
            vs = vstage[gck % 2]
            b1, b2 = nbank(), nbank()
            ps1, ps2 = kb.ps(b1), kb.ps(b2)
            for f in range(8):
                p.pe(lambda e, ps1=ps1, f=f, tcs=tcs: e.matmul(ps1[:, 0:256], lhsT=hT[:, f, tcs], rhs=win[:, f, C_SBV:C_SBV + 256], start=(f == 0), stop=(f == 7)),
                     reads=[("win", f), ("hT", s)], writes=[("ps", b1)])
            for f in range(8):
                p.pe(lambda e, ps1=ps1, f=f, tcs=tcs: e.matmul(ps1[:, 256:512], lhsT=hT[:, f, tcs], rhs=win[:, f, C_MOV:C_MOV + 256], start=(f == 0), stop=(f == 7)),
                     reads=[("win", f), ("hT", s)], writes=[("ps", b1)])
            for f in range(8):
                p.pe(lambda e, ps2=ps2, f=f, tcs=tcs: e.matmul(ps2[:, 0:64], lhsT=hT[:, f, tcs], rhs=win[:, f, C_NSKV + 192:C_NSKV + 256], start=(f == 0), stop=(f == 7)),
                     reads=[("win", f), ("hT", s)], writes=[("ps", b2)])
            for f in range(8):
                p.pe(lambda e, ps2=ps2, f=f, tcs=tcs: e.matmul(ps2[:, 64:128], lhsT=hT[:, f, tcs], rhs=win[:, f, C_NSKV + 320:C_NSKV + 384], start=(f == 0), stop=(f == 7)),
                     reads=[("win", f), ("hT", s)], writes=[("ps", b2)])
            p.act(lambda e, vs=vs, ps1=ps1: e.copy(out=vs[:, 0:512], in_=ps1[:]), reads=[("ps", b1)], writes=[("vstg", gck % 2)])
            p.dve(lambda e, vs=vs, ps2=ps2: e.tensor_copy(out=vs[:, 512:640], in_=ps2[:, 0:128]), reads=[("ps", b2)], writes=[("vstg", gck % 2)])
            p.dma(io["vsb_d"][tcs, :], vs[:, 0:256], reads=[("vstg", gck % 2)], writes=[("vsb_d", gck)])
            p.dma(io["vmo_d"][tcs, :], vs[:, 256:512], reads=[("vstg", gck % 2)], writes=[("vmo_d", gck)])
            p.dma(io["vsw_d"][tcs, :], vs[:, 512:640], reads=[("vstg", gck % 2)], writes=[("vsw_d", gck)])


A_OUTS = {
    "hT_d": ([1024, TOK], BF16),
    "qsb_d": ([256, TOK], BF16), "ksb_d": ([256, TOK], BF16), "vsb_d": ([TOK, 256], BF16),
    "qmo_d": ([256, TOK], BF16), "kmo_d": ([256, TOK], BF16), "vmo_d": ([TOK, 256], BF16),
    "qml_d": ([4, 96, TOK], BF16), "kml_d": ([4, 64, TOK], BF16), "krl_d": ([32, TOK], BF16), "vml_d": ([TOK, 256], BF16),
    "qns_d": ([256, TOK], BF16), "kcv_d": ([128, TOK], BF16), "ksl_d": ([64, TOK], BF16), "kwi_d": ([64, TOK], BF16),
    "vsw_d": ([TOK, 128], BF16), "gns_d": ([12, TOK], F32), "qme_d": ([256, TOK], BF16),
}


def load_consts(kb, io):
    p = kb.p
    c = {}
    c["identf"] = kb.sb("identf", [128, 128], F32)
    c["identb"] = kb.sb("identb", [128, 128], BF16)
    c["onesb"] = kb.sb("onesb", [128, 128], BF16)
    c["onesf"] = kb.sb("onesf", [128, 128], F32)
    c["cstf"] = kb.sb("cstf", [128, 8], F32)
    p.dma(c["identf"][:], io["c_ident"], writes=["identf"])
    p.dma(c["identb"][:], io["c_ident"], writes=["identb"], q="pool")
    p.dma(c["onesf"][:], io["c_ones"], writes=["onesf"])
    p.dma(c["onesb"][:], io["c_ones"], writes=["onesb"], q="pool")
    p.dma(c["cstf"][:], io["c_cst"], writes=["cst"])
    c["eps_rms"] = c["cstf"][:, 0:1]
    c["eps_ln"] = c["cstf"][:, 1:2]
    c["tiny"] = c["cstf"][:, 2:3]
    c["zrow"] = kb.sb("zrow", [1, 8], BF16)
    p.pool(lambda e: e.memset(c["zrow"][:], 0.0), writes=["zrow"])
    return c


def host_consts():
    cst = np.zeros((128, 8), np.float32)
    cst[:, 0] = RMS_EPS
    cst[:, 1] = LN_EPS
    cst[:, 2] = 1e-30
    cst[:, 3] = 1.0
    return {"c_ident": np.eye(128, dtype=np.float32), "c_ones": np.ones((128, 128), np.float32), "c_cst": cst}


def rope_tables(r):
    half = 16
    freqs = np.power(np.float32(10000.0), -np.arange(half, dtype=np.float32) / half).astype(np.float32)
    rq = np.zeros((NSLOT, 96, 2, 512), np.float32)
    rk = np.zeros((NSLOT, 32, 2, 512), np.float32)
    sc = np.float32(96.0 ** -0.5)
    for s in range(NSLOT):
        pos = (512 * (2 * s + r) + np.arange(512)).astype(np.float32)
        ang = pos[None, :] * freqs[:, None]
        cos, sin = np.cos(ang).astype(np.float32), np.sin(ang).astype(np.float32)
        c2 = np.concatenate([cos, cos], 0)
        s2 = np.concatenate([sin, sin], 0)
        rq[s, 0:64, 0, :] = sc
        rq[s, 64:96, 0, :] = sc * c2
        rq[s, 64:96, 1, :] = sc * s2
        rk[s, :, 0, :] = c2
        rk[s, :, 1, :] = s2
    return rq, rk


def build_a():
    kb = KB()
    io = {}
    io["h_tok"] = kb.din("h_tok", [TOK, D])
    io["c_ident"] = kb.din("c_ident", [128, 128])
    io["c_ones"] = kb.din("c_ones", [128, 128])
    io["c_cst"] = kb.din("c_cst", [128, 8])
    io["ropeq_t"] = kb.din("ropeq_t", [NSLOT, 96, 2, 512])
    io["ropek_t"] = kb.din("ropek_t", [NSLOT, 32, 2, 512])
    w = {"w_in": kb.din("w_in", [D, IN_TOTAL]), "w_uq": kb.din("w_uq", [256, 384]), "g_cq": kb.din("g_cq", [256]),
         "w_ukv": kb.din("w_ukv", [128, 512]), "g_ckv": kb.din("g_ckv", [128])}
    for n, (shp, dt) in A_OUTS.items():
        io[n] = kb.dout(n, shp, dt)
    c = load_consts(kb, io)
    phase_a(kb, c, io, w)
    return kb.finish()


def bconsts_host(r):
    bf = ml_dtypes.bfloat16
    o = {}
    kl = np.arange(128)[:, None]
    ql = np.arange(512)[None, :]
    cm = np.zeros((8, 128, 512), np.float32)
    cms = np.zeros((8, 128, 512), np.float32)
    for jj in range(8):
        kp = 128 * jj + kl
        qp = 512 * r + ql
        cm[jj] = np.where(kp <= qp, 0.0, MASKV)
        cms[jj] = np.where(kp < qp, 0.0, MASKV)
    o["c_cm"] = cm.transpose(1, 0, 2).astype(bf)
    o["c_cms"] = cms.transpose(1, 0, 2).astype(bf)
    wm = np.zeros((12, 128, 512), np.float32)
    for ji, jrel in enumerate(range(-4, 8)):
        dist = 512 * r + ql - 128 * jrel - kl
        wm[ji] = np.where((dist >= 0) & (dist < 512), 0.0, MASKV)
    o["c_wm"] = wm.transpose(1, 0, 2).astype(bf)
    pm = np.zeros((3, 128, 512), np.float32)
    for ii, idx in enumerate((6, 7, 8)):
        pm[ii] = np.where(16 * kl + 31 - 512 * r - ql <= 1024 * (idx - 6), 0.0, MASKV)
    o["c_pm"] = pm.transpose(1, 0, 2).astype(bf)
    kp = np.arange(S)
    o["c_kaug"] = np.stack([kp // 128, kp % 128, np.ones(S), np.ones(S)]).astype(bf)
    ce = 16 * np.arange(512) + 31
    caug = np.stack([ce // 128, ce % 128, np.ones(512), np.ones(512)]).astype(np.float32)
    caug[0, 511] = -30000.0
    o["c_caug"] = caug.astype(bf)
    tl = np.arange(TOK)
    qp = 512 * (2 * (tl // 512) + r) + tl % 512
    qa = np.zeros((4, 4, TOK), np.float32)
    for h in range(4):
        sl = SLOPES[h]
        qa[h, 0] = 128 * sl
        qa[h, 1] = sl
        qa[h, 2] = -sl * 128 * (qp // 128)
        qa[h, 3] = -sl * (qp % 128)
    o["c_qaug"] = qa.astype(bf)
    o["c_g32"] = ((np.arange(S)[None, :] // 64) % 32 == np.arange(32)[:, None]).astype(np.float32).astype(bf)
    o["c_tm"] = (np.arange(S)[None, :] // 256 == np.arange(32)[:, None]).astype(np.float32).astype(bf)
    n = np.arange(512)[:, None]
    s_ = np.arange(128)[None, :]
    ov = ((16 * n < 64 * s_ + 64) & (16 * n + 32 > 64 * s_)).astype(np.float32)
    ov = np.concatenate([ov, np.ones((512, 1), np.float32)], 1)
    ov[511] = 0.0
    o["c_nui"] = -(np.arange(128)[:, None] >= np.arange(128)[None, :]).astype(np.float32).astype(bf)
    o["c_ov"] = ov.reshape(4, 128, 129).transpose(1, 0, 2).astype(bf)
    vb = np.zeros((8, 4, 32), np.float32)
    own_t = np.zeros((8, 4, 32), np.float32)
    for s in range(8):
        for qb in range(4):
            own = (4 * (2 * s + r) + qb) // 2
            vb[s, qb] = np.where(np.arange(32) < own, 0.0, -1e30)
            own_t[s, qb, own] = 1.0
    o["c_vb"] = np.broadcast_to(vb[None], (128, 8, 4, 32)).copy()
    o["c_own"] = np.broadcast_to(own_t[None], (128, 8, 4, 32)).copy()
    M = np.zeros((8, 128, 4, 128), np.float32)
    C = np.zeros((8, 128, 4, 128), np.float32)
    sid = np.arange(128)[None, :]
    for s in range(8):
        for qb in range(4):
            qpos = 512 * (2 * s + r) + 128 * qb + np.arange(128)[:, None]
            cur = qpos // 64
            forced_cur = sid == cur
            forced0 = (sid == 0) & ~forced_cur
            past = (sid < cur) & ~forced0 & ~forced_cur
            M[s, :, qb] = past
            C[s, :, qb] = np.where(forced_cur, 1e30, np.where(forced0, 5e29, np.where(past, 0.0, -1e30)))
    o["c_selm"] = M
    o["c_selc"] = C
    return o


B_CONST_SHAPES = {
    "c_cm": ([128, 8, 512], BF16), "c_cms": ([128, 8, 512], BF16), "c_wm": ([128, 12, 512], BF16), "c_pm": ([128, 3, 512], BF16),
    "c_kaug": ([4, S], BF16), "c_caug": ([4, 512], BF16), "c_qaug": ([4, 4, TOK], BF16),
    "c_g32": ([32, S], BF16), "c_tm": ([32, S], BF16), "c_ov": ([128, 4, 129], BF16),
    "c_nui": ([128, 128], BF16), "c_vb": ([128, 8, 4, 32], F32), "c_own": ([128, 8, 4, 32], F32),
    "c_selm": ([8, 128, 4, 128], F32), "c_selc": ([8, 128, 4, 128], F32),
}

B_INS = {
    "qsb_d": ([256, TOK], BF16), "qmo_d": ([256, TOK], BF16), "qml_d": ([4, 96, TOK], BF16), "qns_d": ([256, TOK], BF16),
    "qme_d": ([256, TOK], BF16), "gns_d": ([12, TOK], F32),
    "ksb_f": ([256, S], BF16), "vsb_f": ([S, 256], BF16), "kmo_f": ([256, S], BF16), "vmo_f": ([S, 256], BF16),
    "kml_f": ([4, 64, S], BF16), "krl_f": ([32, S], BF16), "vml_f": ([S, 256], BF16),
    "kcv_f": ([128, S], BF16), "ksl_f": ([64, S], BF16), "kwi_f": ([64, S], BF16), "vsw_f": ([S, 128], BF16),
    "mem": ([256, D], F32),
}


class AttnBufs:
    pass


class KFull:
    def __init__(self, ap):
        self.ap = ap

    def rows(self, lo, hi):
        return ("full", self.ap[lo:hi, :])


class KPair:
    def __init__(self, ap, R, base=0):
        self.ap, self.R, self.base = ap, R, base

    def rows(self, lo, hi):
        return ("pair", self.ap, self.R, self.base + lo, hi - lo)


class VFull:
    def __init__(self, ap, cbase=0):
        self.ap, self.cbase = ap, cbase

    def cols(self, c0):
        return ("full", self.ap, self.cbase + c0)


class VPair:
    def __init__(self, chunks, cbase=0):
        self.chunks, self.cbase = chunks, cbase

    def cols(self, c0):
        return ("pair", self.chunks, self.cbase + c0)


class TMChunks:
    def __init__(self, chunks, c0, c1):
        self.chunks, self.c0, self.c1 = chunks, c0, c1

    def __getitem__(self, key):
        rs, cs = key
        k = rs.start // 1024
        a = self.chunks[k][rs.start - 1024 * k:rs.stop - 1024 * k, self.c0:self.c1]
        return a[:, cs]


def phase_b(kb, c, io, w, branches=("sb", "moba", "mla", "nsa", "mem")):
    nc, p = kb.nc, kb.p
    A = AttnBufs()
    A.KT = [kb.sb("KT", [128, S], BF16) for _ in range(2)]
    A.V = [kb.sb("V", [128, 64, 65], BF16) for _ in range(2)]
    A.QT = kb.sb("QT", [128, 4, TOK], BF16)
    A.cm = kb.sb("cm", [128, 8, 512], BF16)
    A.cms = kb.sb("cms", [128, 8, 512], BF16)
    A.P = [kb.sb("P", [128, 512], BF16) for _ in range(3)]
    A.rden = [kb.sb("rden", [65, 512], F32) for _ in range(2)]
    A.bcs = [kb.sb("bcs", [64, 512], F32) for _ in range(2)]
    A.ost = [kb.sb("ost", [64, 512], BF16) for _ in range(2)]
    A.cnt = {"kt": 0, "v": 0, "P": 0, "fin": 0, "sc": 0}
    p.dma(A.cm[:], io["c_cm"], writes=["cm"])
    p.dma(A.cms[:], io["c_cms"], writes=["cms"])
    for i in range(2):
        p.pool(lambda e, i=i: e.memset(A.V[i][:, :, 64:65], 1.0), writes=[("V", i)])

    def load_K(rows_src, dk, aug=None):
        i = A.cnt["kt"] % 2
        A.cnt["kt"] += 1
        kt = A.KT[i]
        for (src, r0) in rows_src:
            if src[0] == "full":
                n = src[1].shape[0]
                p.dma(kt[r0:r0 + n, :], src[1], writes=[("KT", i)])
            else:
                _, ap, R, row0, n = src
                for rr in range(2):
                    p.dma(kt[r0:r0 + n, :].rearrange("p (s r i) -> p s r i", r=2, i=512)[:, :, rr, :],
                          ap[rr * R + row0:rr * R + row0 + n, :].rearrange("p (s i) -> p s i", i=512), writes=[("KT", i)])
        if aug is not None:
            p.dma(kt[dk:dk + 4, :], aug, writes=[("KT", i)])
        return kt, ("KT", i)

    def load_V(src, col0):
        i = A.cnt["v"] % 2
        A.cnt["v"] += 1
        v = A.V[i]
        sp_ = src.cols(col0)
        if sp_[0] == "full":
            p.dma(v[:, :, 0:64], sp_[1].rearrange("(n p) c -> p n c", p=128)[:, :, sp_[2]:sp_[2] + 64], writes=[("V", i)])
        else:
            _, chunks, cc0 = sp_
            for k, ch in enumerate(chunks):
                for rr in range(2):
                    for s2 in range(2):
                        n0 = 16 * k + 8 * s2 + 4 * rr
                        p.dma(v[:, n0:n0 + 4, 0:64],
                              ch[rr * 1024 + s2 * 512:rr * 1024 + (s2 + 1) * 512, cc0:cc0 + 64].rearrange("(q p) c -> p q c", p=128),
                              writes=[("V", i)])
        return v, ("V", i)

    def load_Q(src_rows, h, dk, aug=None):
        p.dma(A.QT[0:dk, h, :], src_rows, writes=[("QT", h)])
        if aug is not None:
            p.dma(A.QT[dk:dk + 4, h, :], aug, writes=[("QT", h)])
        return ("QT", h)

    def finalize_plain(ops_bank, dst, h, s, gate=None, acc=None, acc_tok=None, first=True, last=True):
        k = A.cnt["fin"] % 2
        A.cnt["fin"] += 1
        ps = kb.ps(ops_bank)
        rd, bcs, ost = A.rden[k], A.bcs[k], A.ost[k]
        bcb = 6 + k

        def part1():
            p.dve(lambda e: e.tensor_scalar(out=rd[64:65, :], in0=ps[64:65, :], scalar1=1e-30, scalar2=None, op0=ALU.max),
                  reads=[("ps", ops_bank)], writes=[("rden", k)])
            p.dve(lambda e: e.reciprocal(out=rd[64:65, :], in_=rd[64:65, :]), reads=[("rden", k)], writes=[("rden", k)])
            if gate is not None:
                gt, gtok, gidx = gate
                p.dve(lambda e: e.tensor_tensor(out=rd[64:65, :], in0=rd[64:65, :], in1=gt[64:65, gidx, :], op=ALU.mult),
                      reads=[("rden", k), gtok], writes=[("rden", k)])

        def part2():
            pb = kb.ps(bcb)
            p.pe(lambda e: e.matmul(pb[0:64, :], lhsT=c["onesf"][64:65, 0:64], rhs=rd[64:65, :], start=True, stop=True),
                 reads=[("rden", k), "onesf"], writes=[("ps", bcb)])
            p.act(lambda e: e.copy(out=bcs[:], in_=pb[0:64, :]), reads=[("ps", bcb)], writes=[("bcs", k)])
            if acc is None:
                p.dve(lambda e: e.tensor_tensor(out=ost[:], in0=ps[0:64, :], in1=bcs[:], op=ALU.mult),
                      reads=[("ps", ops_bank), ("bcs", k)], writes=[("ost", k)])
                p.dma(dst, ost[:], reads=[("ost", k)], writes=[("o_d", h, s, id(dst) % 997)])
            else:
                if first:
                    p.dve(lambda e: e.tensor_tensor(out=acc, in0=ps[0:64, :], in1=bcs[:], op=ALU.mult),
                          reads=[("ps", ops_bank), ("bcs", k)], writes=[acc_tok])
                else:
                    p.dve(lambda e: e.tensor_tensor(out=bcs[:], in0=ps[0:64, :], in1=bcs[:], op=ALU.mult),
                          reads=[("ps", ops_bank), ("bcs", k)], writes=[("bcs", k)])
                    p.dve(lambda e: e.tensor_tensor(out=acc, in0=acc, in1=bcs[:], op=ALU.add),
                          reads=[acc_tok, ("bcs", k)], writes=[acc_tok])
                if last:
                    p.dve(lambda e: e.tensor_copy(out=ost[:], in_=acc), reads=[acc_tok], writes=[("ost", k)])
                    p.dma(dst, ost[:], reads=[("ost", k)], writes=[("o_d", h, s, id(dst) % 997)])
        return part1, part2

    def run_softmax(items, KT, ktok, dk, V, vtok, qh, qtok, pending, hook=None):
        n = len(items)

        def stage1(i):
            it = items[i]
            if "pre" in it:
                it["pre"]()
            b = i % 2
            ps = kb.ps(b)
            ex = it["extras"]
            s, j = it["s"], it["j"]
            qr = it["qrhs"] if "qrhs" in it else A.QT[0:dk, qh, s * 512:(s + 1) * 512]
            p.pe(lambda e: e.matmul(ps[:], lhsT=KT[0:dk, j * 128:(j + 1) * 128], rhs=qr,
                                    start=True, stop=(len(ex) == 0)),
                 reads=[ktok, qtok] + list(it.get("qreads", ())), writes=[("ps", b)])
            for xi, (lh, rh, toks) in enumerate(ex):
                p.pe(lambda e, lh=lh, rh=rh, xi=xi: e.matmul(ps[:], lhsT=lh, rhs=rh, start=False, stop=(xi == len(ex) - 1)),
                     reads=list(toks), writes=[("ps", b)])
            if "pdst" in it:
                P, ptok = it["pdst"]
            else:
                pk = A.cnt["P"] % 3
                A.cnt["P"] += 1
                P, ptok = A.P[pk][:], ("P", pk)
            it["P"], it["ptok"] = P, ptok
            p.act(lambda e: e.activation(out=P, in_=ps[:], func=AF.Exp), reads=[("ps", b)], writes=[ptok])

        def stage2(i):
            it = items[i]
            P, ptok = it["P"], it["ptok"]
            ob = it["obank"]
            po = kb.ps(ob)
            jv = it.get("jv", it["j"])
            p.pe(lambda e: e.matmul(po[0:65, :], lhsT=V[:, jv, 0:65], rhs=P, start=it["first"], stop=it["last"]),
                 reads=[vtok, ptok], writes=[("ps", ob)])
            if it["last"]:
                p1, p2 = it["fin"]
                p1()
                pending.append([2, p2])

        for i in range(n + 1):
            if i < n:
                stage1(i)
            if i >= 1:
                stage2(i - 1)
            for pd in list(pending):
                pd[0] -= 1
                if pd[0] <= 0:
                    pd[1]()
                    pending.remove(pd)

    def flush(pending):
        for pd in pending:
            pd[1]()
        pending.clear()

    A.load_K, A.load_V, A.load_Q = load_K, load_V, load_Q
    A.finalize_plain, A.run_softmax, A.flush = finalize_plain, run_softmax, flush
    oT = io["oT_d"]

    if "mla" in branches:
        pending = []
        for h in range(4):
            KT, ktok = load_K([(io["kml_f"].rows(h * 64, h * 64 + 64), 0), (io["krl_f"].rows(0, 32), 64)], 96)
            V, vtok = load_V(io["vml_f"], h * 64)
            qtok = load_Q(io["qml_d"][h], h, 96)
            items = []
            for s in range(NSLOT):
                nkb = 8 * s + 8
                ob = 4 + (s % 2)
                for j in range(nkb):
                    jj = j - 8 * s
                    ex = []
                    if jj >= 0:
                        ex.append((c["identb"][:], A.cm[:, jj, :], ["identb", "cm"]))
                    it = dict(j=j, s=s, extras=ex, first=(j == 0), last=(j == nkb - 1), obank=ob)
                    if j == nkb - 1:
                        it["fin"] = finalize_plain(ob, oT[2, h * 64:(h + 1) * 64, s * 512:(s + 1) * 512], h, s)
                    items.append(it)
            run_softmax(items, KT, ktok, 96, V, vtok, h, qtok, pending)
        flush(pending)

    if "mem" in branches:
        pending = []
        memst = kb.sb("memst", [128, 2, 1024], F32)
        memT = kb.sb("memT", [128, 8, 256], BF16)
        wmk = kb.sb("wmk", [128, 8, 512], BF16)
        KTm = kb.sb("KTm", [64, 4, 256], BF16)
        Vm = kb.sb("Vm", [128, 2, 4, 65], BF16)
        p.dma(memst[:], io["mem"].rearrange("(n p) c -> p n c", p=128), writes=["memst"])
        p.dma(wmk[:], w["w_mem_kv"].rearrange("(f p) c -> p f c", p=128), writes=["wmk"], q="pool")
        p.pool(lambda e: e.memset(Vm[:, :, :, 64:65], 1.0), writes=["Vm"])
        for kc in range(2):
            for half in range(2):
                ps = kb.ps(7)
                for jx in range(4):
                    f = half * 4 + jx
                    p.pe(lambda e, ps=ps, kc=kc, f=f, jx=jx: e.transpose(out=ps[:, jx * 128:(jx + 1) * 128], in_=memst[:, kc, f * 128:(f + 1) * 128], identity=c["identf"][:]),
                         reads=["memst", "identf"], writes=[("ps", 7)])
                p.dve(lambda e, ps=ps, kc=kc, half=half: e.tensor_copy(out=memT[:, half * 4:half * 4 + 4, kc * 128:(kc + 1) * 128], in_=ps[:].rearrange("p (j t) -> p j t", j=4)),
                      reads=[("ps", 7)], writes=["memT"])
        for h in range(4):
            ps = kb.ps(7)
            for f in range(8):
                p.pe(lambda e, ps=ps, f=f, h=h: e.matmul(ps[0:64, 0:256], lhsT=wmk[:, f, h * 64:(h + 1) * 64], rhs=memT[:, f, :], start=(f == 0), stop=(f == 7)),
                     reads=["wmk", "memT"], writes=[("ps", 7)])
            p.dve(lambda e, ps=ps, h=h: e.tensor_copy(out=KTm[:, h, :], in_=ps[0:64, 0:256]), reads=[("ps", 7)], writes=["KTm"])
        for kc in range(2):
            ps = kb.ps(7)
            for f in range(8):
                p.pe(lambda e, ps=ps, f=f, kc=kc: e.matmul(ps[:, 0:256], lhsT=memT[:, f, kc * 128:(kc + 1) * 128], rhs=wmk[:, f, 256:512], start=(f == 0), stop=(f == 7)),
                     reads=["wmk", "memT"], writes=[("ps", 7)])
            p.dve(lambda e, ps=ps, kc=kc: e.tensor_copy(out=Vm[:, kc, :, 0:64], in_=ps[:, 0:256].rearrange("p (h d) -> p h d", h=4)),
                  reads=[("ps", 7)], writes=["Vm"])
        for h in range(4):
            qtok = load_Q(io["qme_d"][h * 64:(h + 1) * 64, :], h, 64)
            items = []
            for s in range(NSLOT):
                ob = 4 + (s % 2)
                for j in range(2):
                    it = dict(j=j, s=s, extras=[], first=(j == 0), last=(j == 1), obank=ob)
                    if j == 1:
                        it["fin"] = finalize_plain(ob, oT[4, h * 64:(h + 1) * 64, s * 512:(s + 1) * 512], h, s)
                    items.append(it)
            run_softmax(items, KTm[:, h, :], "KTm", 64, Vm[:, :, h, :], "Vm", h, qtok, pending)
        flush(pending)

    if "moba" in branches:
        pending = []
        vbt = kb.sb("vbt", [128, 8, 4, 32], F32)
        ownt = kb.sb("ownt", [128, 8, 4, 32], F32)
        kmf = kb.sb("kmf", [64, 32], F32)
        kmb = kb.sb("kmb", [64, 32], BF16)
        gsv = kb.sb("gsv", [128, 4, 32], F32)
        m8 = kb.sb("m8", [128, 4, 8], F32)
        m1p = kb.sb("m1p", [128, 4, 128], F32)
        m1 = m1p[:, :, 64:96]
        m2 = kb.sb("m2", [128, 4, 32], F32)
        p.pool(lambda e: e.memset(m1p[:], 0.0), writes=["m1"])
        p.dma(vbt[:], io["c_vb"], writes=["vbt"])
        p.dma(ownt[:], io["c_own"], writes=["ownt"])
        for h in range(4):
            KT, ktok = load_K([(io["kmo_f"].rows(h * 64, h * 64 + 64), 0), (("full", io["c_tm"]), 64)], 96, aug=io["c_kaug"])
            V, vtok = load_V(io["vmo_f"], h * 64)
            qtok = ("QT", h)
            p.dma(A.QT[0:64, h, :], io["qmo_d"][h * 64:(h + 1) * 64, :], writes=[qtok])
            p.dma(A.QT[96:100, h, :], io["c_qaug"][h], writes=[qtok])
            p.dve(lambda e, KT=KT: e.tensor_reduce(out=kmf[:], in_=KT[0:64, :].rearrange("p (n k) -> p n k", k=256), axis=AX.X, op=ALU.add),
                  reads=[ktok], writes=["kmf"])
            p.dve(lambda e: e.tensor_scalar_mul(out=kmb[:], in0=kmf[:], scalar1=1.0 / 256.0), reads=["kmf"], writes=["kmb"])

            def make_pre(s, h=h, qtok=qtok):
                def pre():
                    ps = kb.ps(7)
                    for qb in range(4):
                        c0 = s * 512 + qb * 128
                        p.pe(lambda e, qb=qb, c0=c0: e.matmul(ps[:, qb * 32:(qb + 1) * 32], lhsT=A.QT[0:64, h, c0:c0 + 128], rhs=kmb[:], start=True, stop=True),
                             reads=[qtok, "kmb"], writes=[("ps", 7)])
                    p.dve(lambda e: e.tensor_tensor(out=gsv[:], in0=ps[:, 0:128].rearrange("p (a b) -> p a b", a=4), in1=vbt[:, s, :, :], op=ALU.add),
                          reads=[("ps", 7), "vbt"], writes=["gsv"])
                    for qb in range(4):
                        p.dve(lambda e, qb=qb: e.max(out=m8[:, qb, :], in_=gsv[:, qb, :]), reads=["gsv"], writes=["m8"])
                    for qb in range(4):
                        p.dve(lambda e, qb=qb: e.tensor_scalar(out=m1[:, qb, :], in0=gsv[:, qb, :], scalar1=m8[:, qb, 2:3], scalar2=None, op0=ALU.is_ge),
                              reads=["gsv", "m8"], writes=["m1"])
                    p.dve(lambda e: e.tensor_scalar(out=m2[:], in0=gsv[:], scalar1=-1e29, scalar2=None, op0=ALU.is_gt), reads=["gsv"], writes=["m2"])
                    p.dve(lambda e: e.tensor_tensor(out=m1, in0=m1, in1=m2[:], op=ALU.mult), reads=["m1", "m2"], writes=["m1"])
                    p.dve(lambda e: e.tensor_tensor(out=m1, in0=m1, in1=ownt[:, s, :, :], op=ALU.add), reads=["m1", "ownt"], writes=["m1"])
                    p.dve(lambda e: e.tensor_scalar(out=m1, in0=m1, scalar1=1.0, scalar2=-MASKV, op0=ALU.subtract, op1=ALU.mult),
                          reads=["m1"], writes=["m1"])
                    ps2 = kb.ps(7)
                    for qb in range(4):
                        p.pe(lambda e, qb=qb: e.transpose(out=ps2[:, qb * 128:(qb + 1) * 128], in_=m1p[:, qb, :], identity=c["identf"][:]),
                             reads=["m1", "identf"], writes=[("ps", 7)])
                    p.act(lambda e: e.copy(out=A.QT[64:96, h, s * 512:(s + 1) * 512], in_=ps2[64:96, :]), reads=[("ps", 7)], writes=[("QTs", h, s)])
                return pre

            items = []
            for s in range(NSLOT):
                nkb = 8 * s + 8
                ob = 4 + (s % 2)
                for j in range(nkb):
                    jj = j - 8 * s
                    ex = []
                    if jj >= 0:
                        ex.append((c["identb"][:], A.cm[:, jj, :], ["identb", "cm"]))
                    it = dict(j=j, s=s, extras=ex, first=(j == 0), last=(j == nkb - 1), obank=ob, qreads=[("QTs", h, s)])
                    if j == 0:
                        it["pre"] = make_pre(s)
                    if j == nkb - 1:
                        it["fin"] = finalize_plain(ob, oT[1, h * 64:(h + 1) * 64, s * 512:(s + 1) * 512], h, s)
                    items.append(it)
            run_softmax(items, KT, ktok, 100, V, vtok, h, qtok, pending)
        flush(pending)

    if "sb" in branches:
        nui = kb.sb("nui", [128, 128], BF16)
        negone = kb.sb("negone", [1, 128], BF16)
        p.dma(nui[:], io["c_nui"], writes=["nui"])
        p.pool(lambda e: e.memset(negone[:], -1.0), writes=["negone"])
        E = [kb.sb("E", [128, 512], F32) for _ in range(2)]
        SP = [kb.sb("SP", [128, 512], BF16) for _ in range(2)]
        AB = [kb.sb("AB", [128, 512], BF16) for _ in range(2)]
        carf = [kb.sb("carf", [1, 512], F32) for _ in range(2)]
        carb = [kb.sb("carb", [1, 512], BF16) for _ in range(2)]
        sbo = [kb.sb("sbo", [64, 512], BF16) for _ in range(2)]
        one_ap = c["cstf"][:, 3:4]
        for hp in range(2):
            hs = (2 * hp, 2 * hp + 1)
            KTs, ktoks, Vs, vtoks, qtoks = [], [], [], [], []
            for h in hs:
                KT, ktok = load_K([(io["ksb_f"].rows(h * 64, h * 64 + 64), 0)], 64)
                V, vtok = load_V(io["vsb_f"], h * 64)
                qtok = load_Q(io["qsb_d"][h * 64:(h + 1) * 64, :], h, 64)
                KTs.append(KT); ktoks.append(ktok); Vs.append(V); vtoks.append(vtok); qtoks.append(qtok)
            merged = []
            for s_ in range(NSLOT):
                nkb = 8 * s_ + 8
                for j in range(nkb - 1, -1, -1):
                    for st in range(2):
                        merged.append(dict(j=j, s=s_, st=st, h=hs[st], first=(j == nkb - 1), last=(j == 0)))
            for i, it in enumerate(merged):
                it["i"] = i

            def qk(it, bank, more):
                ps = kb.ps(bank)
                j, s_, st, h = it["j"], it["s"], it["st"], it["h"]
                jj = j - 8 * s_
                KT = KTs[st]
                p.pe(lambda e: e.matmul(ps[:], lhsT=KT[0:64, j * 128:(j + 1) * 128], rhs=A.QT[0:64, h, s_ * 512:(s_ + 1) * 512],
                                        start=True, stop=(jj < 0 and not more)),
                     reads=[ktoks[st], qtoks[st]], writes=[("ps", bank)])
                if jj >= 0:
                    p.pe(lambda e: e.matmul(ps[:], lhsT=c["identb"][:], rhs=A.cms[:, jj, :], start=False, stop=(not more)),
                         reads=["identb", "cms"], writes=[("ps", bank)])
                return ps

            def s1(it):
                k = it["i"] % 2
                b4 = it["i"] % 4
                ps = qk(it, b4, False)
                p.act(lambda e: e.activation(out=E[k][:], in_=ps[:], func=AF.Exp), reads=[("ps", b4)], writes=[("E", k)])
                p.act(lambda e: e.activation(out=SP[k][:], in_=E[k][:], func=AF.Ln, bias=one_ap), reads=[("E", k), "cst"], writes=[("SP", k)])

            def s2a(it):
                k = it["i"] % 2
                b4 = it["i"] % 4
                st = it["st"]
                ps = kb.ps(b4)
                fin_carry = not it["first"]
                p.pe(lambda e: e.matmul(ps[:], lhsT=nui[:], rhs=SP[k][:], start=False, stop=(not fin_carry)),
                     reads=["nui", ("SP", k)], writes=[("ps", b4)])
                if fin_carry:
                    p.pe(lambda e: e.matmul(ps[:], lhsT=negone[0:1, :], rhs=carb[st][0:1, :], start=False, stop=True),
                         reads=["negone", ("carb", st)], writes=[("ps", b4)])
                if not it["last"]:
                    pc = kb.ps(6 + st)
                    p.pe(lambda e: e.matmul(pc[0:1, :], lhsT=c["onesb"][:, 0:1], rhs=SP[k][:], start=True, stop=True),
                         reads=["onesb", ("SP", k)], writes=[("ps", 6 + st)])
                    if it["first"]:
                        p.dve(lambda e: e.tensor_copy(out=carf[st][:], in_=pc[0:1, :]), reads=[("ps", 6 + st)], writes=[("carf", st)])
                    else:
                        p.dve(lambda e: e.tensor_tensor(out=carf[st][:], in0=carf[st][:], in1=pc[0:1, :], op=ALU.add),
                              reads=[("ps", 6 + st), ("carf", st)], writes=[("carf", st)])
                    p.dve(lambda e: e.tensor_copy(out=carb[st][:], in_=carf[st][:]), reads=[("carf", st)], writes=[("carb", st)])
                p.act(lambda e: e.activation(out=AB[k][:], in_=ps[:], func=AF.Exp), reads=[("ps", b4)], writes=[("AB", k)])

            def s2b(it):
                k = it["i"] % 2
                st, j, s_, h = it["st"], it["j"], it["s"], it["h"]
                po = kb.ps(4 + st)
                V = Vs[st]
                p.pe(lambda e: e.matmul(po[0:64, :], lhsT=V[:, j, 0:64], rhs=AB[k][:], start=it["first"], stop=it["last"]),
                     reads=[vtoks[st], ("AB", k)], writes=[("ps", 4 + st)])
                if it["last"]:
                    p.dve(lambda e: e.tensor_copy(out=sbo[st][:], in_=po[0:64, :]), reads=[("ps", 4 + st)], writes=[("sbo", st)])
                    p.dma(oT[0, h * 64:(h + 1) * 64, s_ * 512:(s_ + 1) * 512], sbo[st][:], reads=[("sbo", st)], writes=[("o_sb", h, s_)])

            n = len(merged)
            for i in range(n + 2):
                if i < n:
                    s1(merged[i])
                if 1 <= i <= n:
                    s2a(merged[i - 1])
                if i >= 2:
                    s2b(merged[i - 2])

    if "nsa" in branches:
        pending = []
        QS = kb.sb("QS", [128, 4, 4, 512], BF16)
        OV = kb.sb("OV", [128, 4, 129], BF16)
        pm = kb.sb("pm", [128, 3, 512], BF16)
        wm = kb.sb("wm", [128, 12, 512], BF16)
        wphi = kb.sb("wphi", [128, 32, 128], BF16)
        w2 = kb.sb("w2", [128, 2, 64], BF16)
        peT = kb.sb("peT", [128, 32], BF16)
        peb = kb.sb("peb", [128, 2], F32)
        gx = kb.sb("gx", [128, 512], F32)
        gt_ = kb.sb("gtmp", [128, 512], F32)
        gact = [kb.sb("gact", [128, 512], BF16) for _ in range(2)]
        kcT = kb.sb("kcT", [68, 512], BF16)
        Vc = kb.sb("Vc", [128, 4, 65], BF16)
        psave = kb.sb("psave", [128, 4, 4, 512], BF16)
        gts = [kb.sb("gts", [65, 512], F32) for _ in range(4)]
        nacc = kb.sb("nacc", [64, 4, 512], F32)
        impacc = kb.sb("impacc", [128, 4, 128], F32)
        rdn = kb.sb("rdn", [128, 2], F32)
        selm = [kb.sb("selm", [128, 4, 128], F32) for _ in range(2)]
        selc = [kb.sb("selc", [128, 4, 128], F32) for _ in range(2)]
        scr2 = kb.sb("scr2", [128, 4, 128], F32)
        m16 = kb.sb("m16", [128, 4, 16], F32)
        selvp = kb.sb("selvp", [128, 4, 256], F32)
        selv = selvp[:, :, 64:192]
        gcnt = {"g": 0}
        p.pool(lambda e: e.memset(selvp[:], 0.0), writes=["selv"])
        p.dma(OV[:], io["c_ov"], writes=["OV"])
        p.dma(pm[:], io["c_pm"], writes=["pm"])
        p.dma(wm[:], io["c_wm"], writes=["wm"])
        p.dma(wphi[0:64, :, :], w["w_phi_k1"].rearrange("(t d) h -> d t h", d=64), writes=["wphi"], q="pool")
        p.dma(wphi[64:128, :, :], w["w_phi_v1"].rearrange("(t d) h -> d t h", d=64), writes=["wphi"], q="pool")
        p.dma(w2[:, 0, :], w["w_phi_k2"], writes=["w2"], q="pool")
        p.dma(w2[:, 1, :], w["w_phi_v2"], writes=["w2"], q="pool")
        p.dma(peT[0:64, :], w["nsa_pe"].rearrange("t d -> d t"), writes=["peT"], q="pool", slow=True)
        p.dma(peT[64:128, :], w["nsa_pe"].rearrange("t d -> d t"), writes=["peT"], q="pool", slow=True)
        kcv, kcvtok = load_K([(io["kcv_f"].rows(0, 128), 0)], 128)
        p.pool(lambda e: e.memset(kcT[:], 0.0), writes=["kcT"])
        p.pool(lambda e: e.memset(Vc[:, :, 64:65], 1.0), writes=["Vc"])
        for which in range(2):
            lo = which * 64
            pb = kb.ps(7)
            for t in range(32):
                p.pe(lambda e, t=t, lo=lo, pb=pb: e.matmul(pb[:, 0:1], lhsT=wphi[lo:lo + 64, t, :], rhs=peT[lo:lo + 64, t:t + 1], start=(t == 0), stop=(t == 31)),
                     reads=["wphi", "peT"], writes=[("ps", 7)])
            p.dve(lambda e, pb=pb, which=which: e.tensor_copy(out=peb[:, which:which + 1], in_=pb[:, 0:1]), reads=[("ps", 7)], writes=["peb"])
            ph = kb.ps(6)
            for t in range(32):
                p.pe(lambda e, t=t, lo=lo, ph=ph: e.matmul(ph[:, 0:511], lhsT=wphi[lo:lo + 64, t, :], rhs=kcv[lo:lo + 64, t:t + 16 * 510 + 1:16], start=(t == 0), stop=(t == 31)),
                     reads=["wphi", kcvtok], writes=[("ps", 6)])
            ga = gact[which]
            p.act(lambda e, ph=ph, which=which: e.activation(out=gx[:, 0:511], in_=ph[:, 0:511], func=AF.Identity, bias=peb[:, which:which + 1]),
                  reads=[("ps", 6), "peb"], writes=["gx"])
            p.dve(lambda e: e.tensor_tensor(out=gt_[:, 0:511], in0=gx[:, 0:511], in1=gx[:, 0:511], op=ALU.mult), reads=["gx"], writes=["gtmp"])
            p.dve(lambda e: e.tensor_scalar(out=gt_[:, 0:511], in0=gt_[:, 0:511], scalar1=0.044715, scalar2=1.0, op0=ALU.mult, op1=ALU.add), reads=["gtmp"], writes=["gtmp"])
            p.dve(lambda e: e.tensor_tensor(out=gt_[:, 0:511], in0=gt_[:, 0:511], in1=gx[:, 0:511], op=ALU.mult), reads=["gtmp", "gx"], writes=["gtmp"])
            p.act(lambda e: e.activation(out=gt_[:, 0:511], in_=gt_[:, 0:511], func=AF.Tanh, scale=0.7978845608028654), reads=["gtmp"], writes=["gtmp"])
            p.dve(lambda e: e.tensor_scalar(out=gt_[:, 0:511], in0=gt_[:, 0:511], scalar1=1.0, scalar2=0.5, op0=ALU.add, op1=ALU.mult), reads=["gtmp"], writes=["gtmp"])
            p.pool(lambda e, ga=ga: e.memset(ga[:], 0.0), writes=[("gact", which)])
            p.dve(lambda e, ga=ga: e.tensor_tensor(out=ga[:, 0:511], in0=gt_[:, 0:511], in1=gx[:, 0:511], op=ALU.mult), reads=["gtmp", "gx"], writes=[("gact", which)])
        pk_ = kb.ps(7)
        p.pe(lambda e: e.matmul(pk_[0:64, :], lhsT=w2[:, 0, :], rhs=gact[0][:], start=True, stop=True), reads=["w2", ("gact", 0)], writes=[("ps", 7)])
        p.dve(lambda e: e.tensor_copy(out=kcT[0:64, 0:511], in_=pk_[0:64, 0:511]), reads=[("ps", 7)], writes=["kcT"])
        p.dma(kcT[64:68, :], io["c_caug"], writes=["kcT"])
        pv_ = kb.ps(6)
        for cc in range(4):
            p.pe(lambda e, cc=cc: e.matmul(pv_[:, cc * 64:(cc + 1) * 64], lhsT=gact[1][:, cc * 128:(cc + 1) * 128], rhs=w2[:, 1, :], start=True, stop=True),
                 reads=["w2", ("gact", 1)], writes=[("ps", 6)])
        p.dve(lambda e: e.tensor_copy(out=Vc[:, :, 0:64], in_=pv_[:, 0:256].rearrange("p (c d) -> p c d", c=4)), reads=[("ps", 6)], writes=["Vc"])

        KsT, kstok = load_K([(io["ksl_f"].rows(0, 64), 0), (("full", io["c_g32"]), 64)], 96, aug=io["c_kaug"])
        KwT, kwtok = load_K([(io["kwi_f"].rows(0, 64), 0)], 64, aug=io["c_kaug"])
        Vs, vstok = load_V(io["vsw_f"], 0)
        Vw, vwtok = load_V(io["vsw_f"], 64)
        qtoks = [load_Q(io["qns_d"][h * 64:(h + 1) * 64, :], h, 64, aug=io["c_qaug"][h]) for h in range(4)]

        def gate_row(h, br, s):
            k = gcnt["g"] % 4
            gcnt["g"] += 1
            g = gts[k]
            p.dma(g[64:65, :], io["gns_d"][3 * h + br:3 * h + br + 1, s * 512:(s + 1) * 512], writes=[("gts", k)])
            return (g[:].rearrange("p (o n) -> p o n", o=1), ("gts", k), 0)

        for s in range(NSLOT):
            sl = slice(s * 512, (s + 1) * 512)
            ncmp = s // 2 + 1
            dst = lambda h: oT[3, h * 64:(h + 1) * 64, sl]
            p.dma(selm[s % 2][:], io["c_selm"][s], writes=[("selm", s % 2)])
            p.dma(selc[s % 2][:], io["c_selc"][s], writes=[("selc", s % 2)])
            for h in range(4):
                items = []
                for cc in range(ncmp):
                    idx = s - 2 * cc + 6
                    ex = []
                    if idx <= 8:
                        ex.append((c["identb"][:], pm[:, idx - 6, :], ["identb", "pm"]))
                    it = dict(j=cc, s=s, extras=ex, first=(cc == 0), last=(cc == ncmp - 1), obank=4 + (h % 2),
                              pdst=(psave[:, h, cc, :], ("psave", h, cc)))
                    if cc == ncmp - 1:
                        it["fin"] = finalize_plain(4 + (h % 2), dst(h), h, s, gate=gate_row(h, 0, s), acc=nacc[:, h, :], acc_tok=("nacc", h), first=True, last=False)
                    items.append(it)
                run_softmax(items, kcT, "kcT", 68, Vc, "Vc", h, qtoks[h], pending)
            flush(pending)
            for qb in range(4):
                for h in range(4):
                    bk = 6 + ((qb * 4 + h) % 2)
                    pi = kb.ps(bk)
                    for cc in range(ncmp):
                        p.pe(lambda e, pi=pi, h=h, cc=cc, qb=qb: e.matmul(pi[:, 0:129], lhsT=psave[:, h, cc, qb * 128:(qb + 1) * 128], rhs=OV[:, cc, :],
                                                                          start=(cc == 0), stop=(cc == ncmp - 1)),
                             reads=[("psave", h, cc), "OV"], writes=[("ps", bk)])
                    p.dve(lambda e, pi=pi: e.tensor_scalar(out=rdn[:, 0:1], in0=pi[:, 128:129], scalar1=1e-30, scalar2=None, op0=ALU.max),
                          reads=[("ps", bk)], writes=["rdn"])
                    p.dve(lambda e: e.reciprocal(out=rdn[:, 1:2], in_=rdn[:, 0:1]), reads=["rdn"], writes=["rdn"])
                    if h == 0:
                        p.dve(lambda e, pi=pi, qb=qb: e.tensor_scalar(out=impacc[:, qb, :], in0=pi[:, 0:128], scalar1=rdn[:, 1:2], scalar2=None, op0=ALU.mult),
                              reads=[("ps", bk), "rdn"], writes=["impacc"])
                    else:
                        p.dve(lambda e, pi=pi, qb=qb: e.scalar_tensor_tensor(out=impacc[:, qb, :], in0=pi[:, 0:128], scalar=rdn[:, 1:2], in1=impacc[:, qb, :],
                                                                             op0=ALU.mult, op1=ALU.add),
                              reads=[("ps", bk), "rdn", "impacc"], writes=["impacc"])
            sm, sc_ = selm[s % 2], selc[s % 2]
            p.dve(lambda e, sm=sm: e.tensor_tensor(out=impacc[:], in0=impacc[:], in1=sm[:], op=ALU.mult), reads=["impacc", ("selm", s % 2)], writes=["impacc"])
            p.dve(lambda e, sc_=sc_: e.tensor_tensor(out=impacc[:], in0=impacc[:], in1=sc_[:], op=ALU.add), reads=["impacc", ("selc", s % 2)], writes=["impacc"])
            for qb in range(4):
                p.dve(lambda e, qb=qb: e.max(out=m16[:, qb, 0:8], in_=impacc[:, qb, :]), reads=["impacc"], writes=["m16"])
                p.dve(lambda e, qb=qb: e.match_replace(out=scr2[:, qb, :], in_to_replace=m16[:, qb, 0:8], in_values=impacc[:, qb, :], imm_value=-3.0e38),
                      reads=["impacc", "m16"], writes=["scr2"])
                p.dve(lambda e, qb=qb: e.max(out=m16[:, qb, 8:16], in_=scr2[:, qb, :]), reads=["scr2"], writes=["m16"])
                p.dve(lambda e, qb=qb: e.tensor_scalar(out=selv[:, qb, :], in0=impacc[:, qb, :], scalar1=m16[:, qb, 15:16], scalar2=None, op0=ALU.is_ge),
                      reads=["impacc", "m16"], writes=["selv"])
            p.dve(lambda e: e.tensor_scalar(out=scr2[:], in0=impacc[:], scalar1=-5e29, scalar2=None, op0=ALU.is_gt), reads=["impacc"], writes=["scr2"])
            p.dve(lambda e: e.tensor_tensor(out=selv, in0=selv, in1=scr2[:], op=ALU.mult), reads=["selv", "scr2"], writes=["selv"])
            p.dve(lambda e: e.tensor_scalar(out=selv, in0=selv, scalar1=1.0, scalar2=-MASKV, op0=ALU.subtract, op1=ALU.mult), reads=["selv"], writes=["selv"])
            for h in range(4):
                items = []
                js = [j for j in range(8 * s - 4, 8 * s + 8) if j >= 0]
                for j in js:
                    jrel = j - 8 * s
                    it = dict(j=j, s=s, extras=[(c["identb"][:], wm[:, jrel + 4, :], ["identb", "wm"])], first=(j == js[0]), last=(j == js[-1]), obank=4 + (h % 2))
                    if j == js[-1]:
                        it["fin"] = finalize_plain(4 + (h % 2), dst(h), h, s, gate=gate_row(h, 2, s), acc=nacc[:, h, :], acc_tok=("nacc", h), first=False, last=False)
                    items.append(it)
                run_softmax(items, KwT, kwtok, 68, Vw, vwtok, h, qtoks[h], pending)
            flush(pending)
            nkb = 8 * s + 8
            ngi = (nkb - 1) // 16 + 1
            for gi in range(ngi):
                pt = kb.ps(7)
                for qb in range(4):
                    p.pe(lambda e, qb=qb, gi=gi, pt=pt: e.transpose(out=pt[:, qb * 128:(qb + 1) * 128], in_=selvp[:, qb, 32 * gi:32 * gi + 128], identity=c["identf"][:]),
                         reads=["selv", "identf"], writes=[("ps", 7)])
                p.act(lambda e, gi=gi, pt=pt: e.copy(out=QS[64:96, 0, gi, :], in_=pt[64:96, :]), reads=[("ps", 7)], writes=[("QS", gi)])
                for h in range(4):
                    if h > 0:
                        p.pool(lambda e, gi=gi, h=h: e.tensor_copy(out=QS[64:96, h, gi, :], in_=QS[64:96, 0, gi, :]), reads=[("QS", gi)], writes=[("QS", gi)])
                    p.pool(lambda e, gi=gi, h=h: e.tensor_copy(out=QS[0:64, h, gi, :], in_=A.QT[0:64, h, sl]), reads=[qtoks[h]], writes=[("QS", gi)])
                    p.dma(QS[96:100, h, gi, :], io["c_qaug"][h][:, sl], writes=[("QS", gi)])
            for h in range(4):
                items = []
                for j in range(nkb):
                    jj = j - 8 * s
                    ex = []
                    if jj >= 0:
                        ex.append((c["identb"][:], A.cm[:, jj, :], ["identb", "cm"]))
                    it = dict(j=j, s=s, extras=ex, first=(j == 0), last=(j == nkb - 1), obank=4 + (h % 2),
                              qrhs=QS[0:100, h, j // 16, :], qreads=[("QS", j // 16)])
                    if j == nkb - 1:
                        it["fin"] = finalize_plain(4 + (h % 2), dst(h), h, s, gate=gate_row(h, 1, s), acc=nacc[:, h, :], acc_tok=("nacc", h), first=False, last=True)
                    items.append(it)
                run_softmax(items, KsT, kstok, 100, Vs, vstok, h, qtoks[h], pending)
            flush(pending)
    return A


B_WEIGHTS = {"w_mem_kv": [D, 512], "nsa_pe": [32, 64], "w_phi_k1": [2048, 128], "w_phi_k2": [128, 64],
             "w_phi_v1": [2048, 128], "w_phi_v2": [128, 64]}


def build_b(branches):
    kb = KB()
    io = {}
    for n in ("c_ident", "c_ones"):
        io[n] = kb.din(n, [128, 128])
    io["c_cst"] = kb.din("c_cst", [128, 8])
    for n, (shp, dt) in B_CONST_SHAPES.items():
        io[n] = kb.din(n, shp, dt)
    for n, (shp, dt) in B_INS.items():
        io[n] = kb.din(n, shp, dt)
    w = {n: kb.din(n, shp) for n, shp in B_WEIGHTS.items()}
    io["oT_d"] = kb.dout("oT_d", [5, 256, TOK], BF16)
    io["kml_f"] = KFull(io["kml_f"].rearrange("h d t -> (h d) t"))
    for n in ("ksb_f", "kmo_f", "krl_f", "kcv_f", "ksl_f", "kwi_f"):
        io[n] = KFull(io[n])
    for n in ("vsb_f", "vmo_f", "vml_f", "vsw_f"):
        io[n] = VFull(io[n])
    c = load_consts(kb, io)
    phase_b(kb, c, io, w, branches)
    return kb.finish()


def layer_norm_chunk(kb, c, v, vtok, gbc, bbc, out, otok, tmp):
    p = kb.p
    st, mv, sm = tmp["st"], tmp["mv"], tmp["sm"]
    for hf in range(2):
        p.dve(lambda e, hf=hf: e.bn_stats(out=st[:, hf, :], in_=v[:, hf * 512:(hf + 1) * 512]), reads=[vtok], writes=["lnst"])
    p.dve(lambda e: e.bn_aggr(out=mv[:], in_=st[:]), reads=["lnst"], writes=["lnmv"])
    p.act(lambda e: e.activation(out=sm[:, 0:1], in_=mv[:, 1:2], func=AF.Ln, bias=c["eps_ln"]), reads=["lnmv", "cst"], writes=["lnsm"])
    p.act(lambda e: e.activation(out=sm[:, 0:1], in_=sm[:, 0:1], func=AF.Exp, scale=-0.5), reads=["lnsm"], writes=["lnsm"])
    p.dve(lambda e: e.scalar_tensor_tensor(out=sm[:, 1:2], in0=mv[:, 0:1], scalar=-1.0, in1=sm[:, 0:1], op0=ALU.mult, op1=ALU.mult),
          reads=["lnmv", "lnsm"], writes=["lnsm2"])
    p.act(lambda e: e.activation(out=v[:], in_=v[:], func=AF.Identity, scale=sm[:, 0:1], bias=sm[:, 1:2]), reads=[vtok, "lnsm", "lnsm2"], writes=[vtok])
    p.dve(lambda e: e.tensor_tensor(out=v[:], in0=v[:], in1=gbc[:], op=ALU.mult), reads=[vtok, "lngb"], writes=[vtok])
    p.dve(lambda e: e.tensor_tensor(out=out[:], in0=v[:], in1=bbc[:], op=ALU.add), reads=[vtok, "lngb"], writes=[otok])


def phase_c1(kb, c, io, w):
    nc, p = kb.nc, kb.p
    wg = kb.sb("wg", [128, 5, 8, 1024], BF16)
    wbr = kb.sb("wbr", [128, 5, 2, 1024], BF16)
    wout = kb.sb("wout", [128, 8, 1024], BF16)
    bg = kb.sb("bg", [128, 5, 8], F32)
    gbc = kb.sb("gbc", [128, 1024], F32)
    bbc = kb.sb("bbc", [128, 1024], F32)
    wr = kb.sb("wr", [128, 8, 20], F32)
    brow = kb.sb("brow", [1, 20], F32)
    for i in range(5):
        for f in range(8):
            p.dma(wg[:, i, f, :], w["w_gate"][i, f * 128:(f + 1) * 128, :], writes=[("wg", i)], q="pool")
        p.dma(wbr[:, i, :, :], w["w_br"][i].rearrange("(j p) c -> p j c", p=128), writes=["wbr"], q="pool")
    p.dma(wout[:], w["w_out"].rearrange("(f p) c -> p f c", p=128), writes=["wout"], q="pool")
    p.dma(bg[:], w["b_gate"].rearrange("i (c p) -> p i c", p=128), writes=["bg"], slow=True)
    p.dma(gbc[:], w["ln1_g"].partition_broadcast(128), writes=["lngb"])
    p.dma(bbc[:], w["ln1_b"].partition_broadcast(128), writes=["lngb"])
    p.dma(wr[:, :, 0:4], w["w_rg"].rearrange("(f p) g -> p f g", p=128), writes=["wr"], slow=True)
    for g in range(4):
        p.dma(wr[:, :, 4 + 4 * g:8 + 4 * g], w["w_re"][g].rearrange("(f p) e -> p f e", p=128), writes=["wr"], slow=True)
    p.dma(brow[0:1, 0:4], w["b_rg"].rearrange("(o g) -> o g", o=1), writes=["brow"])
    p.dma(brow[0:1, 4:20], w["b_re"].rearrange("(o g) e -> o (g e)", o=1), writes=["brow"])

    hTt = [kb.sb("hTt", [128, 8, 512], BF16) for _ in range(2)]
    oTt = [kb.sb("oTt", [128, 5, 2, 512], BF16)] * 2
    mT = [kb.sb("mT", [128, 8, 512], BF16) for _ in range(2)]
    sg = [kb.sb("sg", [128, 512], F32) for _ in range(2)]
    acc = kb.sb("macc", [128, 512], F32)
    tmpm = kb.sb("tmpm", [128, 512], F32)
    hch = [kb.sb("hch", [128, 1024], F32)] * 2
    vch = [kb.sb("vch", [128, 1024], F32)] * 2
    h1c = [kb.sb("h1c", [128, 1024], F32) for _ in range(2)]
    h1Tf = [kb.sb("h1Tf", [128, 8, 128], F32) for _ in range(2)]
    h1Tb = [kb.sb("h1Tb", [128, 8, 128], BF16) for _ in range(2)]
    lnt = {"st": kb.sb("lnst", [128, 2, 6], F32), "mv": kb.sb("lnmv", [128, 2], F32), "sm": kb.sb("lnsm", [128, 2], F32)}
    lg = kb.sb("lg", [128, 20], F32)
    r1 = kb.sb("r1", [128, 8], F32)
    goh = kb.sb("goh", [128, 4], F32)
    el = kb.sb("el", [128, 4], F32)
    ee = kb.sb("ee", [128, 4], F32)
    ee2 = kb.sb("ee2", [128, 4], F32)
    gd = [kb.sb("gd", [128, 16], F32) for _ in range(2)]
    bk = {"n": 0}

    def nbank():
        b = bk["n"] % 6
        bk["n"] += 1
        return b

    pend = []

    def do_slot(s):
        d2 = s % 2
        tsl = slice(s * 512, (s + 1) * 512)
        ht, ot, mt = hTt[d2], oTt[d2], mT[d2]
        p.dma(ht[:], io["hT_d"].rearrange("(f p) t -> p f t", p=128)[:, :, tsl], writes=[("hTt", d2)])
        for i in range(5):
            p.dma(ot[:, i, :, :], io["oT_d"][i].rearrange("(j p) t -> p j t", p=128)[:, :, tsl], writes=["oTt"])
        for cc in range(8):
            csl = slice(cc * 128, (cc + 1) * 128)
            for i in range(5):
                bgt, bbr = nbank(), nbank()
                pg, pb = kb.ps(bgt), kb.ps(bbr)
                for f in range(8):
                    p.pe(lambda e, pg=pg, i=i, f=f, csl=csl: e.matmul(pg[:], lhsT=wg[:, i, f, csl], rhs=ht[:, f, :], start=(f == 0), stop=(f == 7)),
                         reads=[("wg", i), ("hTt", d2)], writes=[("ps", bgt)])
                for jc in range(2):
                    p.pe(lambda e, pb=pb, i=i, jc=jc, csl=csl: e.matmul(pb[:], lhsT=wbr[:, i, jc, csl], rhs=ot[:, i, jc, :], start=(jc == 0), stop=(jc == 1)),
                         reads=["wbr", "oTt"], writes=[("ps", bbr)])
                sgi = sg[i % 2]
                p.act(lambda e, sgi=sgi, pg=pg, i=i, cc=cc: e.activation(out=sgi[:], in_=pg[:], func=AF.Sigmoid, bias=bg[:, i, cc:cc + 1]),
                      reads=[("ps", bgt), "bg"], writes=[("sg", i % 2)])
                if i == 0:
                    p.dve(lambda e, sgi=sgi, pb=pb: e.tensor_tensor(out=acc[:], in0=sgi[:], in1=pb[:], op=ALU.mult),
                          reads=[("sg", i % 2), ("ps", bbr)], writes=["macc"])
                else:
                    p.dve(lambda e, sgi=sgi, pb=pb: e.tensor_tensor(out=tmpm[:], in0=sgi[:], in1=pb[:], op=ALU.mult),
                          reads=[("sg", i % 2), ("ps", bbr)], writes=["tmpm"])
                    if i < 4:
                        p.dve(lambda e: e.tensor_tensor(out=acc[:], in0=acc[:], in1=tmpm[:], op=ALU.add), reads=["macc", "tmpm"], writes=["macc"])
                    else:
                        p.dve(lambda e, cc=cc: e.tensor_tensor(out=mt[:, cc, :], in0=acc[:], in1=tmpm[:], op=ALU.add), reads=["macc", "tmpm"], writes=[("mT", d2)])
            for _ in range(2 if cc < 4 else 1):
                if pend:
                    pend.pop(0)()
        if "dbg_mt" in io and s == 0:
            p.dma(io["dbg_mt"], mt[:], reads=[("mT", d2)], writes=["dbg_mt"])
        def make_chunk(tc):
            gck = s * 4 + tc
            k2 = gck % 2
            rows = slice(gck * 128, (gck + 1) * 128)
            hc, vc, h1 = hch[k2], vch[k2], h1c[k2]
            tf, tb = h1Tf[k2], h1Tb[k2]
            gdt = gd[k2]
            rb = {}

            def stA():
                p.dma(hc[:], io["h_tok"][rows, :], writes=["hch"])
                for hf in range(2):
                    b = nbank()
                    ps = kb.ps(b)
                    for cc in range(8):
                        p.pe(lambda e, ps=ps, cc=cc, tc=tc, hf=hf: e.matmul(ps[:], lhsT=mt[:, cc, tc * 128:(tc + 1) * 128], rhs=wout[:, cc, hf * 512:(hf + 1) * 512],
                                                                          start=(cc == 0), stop=(cc == 7)),
                             reads=["wout", ("mT", d2)], writes=[("ps", b)])
                    p.dve(lambda e, ps=ps, hf=hf: e.scalar_tensor_tensor(out=vc[:, hf * 512:(hf + 1) * 512], in0=hc[:, hf * 512:(hf + 1) * 512], scalar=ALPHA, in1=ps[:],
                                                                          op0=ALU.mult, op1=ALU.add),
                          reads=["hch", ("ps", b)], writes=["vch"])
                layer_norm_chunk(kb, c, vc, "vch", gbc, bbc, h1, ("h1c", k2), lnt)
                p.dma(io["h1_d"][rows, :], h1[:], reads=[("h1c", k2)], writes=[("h1_d", gck)])

            def stB():
                for hf in range(2):
                    b = nbank()
                    ps = kb.ps(b)
                    for jx in range(4):
                        f = hf * 4 + jx
                        p.pe(lambda e, ps=ps, f=f, jx=jx: e.transpose(out=ps[:, jx * 128:(jx + 1) * 128], in_=h1[:, f * 128:(f + 1) * 128], identity=c["identf"][:]),
                             reads=[("h1c", k2), "identf"], writes=[("ps", b)])
                    p.act(lambda e, ps=ps, hf=hf: e.copy(out=tf[:, hf * 4:hf * 4 + 4, :], in_=ps[:].rearrange("p (j t) -> p j t", j=4)),
                          reads=[("ps", b)], writes=[("h1Tf", k2)])
                p.pool(lambda e: e.tensor_copy(out=tb[:], in_=tf[:]), reads=[("h1Tf", k2)], writes=[("h1Tb", k2)])
                p.dma(io["h1T_d"].rearrange("(f p) t -> p f t", p=128)[:, :, rows], tb[:], reads=[("h1Tb", k2)], writes=[("h1T_d", gck)])
                b = nbank()
                ps = kb.ps(b)
                for f in range(8):
                    p.pe(lambda e, ps=ps, f=f: e.matmul(ps[:, 0:20], lhsT=tf[:, f, :], rhs=wr[:, f, :], start=(f == 0), stop=False),
                         reads=[("h1Tf", k2), "wr"], writes=[("ps", b)])
                p.pe(lambda e, ps=ps: e.matmul(ps[:, 0:20], lhsT=c["onesf"][0:1, :], rhs=brow[0:1, :], start=False, stop=True),
                     reads=["onesf", "brow"], writes=[("ps", b)])
                p.dve(lambda e, ps=ps: e.tensor_copy(out=lg[:], in_=ps[:, 0:20]), reads=[("ps", b)], writes=["lg"])

            def stC():
                p.dve(lambda e: e.tensor_reduce(out=r1[:, 0:1], in_=lg[:, 0:4], axis=AX.X, op=ALU.max), reads=["lg"], writes=["r1a"])
                p.dve(lambda e: e.tensor_scalar(out=goh[:], in0=lg[:, 0:4], scalar1=r1[:, 0:1], scalar2=None, op0=ALU.is_equal), reads=["lg", "r1a"], writes=["goh"])
                p.dve(lambda e: e.tensor_scalar(out=ee[:], in0=lg[:, 0:4], scalar1=r1[:, 0:1], scalar2=None, op0=ALU.subtract), reads=["lg", "r1a"], writes=["ee"])
                p.act(lambda e: e.activation(out=ee[:], in_=ee[:], func=AF.Exp), reads=["ee"], writes=["ee"])
                p.dve(lambda e: e.tensor_reduce(out=r1[:, 1:2], in_=ee[:], axis=AX.X, op=ALU.add), reads=["ee"], writes=["r1b"])
                p.dve(lambda e: e.reciprocal(out=r1[:, 1:2], in_=r1[:, 1:2]), reads=["r1b"], writes=["r1b"])
                p.dve(lambda e: e.tensor_scalar(out=el[:], in0=lg[:, 4:8], scalar1=goh[:, 0:1], scalar2=None, op0=ALU.mult), reads=["lg", "goh"], writes=["el"])
                for g in range(1, 4):
                    p.dve(lambda e, g=g: e.scalar_tensor_tensor(out=el[:], in0=lg[:, 4 + 4 * g:8 + 4 * g], scalar=goh[:, g:g + 1], in1=el[:], op0=ALU.mult, op1=ALU.add),
                          reads=["lg", "goh", "el"], writes=["el"])
                p.dve(lambda e: e.tensor_reduce(out=r1[:, 2:3], in_=el[:], axis=AX.X, op=ALU.max), reads=["el"], writes=["r1c"])
                p.dve(lambda e: e.tensor_scalar(out=ee[:], in0=el[:], scalar1=r1[:, 2:3], scalar2=None, op0=ALU.subtract), reads=["el", "r1c"], writes=["ee"])
                p.act(lambda e: e.activation(out=ee[:], in_=ee[:], func=AF.Exp), reads=["ee"], writes=["ee"])
                p.dve(lambda e: e.tensor_scalar(out=ee2[:], in0=ee[:], scalar1=1.0, scalar2=-2.0, op0=ALU.is_ge, op1=ALU.mult), reads=["ee"], writes=["ee2"])
                p.dve(lambda e: e.tensor_tensor(out=ee2[:], in0=ee2[:], in1=ee[:], op=ALU.add), reads=["ee2", "ee"], writes=["ee2"])
                p.dve(lambda e: e.tensor_reduce(out=r1[:, 3:4], in_=ee2[:], axis=AX.X, op=ALU.max), reads=["ee2"], writes=["r1d"])
                p.dve(lambda e: e.tensor_scalar(out=ee2[:], in0=ee[:], scalar1=r1[:, 3:4], scalar2=None, op0=ALU.is_ge), reads=["ee", "r1d"], writes=["ee2"])
                p.dve(lambda e: e.tensor_tensor(out=ee[:], in0=ee[:], in1=ee2[:], op=ALU.mult), reads=["ee", "ee2"], writes=["ee"])
                p.dve(lambda e: e.tensor_scalar(out=r1[:, 4:5], in0=r1[:, 3:4], scalar1=1.0, scalar2=None, op0=ALU.add), reads=["r1d"], writes=["r1e"])
                p.dve(lambda e: e.reciprocal(out=r1[:, 4:5], in_=r1[:, 4:5]), reads=["r1e"], writes=["r1e"])
                p.dve(lambda e: e.tensor_tensor(out=r1[:, 4:5], in0=r1[:, 4:5], in1=r1[:, 1:2], op=ALU.mult), reads=["r1e", "r1b"], writes=["r1e"])
                p.dve(lambda e: e.tensor_scalar(out=ee[:], in0=ee[:], scalar1=r1[:, 4:5], scalar2=None, op0=ALU.mult), reads=["ee", "r1e"], writes=["ee"])
                for g in range(4):
                    p.dve(lambda e, g=g: e.tensor_scalar(out=gdt[:, 4 * g:4 * g + 4], in0=ee[:], scalar1=goh[:, g:g + 1], scalar2=None, op0=ALU.mult),
                          reads=["ee", "goh"], writes=[("gd", k2)])
                p.dma(io["gd_d"][rows, :], gdt[:], reads=[("gd", k2)], writes=[("gd_d", gck)])
            return stA, stB, stC

        for tc in range(4):
            pend.extend(make_chunk(tc))

    for s in range(NSLOT):
        do_slot(s)
    while pend:
        pend.pop(0)()

C1_W = {"w_br": [5, 256, D], "w_gate": [5, D, D], "b_gate": [5, D], "w_out": [D, D], "ln1_g": [D], "ln1_b": [D],
        "w_rg": [D, 4], "b_rg": [4], "w_re": [4, D, 4], "b_re": [4, 4]}


def build_c1(dbg=False):
    kb = KB()
    io = {}
    if dbg:
        io["dbg_mt"] = kb.dout("dbg_mt", [128, 8, 512], BF16)
    for n in ("c_ident", "c_ones"):
        io[n] = kb.din(n, [128, 128])
    io["c_cst"] = kb.din("c_cst", [128, 8])
    io["h_tok"] = kb.din("h_tok", [TOK, D])
    io["hT_d"] = kb.din("hT_d", [D, TOK], BF16)
    io["oT_d"] = kb.din("oT_d", [5, 256, TOK], BF16)
    w = {n: kb.din(n, shp) for n, shp in C1_W.items()}
    io["h1_d"] = kb.dout("h1_d", [TOK, D])
    io["h1T_d"] = kb.dout("h1T_d", [D, TOK], BF16)
    io["gd_d"] = kb.dout("gd_d", [TOK, 16])
    c = load_consts(kb, io)
    phase_c1(kb, c, io, w)
    return kb.finish()


def phase_c2(kb, c, io, w, nT=4, nE=16):
    nc, p = kb.nc, kb.p
    gbc = kb.sb("gbc2", [128, 1024], F32)
    bbc = kb.sb("bbc2", [128, 1024], F32)
    p.dma(gbc[:], w["ln2_g"].partition_broadcast(128), writes=["lngb"])
    p.dma(bbc[:], w["ln2_b"].partition_broadcast(128), writes=["lngb"])
    hT = [kb.sb("h1Tt", [128, 8, 1024], BF16) for _ in range(2)]
    gdt = [kb.sb("gdt", [128, 8, 16], F32) for _ in range(2)]
    wup = [kb.sb("wup", [128, 8, 512], BF16) for _ in range(3)]
    wdn = [kb.sb("wdn", [128, 2, 1024], BF16) for _ in range(3)]
    yacc = kb.sb("yacc", [128, 8, 1024], F32)
    gT = [kb.sb("gT", [128, 2, 1024], BF16) for _ in range(2)]
    sa = [kb.sb("sa", [128, 512], F32) for _ in range(2)]
    hch = [kb.sb("h1ch", [128, 1024], F32) for _ in range(2)]
    och = [kb.sb("och", [128, 1024], F32) for _ in range(2)]
    lnt = {"st": kb.sb("lnst2", [128, 2, 6], F32), "mv": kb.sb("lnmv2", [128, 2], F32), "sm": kb.sb("lnsm2", [128, 2], F32)}
    st = {"bank": 0, "w": 0, "sa": 0}

    def nbank():
        b = st["bank"] % 8
        st["bank"] += 1
        return b

    def make_expert(T, e, ht, httok, gd, gdtok):
        k = st["w"] % 3
        kg = st["w"] % 2
        st["w"] += 1
        wu, wd, g = wup[k], wdn[k], gT[kg]
        p.dma(wu[:], w["w_up"][e].rearrange("(f p) c -> p f c", p=128), writes=[("wup", k)], q="pool")
        p.dma(wd[:], w["w_down"][e].rearrange("(j p) c -> p j c", p=128), writes=[("wdn", k)], q="pool")
        ups, downs = [], []

        def make_up(ts, jc):
            def up():
                tsl = slice(ts * 512, (ts + 1) * 512)
                ba, bu = nbank(), nbank()
                pa, pu = kb.ps(ba), kb.ps(bu)
                for f in range(8):
                    p.pe(lambda e_, f=f: e_.matmul(pa[:], lhsT=wu[:, f, jc * 128:(jc + 1) * 128], rhs=ht[:, f, tsl], start=(f == 0), stop=(f == 7)),
                         reads=[("wup", k), httok], writes=[("ps", ba)])
                for f in range(8):
                    p.pe(lambda e_, f=f: e_.matmul(pu[:], lhsT=wu[:, f, 256 + jc * 128:256 + (jc + 1) * 128], rhs=ht[:, f, tsl], start=(f == 0), stop=(f == 7)),
                         reads=[("wup", k), httok], writes=[("ps", bu)])
                si = st["sa"] % 2
                st["sa"] += 1
                sat = sa[si]
                p.act(lambda e_: e_.activation(out=sat[:], in_=pa[:], func=AF.Silu), reads=[("ps", ba)], writes=[("sa", si)])
                p.dve(lambda e_: e_.tensor_tensor(out=g[:, jc, tsl], in0=sat[:], in1=pu[:], op=ALU.mult),
                      reads=[("sa", si), ("ps", bu)], writes=[("gT", kg)])
            return up

        def make_down(tc, hf):
            def down():
                b = nbank()
                ps = kb.ps(b)
                for jc in range(2):
                    p.pe(lambda e_, jc=jc: e_.matmul(ps[:], lhsT=g[:, jc, tc * 128:(tc + 1) * 128], rhs=wd[:, jc, hf * 512:(hf + 1) * 512],
                                                       start=(jc == 0), stop=(jc == 1)),
                         reads=[("gT", kg), ("wdn", k)], writes=[("ps", b)])
                ya = yacc[:, tc, hf * 512:(hf + 1) * 512]
                if e == 0:
                    p.dve(lambda e_: e_.tensor_scalar(out=ya, in0=ps[:], scalar1=gd[:, tc, e:e + 1], scalar2=None, op0=ALU.mult),
                          reads=[("ps", b), gdtok], writes=[("yacc", tc)])
                else:
                    p.dve(lambda e_: e_.scalar_tensor_tensor(out=ya, in0=ps[:], scalar=gd[:, tc, e:e + 1], in1=ya, op0=ALU.mult, op1=ALU.add),
                          reads=[("ps", b), gdtok, ("yacc", tc)], writes=[("yacc", tc)])
            return down

        for ts in range(2):
            for jc in range(2):
                ups.append(make_up(ts, jc))
        for tc in range(8):
            for hf in range(2):
                downs.append(make_down(tc, hf))
        return ups, downs

    def do_chunk_out(T, tc):
        gck = T * 8 + tc
        k2 = gck % 2
        rows = slice(gck * 128, (gck + 1) * 128)
        hc, oc = hch[k2], och[k2]
        p.dma(hc[:], io["h1_d"][rows, :], writes=[("h1ch", k2)])
        p.dve(lambda e_: e_.scalar_tensor_tensor(out=hc[:], in0=hc[:], scalar=ALPHA, in1=yacc[:, tc, :], op0=ALU.mult, op1=ALU.add),
              reads=[("h1ch", k2), ("yacc", tc)], writes=[("h1ch", k2)])
        layer_norm_chunk(kb, c, hc, ("h1ch", k2), gbc, bbc, oc, ("och", k2), lnt)
        p.dma(io["h_out"][rows, :], oc[:], reads=[("och", k2)], writes=[("h_out", gck)])

    def do_tile(T):
        d2 = T % 2
        ht, gd = hT[d2], gdt[d2]
        cols = slice(T * 1024, (T + 1) * 1024)
        p.dma(ht[:], io["h1T_d"].rearrange("(f p) t -> p f t", p=128)[:, :, cols], writes=[("h1Tt", d2)])
        p.dma(gd[:], io["gd_d"][T * 1024:(T + 1) * 1024, :].rearrange("(n p) e -> p n e", p=128), writes=[("gdt", d2)])
        prev_down = []
        for e in range(nE):
            ups, downs = make_expert(T, e, ht, ("h1Tt", d2), gd, ("gdt", d2))
            for i, u in enumerate(ups):
                u()
                for dn in prev_down[4 * i:4 * i + 4]:
                    dn()
            prev_down = downs
        for dn in prev_down:
            dn()
        for tc in range(8):
            do_chunk_out(T, tc)

    for T in range(nT):
        do_tile(T)


C2_W = {"w_up": [16, D, 512], "w_down": [16, 256, D], "ln2_g": [D], "ln2_b": [D]}


def build_c2(nT=4, nE=16):
    kb = KB()
    io = {}
    for n in ("c_ident", "c_ones"):
        io[n] = kb.din(n, [128, 128])
    io["c_cst"] = kb.din("c_cst", [128, 8])
    io["h1_d"] = kb.din("h1_d", [TOK, D])
    io["h1T_d"] = kb.din("h1T_d", [D, TOK], BF16)
    io["gd_d"] = kb.din("gd_d", [TOK, 16])
    w = {n: kb.din(n, shp) for n, shp in C2_W.items()}
    io["h_out"] = kb.dout("h_out", [TOK, D])
    c = load_consts(kb, io)
    phase_c2(kb, c, io, w, nT, nE)
    return kb.finish()


W_SHAPES = {
    "w_in": [D, IN_TOTAL], "g_cq": [256], "g_ckv": [128], "w_uq": [256, 384], "w_ukv": [128, 512],
    "nsa_pe": [32, 64], "w_phi_k1": [2048, 128], "w_phi_k2": [128, 64], "w_phi_v1": [2048, 128], "w_phi_v2": [128, 64],
    "w_mem_kv": [D, 512], "w_br": [5, 256, D], "w_gate": [5, D, D], "b_gate": [5, D], "w_out": [D, D],
    "ln1_g": [D], "ln1_b": [D], "w_rg": [D, 4], "b_rg": [4], "w_re": [4, D, 4], "b_re": [4, 4],
    "w_up": [16, D, 512], "w_down": [16, 256, D], "ln2_g": [D], "ln2_b": [D],
}
PAIRS = [[0, 1], [2, 3], [4, 5], [6, 7]]


def build_fused(depth=DEPTH, nlw=DEPTH, stop=None):
    kb = KB()
    io = {}
    for n in ("c_ident", "c_ones"):
        io[n] = kb.din(n, [128, 128])
    io["c_cst"] = kb.din("c_cst", [128, 8])
    for n, (shp, dt) in B_CONST_SHAPES.items():
        io[n] = kb.din(n, shp, dt)
    io["ropeq_t"] = kb.din("ropeq_t", [NSLOT, 96, 2, 512])
    io["ropek_t"] = kb.din("ropek_t", [NSLOT, 32, 2, 512])
    io["x_own"] = kb.din("x_own", [TOK, D])
    io["mem"] = kb.din("mem", [256, D])
    wfull = {n: kb.din(n, [nlw] + shp) for n, shp in W_SHAPES.items()}
    io["h_final"] = kb.dout("h_final", [TOK, D])
    hbuf = [kb.dscratch(f"hbuf{i}", [TOK, D]) for i in range(2)]
    io["hT_d"] = kb.dscratch("hT_d", [D, TOK], BF16)
    for n in ("qsb_d", "qmo_d", "qns_d", "qme_d"):
        io[n] = kb.dscratch(n, [256, TOK], BF16)
    io["qml_d"] = kb.dscratch("qml_d", [4, 96, TOK], BF16)
    io["gns_d"] = kb.dscratch("gns_d", [12, TOK])
    io["oT_d"] = kb.dscratch("oT_d", [5, 256, TOK], BF16)
    io["h1_d"] = kb.dscratch("h1_d", [TOK, D])
    io["h1T_d"] = kb.dscratch("h1T_d", [D, TOK], BF16)
    io["gd_d"] = kb.dscratch("gd_d", [TOK, 16])
    xk_rows = [256, 256, 256, 256, 64]
    xk_in = [kb.dscratch(f"xk_in{k}", [r, TOK], BF16) for k, r in enumerate(xk_rows)]
    xk_out = [kb.dscratch(f"xk_out{k}", [2 * r, TOK], BF16) for k, r in enumerate(xk_rows)]
    xv_in = [kb.dscratch(f"xv_in{k}", [1024, 896], BF16) for k in range(4)]
    xv_out = [kb.dscratch(f"xv_out{k}", [2048, 896], BF16) for k in range(4)]
    io["ksb_d"], io["kmo_d"] = xk_in[0], xk_in[1]
    io["kml_d"] = xk_in[2].rearrange("(h d) t -> h d t", h=4)
    io["krl_d"], io["kcv_d"], io["ksl_d"] = xk_in[3][0:32, :], xk_in[3][32:160, :], xk_in[3][160:224, :]
    io["kwi_d"] = xk_in[4]
    io["vsb_d"], io["vmo_d"] = TMChunks(xv_in, 0, 256), TMChunks(xv_in, 256, 512)
    io["vml_d"], io["vsw_d"] = TMChunks(xv_in, 512, 768), TMChunks(xv_in, 768, 896)
    io["ksb_f"], io["kmo_f"], io["kml_f"] = KPair(xk_out[0], 256), KPair(xk_out[1], 256), KPair(xk_out[2], 256)
    io["krl_f"], io["kcv_f"], io["ksl_f"] = KPair(xk_out[3], 256, 0), KPair(xk_out[3], 256, 32), KPair(xk_out[3], 256, 160)
    io["kwi_f"] = KPair(xk_out[4], 64)
    io["vsb_f"], io["vmo_f"] = VPair(xv_out, 0), VPair(xv_out, 256)
    io["vml_f"], io["vsw_f"] = VPair(xv_out, 512), VPair(xv_out, 768)

    for l in range(depth):
        w = {n: ap[l] for n, ap in wfull.items()}
        io["h_tok"] = io["x_own"] if l == 0 else hbuf[(l - 1) % 2]
        io["h_out"] = io["h_final"] if l == depth - 1 else hbuf[l % 2]
        c = load_consts(kb, io)
        phase_a(kb, c, io, w)
        kb.end_phase()
        if stop == "A":
            break
        c = load_consts(kb, io)
        phase_b(kb, c, io, w, ("mem",))
        for k in range(5):
            kb.p.allgather(xk_out[k], xk_in[k], PAIRS, writes=[("xk", k)])
        for k in range(4):
            kb.p.allgather(xv_out[k], xv_in[k], PAIRS, writes=[("xv", k)])
        kb.end_phase()
        if stop == "AG":
            break
        c = load_consts(kb, io)
        phase_b(kb, c, io, w, ("sb", "moba", "mla"))
        kb.end_phase()
        if stop == "B1":
            break
        c = load_consts(kb, io)
        phase_b(kb, c, io, w, ("nsa",))
        kb.end_phase()
        if stop == "B2":
            break
        c = load_consts(kb, io)
        phase_c1(kb, c, io, w)
        kb.end_phase()
        if stop == "C1":
            break
        c = load_consts(kb, io)
        phase_c2(kb, c, io, w)
        kb.end_phase()
    return kb.finish()


def fused_inputs(inp, c):
    b, r = c // 2, c % 2
    m = dict(host_consts())
    m.update(bconsts_host(r))
    m["ropeq_t"], m["ropek_t"] = rope_tables(r)
    m["x_own"] = np.ascontiguousarray(inp["x"][b][_own_tokens(r)])
    m["mem"] = inp["mem"][b]
    for n in W_SHAPES:
        m[n] = inp[n]
    return m


_PROGS = {}


def _prog(name, fn):
    if name not in _PROGS:
        _PROGS[name] = fn()
    return _PROGS[name]


def _own_tokens(r):
    t = np.arange(TOK)
    return t // 512 * 1024 + r * 512 + t % 512


def _interleave_fm(a0, a1):
    sh = a0.shape[:-1]
    out = np.empty(sh + (S,), a0.dtype)
    o = out.reshape(sh + (8, 2, 512))
    o[..., 0, :] = a0.reshape(sh + (8, 512))
    o[..., 1, :] = a1.reshape(sh + (8, 512))
    return out


def _interleave_tm(a0, a1):
    C = a0.shape[1]
    out = np.empty((S, C), a0.dtype)
    o = out.reshape(8, 2, 512, C)
    o[:, 0] = a0.reshape(8, 512, C)
    o[:, 1] = a1.reshape(8, 512, C)
    return out


A_WEIGHTS = ("w_in", "w_uq", "g_cq", "w_ukv", "g_ckv")
K_FM = {"ksb_d": "ksb_f", "kmo_d": "kmo_f", "kml_d": "kml_f", "krl_d": "krl_f", "kcv_d": "kcv_f", "ksl_d": "ksl_f", "kwi_d": "kwi_f"}
V_TM = {"vsb_d": "vsb_f", "vmo_d": "vmo_f", "vml_d": "vml_f", "vsw_d": "vsw_f"}
Q_OWN = ("qsb_d", "qmo_d", "qml_d", "qns_d", "qme_d", "gns_d")


def kernel_unfused(**inputs):
    inp = {k: np.ascontiguousarray(np.asarray(v)) for k, v in inputs.items()}
    ncores = 8
    cores = list(range(ncores))
    hc = host_consts()
    bc = [bconsts_host(r) for r in range(2)]
    rp = [rope_tables(r) for r in range(2)]
    own = [_own_tokens(r) for r in range(2)]
    h = [np.ascontiguousarray(inp["x"][c // 2][own[c % 2]]) for c in cores]
    nca = _prog("a", build_a)
    ncb1 = _prog("b1", lambda: build_b(("sb", "moba", "mla", "mem")))
    ncb2 = _prog("b2", lambda: build_b(("nsa",)))
    ncc1 = _prog("c1", build_c1)
    ncc2 = _prog("c2", build_c2)
    for l in range(DEPTH):
        maps = []
        for c in cores:
            m = dict(hc)
            m["h_tok"] = h[c]
            m["ropeq_t"], m["ropek_t"] = rp[c % 2]
            for n in A_WEIGHTS:
                m[n] = inp[n][l]
            maps.append(m)
        ra = run_bass_kernel_spmd(nca, maps, core_ids=cores).results
        maps = []
        for c in cores:
            b, r = c // 2, c % 2
            m = dict(hc)
            m.update(bc[r])
            for n in Q_OWN:
                m[n] = ra[c][n]
            for kd, kf in K_FM.items():
                m[kf] = _interleave_fm(np.asarray(ra[2 * b][kd]), np.asarray(ra[2 * b + 1][kd]))
            for vd, vf in V_TM.items():
                m[vf] = _interleave_tm(np.asarray(ra[2 * b][vd]), np.asarray(ra[2 * b + 1][vd]))
            m["mem"] = inp["mem"][b]
            for n in B_WEIGHTS:
                m[n] = inp[n][l]
            maps.append(m)
        rb1 = run_bass_kernel_spmd(ncb1, maps, core_ids=cores).results
        rb2 = run_bass_kernel_spmd(ncb2, maps, core_ids=cores).results
        rb = []
        for c in cores:
            o = np.array(rb1[c]["oT_d"])
            o[3] = np.asarray(rb2[c]["oT_d"])[3]
            rb.append({"oT_d": o})
        maps = []
        for c in cores:
            m = dict(hc)
            m["h_tok"] = h[c]
            m["hT_d"] = ra[c]["hT_d"]
            m["oT_d"] = rb[c]["oT_d"]
            for n in C1_W:
                m[n] = inp[n][l]
            maps.append(m)
        rc1 = run_bass_kernel_spmd(ncc1, maps, core_ids=cores).results
        maps = []
        for c in cores:
            m = dict(hc)
            for n in ("h1_d", "h1T_d", "gd_d"):
                m[n] = rc1[c][n]
            for n in C2_W:
                m[n] = inp[n][l]
            maps.append(m)
        rc2 = run_bass_kernel_spmd(ncc2, maps, core_ids=cores).results
        h = [np.asarray(rc2[c]["h_out"]) for c in cores]
    out = np.empty((NB, S, D), np.float32)
    for c in cores:
        out[c // 2][own[c % 2]] = h[c]
    return out


def kernel(**inputs):
    inp = {k: np.ascontiguousarray(np.asarray(v)) for k, v in inputs.items()}
    cores = list(range(8))
    nc = _prog("fused", build_fused)
    maps = [fused_inputs(inp, c) for c in cores]
    res = run_bass_kernel_spmd(nc, maps, core_ids=cores).results
    out = np.empty((NB, S, D), np.float32)
    for c in cores:
        out[c // 2][_own_tokens(c % 2)] = np.asarray(res[c]["h_final"])
    return out
```

```python
import contextlib
import types
import numpy as np
import ml_dtypes
import concourse.bass as bass
import concourse.mybir as mybir
from concourse.bass_utils import run_bass_kernel_spmd

F32 = mybir.dt.float32
BF16 = mybir.dt.bfloat16
AF = mybir.ActivationFunctionType
ALU = mybir.AluOpType
AX = mybir.AxisListType

D = 1024
S = 8192
NB = 4
DEPTH = 4
TOK = 4096
NSLOT = 8
IN_TOTAL = 2860
ALPHA = (2.0 * DEPTH) ** 0.25
LN_EPS = 1e-5
RMS_EPS = 1e-6
MASKV = -30000.0
SLOPES = [2.0 ** (-2.0 * (i + 1)) for i in range(4)]

ENGS = ("pe", "act", "dve", "pool", "sp")
SIG_EPOCH = 30000


def _freeze(fn):
    if fn.__closure__ is None:
        return fn
    cells = []
    for cl in fn.__closure__:
        try:
            cells.append(types.CellType(cl.cell_contents))
        except ValueError:
            cells.append(cl)
    g = types.FunctionType(fn.__code__, fn.__globals__, fn.__name__, fn.__defaults__, tuple(cells))
    g.__kwdefaults__ = fn.__kwdefaults__
    return g


class Op:
    __slots__ = ("eng", "fn", "reads", "writes", "dma", "deps", "sig", "dticket", "idx", "dprev")

    def __init__(self, eng, fn, reads, writes, dma):
        self.eng = eng
        self.fn = fn
        self.reads = tuple(reads)
        self.writes = tuple(writes)
        self.dma = dma
        self.deps = []
        self.sig = None
        self.dticket = None
        self.dprev = None


class Prog:
    NDSEM = 12
    _phase_id = 0

    def __init__(self, nc):
        self.nc = nc
        self.ops = []

    def add(self, eng, fn, reads=(), writes=(), dma=False):
        op = Op(eng, _freeze(fn), reads, writes, dma)
        op.idx = len(self.ops)
        self.ops.append(op)
        return op

    def pe(self, fn, reads=(), writes=()):
        return self.add("pe", fn, reads, writes)

    def act(self, fn, reads=(), writes=()):
        return self.add("act", fn, reads, writes)

    def dve(self, fn, reads=(), writes=()):
        return self.add("dve", fn, reads, writes)

    def pool(self, fn, reads=(), writes=()):
        return self.add("pool", fn, reads, writes)

    def allgather(self, out, in_, groups, reads=(), writes=()):
        return self.add("pool", lambda e: e.collective_compute("AllGather", ALU.bypass, replica_groups=groups, ins=[in_.opt()], outs=[out.opt()]),
                        reads, writes, dma="cc")

    def dma(self, out, in_, reads=(), writes=(), q="sp", slow=False):
        if slow:
            return self.add(q, lambda e: e.dma_start(out=out, in_=in_, allow_slow_non_contiguous=True), reads, writes, dma=True)
        return self.add(q, lambda e: e.dma_start(out=out, in_=in_), reads, writes, dma=True)

    def analyze(self):
        last_w = {}
        readers = {}
        for op in self.ops:
            deps = set()
            for t in op.reads:
                if t in last_w:
                    deps.add(last_w[t])
            for t in op.writes:
                if t in last_w:
                    deps.add(last_w[t])
                for r in readers.get(t, ()):
                    deps.add(r)
            deps.discard(op.idx)
            op.deps = sorted(deps)
            for t in op.reads:
                readers.setdefault(t, []).append(op.idx)
            for t in op.writes:
                last_w[t] = op.idx
                readers[t] = []
        qcount = {e: 0 for e in ENGS}
        qhist = {e: [] for e in ENGS}
        for op in self.ops:
            if op.dma == "cc":
                op.dticket = ("cc", op.idx, 1)
                continue
            if op.dma:
                n = qcount[op.eng]
                qcount[op.eng] += 1
                op.dticket = (op.eng, n % self.NDSEM, 16 * (n // self.NDSEM + 1))
                if n >= self.NDSEM:
                    op.dprev = qhist[op.eng][n - self.NDSEM]
                qhist[op.eng].append(op.idx)
        waited_eng = {e: {p: -1 for p in ENGS} for e in ENGS}
        waited_dma = {e: set() for e in ENGS}
        last_on = {e: -1 for e in ENGS}
        need_sig = set()
        for op in self.ops:
            e = op.eng
            final = []
            best = {}
            dl = list(op.deps)
            if op.dprev is not None:
                dl.append(op.dprev)
            for d in dl:
                p = self.ops[d]
                if p.dma:
                    if d not in waited_dma[e]:
                        waited_dma[e].add(d)
                        final.append(("dma", d))
                else:
                    if p.eng == e and e == "pe":
                        continue
                    if d <= waited_eng[e][p.eng]:
                        continue
                    if p.eng not in best or d > best[p.eng]:
                        best[p.eng] = d
            for pe_, d in best.items():
                waited_eng[e][pe_] = d
                need_sig.add(d)
                final.append(("eng", d))
            op.deps = final
        cnt = {e: 0 for e in ENGS}
        for op in self.ops:
            if not op.dma and op.idx in need_sig:
                cnt[op.eng] += 1
                op.sig = cnt[op.eng]
        self.sig_total = cnt
        self.dma_total = qcount

    def emit(self, barrier=False):
        nc = self.nc
        self.analyze()
        allsem = []

        Prog._phase_id += 1
        pid = Prog._phase_id

        def newsem(name):
            h = nc.alloc_semaphore(f"{name}_ph{pid}")
            allsem.append(h)
            return h

        if True:
            esem = {}
            for e in ENGS:
                n_ep = self.sig_total[e] // SIG_EPOCH + 1
                esem[e] = [newsem(f"s_{e}_{i}") for i in range(n_ep)]
            dsem = {}
            for e in ENGS:
                if self.dma_total[e]:
                    dsem[e] = [newsem(f"d_{e}_{i}") for i in range(self.NDSEM)]
            dsem["cc"] = {op.idx: newsem(f"cc_{op.idx}") for op in self.ops if op.dma == "cc"}

            def waitspec(dep):
                kind, d = dep
                p = self.ops[d]
                if kind == "dma":
                    q, si, val = p.dticket
                    return dsem[q][si], val
                k = p.sig - 1
                return esem[p.eng][k // SIG_EPOCH], k % SIG_EPOCH + 1

            def run(engname):
                def body(eng):
                    last_dma = {}
                    for op in self.ops:
                        if op.eng != engname:
                            continue
                        ws = [waitspec(d) for d in op.deps]
                        for (sem, val) in ws[1:]:
                            eng.wait_ge(sem, val)
                        ins = op.fn(eng)
                        if ws:
                            ins._wait_ge(ws[0][0], ws[0][1])
                        if op.dma == "cc":
                            ins.then_inc(dsem["cc"][op.idx])
                            eng.wait_ge(dsem["cc"][op.idx], 1)
                        elif op.dma:
                            q, si, val = op.dticket
                            ins.then_inc(dsem[q][si], 16)
                            last_dma[si] = val
                        elif op.sig is not None:
                            k = op.sig - 1
                            ins.then_inc(esem[engname][k // SIG_EPOCH], 1)
                    for si, val in last_dma.items():
                        eng.wait_ge(dsem[engname][si], val)
                return body

            with nc.Block() as block:
                block.tensor(run("pe"))
                block.scalar(run("act"))
                block.vector(run("dve"))
                block.gpsimd(run("pool"))
                block.sync(run("sp"))
        if barrier:
            nc.all_engine_barrier()
            nc.clear_and_free_semaphores(allsem)
            nc.all_engine_barrier()
        else:
            for h in allsem:
                nc.release_semaphore(h)


class KB:
    def __init__(self):
        self.nc = bass.Bass("TRN2", target_bir_lowering=False)
        self.p = Prog(self.nc)
        self.es = contextlib.ExitStack()
        self.dram = {}
        self._uid = 0
        self.es0 = contextlib.ExitStack()
        self.psf = [self.es0.enter_context(self.nc.psum_tensor(f"psb{i}", [128, 512], F32)) for i in range(8)]

    def uid(self, s):
        self._uid += 1
        return f"{s}_{self._uid}"

    def din(self, name, shape, dt=F32):
        t = self.nc.dram_tensor(name, list(shape), dt, kind="ExternalInput").ap()
        self.dram[name] = t
        return t

    def dout(self, name, shape, dt=F32, kind="ExternalOutput"):
        t = self.nc.dram_tensor(name, list(shape), dt, kind=kind).ap()
        self.dram[name] = t
        return t

    def sb(self, name, shape, dt=F32):
        return self.es.enter_context(self.nc.sbuf_tensor(self.uid(name), list(shape), dt))

    def dscratch(self, name, shape, dt=F32):
        t = self.nc.dram_tensor(name, list(shape), dt, kind="Internal").ap()
        self.dram[name] = t
        return t

    def end_phase(self):
        self.p.emit(barrier=True)
        self.es.close()
        self.es = contextlib.ExitStack()
        self.p = Prog(self.nc)

    def ps(self, i):
        return self.psf[i]

    def finish(self):
        self.p.emit()
        self.es.close()
        self.es0.close()
        return self.nc


C_SBQ, C_SBK, C_SBV = 0, 256, 512
C_MOQ, C_MOK, C_MOV = 768, 1024, 1280
C_CQ, C_CKV, C_KR = 1536, 1792, 1920
C_NSQ, C_NSKV, C_NSG, C_MEQ = 1952, 2208, 2592, 2604


def consts_common(kb):
    nc, p = kb.nc, kb.p
    c = {}
    c["identf"] = kb.sb("identf", [128, 128], F32)
    c["identb"] = kb.sb("identb", [128, 128], BF16)
    c["onesb"] = kb.sb("onesb", [128, 128], BF16)
    c["onesf"] = kb.sb("onesf", [128, 128], F32)
    idf, idb = c["identf"], c["identb"]
    p.pool(lambda e: e.memset(c["onesf"][:], 1.0), writes=["onesf"])
    p.pool(lambda e: e.memset(c["onesb"][:], 1.0), writes=["onesb"])
    p.pool(lambda e: e.affine_select(out=idf[:], in_=c["onesf"][:], pattern=[[-1, 128]], compare_op=ALU.is_equal,
                                     fill=0.0, base=0, channel_multiplier=1), reads=["onesf"], writes=["identf"])
    p.pool(lambda e: e.tensor_copy(out=idb[:], in_=idf[:]), reads=["identf"], writes=["identb"])
    return c


def tm_view(ap2d, p=128):
    return ap2d.rearrange("(n p) c -> p n c", p=p)


def phase_a(kb, c, io, w):
    nc, p = kb.nc, kb.p
    identf, onesb = c["identf"], c["onesb"]

    hT = kb.sb("hT", [128, 8, TOK], BF16)
    win = kb.sb("win", [128, 8, IN_TOTAL], BF16)
    wkrot = kb.sb("wkrot", [128, 8, 32], BF16)
    for f in range(8):
        p.dma(win[:, f, :], w["w_in"][f * 128:(f + 1) * 128, :], writes=[("win", f)], q="pool")
    for f in range(8):
        p.dve(lambda e, f=f: e.tensor_scalar_mul(out=wkrot[:, f, 0:16], in0=win[:, f, C_KR + 16:C_KR + 32], scalar1=-1.0),
              reads=[("win", f)], writes=[("wkrot", f)])
        p.dve(lambda e, f=f: e.tensor_copy(out=wkrot[:, f, 16:32], in_=win[:, f, C_KR:C_KR + 16]),
              reads=[("win", f)], writes=[("wkrot", f)])
    wuq_f = kb.sb("wuq_f", [128, 2, 384], F32)
    wuq = kb.sb("wuq", [128, 2, 384], BF16)
    wuqr = kb.sb("wuqr", [128, 2, 384], BF16)
    gcq = kb.sb("gcq", [128, 2], F32)
    wukv_f = kb.sb("wukv_f", [128, 512], F32)
    wukv = kb.sb("wukv", [128, 512], BF16)
    gckv = kb.sb("gckv", [128, 1], F32)
    p.dma(wuq_f[:], w["w_uq"].rearrange("(n p) c -> p n c", p=128), writes=["wuq_f"])
    p.dma(gcq[:], w["g_cq"].rearrange("(n p) -> p n", p=128), writes=["gcq"], slow=True)
    p.dma(wukv_f[:], w["w_ukv"], writes=["wukv_f"])
    p.dma(gckv[:], w["g_ckv"].rearrange("(n p) -> p n", p=128), writes=["gckv"], slow=True)
    for rc in range(2):
        p.dve(lambda e, rc=rc: e.tensor_scalar_mul(out=wuq[:, rc, :], in0=wuq_f[:, rc, :], scalar1=gcq[:, rc:rc + 1]),
              reads=["wuq_f", "gcq"], writes=["wuq"])
    p.dve(lambda e: e.memset(wuqr[:], 0.0), writes=["wuqr"])
    for rc in range(2):
        for h in range(4):
            b0 = h * 96
            p.dve(lambda e, rc=rc, b0=b0: e.tensor_scalar_mul(out=wuqr[:, rc, b0 + 64:b0 + 80], in0=wuq[:, rc, b0 + 80:b0 + 96], scalar1=-1.0),
                  reads=["wuq"], writes=["wuqr"])
            p.dve(lambda e, rc=rc, b0=b0: e.tensor_copy(out=wuqr[:, rc, b0 + 80:b0 + 96], in_=wuq[:, rc, b0 + 64:b0 + 80]),
                  reads=["wuq"], writes=["wuqr"])
    p.dve(lambda e: e.tensor_scalar_mul(out=wukv[:], in0=wukv_f[:], scalar1=gckv[:, 0:1]), reads=["wukv_f", "gckv"], writes=["wukv"])

    hst = [kb.sb("hst", [128, 1024], F32) for _ in range(2)]
    for ck in range(TOK // 128):
        st = hst[ck % 2]
        tk = ("hst", ck % 2)
        p.dma(st[:], io["h_tok"][ck * 128:(ck + 1) * 128, :], writes=[tk])
        for half in range(2):
            bank = (ck * 2 + half) % 2
            ps = kb.ps(bank)
            for j in range(4):
                f = half * 4 + j
                p.pe(lambda e, ps=ps, st=st, f=f, j=j: e.transpose(out=ps[:, j * 128:(j + 1) * 128], in_=st[:, f * 128:(f + 1) * 128], identity=identf[:]),
                     reads=[tk, "identf"], writes=[("ps", bank)])
            dst = hT[:, half * 4:half * 4 + 4, ck * 128:(ck + 1) * 128]
            src = ps[:].rearrange("p (j t) -> p j t", j=4)
            if half == 0:
                p.act(lambda e, dst=dst, src=src: e.copy(out=dst, in_=src), reads=[("ps", bank)], writes=[("hT", ck // 4)])
            else:
                p.dve(lambda e, dst=dst, src=src: e.tensor_copy(out=dst, in_=src), reads=[("ps", bank)], writes=[("hT", ck // 4)])
    for f in range(8):
        p.dma(io["hT_d"][f * 128:(f + 1) * 128, :], hT[:, f, :], reads=[("hT", s) for s in range(8)], writes=[("hT_d", f)])

    ostage = [kb.sb("ostg", [128, 512], BF16) for _ in range(4)]
    gstage = [kb.sb("gstg", [12, 512], F32) for _ in range(2)]
    cq_sb = [kb.sb("cq_sb", [128, 2, 512], BF16) for _ in range(2)]
    ckv_sb = [kb.sb("ckv_sb", [128, 512], BF16) for _ in range(2)]
    krr_sb = [kb.sb("krr", [32, 2, 512], F32) for _ in range(2)]
    sq_sb = [kb.sb("sq", [128, 3, 512], BF16) for _ in range(2)]
    rstd_q = [kb.sb("rstdq", [128, 512], F32) for _ in range(2)]
    rstd_kv = [kb.sb("rstdkv", [128, 512], F32) for _ in range(2)]
    rkv_tok = [kb.sb("rkvtok", [128, 4], F32) for _ in range(2)]
    ropeq = [kb.sb("ropeq", [96, 2, 512], F32) for _ in range(2)]
    ropek = [kb.sb("ropek", [32, 2, 512], F32) for _ in range(2)]
    t1 = [kb.sb("t1", [96, 512], F32) for _ in range(2)]
    t2 = [kb.sb("t2", [96, 512], F32) for _ in range(2)]
    vstage = [kb.sb("vstg", [128, 640], BF16) for _ in range(2)]
    vmst = [kb.sb("vmst", [128, 256], BF16) for _ in range(2)]
    cnt = {"o": 0, "bank": 0, "v": 0}

    def nbank():
        b = 2 + cnt["bank"] % 6
        cnt["bank"] += 1
        return b

    fm_list = [
        ("qsb_d", 0, C_SBQ, 128, 0.125), ("qsb_d", 128, C_SBQ + 128, 128, 0.125),
        ("ksb_d", 0, C_SBK, 128, 1.0), ("ksb_d", 128, C_SBK + 128, 128, 1.0),
        ("qmo_d", 0, C_MOQ, 128, 0.125), ("qmo_d", 128, C_MOQ + 128, 128, 0.125),
        ("kmo_d", 0, C_MOK, 128, 1.0), ("kmo_d", 128, C_MOK + 128, 128, 1.0),
        ("qns_d", 0, C_NSQ, 128, 0.125), ("qns_d", 128, C_NSQ + 128, 128, 0.125),
        ("kcv_d", 0, C_NSKV, 128, 1.0),
        ("ksl_d", 0, C_NSKV + 128, 64, 1.0),
        ("kwi_d", 0, C_NSKV + 256, 64, 1.0),
        ("qme_d", 0, C_MEQ, 128, 0.125), ("qme_d", 128, C_MEQ + 128, 128, 0.125),
    ]

    def proj_fm(s, col0, ncols, wsrc=None):
        b = nbank()
        ps = kb.ps(b)
        for f in range(8):
            if wsrc is None:
                lhsT = win[:, f, col0:col0 + ncols]
                rd = [("win", f)]
            else:
                lhsT = wsrc[:, f, col0:col0 + ncols]
                rd = [("wkrot", f)]
            p.pe(lambda e, ps=ps, lhsT=lhsT, f=f, s=s, ncols=ncols: e.matmul(ps[0:ncols, :], lhsT=lhsT, rhs=hT[:, f, s * 512:(s + 1) * 512],
                                                                            start=(f == 0), stop=(f == 7)),
                 reads=rd + [("hT", s)], writes=[("ps", b)])
        return b

    for s in range(NSLOT):
        tsl = slice(s * 512, (s + 1) * 512)
        for (dn, r0, col0, ncols, scale) in fm_list:
            b = proj_fm(s, col0, ncols)
            ps = kb.ps(b)
            k = cnt["o"] % 4
            cnt["o"] += 1
            og = ostage[k]
            p.act(lambda e, og=og, ps=ps, ncols=ncols, scale=scale: e.activation(out=og[0:ncols, :], in_=ps[0:ncols, :], func=AF.Copy, scale=scale),
                  reads=[("ps", b)], writes=[("ostg", k)])
            p.dma(io[dn][r0:r0 + ncols, tsl], og[0:ncols, :], reads=[("ostg", k)], writes=[(dn, s)])
        b = proj_fm(s, C_NSG, 12)
        ps = kb.ps(b)
        gs = gstage[s % 2]
        p.act(lambda e, gs=gs, ps=ps: e.activation(out=gs[:], in_=ps[0:12, :], func=AF.Sigmoid), reads=[("ps", b)], writes=[("gstg", s % 2)])
        p.dma(io["gns_d"][:, tsl], gs[:], reads=[("gstg", s % 2)], writes=[("gns_d", s)])

        d2 = s % 2
        cq, ckv, sq = cq_sb[d2], ckv_sb[d2], sq_sb[d2]
        for rc in range(2):
            b = proj_fm(s, C_CQ + rc * 128, 128)
            ps = kb.ps(b)
            p.act(lambda e, cq=cq, ps=ps, rc=rc: e.copy(out=cq[:, rc, :], in_=ps[:]), reads=[("ps", b)], writes=[("cq", d2)])
            p.act(lambda e, sq=sq, ps=ps, rc=rc: e.activation(out=sq[:, rc, :], in_=ps[:], func=AF.Square), reads=[("ps", b)], writes=[("sq", d2)])
        b = proj_fm(s, C_CKV, 128)
        ps = kb.ps(b)
        p.act(lambda e, ckv=ckv, ps=ps: e.copy(out=ckv[:], in_=ps[:]), reads=[("ps", b)], writes=[("ckv", d2)])
        p.act(lambda e, sq=sq, ps=ps: e.activation(out=sq[:, 2, :], in_=ps[:], func=AF.Square), reads=[("ps", b)], writes=[("sq", d2)])
        krr = krr_sb[d2]
        b = proj_fm(s, C_KR, 32)
        ps = kb.ps(b)
        p.act(lambda e, krr=krr, ps=ps: e.copy(out=krr[:, 0, :], in_=ps[0:32, :]), reads=[("ps", b)], writes=[("krr", d2)])
        b = proj_fm(s, 0, 32, wsrc=wkrot)
        ps = kb.ps(b)
        p.act(lambda e, krr=krr, ps=ps: e.copy(out=krr[:, 1, :], in_=ps[0:32, :]), reads=[("ps", b)], writes=[("krr", d2)])
        rq, rkv = rstd_q[d2], rstd_kv[d2]
        b = nbank()
        ps = kb.ps(b)
        for rc in range(2):
            p.pe(lambda e, ps=ps, sq=sq, rc=rc: e.matmul(ps[:], lhsT=onesb[:], rhs=sq[:, rc, :], start=(rc == 0), stop=(rc == 1)),
                 reads=[("sq", d2), "onesb"], writes=[("ps", b)])
        p.act(lambda e, rq=rq, ps=ps: e.activation(out=rq[:], in_=ps[:], func=AF.Ln, scale=1.0 / 256.0, bias=c["eps_rms"][:, 0:1]),
              reads=[("ps", b), "cst"], writes=[("rq", d2)])
        p.act(lambda e, rq=rq: e.activation(out=rq[:], in_=rq[:], func=AF.Exp, scale=-0.5), reads=[("rq", d2)], writes=[("rq", d2)])
        b = nbank()
        ps = kb.ps(b)
        p.pe(lambda e, ps=ps, sq=sq: e.matmul(ps[:], lhsT=onesb[:], rhs=sq[:, 2, :], start=True, stop=True),
             reads=[("sq", d2), "onesb"], writes=[("ps", b)])
        p.act(lambda e, rkv=rkv, ps=ps: e.activation(out=rkv[:], in_=ps[:], func=AF.Ln, scale=1.0 / 128.0, bias=c["eps_rms"][:, 0:1]),
              reads=[("ps", b), "cst"], writes=[("rkv", d2)])
        p.act(lambda e, rkv=rkv: e.activation(out=rkv[:], in_=rkv[:], func=AF.Exp, scale=-0.5), reads=[("rkv", d2)], writes=[("rkv", d2)])
        rkt = rkv_tok[d2]
        b = nbank()
        ps = kb.ps(b)
        for ck in range(4):
            p.pe(lambda e, ps=ps, sq=sq, ck=ck: e.matmul(ps[:, ck:ck + 1], lhsT=sq[:, 2, ck * 128:(ck + 1) * 128], rhs=onesb[:, 0:1], start=True, stop=True),
                 reads=[("sq", d2), "onesb"], writes=[("ps", b)])
        p.act(lambda e, rkt=rkt, ps=ps: e.activation(out=rkt[:], in_=ps[:, 0:4], func=AF.Ln, scale=1.0 / 128.0, bias=c["eps_rms"][:, 0:1]),
              reads=[("ps", b), "cst"], writes=[("rkt", d2)])
        p.act(lambda e, rkt=rkt: e.activation(out=rkt[:], in_=rkt[:], func=AF.Exp, scale=-0.5), reads=[("rkt", d2)], writes=[("rkt", d2)])
        rpq, rpk = ropeq[d2], ropek[d2]
        p.dma(rpq[:], io["ropeq_t"][s], writes=[("rpq", d2)])
        p.dma(rpk[:], io["ropek_t"][s], writes=[("rpk", d2)])
        for h in range(4):
            bA, bB = nbank(), nbank()
            psA, psB = kb.ps(bA), kb.ps(bB)
            for rc in range(2):
                p.pe(lambda e, psA=psA, cq=cq, rc=rc, h=h: e.matmul(psA[0:96, :], lhsT=wuq[:, rc, h * 96:(h + 1) * 96], rhs=cq[:, rc, :], start=(rc == 0), stop=(rc == 1)),
                     reads=["wuq", ("cq", d2)], writes=[("ps", bA)])
            for rc in range(2):
                p.pe(lambda e, psB=psB, cq=cq, rc=rc, h=h: e.matmul(psB[0:96, :], lhsT=wuqr[:, rc, h * 96:(h + 1) * 96], rhs=cq[:, rc, :], start=(rc == 0), stop=(rc == 1)),
                     reads=["wuqr", ("cq", d2)], writes=[("ps", bB)])
            a1, a2 = t1[h % 2], t2[h % 2]
            k = cnt["o"] % 4
            cnt["o"] += 1
            og = ostage[k]
            p.dve(lambda e, a1=a1, psA=psA, rpq=rpq: e.tensor_tensor(out=a1[:], in0=psA[0:96, :], in1=rpq[:, 0, :], op=ALU.mult),
                  reads=[("ps", bA), ("rpq", d2)], writes=[("t1", h % 2)])
            p.dve(lambda e, a2=a2, psB=psB, rpq=rpq: e.tensor_tensor(out=a2[:], in0=psB[0:96, :], in1=rpq[:, 1, :], op=ALU.mult),
                  reads=[("ps", bB), ("rpq", d2)], writes=[("t2", h % 2)])
            p.dve(lambda e, a1=a1, a2=a2: e.tensor_tensor(out=a1[:], in0=a1[:], in1=a2[:], op=ALU.add),
                  reads=[("t1", h % 2), ("t2", h % 2)], writes=[("t1", h % 2)])
            p.dve(lambda e, a1=a1, og=og, rq=rq: e.tensor_tensor(out=og[0:96, :], in0=a1[:], in1=rq[0:96, :], op=ALU.mult),
                  reads=[("t1", h % 2), ("rq", d2)], writes=[("ostg", k)])
            p.dma(io["qml_d"][h, :, tsl], og[0:96, :], reads=[("ostg", k)], writes=[("qml_d", s, h)])
            b = nbank()
            ps = kb.ps(b)
            p.pe(lambda e, ps=ps, ckv=ckv, h=h: e.matmul(ps[0:64, :], lhsT=wukv[:, h * 128:h * 128 + 64], rhs=ckv[:], start=True, stop=True),
                 reads=["wukv", ("ckv", d2)], writes=[("ps", b)])
            k = cnt["o"] % 4
            cnt["o"] += 1
            og = ostage[k]
            p.dve(lambda e, og=og, ps=ps, rkv=rkv: e.tensor_tensor(out=og[0:64, :], in0=ps[0:64, :], in1=rkv[0:64, :], op=ALU.mult),
                  reads=[("ps", b), ("rkv", d2)], writes=[("ostg", k)])
            p.dma(io["kml_d"][h, :, tsl], og[0:64, :], reads=[("ostg", k)], writes=[("kml_d", s, h)])
        a1, a2 = t1[0], t2[0]
        k = cnt["o"] % 4
        cnt["o"] += 1
        og = ostage[k]
        p.dve(lambda e, a1=a1, krr=krr, rpk=rpk: e.tensor_tensor(out=a1[0:32, :], in0=krr[:, 0, :], in1=rpk[:, 0, :], op=ALU.mult),
              reads=[("krr", d2), ("rpk", d2)], writes=[("t1", 0)])
        p.dve(lambda e, a2=a2, krr=krr, rpk=rpk: e.tensor_tensor(out=a2[0:32, :], in0=krr[:, 1, :], in1=rpk[:, 1, :], op=ALU.mult),
              reads=[("krr", d2), ("rpk", d2)], writes=[("t2", 0)])
        p.dve(lambda e, a1=a1, a2=a2, og=og: e.tensor_tensor(out=og[0:32, :], in0=a1[0:32, :], in1=a2[0:32, :], op=ALU.add),
              reads=[("t1", 0), ("t2", 0)], writes=[("ostg", k)])
        p.dma(io["krl_d"][:, tsl], og[0:32, :], reads=[("ostg", k)], writes=[("krl_d", s)])
        for ck in range(4):
            gck = s * 4 + ck
            b = nbank()
            ps = kb.ps(b)
            for h in range(4):
                p.pe(lambda e, ps=ps, ckv=ckv, ck=ck, h=h: e.matmul(ps[:, h * 64:(h + 1) * 64], lhsT=ckv[:, ck * 128:(ck + 1) * 128], rhs=wukv[:, h * 128 + 64:h * 128 + 128],
                                                                 start=True, stop=True),
                     reads=["wukv", ("ckv", d2)], writes=[("ps", b)])
            vm = vmst[gck % 2]
            p.act(lambda e, vm=vm, ps=ps, rkt=rkt, ck=ck: e.activation(out=vm[:], in_=ps[:, 0:256], func=AF.Copy, scale=rkt[:, ck:ck + 1]),
                  reads=[("ps", b), ("rkt", d2)], writes=[("vmst", gck % 2)])
            p.dma(io["vml_d"][gck * 128:(gck + 1) * 128, :], vm[:], reads=[("vmst", gck % 2)], writes=[("vml_d", gck)])

        for ck in range(4):
            gck = s * 4 + ck
            tcs = slice(gck * 128, (gck + 1) * 128)
            vs = vstage[gck % 2]
            b1, b2 = nbank(), nbank()
            ps1, ps2 = kb.ps(b1), kb.ps(b2)
            for f in range(8):
                p.pe(lambda e, ps1=ps1, f=f, tcs=tcs: e.matmul(ps1[:, 0:256], lhsT=hT[:, f, tcs], rhs=win[:, f, C_SBV:C_SBV + 256], start=(f == 0), stop=(f == 7)),
                     reads=[("win", f), ("hT", s)], writes=[("ps", b1)])
            for f in range(8):
                p.pe(lambda e, ps1=ps1, f=f, tcs=tcs: e.matmul(ps1[:, 256:512], lhsT=hT[:, f, tcs], rhs=win[:, f, C_MOV:C_MOV + 256], start=(f == 0), stop=(f == 7)),
                     reads=[("win", f), ("hT", s)], writes=[("ps", b1)])
            for f in range(8):
                p.pe(lambda e, ps2=ps2, f=f, tcs=tcs: e.matmul(ps2[:, 0:64], lhsT=hT[:, f, tcs], rhs=win[:, f, C_NSKV + 192:C_NSKV + 256], start=(f == 0), stop=(f == 7)),
                     reads=[("win", f), ("hT", s)], writes=[("ps", b2)])
            for f in range(8):
                p.pe(lambda e, ps2=ps2, f=f, tcs=tcs: e.matmul(ps2[:, 64:128], lhsT=hT[:, f, tcs], rhs=win[:, f, C_NSKV + 320:C_NSKV + 384], start=(f == 0), stop=(f == 7)),
                     reads=[("win", f), ("hT", s)], writes=[("ps", b2)])
            p.act(lambda e, vs=vs, ps1=ps1: e.copy(out=vs[:, 0:512], in_=ps1[:]), reads=[("ps", b1)], writes=[("vstg", gck % 2)])
            p.dve(lambda e, vs=vs, ps2=ps2: e.tensor_copy(out=vs[:, 512:640], in_=ps2[:, 0:128]), reads=[("ps", b2)], writes=[("vstg", gck % 2)])
            p.dma(io["vsb_d"][tcs, :], vs[:, 0:256], reads=[("vstg", gck % 2)], writes=[("vsb_d", gck)])
            p.dma(io["vmo_d"][tcs, :], vs[:, 256:512], reads=[("vstg", gck % 2)], writes=[("vmo_d", gck)])
            p.dma(io["vsw_d"][tcs, :], vs[:, 512:640], reads=[("vstg", gck % 2)], writes=[("vsw_d", gck)])


A_OUTS = {
    "hT_d": ([1024, TOK], BF16),
    "qsb_d": ([256, TOK], BF16), "ksb_d": ([256, TOK], BF16), "vsb_d": ([TOK, 256], BF16),
    "qmo_d": ([256, TOK], BF16), "kmo_d": ([256, TOK], BF16), "vmo_d": ([TOK, 256], BF16),
    "qml_d": ([4, 96, TOK], BF16), "kml_d": ([4, 64, TOK], BF16), "krl_d": ([32, TOK], BF16), "vml_d": ([TOK, 256], BF16),
    "qns_d": ([256, TOK], BF16), "kcv_d": ([128, TOK], BF16), "ksl_d": ([64, TOK], BF16), "kwi_d": ([64, TOK], BF16),
    "vsw_d": ([TOK, 128], BF16), "gns_d": ([12, TOK], F32), "qme_d": ([256, TOK], BF16),
}


def load_consts(kb, io):
    p = kb.p
    c = {}
    c["identf"] = kb.sb("identf", [128, 128], F32)
    c["identb"] = kb.sb("identb", [128, 128], BF16)
    c["onesb"] = kb.sb("onesb", [128, 128], BF16)
    c["onesf"] = kb.sb("onesf", [128, 128], F32)
    c["cstf"] = kb.sb("cstf", [128, 8], F32)
    p.dma(c["identf"][:], io["c_ident"], writes=["identf"])
    p.dma(c["identb"][:], io["c_ident"], writes=["identb"], q="pool")
    p.dma(c["onesf"][:], io["c_ones"], writes=["onesf"])
    p.dma(c["onesb"][:], io["c_ones"], writes=["onesb"], q="pool")
    p.dma(c["cstf"][:], io["c_cst"], writes=["cst"])
    c["eps_rms"] = c["cstf"][:, 0:1]
    c["eps_ln"] = c["cstf"][:, 1:2]
    c["tiny"] = c["cstf"][:, 2:3]
    c["zrow"] = kb.sb("zrow", [1, 8], BF16)
    p.pool(lambda e: e.memset(c["zrow"][:], 0.0), writes=["zrow"])
    return c


def host_consts():
    cst = np.zeros((128, 8), np.float32)
    cst[:, 0] = RMS_EPS
    cst[:, 1] = LN_EPS
    cst[:, 2] = 1e-30
    cst[:, 3] = 1.0
    return {"c_ident": np.eye(128, dtype=np.float32), "c_ones": np.ones((128, 128), np.float32), "c_cst": cst}


def rope_tables(r):
    half = 16
    freqs = np.power(np.float32(10000.0), -np.arange(half, dtype=np.float32) / half).astype(np.float32)
    rq = np.zeros((NSLOT, 96, 2, 512), np.float32)
    rk = np.zeros((NSLOT, 32, 2, 512), np.float32)
    sc = np.float32(96.0 ** -0.5)
    for s in range(NSLOT):
        pos = (512 * (2 * s + r) + np.arange(512)).astype(np.float32)
        ang = pos[None, :] * freqs[:, None]
        cos, sin = np.cos(ang).astype(np.float32), np.sin(ang).astype(np.float32)
        c2 = np.concatenate([cos, cos], 0)
        s2 = np.concatenate([sin, sin], 0)
        rq[s, 0:64, 0, :] = sc
        rq[s, 64:96, 0, :] = sc * c2
        rq[s, 64:96, 1, :] = sc * s2
        rk[s, :, 0, :] = c2
        rk[s, :, 1, :] = s2
    return rq, rk


def build_a():
    kb = KB()
    io = {}
    io["h_tok"] = kb.din("h_tok", [TOK, D])
    io["c_ident"] = kb.din("c_ident", [128, 128])
    io["c_ones"] = kb.din("c_ones", [128, 128])
    io["c_cst"] = kb.din("c_cst", [128, 8])
    io["ropeq_t"] = kb.din("ropeq_t", [NSLOT, 96, 2, 512])
    io["ropek_t"] = kb.din("ropek_t", [NSLOT, 32, 2, 512])
    w = {"w_in": kb.din("w_in", [D, IN_TOTAL]), "w_uq": kb.din("w_uq", [256, 384]), "g_cq": kb.din("g_cq", [256]),
         "w_ukv": kb.din("w_ukv", [128, 512]), "g_ckv": kb.din("g_ckv", [128])}
    for n, (shp, dt) in A_OUTS.items():
        io[n] = kb.dout(n, shp, dt)
    c = load_consts(kb, io)
    phase_a(kb, c, io, w)
    return kb.finish()


def bconsts_host(r):
    bf = ml_dtypes.bfloat16
    o = {}
    kl = np.arange(128)[:, None]
    ql = np.arange(512)[None, :]
    cm = np.zeros((8, 128, 512), np.float32)
    cms = np.zeros((8, 128, 512), np.float32)
    for jj in range(8):
        kp = 128 * jj + kl
        qp = 512 * r + ql
        cm[jj] = np.where(kp <= qp, 0.0, MASKV)
        cms[jj] = np.where(kp < qp, 0.0, MASKV)
    o["c_cm"] = cm.transpose(1, 0, 2).astype(bf)
    o["c_cms"] = cms.transpose(1, 0, 2).astype(bf)
    wm = np.zeros((12, 128, 512), np.float32)
    for ji, jrel in enumerate(range(-4, 8)):
        dist = 512 * r + ql - 128 * jrel - kl
        wm[ji] = np.where((dist >= 0) & (dist < 512), 0.0, MASKV)
    o["c_wm"] = wm.transpose(1, 0, 2).astype(bf)
    pm = np.zeros((3, 128, 512), np.float32)
    for ii, idx in enumerate((6, 7, 8)):
        pm[ii] = np.where(16 * kl + 31 - 512 * r - ql <= 1024 * (idx - 6), 0.0, MASKV)
    o["c_pm"] = pm.transpose(1, 0, 2).astype(bf)
    kp = np.arange(S)
    o["c_kaug"] = np.stack([kp // 128, kp % 128, np.ones(S), np.ones(S)]).astype(bf)
    ce = 16 * np.arange(512) + 31
    caug = np.stack([ce // 128, ce % 128, np.ones(512), np.ones(512)]).astype(np.float32)
    caug[0, 511] = -30000.0
    o["c_caug"] = caug.astype(bf)
    tl = np.arange(TOK)
    qp = 512 * (2 * (tl // 512) + r) + tl % 512
    qa = np.zeros((4, 4, TOK), np.float32)
    for h in range(4):
        sl = SLOPES[h]
        qa[h, 0] = 128 * sl
        qa[h, 1] = sl
        qa[h, 2] = -sl * 128 * (qp // 128)
        qa[h, 3] = -sl * (qp % 128)
    o["c_qaug"] = qa.astype(bf)
    o["c_g32"] = ((np.arange(S)[None, :] // 64) % 32 == np.arange(32)[:, None]).astype(np.float32).astype(bf)
    o["c_tm"] = (np.arange(S)[None, :] // 256 == np.arange(32)[:, None]).astype(np.float32).astype(bf)
    n = np.arange(512)[:, None]
    s_ = np.arange(128)[None, :]
    ov = ((16 * n < 64 * s_ + 64) & (16 * n + 32 > 64 * s_)).astype(np.float32)
    ov = np.concatenate([ov, np.ones((512, 1), np.float32)], 1)
    ov[511] = 0.0
    o["c_nui"] = -(np.arange(128)[:, None] >= np.arange(128)[None, :]).astype(np.float32).astype(bf)
    o["c_ov"] = ov.reshape(4, 128, 129).transpose(1, 0, 2).astype(bf)
    vb = np.zeros((8, 4, 32), np.float32)
    own_t = np.zeros((8, 4, 32), np.float32)
    for s in range(8):
        for qb in range(4):
            own = (4 * (2 * s + r) + qb) // 2
            vb[s, qb] = np.where(np.arange(32) < own, 0.0, -1e30)
            own_t[s, qb, own] = 1.0
    o["c_vb"] = np.broadcast_to(vb[None], (128, 8, 4, 32)).copy()
    o["c_own"] = np.broadcast_to(own_t[None], (128, 8, 4, 32)).copy()
    M = np.zeros((8, 128, 4, 128), np.float32)
    C = np.zeros((8, 128, 4, 128), np.float32)
    sid = np.arange(128)[None, :]
    for s in range(8):
        for qb in range(4):
            qpos = 512 * (2 * s + r) + 128 * qb + np.arange(128)[:, None]
            cur = qpos // 64
            forced_cur = sid == cur
            forced0 = (sid == 0) & ~forced_cur
            past = (sid < cur) & ~forced0 & ~forced_cur
            M[s, :, qb] = past
            C[s, :, qb] = np.where(forced_cur, 1e30, np.where(forced0, 5e29, np.where(past, 0.0, -1e30)))
    o["c_selm"] = M
    o["c_selc"] = C
    return o


B_CONST_SHAPES = {
    "c_cm": ([128, 8, 512], BF16), "c_cms": ([128, 8, 512], BF16), "c_wm": ([128, 12, 512], BF16), "c_pm": ([128, 3, 512], BF16),
    "c_kaug": ([4, S], BF16), "c_caug": ([4, 512], BF16), "c_qaug": ([4, 4, TOK], BF16),
    "c_g32": ([32, S], BF16), "c_tm": ([32, S], BF16), "c_ov": ([128, 4, 129], BF16),
    "c_nui": ([128, 128], BF16), "c_vb": ([128, 8, 4, 32], F32), "c_own": ([128, 8, 4, 32], F32),
    "c_selm": ([8, 128, 4, 128], F32), "c_selc": ([8, 128, 4, 128], F32),
}

B_INS = {
    "qsb_d": ([256, TOK], BF16), "qmo_d": ([256, TOK], BF16), "qml_d": ([4, 96, TOK], BF16), "qns_d": ([256, TOK], BF16),
    "qme_d": ([256, TOK], BF16), "gns_d": ([12, TOK], F32),
    "ksb_f": ([256, S], BF16), "vsb_f": ([S, 256], BF16), "kmo_f": ([256, S], BF16), "vmo_f": ([S, 256], BF16),
    "kml_f": ([4, 64, S], BF16), "krl_f": ([32, S], BF16), "vml_f": ([S, 256], BF16),
    "kcv_f": ([128, S], BF16), "ksl_f": ([64, S], BF16), "kwi_f": ([64, S], BF16), "vsw_f": ([S, 128], BF16),
    "mem": ([256, D], F32),
}


class AttnBufs:
    pass


class KFull:
    def __init__(self, ap):
        self.ap = ap

    def rows(self, lo, hi):
        return ("full", self.ap[lo:hi, :])


class KPair:
    def __init__(self, ap, R, base=0):
        self.ap, self.R, self.base = ap, R, base

    def rows(self, lo, hi):
        return ("pair", self.ap, self.R, self.base + lo, hi - lo)


class VFull:
    def __init__(self, ap, cbase=0):
        self.ap, self.cbase = ap, cbase

    def cols(self, c0):
        return ("full", self.ap, self.cbase + c0)


class VPair:
    def __init__(self, chunks, cbase=0):
        self.chunks, self.cbase = chunks, cbase

    def cols(self, c0):
        return ("pair", self.chunks, self.cbase + c0)


class TMChunks:
    def __init__(self, chunks, c0, c1):
        self.chunks, self.c0, self.c1 = chunks, c0, c1

    def __getitem__(self, key):
        rs, cs = key
        k = rs.start // 1024
        a = self.chunks[k][rs.start - 1024 * k:rs.stop - 1024 * k, self.c0:self.c1]
        return a[:, cs]


def phase_b(kb, c, io, w, branches=("sb", "moba", "mla", "nsa", "mem")):
    nc, p = kb.nc, kb.p
    A = AttnBufs()
    A.KT = [kb.sb("KT", [128, S], BF16) for _ in range(2)]
    A.V = [kb.sb("V", [128, 64, 65], BF16) for _ in range(2)]
    A.QT = kb.sb("QT", [128, 4, TOK], BF16)
    A.cm = kb.sb("cm", [128, 8, 512], BF16)
    A.cms = kb.sb("cms", [128, 8, 512], BF16)
    A.P = [kb.sb("P", [128, 512], BF16) for _ in range(3)]
    A.rden = [kb.sb("rden", [65, 512], F32) for _ in range(2)]
    A.bcs = [kb.sb("bcs", [64, 512], F32) for _ in range(2)]
    A.ost = [kb.sb("ost", [64, 512], BF16) for _ in range(2)]
    A.cnt = {"kt": 0, "v": 0, "P": 0, "fin": 0, "sc": 0}
    p.dma(A.cm[:], io["c_cm"], writes=["cm"])
    p.dma(A.cms[:], io["c_cms"], writes=["cms"])
    for i in range(2):
        p.pool(lambda e, i=i: e.memset(A.V[i][:, :, 64:65], 1.0), writes=[("V", i)])

    def load_K(rows_src, dk, aug=None):
        i = A.cnt["kt"] % 2
        A.cnt["kt"] += 1
        kt = A.KT[i]
        for (src, r0) in rows_src:
            if src[0] == "full":
                n = src[1].shape[0]
                p.dma(kt[r0:r0 + n, :], src[1], writes=[("KT", i)])
            else:
                _, ap, R, row0, n = src
                for rr in range(2):
                    p.dma(kt[r0:r0 + n, :].rearrange("p (s r i) -> p s r i", r=2, i=512)[:, :, rr, :],
                          ap[rr * R + row0:rr * R + row0 + n, :].rearrange("p (s i) -> p s i", i=512), writes=[("KT", i)])
        if aug is not None:
            p.dma(kt[dk:dk + 4, :], aug, writes=[("KT", i)])
        return kt, ("KT", i)

    def load_V(src, col0):
        i = A.cnt["v"] % 2
        A.cnt["v"] += 1
        v = A.V[i]
        sp_ = src.cols(col0)
        if sp_[0] == "full":
            p.dma(v[:, :, 0:64], sp_[1].rearrange("(n p) c -> p n c", p=128)[:, :, sp_[2]:sp_[2] + 64], writes=[("V", i)])
        else:
            _, chunks, cc0 = sp_
            for k, ch in enumerate(chunks):
                for rr in range(2):
                    for s2 in range(2):
                        n0 = 16 * k + 8 * s2 + 4 * rr
                        p.dma(v[:, n0:n0 + 4, 0:64],
                              ch[rr * 1024 + s2 * 512:rr * 1024 + (s2 + 1) * 512, cc0:cc0 + 64].rearrange("(q p) c -> p q c", p=128),
                              writes=[("V", i)])
        return v, ("V", i)

    def load_Q(src_rows, h, dk, aug=None):
        p.dma(A.QT[0:dk, h, :], src_rows, writes=[("QT", h)])
        if aug is not None:
            p.dma(A.QT[dk:dk + 4, h, :], aug, writes=[("QT", h)])
        return ("QT", h)

    def finalize_plain(ops_bank, dst, h, s, gate=None, acc=None, acc_tok=None, first=True, last=True):
        k = A.cnt["fin"] % 2
        A.cnt["fin"] += 1
        ps = kb.ps(ops_bank)
        rd, bcs, ost = A.rden[k], A.bcs[k], A.ost[k]
        bcb = 6 + k

        def part1():
            p.dve(lambda e: e.tensor_scalar(out=rd[64:65, :], in0=ps[64:65, :], scalar1=1e-30, scalar2=None, op0=ALU.max),
                  reads=[("ps", ops_bank)], writes=[("rden", k)])
            p.dve(lambda e: e.reciprocal(out=rd[64:65, :], in_=rd[64:65, :]), reads=[("rden", k)], writes=[("rden", k)])
            if gate is not None:
                gt, gtok, gidx = gate
                p.dve(lambda e: e.tensor_tensor(out=rd[64:65, :], in0=rd[64:65, :], in1=gt[64:65, gidx, :], op=ALU.mult),
                      reads=[("rden", k), gtok], writes=[("rden", k)])

        def part2():
            pb = kb.ps(bcb)
            p.pe(lambda e: e.matmul(pb[0:64, :], lhsT=c["onesf"][64:65, 0:64], rhs=rd[64:65, :], start=True, stop=True),
                 reads=[("rden", k), "onesf"], writes=[("ps", bcb)])
            p.act(lambda e: e.copy(out=bcs[:], in_=pb[0:64, :]), reads=[("ps", bcb)], writes=[("bcs", k)])
            if acc is None:
                p.dve(lambda e: e.tensor_tensor(out=ost[:], in0=ps[0:64, :], in1=bcs[:], op=ALU.mult),
                      reads=[("ps", ops_bank), ("bcs", k)], writes=[("ost", k)])
                p.dma(dst, ost[:], reads=[("ost", k)], writes=[("o_d", h, s, id(dst) % 997)])
            else:
                if first:
                    p.dve(lambda e: e.tensor_tensor(out=acc, in0=ps[0:64, :], in1=bcs[:], op=ALU.mult),
                          reads=[("ps", ops_bank), ("bcs", k)], writes=[acc_tok])
                else:
                    p.dve(lambda e: e.tensor_tensor(out=bcs[:], in0=ps[0:64, :], in1=bcs[:], op=ALU.mult),
                          reads=[("ps", ops_bank), ("bcs", k)], writes=[("bcs", k)])
                    p.dve(lambda e: e.tensor_tensor(out=acc, in0=acc, in1=bcs[:], op=ALU.add),
                          reads=[acc_tok, ("bcs", k)], writes=[acc_tok])
                if last:
                    p.dve(lambda e: e.tensor_copy(out=ost[:], in_=acc), reads=[acc_tok], writes=[("ost", k)])
                    p.dma(dst, ost[:], reads=[("ost", k)], writes=[("o_d", h, s, id(dst) % 997)])
        return part1, part2

    def run_softmax(items, KT, ktok, dk, V, vtok, qh, qtok, pending, hook=None):
        n = len(items)

        def stage1(i):
            it = items[i]
            if "pre" in it:
                it["pre"]()
            b = i % 2
            ps = kb.ps(b)
            ex = it["extras"]
            s, j = it["s"], it["j"]
            qr = it["qrhs"] if "qrhs" in it else A.QT[0:dk, qh, s * 512:(s + 1) * 512]
            p.pe(lambda e: e.matmul(ps[:], lhsT=KT[0:dk, j * 128:(j + 1) * 128], rhs=qr,
                                    start=True, stop=(len(ex) == 0)),
                 reads=[ktok, qtok] + list(it.get("qreads", ())), writes=[("ps", b)])
            for xi, (lh, rh, toks) in enumerate(ex):
                p.pe(lambda e, lh=lh, rh=rh, xi=xi: e.matmul(ps[:], lhsT=lh, rhs=rh, start=False, stop=(xi == len(ex) - 1)),
                     reads=list(toks), writes=[("ps", b)])
            if "pdst" in it:
                P, ptok = it["pdst"]
            else:
                pk = A.cnt["P"] % 3
                A.cnt["P"] += 1
                P, ptok = A.P[pk][:], ("P", pk)
            it["P"], it["ptok"] = P, ptok
            p.act(lambda e: e.activation(out=P, in_=ps[:], func=AF.Exp), reads=[("ps", b)], writes=[ptok])

        def stage2(i):
            it = items[i]
            P, ptok = it["P"], it["ptok"]
            ob = it["obank"]
            po = kb.ps(ob)
            jv = it.get("jv", it["j"])
            p.pe(lambda e: e.matmul(po[0:65, :], lhsT=V[:, jv, 0:65], rhs=P, start=it["first"], stop=it["last"]),
                 reads=[vtok, ptok], writes=[("ps", ob)])
            if it["last"]:
                p1, p2 = it["fin"]
                p1()
                pending.append([2, p2])

        for i in range(n + 1):
            if i < n:
                stage1(i)
            if i >= 1:
                stage2(i - 1)
            for pd in list(pending):
                pd[0] -= 1
                if pd[0] <= 0:
                    pd[1]()
                    pending.remove(pd)

    def flush(pending):
        for pd in pending:
            pd[1]()
        pending.clear()

    A.load_K, A.load_V, A.load_Q = load_K, load_V, load_Q
    A.finalize_plain, A.run_softmax, A.flush = finalize_plain, run_softmax, flush
    oT = io["oT_d"]

    if "mla" in branches:
        pending = []
        for h in range(4):
            KT, ktok = load_K([(io["kml_f"].rows(h * 64, h * 64 + 64), 0), (io["krl_f"].rows(0, 32), 64)], 96)
            V, vtok = load_V(io["vml_f"], h * 64)
            qtok = load_Q(io["qml_d"][h], h, 96)
            items = []
            for s in range(NSLOT):
                nkb = 8 * s + 8
                ob = 4 + (s % 2)
                for j in range(nkb):
                    jj = j - 8 * s
                    ex = []
                    if jj >= 0:
                        ex.append((c["identb"][:], A.cm[:, jj, :], ["identb", "cm"]))
                    it = dict(j=j, s=s, extras=ex, first=(j == 0), last=(j == nkb - 1), obank=ob)
                    if j == nkb - 1:
                        it["fin"] = finalize_plain(ob, oT[2, h * 64:(h + 1) * 64, s * 512:(s + 1) * 512], h, s)
                    items.append(it)
            run_softmax(items, KT, ktok, 96, V, vtok, h, qtok, pending)
        flush(pending)

    if "mem" in branches:
        pending = []
        memst = kb.sb("memst", [128, 2, 1024], F32)
        memT = kb.sb("memT", [128, 8, 256], BF16)
        wmk = kb.sb("wmk", [128, 8, 512], BF16)
        KTm = kb.sb("KTm", [64, 4, 256], BF16)
        Vm = kb.sb("Vm", [128, 2, 4, 65], BF16)
        p.dma(memst[:], io["mem"].rearrange("(n p) c -> p n c", p=128), writes=["memst"])
        p.dma(wmk[:], w["w_mem_kv"].rearrange("(f p) c -> p f c", p=128), writes=["wmk"], q="pool")
        p.pool(lambda e: e.memset(Vm[:, :, :, 64:65], 1.0), writes=["Vm"])
        for kc in range(2):
            for half in range(2):
                ps = kb.ps(7)
                for jx in range(4):
                    f = half * 4 + jx
                    p.pe(lambda e, ps=ps, kc=kc, f=f, jx=jx: e.transpose(out=ps[:, jx * 128:(jx + 1) * 128], in_=memst[:, kc, f * 128:(f + 1) * 128], identity=c["identf"][:]),
                         reads=["memst", "identf"], writes=[("ps", 7)])
                p.dve(lambda e, ps=ps, kc=kc, half=half: e.tensor_copy(out=memT[:, half * 4:half * 4 + 4, kc * 128:(kc + 1) * 128], in_=ps[:].rearrange("p (j t) -> p j t", j=4)),
                      reads=[("ps", 7)], writes=["memT"])
        for h in range(4):
            ps = kb.ps(7)
            for f in range(8):
                p.pe(lambda e, ps=ps, f=f, h=h: e.matmul(ps[0:64, 0:256], lhsT=wmk[:, f, h * 64:(h + 1) * 64], rhs=memT[:, f, :], start=(f == 0), stop=(f == 7)),
                     reads=["wmk", "memT"], writes=[("ps", 7)])
            p.dve(lambda e, ps=ps, h=h: e.tensor_copy(out=KTm[:, h, :], in_=ps[0:64, 0:256]), reads=[("ps", 7)], writes=["KTm"])
        for kc in range(2):
            ps = kb.ps(7)
            for f in range(8):
                p.pe(lambda e, ps=ps, f=f, kc=kc: e.matmul(ps[:, 0:256], lhsT=memT[:, f, kc * 128:(kc + 1) * 128], rhs=wmk[:, f, 256:512], start=(f == 0), stop=(f == 7)),
                     reads=["wmk", "memT"], writes=[("ps", 7)])
            p.dve(lambda e, ps=ps, kc=kc: e.tensor_copy(out=Vm[:, kc, :, 0:64], in_=ps[:, 0:256].rearrange("p (h d) -> p h d", h=4)),
                  reads=[("ps", 7)], writes=["Vm"])
        for h in range(4):
            qtok = load_Q(io["qme_d"][h * 64:(h + 1) * 64, :], h, 64)
            items = []
            for s in range(NSLOT):
                ob = 4 + (s % 2)
                for j in range(2):
                    it = dict(j=j, s=s, extras=[], first=(j == 0), last=(j == 1), obank=ob)
                    if j == 1:
                        it["fin"] = finalize_plain(ob, oT[4, h * 64:(h + 1) * 64, s * 512:(s + 1) * 512], h, s)
                    items.append(it)
            run_softmax(items, KTm[:, h, :], "KTm", 64, Vm[:, :, h, :], "Vm", h, qtok, pending)
        flush(pending)

    if "moba" in branches:
        pending = []
        vbt = kb.sb("vbt", [128, 8, 4, 32], F32)
        ownt = kb.sb("ownt", [128, 8, 4, 32], F32)
        kmf = kb.sb("kmf", [64, 32], F32)
        kmb = kb.sb("kmb", [64, 32], BF16)
        gsv = kb.sb("gsv", [128, 4, 32], F32)
        m8 = kb.sb("m8", [128, 4, 8], F32)
        m1p = kb.sb("m1p", [128, 4, 128], F32)
        m1 = m1p[:, :, 64:96]
        m2 = kb.sb("m2", [128, 4, 32], F32)
        p.pool(lambda e: e.memset(m1p[:], 0.0), writes=["m1"])
        p.dma(vbt[:], io["c_vb"], writes=["vbt"])
        p.dma(ownt[:], io["c_own"], writes=["ownt"])
        for h in range(4):
            KT, ktok = load_K([(io["kmo_f"].rows(h * 64, h * 64 + 64), 0), (("full", io["c_tm"]), 64)], 96, aug=io["c_kaug"])
            V, vtok = load_V(io["vmo_f"], h * 64)
            qtok = ("QT", h)
            p.dma(A.QT[0:64, h, :], io["qmo_d"][h * 64:(h + 1) * 64, :], writes=[qtok])
            p.dma(A.QT[96:100, h, :], io["c_qaug"][h], writes=[qtok])
            p.dve(lambda e, KT=KT: e.tensor_reduce(out=kmf[:], in_=KT[0:64, :].rearrange("p (n k) -> p n k", k=256), axis=AX.X, op=ALU.add),
                  reads=[ktok], writes=["kmf"])
            p.dve(lambda e: e.tensor_scalar_mul(out=kmb[:], in0=kmf[:], scalar1=1.0 / 256.0), reads=["kmf"], writes=["kmb"])

            def make_pre(s, h=h, qtok=qtok):
                def pre():
                    ps = kb.ps(7)
                    for qb in range(4):
                        c0 = s * 512 + qb * 128
                        p.pe(lambda e, qb=qb, c0=c0: e.matmul(ps[:, qb * 32:(qb + 1) * 32], lhsT=A.QT[0:64, h, c0:c0 + 128], rhs=kmb[:], start=True, stop=True),
                             reads=[qtok, "kmb"], writes=[("ps", 7)])
                    p.dve(lambda e: e.tensor_tensor(out=gsv[:], in0=ps[:, 0:128].rearrange("p (a b) -> p a b", a=4), in1=vbt[:, s, :, :], op=ALU.add),
                          reads=[("ps", 7), "vbt"], writes=["gsv"])
                    for qb in range(4):
                        p.dve(lambda e, qb=qb: e.max(out=m8[:, qb, :], in_=gsv[:, qb, :]), reads=["gsv"], writes=["m8"])
                    for qb in range(4):
                        p.dve(lambda e, qb=qb: e.tensor_scalar(out=m1[:, qb, :], in0=gsv[:, qb, :], scalar1=m8[:, qb, 2:3], scalar2=None, op0=ALU.is_ge),
                              reads=["gsv", "m8"], writes=["m1"])
                    p.dve(lambda e: e.tensor_scalar(out=m2[:], in0=gsv[:], scalar1=-1e29, scalar2=None, op0=ALU.is_gt), reads=["gsv"], writes=["m2"])
                    p.dve(lambda e: e.tensor_tensor(out=m1, in0=m1, in1=m2[:], op=ALU.mult), reads=["m1", "m2"], writes=["m1"])
                    p.dve(lambda e: e.tensor_tensor(out=m1, in0=m1, in1=ownt[:, s, :, :], op=ALU.add), reads=["m1", "ownt"], writes=["m1"])
                    p.dve(lambda e: e.tensor_scalar(out=m1, in0=m1, scalar1=1.0, scalar2=-MASKV, op0=ALU.subtract, op1=ALU.mult),
                          reads=["m1"], writes=["m1"])

                def pre2():
                    ps2 = kb.ps(7)
                    for qb in range(4):
                        p.pe(lambda e, qb=qb: e.transpose(out=ps2[:, qb * 128:(qb + 1) * 128], in_=m1p[:, qb, :], identity=c["identf"][:]),
                             reads=["m1", "identf"], writes=[("ps", 7)])
                    p.act(lambda e: e.copy(out=A.QT[64:96, h, s * 512:(s + 1) * 512], in_=ps2[64:96, :]), reads=[("ps", 7)], writes=[("QTs", h, s)])
                return pre, pre2

            items = []
            for s in range(NSLOT):
                nkb = 8 * s + 8
                ob = 4 + (s % 2)
                for j in range(nkb):
                    jj = j - 8 * s
                    ex = []
                    if jj >= 0:
                        ex.append((c["identb"][:], A.cm[:, jj, :], ["identb", "cm"]))
                    it = dict(j=j, s=s, extras=ex, first=(j == 0), last=(j == nkb - 1), obank=ob, qreads=[("QTs", h, s)])
                    if j == nkb - 1:
                        it["fin"] = finalize_plain(ob, oT[1, h * 64:(h + 1) * 64, s * 512:(s + 1) * 512], h, s)
                    items.append(it)
            first_of = {}
            for idx, it in enumerate(items):
                if it["j"] == 0:
                    first_of[it["s"]] = idx
            hooks = {}
            for s in range(NSLOT):
                pa, pb_ = make_pre(s)
                f0 = first_of[s]
                i1 = max(0, f0 - 14) if s > 0 else 0
                i2 = max(i1, f0 - 2) if s > 0 else 0
                hooks.setdefault(i1, []).append(pa)
                hooks.setdefault(i2, []).append(pb_)
            for idx, fl in hooks.items():
                items[idx]["pre"] = (lambda fl=fl: [f() for f in fl])
            run_softmax(items, KT, ktok, 100, V, vtok, h, qtok, pending)
        flush(pending)

    if "sb" in branches:
        nui = kb.sb("nui", [128, 128], BF16)
        negone = kb.sb("negone", [1, 128], BF16)
        p.dma(nui[:], io["c_nui"], writes=["nui"])
        p.pool(lambda e: e.memset(negone[:], -1.0), writes=["negone"])
        E = [kb.sb("E", [128, 512], F32) for _ in range(2)]
        SP = [kb.sb("SP", [128, 512], BF16) for _ in range(2)]
        AB = [kb.sb("AB", [128, 512], BF16) for _ in range(2)]
        carf = [kb.sb("carf", [1, 512], F32) for _ in range(2)]
        carb = [kb.sb("carb", [1, 512], BF16) for _ in range(2)]
        sbo = [kb.sb("sbo", [64, 512], BF16) for _ in range(2)]
        one_ap = c["cstf"][:, 3:4]
        for hp in range(2):
            hs = (2 * hp, 2 * hp + 1)
            KTs, ktoks, Vs, vtoks, qtoks = [], [], [], [], []
            for h in hs:
                KT, ktok = load_K([(io["ksb_f"].rows(h * 64, h * 64 + 64), 0)], 64)
                V, vtok = load_V(io["vsb_f"], h * 64)
                qtok = load_Q(io["qsb_d"][h * 64:(h + 1) * 64, :], h, 64)
                KTs.append(KT); ktoks.append(ktok); Vs.append(V); vtoks.append(vtok); qtoks.append(qtok)
            merged = []
            for s_ in range(NSLOT):
                nkb = 8 * s_ + 8
                for j in range(nkb - 1, -1, -1):
                    for st in range(2):
                        merged.append(dict(j=j, s=s_, st=st, h=hs[st], first=(j == nkb - 1), last=(j == 0)))
            for i, it in enumerate(merged):
                it["i"] = i

            def qk(it, bank, more):
                ps = kb.ps(bank)
                j, s_, st, h = it["j"], it["s"], it["st"], it["h"]
                jj = j - 8 * s_
                KT = KTs[st]
                p.pe(lambda e: e.matmul(ps[:], lhsT=KT[0:64, j * 128:(j + 1) * 128], rhs=A.QT[0:64, h, s_ * 512:(s_ + 1) * 512],
                                        start=True, stop=(jj < 0 and not more)),
                     reads=[ktoks[st], qtoks[st]], writes=[("ps", bank)])
                if jj >= 0:
                    p.pe(lambda e: e.matmul(ps[:], lhsT=c["identb"][:], rhs=A.cms[:, jj, :], start=False, stop=(not more)),
                         reads=["identb", "cms"], writes=[("ps", bank)])
                return ps

            def s1(it):
                k = it["i"] % 2
                b4 = it["i"] % 4
                ps = qk(it, b4, False)
                p.act(lambda e: e.activation(out=E[k][:], in_=ps[:], func=AF.Exp), reads=[("ps", b4)], writes=[("E", k)])
                p.act(lambda e: e.activation(out=SP[k][:], in_=E[k][:], func=AF.Ln, bias=one_ap), reads=[("E", k), "cst"], writes=[("SP", k)])

            def s2a(it):
                k = it["i"] % 2
                b4 = it["i"] % 4
                st = it["st"]
                ps = kb.ps(b4)
                fin_carry = not it["first"]
                p.pe(lambda e: e.matmul(ps[:], lhsT=nui[:], rhs=SP[k][:], start=False, stop=(not fin_carry)),
                     reads=["nui", ("SP", k)], writes=[("ps", b4)])
                if fin_carry:
                    p.pe(lambda e: e.matmul(ps[:], lhsT=negone[0:1, :], rhs=carb[st][0:1, :], start=False, stop=True),
                         reads=["negone", ("carb", st)], writes=[("ps", b4)])
                if not it["last"]:
                    pc = kb.ps(6 + st)
                    p.pe(lambda e: e.matmul(pc[0:1, :], lhsT=c["onesb"][:, 0:1], rhs=SP[k][:], start=True, stop=True),
                         reads=["onesb", ("SP", k)], writes=[("ps", 6 + st)])
                    if it["first"]:
                        p.dve(lambda e: e.tensor_copy(out=carf[st][:], in_=pc[0:1, :]), reads=[("ps", 6 + st)], writes=[("carf", st)])
                    else:
                        p.dve(lambda e: e.tensor_tensor(out=carf[st][:], in0=carf[st][:], in1=pc[0:1, :], op=ALU.add),
                              reads=[("ps", 6 + st), ("carf", st)], writes=[("carf", st)])
                    p.dve(lambda e: e.tensor_copy(out=carb[st][:], in_=carf[st][:]), reads=[("carf", st)], writes=[("carb", st)])
                p.act(lambda e: e.activation(out=AB[k][:], in_=ps[:], func=AF.Exp), reads=[("ps", b4)], writes=[("AB", k)])

            def s2b(it):
                k = it["i"] % 2
                st, j, s_, h = it["st"], it["j"], it["s"], it["h"]
                po = kb.ps(4 + st)
                V = Vs[st]
                p.pe(lambda e: e.matmul(po[0:64, :], lhsT=V[:, j, 0:64], rhs=AB[k][:], start=it["first"], stop=it["last"]),
                     reads=[vtoks[st], ("AB", k)], writes=[("ps", 4 + st)])
                if it["last"]:
                    p.dve(lambda e: e.tensor_copy(out=sbo[st][:], in_=po[0:64, :]), reads=[("ps", 4 + st)], writes=[("sbo", st)])
                    p.dma(oT[0, h * 64:(h + 1) * 64, s_ * 512:(s_ + 1) * 512], sbo[st][:], reads=[("sbo", st)], writes=[("o_sb", h, s_)])

            n = len(merged)
            for i in range(n + 2):
                if i < n:
                    s1(merged[i])
                if 1 <= i <= n:
                    s2a(merged[i - 1])
                if i >= 2:
                    s2b(merged[i - 2])

    if "nsa" in branches:
        pending = []
        QS = kb.sb("QS", [128, 4, 4, 512], BF16)
        OV = kb.sb("OV", [128, 4, 129], BF16)
        pm = kb.sb("pm", [128, 3, 512], BF16)
        wm = kb.sb("wm", [128, 12, 512], BF16)
        wphi = kb.sb("wphi", [128, 32, 128], BF16)
        w2 = kb.sb("w2", [128, 2, 64], BF16)
        peT = kb.sb("peT", [128, 32], BF16)
        peb = kb.sb("peb", [128, 2], F32)
        gx = kb.sb("gx", [128, 512], F32)
        gt_ = kb.sb("gtmp", [128, 512], F32)
        gact = [kb.sb("gact", [128, 512], BF16) for _ in range(2)]
        kcT = kb.sb("kcT", [68, 512], BF16)
        Vc = kb.sb("Vc", [128, 4, 65], BF16)
        psave = kb.sb("psave", [128, 4, 4, 512], BF16)
        gts = [kb.sb("gts", [65, 512], F32) for _ in range(4)]
        nacc = kb.sb("nacc", [64, 4, 512], F32)
        impacc = kb.sb("impacc", [128, 4, 128], F32)
        rdn = kb.sb("rdn", [128, 2], F32)
        selm = [kb.sb("selm", [128, 4, 128], F32) for _ in range(2)]
        selc = [kb.sb("selc", [128, 4, 128], F32) for _ in range(2)]
        scr2 = kb.sb("scr2", [128, 4, 128], F32)
        m16 = kb.sb("m16", [128, 4, 16], F32)
        selvp = kb.sb("selvp", [128, 4, 256], F32)
        selv = selvp[:, :, 64:192]
        gcnt = {"g": 0}
        p.pool(lambda e: e.memset(selvp[:], 0.0), writes=["selv"])
        p.dma(OV[:], io["c_ov"], writes=["OV"])
        p.dma(pm[:], io["c_pm"], writes=["pm"])
        p.dma(wm[:], io["c_wm"], writes=["wm"])
        p.dma(wphi[0:64, :, :], w["w_phi_k1"].rearrange("(t d) h -> d t h", d=64), writes=["wphi"], q="pool")
        p.dma(wphi[64:128, :, :], w["w_phi_v1"].rearrange("(t d) h -> d t h", d=64), writes=["wphi"], q="pool")
        p.dma(w2[:, 0, :], w["w_phi_k2"], writes=["w2"], q="pool")
        p.dma(w2[:, 1, :], w["w_phi_v2"], writes=["w2"], q="pool")
        p.dma(peT[0:64, :], w["nsa_pe"].rearrange("t d -> d t"), writes=["peT"], q="pool", slow=True)
        p.dma(peT[64:128, :], w["nsa_pe"].rearrange("t d -> d t"), writes=["peT"], q="pool", slow=True)
        kcv, kcvtok = load_K([(io["kcv_f"].rows(0, 128), 0)], 128)
        p.pool(lambda e: e.memset(kcT[:], 0.0), writes=["kcT"])
        p.pool(lambda e: e.memset(Vc[:, :, 64:65], 1.0), writes=["Vc"])
        for which in range(2):
            lo = which * 64
            pb = kb.ps(7)
            for t in range(32):
                p.pe(lambda e, t=t, lo=lo, pb=pb: e.matmul(pb[:, 0:1], lhsT=wphi[lo:lo + 64, t, :], rhs=peT[lo:lo + 64, t:t + 1], start=(t == 0), stop=(t == 31)),
                     reads=["wphi", "peT"], writes=[("ps", 7)])
            p.dve(lambda e, pb=pb, which=which: e.tensor_copy(out=peb[:, which:which + 1], in_=pb[:, 0:1]), reads=[("ps", 7)], writes=["peb"])
            ph = kb.ps(6)
            for t in range(32):
                p.pe(lambda e, t=t, lo=lo, ph=ph: e.matmul(ph[:, 0:511], lhsT=wphi[lo:lo + 64, t, :], rhs=kcv[lo:lo + 64, t:t + 16 * 510 + 1:16], start=(t == 0), stop=(t == 31)),
                     reads=["wphi", kcvtok], writes=[("ps", 6)])
            ga = gact[which]
            p.act(lambda e, ph=ph, which=which: e.activation(out=gx[:, 0:511], in_=ph[:, 0:511], func=AF.Identity, bias=peb[:, which:which + 1]),
                  reads=[("ps", 6), "peb"], writes=["gx"])
            p.dve(lambda e: e.tensor_tensor(out=gt_[:, 0:511], in0=gx[:, 0:511], in1=gx[:, 0:511], op=ALU.mult), reads=["gx"], writes=["gtmp"])
            p.dve(lambda e: e.tensor_scalar(out=gt_[:, 0:511], in0=gt_[:, 0:511], scalar1=0.044715, scalar2=1.0, op0=ALU.mult, op1=ALU.add), reads=["gtmp"], writes=["gtmp"])
            p.dve(lambda e: e.tensor_tensor(out=gt_[:, 0:511], in0=gt_[:, 0:511], in1=gx[:, 0:511], op=ALU.mult), reads=["gtmp", "gx"], writes=["gtmp"])
            p.act(lambda e: e.activation(out=gt_[:, 0:511], in_=gt_[:, 0:511], func=AF.Tanh, scale=0.7978845608028654), reads=["gtmp"], writes=["gtmp"])
            p.dve(lambda e: e.tensor_scalar(out=gt_[:, 0:511], in0=gt_[:, 0:511], scalar1=1.0, scalar2=0.5, op0=ALU.add, op1=ALU.mult), reads=["gtmp"], writes=["gtmp"])
            p.pool(lambda e, ga=ga: e.memset(ga[:], 0.0), writes=[("gact", which)])
            p.dve(lambda e, ga=ga: e.tensor_tensor(out=ga[:, 0:511], in0=gt_[:, 0:511], in1=gx[:, 0:511], op=ALU.mult), reads=["gtmp", "gx"], writes=[("gact", which)])
        pk_ = kb.ps(7)
        p.pe(lambda e: e.matmul(pk_[0:64, :], lhsT=w2[:, 0, :], rhs=gact[0][:], start=True, stop=True), reads=["w2", ("gact", 0)], writes=[("ps", 7)])
        p.dve(lambda e: e.tensor_copy(out=kcT[0:64, 0:511], in_=pk_[0:64, 0:511]), reads=[("ps", 7)], writes=["kcT"])
        p.dma(kcT[64:68, :], io["c_caug"], writes=["kcT"])
        pv_ = kb.ps(6)
        for cc in range(4):
            p.pe(lambda e, cc=cc: e.matmul(pv_[:, cc * 64:(cc + 1) * 64], lhsT=gact[1][:, cc * 128:(cc + 1) * 128], rhs=w2[:, 1, :], start=True, stop=True),
                 reads=["w2", ("gact", 1)], writes=[("ps", 6)])
        p.dve(lambda e: e.tensor_copy(out=Vc[:, :, 0:64], in_=pv_[:, 0:256].rearrange("p (c d) -> p c d", c=4)), reads=[("ps", 6)], writes=["Vc"])

        KsT, kstok = load_K([(io["ksl_f"].rows(0, 64), 0), (("full", io["c_g32"]), 64)], 96, aug=io["c_kaug"])
        KwT, kwtok = load_K([(io["kwi_f"].rows(0, 64), 0)], 64, aug=io["c_kaug"])
        Vs, vstok = load_V(io["vsw_f"], 0)
        Vw, vwtok = load_V(io["vsw_f"], 64)
        qtoks = [load_Q(io["qns_d"][h * 64:(h + 1) * 64, :], h, 64, aug=io["c_qaug"][h]) for h in range(4)]

        def gate_row(h, br, s):
            k = gcnt["g"] % 4
            gcnt["g"] += 1
            g = gts[k]
            p.dma(g[64:65, :], io["gns_d"][3 * h + br:3 * h + br + 1, s * 512:(s + 1) * 512], writes=[("gts", k)])
            return (g[:].rearrange("p (o n) -> p o n", o=1), ("gts", k), 0)

        for s in range(NSLOT):
            sl = slice(s * 512, (s + 1) * 512)
            ncmp = s // 2 + 1
            dst = lambda h: oT[3, h * 64:(h + 1) * 64, sl]
            p.dma(selm[s % 2][:], io["c_selm"][s], writes=[("selm", s % 2)])
            p.dma(selc[s % 2][:], io["c_selc"][s], writes=[("selc", s % 2)])
            for h in range(4):
                items = []
                for cc in range(ncmp):
                    idx = s - 2 * cc + 6
                    ex = []
                    if idx <= 8:
                        ex.append((c["identb"][:], pm[:, idx - 6, :], ["identb", "pm"]))
                    it = dict(j=cc, s=s, extras=ex, first=(cc == 0), last=(cc == ncmp - 1), obank=4 + (h % 2),
                              pdst=(psave[:, h, cc, :], ("psave", h, cc)))
                    if cc == ncmp - 1:
                        it["fin"] = finalize_plain(4 + (h % 2), dst(h), h, s, gate=gate_row(h, 0, s), acc=nacc[:, h, :], acc_tok=("nacc", h), first=True, last=False)
                    items.append(it)
                run_softmax(items, kcT, "kcT", 68, Vc, "Vc", h, qtoks[h], pending)
            flush(pending)
            for qb in range(4):
                for h in range(4):
                    bk = 6 + ((qb * 4 + h) % 2)
                    pi = kb.ps(bk)
                    for cc in range(ncmp):
                        p.pe(lambda e, pi=pi, h=h, cc=cc, qb=qb: e.matmul(pi[:, 0:129], lhsT=psave[:, h, cc, qb * 128:(qb + 1) * 128], rhs=OV[:, cc, :],
                                                                          start=(cc == 0), stop=(cc == ncmp - 1)),
                             reads=[("psave", h, cc), "OV"], writes=[("ps", bk)])
                    p.dve(lambda e, pi=pi: e.tensor_scalar(out=rdn[:, 0:1], in0=pi[:, 128:129], scalar1=1e-30, scalar2=None, op0=ALU.max),
                          reads=[("ps", bk)], writes=["rdn"])
                    p.dve(lambda e: e.reciprocal(out=rdn[:, 1:2], in_=rdn[:, 0:1]), reads=["rdn"], writes=["rdn"])
                    if h == 0:
                        p.dve(lambda e, pi=pi, qb=qb: e.tensor_scalar(out=impacc[:, qb, :], in0=pi[:, 0:128], scalar1=rdn[:, 1:2], scalar2=None, op0=ALU.mult),
                              reads=[("ps", bk), "rdn"], writes=["impacc"])
                    else:
                        p.dve(lambda e, pi=pi, qb=qb: e.scalar_tensor_tensor(out=impacc[:, qb, :], in0=pi[:, 0:128], scalar=rdn[:, 1:2], in1=impacc[:, qb, :],
                                                                             op0=ALU.mult, op1=ALU.add),
                              reads=[("ps", bk), "rdn", "impacc"], writes=["impacc"])
            sm, sc_ = selm[s % 2], selc[s % 2]
            p.dve(lambda e, sm=sm: e.tensor_tensor(out=impacc[:], in0=impacc[:], in1=sm[:], op=ALU.mult), reads=["impacc", ("selm", s % 2)], writes=["impacc"])
            p.dve(lambda e, sc_=sc_: e.tensor_tensor(out=impacc[:], in0=impacc[:], in1=sc_[:], op=ALU.add), reads=["impacc", ("selc", s % 2)], writes=["impacc"])
            for qb in range(4):
                p.dve(lambda e, qb=qb: e.max(out=m16[:, qb, 0:8], in_=impacc[:, qb, :]), reads=["impacc"], writes=["m16"])
                p.dve(lambda e, qb=qb: e.match_replace(out=scr2[:, qb, :], in_to_replace=m16[:, qb, 0:8], in_values=impacc[:, qb, :], imm_value=-3.0e38),
                      reads=["impacc", "m16"], writes=["scr2"])
                p.dve(lambda e, qb=qb: e.max(out=m16[:, qb, 8:16], in_=scr2[:, qb, :]), reads=["scr2"], writes=["m16"])
                p.dve(lambda e, qb=qb: e.tensor_scalar(out=selv[:, qb, :], in0=impacc[:, qb, :], scalar1=m16[:, qb, 15:16], scalar2=None, op0=ALU.is_ge),
                      reads=["impacc", "m16"], writes=["selv"])
            p.dve(lambda e: e.tensor_scalar(out=scr2[:], in0=impacc[:], scalar1=-5e29, scalar2=None, op0=ALU.is_gt), reads=["impacc"], writes=["scr2"])
            p.dve(lambda e: e.tensor_tensor(out=selv, in0=selv, in1=scr2[:], op=ALU.mult), reads=["selv", "scr2"], writes=["selv"])
            p.dve(lambda e: e.tensor_scalar(out=selv, in0=selv, scalar1=1.0, scalar2=-MASKV, op0=ALU.subtract, op1=ALU.mult), reads=["selv"], writes=["selv"])
            for h in range(4):
                items = []
                js = [j for j in range(8 * s - 4, 8 * s + 8) if j >= 0]
                for j in js:
                    jrel = j - 8 * s
                    it = dict(j=j, s=s, extras=[(c["identb"][:], wm[:, jrel + 4, :], ["identb", "wm"])], first=(j == js[0]), last=(j == js[-1]), obank=4 + (h % 2))
                    if j == js[-1]:
                        it["fin"] = finalize_plain(4 + (h % 2), dst(h), h, s, gate=gate_row(h, 2, s), acc=nacc[:, h, :], acc_tok=("nacc", h), first=False, last=False)
                    items.append(it)
                run_softmax(items, KwT, kwtok, 68, Vw, vwtok, h, qtoks[h], pending)
            flush(pending)
            nkb = 8 * s + 8
            ngi = (nkb - 1) // 16 + 1
            for gi in range(ngi):
                pt = kb.ps(7)
                for qb in range(4):
                    p.pe(lambda e, qb=qb, gi=gi, pt=pt: e.transpose(out=pt[:, qb * 128:(qb + 1) * 128], in_=selvp[:, qb, 32 * gi:32 * gi + 128], identity=c["identf"][:]),
                         reads=["selv", "identf"], writes=[("ps", 7)])
                p.act(lambda e, gi=gi, pt=pt: e.copy(out=QS[64:96, 0, gi, :], in_=pt[64:96, :]), reads=[("ps", 7)], writes=[("QS", gi)])
                for h in range(4):
                    if h > 0:
                        p.pool(lambda e, gi=gi, h=h: e.tensor_copy(out=QS[64:96, h, gi, :], in_=QS[64:96, 0, gi, :]), reads=[("QS", gi)], writes=[("QS", gi)])
                    p.pool(lambda e, gi=gi, h=h: e.tensor_copy(out=QS[0:64, h, gi, :], in_=A.QT[0:64, h, sl]), reads=[qtoks[h]], writes=[("QS", gi)])
                    p.dma(QS[96:100, h, gi, :], io["c_qaug"][h][:, sl], writes=[("QS", gi)])
            for h in range(4):
                items = []
                for j in range(nkb):
                    jj = j - 8 * s
                    ex = []
                    if jj >= 0:
                        ex.append((c["identb"][:], A.cm[:, jj, :], ["identb", "cm"]))
                    it = dict(j=j, s=s, extras=ex, first=(j == 0), last=(j == nkb - 1), obank=4 + (h % 2),
                              qrhs=QS[0:100, h, j // 16, :], qreads=[("QS", j // 16)])
                    if j == nkb - 1:
                        it["fin"] = finalize_plain(4 + (h % 2), dst(h), h, s, gate=gate_row(h, 1, s), acc=nacc[:, h, :], acc_tok=("nacc", h), first=False, last=True)
                    items.append(it)
                run_softmax(items, KsT, kstok, 100, Vs, vstok, h, qtoks[h], pending)
            flush(pending)
    return A


B_WEIGHTS = {"w_mem_kv": [D, 512], "nsa_pe": [32, 64], "w_phi_k1": [2048, 128], "w_phi_k2": [128, 64],
             "w_phi_v1": [2048, 128], "w_phi_v2": [128, 64]}


def build_b(branches):
    kb = KB()
    io = {}
    for n in ("c_ident", "c_ones"):
        io[n] = kb.din(n, [128, 128])
    io["c_cst"] = kb.din("c_cst", [128, 8])
    for n, (shp, dt) in B_CONST_SHAPES.items():
        io[n] = kb.din(n, shp, dt)
    for n, (shp, dt) in B_INS.items():
        io[n] = kb.din(n, shp, dt)
    w = {n: kb.din(n, shp) for n, shp in B_WEIGHTS.items()}
    io["oT_d"] = kb.dout("oT_d", [5, 256, TOK], BF16)
    io["kml_f"] = KFull(io["kml_f"].rearrange("h d t -> (h d) t"))
    for n in ("ksb_f", "kmo_f", "krl_f", "kcv_f", "ksl_f", "kwi_f"):
        io[n] = KFull(io[n])
    for n in ("vsb_f", "vmo_f", "vml_f", "vsw_f"):
        io[n] = VFull(io[n])
    c = load_consts(kb, io)
    phase_b(kb, c, io, w, branches)
    return kb.finish()


def layer_norm_chunk(kb, c, v, vtok, gbc, bbc, out, otok, tmp):
    p = kb.p
    st, mv, sm = tmp["st"], tmp["mv"], tmp["sm"]
    for hf in range(2):
        p.dve(lambda e, hf=hf: e.bn_stats(out=st[:, hf, :], in_=v[:, hf * 512:(hf + 1) * 512]), reads=[vtok], writes=["lnst"])
    p.dve(lambda e: e.bn_aggr(out=mv[:], in_=st[:]), reads=["lnst"], writes=["lnmv"])
    p.act(lambda e: e.activation(out=sm[:, 0:1], in_=mv[:, 1:2], func=AF.Ln, bias=c["eps_ln"]), reads=["lnmv", "cst"], writes=["lnsm"])
    p.act(lambda e: e.activation(out=sm[:, 0:1], in_=sm[:, 0:1], func=AF.Exp, scale=-0.5), reads=["lnsm"], writes=["lnsm"])
    p.dve(lambda e: e.scalar_tensor_tensor(out=sm[:, 1:2], in0=mv[:, 0:1], scalar=-1.0, in1=sm[:, 0:1], op0=ALU.mult, op1=ALU.mult),
          reads=["lnmv", "lnsm"], writes=["lnsm2"])
    p.act(lambda e: e.activation(out=v[:], in_=v[:], func=AF.Identity, scale=sm[:, 0:1], bias=sm[:, 1:2]), reads=[vtok, "lnsm", "lnsm2"], writes=[vtok])
    p.dve(lambda e: e.tensor_tensor(out=v[:], in0=v[:], in1=gbc[:], op=ALU.mult), reads=[vtok, "lngb"], writes=[vtok])
    p.dve(lambda e: e.tensor_tensor(out=out[:], in0=v[:], in1=bbc[:], op=ALU.add), reads=[vtok, "lngb"], writes=[otok])


def phase_c1(kb, c, io, w):
    nc, p = kb.nc, kb.p
    wg = kb.sb("wg", [128, 5, 8, 1024], BF16)
    wbr = kb.sb("wbr", [128, 5, 2, 1024], BF16)
    wout = kb.sb("wout", [128, 8, 1024], BF16)
    bg = kb.sb("bg", [128, 5, 8], F32)
    gbc = kb.sb("gbc", [128, 1024], F32)
    bbc = kb.sb("bbc", [128, 1024], F32)
    wr = kb.sb("wr", [128, 8, 20], F32)
    brow = kb.sb("brow", [1, 20], F32)
    for i in range(5):
        for f in range(8):
            p.dma(wg[:, i, f, :], w["w_gate"][i, f * 128:(f + 1) * 128, :], writes=[("wg", i)], q="pool")
        p.dma(wbr[:, i, :, :], w["w_br"][i].rearrange("(j p) c -> p j c", p=128), writes=["wbr"], q="pool")
    p.dma(wout[:], w["w_out"].rearrange("(f p) c -> p f c", p=128), writes=["wout"], q="pool")
    p.dma(bg[:], w["b_gate"].rearrange("i (c p) -> p i c", p=128), writes=["bg"], slow=True)
    p.dma(gbc[:], w["ln1_g"].partition_broadcast(128), writes=["lngb"])
    p.dma(bbc[:], w["ln1_b"].partition_broadcast(128), writes=["lngb"])
    p.dma(wr[:, :, 0:4], w["w_rg"].rearrange("(f p) g -> p f g", p=128), writes=["wr"], slow=True)
    for g in range(4):
        p.dma(wr[:, :, 4 + 4 * g:8 + 4 * g], w["w_re"][g].rearrange("(f p) e -> p f e", p=128), writes=["wr"], slow=True)
    p.dma(brow[0:1, 0:4], w["b_rg"].rearrange("(o g) -> o g", o=1), writes=["brow"])
    p.dma(brow[0:1, 4:20], w["b_re"].rearrange("(o g) e -> o (g e)", o=1), writes=["brow"])

    hTt = [kb.sb("hTt", [128, 8, 512], BF16) for _ in range(2)]
    oTt = [kb.sb("oTt", [128, 5, 2, 512], BF16)] * 2
    mT = [kb.sb("mT", [128, 8, 512], BF16) for _ in range(2)]
    sg = [kb.sb("sg", [128, 512], F32) for _ in range(2)]
    acc = kb.sb("macc", [128, 512], F32)
    tmpm = kb.sb("tmpm", [128, 512], F32)
    hch = [kb.sb("hch", [128, 1024], F32)] * 2
    vch = [kb.sb("vch", [128, 1024], F32)] * 2
    h1c = [kb.sb("h1c", [128, 1024], F32) for _ in range(2)]
    h1Tf = [kb.sb("h1Tf", [128, 8, 128], F32) for _ in range(2)]
    h1Tb = [kb.sb("h1Tb", [128, 8, 128], BF16) for _ in range(2)]
    lnt = {"st": kb.sb("lnst", [128, 2, 6], F32), "mv": kb.sb("lnmv", [128, 2], F32), "sm": kb.sb("lnsm", [128, 2], F32)}
    lg = kb.sb("lg", [128, 20], F32)
    r1 = kb.sb("r1", [128, 8], F32)
    goh = kb.sb("goh", [128, 4], F32)
    el = kb.sb("el", [128, 4], F32)
    ee = kb.sb("ee", [128, 4], F32)
    ee2 = kb.sb("ee2", [128, 4], F32)
    gd = [kb.sb("gd", [128, 16], F32) for _ in range(2)]
    bk = {"n": 0}

    def nbank():
        b = bk["n"] % 6
        bk["n"] += 1
        return b

    pend = []

    def do_slot(s):
        d2 = s % 2
        tsl = slice(s * 512, (s + 1) * 512)
        ht, ot, mt = hTt[d2], oTt[d2], mT[d2]
        p.dma(ht[:], io["hT_d"].rearrange("(f p) t -> p f t", p=128)[:, :, tsl], writes=[("hTt", d2)])
        for i in range(5):
            p.dma(ot[:, i, :, :], io["oT_d"][i].rearrange("(j p) t -> p j t", p=128)[:, :, tsl], writes=["oTt"])
        for cc in range(8):
            csl = slice(cc * 128, (cc + 1) * 128)
            for i in range(5):
                bgt, bbr = nbank(), nbank()
                pg, pb = kb.ps(bgt), kb.ps(bbr)
                for f in range(8):
                    p.pe(lambda e, pg=pg, i=i, f=f, csl=csl: e.matmul(pg[:], lhsT=wg[:, i, f, csl], rhs=ht[:, f, :], start=(f == 0), stop=(f == 7)),
                         reads=[("wg", i), ("hTt", d2)], writes=[("ps", bgt)])
                for jc in range(2):
                    p.pe(lambda e, pb=pb, i=i, jc=jc, csl=csl: e.matmul(pb[:], lhsT=wbr[:, i, jc, csl], rhs=ot[:, i, jc, :], start=(jc == 0), stop=(jc == 1)),
                         reads=["wbr", "oTt"], writes=[("ps", bbr)])
                sgi = sg[i % 2]
                p.act(lambda e, sgi=sgi, pg=pg, i=i, cc=cc: e.activation(out=sgi[:], in_=pg[:], func=AF.Sigmoid, bias=bg[:, i, cc:cc + 1]),
                      reads=[("ps", bgt), "bg"], writes=[("sg", i % 2)])
                if i == 0:
                    p.dve(lambda e, sgi=sgi, pb=pb: e.tensor_tensor(out=acc[:], in0=sgi[:], in1=pb[:], op=ALU.mult),
                          reads=[("sg", i % 2), ("ps", bbr)], writes=["macc"])
                else:
                    p.dve(lambda e, sgi=sgi, pb=pb: e.tensor_tensor(out=tmpm[:], in0=sgi[:], in1=pb[:], op=ALU.mult),
                          reads=[("sg", i % 2), ("ps", bbr)], writes=["tmpm"])
                    if i < 4:
                        p.dve(lambda e: e.tensor_tensor(out=acc[:], in0=acc[:], in1=tmpm[:], op=ALU.add), reads=["macc", "tmpm"], writes=["macc"])
                    else:
                        p.dve(lambda e, cc=cc: e.tensor_tensor(out=mt[:, cc, :], in0=acc[:], in1=tmpm[:], op=ALU.add), reads=["macc", "tmpm"], writes=[("mT", d2)])
            for _ in range(2 if cc < 4 else 1):
                if pend:
                    pend.pop(0)()
        if "dbg_mt" in io and s == 0:
            p.dma(io["dbg_mt"], mt[:], reads=[("mT", d2)], writes=["dbg_mt"])
        def make_chunk(tc):
            gck = s * 4 + tc
            k2 = gck % 2
            rows = slice(gck * 128, (gck + 1) * 128)
            hc, vc, h1 = hch[k2], vch[k2], h1c[k2]
            tf, tb = h1Tf[k2], h1Tb[k2]
            gdt = gd[k2]
            rb = {}

            def stA():
                p.dma(hc[:], io["h_tok"][rows, :], writes=["hch"])
                for hf in range(2):
                    b = nbank()
                    ps = kb.ps(b)
                    for cc in range(8):
                        p.pe(lambda e, ps=ps, cc=cc, tc=tc, hf=hf: e.matmul(ps[:], lhsT=mt[:, cc, tc * 128:(tc + 1) * 128], rhs=wout[:, cc, hf * 512:(hf + 1) * 512],
                                                                          start=(cc == 0), stop=(cc == 7)),
                             reads=["wout", ("mT", d2)], writes=[("ps", b)])
                    p.dve(lambda e, ps=ps, hf=hf: e.scalar_tensor_tensor(out=vc[:, hf * 512:(hf + 1) * 512], in0=hc[:, hf * 512:(hf + 1) * 512], scalar=ALPHA, in1=ps[:],
                                                                          op0=ALU.mult, op1=ALU.add),
                          reads=["hch", ("ps", b)], writes=["vch"])
                layer_norm_chunk(kb, c, vc, "vch", gbc, bbc, h1, ("h1c", k2), lnt)
                p.dma(io["h1_d"][rows, :], h1[:], reads=[("h1c", k2)], writes=[("h1_d", gck)])

            def stB():
                for hf in range(2):
                    b = nbank()
                    ps = kb.ps(b)
                    for jx in range(4):
                        f = hf * 4 + jx
                        p.pe(lambda e, ps=ps, f=f, jx=jx: e.transpose(out=ps[:, jx * 128:(jx + 1) * 128], in_=h1[:, f * 128:(f + 1) * 128], identity=c["identf"][:]),
                             reads=[("h1c", k2), "identf"], writes=[("ps", b)])
                    p.act(lambda e, ps=ps, hf=hf: e.copy(out=tf[:, hf * 4:hf * 4 + 4, :], in_=ps[:].rearrange("p (j t) -> p j t", j=4)),
                          reads=[("ps", b)], writes=[("h1Tf", k2)])
                p.pool(lambda e: e.tensor_copy(out=tb[:], in_=tf[:]), reads=[("h1Tf", k2)], writes=[("h1Tb", k2)])
                p.dma(io["h1T_d"].rearrange("(f p) t -> p f t", p=128)[:, :, rows], tb[:], reads=[("h1Tb", k2)], writes=[("h1T_d", gck)])
                b = nbank()
                ps = kb.ps(b)
                for f in range(8):
                    p.pe(lambda e, ps=ps, f=f: e.matmul(ps[:, 0:20], lhsT=tf[:, f, :], rhs=wr[:, f, :], start=(f == 0), stop=False),
                         reads=[("h1Tf", k2), "wr"], writes=[("ps", b)])
                p.pe(lambda e, ps=ps: e.matmul(ps[:, 0:20], lhsT=c["onesf"][0:1, :], rhs=brow[0:1, :], start=False, stop=True),
                     reads=["onesf", "brow"], writes=[("ps", b)])
                p.dve(lambda e, ps=ps: e.tensor_copy(out=lg[:], in_=ps[:, 0:20]), reads=[("ps", b)], writes=["lg"])

            def stC():
                p.dve(lambda e: e.tensor_reduce(out=r1[:, 0:1], in_=lg[:, 0:4], axis=AX.X, op=ALU.max), reads=["lg"], writes=["r1a"])
                p.dve(lambda e: e.tensor_scalar(out=goh[:], in0=lg[:, 0:4], scalar1=r1[:, 0:1], scalar2=None, op0=ALU.is_equal), reads=["lg", "r1a"], writes=["goh"])
                p.dve(lambda e: e.tensor_scalar(out=ee[:], in0=lg[:, 0:4], scalar1=r1[:, 0:1], scalar2=None, op0=ALU.subtract), reads=["lg", "r1a"], writes=["ee"])
                p.act(lambda e: e.activation(out=ee[:], in_=ee[:], func=AF.Exp), reads=["ee"], writes=["ee"])
                p.dve(lambda e: e.tensor_reduce(out=r1[:, 1:2], in_=ee[:], axis=AX.X, op=ALU.add), reads=["ee"], writes=["r1b"])
                p.dve(lambda e: e.reciprocal(out=r1[:, 1:2], in_=r1[:, 1:2]), reads=["r1b"], writes=["r1b"])
                p.dve(lambda e: e.tensor_scalar(out=el[:], in0=lg[:, 4:8], scalar1=goh[:, 0:1], scalar2=None, op0=ALU.mult), reads=["lg", "goh"], writes=["el"])
                for g in range(1, 4):
                    p.dve(lambda e, g=g: e.scalar_tensor_tensor(out=el[:], in0=lg[:, 4 + 4 * g:8 + 4 * g], scalar=goh[:, g:g + 1], in1=el[:], op0=ALU.mult, op1=ALU.add),
                          reads=["lg", "goh", "el"], writes=["el"])
                p.dve(lambda e: e.tensor_reduce(out=r1[:, 2:3], in_=el[:], axis=AX.X, op=ALU.max), reads=["el"], writes=["r1c"])
                p.dve(lambda e: e.tensor_scalar(out=ee[:], in0=el[:], scalar1=r1[:, 2:3], scalar2=None, op0=ALU.subtract), reads=["el", "r1c"], writes=["ee"])
                p.act(lambda e: e.activation(out=ee[:], in_=ee[:], func=AF.Exp), reads=["ee"], writes=["ee"])
                p.dve(lambda e: e.tensor_scalar(out=ee2[:], in0=ee[:], scalar1=1.0, scalar2=-2.0, op0=ALU.is_ge, op1=ALU.mult), reads=["ee"], writes=["ee2"])
                p.dve(lambda e: e.tensor_tensor(out=ee2[:], in0=ee2[:], in1=ee[:], op=ALU.add), reads=["ee2", "ee"], writes=["ee2"])
                p.dve(lambda e: e.tensor_reduce(out=r1[:, 3:4], in_=ee2[:], axis=AX.X, op=ALU.max), reads=["ee2"], writes=["r1d"])
                p.dve(lambda e: e.tensor_scalar(out=ee2[:], in0=ee[:], scalar1=r1[:, 3:4], scalar2=None, op0=ALU.is_ge), reads=["ee", "r1d"], writes=["ee2"])
                p.dve(lambda e: e.tensor_tensor(out=ee[:], in0=ee[:], in1=ee2[:], op=ALU.mult), reads=["ee", "ee2"], writes=["ee"])
                p.dve(lambda e: e.tensor_scalar(out=r1[:, 4:5], in0=r1[:, 3:4], scalar1=1.0, scalar2=None, op0=ALU.add), reads=["r1d"], writes=["r1e"])
                p.dve(lambda e: e.reciprocal(out=r1[:, 4:5], in_=r1[:, 4:5]), reads=["r1e"], writes=["r1e"])
                p.dve(lambda e: e.tensor_tensor(out=r1[:, 4:5], in0=r1[:, 4:5], in1=r1[:, 1:2], op=ALU.mult), reads=["r1e", "r1b"], writes=["r1e"])
                p.dve(lambda e: e.tensor_scalar(out=ee[:], in0=ee[:], scalar1=r1[:, 4:5], scalar2=None, op0=ALU.mult), reads=["ee", "r1e"], writes=["ee"])
                for g in range(4):
                    p.dve(lambda e, g=g: e.tensor_scalar(out=gdt[:, 4 * g:4 * g + 4], in0=ee[:], scalar1=goh[:, g:g + 1], scalar2=None, op0=ALU.mult),
                          reads=["ee", "goh"], writes=[("gd", k2)])
                p.dma(io["gd_d"][rows, :], gdt[:], reads=[("gd", k2)], writes=[("gd_d", gck)])
            return stA, stB, stC

        for tc in range(4):
            pend.extend(make_chunk(tc))

    for s in range(NSLOT):
        do_slot(s)
    while pend:
        pend.pop(0)()

C1_W = {"w_br": [5, 256, D], "w_gate": [5, D, D], "b_gate": [5, D], "w_out": [D, D], "ln1_g": [D], "ln1_b": [D],
        "w_rg": [D, 4], "b_rg": [4], "w_re": [4, D, 4], "b_re": [4, 4]}


def build_c1(dbg=False):
    kb = KB()
    io = {}
    if dbg:
        io["dbg_mt"] = kb.dout("dbg_mt", [128, 8, 512], BF16)
    for n in ("c_ident", "c_ones"):
        io[n] = kb.din(n, [128, 128])
    io["c_cst"] = kb.din("c_cst", [128, 8])
    io["h_tok"] = kb.din("h_tok", [TOK, D])
    io["hT_d"] = kb.din("hT_d", [D, TOK], BF16)
    io["oT_d"] = kb.din("oT_d", [5, 256, TOK], BF16)
    w = {n: kb.din(n, shp) for n, shp in C1_W.items()}
    io["h1_d"] = kb.dout("h1_d", [TOK, D])
    io["h1T_d"] = kb.dout("h1T_d", [D, TOK], BF16)
    io["gd_d"] = kb.dout("gd_d", [TOK, 16])
    c = load_consts(kb, io)
    phase_c1(kb, c, io, w)
    return kb.finish()


def phase_c2(kb, c, io, w, nT=4, nE=16):
    nc, p = kb.nc, kb.p
    gbc = kb.sb("gbc2", [128, 1024], F32)
    bbc = kb.sb("bbc2", [128, 1024], F32)
    p.dma(gbc[:], w["ln2_g"].partition_broadcast(128), writes=["lngb"])
    p.dma(bbc[:], w["ln2_b"].partition_broadcast(128), writes=["lngb"])
    hT = [kb.sb("h1Tt", [128, 8, 1024], BF16) for _ in range(2)]
    gdt = [kb.sb("gdt", [128, 8, 16], F32) for _ in range(2)]
    wup = [kb.sb("wup", [128, 8, 512], BF16) for _ in range(3)]
    wdn = [kb.sb("wdn", [128, 2, 1024], BF16) for _ in range(3)]
    yacc = kb.sb("yacc", [128, 8, 1024], F32)
    gT = [kb.sb("gT", [128, 2, 1024], BF16) for _ in range(2)]
    sa = [kb.sb("sa", [128, 512], F32) for _ in range(2)]
    hch = [kb.sb("h1ch", [128, 1024], F32) for _ in range(2)]
    och = [kb.sb("och", [128, 1024], F32) for _ in range(2)]
    lnt = {"st": kb.sb("lnst2", [128, 2, 6], F32), "mv": kb.sb("lnmv2", [128, 2], F32), "sm": kb.sb("lnsm2", [128, 2], F32)}
    st = {"bank": 0, "w": 0, "sa": 0}

    def nbank():
        b = st["bank"] % 8
        st["bank"] += 1
        return b

    def make_expert(T, e, ht, httok, gd, gdtok):
        k = st["w"] % 3
        kg = st["w"] % 2
        st["w"] += 1
        wu, wd, g = wup[k], wdn[k], gT[kg]
        p.dma(wu[:], w["w_up"][e].rearrange("(f p) c -> p f c", p=128), writes=[("wup", k)], q="pool")
        p.dma(wd[:], w["w_down"][e].rearrange("(j p) c -> p j c", p=128), writes=[("wdn", k)], q="pool")
        ups, downs = [], []

        def make_up(ts, jc):
            def up():
                tsl = slice(ts * 512, (ts + 1) * 512)
                ba, bu = nbank(), nbank()
                pa, pu = kb.ps(ba), kb.ps(bu)
                for f in range(8):
                    p.pe(lambda e_, f=f: e_.matmul(pa[:], lhsT=wu[:, f, jc * 128:(jc + 1) * 128], rhs=ht[:, f, tsl], start=(f == 0), stop=(f == 7)),
                         reads=[("wup", k), httok], writes=[("ps", ba)])
                for f in range(8):
                    p.pe(lambda e_, f=f: e_.matmul(pu[:], lhsT=wu[:, f, 256 + jc * 128:256 + (jc + 1) * 128], rhs=ht[:, f, tsl], start=(f == 0), stop=(f == 7)),
                         reads=[("wup", k), httok], writes=[("ps", bu)])
                si = st["sa"] % 2
                st["sa"] += 1
                sat = sa[si]
                p.act(lambda e_: e_.activation(out=sat[:], in_=pa[:], func=AF.Silu), reads=[("ps", ba)], writes=[("sa", si)])
                p.dve(lambda e_: e_.tensor_tensor(out=g[:, jc, tsl], in0=sat[:], in1=pu[:], op=ALU.mult),
                      reads=[("sa", si), ("ps", bu)], writes=[("gT", kg)])
            return up

        def make_down(tc, hf):
            def down():
                b = nbank()
                ps = kb.ps(b)
                for jc in range(2):
                    p.pe(lambda e_, jc=jc: e_.matmul(ps[:], lhsT=g[:, jc, tc * 128:(tc + 1) * 128], rhs=wd[:, jc, hf * 512:(hf + 1) * 512],
                                                       start=(jc == 0), stop=(jc == 1)),
                         reads=[("gT", kg), ("wdn", k)], writes=[("ps", b)])
                ya = yacc[:, tc, hf * 512:(hf + 1) * 512]
                if e == 0:
                    p.dve(lambda e_: e_.tensor_scalar(out=ya, in0=ps[:], scalar1=gd[:, tc, e:e + 1], scalar2=None, op0=ALU.mult),
                          reads=[("ps", b), gdtok], writes=[("yacc", tc)])
                else:
                    p.dve(lambda e_: e_.scalar_tensor_tensor(out=ya, in0=ps[:], scalar=gd[:, tc, e:e + 1], in1=ya, op0=ALU.mult, op1=ALU.add),
                          reads=[("ps", b), gdtok, ("yacc", tc)], writes=[("yacc", tc)])
            return down

        for ts in range(2):
            for jc in range(2):
                ups.append(make_up(ts, jc))
        for tc in range(8):
            for hf in range(2):
                downs.append(make_down(tc, hf))
        return ups, downs

    def do_chunk_out(T, tc):
        gck = T * 8 + tc
        k2 = gck % 2
        rows = slice(gck * 128, (gck + 1) * 128)
        hc, oc = hch[k2], och[k2]
        p.dma(hc[:], io["h1_d"][rows, :], writes=[("h1ch", k2)])
        p.dve(lambda e_: e_.scalar_tensor_tensor(out=hc[:], in0=hc[:], scalar=ALPHA, in1=yacc[:, tc, :], op0=ALU.mult, op1=ALU.add),
              reads=[("h1ch", k2), ("yacc", tc)], writes=[("h1ch", k2)])
        layer_norm_chunk(kb, c, hc, ("h1ch", k2), gbc, bbc, oc, ("och", k2), lnt)
        p.dma(io["h_out"][rows, :], oc[:], reads=[("och", k2)], writes=[("h_out", gck)])

    def do_tile(T):
        d2 = T % 2
        ht, gd = hT[d2], gdt[d2]
        cols = slice(T * 1024, (T + 1) * 1024)
        p.dma(ht[:], io["h1T_d"].rearrange("(f p) t -> p f t", p=128)[:, :, cols], writes=[("h1Tt", d2)])
        p.dma(gd[:], io["gd_d"][T * 1024:(T + 1) * 1024, :].rearrange("(n p) e -> p n e", p=128), writes=[("gdt", d2)])
        prev_down = []
        for e in range(nE):
            ups, downs = make_expert(T, e, ht, ("h1Tt", d2), gd, ("gdt", d2))
            for i, u in enumerate(ups):
                u()
                for dn in prev_down[4 * i:4 * i + 4]:
                    dn()
            prev_down = downs
        for dn in prev_down:
            dn()
        for tc in range(8):
            do_chunk_out(T, tc)

    for T in range(nT):
        do_tile(T)


C2_W = {"w_up": [16, D, 512], "w_down": [16, 256, D], "ln2_g": [D], "ln2_b": [D]}


def build_c2(nT=4, nE=16):
    kb = KB()
    io = {}
    for n in ("c_ident", "c_ones"):
        io[n] = kb.din(n, [128, 128])
    io["c_cst"] = kb.din("c_cst", [128, 8])
    io["h1_d"] = kb.din("h1_d", [TOK, D])
    io["h1T_d"] = kb.din("h1T_d", [D, TOK], BF16)
    io["gd_d"] = kb.din("gd_d", [TOK, 16])
    w = {n: kb.din(n, shp) for n, shp in C2_W.items()}
    io["h_out"] = kb.dout("h_out", [TOK, D])
    c = load_consts(kb, io)
    phase_c2(kb, c, io, w, nT, nE)
    return kb.finish()


W_SHAPES = {
    "w_in": [D, IN_TOTAL], "g_cq": [256], "g_ckv": [128], "w_uq": [256, 384], "w_ukv": [128, 512],
    "nsa_pe": [32, 64], "w_phi_k1": [2048, 128], "w_phi_k2": [128, 64], "w_phi_v1": [2048, 128], "w_phi_v2": [128, 64],
    "w_mem_kv": [D, 512], "w_br": [5, 256, D], "w_gate": [5, D, D], "b_gate": [5, D], "w_out": [D, D],
    "ln1_g": [D], "ln1_b": [D], "w_rg": [D, 4], "b_rg": [4], "w_re": [4, D, 4], "b_re": [4, 4],
    "w_up": [16, D, 512], "w_down": [16, 256, D], "ln2_g": [D], "ln2_b": [D],
}
PAIRS = [[0, 1], [2, 3], [4, 5], [6, 7]]


def build_fused(depth=DEPTH, nlw=DEPTH, stop=None):
    kb = KB()
    io = {}
    for n in ("c_ident", "c_ones"):
        io[n] = kb.din(n, [128, 128])
    io["c_cst"] = kb.din("c_cst", [128, 8])
    for n, (shp, dt) in B_CONST_SHAPES.items():
        io[n] = kb.din(n, shp, dt)
    io["ropeq_t"] = kb.din("ropeq_t", [NSLOT, 96, 2, 512])
    io["ropek_t"] = kb.din("ropek_t", [NSLOT, 32, 2, 512])
    io["x_own"] = kb.din("x_own", [TOK, D])
    io["mem"] = kb.din("mem", [256, D])
    wfull = {n: kb.din(n, [nlw] + shp) for n, shp in W_SHAPES.items()}
    io["h_final"] = kb.dout("h_final", [TOK, D])
    hbuf = [kb.dscratch(f"hbuf{i}", [TOK, D]) for i in range(2)]
    io["hT_d"] = kb.dscratch("hT_d", [D, TOK], BF16)
    for n in ("qsb_d", "qmo_d", "qns_d", "qme_d"):
        io[n] = kb.dscratch(n, [256, TOK], BF16)
    io["qml_d"] = kb.dscratch("qml_d", [4, 96, TOK], BF16)
    io["gns_d"] = kb.dscratch("gns_d", [12, TOK])
    io["oT_d"] = kb.dscratch("oT_d", [5, 256, TOK], BF16)
    io["h1_d"] = kb.dscratch("h1_d", [TOK, D])
    io["h1T_d"] = kb.dscratch("h1T_d", [D, TOK], BF16)
    io["gd_d"] = kb.dscratch("gd_d", [TOK, 16])
    xk_rows = [256, 256, 256, 256, 64]
    xk_in = [kb.dscratch(f"xk_in{k}", [r, TOK], BF16) for k, r in enumerate(xk_rows)]
    xk_out = [kb.dscratch(f"xk_out{k}", [2 * r, TOK], BF16) for k, r in enumerate(xk_rows)]
    xv_in = [kb.dscratch(f"xv_in{k}", [1024, 896], BF16) for k in range(4)]
    xv_out = [kb.dscratch(f"xv_out{k}", [2048, 896], BF16) for k in range(4)]
    io["ksb_d"], io["kmo_d"] = xk_in[0], xk_in[1]
    io["kml_d"] = xk_in[2].rearrange("(h d) t -> h d t", h=4)
    io["krl_d"], io["kcv_d"], io["ksl_d"] = xk_in[3][0:32, :], xk_in[3][32:160, :], xk_in[3][160:224, :]
    io["kwi_d"] = xk_in[4]
    io["vsb_d"], io["vmo_d"] = TMChunks(xv_in, 0, 256), TMChunks(xv_in, 256, 512)
    io["vml_d"], io["vsw_d"] = TMChunks(xv_in, 512, 768), TMChunks(xv_in, 768, 896)
    io["ksb_f"], io["kmo_f"], io["kml_f"] = KPair(xk_out[0], 256), KPair(xk_out[1], 256), KPair(xk_out[2], 256)
    io["krl_f"], io["kcv_f"], io["ksl_f"] = KPair(xk_out[3], 256, 0), KPair(xk_out[3], 256, 32), KPair(xk_out[3], 256, 160)
    io["kwi_f"] = KPair(xk_out[4], 64)
    io["vsb_f"], io["vmo_f"] = VPair(xv_out, 0), VPair(xv_out, 256)
    io["vml_f"], io["vsw_f"] = VPair(xv_out, 512), VPair(xv_out, 768)

    for l in range(depth):
        w = {n: ap[l] for n, ap in wfull.items()}
        io["h_tok"] = io["x_own"] if l == 0 else hbuf[(l - 1) % 2]
        io["h_out"] = io["h_final"] if l == depth - 1 else hbuf[l % 2]
        c = load_consts(kb, io)
        phase_a(kb, c, io, w)
        kb.end_phase()
        if stop == "A":
            break
        c = load_consts(kb, io)
        phase_b(kb, c, io, w, ("mem",))
        for k in range(5):
            kb.p.allgather(xk_out[k], xk_in[k], PAIRS, writes=[("xk", k)])
        for k in range(4):
            kb.p.allgather(xv_out[k], xv_in[k], PAIRS, writes=[("xv", k)])
        kb.end_phase()
        if stop == "AG":
            break
        c = load_consts(kb, io)
        phase_b(kb, c, io, w, ("sb", "moba", "mla"))
        kb.end_phase()
        if stop == "B1":
            break
        c = load_consts(kb, io)
        phase_b(kb, c, io, w, ("nsa",))
        kb.end_phase()
        if stop == "B2":
            break
        c = load_consts(kb, io)
        phase_c1(kb, c, io, w)
        kb.end_phase()
        if stop == "C1":
            break
        c = load_consts(kb, io)
        phase_c2(kb, c, io, w)
        kb.end_phase()
    return kb.finish()


def fused_inputs(inp, c):
    b, r = c // 2, c % 2
    m = dict(host_consts())
    m.update(bconsts_host(r))
    m["ropeq_t"], m["ropek_t"] = rope_tables(r)
    m["x_own"] = np.ascontiguousarray(inp["x"][b][_own_tokens(r)])
    m["mem"] = inp["mem"][b]
    for n in W_SHAPES:
        m[n] = inp[n]
    return m


_PROGS = {}


def _prog(name, fn):
    if name not in _PROGS:
        _PROGS[name] = fn()
    return _PROGS[name]


def _own_tokens(r):
    t = np.arange(TOK)
    return t // 512 * 1024 + r * 512 + t % 512


def _interleave_fm(a0, a1):
    sh = a0.shape[:-1]
    out = np.empty(sh + (S,), a0.dtype)
    o = out.reshape(sh + (8, 2, 512))
    o[..., 0, :] = a0.reshape(sh + (8, 512))
    o[..., 1, :] = a1.reshape(sh + (8, 512))
    return out


def _interleave_tm(a0, a1):
    C = a0.shape[1]
    out = np.empty((S, C), a0.dtype)
    o = out.reshape(8, 2, 512, C)
    o[:, 0] = a0.reshape(8, 512, C)
    o[:, 1] = a1.reshape(8, 512, C)
    return out


A_WEIGHTS = ("w_in", "w_uq", "g_cq", "w_ukv", "g_ckv")
K_FM = {"ksb_d": "ksb_f", "kmo_d": "kmo_f", "kml_d": "kml_f", "krl_d": "krl_f", "kcv_d": "kcv_f", "ksl_d": "ksl_f", "kwi_d": "kwi_f"}
V_TM = {"vsb_d": "vsb_f", "vmo_d": "vmo_f", "vml_d": "vml_f", "vsw_d": "vsw_f"}
Q_OWN = ("qsb_d", "qmo_d", "qml_d", "qns_d", "qme_d", "gns_d")


def kernel_unfused(**inputs):
    inp = {k: np.ascontiguousarray(np.asarray(v)) for k, v in inputs.items()}
    ncores = 8
    cores = list(range(ncores))
    hc = host_consts()
    bc = [bconsts_host(r) for r in range(2)]
    rp = [rope_tables(r) for r in range(2)]
    own = [_own_tokens(r) for r in range(2)]
    h = [np.ascontiguousarray(inp["x"][c // 2][own[c % 2]]) for c in cores]
    nca = _prog("a", build_a)
    ncb1 = _prog("b1", lambda: build_b(("sb", "moba", "mla", "mem")))
    ncb2 = _prog("b2", lambda: build_b(("nsa",)))
    ncc1 = _prog("c1", build_c1)
    ncc2 = _prog("c2", build_c2)
    for l in range(DEPTH):
        maps = []
        for c in cores:
            m = dict(hc)
            m["h_tok"] = h[c]
            m["ropeq_t"], m["ropek_t"] = rp[c % 2]
            for n in A_WEIGHTS:
                m[n] = inp[n][l]
            maps.append(m)
        ra = run_bass_kernel_spmd(nca, maps, core_ids=cores).results
        maps = []
        for c in cores:
            b, r = c // 2, c % 2
            m = dict(hc)
            m.update(bc[r])
            for n in Q_OWN:
                m[n] = ra[c][n]
            for kd, kf in K_FM.items():
                m[kf] = _interleave_fm(np.asarray(ra[2 * b][kd]), np.asarray(ra[2 * b + 1][kd]))
            for vd, vf in V_TM.items():
                m[vf] = _interleave_tm(np.asarray(ra[2 * b][vd]), np.asarray(ra[2 * b + 1][vd]))
            m["mem"] = inp["mem"][b]
            for n in B_WEIGHTS:
                m[n] = inp[n][l]
            maps.append(m)
        rb1 = run_bass_kernel_spmd(ncb1, maps, core_ids=cores).results
        rb2 = run_bass_kernel_spmd(ncb2, maps, core_ids=cores).results
        rb = []
        for c in cores:
            o = np.array(rb1[c]["oT_d"])
            o[3] = np.asarray(rb2[c]["oT_d"])[3]
            rb.append({"oT_d": o})
        maps = []
        for c in cores:
            m = dict(hc)
            m["h_tok"] = h[c]
            m["hT_d"] = ra[c]["hT_d"]
            m["oT_d"] = rb[c]["oT_d"]
            for n in C1_W:
                m[n] = inp[n][l]
            maps.append(m)
        rc1 = run_bass_kernel_spmd(ncc1, maps, core_ids=cores).results
        maps = []
        for c in cores:
            m = dict(hc)
            for n in ("h1_d", "h1T_d", "gd_d"):
                m[n] = rc1[c][n]
            for n in C2_W:
                m[n] = inp[n][l]
            maps.append(m)
        rc2 = run_bass_kernel_spmd(ncc2, maps, core_ids=cores).results
        h = [np.asarray(rc2[c]["h_out"]) for c in cores]
    out = np.empty((NB, S, D), np.float32)
    for c in cores:
        out[c // 2][own[c % 2]] = h[c]
    return out


def kernel(**inputs):
    inp = {k: np.ascontiguousarray(np.asarray(v)) for k, v in inputs.items()}
    cores = list(range(8))
    nc = _prog("fused", build_fused)
    maps = [fused_inputs(inp, c) for c in cores]
    res = run_bass_kernel_spmd(nc, maps, core_ids=cores).results
    out = np.empty((NB, S, D), np.float32)
    for c in cores:
        out[c // 2][_own_tokens(c % 2)] = np.asarray(res[c]["h_final"])
    return out
```

```python
import contextlib
import types
import numpy as np
import ml_dtypes
import concourse.bass as bass
import concourse.mybir as mybir
from concourse.bass_utils import run_bass_kernel_spmd

F32 = mybir.dt.float32
BF16 = mybir.dt.bfloat16
AF = mybir.ActivationFunctionType
ALU = mybir.AluOpType
AX = mybir.AxisListType

D = 1024
S = 8192
NB = 4
DEPTH = 4
TOK = 4096
NSLOT = 8
IN_TOTAL = 2860
ALPHA = (2.0 * DEPTH) ** 0.25
LN_EPS = 1e-5
RMS_EPS = 1e-6
MASKV = -30000.0
SLOPES = [2.0 ** (-2.0 * (i + 1)) for i in range(4)]

ENGS = ("pe", "act", "dve", "pool", "sp")
SIG_EPOCH = 30000


def _freeze(fn):
    if fn.__closure__ is None:
        return fn
    cells = []
    for cl in fn.__closure__:
        try:
            cells.append(types.CellType(cl.cell_contents))
        except ValueError:
            cells.append(cl)
    g = types.FunctionType(fn.__code__, fn.__globals__, fn.__name__, fn.__defaults__, tuple(cells))
    g.__kwdefaults__ = fn.__kwdefaults__
    return g


class Op:
    __slots__ = ("eng", "fn", "reads", "writes", "dma", "deps", "sig", "dticket", "idx", "dprev")

    def __init__(self, eng, fn, reads, writes, dma):
        self.eng = eng
        self.fn = fn
        self.reads = tuple(reads)
        self.writes = tuple(writes)
        self.dma = dma
        self.deps = []
        self.sig = None
        self.dticket = None
        self.dprev = None


class Prog:
    NDSEM = 12
    _phase_id = 0

    def __init__(self, nc):
        self.nc = nc
        self.ops = []

    def add(self, eng, fn, reads=(), writes=(), dma=False):
        op = Op(eng, _freeze(fn), reads, writes, dma)
        op.idx = len(self.ops)
        self.ops.append(op)
        return op

    def pe(self, fn, reads=(), writes=()):
        return self.add("pe", fn, reads, writes)

    def act(self, fn, reads=(), writes=()):
        return self.add("act", fn, reads, writes)

    def dve(self, fn, reads=(), writes=()):
        return self.add("dve", fn, reads, writes)

    def pool(self, fn, reads=(), writes=()):
        return self.add("pool", fn, reads, writes)

    def allgather(self, out, in_, groups, reads=(), writes=()):
        return self.add("pool", lambda e: e.collective_compute("AllGather", ALU.bypass, replica_groups=groups, ins=[in_.opt()], outs=[out.opt()]),
                        reads, writes, dma="cc")

    def dma(self, out, in_, reads=(), writes=(), q="sp", slow=False):
        if slow:
            return self.add(q, lambda e: e.dma_start(out=out, in_=in_, allow_slow_non_contiguous=True), reads, writes, dma=True)
        return self.add(q, lambda e: e.dma_start(out=out, in_=in_), reads, writes, dma=True)

    def analyze(self):
        last_w = {}
        readers = {}
        for op in self.ops:
            deps = set()
            for t in op.reads:
                if t in last_w:
                    deps.add(last_w[t])
            for t in op.writes:
                if t in last_w:
                    deps.add(last_w[t])
                for r in readers.get(t, ()):
                    deps.add(r)
            deps.discard(op.idx)
            op.deps = sorted(deps)
            for t in op.reads:
                readers.setdefault(t, []).append(op.idx)
            for t in op.writes:
                last_w[t] = op.idx
                readers[t] = []
        qcount = {e: 0 for e in ENGS}
        qhist = {e: [] for e in ENGS}
        for op in self.ops:
            if op.dma == "cc":
                op.dticket = ("cc", op.idx, 1)
                continue
            if op.dma:
                n = qcount[op.eng]
                qcount[op.eng] += 1
                op.dticket = (op.eng, n % self.NDSEM, 16 * (n // self.NDSEM + 1))
                if n >= self.NDSEM:
                    op.dprev = qhist[op.eng][n - self.NDSEM]
                qhist[op.eng].append(op.idx)
        waited_eng = {e: {p: -1 for p in ENGS} for e in ENGS}
        waited_dma = {e: set() for e in ENGS}
        last_on = {e: -1 for e in ENGS}
        need_sig = set()
        for op in self.ops:
            e = op.eng
            final = []
            best = {}
            dl = list(op.deps)
            if op.dprev is not None:
                dl.append(op.dprev)
            for d in dl:
                p = self.ops[d]
                if p.dma:
                    if d not in waited_dma[e]:
                        waited_dma[e].add(d)
                        final.append(("dma", d))
                else:
                    if p.eng == e and e == "pe":
                        continue
                    if d <= waited_eng[e][p.eng]:
                        continue
                    if p.eng not in best or d > best[p.eng]:
                        best[p.eng] = d
            for pe_, d in best.items():
                waited_eng[e][pe_] = d
                need_sig.add(d)
                final.append(("eng", d))
            op.deps = final
        cnt = {e: 0 for e in ENGS}
        for op in self.ops:
            if not op.dma and op.idx in need_sig:
                cnt[op.eng] += 1
                op.sig = cnt[op.eng]
        self.sig_total = cnt
        self.dma_total = qcount

    def emit(self, barrier=False):
        nc = self.nc
        self.analyze()
        allsem = []

        Prog._phase_id += 1
        pid = Prog._phase_id

        def newsem(name):
            h = nc.alloc_semaphore(f"{name}_ph{pid}")
            allsem.append(h)
            return h

        if True:
            esem = {}
            for e in ENGS:
                n_ep = self.sig_total[e] // SIG_EPOCH + 1
                esem[e] = [newsem(f"s_{e}_{i}") for i in range(n_ep)]
            dsem = {}
            for e in ENGS:
                if self.dma_total[e]:
                    dsem[e] = [newsem(f"d_{e}_{i}") for i in range(self.NDSEM)]
            dsem["cc"] = {op.idx: newsem(f"cc_{op.idx}") for op in self.ops if op.dma == "cc"}

            def waitspec(dep):
                kind, d = dep
                p = self.ops[d]
                if kind == "dma":
                    q, si, val = p.dticket
                    return dsem[q][si], val
                k = p.sig - 1
                return esem[p.eng][k // SIG_EPOCH], k % SIG_EPOCH + 1

            def run(engname):
                def body(eng):
                    last_dma = {}
                    for op in self.ops:
                        if op.eng != engname:
                            continue
                        ws = [waitspec(d) for d in op.deps]
                        for (sem, val) in ws[1:]:
                            eng.wait_ge(sem, val)
                        ins = op.fn(eng)
                        if ws:
                            ins._wait_ge(ws[0][0], ws[0][1])
                        if op.dma == "cc":
                            ins.then_inc(dsem["cc"][op.idx])
                            eng.wait_ge(dsem["cc"][op.idx], 1)
                        elif op.dma:
                            q, si, val = op.dticket
                            ins.then_inc(dsem[q][si], 16)
                            last_dma[si] = val
                        elif op.sig is not None:
                            k = op.sig - 1
                            ins.then_inc(esem[engname][k // SIG_EPOCH], 1)
                    for si, val in last_dma.items():
                        eng.wait_ge(dsem[engname][si], val)
                return body

            with nc.Block() as block:
                block.tensor(run("pe"))
                block.scalar(run("act"))
                block.vector(run("dve"))
                block.gpsimd(run("pool"))
                block.sync(run("sp"))
        if barrier:
            nc.all_engine_barrier()
            nc.clear_and_free_semaphores(allsem)
            nc.all_engine_barrier()
        else:
            for h in allsem:
                nc.release_semaphore(h)


class KB:
    def __init__(self):
        self.nc = bass.Bass("TRN2", target_bir_lowering=False)
        self.p = Prog(self.nc)
        self.es = contextlib.ExitStack()
        self.dram = {}
        self._uid = 0
        self.es0 = contextlib.ExitStack()
        self.psf = [self.es0.enter_context(self.nc.psum_tensor(f"psb{i}", [128, 512], F32)) for i in range(8)]

    def uid(self, s):
        self._uid += 1
        return f"{s}_{self._uid}"

    def din(self, name, shape, dt=F32):
        t = self.nc.dram_tensor(name, list(shape), dt, kind="ExternalInput").ap()
        self.dram[name] = t
        return t

    def dout(self, name, shape, dt=F32, kind="ExternalOutput"):
        t = self.nc.dram_tensor(name, list(shape), dt, kind=kind).ap()
        self.dram[name] = t
        return t

    def sb(self, name, shape, dt=F32):
        return self.es.enter_context(self.nc.sbuf_tensor(self.uid(name), list(shape), dt))

    def dscratch(self, name, shape, dt=F32):
        t = self.nc.dram_tensor(name, list(shape), dt, kind="Internal").ap()
        self.dram[name] = t
        return t

    def end_phase(self):
        self.p.emit(barrier=True)
        self.es.close()
        self.es = contextlib.ExitStack()
        self.p = Prog(self.nc)

    def ps(self, i):
        return self.psf[i]

    def finish(self):
        self.p.emit()
        self.es.close()
        self.es0.close()
        return self.nc


C_SBQ, C_SBK, C_SBV = 0, 256, 512
C_MOQ, C_MOK, C_MOV = 768, 1024, 1280
C_CQ, C_CKV, C_KR = 1536, 1792, 1920
C_NSQ, C_NSKV, C_NSG, C_MEQ = 1952, 2208, 2592, 2604


def consts_common(kb):
    nc, p = kb.nc, kb.p
    c = {}
    c["identf"] = kb.sb("identf", [128, 128], F32)
    c["identb"] = kb.sb("identb", [128, 128], BF16)
    c["onesb"] = kb.sb("onesb", [128, 128], BF16)
    c["onesf"] = kb.sb("onesf", [128, 128], F32)
    idf, idb = c["identf"], c["identb"]
    p.pool(lambda e: e.memset(c["onesf"][:], 1.0), writes=["onesf"])
    p.pool(lambda e: e.memset(c["onesb"][:], 1.0), writes=["onesb"])
    p.pool(lambda e: e.affine_select(out=idf[:], in_=c["onesf"][:], pattern=[[-1, 128]], compare_op=ALU.is_equal,
                                     fill=0.0, base=0, channel_multiplier=1), reads=["onesf"], writes=["identf"])
    p.pool(lambda e: e.tensor_copy(out=idb[:], in_=idf[:]), reads=["identf"], writes=["identb"])
    return c


def tm_view(ap2d, p=128):
    return ap2d.rearrange("(n p) c -> p n c", p=p)


def phase_a(kb, c, io, w):
    nc, p = kb.nc, kb.p
    identf, onesb = c["identf"], c["onesb"]

    hT = kb.sb("hT", [128, 8, TOK], BF16)
    win = kb.sb("win", [128, 8, IN_TOTAL], BF16)
    wkrot = kb.sb("wkrot", [128, 8, 32], BF16)
    for f in range(8):
        p.dma(win[:, f, :], w["w_in"][f * 128:(f + 1) * 128, :], writes=[("win", f)], q="pool")
    for f in range(8):
        p.dve(lambda e, f=f: e.tensor_scalar_mul(out=wkrot[:, f, 0:16], in0=win[:, f, C_KR + 16:C_KR + 32], scalar1=-1.0),
              reads=[("win", f)], writes=[("wkrot", f)])
        p.dve(lambda e, f=f: e.tensor_copy(out=wkrot[:, f, 16:32], in_=win[:, f, C_KR:C_KR + 16]),
              reads=[("win", f)], writes=[("wkrot", f)])
    wuq_f = kb.sb("wuq_f", [128, 2, 384], F32)
    wuq = kb.sb("wuq", [128, 2, 384], BF16)
    wuqr = kb.sb("wuqr", [128, 2, 384], BF16)
    gcq = kb.sb("gcq", [128, 2], F32)
    wukv_f = kb.sb("wukv_f", [128, 512], F32)
    wukv = kb.sb("wukv", [128, 512], BF16)
    gckv = kb.sb("gckv", [128, 1], F32)
    p.dma(wuq_f[:], w["w_uq"].rearrange("(n p) c -> p n c", p=128), writes=["wuq_f"])
    p.dma(gcq[:], w["g_cq"].rearrange("(n p) -> p n", p=128), writes=["gcq"], slow=True)
    p.dma(wukv_f[:], w["w_ukv"], writes=["wukv_f"])
    p.dma(gckv[:], w["g_ckv"].rearrange("(n p) -> p n", p=128), writes=["gckv"], slow=True)
    for rc in range(2):
        p.dve(lambda e, rc=rc: e.tensor_scalar_mul(out=wuq[:, rc, :], in0=wuq_f[:, rc, :], scalar1=gcq[:, rc:rc + 1]),
              reads=["wuq_f", "gcq"], writes=["wuq"])
    p.dve(lambda e: e.memset(wuqr[:], 0.0), writes=["wuqr"])
    for rc in range(2):
        for h in range(4):
            b0 = h * 96
            p.dve(lambda e, rc=rc, b0=b0: e.tensor_scalar_mul(out=wuqr[:, rc, b0 + 64:b0 + 80], in0=wuq[:, rc, b0 + 80:b0 + 96], scalar1=-1.0),
                  reads=["wuq"], writes=["wuqr"])
            p.dve(lambda e, rc=rc, b0=b0: e.tensor_copy(out=wuqr[:, rc, b0 + 80:b0 + 96], in_=wuq[:, rc, b0 + 64:b0 + 80]),
                  reads=["wuq"], writes=["wuqr"])
    p.dve(lambda e: e.tensor_scalar_mul(out=wukv[:], in0=wukv_f[:], scalar1=gckv[:, 0:1]), reads=["wukv_f", "gckv"], writes=["wukv"])

    hst = [kb.sb("hst", [128, 1024], F32) for _ in range(2)]
    for ck in range(TOK // 128):
        st = hst[ck % 2]
        tk = ("hst", ck % 2)
        p.dma(st[:], io["h_tok"][ck * 128:(ck + 1) * 128, :], writes=[tk])
        for half in range(2):
            bank = (ck * 2 + half) % 2
            ps = kb.ps(bank)
            for j in range(4):
                f = half * 4 + j
                p.pe(lambda e, ps=ps, st=st, f=f, j=j: e.transpose(out=ps[:, j * 128:(j + 1) * 128], in_=st[:, f * 128:(f + 1) * 128], identity=identf[:]),
                     reads=[tk, "identf"], writes=[("ps", bank)])
            dst = hT[:, half * 4:half * 4 + 4, ck * 128:(ck + 1) * 128]
            src = ps[:].rearrange("p (j t) -> p j t", j=4)
            if half == 0:
                p.act(lambda e, dst=dst, src=src: e.copy(out=dst, in_=src), reads=[("ps", bank)], writes=[("hT", ck // 4)])
            else:
                p.dve(lambda e, dst=dst, src=src: e.tensor_copy(out=dst, in_=src), reads=[("ps", bank)], writes=[("hT", ck // 4)])
    for f in range(8):
        p.dma(io["hT_d"][f * 128:(f + 1) * 128, :], hT[:, f, :], reads=[("hT", s) for s in range(8)], writes=[("hT_d", f)])

    ostage = [kb.sb("ostg", [128, 512], BF16) for _ in range(4)]
    gstage = [kb.sb("gstg", [12, 512], F32) for _ in range(2)]
    cq_sb = [kb.sb("cq_sb", [128, 2, 512], BF16) for _ in range(2)]
    ckv_sb = [kb.sb("ckv_sb", [128, 512], BF16) for _ in range(2)]
    krr_sb = [kb.sb("krr", [32, 2, 512], F32) for _ in range(2)]
    sq_sb = [kb.sb("sq", [128, 3, 512], BF16) for _ in range(2)]
    rstd_q = [kb.sb("rstdq", [128, 512], F32) for _ in range(2)]
    rstd_kv = [kb.sb("rstdkv", [128, 512], F32) for _ in range(2)]
    rkv_tok = [kb.sb("rkvtok", [128, 4], F32) for _ in range(2)]
    ropeq = [kb.sb("ropeq", [96, 2, 512], F32) for _ in range(2)]
    ropek = [kb.sb("ropek", [32, 2, 512], F32) for _ in range(2)]
    t1 = [kb.sb("t1", [96, 512], F32) for _ in range(2)]
    t2 = [kb.sb("t2", [96, 512], F32) for _ in range(2)]
    vstage = [kb.sb("vstg", [128, 640], BF16) for _ in range(2)]
    vmst = [kb.sb("vmst", [128, 256], BF16) for _ in range(2)]
    cnt = {"o": 0, "bank": 0, "v": 0}

    def nbank():
        b = 2 + cnt["bank"] % 6
        cnt["bank"] += 1
        return b

    fm_list = [
        ("qsb_d", 0, C_SBQ, 128, 0.125), ("qsb_d", 128, C_SBQ + 128, 128, 0.125),
        ("ksb_d", 0, C_SBK, 128, 1.0), ("ksb_d", 128, C_SBK + 128, 128, 1.0),
        ("qmo_d", 0, C_MOQ, 128, 0.125), ("qmo_d", 128, C_MOQ + 128, 128, 0.125),
        ("kmo_d", 0, C_MOK, 128, 1.0), ("kmo_d", 128, C_MOK + 128, 128, 1.0),
        ("qns_d", 0, C_NSQ, 128, 0.125), ("qns_d", 128, C_NSQ + 128, 128, 0.125),
        ("kcv_d", 0, C_NSKV, 128, 1.0),
        ("ksl_d", 0, C_NSKV + 128, 64, 1.0),
        ("kwi_d", 0, C_NSKV + 256, 64, 1.0),
        ("qme_d", 0, C_MEQ, 128, 0.125), ("qme_d", 128, C_MEQ + 128, 128, 0.125),
    ]

    def proj_fm(s, col0, ncols, wsrc=None):
        b = nbank()
        ps = kb.ps(b)
        for f in range(8):
            if wsrc is None:
                lhsT = win[:, f, col0:col0 + ncols]
                rd = [("win", f)]
            else:
                lhsT = wsrc[:, f, col0:col0 + ncols]
                rd = [("wkrot", f)]
            p.pe(lambda e, ps=ps, lhsT=lhsT, f=f, s=s, ncols=ncols: e.matmul(ps[0:ncols, :], lhsT=lhsT, rhs=hT[:, f, s * 512:(s + 1) * 512],
                                                                            start=(f == 0), stop=(f == 7)),
                 reads=rd + [("hT", s)], writes=[("ps", b)])
        return b

    for s in range(NSLOT):
        tsl = slice(s * 512, (s + 1) * 512)
        for (dn, r0, col0, ncols, scale) in fm_list:
            b = proj_fm(s, col0, ncols)
            ps = kb.ps(b)
            k = cnt["o"] % 4
            cnt["o"] += 1
            og = ostage[k]
            p.act(lambda e, og=og, ps=ps, ncols=ncols, scale=scale: e.activation(out=og[0:ncols, :], in_=ps[0:ncols, :], func=AF.Copy, scale=scale),
                  reads=[("ps", b)], writes=[("ostg", k)])
            p.dma(io[dn][r0:r0 + ncols, tsl], og[0:ncols, :], reads=[("ostg", k)], writes=[(dn, s)])
        b = proj_fm(s, C_NSG, 12)
        ps = kb.ps(b)
        gs = gstage[s % 2]
        p.act(lambda e, gs=gs, ps=ps: e.activation(out=gs[:], in_=ps[0:12, :], func=AF.Sigmoid), reads=[("ps", b)], writes=[("gstg", s % 2)])
        p.dma(io["gns_d"][:, tsl], gs[:], reads=[("gstg", s % 2)], writes=[("gns_d", s)])

        d2 = s % 2
        cq, ckv, sq = cq_sb[d2], ckv_sb[d2], sq_sb[d2]
        for rc in range(2):
            b = proj_fm(s, C_CQ + rc * 128, 128)
            ps = kb.ps(b)
            p.act(lambda e, cq=cq, ps=ps, rc=rc: e.copy(out=cq[:, rc, :], in_=ps[:]), reads=[("ps", b)], writes=[("cq", d2)])
            p.act(lambda e, sq=sq, ps=ps, rc=rc: e.activation(out=sq[:, rc, :], in_=ps[:], func=AF.Square), reads=[("ps", b)], writes=[("sq", d2)])
        b = proj_fm(s, C_CKV, 128)
        ps = kb.ps(b)
        p.act(lambda e, ckv=ckv, ps=ps: e.copy(out=ckv[:], in_=ps[:]), reads=[("ps", b)], writes=[("ckv", d2)])
        p.act(lambda e, sq=sq, ps=ps: e.activation(out=sq[:, 2, :], in_=ps[:], func=AF.Square), reads=[("ps", b)], writes=[("sq", d2)])
        krr = krr_sb[d2]
        b = proj_fm(s, C_KR, 32)
        ps = kb.ps(b)
        p.act(lambda e, krr=krr, ps=ps: e.copy(out=krr[:, 0, :], in_=ps[0:32, :]), reads=[("ps", b)], writes=[("krr", d2)])
        b = proj_fm(s, 0, 32, wsrc=wkrot)
        ps = kb.ps(b)
        p.act(lambda e, krr=krr, ps=ps: e.copy(out=krr[:, 1, :], in_=ps[0:32, :]), reads=[("ps", b)], writes=[("krr", d2)])
        rq, rkv = rstd_q[d2], rstd_kv[d2]
        b = nbank()
        ps = kb.ps(b)
        for rc in range(2):
            p.pe(lambda e, ps=ps, sq=sq, rc=rc: e.matmul(ps[:], lhsT=onesb[:], rhs=sq[:, rc, :], start=(rc == 0), stop=(rc == 1)),
                 reads=[("sq", d2), "onesb"], writes=[("ps", b)])
        p.act(lambda e, rq=rq, ps=ps: e.activation(out=rq[:], in_=ps[:], func=AF.Ln, scale=1.0 / 256.0, bias=c["eps_rms"][:, 0:1]),
              reads=[("ps", b), "cst"], writes=[("rq", d2)])
        p.act(lambda e, rq=rq: e.activation(out=rq[:], in_=rq[:], func=AF.Exp, scale=-0.5), reads=[("rq", d2)], writes=[("rq", d2)])
        b = nbank()
        ps = kb.ps(b)
        p.pe(lambda e, ps=ps, sq=sq: e.matmul(ps[:], lhsT=onesb[:], rhs=sq[:, 2, :], start=True, stop=True),
             reads=[("sq", d2), "onesb"], writes=[("ps", b)])
        p.act(lambda e, rkv=rkv, ps=ps: e.activation(out=rkv[:], in_=ps[:], func=AF.Ln, scale=1.0 / 128.0, bias=c["eps_rms"][:, 0:1]),
              reads=[("ps", b), "cst"], writes=[("rkv", d2)])
        p.act(lambda e, rkv=rkv: e.activation(out=rkv[:], in_=rkv[:], func=AF.Exp, scale=-0.5), reads=[("rkv", d2)], writes=[("rkv", d2)])
        rkt = rkv_tok[d2]
        b = nbank()
        ps = kb.ps(b)
        for ck in range(4):
            p.pe(lambda e, ps=ps, sq=sq, ck=ck: e.matmul(ps[:, ck:ck + 1], lhsT=sq[:, 2, ck * 128:(ck + 1) * 128], rhs=onesb[:, 0:1], start=True, stop=True),
                 reads=[("sq", d2), "onesb"], writes=[("ps", b)])
        p.act(lambda e, rkt=rkt, ps=ps: e.activation(out=rkt[:], in_=ps[:, 0:4], func=AF.Ln, scale=1.0 / 128.0, bias=c["eps_rms"][:, 0:1]),
              reads=[("ps", b), "cst"], writes=[("rkt", d2)])
        p.act(lambda e, rkt=rkt: e.activation(out=rkt[:], in_=rkt[:], func=AF.Exp, scale=-0.5), reads=[("rkt", d2)], writes=[("rkt", d2)])
        rpq, rpk = ropeq[d2], ropek[d2]
        p.dma(rpq[:], io["ropeq_t"][s], writes=[("rpq", d2)])
        p.dma(rpk[:], io["ropek_t"][s], writes=[("rpk", d2)])
        for h in range(4):
            bA, bB = nbank(), nbank()
            psA, psB = kb.ps(bA), kb.ps(bB)
            for rc in range(2):
                p.pe(lambda e, psA=psA, cq=cq, rc=rc, h=h: e.matmul(psA[0:96, :], lhsT=wuq[:, rc, h * 96:(h + 1) * 96], rhs=cq[:, rc, :], start=(rc == 0), stop=(rc == 1)),
                     reads=["wuq", ("cq", d2)], writes=[("ps", bA)])
            for rc in range(2):
                p.pe(lambda e, psB=psB, cq=cq, rc=rc, h=h: e.matmul(psB[0:96, :], lhsT=wuqr[:, rc, h * 96:(h + 1) * 96], rhs=cq[:, rc, :], start=(rc == 0), stop=(rc == 1)),
                     reads=["wuqr", ("cq", d2)], writes=[("ps", bB)])
            a1, a2 = t1[h % 2], t2[h % 2]
            k = cnt["o"] % 4
            cnt["o"] += 1
            og = ostage[k]
            p.dve(lambda e, a1=a1, psA=psA, rpq=rpq: e.tensor_tensor(out=a1[:], in0=psA[0:96, :], in1=rpq[:, 0, :], op=ALU.mult),
                  reads=[("ps", bA), ("rpq", d2)], writes=[("t1", h % 2)])
            p.dve(lambda e, a2=a2, psB=psB, rpq=rpq: e.tensor_tensor(out=a2[:], in0=psB[0:96, :], in1=rpq[:, 1, :], op=ALU.mult),
                  reads=[("ps", bB), ("rpq", d2)], writes=[("t2", h % 2)])
            p.dve(lambda e, a1=a1, a2=a2: e.tensor_tensor(out=a1[:], in0=a1[:], in1=a2[:], op=ALU.add),
                  reads=[("t1", h % 2), ("t2", h % 2)], writes=[("t1", h % 2)])
            p.dve(lambda e, a1=a1, og=og, rq=rq: e.tensor_tensor(out=og[0:96, :], in0=a1[:], in1=rq[0:96, :], op=ALU.mult),
                  reads=[("t1", h % 2), ("rq", d2)], writes=[("ostg", k)])
            p.dma(io["qml_d"][h, :, tsl], og[0:96, :], reads=[("ostg", k)], writes=[("qml_d", s, h)])
            b = nbank()
            ps = kb.ps(b)
            p.pe(lambda e, ps=ps, ckv=ckv, h=h: e.matmul(ps[0:64, :], lhsT=wukv[:, h * 128:h * 128 + 64], rhs=ckv[:], start=True, stop=True),
                 reads=["wukv", ("ckv", d2)], writes=[("ps", b)])
            k = cnt["o"] % 4
            cnt["o"] += 1
            og = ostage[k]
            p.dve(lambda e, og=og, ps=ps, rkv=rkv: e.tensor_tensor(out=og[0:64, :], in0=ps[0:64, :], in1=rkv[0:64, :], op=ALU.mult),
                  reads=[("ps", b), ("rkv", d2)], writes=[("ostg", k)])
            p.dma(io["kml_d"][h, :, tsl], og[0:64, :], reads=[("ostg", k)], writes=[("kml_d", s, h)])
        a1, a2 = t1[0], t2[0]
        k = cnt["o"] % 4
        cnt["o"] += 1
        og = ostage[k]
        p.dve(lambda e, a1=a1, krr=krr, rpk=rpk: e.tensor_tensor(out=a1[0:32, :], in0=krr[:, 0, :], in1=rpk[:, 0, :], op=ALU.mult),
              reads=[("krr", d2), ("rpk", d2)], writes=[("t1", 0)])
        p.dve(lambda e, a2=a2, krr=krr, rpk=rpk: e.tensor_tensor(out=a2[0:32, :], in0=krr[:, 1, :], in1=rpk[:, 1, :], op=ALU.mult),
              reads=[("krr", d2), ("rpk", d2)], writes=[("t2", 0)])
        p.dve(lambda e, a1=a1, a2=a2, og=og: e.tensor_tensor(out=og[0:32, :], in0=a1[0:32, :], in1=a2[0:32, :], op=ALU.add),
              reads=[("t1", 0), ("t2", 0)], writes=[("ostg", k)])
        p.dma(io["krl_d"][:, tsl], og[0:32, :], reads=[("ostg", k)], writes=[("krl_d", s)])
        for ck in range(4):
            gck = s * 4 + ck
            b = nbank()
            ps = kb.ps(b)
            for h in range(4):
                p.pe(lambda e, ps=ps, ckv=ckv, ck=ck, h=h: e.matmul(ps[:, h * 64:(h + 1) * 64], lhsT=ckv[:, ck * 128:(ck + 1) * 128], rhs=wukv[:, h * 128 + 64:h * 128 + 128],
                                                                 start=True, stop=True),
                     reads=["wukv", ("ckv", d2)], writes=[("ps", b)])
            vm = vmst[gck % 2]
            p.act(lambda e, vm=vm, ps=ps, rkt=rkt, ck=ck: e.activation(out=vm[:], in_=ps[:, 0:256], func=AF.Copy, scale=rkt[:, ck:ck + 1]),
                  reads=[("ps", b), ("rkt", d2)], writes=[("vmst", gck % 2)])
            p.dma(io["vml_d"][gck * 128:(gck + 1) * 128, :], vm[:], reads=[("vmst", gck % 2)], writes=[("vml_d", gck)])

        for ck in range(4):
            gck = s * 4 + ck
            tcs = slice(gck * 128, (gck + 1) * 128)
            vs = vstage[gck % 2]
            b1, b2 = nbank(), nbank()
            ps1, ps2 = kb.ps(b1), kb.ps(b2)
            for f in range(8):
                p.pe(lambda e, ps1=ps1, f=f, tcs=tcs: e.matmul(ps1[:, 0:256], lhsT=hT[:, f, tcs], rhs=win[:, f, C_SBV:C_SBV + 256], start=(f == 0), stop=(f == 7)),
                     reads=[("win", f), ("hT", s)], writes=[("ps", b1)])
            for f in range(8):
                p.pe(lambda e, ps1=ps1, f=f, tcs=tcs: e.matmul(ps1[:, 256:512], lhsT=hT[:, f, tcs], rhs=win[:, f, C_MOV:C_MOV + 256], start=(f == 0), stop=(f == 7)),
                     reads=[("win", f), ("hT", s)], writes=[("ps", b1)])
            for f in range(8):
                p.pe(lambda e, ps2=ps2, f=f, tcs=tcs: e.matmul(ps2[:, 0:64], lhsT=hT[:, f, tcs], rhs=win[:, f, C_NSKV + 192:C_NSKV + 256], start=(f == 0), stop=(f == 7)),
                     reads=[("win", f), ("hT", s)], writes=[("ps", b2)])
            for f in range(8):
                p.pe(lambda e, ps2=ps2, f=f, tcs=tcs: e.matmul(ps2[:, 64:128], lhsT=hT[:, f, tcs], rhs=win[:, f, C_NSKV + 320:C_NSKV + 384], start=(f == 0), stop=(f == 7)),
                     reads=[("win", f), ("hT", s)], writes=[("ps", b2)])
            p.act(lambda e, vs=vs, ps1=ps1: e.copy(out=vs[:, 0:512], in_=ps1[:]), reads=[("ps", b1)], writes=[("vstg", gck % 2)])
            p.dve(lambda e, vs=vs, ps2=ps2: e.tensor_copy(out=vs[:, 512:640], in_=ps2[:, 0:128]), reads=[("ps", b2)], writes=[("vstg", gck % 2)])
            p.dma(io["vsb_d"][tcs, :], vs[:, 0:256], reads=[("vstg", gck % 2)], writes=[("vsb_d", gck)])
            p.dma(io["vmo_d"][tcs, :], vs[:, 256:512], reads=[("vstg", gck % 2)], writes=[("vmo_d", gck)])
            p.dma(io["vsw_d"][tcs, :], vs[:, 512:640], reads=[("vstg", gck % 2)], writes=[("vsw_d", gck)])


A_OUTS = {
    "hT_d": ([1024, TOK], BF16),
    "qsb_d": ([256, TOK], BF16), "ksb_d": ([256, TOK], BF16), "vsb_d": ([TOK, 256], BF16),
    "qmo_d": ([256, TOK], BF16), "kmo_d": ([256, TOK], BF16), "vmo_d": ([TOK, 256], BF16),
    "qml_d": ([4, 96, TOK], BF16), "kml_d": ([4, 64, TOK], BF16), "krl_d": ([32, TOK], BF16), "vml_d": ([TOK, 256], BF16),
    "qns_d": ([256, TOK], BF16), "kcv_d": ([128, TOK], BF16), "ksl_d": ([64, TOK], BF16), "kwi_d": ([64, TOK], BF16),
    "vsw_d": ([TOK, 128], BF16), "gns_d": ([12, TOK], F32), "qme_d": ([256, TOK], BF16),
}


def load_consts(kb, io):
    p = kb.p
    c = {}
    c["identf"] = kb.sb("identf", [128, 128], F32)
    c["identb"] = kb.sb("identb", [128, 128], BF16)
    c["onesb"] = kb.sb("onesb", [128, 128], BF16)
    c["onesf"] = kb.sb("onesf", [128, 128], F32)
    c["cstf"] = kb.sb("cstf", [128, 8], F32)
    p.dma(c["identf"][:], io["c_ident"], writes=["identf"])
    p.dma(c["identb"][:], io["c_ident"], writes=["identb"], q="pool")
    p.dma(c["onesf"][:], io["c_ones"], writes=["onesf"])
    p.dma(c["onesb"][:], io["c_ones"], writes=["onesb"], q="pool")
    p.dma(c["cstf"][:], io["c_cst"], writes=["cst"])
    c["eps_rms"] = c["cstf"][:, 0:1]
    c["eps_ln"] = c["cstf"][:, 1:2]
    c["tiny"] = c["cstf"][:, 2:3]
    c["zrow"] = kb.sb("zrow", [1, 8], BF16)
    p.pool(lambda e: e.memset(c["zrow"][:], 0.0), writes=["zrow"])
    return c


def host_consts():
    cst = np.zeros((128, 8), np.float32)
    cst[:, 0] = RMS_EPS
    cst[:, 1] = LN_EPS
    cst[:, 2] = 1e-30
    cst[:, 3] = 1.0
    return {"c_ident": np.eye(128, dtype=np.float32), "c_ones": np.ones((128, 128), np.float32), "c_cst": cst}


def rope_tables(r):
    half = 16
    freqs = np.power(np.float32(10000.0), -np.arange(half, dtype=np.float32) / half).astype(np.float32)
    rq = np.zeros((NSLOT, 96, 2, 512), np.float32)
    rk = np.zeros((NSLOT, 32, 2, 512), np.float32)
    sc = np.float32(96.0 ** -0.5)
    for s in range(NSLOT):
        pos = (512 * (2 * s + r) + np.arange(512)).astype(np.float32)
        ang = pos[None, :] * freqs[:, None]
        cos, sin = np.cos(ang).astype(np.float32), np.sin(ang).astype(np.float32)
        c2 = np.concatenate([cos, cos], 0)
        s2 = np.concatenate([sin, sin], 0)
        rq[s, 0:64, 0, :] = sc
        rq[s, 64:96, 0, :] = sc * c2
        rq[s, 64:96, 1, :] = sc * s2
        rk[s, :, 0, :] = c2
        rk[s, :, 1, :] = s2
    return rq, rk


def build_a():
    kb = KB()
    io = {}
    io["h_tok"] = kb.din("h_tok", [TOK, D])
    io["c_ident"] = kb.din("c_ident", [128, 128])
    io["c_ones"] = kb.din("c_ones", [128, 128])
    io["c_cst"] = kb.din("c_cst", [128, 8])
    io["ropeq_t"] = kb.din("ropeq_t", [NSLOT, 96, 2, 512])
    io["ropek_t"] = kb.din("ropek_t", [NSLOT, 32, 2, 512])
    w = {"w_in": kb.din("w_in", [D, IN_TOTAL]), "w_uq": kb.din("w_uq", [256, 384]), "g_cq": kb.din("g_cq", [256]),
         "w_ukv": kb.din("w_ukv", [128, 512]), "g_ckv": kb.din("g_ckv", [128])}
    for n, (shp, dt) in A_OUTS.items():
        io[n] = kb.dout(n, shp, dt)
    c = load_consts(kb, io)
    phase_a(kb, c, io, w)
    return kb.finish()


def bconsts_host(r):
    bf = ml_dtypes.bfloat16
    o = {}
    kl = np.arange(128)[:, None]
    ql = np.arange(512)[None, :]
    cm = np.zeros((8, 128, 512), np.float32)
    cms = np.zeros((8, 128, 512), np.float32)
    for jj in range(8):
        kp = 128 * jj + kl
        qp = 512 * r + ql
        cm[jj] = np.where(kp <= qp, 0.0, MASKV)
        cms[jj] = np.where(kp < qp, 0.0, MASKV)
    o["c_cm"] = cm.transpose(1, 0, 2).astype(bf)
    o["c_cms"] = cms.transpose(1, 0, 2).astype(bf)
    wm = np.zeros((12, 128, 512), np.float32)
    for ji, jrel in enumerate(range(-4, 8)):
        dist = 512 * r + ql - 128 * jrel - kl
        wm[ji] = np.where((dist >= 0) & (dist < 512), 0.0, MASKV)
    o["c_wm"] = wm.transpose(1, 0, 2).astype(bf)
    pm = np.zeros((3, 128, 512), np.float32)
    for ii, idx in enumerate((6, 7, 8)):
        pm[ii] = np.where(16 * kl + 31 - 512 * r - ql <= 1024 * (idx - 6), 0.0, MASKV)
    o["c_pm"] = pm.transpose(1, 0, 2).astype(bf)
    kp = np.arange(S)
    o["c_kaug"] = np.stack([kp // 128, kp % 128, np.ones(S), np.ones(S)]).astype(bf)
    ce = 16 * np.arange(512) + 31
    caug = np.stack([ce // 128, ce % 128, np.ones(512), np.ones(512)]).astype(np.float32)
    caug[0, 511] = -30000.0
    o["c_caug"] = caug.astype(bf)
    tl = np.arange(TOK)
    qp = 512 * (2 * (tl // 512) + r) + tl % 512
    qa = np.zeros((4, 4, TOK), np.float32)
    for h in range(4):
        sl = SLOPES[h]
        qa[h, 0] = 128 * sl
        qa[h, 1] = sl
        qa[h, 2] = -sl * 128 * (qp // 128)
        qa[h, 3] = -sl * (qp % 128)
    o["c_qaug"] = qa.astype(bf)
    o["c_g32"] = ((np.arange(S)[None, :] // 64) % 32 == np.arange(32)[:, None]).astype(np.float32).astype(bf)
    o["c_tm"] = (np.arange(S)[None, :] // 256 == np.arange(32)[:, None]).astype(np.float32).astype(bf)
    n = np.arange(512)[:, None]
    s_ = np.arange(128)[None, :]
    ov = ((16 * n < 64 * s_ + 64) & (16 * n + 32 > 64 * s_)).astype(np.float32)
    ov = np.concatenate([ov, np.ones((512, 1), np.float32)], 1)
    ov[511] = 0.0
    o["c_nui"] = -(np.arange(128)[:, None] >= np.arange(128)[None, :]).astype(np.float32).astype(bf)
    o["c_ov"] = ov.reshape(4, 128, 129).transpose(1, 0, 2).astype(bf)
    vb = np.zeros((8, 4, 32), np.float32)
    own_t = np.zeros((8, 4, 32), np.float32)
    for s in range(8):
        for qb in range(4):
            own = (4 * (2 * s + r) + qb) // 2
            vb[s, qb] = np.where(np.arange(32) < own, 0.0, -1e30)
            own_t[s, qb, own] = 1.0
    o["c_vb"] = np.broadcast_to(vb[None], (128, 8, 4, 32)).copy()
    o["c_own"] = np.broadcast_to(own_t[None], (128, 8, 4, 32)).copy()
    M = np.zeros((8, 128, 4, 128), np.float32)
    C = np.zeros((8, 128, 4, 128), np.float32)
    sid = np.arange(128)[None, :]
    for s in range(8):
        for qb in range(4):
            qpos = 512 * (2 * s + r) + 128 * qb + np.arange(128)[:, None]
            cur = qpos // 64
            forced_cur = sid == cur
            forced0 = (sid == 0) & ~forced_cur
            past = (sid < cur) & ~forced0 & ~forced_cur
            M[s, :, qb] = past
            C[s, :, qb] = np.where(forced_cur, 1e30, np.where(forced0, 5e29, np.where(past, 0.0, -1e30)))
    o["c_selm"] = M
    o["c_selc"] = C
    return o


B_CONST_SHAPES = {
    "c_cm": ([128, 8, 512], BF16), "c_cms": ([128, 8, 512], BF16), "c_wm": ([128, 12, 512], BF16), "c_pm": ([128, 3, 512], BF16),
    "c_kaug": ([4, S], BF16), "c_caug": ([4, 512], BF16), "c_qaug": ([4, 4, TOK], BF16),
    "c_g32": ([32, S], BF16), "c_tm": ([32, S], BF16), "c_ov": ([128, 4, 129], BF16),
    "c_nui": ([128, 128], BF16), "c_vb": ([128, 8, 4, 32], F32), "c_own": ([128, 8, 4, 32], F32),
    "c_selm": ([8, 128, 4, 128], F32), "c_selc": ([8, 128, 4, 128], F32),
}

B_INS = {
    "qsb_d": ([256, TOK], BF16), "qmo_d": ([256, TOK], BF16), "qml_d": ([4, 96, TOK], BF16), "qns_d": ([256, TOK], BF16),
    "qme_d": ([256, TOK], BF16), "gns_d": ([12, TOK], F32),
    "ksb_f": ([256, S], BF16), "vsb_f": ([S, 256], BF16), "kmo_f": ([256, S], BF16), "vmo_f": ([S, 256], BF16),
    "kml_f": ([4, 64, S], BF16), "krl_f": ([32, S], BF16), "vml_f": ([S, 256], BF16),
    "kcv_f": ([128, S], BF16), "ksl_f": ([64, S], BF16), "kwi_f": ([64, S], BF16), "vsw_f": ([S, 128], BF16),
    "mem": ([256, D], F32),
}


class AttnBufs:
    pass


class KFull:
    def __init__(self, ap):
        self.ap = ap

    def rows(self, lo, hi):
        return ("full", self.ap[lo:hi, :])


class KPair:
    def __init__(self, ap, R, base=0):
        self.ap, self.R, self.base = ap, R, base

    def rows(self, lo, hi):
        return ("pair", self.ap, self.R, self.base + lo, hi - lo)


class VFull:
    def __init__(self, ap, cbase=0):
        self.ap, self.cbase = ap, cbase

    def cols(self, c0):
        return ("full", self.ap, self.cbase + c0)


class VPair:
    def __init__(self, chunks, cbase=0):
        self.chunks, self.cbase = chunks, cbase

    def cols(self, c0):
        return ("pair", self.chunks, self.cbase + c0)


class TMChunks:
    def __init__(self, chunks, c0, c1):
        self.chunks, self.c0, self.c1 = chunks, c0, c1

    def __getitem__(self, key):
        rs, cs = key
        k = rs.start // 1024
        a = self.chunks[k][rs.start - 1024 * k:rs.stop - 1024 * k, self.c0:self.c1]
        return a[:, cs]


def phase_b(kb, c, io, w, branches=("sb", "moba", "mla", "nsa", "mem")):
    nc, p = kb.nc, kb.p
    A = AttnBufs()
    A.KT = [kb.sb("KT", [128, S], BF16) for _ in range(2)]
    A.V = [kb.sb("V", [128, 64, 65], BF16) for _ in range(2)]
    A.QT = kb.sb("QT", [128, 4, TOK], BF16)
    A.cm = kb.sb("cm", [128, 8, 512], BF16)
    A.cms = kb.sb("cms", [128, 8, 512], BF16)
    A.P = [kb.sb("P", [128, 512], BF16) for _ in range(3)]
    A.rden = [kb.sb("rden", [65, 512], F32) for _ in range(2)]
    A.bcs = [kb.sb("bcs", [64, 512], F32) for _ in range(2)]
    A.ost = [kb.sb("ost", [64, 512], BF16) for _ in range(2)]
    A.cnt = {"kt": 0, "v": 0, "P": 0, "fin": 0, "sc": 0}
    p.dma(A.cm[:], io["c_cm"], writes=["cm"])
    p.dma(A.cms[:], io["c_cms"], writes=["cms"])
    for i in range(2):
        p.pool(lambda e, i=i: e.memset(A.V[i][:, :, 64:65], 1.0), writes=[("V", i)])

    def load_K(rows_src, dk, aug=None):
        i = A.cnt["kt"] % 2
        A.cnt["kt"] += 1
        kt = A.KT[i]
        for (src, r0) in rows_src:
            if src[0] == "full":
                n = src[1].shape[0]
                p.dma(kt[r0:r0 + n, :], src[1], writes=[("KT", i)])
            else:
                _, ap, R, row0, n = src
                for rr in range(2):
                    p.dma(kt[r0:r0 + n, :].rearrange("p (s r i) -> p s r i", r=2, i=512)[:, :, rr, :],
                          ap[rr * R + row0:rr * R + row0 + n, :].rearrange("p (s i) -> p s i", i=512), writes=[("KT", i)])
        if aug is not None:
            p.dma(kt[dk:dk + 4, :], aug, writes=[("KT", i)])
        return kt, ("KT", i)

    def load_V(src, col0):
        i = A.cnt["v"] % 2
        A.cnt["v"] += 1
        v = A.V[i]
        sp_ = src.cols(col0)
        if sp_[0] == "full":
            p.dma(v[:, :, 0:64], sp_[1].rearrange("(n p) c -> p n c", p=128)[:, :, sp_[2]:sp_[2] + 64], writes=[("V", i)])
        else:
            _, chunks, cc0 = sp_
            for k, ch in enumerate(chunks):
                for rr in range(2):
                    for s2 in range(2):
                        n0 = 16 * k + 8 * s2 + 4 * rr
                        p.dma(v[:, n0:n0 + 4, 0:64],
                              ch[rr * 1024 + s2 * 512:rr * 1024 + (s2 + 1) * 512, cc0:cc0 + 64].rearrange("(q p) c -> p q c", p=128),
                              writes=[("V", i)])
        return v, ("V", i)

    def load_Q(src_rows, h, dk, aug=None):
        p.dma(A.QT[0:dk, h, :], src_rows, writes=[("QT", h)])
        if aug is not None:
            p.dma(A.QT[dk:dk + 4, h, :], aug, writes=[("QT", h)])
        return ("QT", h)

    def finalize_plain(ops_bank, dst, h, s, gate=None, acc=None, acc_tok=None, first=True, last=True):
        k = A.cnt["fin"] % 2
        A.cnt["fin"] += 1
        ps = kb.ps(ops_bank)
        rd, bcs, ost = A.rden[k], A.bcs[k], A.ost[k]
        bcb = 6 + k

        def part1():
            p.dve(lambda e: e.tensor_scalar(out=rd[64:65, :], in0=ps[64:65, :], scalar1=1e-30, scalar2=None, op0=ALU.max),
                  reads=[("ps", ops_bank)], writes=[("rden", k)])
            p.dve(lambda e: e.reciprocal(out=rd[64:65, :], in_=rd[64:65, :]), reads=[("rden", k)], writes=[("rden", k)])
            if gate is not None:
                gt, gtok, gidx = gate
                p.dve(lambda e: e.tensor_tensor(out=rd[64:65, :], in0=rd[64:65, :], in1=gt[64:65, gidx, :], op=ALU.mult),
                      reads=[("rden", k), gtok], writes=[("rden", k)])

        def part2():
            pb = kb.ps(bcb)
            p.pe(lambda e: e.matmul(pb[0:64, :], lhsT=c["onesf"][64:65, 0:64], rhs=rd[64:65, :], start=True, stop=True),
                 reads=[("rden", k), "onesf"], writes=[("ps", bcb)])
            p.act(lambda e: e.copy(out=bcs[:], in_=pb[0:64, :]), reads=[("ps", bcb)], writes=[("bcs", k)])
            if acc is None:
                p.dve(lambda e: e.tensor_tensor(out=ost[:], in0=ps[0:64, :], in1=bcs[:], op=ALU.mult),
                      reads=[("ps", ops_bank), ("bcs", k)], writes=[("ost", k)])
                p.dma(dst, ost[:], reads=[("ost", k)], writes=[("o_d", h, s, id(dst) % 997)])
            else:
                if first:
                    p.dve(lambda e: e.tensor_tensor(out=acc, in0=ps[0:64, :], in1=bcs[:], op=ALU.mult),
                          reads=[("ps", ops_bank), ("bcs", k)], writes=[acc_tok])
                else:
                    p.dve(lambda e: e.tensor_tensor(out=bcs[:], in0=ps[0:64, :], in1=bcs[:], op=ALU.mult),
                          reads=[("ps", ops_bank), ("bcs", k)], writes=[("bcs", k)])
                    p.dve(lambda e: e.tensor_tensor(out=acc, in0=acc, in1=bcs[:], op=ALU.add),
                          reads=[acc_tok, ("bcs", k)], writes=[acc_tok])
                if last:
                    p.dve(lambda e: e.tensor_copy(out=ost[:], in_=acc), reads=[acc_tok], writes=[("ost", k)])
                    p.dma(dst, ost[:], reads=[("ost", k)], writes=[("o_d", h, s, id(dst) % 997)])
        return part1, part2

    def run_softmax(items, KT, ktok, dk, V, vtok, qh, qtok, pending, hook=None):
        n = len(items)

        def stage1(i):
            it = items[i]
            if "pre" in it:
                it["pre"]()
            b = i % 2
            ps = kb.ps(b)
            ex = it["extras"]
            s, j = it["s"], it["j"]
            qr = it["qrhs"] if "qrhs" in it else A.QT[0:dk, qh, s * 512:(s + 1) * 512]
            p.pe(lambda e: e.matmul(ps[:], lhsT=KT[0:dk, j * 128:(j + 1) * 128], rhs=qr,
                                    start=True, stop=(len(ex) == 0)),
                 reads=[ktok, qtok] + list(it.get("qreads", ())), writes=[("ps", b)])
            for xi, (lh, rh, toks) in enumerate(ex):
                p.pe(lambda e, lh=lh, rh=rh, xi=xi: e.matmul(ps[:], lhsT=lh, rhs=rh, start=False, stop=(xi == len(ex) - 1)),
                     reads=list(toks), writes=[("ps", b)])
            if "pdst" in it:
                P, ptok = it["pdst"]
            else:
                pk = A.cnt["P"] % 3
                A.cnt["P"] += 1
                P, ptok = A.P[pk][:], ("P", pk)
            it["P"], it["ptok"] = P, ptok
            p.act(lambda e: e.activation(out=P, in_=ps[:], func=AF.Exp), reads=[("ps", b)], writes=[ptok])

        def stage2(i):
            it = items[i]
            P, ptok = it["P"], it["ptok"]
            ob = it["obank"]
            po = kb.ps(ob)
            jv = it.get("jv", it["j"])
            p.pe(lambda e: e.matmul(po[0:65, :], lhsT=V[:, jv, 0:65], rhs=P, start=it["first"], stop=it["last"]),
                 reads=[vtok, ptok], writes=[("ps", ob)])
            if it["last"]:
                p1, p2 = it["fin"]
                p1()
                pending.append([2, p2])

        for i in range(n + 1):
            if i < n:
                stage1(i)
            if i >= 1:
                stage2(i - 1)
            for pd in list(pending):
                pd[0] -= 1
                if pd[0] <= 0:
                    pd[1]()
                    pending.remove(pd)

    def flush(pending):
        for pd in pending:
            pd[1]()
        pending.clear()

    A.load_K, A.load_V, A.load_Q = load_K, load_V, load_Q
    A.finalize_plain, A.run_softmax, A.flush = finalize_plain, run_softmax, flush
    oT = io["oT_d"]

    if "mla" in branches:
        pending = []
        for h in range(4):
            KT, ktok = load_K([(io["kml_f"].rows(h * 64, h * 64 + 64), 0), (io["krl_f"].rows(0, 32), 64)], 96)
            V, vtok = load_V(io["vml_f"], h * 64)
            qtok = load_Q(io["qml_d"][h], h, 96)
            items = []
            for s in range(NSLOT):
                nkb = 8 * s + 8
                ob = 4 + (s % 2)
                for j in range(nkb):
                    jj = j - 8 * s
                    ex = []
                    if jj >= 0:
                        ex.append((c["identb"][:], A.cm[:, jj, :], ["identb", "cm"]))
                    it = dict(j=j, s=s, extras=ex, first=(j == 0), last=(j == nkb - 1), obank=ob)
                    if j == nkb - 1:
                        it["fin"] = finalize_plain(ob, oT[2, h * 64:(h + 1) * 64, s * 512:(s + 1) * 512], h, s)
                    items.append(it)
            run_softmax(items, KT, ktok, 96, V, vtok, h, qtok, pending)
        flush(pending)

    if "mem" in branches:
        pending = []
        memst = kb.sb("memst", [128, 2, 1024], F32)
        memT = kb.sb("memT", [128, 8, 256], BF16)
        wmk = kb.sb("wmk", [128, 8, 512], BF16)
        KTm = kb.sb("KTm", [64, 4, 256], BF16)
        Vm = kb.sb("Vm", [128, 2, 4, 65], BF16)
        p.dma(memst[:], io["mem"].rearrange("(n p) c -> p n c", p=128), writes=["memst"])
        p.dma(wmk[:], w["w_mem_kv"].rearrange("(f p) c -> p f c", p=128), writes=["wmk"], q="pool")
        p.pool(lambda e: e.memset(Vm[:, :, :, 64:65], 1.0), writes=["Vm"])
        for kc in range(2):
            for half in range(2):
                ps = kb.ps(7)
                for jx in range(4):
                    f = half * 4 + jx
                    p.pe(lambda e, ps=ps, kc=kc, f=f, jx=jx: e.transpose(out=ps[:, jx * 128:(jx + 1) * 128], in_=memst[:, kc, f * 128:(f + 1) * 128], identity=c["identf"][:]),
                         reads=["memst", "identf"], writes=[("ps", 7)])
                p.dve(lambda e, ps=ps, kc=kc, half=half: e.tensor_copy(out=memT[:, half * 4:half * 4 + 4, kc * 128:(kc + 1) * 128], in_=ps[:].rearrange("p (j t) -> p j t", j=4)),
                      reads=[("ps", 7)], writes=["memT"])
        for h in range(4):
            ps = kb.ps(7)
            for f in range(8):
                p.pe(lambda e, ps=ps, f=f, h=h: e.matmul(ps[0:64, 0:256], lhsT=wmk[:, f, h * 64:(h + 1) * 64], rhs=memT[:, f, :], start=(f == 0), stop=(f == 7)),
                     reads=["wmk", "memT"], writes=[("ps", 7)])
            p.dve(lambda e, ps=ps, h=h: e.tensor_copy(out=KTm[:, h, :], in_=ps[0:64, 0:256]), reads=[("ps", 7)], writes=["KTm"])
        for kc in range(2):
            ps = kb.ps(7)
            for f in range(8):
                p.pe(lambda e, ps=ps, f=f, kc=kc: e.matmul(ps[:, 0:256], lhsT=memT[:, f, kc * 128:(kc + 1) * 128], rhs=wmk[:, f, 256:512], start=(f == 0), stop=(f == 7)),
                     reads=["wmk", "memT"], writes=[("ps", 7)])
            p.dve(lambda e, ps=ps, kc=kc: e.tensor_copy(out=Vm[:, kc, :, 0:64], in_=ps[:, 0:256].rearrange("p (h d) -> p h d", h=4)),
                  reads=[("ps", 7)], writes=["Vm"])
        for h in range(4):
            qtok = load_Q(io["qme_d"][h * 64:(h + 1) * 64, :], h, 64)
            items = []
            for s in range(NSLOT):
                ob = 4 + (s % 2)
                for j in range(2):
                    it = dict(j=j, s=s, extras=[], first=(j == 0), last=(j == 1), obank=ob)
                    if j == 1:
                        it["fin"] = finalize_plain(ob, oT[4, h * 64:(h + 1) * 64, s * 512:(s + 1) * 512], h, s)
                    items.append(it)
            run_softmax(items, KTm[:, h, :], "KTm", 64, Vm[:, :, h, :], "Vm", h, qtok, pending)
        flush(pending)

    if "moba" in branches:
        pending = []
        vbt = kb.sb("vbt", [128, 8, 4, 32], F32)
        ownt = kb.sb("ownt", [128, 8, 4, 32], F32)
        kmf = kb.sb("kmf", [64, 32], F32)
        kmb = kb.sb("kmb", [64, 32], BF16)
        gsv = kb.sb("gsv", [128, 4, 32], F32)
        m8 = kb.sb("m8", [128, 4, 8], F32)
        m1p = kb.sb("m1p", [128, 4, 128], F32)
        m1 = m1p[:, :, 64:96]
        m2 = kb.sb("m2", [128, 4, 32], F32)
        p.pool(lambda e: e.memset(m1p[:], 0.0), writes=["m1"])
        p.dma(vbt[:], io["c_vb"], writes=["vbt"])
        p.dma(ownt[:], io["c_own"], writes=["ownt"])
        for h in range(4):
            KT, ktok = load_K([(io["kmo_f"].rows(h * 64, h * 64 + 64), 0), (("full", io["c_tm"]), 64)], 96, aug=io["c_kaug"])
            V, vtok = load_V(io["vmo_f"], h * 64)
            qtok = ("QT", h)
            p.dma(A.QT[0:64, h, :], io["qmo_d"][h * 64:(h + 1) * 64, :], writes=[qtok])
            p.dma(A.QT[96:100, h, :], io["c_qaug"][h], writes=[qtok])
            p.dve(lambda e, KT=KT: e.tensor_reduce(out=kmf[:], in_=KT[0:64, :].rearrange("p (n k) -> p n k", k=256), axis=AX.X, op=ALU.add),
                  reads=[ktok], writes=["kmf"])
            p.dve(lambda e: e.tensor_scalar_mul(out=kmb[:], in0=kmf[:], scalar1=1.0 / 256.0), reads=["kmf"], writes=["kmb"])

            def make_pre(s, h=h, qtok=qtok):
                def pre():
                    ps = kb.ps(7)
                    for qb in range(4):
                        c0 = s * 512 + qb * 128
                        p.pe(lambda e, qb=qb, c0=c0: e.matmul(ps[:, qb * 32:(qb + 1) * 32], lhsT=A.QT[0:64, h, c0:c0 + 128], rhs=kmb[:], start=True, stop=True),
                             reads=[qtok, "kmb"], writes=[("ps", 7)])
                    p.dve(lambda e: e.tensor_tensor(out=gsv[:], in0=ps[:, 0:128].rearrange("p (a b) -> p a b", a=4), in1=vbt[:, s, :, :], op=ALU.add),
                          reads=[("ps", 7), "vbt"], writes=["gsv"])
                    for qb in range(4):
                        p.dve(lambda e, qb=qb: e.max(out=m8[:, qb, :], in_=gsv[:, qb, :]), reads=["gsv"], writes=["m8"])
                    for qb in range(4):
                        p.dve(lambda e, qb=qb: e.tensor_scalar(out=m1[:, qb, :], in0=gsv[:, qb, :], scalar1=m8[:, qb, 2:3], scalar2=None, op0=ALU.is_ge),
                              reads=["gsv", "m8"], writes=["m1"])
                    p.dve(lambda e: e.tensor_scalar(out=m2[:], in0=gsv[:], scalar1=-1e29, scalar2=None, op0=ALU.is_gt), reads=["gsv"], writes=["m2"])
                    p.dve(lambda e: e.tensor_tensor(out=m1, in0=m1, in1=m2[:], op=ALU.mult), reads=["m1", "m2"], writes=["m1"])
                    p.dve(lambda e: e.tensor_tensor(out=m1, in0=m1, in1=ownt[:, s, :, :], op=ALU.add), reads=["m1", "ownt"], writes=["m1"])
                    p.dve(lambda e: e.tensor_scalar(out=m1, in0=m1, scalar1=1.0, scalar2=-MASKV, op0=ALU.subtract, op1=ALU.mult),
                          reads=["m1"], writes=["m1"])

                def pre2():
                    ps2 = kb.ps(7)
                    for qb in range(4):
                        p.pe(lambda e, qb=qb: e.transpose(out=ps2[:, qb * 128:(qb + 1) * 128], in_=m1p[:, qb, :], identity=c["identf"][:]),
                             reads=["m1", "identf"], writes=[("ps", 7)])
                    p.act(lambda e: e.copy(out=A.QT[64:96, h, s * 512:(s + 1) * 512], in_=ps2[64:96, :]), reads=[("ps", 7)], writes=[("QTs", h, s)])
                return pre, pre2

            items = []
            for s in range(NSLOT):
                nkb = 8 * s + 8
                ob = 4 + (s % 2)
                for j in range(nkb):
                    jj = j - 8 * s
                    ex = []
                    if jj >= 0:
                        ex.append((c["identb"][:], A.cm[:, jj, :], ["identb", "cm"]))
                    it = dict(j=j, s=s, extras=ex, first=(j == 0), last=(j == nkb - 1), obank=ob, qreads=[("QTs", h, s)])
                    if j == nkb - 1:
                        it["fin"] = finalize_plain(ob, oT[1, h * 64:(h + 1) * 64, s * 512:(s + 1) * 512], h, s)
                    items.append(it)
            first_of = {}
            for idx, it in enumerate(items):
                if it["j"] == 0:
                    first_of[it["s"]] = idx
            hooks = {}
            for s in range(NSLOT):
                pa, pb_ = make_pre(s)
                f0 = first_of[s]
                i1 = max(0, f0 - 14) if s > 0 else 0
                i2 = max(i1, f0 - 2) if s > 0 else 0
                hooks.setdefault(i1, []).append(pa)
                hooks.setdefault(i2, []).append(pb_)
            for idx, fl in hooks.items():
                items[idx]["pre"] = (lambda fl=fl: [f() for f in fl])
            run_softmax(items, KT, ktok, 100, V, vtok, h, qtok, pending)
        flush(pending)

    if "sb" in branches:
        nui = kb.sb("nui", [128, 128], BF16)
        negone = kb.sb("negone", [1, 128], BF16)
        p.dma(nui[:], io["c_nui"], writes=["nui"])
        p.pool(lambda e: e.memset(negone[:], -1.0), writes=["negone"])
        E = [kb.sb("E", [128, 512], F32) for _ in range(2)]
        SP = [kb.sb("SP", [128, 512], BF16) for _ in range(2)]
        AB = [kb.sb("AB", [128, 512], BF16) for _ in range(2)]
        carf = [kb.sb("carf", [1, 512], F32) for _ in range(2)]
        carb = [kb.sb("carb", [1, 512], BF16) for _ in range(2)]
        sbo = [kb.sb("sbo", [64, 512], BF16) for _ in range(2)]
        one_ap = c["cstf"][:, 3:4]
        for hp in range(2):
            hs = (2 * hp, 2 * hp + 1)
            KTs, ktoks, Vs, vtoks, qtoks = [], [], [], [], []
            for h in hs:
                KT, ktok = load_K([(io["ksb_f"].rows(h * 64, h * 64 + 64), 0)], 64)
                V, vtok = load_V(io["vsb_f"], h * 64)
                qtok = load_Q(io["qsb_d"][h * 64:(h + 1) * 64, :], h, 64)
                KTs.append(KT); ktoks.append(ktok); Vs.append(V); vtoks.append(vtok); qtoks.append(qtok)
            merged = []
            for s_ in range(NSLOT):
                nkb = 8 * s_ + 8
                for j in range(nkb - 1, -1, -1):
                    for st in range(2):
                        merged.append(dict(j=j, s=s_, st=st, h=hs[st], first=(j == nkb - 1), last=(j == 0)))
            for i, it in enumerate(merged):
                it["i"] = i

            def qk(it, bank, more):
                ps = kb.ps(bank)
                j, s_, st, h = it["j"], it["s"], it["st"], it["h"]
                jj = j - 8 * s_
                KT = KTs[st]
                p.pe(lambda e: e.matmul(ps[:], lhsT=KT[0:64, j * 128:(j + 1) * 128], rhs=A.QT[0:64, h, s_ * 512:(s_ + 1) * 512],
                                        start=True, stop=(jj < 0 and not more)),
                     reads=[ktoks[st], qtoks[st]], writes=[("ps", bank)])
                if jj >= 0:
                    p.pe(lambda e: e.matmul(ps[:], lhsT=c["identb"][:], rhs=A.cms[:, jj, :], start=False, stop=(not more)),
                         reads=["identb", "cms"], writes=[("ps", bank)])
                return ps

            def s1(it):
                k = it["i"] % 2
                b4 = it["i"] % 4
                ps = qk(it, b4, False)
                p.act(lambda e: e.activation(out=E[k][:], in_=ps[:], func=AF.Exp), reads=[("ps", b4)], writes=[("E", k)])
                p.act(lambda e: e.activation(out=SP[k][:], in_=E[k][:], func=AF.Ln, bias=one_ap), reads=[("E", k), "cst"], writes=[("SP", k)])

            def s2a(it):
                k = it["i"] % 2
                b4 = it["i"] % 4
                st = it["st"]
                ps = kb.ps(b4)
                fin_carry = not it["first"]
                p.pe(lambda e: e.matmul(ps[:], lhsT=nui[:], rhs=SP[k][:], start=False, stop=(not fin_carry)),
                     reads=["nui", ("SP", k)], writes=[("ps", b4)])
                if fin_carry:
                    p.pe(lambda e: e.matmul(ps[:], lhsT=negone[0:1, :], rhs=carb[st][0:1, :], start=False, stop=True),
                         reads=["negone", ("carb", st)], writes=[("ps", b4)])
                if not it["last"]:
                    pc = kb.ps(6 + st)
                    p.pe(lambda e: e.matmul(pc[0:1, :], lhsT=c["onesb"][:, 0:1], rhs=SP[k][:], start=True, stop=True),
                         reads=["onesb", ("SP", k)], writes=[("ps", 6 + st)])
                    if it["first"]:
                        p.dve(lambda e: e.tensor_copy(out=carf[st][:], in_=pc[0:1, :]), reads=[("ps", 6 + st)], writes=[("carf", st)])
                    else:
                        p.dve(lambda e: e.tensor_tensor(out=carf[st][:], in0=carf[st][:], in1=pc[0:1, :], op=ALU.add),
                              reads=[("ps", 6 + st), ("carf", st)], writes=[("carf", st)])
                    p.dve(lambda e: e.tensor_copy(out=carb[st][:], in_=carf[st][:]), reads=[("carf", st)], writes=[("carb", st)])
                p.act(lambda e: e.activation(out=AB[k][:], in_=ps[:], func=AF.Exp), reads=[("ps", b4)], writes=[("AB", k)])

            def s2b(it):
                k = it["i"] % 2
                st, j, s_, h = it["st"], it["j"], it["s"], it["h"]
                po = kb.ps(4 + st)
                V = Vs[st]
                p.pe(lambda e: e.matmul(po[0:64, :], lhsT=V[:, j, 0:64], rhs=AB[k][:], start=it["first"], stop=it["last"]),
                     reads=[vtoks[st], ("AB", k)], writes=[("ps", 4 + st)])
                if it["last"]:
                    p.dve(lambda e: e.tensor_copy(out=sbo[st][:], in_=po[0:64, :]), reads=[("ps", 4 + st)], writes=[("sbo", st)])
                    p.dma(oT[0, h * 64:(h + 1) * 64, s_ * 512:(s_ + 1) * 512], sbo[st][:], reads=[("sbo", st)], writes=[("o_sb", h, s_)])

            n = len(merged)
            for i in range(n + 2):
                if i < n:
                    s1(merged[i])
                if 1 <= i <= n:
                    s2a(merged[i - 1])
                if i >= 2:
                    s2b(merged[i - 2])

    if "nsa" in branches:
        pending = []
        QS = kb.sb("QS", [128, 4, 4, 512], BF16)
        OV = kb.sb("OV", [128, 4, 129], BF16)
        pm = kb.sb("pm", [128, 3, 512], BF16)
        wm = kb.sb("wm", [128, 12, 512], BF16)
        wphi = kb.sb("wphi", [128, 32, 128], BF16)
        w2 = kb.sb("w2", [128, 2, 64], BF16)
        peT = kb.sb("peT", [128, 32], BF16)
        peb = kb.sb("peb", [128, 2], F32)
        gx = kb.sb("gx", [128, 512], F32)
        gt_ = kb.sb("gtmp", [128, 512], F32)
        gact = [kb.sb("gact", [128, 512], BF16) for _ in range(2)]
        kcT = kb.sb("kcT", [68, 512], BF16)
        Vc = kb.sb("Vc", [128, 4, 65], BF16)
        psave = kb.sb("psave", [128, 4, 4, 512], BF16)
        gts = [kb.sb("gts", [65, 512], F32) for _ in range(4)]
        nacc = kb.sb("nacc", [64, 4, 512], F32)
        impacc = kb.sb("impacc", [128, 4, 128], F32)
        rdn = kb.sb("rdn", [128, 2], F32)
        selm = [kb.sb("selm", [128, 4, 128], F32) for _ in range(2)]
        selc = [kb.sb("selc", [128, 4, 128], F32) for _ in range(2)]
        scr2 = kb.sb("scr2", [128, 4, 128], F32)
        m16 = kb.sb("m16", [128, 4, 16], F32)
        selvp = kb.sb("selvp", [128, 4, 256], F32)
        selv = selvp[:, :, 64:192]
        gcnt = {"g": 0}
        p.pool(lambda e: e.memset(selvp[:], 0.0), writes=["selv"])
        p.dma(OV[:], io["c_ov"], writes=["OV"])
        p.dma(pm[:], io["c_pm"], writes=["pm"])
        p.dma(wm[:], io["c_wm"], writes=["wm"])
        p.dma(wphi[0:64, :, :], w["w_phi_k1"].rearrange("(t d) h -> d t h", d=64), writes=["wphi"], q="pool")
        p.dma(wphi[64:128, :, :], w["w_phi_v1"].rearrange("(t d) h -> d t h", d=64), writes=["wphi"], q="pool")
        p.dma(w2[:, 0, :], w["w_phi_k2"], writes=["w2"], q="pool")
        p.dma(w2[:, 1, :], w["w_phi_v2"], writes=["w2"], q="pool")
        p.dma(peT[0:64, :], w["nsa_pe"].rearrange("t d -> d t"), writes=["peT"], q="pool", slow=True)
        p.dma(peT[64:128, :], w["nsa_pe"].rearrange("t d -> d t"), writes=["peT"], q="pool", slow=True)
        kcv, kcvtok = load_K([(io["kcv_f"].rows(0, 128), 0)], 128)
        p.pool(lambda e: e.memset(kcT[:], 0.0), writes=["kcT"])
        p.pool(lambda e: e.memset(Vc[:, :, 64:65], 1.0), writes=["Vc"])
        for which in range(2):
            lo = which * 64
            pb = kb.ps(7)
            for t in range(32):
                p.pe(lambda e, t=t, lo=lo, pb=pb: e.matmul(pb[:, 0:1], lhsT=wphi[lo:lo + 64, t, :], rhs=peT[lo:lo + 64, t:t + 1], start=(t == 0), stop=(t == 31)),
                     reads=["wphi", "peT"], writes=[("ps", 7)])
            p.dve(lambda e, pb=pb, which=which: e.tensor_copy(out=peb[:, which:which + 1], in_=pb[:, 0:1]), reads=[("ps", 7)], writes=["peb"])
            ph = kb.ps(6)
            for t in range(32):
                p.pe(lambda e, t=t, lo=lo, ph=ph: e.matmul(ph[:, 0:511], lhsT=wphi[lo:lo + 64, t, :], rhs=kcv[lo:lo + 64, t:t + 16 * 510 + 1:16], start=(t == 0), stop=(t == 31)),
                     reads=["wphi", kcvtok], writes=[("ps", 6)])
            ga = gact[which]
            p.act(lambda e, ph=ph, which=which: e.activation(out=gx[:, 0:511], in_=ph[:, 0:511], func=AF.Identity, bias=peb[:, which:which + 1]),
                  reads=[("ps", 6), "peb"], writes=["gx"])
            p.dve(lambda e: e.tensor_tensor(out=gt_[:, 0:511], in0=gx[:, 0:511], in1=gx[:, 0:511], op=ALU.mult), reads=["gx"], writes=["gtmp"])
            p.dve(lambda e: e.tensor_scalar(out=gt_[:, 0:511], in0=gt_[:, 0:511], scalar1=0.044715, scalar2=1.0, op0=ALU.mult, op1=ALU.add), reads=["gtmp"], writes=["gtmp"])
            p.dve(lambda e: e.tensor_tensor(out=gt_[:, 0:511], in0=gt_[:, 0:511], in1=gx[:, 0:511], op=ALU.mult), reads=["gtmp", "gx"], writes=["gtmp"])
            p.act(lambda e: e.activation(out=gt_[:, 0:511], in_=gt_[:, 0:511], func=AF.Tanh, scale=0.7978845608028654), reads=["gtmp"], writes=["gtmp"])
            p.dve(lambda e: e.tensor_scalar(out=gt_[:, 0:511], in0=gt_[:, 0:511], scalar1=1.0, scalar2=0.5, op0=ALU.add, op1=ALU.mult), reads=["gtmp"], writes=["gtmp"])
            p.pool(lambda e, ga=ga: e.memset(ga[:], 0.0), writes=[("gact", which)])
            p.dve(lambda e, ga=ga: e.tensor_tensor(out=ga[:, 0:511], in0=gt_[:, 0:511], in1=gx[:, 0:511], op=ALU.mult), reads=["gtmp", "gx"], writes=[("gact", which)])
        pk_ = kb.ps(7)
        p.pe(lambda e: e.matmul(pk_[0:64, :], lhsT=w2[:, 0, :], rhs=gact[0][:], start=True, stop=True), reads=["w2", ("gact", 0)], writes=[("ps", 7)])
        p.dve(lambda e: e.tensor_copy(out=kcT[0:64, 0:511], in_=pk_[0:64, 0:511]), reads=[("ps", 7)], writes=["kcT"])
        p.dma(kcT[64:68, :], io["c_caug"], writes=["kcT"])
        pv_ = kb.ps(6)
        for cc in range(4):
            p.pe(lambda e, cc=cc: e.matmul(pv_[:, cc * 64:(cc + 1) * 64], lhsT=gact[1][:, cc * 128:(cc + 1) * 128], rhs=w2[:, 1, :], start=True, stop=True),
                 reads=["w2", ("gact", 1)], writes=[("ps", 6)])
        p.dve(lambda e: e.tensor_copy(out=Vc[:, :, 0:64], in_=pv_[:, 0:256].rearrange("p (c d) -> p c d", c=4)), reads=[("ps", 6)], writes=["Vc"])

        KsT, kstok = load_K([(io["ksl_f"].rows(0, 64), 0), (("full", io["c_g32"]), 64)], 96, aug=io["c_kaug"])
        KwT, kwtok = load_K([(io["kwi_f"].rows(0, 64), 0)], 64, aug=io["c_kaug"])
        Vs, vstok = load_V(io["vsw_f"], 0)
        Vw, vwtok = load_V(io["vsw_f"], 64)
        qtoks = [load_Q(io["qns_d"][h * 64:(h + 1) * 64, :], h, 64, aug=io["c_qaug"][h]) for h in range(4)]

        def gate_row(h, br, s):
            k = gcnt["g"] % 4
            gcnt["g"] += 1
            g = gts[k]
            p.dma(g[64:65, :], io["gns_d"][3 * h + br:3 * h + br + 1, s * 512:(s + 1) * 512], writes=[("gts", k)])
            return (g[:].rearrange("p (o n) -> p o n", o=1), ("gts", k), 0)

        for s in range(NSLOT):
            sl = slice(s * 512, (s + 1) * 512)
            ncmp = s // 2 + 1
            dst = lambda h: oT[3, h * 64:(h + 1) * 64, sl]
            p.dma(selm[s % 2][:], io["c_selm"][s], writes=[("selm", s % 2)])
            p.dma(selc[s % 2][:], io["c_selc"][s], writes=[("selc", s % 2)])
            for h in range(4):
                items = []
                for cc in range(ncmp):
                    idx = s - 2 * cc + 6
                    ex = []
                    if idx <= 8:
                        ex.append((c["identb"][:], pm[:, idx - 6, :], ["identb", "pm"]))
                    it = dict(j=cc, s=s, extras=ex, first=(cc == 0), last=(cc == ncmp - 1), obank=4 + (h % 2),
                              pdst=(psave[:, h, cc, :], ("psave", h, cc)))
                    if cc == ncmp - 1:
                        it["fin"] = finalize_plain(4 + (h % 2), dst(h), h, s, gate=gate_row(h, 0, s), acc=nacc[:, h, :], acc_tok=("nacc", h), first=True, last=False)
                    items.append(it)
                run_softmax(items, kcT, "kcT", 68, Vc, "Vc", h, qtoks[h], pending)
            flush(pending)
            for qb in range(4):
                for h in range(4):
                    bk = 6 + ((qb * 4 + h) % 2)
                    pi = kb.ps(bk)
                    for cc in range(ncmp):
                        p.pe(lambda e, pi=pi, h=h, cc=cc, qb=qb: e.matmul(pi[:, 0:129], lhsT=psave[:, h, cc, qb * 128:(qb + 1) * 128], rhs=OV[:, cc, :],
                                                                          start=(cc == 0), stop=(cc == ncmp - 1)),
                             reads=[("psave", h, cc), "OV"], writes=[("ps", bk)])
                    p.dve(lambda e, pi=pi: e.tensor_scalar(out=rdn[:, 0:1], in0=pi[:, 128:129], scalar1=1e-30, scalar2=None, op0=ALU.max),
                          reads=[("ps", bk)], writes=["rdn"])
                    p.dve(lambda e: e.reciprocal(out=rdn[:, 1:2], in_=rdn[:, 0:1]), reads=["rdn"], writes=["rdn"])
                    if h == 0:
                        p.dve(lambda e, pi=pi, qb=qb: e.tensor_scalar(out=impacc[:, qb, :], in0=pi[:, 0:128], scalar1=rdn[:, 1:2], scalar2=None, op0=ALU.mult),
                              reads=[("ps", bk), "rdn"], writes=["impacc"])
                    else:
                        p.dve(lambda e, pi=pi, qb=qb: e.scalar_tensor_tensor(out=impacc[:, qb, :], in0=pi[:, 0:128], scalar=rdn[:, 1:2], in1=impacc[:, qb, :],
                                                                             op0=ALU.mult, op1=ALU.add),
                              reads=[("ps", bk), "rdn", "impacc"], writes=["impacc"])
            sm, sc_ = selm[s % 2], selc[s % 2]
            p.dve(lambda e, sm=sm: e.tensor_tensor(out=impacc[:], in0=impacc[:], in1=sm[:], op=ALU.mult), reads=["impacc", ("selm", s % 2)], writes=["impacc"])
            p.dve(lambda e, sc_=sc_: e.tensor_tensor(out=impacc[:], in0=impacc[:], in1=sc_[:], op=ALU.add), reads=["impacc", ("selc", s % 2)], writes=["impacc"])
            for qb in range(4):
                p.dve(lambda e, qb=qb: e.max(out=m16[:, qb, 0:8], in_=impacc[:, qb, :]), reads=["impacc"], writes=["m16"])
                p.dve(lambda e, qb=qb: e.match_replace(out=scr2[:, qb, :], in_to_replace=m16[:, qb, 0:8], in_values=impacc[:, qb, :], imm_value=-3.0e38),
                      reads=["impacc", "m16"], writes=["scr2"])
                p.dve(lambda e, qb=qb: e.max(out=m16[:, qb, 8:16], in_=scr2[:, qb, :]), reads=["scr2"], writes=["m16"])
                p.dve(lambda e, qb=qb: e.tensor_scalar(out=selv[:, qb, :], in0=impacc[:, qb, :], scalar1=m16[:, qb, 15:16], scalar2=None, op0=ALU.is_ge),
                      reads=["impacc", "m16"], writes=["selv"])
            p.dve(lambda e: e.tensor_scalar(out=scr2[:], in0=impacc[:], scalar1=-5e29, scalar2=None, op0=ALU.is_gt), reads=["impacc"], writes=["scr2"])
            p.dve(lambda e: e.tensor_tensor(out=selv, in0=selv, in1=scr2[:], op=ALU.mult), reads=["selv", "scr2"], writes=["selv"])
            p.dve(lambda e: e.tensor_scalar(out=selv, in0=selv, scalar1=1.0, scalar2=-MASKV, op0=ALU.subtract, op1=ALU.mult), reads=["selv"], writes=["selv"])
            for gi in range((8 * s + 7) // 16 + 1):
                for h in range(4):
                    p.pool(lambda e, gi=gi, h=h: e.tensor_copy(out=QS[0:64, h, gi, :], in_=A.QT[0:64, h, sl]), reads=[qtoks[h]], writes=[("QS", gi)])
                    p.dma(QS[96:100, h, gi, :], io["c_qaug"][h][:, sl], writes=[("QS", gi)])
            for h in range(4):
                items = []
                js = [j for j in range(8 * s - 4, 8 * s + 8) if j >= 0]
                for j in js:
                    jrel = j - 8 * s
                    it = dict(j=j, s=s, extras=[(c["identb"][:], wm[:, jrel + 4, :], ["identb", "wm"])], first=(j == js[0]), last=(j == js[-1]), obank=4 + (h % 2))
                    if j == js[-1]:
                        it["fin"] = finalize_plain(4 + (h % 2), dst(h), h, s, gate=gate_row(h, 2, s), acc=nacc[:, h, :], acc_tok=("nacc", h), first=False, last=False)
                    items.append(it)
                run_softmax(items, KwT, kwtok, 68, Vw, vwtok, h, qtoks[h], pending)
            flush(pending)
            nkb = 8 * s + 8
            ngi = (nkb - 1) // 16 + 1
            for gi in range(ngi):
                pt = kb.ps(7)
                for qb in range(4):
                    p.pe(lambda e, qb=qb, gi=gi, pt=pt: e.transpose(out=pt[:, qb * 128:(qb + 1) * 128], in_=selvp[:, qb, 32 * gi:32 * gi + 128], identity=c["identf"][:]),
                         reads=["selv", "identf"], writes=[("ps", 7)])
                p.act(lambda e, gi=gi, pt=pt: e.copy(out=QS[64:96, 0, gi, :], in_=pt[64:96, :]), reads=[("ps", 7)], writes=[("QS", gi)])
                for h in range(1, 4):
                    p.pool(lambda e, gi=gi, h=h: e.tensor_copy(out=QS[64:96, h, gi, :], in_=QS[64:96, 0, gi, :]), reads=[("QS", gi)], writes=[("QS", gi)])
            for h in range(4):
                items = []
                for j in range(nkb):
                    jj = j - 8 * s
                    ex = []
                    if jj >= 0:
                        ex.append((c["identb"][:], A.cm[:, jj, :], ["identb", "cm"]))
                    it = dict(j=j, s=s, extras=ex, first=(j == 0), last=(j == nkb - 1), obank=4 + (h % 2),
                              qrhs=QS[0:100, h, j // 16, :], qreads=[("QS", j // 16)])
                    if j == nkb - 1:
                        it["fin"] = finalize_plain(4 + (h % 2), dst(h), h, s, gate=gate_row(h, 1, s), acc=nacc[:, h, :], acc_tok=("nacc", h), first=False, last=True)
                    items.append(it)
                run_softmax(items, KsT, kstok, 100, Vs, vstok, h, qtoks[h], pending)
            flush(pending)
    return A


B_WEIGHTS = {"w_mem_kv": [D, 512], "nsa_pe": [32, 64], "w_phi_k1": [2048, 128], "w_phi_k2": [128, 64],
             "w_phi_v1": [2048, 128], "w_phi_v2": [128, 64]}


def build_b(branches):
    kb = KB()
    io = {}
    for n in ("c_ident", "c_ones"):
        io[n] = kb.din(n, [128, 128])
    io["c_cst"] = kb.din("c_cst", [128, 8])
    for n, (shp, dt) in B_CONST_SHAPES.items():
        io[n] = kb.din(n, shp, dt)
    for n, (shp, dt) in B_INS.items():
        io[n] = kb.din(n, shp, dt)
    w = {n: kb.din(n, shp) for n, shp in B_WEIGHTS.items()}
    io["oT_d"] = kb.dout("oT_d", [5, 256, TOK], BF16)
    io["kml_f"] = KFull(io["kml_f"].rearrange("h d t -> (h d) t"))
    for n in ("ksb_f", "kmo_f", "krl_f", "kcv_f", "ksl_f", "kwi_f"):
        io[n] = KFull(io[n])
    for n in ("vsb_f", "vmo_f", "vml_f", "vsw_f"):
        io[n] = VFull(io[n])
    c = load_consts(kb, io)
    phase_b(kb, c, io, w, branches)
    return kb.finish()


def layer_norm_chunk(kb, c, v, vtok, gbc, bbc, out, otok, tmp):
    p = kb.p
    st, mv, sm = tmp["st"], tmp["mv"], tmp["sm"]
    for hf in range(2):
        p.dve(lambda e, hf=hf: e.bn_stats(out=st[:, hf, :], in_=v[:, hf * 512:(hf + 1) * 512]), reads=[vtok], writes=["lnst"])
    p.dve(lambda e: e.bn_aggr(out=mv[:], in_=st[:]), reads=["lnst"], writes=["lnmv"])
    p.act(lambda e: e.activation(out=sm[:, 0:1], in_=mv[:, 1:2], func=AF.Ln, bias=c["eps_ln"]), reads=["lnmv", "cst"], writes=["lnsm"])
    p.act(lambda e: e.activation(out=sm[:, 0:1], in_=sm[:, 0:1], func=AF.Exp, scale=-0.5), reads=["lnsm"], writes=["lnsm"])
    p.dve(lambda e: e.scalar_tensor_tensor(out=sm[:, 1:2], in0=mv[:, 0:1], scalar=-1.0, in1=sm[:, 0:1], op0=ALU.mult, op1=ALU.mult),
          reads=["lnmv", "lnsm"], writes=["lnsm2"])
    p.act(lambda e: e.activation(out=v[:], in_=v[:], func=AF.Identity, scale=sm[:, 0:1], bias=sm[:, 1:2]), reads=[vtok, "lnsm", "lnsm2"], writes=[vtok])
    p.dve(lambda e: e.tensor_tensor(out=v[:], in0=v[:], in1=gbc[:], op=ALU.mult), reads=[vtok, "lngb"], writes=[vtok])
    p.dve(lambda e: e.tensor_tensor(out=out[:], in0=v[:], in1=bbc[:], op=ALU.add), reads=[vtok, "lngb"], writes=[otok])


def phase_c1(kb, c, io, w):
    nc, p = kb.nc, kb.p
    wg = kb.sb("wg", [128, 5, 8, 1024], BF16)
    wbr = kb.sb("wbr", [128, 5, 2, 1024], BF16)
    wout = kb.sb("wout", [128, 8, 1024], BF16)
    bg = kb.sb("bg", [128, 5, 8], F32)
    gbc = kb.sb("gbc", [128, 1024], F32)
    bbc = kb.sb("bbc", [128, 1024], F32)
    wr = kb.sb("wr", [128, 8, 20], F32)
    brow = kb.sb("brow", [1, 20], F32)
    for i in range(5):
        for f in range(8):
            p.dma(wg[:, i, f, :], w["w_gate"][i, f * 128:(f + 1) * 128, :], writes=[("wg", i)], q="pool")
        p.dma(wbr[:, i, :, :], w["w_br"][i].rearrange("(j p) c -> p j c", p=128), writes=["wbr"], q="pool")
    p.dma(wout[:], w["w_out"].rearrange("(f p) c -> p f c", p=128), writes=["wout"], q="pool")
    p.dma(bg[:], w["b_gate"].rearrange("i (c p) -> p i c", p=128), writes=["bg"], slow=True)
    p.dma(gbc[:], w["ln1_g"].partition_broadcast(128), writes=["lngb"])
    p.dma(bbc[:], w["ln1_b"].partition_broadcast(128), writes=["lngb"])
    p.dma(wr[:, :, 0:4], w["w_rg"].rearrange("(f p) g -> p f g", p=128), writes=["wr"], slow=True)
    for g in range(4):
        p.dma(wr[:, :, 4 + 4 * g:8 + 4 * g], w["w_re"][g].rearrange("(f p) e -> p f e", p=128), writes=["wr"], slow=True)
    p.dma(brow[0:1, 0:4], w["b_rg"].rearrange("(o g) -> o g", o=1), writes=["brow"])
    p.dma(brow[0:1, 4:20], w["b_re"].rearrange("(o g) e -> o (g e)", o=1), writes=["brow"])

    hTt = [kb.sb("hTt", [128, 8, 512], BF16) for _ in range(2)]
    oTt = [kb.sb("oTt", [128, 5, 2, 512], BF16)] * 2
    mT = [kb.sb("mT", [128, 8, 512], BF16) for _ in range(2)]
    sg = [kb.sb("sg", [128, 512], F32) for _ in range(2)]
    acc = kb.sb("macc", [128, 512], F32)
    tmpm = kb.sb("tmpm", [128, 512], F32)
    hch = [kb.sb("hch", [128, 1024], F32)] * 2
    vch = [kb.sb("vch", [128, 1024], F32)] * 2
    h1c = [kb.sb("h1c", [128, 1024], F32) for _ in range(2)]
    h1Tf = [kb.sb("h1Tf", [128, 8, 128], F32) for _ in range(2)]
    h1Tb = [kb.sb("h1Tb", [128, 8, 128], BF16) for _ in range(2)]
    lnt = {"st": kb.sb("lnst", [128, 2, 6], F32), "mv": kb.sb("lnmv", [128, 2], F32), "sm": kb.sb("lnsm", [128, 2], F32)}
    lg = kb.sb("lg", [128, 20], F32)
    r1 = kb.sb("r1", [128, 8], F32)
    goh = kb.sb("goh", [128, 4], F32)
    el = kb.sb("el", [128, 4], F32)
    ee = kb.sb("ee", [128, 4], F32)
    ee2 = kb.sb("ee2", [128, 4], F32)
    gd = [kb.sb("gd", [128, 16], F32) for _ in range(2)]
    bk = {"n": 0}

    def nbank():
        b = bk["n"] % 6
        bk["n"] += 1
        return b

    pend = []

    def do_slot(s):
        d2 = s % 2
        tsl = slice(s * 512, (s + 1) * 512)
        ht, ot, mt = hTt[d2], oTt[d2], mT[d2]
        p.dma(ht[:], io["hT_d"].rearrange("(f p) t -> p f t", p=128)[:, :, tsl], writes=[("hTt", d2)])
        for i in range(5):
            p.dma(ot[:, i, :, :], io["oT_d"][i].rearrange("(j p) t -> p j t", p=128)[:, :, tsl], writes=["oTt"])
        for cc in range(8):
            csl = slice(cc * 128, (cc + 1) * 128)
            for i in range(5):
                bgt, bbr = nbank(), nbank()
                pg, pb = kb.ps(bgt), kb.ps(bbr)
                for f in range(8):
                    p.pe(lambda e, pg=pg, i=i, f=f, csl=csl: e.matmul(pg[:], lhsT=wg[:, i, f, csl], rhs=ht[:, f, :], start=(f == 0), stop=(f == 7)),
                         reads=[("wg", i), ("hTt", d2)], writes=[("ps", bgt)])
                for jc in range(2):
                    p.pe(lambda e, pb=pb, i=i, jc=jc, csl=csl: e.matmul(pb[:], lhsT=wbr[:, i, jc, csl], rhs=ot[:, i, jc, :], start=(jc == 0), stop=(jc == 1)),
                         reads=["wbr", "oTt"], writes=[("ps", bbr)])
                sgi = sg[i % 2]
                p.act(lambda e, sgi=sgi, pg=pg, i=i, cc=cc: e.activation(out=sgi[:], in_=pg[:], func=AF.Sigmoid, bias=bg[:, i, cc:cc + 1]),
                      reads=[("ps", bgt), "bg"], writes=[("sg", i % 2)])
                if i == 0:
                    p.dve(lambda e, sgi=sgi, pb=pb: e.tensor_tensor(out=acc[:], in0=sgi[:], in1=pb[:], op=ALU.mult),
                          reads=[("sg", i % 2), ("ps", bbr)], writes=["macc"])
                else:
                    p.dve(lambda e, sgi=sgi, pb=pb: e.tensor_tensor(out=tmpm[:], in0=sgi[:], in1=pb[:], op=ALU.mult),
                          reads=[("sg", i % 2), ("ps", bbr)], writes=["tmpm"])
                    if i < 4:
                        p.dve(lambda e: e.tensor_tensor(out=acc[:], in0=acc[:], in1=tmpm[:], op=ALU.add), reads=["macc", "tmpm"], writes=["macc"])
                    else:
                        p.dve(lambda e, cc=cc: e.tensor_tensor(out=mt[:, cc, :], in0=acc[:], in1=tmpm[:], op=ALU.add), reads=["macc", "tmpm"], writes=[("mT", d2)])
            for _ in range(2 if cc < 4 else 1):
                if pend:
                    pend.pop(0)()
        if "dbg_mt" in io and s == 0:
            p.dma(io["dbg_mt"], mt[:], reads=[("mT", d2)], writes=["dbg_mt"])
        def make_chunk(tc):
            gck = s * 4 + tc
            k2 = gck % 2
            rows = slice(gck * 128, (gck + 1) * 128)
            hc, vc, h1 = hch[k2], vch[k2], h1c[k2]
            tf, tb = h1Tf[k2], h1Tb[k2]
            gdt = gd[k2]
            rb = {}

            def stA():
                p.dma(hc[:], io["h_tok"][rows, :], writes=["hch"])
                for hf in range(2):
                    b = nbank()
                    ps = kb.ps(b)
                    for cc in range(8):
                        p.pe(lambda e, ps=ps, cc=cc, tc=tc, hf=hf: e.matmul(ps[:], lhsT=mt[:, cc, tc * 128:(tc + 1) * 128], rhs=wout[:, cc, hf * 512:(hf + 1) * 512],
                                                                          start=(cc == 0), stop=(cc == 7)),
                             reads=["wout", ("mT", d2)], writes=[("ps", b)])
                    p.dve(lambda e, ps=ps, hf=hf: e.scalar_tensor_tensor(out=vc[:, hf * 512:(hf + 1) * 512], in0=hc[:, hf * 512:(hf + 1) * 512], scalar=ALPHA, in1=ps[:],
                                                                          op0=ALU.mult, op1=ALU.add),
                          reads=["hch", ("ps", b)], writes=["vch"])
                layer_norm_chunk(kb, c, vc, "vch", gbc, bbc, h1, ("h1c", k2), lnt)
                p.dma(io["h1_d"][rows, :], h1[:], reads=[("h1c", k2)], writes=[("h1_d", gck)])

            def stB():
                for hf in range(2):
                    b = nbank()
                    ps = kb.ps(b)
                    for jx in range(4):
                        f = hf * 4 + jx
                        p.pe(lambda e, ps=ps, f=f, jx=jx: e.transpose(out=ps[:, jx * 128:(jx + 1) * 128], in_=h1[:, f * 128:(f + 1) * 128], identity=c["identf"][:]),
                             reads=[("h1c", k2), "identf"], writes=[("ps", b)])
                    p.act(lambda e, ps=ps, hf=hf: e.copy(out=tf[:, hf * 4:hf * 4 + 4, :], in_=ps[:].rearrange("p (j t) -> p j t", j=4)),
                          reads=[("ps", b)], writes=[("h1Tf", k2)])
                p.pool(lambda e: e.tensor_copy(out=tb[:], in_=tf[:]), reads=[("h1Tf", k2)], writes=[("h1Tb", k2)])
                p.dma(io["h1T_d"].rearrange("(f p) t -> p f t", p=128)[:, :, rows], tb[:], reads=[("h1Tb", k2)], writes=[("h1T_d", gck)])
                b = nbank()
                ps = kb.ps(b)
                for f in range(8):
                    p.pe(lambda e, ps=ps, f=f: e.matmul(ps[:, 0:20], lhsT=tf[:, f, :], rhs=wr[:, f, :], start=(f == 0), stop=False),
                         reads=[("h1Tf", k2), "wr"], writes=[("ps", b)])
                p.pe(lambda e, ps=ps: e.matmul(ps[:, 0:20], lhsT=c["onesf"][0:1, :], rhs=brow[0:1, :], start=False, stop=True),
                     reads=["onesf", "brow"], writes=[("ps", b)])
                p.dve(lambda e, ps=ps: e.tensor_copy(out=lg[:], in_=ps[:, 0:20]), reads=[("ps", b)], writes=["lg"])

            def stC():
                p.dve(lambda e: e.tensor_reduce(out=r1[:, 0:1], in_=lg[:, 0:4], axis=AX.X, op=ALU.max), reads=["lg"], writes=["r1a"])
                p.dve(lambda e: e.tensor_scalar(out=goh[:], in0=lg[:, 0:4], scalar1=r1[:, 0:1], scalar2=None, op0=ALU.is_equal), reads=["lg", "r1a"], writes=["goh"])
                p.dve(lambda e: e.tensor_scalar(out=ee[:], in0=lg[:, 0:4], scalar1=r1[:, 0:1], scalar2=None, op0=ALU.subtract), reads=["lg", "r1a"], writes=["ee"])
                p.act(lambda e: e.activation(out=ee[:], in_=ee[:], func=AF.Exp), reads=["ee"], writes=["ee"])
                p.dve(lambda e: e.tensor_reduce(out=r1[:, 1:2], in_=ee[:], axis=AX.X, op=ALU.add), reads=["ee"], writes=["r1b"])
                p.dve(lambda e: e.reciprocal(out=r1[:, 1:2], in_=r1[:, 1:2]), reads=["r1b"], writes=["r1b"])
                p.dve(lambda e: e.tensor_scalar(out=el[:], in0=lg[:, 4:8], scalar1=goh[:, 0:1], scalar2=None, op0=ALU.mult), reads=["lg", "goh"], writes=["el"])
                for g in range(1, 4):
                    p.dve(lambda e, g=g: e.scalar_tensor_tensor(out=el[:], in0=lg[:, 4 + 4 * g:8 + 4 * g], scalar=goh[:, g:g + 1], in1=el[:], op0=ALU.mult, op1=ALU.add),
                          reads=["lg", "goh", "el"], writes=["el"])
                p.dve(lambda e: e.tensor_reduce(out=r1[:, 2:3], in_=el[:], axis=AX.X, op=ALU.max), reads=["el"], writes=["r1c"])
                p.dve(lambda e: e.tensor_scalar(out=ee[:], in0=el[:], scalar1=r1[:, 2:3], scalar2=None, op0=ALU.subtract), reads=["el", "r1c"], writes=["ee"])
                p.act(lambda e: e.activation(out=ee[:], in_=ee[:], func=AF.Exp), reads=["ee"], writes=["ee"])
                p.dve(lambda e: e.tensor_scalar(out=ee2[:], in0=ee[:], scalar1=1.0, scalar2=-2.0, op0=ALU.is_ge, op1=ALU.mult), reads=["ee"], writes=["ee2"])
                p.dve(lambda e: e.tensor_tensor(out=ee2[:], in0=ee2[:], in1=ee[:], op=ALU.add), reads=["ee2", "ee"], writes=["ee2"])
                p.dve(lambda e: e.tensor_reduce(out=r1[:, 3:4], in_=ee2[:], axis=AX.X, op=ALU.max), reads=["ee2"], writes=["r1d"])
                p.dve(lambda e: e.tensor_scalar(out=ee2[:], in0=ee[:], scalar1=r1[:, 3:4], scalar2=None, op0=ALU.is_ge), reads=["ee", "r1d"], writes=["ee2"])
                p.dve(lambda e: e.tensor_tensor(out=ee[:], in0=ee[:], in1=ee2[:], op=ALU.mult), reads=["ee", "ee2"], writes=["ee"])
                p.dve(lambda e: e.tensor_scalar(out=r1[:, 4:5], in0=r1[:, 3:4], scalar1=1.0, scalar2=None, op0=ALU.add), reads=["r1d"], writes=["r1e"])
                p.dve(lambda e: e.reciprocal(out=r1[:, 4:5], in_=r1[:, 4:5]), reads=["r1e"], writes=["r1e"])
                p.dve(lambda e: e.tensor_tensor(out=r1[:, 4:5], in0=r1[:, 4:5], in1=r1[:, 1:2], op=ALU.mult), reads=["r1e", "r1b"], writes=["r1e"])
                p.dve(lambda e: e.tensor_scalar(out=ee[:], in0=ee[:], scalar1=r1[:, 4:5], scalar2=None, op0=ALU.mult), reads=["ee", "r1e"], writes=["ee"])
                for g in range(4):
                    p.dve(lambda e, g=g: e.tensor_scalar(out=gdt[:, 4 * g:4 * g + 4], in0=ee[:], scalar1=goh[:, g:g + 1], scalar2=None, op0=ALU.mult),
                          reads=["ee", "goh"], writes=[("gd", k2)])
                p.dma(io["gd_d"][rows, :], gdt[:], reads=[("gd", k2)], writes=[("gd_d", gck)])
            return stA, stB, stC

        for tc in range(4):
            pend.extend(make_chunk(tc))

    for s in range(NSLOT):
        do_slot(s)
    while pend:
        pend.pop(0)()

C1_W = {"w_br": [5, 256, D], "w_gate": [5, D, D], "b_gate": [5, D], "w_out": [D, D], "ln1_g": [D], "ln1_b": [D],
        "w_rg": [D, 4], "b_rg": [4], "w_re": [4, D, 4], "b_re": [4, 4]}


def build_c1(dbg=False):
    kb = KB()
    io = {}
    if dbg:
        io["dbg_mt"] = kb.dout("dbg_mt", [128, 8, 512], BF16)
    for n in ("c_ident", "c_ones"):
        io[n] = kb.din(n, [128, 128])
    io["c_cst"] = kb.din("c_cst", [128, 8])
    io["h_tok"] = kb.din("h_tok", [TOK, D])
    io["hT_d"] = kb.din("hT_d", [D, TOK], BF16)
    io["oT_d"] = kb.din("oT_d", [5, 256, TOK], BF16)
    w = {n: kb.din(n, shp) for n, shp in C1_W.items()}
    io["h1_d"] = kb.dout("h1_d", [TOK, D])
    io["h1T_d"] = kb.dout("h1T_d", [D, TOK], BF16)
    io["gd_d"] = kb.dout("gd_d", [TOK, 16])
    c = load_consts(kb, io)
    phase_c1(kb, c, io, w)
    return kb.finish()


def phase_c2(kb, c, io, w, nT=4, nE=16):
    nc, p = kb.nc, kb.p
    gbc = kb.sb("gbc2", [128, 1024], F32)
    bbc = kb.sb("bbc2", [128, 1024], F32)
    p.dma(gbc[:], w["ln2_g"].partition_broadcast(128), writes=["lngb"])
    p.dma(bbc[:], w["ln2_b"].partition_broadcast(128), writes=["lngb"])
    hT = [kb.sb("h1Tt", [128, 8, 1024], BF16) for _ in range(2)]
    gdt = [kb.sb("gdt", [128, 8, 16], F32) for _ in range(2)]
    wup = [kb.sb("wup", [128, 8, 512], BF16) for _ in range(3)]
    wdn = [kb.sb("wdn", [128, 2, 1024], BF16) for _ in range(3)]
    yacc = kb.sb("yacc", [128, 8, 1024], F32)
    gT = [kb.sb("gT", [128, 2, 1024], BF16) for _ in range(2)]
    sa = [kb.sb("sa", [128, 512], F32) for _ in range(2)]
    hch = [kb.sb("h1ch", [128, 1024], F32) for _ in range(2)]
    och = [kb.sb("och", [128, 1024], F32) for _ in range(2)]
    lnt = {"st": kb.sb("lnst2", [128, 2, 6], F32), "mv": kb.sb("lnmv2", [128, 2], F32), "sm": kb.sb("lnsm2", [128, 2], F32)}
    st = {"bank": 0, "w": 0, "sa": 0}

    def nbank():
        b = st["bank"] % 8
        st["bank"] += 1
        return b

    def make_expert(T, e, ht, httok, gd, gdtok):
        k = st["w"] % 3
        kg = st["w"] % 2
        st["w"] += 1
        wu, wd, g = wup[k], wdn[k], gT[kg]
        p.dma(wu[:], w["w_up"][e].rearrange("(f p) c -> p f c", p=128), writes=[("wup", k)], q="pool")
        p.dma(wd[:], w["w_down"][e].rearrange("(j p) c -> p j c", p=128), writes=[("wdn", k)], q="pool")
        ups, downs = [], []

        def make_up(ts, jc):
            def up():
                tsl = slice(ts * 512, (ts + 1) * 512)
                ba, bu = nbank(), nbank()
                pa, pu = kb.ps(ba), kb.ps(bu)
                for f in range(8):
                    p.pe(lambda e_, f=f: e_.matmul(pa[:], lhsT=wu[:, f, jc * 128:(jc + 1) * 128], rhs=ht[:, f, tsl], start=(f == 0), stop=(f == 7)),
                         reads=[("wup", k), httok], writes=[("ps", ba)])
                for f in range(8):
                    p.pe(lambda e_, f=f: e_.matmul(pu[:], lhsT=wu[:, f, 256 + jc * 128:256 + (jc + 1) * 128], rhs=ht[:, f, tsl], start=(f == 0), stop=(f == 7)),
                         reads=[("wup", k), httok], writes=[("ps", bu)])
                si = st["sa"] % 2
                st["sa"] += 1
                sat = sa[si]
                p.act(lambda e_: e_.activation(out=sat[:], in_=pa[:], func=AF.Silu), reads=[("ps", ba)], writes=[("sa", si)])
                p.dve(lambda e_: e_.tensor_tensor(out=g[:, jc, tsl], in0=sat[:], in1=pu[:], op=ALU.mult),
                      reads=[("sa", si), ("ps", bu)], writes=[("gT", kg)])
            return up

        def make_down(tc, hf):
            def down():
                b = nbank()
                ps = kb.ps(b)
                for jc in range(2):
                    p.pe(lambda e_, jc=jc: e_.matmul(ps[:], lhsT=g[:, jc, tc * 128:(tc + 1) * 128], rhs=wd[:, jc, hf * 512:(hf + 1) * 512],
                                                       start=(jc == 0), stop=(jc == 1)),
                         reads=[("gT", kg), ("wdn", k)], writes=[("ps", b)])
                ya = yacc[:, tc, hf * 512:(hf + 1) * 512]
                if e == 0:
                    p.dve(lambda e_: e_.tensor_scalar(out=ya, in0=ps[:], scalar1=gd[:, tc, e:e + 1], scalar2=None, op0=ALU.mult),
                          reads=[("ps", b), gdtok], writes=[("yacc", tc)])
                else:
                    p.dve(lambda e_: e_.scalar_tensor_tensor(out=ya, in0=ps[:], scalar=gd[:, tc, e:e + 1], in1=ya, op0=ALU.mult, op1=ALU.add),
                          reads=[("ps", b), gdtok, ("yacc", tc)], writes=[("yacc", tc)])
            return down

        for ts in range(2):
            for jc in range(2):
                ups.append(make_up(ts, jc))
        for tc in range(8):
            for hf in range(2):
                downs.append(make_down(tc, hf))
        return ups, downs

    def do_chunk_out(T, tc):
        gck = T * 8 + tc
        k2 = gck % 2
        rows = slice(gck * 128, (gck + 1) * 128)
        hc, oc = hch[k2], och[k2]
        p.dma(hc[:], io["h1_d"][rows, :], writes=[("h1ch", k2)])
        p.dve(lambda e_: e_.scalar_tensor_tensor(out=hc[:], in0=hc[:], scalar=ALPHA, in1=yacc[:, tc, :], op0=ALU.mult, op1=ALU.add),
              reads=[("h1ch", k2), ("yacc", tc)], writes=[("h1ch", k2)])
        layer_norm_chunk(kb, c, hc, ("h1ch", k2), gbc, bbc, oc, ("och", k2), lnt)
        p.dma(io["h_out"][rows, :], oc[:], reads=[("och", k2)], writes=[("h_out", gck)])

    def do_tile(T):
        d2 = T % 2
        ht, gd = hT[d2], gdt[d2]
        cols = slice(T * 1024, (T + 1) * 1024)
        p.dma(ht[:], io["h1T_d"].rearrange("(f p) t -> p f t", p=128)[:, :, cols], writes=[("h1Tt", d2)])
        p.dma(gd[:], io["gd_d"][T * 1024:(T + 1) * 1024, :].rearrange("(n p) e -> p n e", p=128), writes=[("gdt", d2)])
        prev_down = []
        for e in range(nE):
            ups, downs = make_expert(T, e, ht, ("h1Tt", d2), gd, ("gdt", d2))
            for i, u in enumerate(ups):
                u()
                for dn in prev_down[4 * i:4 * i + 4]:
                    dn()
            prev_down = downs
        for dn in prev_down:
            dn()
        for tc in range(8):
            do_chunk_out(T, tc)

    for T in range(nT):
        do_tile(T)


C2_W = {"w_up": [16, D, 512], "w_down": [16, 256, D], "ln2_g": [D], "ln2_b": [D]}


def build_c2(nT=4, nE=16):
    kb = KB()
    io = {}
    for n in ("c_ident", "c_ones"):
        io[n] = kb.din(n, [128, 128])
    io["c_cst"] = kb.din("c_cst", [128, 8])
    io["h1_d"] = kb.din("h1_d", [TOK, D])
    io["h1T_d"] = kb.din("h1T_d", [D, TOK], BF16)
    io["gd_d"] = kb.din("gd_d", [TOK, 16])
    w = {n: kb.din(n, shp) for n, shp in C2_W.items()}
    io["h_out"] = kb.dout("h_out", [TOK, D])
    c = load_consts(kb, io)
    phase_c2(kb, c, io, w, nT, nE)
    return kb.finish()


W_SHAPES = {
    "w_in": [D, IN_TOTAL], "g_cq": [256], "g_ckv": [128], "w_uq": [256, 384], "w_ukv": [128, 512],
    "nsa_pe": [32, 64], "w_phi_k1": [2048, 128], "w_phi_k2": [128, 64], "w_phi_v1": [2048, 128], "w_phi_v2": [128, 64],
    "w_mem_kv": [D, 512], "w_br": [5, 256, D], "w_gate": [5, D, D], "b_gate": [5, D], "w_out": [D, D],
    "ln1_g": [D], "ln1_b": [D], "w_rg": [D, 4], "b_rg": [4], "w_re": [4, D, 4], "b_re": [4, 4],
    "w_up": [16, D, 512], "w_down": [16, 256, D], "ln2_g": [D], "ln2_b": [D],
}
PAIRS = [[0, 1], [2, 3], [4, 5], [6, 7]]


def build_fused(depth=DEPTH, nlw=DEPTH, stop=None):
    kb = KB()
    io = {}
    for n in ("c_ident", "c_ones"):
        io[n] = kb.din(n, [128, 128])
    io["c_cst"] = kb.din("c_cst", [128, 8])
    for n, (shp, dt) in B_CONST_SHAPES.items():
        io[n] = kb.din(n, shp, dt)
    io["ropeq_t"] = kb.din("ropeq_t", [NSLOT, 96, 2, 512])
    io["ropek_t"] = kb.din("ropek_t", [NSLOT, 32, 2, 512])
    io["x_own"] = kb.din("x_own", [TOK, D])
    io["mem"] = kb.din("mem", [256, D])
    wfull = {n: kb.din(n, [nlw] + shp) for n, shp in W_SHAPES.items()}
    io["h_final"] = kb.dout("h_final", [TOK, D])
    hbuf = [kb.dscratch(f"hbuf{i}", [TOK, D]) for i in range(2)]
    io["hT_d"] = kb.dscratch("hT_d", [D, TOK], BF16)
    for n in ("qsb_d", "qmo_d", "qns_d", "qme_d"):
        io[n] = kb.dscratch(n, [256, TOK], BF16)
    io["qml_d"] = kb.dscratch("qml_d", [4, 96, TOK], BF16)
    io["gns_d"] = kb.dscratch("gns_d", [12, TOK])
    io["oT_d"] = kb.dscratch("oT_d", [5, 256, TOK], BF16)
    io["h1_d"] = kb.dscratch("h1_d", [TOK, D])
    io["h1T_d"] = kb.dscratch("h1T_d", [D, TOK], BF16)
    io["gd_d"] = kb.dscratch("gd_d", [TOK, 16])
    xk_rows = [256, 256, 256, 256, 64]
    xk_in = [kb.dscratch(f"xk_in{k}", [r, TOK], BF16) for k, r in enumerate(xk_rows)]
    xk_out = [kb.dscratch(f"xk_out{k}", [2 * r, TOK], BF16) for k, r in enumerate(xk_rows)]
    xv_in = [kb.dscratch(f"xv_in{k}", [1024, 896], BF16) for k in range(4)]
    xv_out = [kb.dscratch(f"xv_out{k}", [2048, 896], BF16) for k in range(4)]
    io["ksb_d"], io["kmo_d"] = xk_in[0], xk_in[1]
    io["kml_d"] = xk_in[2].rearrange("(h d) t -> h d t", h=4)
    io["krl_d"], io["kcv_d"], io["ksl_d"] = xk_in[3][0:32, :], xk_in[3][32:160, :], xk_in[3][160:224, :]
    io["kwi_d"] = xk_in[4]
    io["vsb_d"], io["vmo_d"] = TMChunks(xv_in, 0, 256), TMChunks(xv_in, 256, 512)
    io["vml_d"], io["vsw_d"] = TMChunks(xv_in, 512, 768), TMChunks(xv_in, 768, 896)
    io["ksb_f"], io["kmo_f"], io["kml_f"] = KPair(xk_out[0], 256), KPair(xk_out[1], 256), KPair(xk_out[2], 256)
    io["krl_f"], io["kcv_f"], io["ksl_f"] = KPair(xk_out[3], 256, 0), KPair(xk_out[3], 256, 32), KPair(xk_out[3], 256, 160)
    io["kwi_f"] = KPair(xk_out[4], 64)
    io["vsb_f"], io["vmo_f"] = VPair(xv_out, 0), VPair(xv_out, 256)
    io["vml_f"], io["vsw_f"] = VPair(xv_out, 512), VPair(xv_out, 768)

    for l in range(depth):
        w = {n: ap[l] for n, ap in wfull.items()}
        io["h_tok"] = io["x_own"] if l == 0 else hbuf[(l - 1) % 2]
        io["h_out"] = io["h_final"] if l == depth - 1 else hbuf[l % 2]
        c = load_consts(kb, io)
        phase_a(kb, c, io, w)
        kb.end_phase()
        if stop == "A":
            break
        c = load_consts(kb, io)
        phase_b(kb, c, io, w, ("mem",))
        for k in range(5):
            kb.p.allgather(xk_out[k], xk_in[k], PAIRS, writes=[("xk", k)])
        for k in range(4):
            kb.p.allgather(xv_out[k], xv_in[k], PAIRS, writes=[("xv", k)])
        kb.end_phase()
        if stop == "AG":
            break
        c = load_consts(kb, io)
        phase_b(kb, c, io, w, ("sb", "moba", "mla"))
        kb.end_phase()
        if stop == "B1":
            break
        c = load_consts(kb, io)
        phase_b(kb, c, io, w, ("nsa",))
        kb.end_phase()
        if stop == "B2":
            break
        c = load_consts(kb, io)
        phase_c1(kb, c, io, w)
        kb.end_phase()
        if stop == "C1":
            break
        c = load_consts(kb, io)
        phase_c2(kb, c, io, w)
        kb.end_phase()
    return kb.finish()


def fused_inputs(inp, c):
    b, r = c // 2, c % 2
    m = dict(host_consts())
    m.update(bconsts_host(r))
    m["ropeq_t"], m["ropek_t"] = rope_tables(r)
    m["x_own"] = np.ascontiguousarray(inp["x"][b][_own_tokens(r)])
    m["mem"] = inp["mem"][b]
    for n in W_SHAPES:
        m[n] = inp[n]
    return m


_PROGS = {}


def _prog(name, fn):
    if name not in _PROGS:
        _PROGS[name] = fn()
    return _PROGS[name]


def _own_tokens(r):
    t = np.arange(TOK)
    return t // 512 * 1024 + r * 512 + t % 512


def _interleave_fm(a0, a1):
    sh = a0.shape[:-1]
    out = np.empty(sh + (S,), a0.dtype)
    o = out.reshape(sh + (8, 2, 512))
    o[..., 0, :] = a0.reshape(sh + (8, 512))
    o[..., 1, :] = a1.reshape(sh + (8, 512))
    return out


def _interleave_tm(a0, a1):
    C = a0.shape[1]
    out = np.empty((S, C), a0.dtype)
    o = out.reshape(8, 2, 512, C)
    o[:, 0] = a0.reshape(8, 512, C)
    o[:, 1] = a1.reshape(8, 512, C)
    return out


A_WEIGHTS = ("w_in", "w_uq", "g_cq", "w_ukv", "g_ckv")
K_FM = {"ksb_d": "ksb_f", "kmo_d": "kmo_f", "kml_d": "kml_f", "krl_d": "krl_f", "kcv_d": "kcv_f", "ksl_d": "ksl_f", "kwi_d": "kwi_f"}
V_TM = {"vsb_d": "vsb_f", "vmo_d": "vmo_f", "vml_d": "vml_f", "vsw_d": "vsw_f"}
Q_OWN = ("qsb_d", "qmo_d", "qml_d", "qns_d", "qme_d", "gns_d")


def kernel_unfused(**inputs):
    inp = {k: np.ascontiguousarray(np.asarray(v)) for k, v in inputs.items()}
    ncores = 8
    cores = list(range(ncores))
    hc = host_consts()
    bc = [bconsts_host(r) for r in range(2)]
    rp = [rope_tables(r) for r in range(2)]
    own = [_own_tokens(r) for r in range(2)]
    h = [np.ascontiguousarray(inp["x"][c // 2][own[c % 2]]) for c in cores]
    nca = _prog("a", build_a)
    ncb1 = _prog("b1", lambda: build_b(("sb", "moba", "mla", "mem")))
    ncb2 = _prog("b2", lambda: build_b(("nsa",)))
    ncc1 = _prog("c1", build_c1)
    ncc2 = _prog("c2", build_c2)
    for l in range(DEPTH):
        maps = []
        for c in cores:
            m = dict(hc)
            m["h_tok"] = h[c]
            m["ropeq_t"], m["ropek_t"] = rp[c % 2]
            for n in A_WEIGHTS:
                m[n] = inp[n][l]
            maps.append(m)
        ra = run_bass_kernel_spmd(nca, maps, core_ids=cores).results
        maps = []
        for c in cores:
            b, r = c // 2, c % 2
            m = dict(hc)
            m.update(bc[r])
            for n in Q_OWN:
                m[n] = ra[c][n]
            for kd, kf in K_FM.items():
                m[kf] = _interleave_fm(np.asarray(ra[2 * b][kd]), np.asarray(ra[2 * b + 1][kd]))
            for vd, vf in V_TM.items():
                m[vf] = _interleave_tm(np.asarray(ra[2 * b][vd]), np.asarray(ra[2 * b + 1][vd]))
            m["mem"] = inp["mem"][b]
            for n in B_WEIGHTS:
                m[n] = inp[n][l]
            maps.append(m)
        rb1 = run_bass_kernel_spmd(ncb1, maps, core_ids=cores).results
        rb2 = run_bass_kernel_spmd(ncb2, maps, core_ids=cores).results
        rb = []
        for c in cores:
            o = np.array(rb1[c]["oT_d"])
            o[3] = np.asarray(rb2[c]["oT_d"])[3]
            rb.append({"oT_d": o})
        maps = []
        for c in cores:
            m = dict(hc)
            m["h_tok"] = h[c]
            m["hT_d"] = ra[c]["hT_d"]
            m["oT_d"] = rb[c]["oT_d"]
            for n in C1_W:
                m[n] = inp[n][l]
            maps.append(m)
        rc1 = run_bass_kernel_spmd(ncc1, maps, core_ids=cores).results
        maps = []
        for c in cores:
            m = dict(hc)
            for n in ("h1_d", "h1T_d", "gd_d"):
                m[n] = rc1[c][n]
            for n in C2_W:
                m[n] = inp[n][l]
            maps.append(m)
        rc2 = run_bass_kernel_spmd(ncc2, maps, core_ids=cores).results
        h = [np.asarray(rc2[c]["h_out"]) for c in cores]
    out = np.empty((NB, S, D), np.float32)
    for c in cores:
        out[c // 2][own[c % 2]] = h[c]
    return out


def kernel(**inputs):
    inp = {k: np.ascontiguousarray(np.asarray(v)) for k, v in inputs.items()}
    cores = list(range(8))
    nc = _prog("fused", build_fused)
    maps = [fused_inputs(inp, c) for c in cores]
    res = run_bass_kernel_spmd(nc, maps, core_ids=cores).results
    out = np.empty((NB, S, D), np.float32)
    for c in cores:
        out[c // 2][_own_tokens(c % 2)] = np.asarray(res[c]["h_final"])
    return out
```
